# Optimizing a Trainium2 kernel written in Bass

```python
import math
import jax, jax.numpy as jnp
from jax import lax
import numpy as np

D_MODEL = 1024
BATCH = 4
SEQ = 4096
DEPTH = 1

ATTN_HEADS = 4
QK_DIM = 64
V_DIM = 2 * QK_DIM
ATTN_WIDTH = ATTN_HEADS * V_DIM
ROPE_DIM = QK_DIM // 4
ROPE_THETA = 500000.0
Q_BLOCK = 128
HYENA_WIDTH = D_MODEL // 2
HYENA_ORDER = 2
FILTER_BANDS = 16
FILTER_EMB = 2 * FILTER_BANDS + 1
FILTER_HIDDEN = 64
DECAY_TARGET = 1e-2
FAST_DECAY_PCT = 0.3
SLOW_DECAY_PCT = 1.5
Q_COLS = ATTN_HEADS * 2 * QK_DIM
K_COLS = ATTN_HEADS * 2 * QK_DIM
V_COLS = ATTN_WIDTH
HY_COLS = (HYENA_ORDER + 1) * HYENA_WIDTH
GATE_COLS = 2 * D_MODEL
IN_COLS = Q_COLS + K_COLS + V_COLS + HY_COLS + GATE_COLS
N_EXPERTS = 16
CAPACITY_FACTOR = 2
EXPERT_HIDDEN = 1024
EPS = 1e-6

kernel_name = "hybrid_diffattn_hyena_ecmoe_block"


def rms_norm(x, g):
    xf = x.astype(jnp.float32)
    y = xf * lax.rsqrt(jnp.mean(xf * xf, axis=-1, keepdims=True) + EPS)
    return (y * g.astype(jnp.float32)).astype(x.dtype)


def rope_partial(t, cos, sin):
    half = ROPE_DIM // 2
    r1 = t[..., :half]
    r2 = t[..., half:ROPE_DIM]
    rot = jnp.concatenate([r1 * cos - r2 * sin, r2 * cos + r1 * sin], axis=-1)
    return jnp.concatenate([rot.astype(t.dtype), t[..., ROPE_DIM:]], axis=-1)


def diff_attention(q, k, v, lam, subln_g, lambda_init):
    b, h, _, l, dk = q.shape
    nb = l // Q_BLOCK
    scale = 1.0 / math.sqrt(dk)
    qb = q.reshape(b, h, 2, nb, Q_BLOCK, dk).transpose(3, 0, 1, 2, 4, 5)

    def block(qi):
        s = jnp.einsum('bhmqd,bhmkd->bhmqk', qi, k).astype(jnp.float32) * scale
        pr = jax.nn.softmax(s, axis=-1)
        a = pr[:, :, 0] - lam * pr[:, :, 1]
        return jnp.einsum('bhqk,bhkd->bhqd', a.astype(v.dtype), v)

    o = lax.map(block, qb)
    o = o.transpose(1, 2, 0, 3, 4).reshape(b, h, l, V_DIM)
    o = rms_norm(o, subln_g) * (1.0 - lambda_init)
    return o.transpose(0, 2, 1, 3).reshape(b, l, h * V_DIM)


def hyena_filter_spectra(l, w1, b1, w2, b2, w3, b3, w_out, freq):
    f32 = jnp.float32
    pos = jnp.arange(l, dtype=f32)
    t = pos / max(l - 1, 1)
    bands = jnp.linspace(1e-4, FILTER_BANDS - 1, FILTER_BANDS, dtype=f32)
    ang = (2.0 * math.pi / l) * pos[:, None] * bands[None, :]
    emb = jnp.concatenate([t[:, None], jnp.cos(ang), -jnp.sin(ang)], axis=-1)
    fr = freq.astype(f32)
    hid = jnp.sin(fr * (emb @ w1.astype(f32) + b1.astype(f32)))
    hid = jnp.sin(fr * (hid @ w2.astype(f32) + b2.astype(f32)))
    hid = jnp.sin(fr * (hid @ w3.astype(f32) + b3.astype(f32)))
    filt = (hid @ w_out.astype(f32)).reshape(l, HYENA_ORDER, 2, HYENA_WIDTH)
    min_decay = math.log(DECAY_TARGET) / SLOW_DECAY_PCT
    max_decay = math.log(DECAY_TARGET) / FAST_DECAY_PCT
    deltas = jnp.abs(jnp.linspace(min_decay, max_decay, HYENA_WIDTH, dtype=f32))
    filt = filt * jnp.exp(-t[:, None] * deltas[None, :])[:, None, None, :]
    kern = jnp.concatenate([filt[:, :, 0],
                            jnp.zeros((1, HYENA_ORDER, HYENA_WIDTH), f32),
                            filt[:0:-1, :, 1]], axis=0)
    return jnp.fft.rfft(kern, axis=0)


def long_conv(z, spec):
    l = z.shape[1]
    zf = jnp.fft.rfft(z.astype(jnp.float32), n=2 * l, axis=1)
    y = jnp.fft.irfft(zf * spec[None], n=2 * l, axis=1)[:, :l]
    return y.astype(z.dtype)


def short_conv(z, w, b):
    zp = jnp.pad(z, ((0, 0), (1, 1), (0, 0)))
    return zp[:, :-2] * w[0] + zp[:, 1:-1] * w[1] + zp[:, 2:] * w[2] + b


def hyena_mixer(hy, conv_w, conv_b, spec, skip):
    hy = short_conv(hy, conv_w, conv_b)
    z, g1, g2 = jnp.split(hy, HYENA_ORDER + 1, axis=-1)
    gates = (g1, g2)
    for n in range(HYENA_ORDER):
        z = gates[n] * (long_conv(z, spec[:, n]) + skip[n] * z)
    return z


def expert_choice_ffn(u, w_router, w_gate, w_up, w_down):
    b, l, d = u.shape
    cap = CAPACITY_FACTOR * l // N_EXPERTS
    logits = jnp.einsum('bld,de->ble', u, w_router).astype(jnp.float32)
    aff = jax.nn.softmax(logits, axis=-1)
    g, idx = lax.top_k(aff.transpose(0, 2, 1), cap)
    xin = jax.vmap(lambda ub, ib: ub[ib])(u, idx)
    hg = jnp.einsum('becd,edf->becf', xin, w_gate)
    hu = jnp.einsum('becd,edf->becf', xin, w_up)
    eo = jnp.einsum('becf,efd->becd', jax.nn.silu(hg) * hu, w_down)
    weighted = eo * g[..., None].astype(eo.dtype)
    return jax.vmap(lambda ib, wb: jnp.zeros((l, d), wb.dtype).at[ib.reshape(-1)].add(wb.reshape(-1, d)))(idx, weighted)


def setup_inputs(seed: int = 0) -> dict:
    key = jax.random.key(seed)
    ks = iter(jax.random.split(key, 32))

    def nrm(shape, std):
        return jax.random.normal(next(ks), shape, jnp.float32) * std

    def gain(shape):
        return 1.0 + nrm(shape, 0.02)

    L_ = DEPTH
    return {
        "x": nrm((BATCH, SEQ, D_MODEL), 1.0),
        "norm1_g": gain((L_, D_MODEL)),
        "w_in": nrm((L_, D_MODEL, IN_COLS), D_MODEL ** -0.5),
        "short_conv_w": nrm((L_, 3, HY_COLS), 3 ** -0.5),
        "short_conv_b": nrm((L_, HY_COLS), 0.01),
        "q_norm_g": gain((L_, QK_DIM)),
        "k_norm_g": gain((L_, QK_DIM)),
        "lambda_q1": nrm((L_, QK_DIM), 0.1),
        "lambda_k1": nrm((L_, QK_DIM), 0.1),
        "lambda_q2": nrm((L_, QK_DIM), 0.1),
        "lambda_k2": nrm((L_, QK_DIM), 0.1),
        "subln_g": gain((L_, V_DIM)),
        "filt_w1": nrm((L_, FILTER_EMB, FILTER_HIDDEN), FILTER_EMB ** -0.5),
        "filt_b1": nrm((L_, FILTER_HIDDEN), 0.01),
        "filt_w2": nrm((L_, FILTER_HIDDEN, FILTER_HIDDEN), FILTER_HIDDEN ** -0.5),
        "filt_b2": nrm((L_, FILTER_HIDDEN), 0.01),
        "filt_w3": nrm((L_, FILTER_HIDDEN, FILTER_HIDDEN), FILTER_HIDDEN ** -0.5),
        "filt_b3": nrm((L_, FILTER_HIDDEN), 0.01),
        "filt_w_out": nrm((L_, FILTER_HIDDEN, HYENA_ORDER * 2 * HYENA_WIDTH), 0.003),
        "filt_freq": 1.0 + nrm((L_, FILTER_HIDDEN), 0.1),
        "hyena_skip": nrm((L_, HYENA_ORDER, HYENA_WIDTH), 0.5),
        "w_branch_attn": nrm((L_, ATTN_WIDTH, D_MODEL), ATTN_WIDTH ** -0.5),
        "w_branch_hyena": nrm((L_, HYENA_WIDTH, D_MODEL), HYENA_WIDTH ** -0.5),
        "w_out": nrm((L_, D_MODEL, D_MODEL), D_MODEL ** -0.5),
        "norm2_g": gain((L_, D_MODEL)),
        "w_router": nrm((L_, D_MODEL, N_EXPERTS), D_MODEL ** -0.5),
        "w_gate": nrm((L_, N_EXPERTS, D_MODEL, EXPERT_HIDDEN), D_MODEL ** -0.5),
        "w_up": nrm((L_, N_EXPERTS, D_MODEL, EXPERT_HIDDEN), D_MODEL ** -0.5),
        "w_down": nrm((L_, N_EXPERTS, EXPERT_HIDDEN, D_MODEL), EXPERT_HIDDEN ** -0.5),
    }


def reference(x, norm1_g, w_in, short_conv_w, short_conv_b, q_norm_g, k_norm_g,
              lambda_q1, lambda_k1, lambda_q2, lambda_k2, subln_g,
              filt_w1, filt_b1, filt_w2, filt_b2, filt_w3, filt_b3, filt_w_out, filt_freq,
              hyena_skip, w_branch_attn, w_branch_hyena, w_out, norm2_g,
              w_router, w_gate, w_up, w_down):
    b, l, _ = x.shape
    pos = jnp.arange(l, dtype=jnp.float32)
    inv_freq = ROPE_THETA ** (-jnp.arange(0, ROPE_DIM, 2, dtype=jnp.float32) / ROPE_DIM)
    ang = pos[:, None] * inv_freq[None, :]
    cos, sin = jnp.cos(ang), jnp.sin(ang)
    splits = [Q_COLS, Q_COLS + K_COLS, Q_COLS + K_COLS + V_COLS,
              Q_COLS + K_COLS + V_COLS + HY_COLS]

    for li in range(DEPTH):
        lambda_init = 0.8 - 0.6 * math.exp(-0.3 * li)
        u = rms_norm(x, norm1_g[li])
        proj = jnp.einsum('bld,dc->blc', u, w_in[li])
        q, k, v, hy, gts = jnp.split(proj, splits, axis=-1)
        q = q.reshape(b, l, ATTN_HEADS, 2, QK_DIM).transpose(0, 2, 3, 1, 4)
        k = k.reshape(b, l, ATTN_HEADS, 2, QK_DIM).transpose(0, 2, 3, 1, 4)
        v = v.reshape(b, l, ATTN_HEADS, V_DIM).transpose(0, 2, 1, 3)
        q = rope_partial(rms_norm(q, q_norm_g[li]), cos, sin)
        k = rope_partial(rms_norm(k, k_norm_g[li]), cos, sin)
        lam = (jnp.exp(jnp.sum(lambda_q1[li] * lambda_k1[li]).astype(jnp.float32))
               - jnp.exp(jnp.sum(lambda_q2[li] * lambda_k2[li]).astype(jnp.float32))
               + lambda_init)
        attn = diff_attention(q, k, v, lam, subln_g[li], lambda_init)

        spec = hyena_filter_spectra(l, filt_w1[li], filt_b1[li], filt_w2[li], filt_b2[li],
                                    filt_w3[li], filt_b3[li], filt_w_out[li], filt_freq[li])
        hyena = hyena_mixer(hy, short_conv_w[li], short_conv_b[li], spec, hyena_skip[li])

        gate = jax.nn.sigmoid(gts.astype(jnp.float32)).astype(x.dtype)
        g_attn, g_hyena = jnp.split(gate, 2, axis=-1)
        merged = (g_attn * jnp.einsum('blc,cd->bld', attn, w_branch_attn[li])
                  + g_hyena * jnp.einsum('blc,cd->bld', hyena, w_branch_hyena[li]))
        x = x + jnp.einsum('bld,de->ble', merged, w_out[li])
        x = x + expert_choice_ffn(rms_norm(x, norm2_g[li]), w_router[li], w_gate[li], w_up[li], w_down[li])
    return x
```

```python
import math
from contextlib import ExitStack
import numpy as np
import ml_dtypes
import concourse.bass as bass
import concourse.mybir as mybir
from concourse.bass_utils import run_bass_kernel_spmd

F32 = mybir.dt.float32
BF16 = mybir.dt.bfloat16
I32 = mybir.dt.int32
AF = mybir.ActivationFunctionType
ALU = mybir.AluOpType
AX = mybir.AxisListType

L = 4096
D = 1024
NT = 32
NCORES = 8
CAP = 512
NE = 16
GC = 32
DEBUG = False


class Trk:
    def __init__(s, nc):
        s.nc = nc
        s.eng = dict(pe=nc.tensor, act=nc.scalar, dve=nc.vector, pool=nc.gpsimd, sp=nc.sync)
        s.sem = {}
        s.cnt = {}
        for k in ("pe", "act", "dve", "pool"):
            s.sem[k] = nc.alloc_semaphore(name="s_" + k)
            s.cnt[k] = 0
        s.ND = 16
        for i in range(s.ND):
            k = "d%d" % i
            s.sem[k] = nc.alloc_semaphore(name="s_" + k)
            s.cnt[k] = 0
        s.rr = 0
        s.seen = {e: {} for e in s.eng}
        s.lw = {}
        s.rd = {}

    def _wait(s, e, deps):
        for k, v in deps.items():
            if e == "pe" and k == "pe":
                continue
            if s.seen[e].get(k, 0) < v:
                s.eng[e].wait_ge(s.sem[k], v)
                s.seen[e][k] = v

    def _deps(s, r, w):
        d = {}

        def add(k, v):
            if d.get(k, 0) < v:
                d[k] = v
        for x in r:
            for k, v in s.lw.get(x, {}).items():
                add(k, v)
        for x in w:
            for k, v in s.lw.get(x, {}).items():
                add(k, v)
            for k, v in s.rd.get(x, {}).items():
                add(k, v)
        return d

    def _upd(s, toks, r, w):
        if isinstance(toks, tuple):
            toks = [toks]
        for x in r:
            m = s.rd.setdefault(x, {})
            for tok in toks:
                if m.get(tok[0], 0) < tok[1]:
                    m[tok[0]] = tok[1]
        for x in w:
            m = {}
            for tok in toks:
                if m.get(tok[0], 0) < tok[1]:
                    m[tok[0]] = tok[1]
            s.lw[x] = m
            s.rd[x] = {}

    def op(s, e, fn, r=(), w=()):
        s._wait(e, s._deps(r, w))
        ins = fn(s.eng[e])
        s.cnt[e] += 1
        ins.then_inc(s.sem[e], 1)
        s._upd((e, s.cnt[e]), r, w)

    def dma(s, q, fn, r=(), w=()):
        s._wait(q, s._deps(r, w))
        ins = fn(s.eng[q])
        k = "d%d" % s.rr
        s.rr = (s.rr + 1) % s.ND
        s.cnt[k] += 16
        ins.then_inc(s.sem[k], 16)
        s._upd((k, s.cnt[k]), r, w)

    def dmas(s, q, fns, r=(), w=()):
        s._wait(q, s._deps(r, w))
        toks = []
        for fn in fns:
            ins = fn(s.eng[q])
            k = "d%d" % s.rr
            s.rr = (s.rr + 1) % s.ND
            s.cnt[k] += 16
            ins.then_inc(s.sem[k], 16)
            toks.append((k, s.cnt[k]))
        s._upd(toks, r, w)

    def barrier(s, engines=None):
        tot = {k: v for k, v in s.cnt.items() if v > 0}
        for e in (engines or s.eng):
            s._wait(e, dict(tot))
        s.lw = {}
        s.rd = {}


def _bf(a):
    return np.ascontiguousarray(a.astype(np.float32)).astype(ml_dtypes.bfloat16)


def host_consts():
    c = {}
    t = np.arange(L, dtype=np.float32)
    inv_freq = (np.float32(500000.0) ** (-np.arange(0, 16, 2, dtype=np.float32) / np.float32(16))).astype(np.float32)
    ang = (t[:, None] * inv_freq[None, :]).astype(np.float32)
    cs = np.concatenate([np.cos(ang), np.sin(ang)], axis=-1).astype(np.float32)
    c["ropecs"] = np.ascontiguousarray(cs.reshape(NT, 128, 16).transpose(1, 0, 2))
    tt = t / np.float32(L - 1)
    bands = np.linspace(1e-4, 15, 16, dtype=np.float32)
    a2 = (np.float32(2.0 * math.pi / L) * t[:, None] * bands[None, :]).astype(np.float32)
    emb = np.concatenate([tt[:, None], np.cos(a2), -np.sin(a2)], axis=-1).astype(np.float32)
    c["embT"] = np.ascontiguousarray(emb.T)
    c["posrow"] = np.ascontiguousarray(np.broadcast_to(tt[None, :], (128, L))).astype(np.float32)
    min_decay = math.log(1e-2) / 1.5
    max_decay = math.log(1e-2) / 0.3
    deltas = np.abs(np.linspace(min_decay, max_decay, 512, dtype=np.float32))
    c["ndelta"] = np.ascontiguousarray((-deltas).reshape(4, 128).T).astype(np.float32)
    n1 = np.arange(32)[:, None]
    k1 = np.arange(64)[None, :]
    th = 2 * np.pi * n1 * k1 / 64.0
    c["FA"] = _bf(np.concatenate([np.cos(th), -np.sin(th)], axis=1))
    n2 = np.arange(128)[:, None, None]
    k1 = np.arange(64)[None, :, None]
    k2 = np.arange(128)[None, None, :]
    th = 2 * np.pi * ((n2 * (k1 + 64 * k2)) % 8192) / 8192.0
    c["TGr"] = _bf(np.cos(th))
    c["TGi"] = _bf(-np.sin(th))
    c["TGn"] = _bf(np.sin(th))
    kk2 = np.arange(128)[:, None]
    nn2 = np.arange(128)[None, :]
    th = 2 * np.pi * ((kk2 * nn2) % 128) / 128.0
    fr, fi = np.cos(th), np.sin(th)
    c["R1"] = _bf(np.concatenate([fr, fi], axis=1))
    c["R2"] = _bf(np.concatenate([-fi, fr], axis=1))
    k1 = np.arange(64)[:, None, None]
    n2 = np.arange(128)[None, :, None]
    n1 = np.arange(32)[None, None, :]
    th = 2 * np.pi * ((k1 * (n2 + 128 * n1)) % 8192) / 8192.0
    th_ = np.stack([np.cos(th), -np.sin(th)], axis=2) / 8192.0
    c["TH"] = _bf(th_)
    tid = np.arange(L).reshape(NT, 128).T
    c["tidhl"] = _bf(np.stack([tid // 64, tid % 64], axis=-1))
    c["iota512"] = np.ascontiguousarray(np.broadcast_to(np.arange(512, dtype=np.float32)[None, :], (128, 512)))
    tri = (np.arange(128)[:, None] <= np.arange(128)[None, :]).astype(np.float32)
    c["tri"] = tri
    return c


def build(dbg=None):
    nc = bass.Bass("TRN2", target_bir_lowering=False)
    dbg = dbg or {}

    def din(name, shape, dt=F32):
        return nc.dram_tensor(name, list(shape), dt, kind="ExternalInput").ap()

    def dscr(name, shape, dt=F32):
        kind = "ExternalOutput" if name in dbg.get("_ext", ()) else "Internal"
        return nc.dram_tensor(name, list(shape), dt, kind=kind).ap()

    x_d = din("x", [L, D])
    g1p_d = din("g1p", [128, 8])
    w_in_d = din("w_in", [D, 5120])
    scw_d = din("scw", [128, 12, 4])
    qkg_d = din("qkg", [128, 2, 64])
    lam_d = din("lamv", [1, 4, 64])
    subg_d = din("subg", [128, 128])
    fw1_d = din("fw1", [33, 64])
    fw2_d = din("fw2", [64, 64])
    fw3_d = din("fw3", [64, 64])
    fbf_d = din("fbf", [64, 4])
    fwo_d = din("fwo", [64, 2048])
    skip_d = din("skipbc", [32, 2, 512])
    wpa_d = din("wpa", [512, D])
    wph_d = din("wph", [512, D])
    wo_d = din("wo", [D, D])
    g2bc_d = din("g2bc", [128, D])
    wr_d = din("wr", [128, 8, 16])
    wg_d = din("wg", [NE, D, D])
    wu_d = din("wu", [NE, D, D])
    wd_d = din("wd", [NE, D, D])
    ropecs_d = din("ropecs", [128, NT, 16])
    embT_d = din("embT", [33, L])
    posrow_d = din("posrow", [128, L])
    ndelta_d = din("ndelta", [128, 4])
    FA_d = din("FA", [32, 128], BF16)
    TGr_d = din("TGr", [128, 64, 128], BF16)
    TGi_d = din("TGi", [128, 64, 128], BF16)
    TGn_d = din("TGn", [128, 64, 128], BF16)
    R1_d = din("R1", [128, 256], BF16)
    R2_d = din("R2", [128, 256], BF16)
    TH_d = din("TH", [64, 128, 2, 32], BF16)
    tidhl_d = din("tidhl", [128, NT, 2], BF16)
    iota_d = din("iota512", [128, 512])
    tri_d = din("tri", [128, 128])

    out_d = nc.dram_tensor("out", [L, D], F32, kind="ExternalOutput").ap()

    qT_d = dscr("qT_s", [4, 128, L], BF16)
    kT_d = dscr("kT_s", [4, 128, L], BF16)
    v_d = dscr("v_s", [L, 512], BF16)
    hyc_d = dscr("hyc_s", [1536, L], F32)
    gateT_d = dscr("gateT_s", [2048, L], BF16)
    kern_d = dscr("kern_s", [2, 2, 512, L], F32)
    attnT_d = dscr("attnT_s", [512, L], BF16)
    hyoT_d = dscr("hyoT_s", [512, L], BF16)
    u2_d = dscr("u2_s", [L, D], BF16)

    dbg_d = {}
    for k, (shape, dt) in ((k, v) for k, v in dbg.items() if not k.startswith("_")):
        dbg_d[k] = nc.dram_tensor("dbg_" + k, list(shape), dt, kind="ExternalOutput").ap()

    T = Trk(nc)
    top = ExitStack()

    sbc = [0]

    def sb(es, name, shape, dt=F32):
        sbc[0] += 1
        return es.enter_context(nc.sbuf_tensor("sb%d_%s" % (sbc[0], name), list(shape), dt))

    ps = [top.enter_context(nc.psum_tensor("ps%d" % i, [128, 512], F32)) for i in range(8)]
    PK = ["ps%d" % i for i in range(8)]

    ident_f = sb(top, "ident_f", [128, 128])
    ident_b = sb(top, "ident_b", [128, 128], BF16)
    eps = sb(top, "eps", [128, 1])
    T.op("pool", lambda e: e.memset(ident_f[:], 0.0), w=["ident_f"])
    T.op("pool", lambda e: e.affine_select(out=ident_f[:], in_=ident_f[:], pattern=[[-1, 128]],
                                           compare_op=ALU.not_equal, fill=1.0, base=0, channel_multiplier=1),
         r=["ident_f"], w=["ident_f"])
    T.op("pool", lambda e: e.tensor_copy(out=ident_b[:], in_=ident_f[:]), r=["ident_f"], w=["ident_b"])
    T.op("pool", lambda e: e.memset(eps[:], 1e-6), w=["eps"])

    stages = dbg.get("_stages", "AKBCDEF") if isinstance(dbg.get("_stages", None), str) else "AKBCDEF"

    if "A" in stages:
      with ExitStack() as es:
        uT = sb(es, "uT", [128, 8, L + 2], BF16)
        g1p = sb(es, "g1p", [128, 8])
        scw = sb(es, "scw", [128, 12, 4])
        qkg = sb(es, "qkg", [128, 2, 64])
        ropecs = sb(es, "ropecs", [128, NT, 16])
        xt = [sb(es, "xt%d" % i, [128, D]) for i in range(2)]
        xb = [sb(es, "xb%d" % i, [128, D], BF16) for i in range(2)]
        junk = sb(es, "junkA", [128, D])
        ss = sb(es, "ssA", [128, NT])
        rs = sb(es, "rsA", [128, NT])
        wf = sb(es, "wf", [128, 8, 512])
        wb = [sb(es, "wb%d" % i, [128, 8, 512], BF16) for i in range(2)]
        T.dma("sp", lambda e: e.dma_start(out=g1p[:], in_=g1p_d[:, :]), w=["g1p"])
        T.dma("sp", lambda e: e.dma_start(out=scw[:], in_=scw_d[:, :, :]), w=["scw"])
        T.dma("sp", lambda e: e.dma_start(out=qkg[:], in_=qkg_d[:, :, :]), w=["qkg"])
        T.dma("sp", lambda e: e.dma_start(out=ropecs[:], in_=ropecs_d[:, :, :]), w=["ropecs"])
        T.op("dve", lambda e: e.memset(ss[:], 0.0), w=["ssA"])
        T.op("dve", lambda e: e.memset(uT[:, :, 0:1], 0.0), w=["uTpad0"])
        T.op("dve", lambda e: e.memset(uT[:, :, L + 1:L + 2], 0.0), w=["uTpad1"])
        T.op("dve", lambda e: e.tensor_scalar(out=qkg[:, 0, :], in0=qkg[:, 0, :], scalar1=0.125, scalar2=None,
                                              op0=ALU.mult), r=["qkg"], w=["qkg"])
        psb = [ps[6][:, :].bitcast(BF16), ps[7][:, :].bitcast(BF16)]
        for i in range(NT):
            b = i % 2
            T.dma("sp", lambda e: e.dma_start(out=xt[b][:], in_=x_d[i * 128:(i + 1) * 128, :]), w=["xt%d" % b])
            T.op("act", lambda e: e.activation(out=junk[:], in_=xt[b][:], func=AF.Square,
                                               accum_out=ss[:, i:i + 1]), r=["xt%d" % b, "ssA"], w=["junkA", "ss%d" % i])
            T.op("act", lambda e: e.activation(out=rs[:, i:i + 1], in_=ss[:, i:i + 1], func=AF.Sqrt,
                                               scale=1.0 / D, bias=eps[:]), r=["ss%d" % i, "eps"], w=["rs%d" % i])
            T.op("dve", lambda e: e.reciprocal(out=rs[:, i:i + 1], in_=rs[:, i:i + 1]), r=["rs%d" % i], w=["rs%d" % i])
            T.op("dve", lambda e: e.tensor_scalar(out=xb[b][:], in0=xt[b][:], scalar1=rs[:, i:i + 1], scalar2=None,
                                                  op0=ALU.mult), r=["xt%d" % b, "rs%d" % i], w=["xb%d" % b])
            for j in range(8):
                T.op("pe", lambda e: e.transpose(out=psb[b][:, j * 128:(j + 1) * 128],
                                                 in_=xb[b][:, j * 128:(j + 1) * 128], identity=ident_b[:]),
                     r=["xb%d" % b, "ident_b"], w=[PK[6 + b]])
            T.op("act", lambda e: e.copy(out=uT[:, :, 1 + i * 128:1 + (i + 1) * 128],
                                         in_=psb[b].rearrange("p (j t) -> p j t", j=8)),
                 r=[PK[6 + b]], w=["uT%d" % i])
        UTK = ["uT%d" % i for i in range(NT)] + ["uTpad0", "uTpad1"]

        sq = sb(es, "sqA", [128, 512])
        ssq = sb(es, "ssqA", [128, 8])
        qn = sb(es, "qnA", [128, 8, 64])
        qb = sb(es, "qbA", [128, 512], BF16)
        rt = [sb(es, "rtA%d" % i, [128, 8, 8]) for i in range(4)]
        qTs = [sb(es, "qTs%d" % i, [128, 4, 128], BF16) for i in range(2)]
        vst = [sb(es, "vst%d" % i, [128, 512], BF16) for i in range(2)]
        hraw = sb(es, "hraw", [128, L + 2])
        hc = sb(es, "hcA", [128, L])
        gst = sb(es, "gstA", [128, L], BF16)
        T.op("pool", lambda e: e.memset(hraw[:, 0:1], 0.0), w=["hrawp0"])
        T.op("pool", lambda e: e.memset(hraw[:, L + 1:L + 2], 0.0), w=["hrawp1"])
        pcnt = [0]

        def nextps():
            pcnt[0] += 1
            return pcnt[0] % 4

        for cc in dbg.get('_ccs', range(10)):
            wbk = "wb%d" % (cc % 2)
            wbt = wb[cc % 2]
            T.dma("sp", lambda e: e.dma_start(
                out=wf[:], in_=w_in_d.rearrange("(j p) c -> p j c", p=128)[:, :, cc * 512:(cc + 1) * 512]), w=["wf"])
            for j in range(8):
                T.op("pool", lambda e: e.tensor_scalar(out=wbt[:, j, :], in0=wf[:, j, :], scalar1=g1p[:, j:j + 1],
                                                       scalar2=None, op0=ALU.mult), r=["wf", "g1p"], w=[wbk])
            if cc < 3:
                for i in range(NT):
                    pi = nextps()
                    for j in range(8):
                        T.op("pe", lambda e: e.matmul(ps[pi][:, :], lhsT=uT[:, j, 1 + i * 128:1 + (i + 1) * 128],
                                                      rhs=wbt[:, j, :], start=(j == 0), stop=(j == 7)),
                             r=["uT%d" % i, wbk], w=[PK[pi]])
                    if cc == 2:
                        vs = vst[i % 2]
                        T.op("act", lambda e: e.copy(out=vs[:], in_=ps[pi][:, :]), r=[PK[pi]], w=["vst%d" % (i % 2)])
                        T.dma("sp", lambda e: e.dma_start(out=v_d[i * 128:(i + 1) * 128, :], in_=vs[:]),
                              r=["vst%d" % (i % 2)], w=["v_d"])
                        continue
                    T.op("act", lambda e: e.activation(out=sq[:], in_=ps[pi][:, :], func=AF.Square), r=[PK[pi]], w=["sqA"])
                    T.op("dve", lambda e: e.tensor_reduce(out=ssq[:], in_=sq[:].rearrange("p (g d) -> p g d", d=64),
                                                          axis=AX.X, op=ALU.add), r=["sqA"], w=["ssqA"])
                    T.op("act", lambda e: e.activation(out=ssq[:], in_=ssq[:], func=AF.Sqrt, scale=1.0 / 64,
                                                       bias=eps[:]), r=["ssqA", "eps"], w=["ssqA"])
                    T.op("dve", lambda e: e.reciprocal(out=ssq[:], in_=ssq[:]), r=["ssqA"], w=["ssqA"])
                    T.op("dve", lambda e: e.tensor_tensor(out=qn[:], in0=ps[pi][:, :].rearrange("p (g d) -> p g d", d=64),
                                                          in1=ssq[:, :].unsqueeze(2).broadcast_to([128, 8, 64]),
                                                          op=ALU.mult), r=[PK[pi], "ssqA"], w=["qnA"])
                    T.op("dve", lambda e: e.tensor_tensor(out=qn[:], in0=qn[:],
                                                          in1=qkg[:, cc, :].unsqueeze(1).broadcast_to([128, 8, 64]),
                                                          op=ALU.mult), r=["qnA", "qkg"], w=["qnA"])
                    T.op("act", lambda e: e.copy(out=qb[:].rearrange("p (g d) -> p g d", d=64), in_=qn[:]),
                         r=["qnA"], w=["qbA"])
                    cosb = ropecs[:, i, 0:8].unsqueeze(1).broadcast_to([128, 8, 8])
                    sinb = ropecs[:, i, 8:16].unsqueeze(1).broadcast_to([128, 8, 8])
                    r1 = qn[:, :, 0:8]
                    r2 = qn[:, :, 8:16]
                    T.op("dve", lambda e: e.tensor_tensor(out=rt[0][:], in0=r1, in1=cosb, op=ALU.mult), r=["qnA", "ropecs"], w=["rt0"])
                    T.op("dve", lambda e: e.tensor_tensor(out=rt[1][:], in0=r2, in1=sinb, op=ALU.mult), r=["qnA", "ropecs"], w=["rt1"])
                    T.op("dve", lambda e: e.tensor_tensor(out=rt[2][:], in0=r2, in1=cosb, op=ALU.mult), r=["qnA", "ropecs"], w=["rt2"])
                    T.op("dve", lambda e: e.tensor_tensor(out=rt[3][:], in0=r1, in1=sinb, op=ALU.mult), r=["qnA", "ropecs"], w=["rt3"])
                    qbv = qb[:].rearrange("p (g d) -> p g d", d=64)
                    T.op("dve", lambda e: e.tensor_tensor(out=qbv[:, :, 0:8], in0=rt[0][:], in1=rt[1][:], op=ALU.subtract),
                         r=["rt0", "rt1"], w=["qbA"])
                    T.op("dve", lambda e: e.tensor_tensor(out=qbv[:, :, 8:16], in0=rt[2][:], in1=rt[3][:], op=ALU.add),
                         r=["rt2", "rt3"], w=["qbA"])
                    pb = 6 + (i % 2)
                    for h in range(4):
                        T.op("pe", lambda e: e.transpose(out=psb[i % 2][:, h * 128:(h + 1) * 128],
                                                         in_=qb[:, h * 128:(h + 1) * 128], identity=ident_b[:]),
                             r=["qbA", "ident_b"], w=[PK[pb]])
                    st = qTs[i % 2]
                    T.op("act", lambda e: e.copy(out=st[:], in_=psb[i % 2][:, 0:512].rearrange("p (h t) -> p h t", h=4)),
                         r=[PK[pb]], w=["qTs%d" % (i % 2)])
                    dst = (qT_d if cc == 0 else kT_d).rearrange("h p t -> p h t")[:, :, i * 128:(i + 1) * 128]
                    T.dma("sp", lambda e: e.dma_start(out=dst, in_=st[:]), r=["qTs%d" % (i % 2)], w=["qkT_d"])
            else:
                for ct in range(4):
                    for tq in range(8):
                        pi = nextps()
                        for j in range(8):
                            T.op("pe", lambda e: e.matmul(ps[pi][:, :], lhsT=wbt[:, j, ct * 128:(ct + 1) * 128],
                                                          rhs=uT[:, j, 1 + tq * 512:1 + (tq + 1) * 512],
                                                          start=(j == 0), stop=(j == 7)),
                                 r=UTK[4 * tq:4 * tq + 4] + [wbk], w=[PK[pi]])
                        if cc < 6:
                            T.op("act", lambda e: e.copy(out=hraw[:, 1 + tq * 512:1 + (tq + 1) * 512], in_=ps[pi][:, :]),
                                 r=[PK[pi]], w=["hraw"])
                        else:
                            T.op("act", lambda e: e.activation(out=gst[:, tq * 512:(tq + 1) * 512], in_=ps[pi][:, :],
                                                               func=AF.Sigmoid), r=[PK[pi]], w=["gstA"])
                    if cc < 6:
                        cti = (cc - 3) * 4 + ct
                        lvl = dbg.get('_lvl', 9)
                        if lvl >= 1:
                          T.op("act", lambda e: e.activation(out=hc[:], in_=hraw[:, 1:L + 1], func=AF.Identity,
                                                           scale=scw[:, cti, 1:2], bias=scw[:, cti, 3:4]),
                             r=["hraw", "scw"], w=["hcA"])
                        if lvl >= 2:
                          T.op("dve", lambda e: e.scalar_tensor_tensor(out=hc[:], in0=hraw[:, 0:L], scalar=scw[:, cti, 0:1],
                                                                     in1=hc[:], op0=ALU.mult, op1=ALU.add),
                             r=["hraw", "hrawp0", "scw", "hcA"], w=["hcA"])
                          T.op("dve", lambda e: e.scalar_tensor_tensor(out=hc[:], in0=hraw[:, 2:L + 2], scalar=scw[:, cti, 2:3],
                                                                     in1=hc[:], op0=ALU.mult, op1=ALU.add),
                             r=["hraw", "hrawp1", "scw", "hcA"], w=["hcA"])
                        if lvl >= 3:
                          for q4 in range(4):
                              T.dma("sp", lambda e: e.dma_start(out=hyc_d[cti * 128:(cti + 1) * 128, q4 * 1024:(q4 + 1) * 1024],
                                                                in_=hc[:, q4 * 1024:(q4 + 1) * 1024]),
                                    r=["hcA"], w=["hyc_d%d" % q4])
                    else:
                        gti = (cc - 6) * 4 + ct
                        T.dmas("sp", [(lambda e, a=a: e.dma_start(out=gateT_d[gti * 128:(gti + 1) * 128, a:a + 2048], in_=gst[:, a:a + 2048]))
                                      for a in (0, 2048)], r=["gstA"], w=["gateT_d"])
        T.barrier()

    if "K" in stages:
      with ExitStack() as es:
        embT = sb(es, "embT", [33, L])
        fw1 = sb(es, "fw1", [33, 64])
        fw2 = sb(es, "fw2", [64, 64])
        fw3 = sb(es, "fw3", [64, 64])
        fbf = sb(es, "fbf", [64, 4])
        frb = sb(es, "frb", [64, 3])
        fwo = sb(es, "fwo", [64, 2048])
        posrow = sb(es, "posrow", [128, L])
        ndelta = sb(es, "ndelta", [128, 4])
        hid = [sb(es, "hid%d" % i, [64, L]) for i in range(2)]
        hidb = sb(es, "hidb", [64, L])
        dec = sb(es, "decK", [128, L])
        kf = sb(es, "kfK", [128, L])
        kb = sb(es, "kbK", [128, L])
        hp = sb(es, "hpK", [128, L])
        hm = sb(es, "hmK", [128, L])
        for tns, src, k in ((fw1, fw1_d, "fw1"), (fw2, fw2_d, "fw2"), (fw3, fw3_d, "fw3"),
                            (fbf, fbf_d, "fbf"), (ndelta, ndelta_d, "ndelta")):
            T.dma("sp", lambda e: e.dma_start(out=tns[:], in_=src), w=[k])
        for tns, src, k, n in ((embT, embT_d, "embT", L), (fwo, fwo_d, "fwo", 2048), (posrow, posrow_d, "posrow", L)):
            T.dmas("sp", [(lambda e, a=a: e.dma_start(out=tns[:, a:a + 1024], in_=src[:, a:a + 1024]))
                          for a in range(0, n, 1024)], w=[k])
        for l in range(3):
            T.op("dve", lambda e: e.tensor_tensor(out=frb[:, l:l + 1], in0=fbf[:, l:l + 1], in1=fbf[:, 3:4], op=ALU.mult),
                 r=["fbf"], w=["frb"])
        TWO_PI = 2.0 * math.pi
        srcs = [(embT, "embT", 33, fw1, "fw1"), (hid[0], "hid0", 64, fw2, "fw2"), (hid[1], "hid1", 64, fw3, "fw3")]
        outs = [(hid[0], "hid0"), (hid[1], "hid1"), (hid[0], "hid0")]
        negpi = sb(es, "negpi", [64, 1])
        T.op("dve", lambda e: e.memset(negpi[:], 4.0 * math.pi), w=["negpi"])
        kcnt = sb(es, "kcntK", [64, L])
        for l in range(3):
            src, sk, kk, w_, wk = srcs[l]
            dst, dk = outs[l]
            for tq in range(8):
                pi = tq % 4
                T.op("pe", lambda e: e.matmul(ps[pi][0:64, :], lhsT=w_[0:kk, :], rhs=src[0:kk, tq * 512:(tq + 1) * 512],
                                              start=True, stop=True), r=[sk, wk], w=[PK[pi]])
                T.op("dve", lambda e: e.tensor_scalar(out=hidb[:, tq * 512:(tq + 1) * 512], in0=ps[pi][0:64, :],
                                                      scalar1=fbf[:, 3:4], scalar2=frb[:, l:l + 1], op0=ALU.mult,
                                                      op1=ALU.add), r=[PK[pi], "fbf", "frb"], w=["hidb"])
                xs_ = hidb[:, tq * 512:(tq + 1) * 512]
                ks_ = kcnt[:, tq * 512:(tq + 1) * 512]
                T.op("dve", lambda e: e.tensor_scalar(out=ks_, in0=xs_, scalar1=-3.0 * math.pi, scalar2=None, op0=ALU.is_gt),
                     r=["hidb"], w=["kcnt"])
                for thr in (-math.pi, math.pi, 3.0 * math.pi):
                    T.op("dve", lambda e: e.scalar_tensor_tensor(out=ks_, in0=xs_, scalar=thr, in1=ks_, op0=ALU.is_gt, op1=ALU.add),
                         r=["hidb", "kcnt"], w=["kcnt"])
                T.op("dve", lambda e: e.scalar_tensor_tensor(out=xs_, in0=ks_, scalar=-TWO_PI, in1=xs_, op0=ALU.mult, op1=ALU.add),
                     r=["hidb", "kcnt"], w=["hidb"])
                T.op("act", lambda e: e.activation(out=dst[:, tq * 512:(tq + 1) * 512], in_=xs_,
                                                   func=AF.Sin, bias=negpi[:]), r=["hidb", "negpi"], w=[dk])
        h3 = hid[0]
        for ct in range(4):
            T.op("act", lambda e: e.activation(out=dec[:], in_=posrow[:], func=AF.Exp, scale=ndelta[:, ct:ct + 1]),
                 r=["posrow", "ndelta"], w=["decK"])
            for o in range(2):
                for d_, (dst, dk) in enumerate(((kf, "kfK"), (kb, "kbK"))):
                    col = (o * 2 + d_) * 512 + ct * 128
                    for tq in range(8):
                        pi = tq % 4
                        T.op("pe", lambda e: e.matmul(ps[pi][:, :], lhsT=fwo[:, col:col + 128],
                                                      rhs=h3[:, tq * 512:(tq + 1) * 512], start=True, stop=True),
                             r=["hid0", "fwo"], w=[PK[pi]])
                        T.op("dve", lambda e: e.tensor_tensor(out=dst[:, tq * 512:(tq + 1) * 512], in0=ps[pi][:, :],
                                                              in1=dec[:, tq * 512:(tq + 1) * 512], op=ALU.mult),
                             r=[PK[pi], "decK"], w=[dk])
                T.op("dve", lambda e: e.memset(kb[:, 0:1], 0.0), r=["kbK"], w=["kbK"])
                T.op("pool", lambda e: e.tensor_tensor(out=hp[:], in0=kf[:], in1=kb[:], op=ALU.add), r=["kfK", "kbK"], w=["hpK"])
                T.op("pool", lambda e: e.tensor_tensor(out=hm[:], in0=kf[:], in1=kb[:], op=ALU.subtract), r=["kfK", "kbK"], w=["hmK"])
                T.dmas("sp", [(lambda e, a=a: e.dma_start(out=kern_d[o, 0, ct * 128:(ct + 1) * 128, a:a + 1024], in_=hp[:, a:a + 1024]))
                              for a in range(0, L, 1024)], r=["hpK"], w=["kern_d0"])
                T.dmas("sp", [(lambda e, a=a: e.dma_start(out=kern_d[o, 1, ct * 128:(ct + 1) * 128, a:a + 1024], in_=hm[:, a:a + 1024]))
                              for a in range(0, L, 1024)], r=["hmK"], w=["kern_d1"])
        T.barrier()

    if "B" in stages:
      with ExitStack() as es:
        qT = sb(es, "qT", [128, 4, L], BF16)
        kT = sb(es, "kT", [128, 4, L], BF16)
        V = sb(es, "Vsb", [128, NT, 4, 132], BF16)
        lamv = sb(es, "lamv", [1, 4, 64])
        lamt = sb(es, "lamt", [1, 8])
        ones1 = sb(es, "ones1", [1, 128])
        nlam = sb(es, "nlam", [128, 1])
        subg = sb(es, "subg", [128, 128])
        E = [sb(es, "E%d" % i, [128, 512], BF16) for i in range(3)]
        aacc = sb(es, "aacc", [128, 128])
        ab = sb(es, "abB", [128, 128], BF16)
        junkb = sb(es, "junkB", [128, 128])
        sm = sb(es, "smB", [128, 8])
        aTs = [sb(es, "aTs%d" % i, [128, 512], BF16) for i in range(2)]
        for (tt_, td_, tk_) in ((qT, qT_d, "qT"), (kT, kT_d, "kT")):
            T.dmas("sp", [(lambda e, h=h, a=a: e.dma_start(out=tt_[:, h, a:a + 2048], in_=td_[h, :, a:a + 2048]))
                          for h in range(4) for a in (0, 2048)], w=[tk_])
        T.dmas("sp", [(lambda e, i=i: e.dma_start(out=V[:, i, :, 0:128],
                                                  in_=v_d[i * 128:(i + 1) * 128, :].rearrange("p (h d) -> p h d", d=128)))
                      for i in range(NT)], w=["V"])
        T.op("pool", lambda e: e.memset(V[:, :, :, 128:129], 1.0), w=["Vones"])
        T.dma("sp", lambda e: e.dma_start(out=lamv[:], in_=lam_d[:, :, :]), w=["lamv"])
        T.dma("sp", lambda e: e.dma_start(out=subg[:], in_=subg_d[:, :]), w=["subg"])
        T.op("dve", lambda e: e.tensor_scalar(out=subg[:], in0=subg[:], scalar1=0.8, scalar2=None, op0=ALU.mult),
             r=["subg"], w=["subg"])
        T.op("dve", lambda e: e.memset(ones1[:], 1.0), w=["ones1"])
        T.op("dve", lambda e: e.memset(lamt[:], 0.0), w=["lamt"])
        T.op("dve", lambda e: e.tensor_tensor(out=lamv[:, 0, :], in0=lamv[:, 0, :], in1=lamv[:, 1, :], op=ALU.mult), r=["lamv"], w=["lamv"])
        T.op("dve", lambda e: e.tensor_tensor(out=lamv[:, 2, :], in0=lamv[:, 2, :], in1=lamv[:, 3, :], op=ALU.mult), r=["lamv"], w=["lamv"])
        T.op("dve", lambda e: e.tensor_reduce(out=lamt[:, 0:1], in_=lamv[:, 0, :], axis=AX.X, op=ALU.add), r=["lamv", "lamt"], w=["lamt"])
        T.op("dve", lambda e: e.tensor_reduce(out=lamt[:, 1:2], in_=lamv[:, 2, :], axis=AX.X, op=ALU.add), r=["lamv", "lamt"], w=["lamt"])
        T.op("act", lambda e: e.activation(out=lamt[:, 2:4], in_=lamt[:, 0:2], func=AF.Exp), r=["lamt"], w=["lamt"])
        T.op("dve", lambda e: e.tensor_tensor(out=lamt[:, 4:5], in0=lamt[:, 3:4], in1=lamt[:, 2:3], op=ALU.subtract), r=["lamt"], w=["lamt"])
        T.op("dve", lambda e: e.tensor_scalar(out=lamt[:, 5:6], in0=lamt[:, 4:5], scalar1=-0.2, scalar2=None, op0=ALU.add), r=["lamt"], w=["lamt"])
        T.op("pe", lambda e: e.matmul(ps[7][:, 0:1], lhsT=ones1[:, :], rhs=lamt[:, 5:6], start=True, stop=True),
             r=["ones1", "lamt"], w=[PK[7]])
        T.op("dve", lambda e: e.tensor_copy(out=nlam[:], in_=ps[7][:, 0:1]), r=[PK[7]], w=["nlam"])
        psT = ps[6][:, :].bitcast(BF16)
        ec = 0
        for h in range(4):
            for qc in range(8):
                for m in range(2):
                    pr = slice(m * 64, (m + 1) * 64)
                    for kt in range(NT):
                        sp_ = kt % 2
                        T.op("pe", lambda e: e.matmul(ps[sp_][:, :], lhsT=kT[pr, h, kt * 128:(kt + 1) * 128],
                                                      rhs=qT[pr, h, qc * 512:(qc + 1) * 512], start=True, stop=True),
                             r=["kT", "qT"], w=[PK[sp_]])
                        eb = ec % 3
                        ec += 1
                        T.op("act", lambda e: e.activation(out=E[eb][:], in_=ps[sp_][:, :], func=AF.Exp),
                             r=[PK[sp_]], w=["E%d" % eb])
                        for qs in range(4):
                            bank = 2 + m * 2 + qs // 2
                            T.op("pe", lambda e: e.matmul(ps[bank][:, (qs % 2) * 256:(qs % 2) * 256 + 129],
                                                          lhsT=E[eb][:, qs * 128:(qs + 1) * 128], rhs=V[:, kt, h, 0:129],
                                                          start=(kt == 0 and qs % 2 == 0), stop=(kt == NT - 1),
                                                          skip_group_check=True),
                                 r=["E%d" % eb, "V", "Vones"], w=[PK[bank]])
                for qs in range(4):
                    o1 = ps[2 + qs // 2][:, (qs % 2) * 256:(qs % 2) * 256 + 129]
                    o2 = ps[4 + qs // 2][:, (qs % 2) * 256:(qs % 2) * 256 + 129]
                    k1_, k2_ = PK[2 + qs // 2], PK[4 + qs // 2]
                    T.op("dve", lambda e: e.reciprocal(out=sm[:, 0:1], in_=o1[:, 128:129]), r=[k1_], w=["smB"])
                    T.op("dve", lambda e: e.reciprocal(out=sm[:, 1:2], in_=o2[:, 128:129]), r=[k2_, "smB"], w=["smB"])
                    T.op("dve", lambda e: e.tensor_tensor(out=sm[:, 2:3], in0=sm[:, 1:2], in1=nlam[:], op=ALU.mult),
                         r=["smB", "nlam"], w=["smB"])
                    T.op("dve", lambda e: e.tensor_scalar(out=aacc[:], in0=o1[:, 0:128], scalar1=sm[:, 0:1], scalar2=None,
                                                          op0=ALU.mult), r=[k1_, "smB"], w=["aacc"])
                    T.op("dve", lambda e: e.scalar_tensor_tensor(out=aacc[:], in0=o2[:, 0:128], scalar=sm[:, 2:3], in1=aacc[:],
                                                                 op0=ALU.mult, op1=ALU.add), r=[k2_, "smB", "aacc"], w=["aacc"])
                    T.op("dve", lambda e: e.memset(sm[:, 3:4], 0.0), r=["smB"], w=["smB"])
                    T.op("act", lambda e: e.activation(out=junkb[:], in_=aacc[:], func=AF.Square, accum_out=sm[:, 3:4]),
                         r=["aacc", "smB"], w=["junkB", "smB"])
                    T.op("act", lambda e: e.activation(out=sm[:, 4:5], in_=sm[:, 3:4], func=AF.Sqrt, scale=1.0 / 128,
                                                       bias=eps[:]), r=["smB", "eps"], w=["smB"])
                    T.op("dve", lambda e: e.reciprocal(out=sm[:, 5:6], in_=sm[:, 4:5]), r=["smB"], w=["smB"])
                    T.op("dve", lambda e: e.scalar_tensor_tensor(out=ab[:], in0=aacc[:], scalar=sm[:, 5:6], in1=subg[:],
                                                                 op0=ALU.mult, op1=ALU.mult), r=["aacc", "smB", "subg"], w=["abB"])
                    T.op("pe", lambda e: e.transpose(out=psT[:, qs * 128:(qs + 1) * 128], in_=ab[:], identity=ident_b[:]),
                         r=["abB", "ident_b"], w=[PK[6]])
                st = aTs[qc % 2]
                T.op("act", lambda e: e.copy(out=st[:], in_=psT[:, 0:512]), r=[PK[6]], w=["aTs%d" % (qc % 2)])
                T.dma("sp", lambda e: e.dma_start(out=attnT_d[h * 128:(h + 1) * 128, qc * 512:(qc + 1) * 512], in_=st[:]),
                      r=["aTs%d" % (qc % 2)], w=["attnT_d"])
        T.barrier()

    if "C" in stages:
      with ExitStack() as es:
        FA = sb(es, "FA", [32, 128], BF16)
        TGr = sb(es, "TGr", [128, 64, 128], BF16)
        TGi = sb(es, "TGi", [128, 64, 128], BF16)
        TGn = sb(es, "TGn", [128, 64, 128], BF16)
        R1 = sb(es, "R1", [128, 256], BF16)
        R2 = sb(es, "R2", [128, 256], BF16)
        TH = sb(es, "TH", [64, 128, 2, 32], BF16)
        skipbc = sb(es, "skipbc", [32, 2, GC])
        for tns, src, k in ((FA, FA_d, "FA"), (R1, R1_d, "R1"), (R2, R2_d, "R2")):
            T.dma("sp", lambda e: e.dma_start(out=tns[:], in_=src), w=[k])
        for tns, src, k in ((TGr, TGr_d, "TGr"), (TGi, TGi_d, "TGi"), (TGn, TGn_d, "TGn")):
            T.dmas("sp", [(lambda e, a=a: e.dma_start(out=tns[:, a:a + 16, :], in_=src[:, a:a + 16, :])) for a in range(0, 64, 16)], w=[k])
        T.dmas("sp", [(lambda e, a=a: e.dma_start(out=TH[:, a:a + 32, :, :], in_=TH_d[:, a:a + 32, :, :])) for a in range(0, 128, 32)], w=["TH"])
        zf = sb(es, "zf", [32, GC, 128])
        gt = sb(es, "gtC", [32, GC, 128])
        zs = sb(es, "zsC", [32, GC, 128])
        LDN = 4
        ld = sb(es, "ldC", [32, GC // LDN, 128])
        srcb = [sb(es, "srcb%d" % i, [32, GC, 128], BF16) for i in range(3)]
        Bm_ = [sb(es, "Bm%d" % i, [128, 128, GC], BF16) for i in range(3)]
        srs = sb(es, "srsC", [128, 512])
        sis = sb(es, "sisC", [128, 512])
        tm = [sb(es, "tmC%d" % i, [128, 512]) for i in range(4)]
        Y = sb(es, "YC", [128, GC, 2, 64], BF16)
        Dm = sb(es, "DmC", [64, 256, GC], BF16)
        tmpy = sb(es, "tmpyC", [32, GC, 16])
        hob = srcb[0]

        def flay(ap2d):
            return ap2d.rearrange("c (a b) -> a c b", b=128)

        for g in range(512 // GC):
            c0 = g * GC
            T.dma("sp", lambda e: e.dma_start(out=zf[:], in_=flay(hyc_d[c0:c0 + GC, :])), w=["zf"])
            T.dma("sp", lambda e: e.dma_start(out=skipbc[:], in_=skip_d[:, :, c0:c0 + GC]), w=["skipbc"])
            for o in range(2):
                T.dma("sp", lambda e: e.dma_start(out=gt[:], in_=flay(hyc_d[512 * (o + 1) + c0:512 * (o + 1) + c0 + GC, :])),
                      w=["gtC"])
                T.op("act", lambda e: e.copy(out=srcb[0][:], in_=zf[:]), r=["zf"], w=["srcb0"])
                for pm in range(2):
                    for hf in range(LDN):
                        ch = c0 + hf * (GC // LDN)
                        T.dma("sp", lambda e: e.dma_start(out=ld[:], in_=flay(kern_d[o, pm, ch:ch + GC // LDN, :])), w=["ldC"])
                        T.op("act", lambda e: e.copy(out=srcb[1 + pm][:, hf * (GC // LDN):(hf + 1) * (GC // LDN), :], in_=ld[:]),
                             r=["ldC"], w=["srcb%d" % (1 + pm)])
                T.op("pool", lambda e: e.tensor_tensor(out=zs[:], in0=zf[:],
                                                       in1=skipbc[:, o, :].unsqueeze(2).broadcast_to([32, GC, 128]),
                                                       op=ALU.mult), r=["zf", "skipbc"], w=["zsC"])
                for s_ in range(3):
                    for cq in range(GC // 4):
                        pi = cq % 2
                        for c4 in range(4):
                            c = cq * 4 + c4
                            T.op("pe", lambda e: e.matmul(ps[pi][:, c4 * 128:(c4 + 1) * 128], lhsT=srcb[s_][:, c, :],
                                                          rhs=FA[:, :], start=True, stop=True),
                                 r=["srcb%d" % s_, "FA"], w=[PK[pi]])
                        T.op("act" if cq % 2 == 0 else "dve",
                             lambda e: (e.copy if cq % 2 == 0 else e.tensor_copy)(
                                 out=Bm_[s_][:, :, cq * 4:(cq + 1) * 4].rearrange("p f c -> p c f"),
                                 in_=ps[pi][:, :].rearrange("p (c f) -> p c f", c=4)),
                             r=[PK[pi]], w=["Bm%d" % s_])
                for kc in range(4):
                    for j in range(16):
                        k1 = kc * 16 + j
                        cs_ = slice(j * GC, (j + 1) * GC)
                        bz, bp, bm = Bm_[0], Bm_[1], Bm_[2]
                        T.op("pe", lambda e: e.matmul(ps[2][:, cs_], lhsT=TGr[:, k1, :], rhs=bz[:, k1, :], start=True, stop=False), r=["TGr", "Bm0"], w=[PK[2]])
                        T.op("pe", lambda e: e.matmul(ps[2][:, cs_], lhsT=TGn[:, k1, :], rhs=bz[:, 64 + k1, :], start=False, stop=True), r=["TGn", "Bm0"], w=[PK[2]])
                        T.op("pe", lambda e: e.matmul(ps[3][:, cs_], lhsT=TGi[:, k1, :], rhs=bz[:, k1, :], start=True, stop=False), r=["TGi", "Bm0"], w=[PK[3]])
                        T.op("pe", lambda e: e.matmul(ps[3][:, cs_], lhsT=TGr[:, k1, :], rhs=bz[:, 64 + k1, :], start=False, stop=True), r=["TGr", "Bm0"], w=[PK[3]])
                        T.op("pe", lambda e: e.matmul(ps[4][:, cs_], lhsT=TGr[:, k1, :], rhs=bp[:, k1, :], start=True, stop=False), r=["TGr", "Bm1"], w=[PK[4]])
                        T.op("pe", lambda e: e.matmul(ps[4][:, cs_], lhsT=TGn[:, k1, :], rhs=bp[:, 64 + k1, :], start=False, stop=True), r=["TGn", "Bm1"], w=[PK[4]])
                        T.op("pe", lambda e: e.matmul(ps[5][:, cs_], lhsT=TGi[:, k1, :], rhs=bm[:, k1, :], start=True, stop=False), r=["TGi", "Bm2"], w=[PK[5]])
                        T.op("pe", lambda e: e.matmul(ps[5][:, cs_], lhsT=TGr[:, k1, :], rhs=bm[:, 64 + k1, :], start=False, stop=True), r=["TGr", "Bm2"], w=[PK[5]])
                    T.op("act", lambda e: e.copy(out=srs[:], in_=ps[4][:, :]), r=[PK[4]], w=["srsC"])
                    T.op("act", lambda e: e.copy(out=sis[:], in_=ps[5][:, :]), r=[PK[5]], w=["sisC"])
                    T.op("dve", lambda e: e.tensor_tensor(out=tm[0][:], in0=ps[2][:, :], in1=srs[:], op=ALU.mult), r=[PK[2], "srsC"], w=["tm0"])
                    T.op("dve", lambda e: e.tensor_tensor(out=tm[1][:], in0=ps[3][:, :], in1=sis[:], op=ALU.mult), r=[PK[3], "sisC"], w=["tm1"])
                    T.op("dve", lambda e: e.tensor_tensor(out=tm[2][:], in0=ps[2][:, :], in1=sis[:], op=ALU.mult), r=[PK[2], "sisC"], w=["tm2"])
                    T.op("dve", lambda e: e.tensor_tensor(out=tm[3][:], in0=ps[3][:, :], in1=srs[:], op=ALU.mult), r=[PK[3], "srsC"], w=["tm3"])
                    yv_r = Y[:, :, 0, kc * 16:(kc + 1) * 16].rearrange("p c k -> p k c")
                    yv_i = Y[:, :, 1, kc * 16:(kc + 1) * 16].rearrange("p c k -> p k c")
                    T.op("pool", lambda e: e.tensor_tensor(out=yv_r, in0=tm[0][:].rearrange("p (k c) -> p k c", c=GC),
                                                           in1=tm[1][:].rearrange("p (k c) -> p k c", c=GC), op=ALU.subtract),
                         r=["tm0", "tm1"], w=["YC"])
                    T.op("pool", lambda e: e.tensor_tensor(out=yv_i, in0=tm[2][:].rearrange("p (k c) -> p k c", c=GC),
                                                           in1=tm[3][:].rearrange("p (k c) -> p k c", c=GC), op=ALU.add),
                         r=["tm2", "tm3"], w=["YC"])
                for cq in range(GC // 2):
                    pi = cq % 2
                    for c2 in range(2):
                        c = cq * 2 + c2
                        T.op("pe", lambda e: e.matmul(ps[pi][0:64, c2 * 256:(c2 + 1) * 256], lhsT=Y[:, c, 0, :], rhs=R1[:, :],
                                                      start=True, stop=False), r=["YC", "R1"], w=[PK[pi]])
                        T.op("pe", lambda e: e.matmul(ps[pi][0:64, c2 * 256:(c2 + 1) * 256], lhsT=Y[:, c, 1, :], rhs=R2[:, :],
                                                      start=False, stop=True), r=["YC", "R2"], w=[PK[pi]])
                    T.op("act" if cq % 2 == 0 else "dve",
                         lambda e: (e.copy if cq % 2 == 0 else e.tensor_copy)(
                             out=Dm[:, :, cq * 2:(cq + 1) * 2].rearrange("p f c -> p c f"),
                             in_=ps[pi][0:64, :].rearrange("p (c f) -> p c f", c=2)),
                         r=[PK[pi]], w=["DmC"])
                for nq in range(8):
                    pi = 6 + nq % 2
                    for j in range(16):
                        n2 = nq * 16 + j
                        T.op("pe", lambda e: e.matmul(ps[pi][0:32, j * GC:(j + 1) * GC], lhsT=TH[:, n2, 0, :], rhs=Dm[:, n2, :],
                                                      start=True, stop=False), r=["TH", "DmC"], w=[PK[pi]])
                        T.op("pe", lambda e: e.matmul(ps[pi][0:32, j * GC:(j + 1) * GC], lhsT=TH[:, n2, 1, :], rhs=Dm[:, 128 + n2, :],
                                                      start=False, stop=True), r=["TH", "DmC"], w=[PK[pi]])
                    nsl = slice(nq * 16, (nq + 1) * 16)
                    T.op("dve", lambda e: e.tensor_tensor(out=tmpy[:], in0=ps[pi][0:32, :].rearrange("p (n c) -> p c n", c=GC),
                                                          in1=zs[:, :, nsl], op=ALU.add), r=[PK[pi], "zsC"], w=["tmpyC"])
                    T.op("dve", lambda e: e.tensor_tensor(out=zf[:, :, nsl], in0=tmpy[:], in1=gt[:, :, nsl], op=ALU.mult),
                         r=["tmpyC", "gtC", "srcb0", "zsC"], w=["zf"])
            T.op("act", lambda e: e.copy(out=hob[:], in_=zf[:]), r=["zf"], w=["srcb0"])
            T.dma("sp", lambda e: e.dma_start(out=flay(hyoT_d[c0:c0 + GC, :]), in_=hob[:]), r=["srcb0"], w=["hyoT_d"])
        T.barrier()

    if "D" in stages:
      with ExitStack() as es:
        aff_all = sb(es, "aff_all", [128, NT, NE])
        with ExitStack() as es2:
            attnT = sb(es2, "attnT", [128, 4, L], BF16)
            hyoT = sb(es2, "hyoT", [128, 4, L], BF16)
            wpa = sb(es2, "wpa", [128, 4, D], BF16)
            wph = sb(es2, "wph", [128, 4, D], BF16)
            wo = sb(es2, "wo", [128, 8, D], BF16)
            wst = sb(es2, "wstD", [128, 8, D])
            g2bc = sb(es2, "g2bc", [128, D])
            wr = sb(es2, "wr", [128, 8, 16])
            gat = sb(es2, "gatD", [128, 16, 512], BF16)
            m1 = sb(es2, "m1D", [128, 512])
            m2 = sb(es2, "m2D", [128, 512])
            mT = sb(es2, "mTD", [128, 8, 512], BF16)
            xin = sb(es2, "xinD", [128, D])
            x1 = sb(es2, "x1D", [128, D])
            u2 = sb(es2, "u2D", [128, D])
            u2b = sb(es2, "u2bD", [128, D], BF16)
            u2T = sb(es2, "u2TD", [128, 8, 128])
            junkd = sb(es2, "junkD", [128, D])
            st = sb(es2, "stD", [128, 8])
            lg = sb(es2, "lgD", [128, NE])
            for (tt_, td_, tk_) in ((attnT, attnT_d, "attnT"), (hyoT, hyoT_d, "hyoT")):
                T.dmas("sp", [(lambda e, j=j, a=a: e.dma_start(out=tt_[:, j, a:a + 2048], in_=td_[j * 128:(j + 1) * 128, a:a + 2048]))
                              for j in range(4) for a in (0, 2048)], w=[tk_])
            T.dma("sp", lambda e: e.dma_start(out=g2bc[:], in_=g2bc_d[:, :]), w=["g2bc"])
            T.dma("sp", lambda e: e.dma_start(out=wr[:], in_=wr_d[:, :, :]), w=["wr"])
            for (wsrc, nj, wdst, wk) in ((wpa_d, 4, wpa, "wpa"), (wph_d, 4, wph, "wph"), (wo_d, 8, wo, "wo")):
                T.dma("sp", lambda e: e.dma_start(out=wst[:, 0:nj, :], in_=wsrc.rearrange("(j p) c -> p j c", p=128)), w=["wstD"])
                T.op("pool", lambda e: e.tensor_copy(out=wdst[:], in_=wst[:, 0:nj, :]), r=["wstD"], w=[wk])
            for tq in range(8):
                tsl = slice(tq * 512, (tq + 1) * 512)
                T.dmas("sp", [(lambda e, a=a: e.dma_start(out=gat[:, a:a + 8, :],
                                                          in_=gateT_d.rearrange("(j p) t -> p j t", p=128)[:, a:a + 8, tsl])) for a in (0, 8)],
                       w=["gatD"])
                for dt in range(8):
                    for j in range(4):
                        T.op("pe", lambda e: e.matmul(ps[0][:, :], lhsT=wpa[:, j, dt * 128:(dt + 1) * 128], rhs=attnT[:, j, tsl],
                                                      start=(j == 0), stop=(j == 3)), r=["wpa", "attnT"], w=[PK[0]])
                    for j in range(4):
                        T.op("pe", lambda e: e.matmul(ps[1][:, :], lhsT=wph[:, j, dt * 128:(dt + 1) * 128], rhs=hyoT[:, j, tsl],
                                                      start=(j == 0), stop=(j == 3)), r=["wph", "hyoT"], w=[PK[1]])
                    T.op("dve", lambda e: e.tensor_tensor(out=m1[:], in0=ps[0][:, :], in1=gat[:, dt, :], op=ALU.mult), r=[PK[0], "gatD"], w=["m1D"])
                    T.op("dve", lambda e: e.tensor_tensor(out=m2[:], in0=ps[1][:, :], in1=gat[:, 8 + dt, :], op=ALU.mult), r=[PK[1], "gatD"], w=["m2D"])
                    T.op("pool", lambda e: e.tensor_tensor(out=mT[:, dt, :], in0=m1[:], in1=m2[:], op=ALU.add), r=["m1D", "m2D"], w=["mTD"])
                for ts in range(4):
                    i = tq * 4 + ts
                    T.dma("sp", lambda e: e.dma_start(out=xin[:], in_=x_d[i * 128:(i + 1) * 128, :]), w=["xinD"])
                    for half in range(2):
                        for dt in range(8):
                            T.op("pe", lambda e: e.matmul(ps[2 + half][:, :], lhsT=mT[:, dt, ts * 128:(ts + 1) * 128],
                                                          rhs=wo[:, dt, half * 512:(half + 1) * 512], start=(dt == 0), stop=(dt == 7)),
                                 r=["mTD", "wo"], w=[PK[2 + half]])
                        T.op("dve", lambda e: e.tensor_tensor(out=x1[:, half * 512:(half + 1) * 512], in0=ps[2 + half][:, :],
                                                              in1=xin[:, half * 512:(half + 1) * 512], op=ALU.add),
                             r=[PK[2 + half], "xinD"], w=["x1D"])
                    T.dma("sp", lambda e: e.dma_start(out=out_d[i * 128:(i + 1) * 128, :], in_=x1[:]), r=["x1D"], w=["out_d"])
                    T.op("dve", lambda e: e.memset(st[:, 0:1], 0.0), w=["stD"])
                    T.op("act", lambda e: e.activation(out=junkd[:], in_=x1[:], func=AF.Square, accum_out=st[:, 0:1]),
                         r=["x1D", "stD"], w=["junkD", "stD"])
                    T.op("act", lambda e: e.activation(out=st[:, 1:2], in_=st[:, 0:1], func=AF.Sqrt, scale=1.0 / D, bias=eps[:]),
                         r=["stD", "eps"], w=["stD"])
                    T.op("dve", lambda e: e.reciprocal(out=st[:, 2:3], in_=st[:, 1:2]), r=["stD"], w=["stD"])
                    T.op("dve", lambda e: e.scalar_tensor_tensor(out=u2[:], in0=x1[:], scalar=st[:, 2:3], in1=g2bc[:],
                                                                 op0=ALU.mult, op1=ALU.mult), r=["x1D", "stD", "g2bc"], w=["u2D"])
                    T.op("act", lambda e: e.copy(out=u2b[:], in_=u2[:]), r=["u2D"], w=["u2bD"])
                    T.dma("sp", lambda e: e.dma_start(out=u2_d[i * 128:(i + 1) * 128, :], in_=u2b[:]), r=["u2bD"], w=["u2_d"])
                    for j in range(8):
                        T.op("pe", lambda e: e.transpose(out=ps[4 + j // 4][:, (j % 4) * 128:(j % 4 + 1) * 128],
                                                         in_=u2[:, j * 128:(j + 1) * 128], identity=ident_f[:]),
                             r=["u2D", "ident_f"], w=[PK[4 + j // 4]])
                    for hh in range(2):
                        T.op("act", lambda e: e.copy(out=u2T[:, hh * 4:(hh + 1) * 4, :],
                                                     in_=ps[4 + hh][:, :].rearrange("p (j t) -> p j t", j=4)),
                             r=[PK[4 + hh]], w=["u2TD"])
                    for j in range(8):
                        T.op("pe", lambda e: e.matmul(ps[6][:, 0:NE], lhsT=u2T[:, j, :], rhs=wr[:, j, :], start=(j == 0), stop=(j == 7)),
                             r=["u2TD", "wr"], w=[PK[6]])
                    T.op("dve", lambda e: e.tensor_reduce(out=st[:, 3:4], in_=ps[6][:, 0:NE], axis=AX.X, op=ALU.max), r=[PK[6], "stD"], w=["stD"])
                    T.op("dve", lambda e: e.tensor_scalar(out=st[:, 4:5], in0=st[:, 3:4], scalar1=-1.0, scalar2=None, op0=ALU.mult), r=["stD"], w=["stD"])
                    T.op("dve", lambda e: e.memset(st[:, 5:6], 0.0), r=["stD"], w=["stD"])
                    T.op("act", lambda e: e.activation(out=lg[:], in_=ps[6][:, 0:NE], func=AF.Exp, bias=st[:, 4:5], accum_out=st[:, 5:6]),
                         r=[PK[6], "stD"], w=["lgD", "stD"])
                    T.op("dve", lambda e: e.reciprocal(out=st[:, 6:7], in_=st[:, 5:6]), r=["stD"], w=["stD"])
                    T.op("dve", lambda e: e.tensor_scalar(out=aff_all[:, i, :], in0=lg[:], scalar1=st[:, 6:7], scalar2=None, op0=ALU.mult),
                         r=["lgD", "stD"], w=["aff_all"])
            T.barrier()
        if "aff" in dbg_d:
            T.dma("sp", lambda e: e.dma_start(out=dbg_d["aff"], in_=aff_all[:]), r=["aff_all"], w=["dbgaff"])

        if "E" in stages:
          idx_all = sb(es, "idx_all", [128, NE, 4], I32)
          g_all = sb(es, "g_all", [128, NE, 4])
          with ExitStack() as es2:
            affT = sb(es2, "affT", [NE, L])
            cmpj = sb(es2, "cmpj", [NE, L])
            bs = sb(es2, "bsE", [NE, 8])
            tri = sb(es2, "tri", [128, 128])
            onesf = sb(es2, "onesf", [128, 128])
            iota = sb(es2, "iota", [128, 512])
            tidhl = sb(es2, "tidhl", [128, NT, 2], BF16)
            thbc = sb(es2, "thbc", [128, NE])
            dg = sb(es2, "dgE", [NE, NE])
            M = sb(es2, "ME", [128, NT, NE])
            pos = sb(es2, "posE", [128, NT, NE])
            srun = sb(es2, "srunE", [128, NE])
            cum = sb(es2, "cumE", [128, NE])
            vals = sb(es2, "valsE", [128, NT, NE, 4], BF16)
            afh = sb(es2, "afhE", [128, NT, NE], BF16)
            afr = sb(es2, "afrE", [128, NT, NE])
            oh = [sb(es2, "ohE%d" % i, [128, 512], BF16) for i in range(4)]
            idf = sb(es2, "idfE", [128, 4])
            pvs = sb(es2, "pvsE", [128, 16])
            T.dma("sp", lambda e: e.dma_start(out=tri[:], in_=tri_d[:, :]), w=["tri"])
            T.dma("sp", lambda e: e.dma_start(out=iota[:], in_=iota_d[:, :]), w=["iota"])
            T.dma("sp", lambda e: e.dma_start(out=tidhl[:], in_=tidhl_d[:, :, :]), w=["tidhl"])
            T.op("pool", lambda e: e.memset(onesf[:], 1.0), w=["onesf"])
            for i in range(NT):
                pi = i // 4
                T.op("pe", lambda e: e.transpose(out=ps[pi][0:NE, (i % 4) * 128:(i % 4 + 1) * 128], in_=aff_all[:, i, :],
                                                 identity=ident_f[:]), r=["aff_all", "ident_f"], w=[PK[pi]])
                if i % 4 == 3:
                    T.op("act", lambda e: e.copy(out=affT[:, (i - 3) * 128:(i + 1) * 128], in_=ps[pi][0:NE, :]), r=[PK[pi]], w=["affT"])
            T.op("dve", lambda e: e.memset(bs[:, 0:1], 0.0), w=["bsE"])
            T.op("dve", lambda e: e.memset(bs[:, 1:2], 1.0), r=["bsE"], w=["bsE"])
            for it in range(32):
                T.op("dve", lambda e: e.tensor_scalar(out=bs[:, 2:3], in0=bs[:, 0:1], scalar1=bs[:, 1:2], scalar2=0.5, op0=ALU.add, op1=ALU.mult), r=["bsE"], w=["bsE"])
                T.op("dve", lambda e: e.tensor_scalar(out=cmpj[:], in0=affT[:], scalar1=bs[:, 2:3], scalar2=None, op0=ALU.is_ge), r=["affT", "bsE"], w=["cmpj"])
                T.op("dve", lambda e: e.tensor_reduce(out=bs[:, 3:4], in_=cmpj[:], axis=AX.X, op=ALU.add), r=["cmpj", "bsE"], w=["bsE"])
                T.op("dve", lambda e: e.tensor_scalar(out=bs[:, 4:5], in0=bs[:, 3:4], scalar1=CAP - 0.5, scalar2=None, op0=ALU.is_ge), r=["bsE"], w=["bsE"])
                T.op("dve", lambda e: e.tensor_tensor(out=bs[:, 5:6], in0=bs[:, 2:3], in1=bs[:, 0:1], op=ALU.subtract), r=["bsE"], w=["bsE"])
                T.op("dve", lambda e: e.tensor_tensor(out=bs[:, 6:7], in0=bs[:, 1:2], in1=bs[:, 2:3], op=ALU.subtract), r=["bsE"], w=["bsE"])
                T.op("dve", lambda e: e.scalar_tensor_tensor(out=bs[:, 0:1], in0=bs[:, 5:6], scalar=bs[:, 4:5], in1=bs[:, 0:1], op0=ALU.mult, op1=ALU.add), r=["bsE"], w=["bsE"])
                T.op("dve", lambda e: e.scalar_tensor_tensor(out=bs[:, 1:2], in0=bs[:, 6:7], scalar=bs[:, 4:5], in1=bs[:, 2:3], op0=ALU.mult, op1=ALU.add), r=["bsE"], w=["bsE"])
            T.op("dve", lambda e: e.tensor_scalar(out=dg[:], in0=ident_f[0:NE, 0:NE], scalar1=bs[:, 0:1], scalar2=None, op0=ALU.mult), r=["bsE", "ident_f"], w=["dgE"])
            T.op("pe", lambda e: e.matmul(ps[0][:, 0:NE], lhsT=onesf[0:NE, :], rhs=dg[:, :], start=True, stop=True), r=["onesf", "dgE"], w=[PK[0]])
            T.op("dve", lambda e: e.tensor_copy(out=thbc[:], in_=ps[0][:, 0:NE]), r=[PK[0]], w=["thbc"])
            T.op("dve", lambda e: e.tensor_tensor(out=M[:], in0=aff_all[:], in1=thbc[:, :].unsqueeze(1).broadcast_to([128, NT, NE]), op=ALU.is_ge),
                 r=["aff_all", "thbc"], w=["ME"])
            T.op("dve", lambda e: e.memset(srun[:], 0.0), w=["srunE"])
            for i in range(NT):
                pi = 1 + i % 2
                T.op("pe", lambda e: e.matmul(ps[pi][:, 0:NE], lhsT=tri[:, :], rhs=M[:, i, :], start=True, stop=True), r=["tri", "ME"], w=[PK[pi]])
                T.op("pe", lambda e: e.matmul(ps[pi][:, NE:2 * NE], lhsT=onesf[:, :], rhs=M[:, i, :], start=True, stop=True), r=["onesf", "ME"], w=[PK[pi]])
                T.op("dve", lambda e: e.tensor_tensor(out=cum[:], in0=ps[pi][:, 0:NE], in1=srun[:], op=ALU.add), r=[PK[pi], "srunE"], w=["cumE"])
                T.op("dve", lambda e: e.tensor_tensor(out=srun[:], in0=ps[pi][:, NE:2 * NE], in1=srun[:], op=ALU.add), r=[PK[pi], "srunE", "cumE"], w=["srunE"])
                T.op("dve", lambda e: e.tensor_tensor(out=cum[:], in0=cum[:], in1=M[:, i, :], op=ALU.mult), r=["cumE", "ME"], w=["cumE"])
                T.op("dve", lambda e: e.tensor_scalar(out=pos[:, i, :], in0=cum[:], scalar1=-1.0, scalar2=None, op0=ALU.add), r=["cumE"], w=["posE"])
            T.op("dve", lambda e: e.tensor_copy(out=afh[:], in_=aff_all[:]), r=["aff_all"], w=["afhE"])
            T.op("dve", lambda e: e.tensor_tensor(out=afr[:], in0=aff_all[:], in1=afh[:], op=ALU.subtract), r=["aff_all", "afhE"], w=["afrE"])
            T.op("dve", lambda e: e.tensor_copy(out=vals[:, :, :, 2], in_=afh[:]), r=["afhE"], w=["valsE"])
            T.op("dve", lambda e: e.tensor_copy(out=vals[:, :, :, 3], in_=afr[:]), r=["afrE", "valsE"], w=["valsE"])
            T.op("dve", lambda e: e.tensor_copy(out=vals[:, :, :, 0:2], in_=tidhl[:, :, :].unsqueeze(2).broadcast_to([128, NT, NE, 2])),
                 r=["tidhl", "valsE"], w=["valsE"])
            oc = 0
            for ex in range(NE):
                pi = 3 + ex % 2
                for i in range(NT):
                    ob = oc % 4
                    eng = "dve" if oc % 2 == 0 else "pool"
                    oc += 1
                    T.op(eng, lambda e: e.tensor_scalar(out=oh[ob][:], in0=iota[:], scalar1=pos[:, i, ex:ex + 1], scalar2=None, op0=ALU.is_equal),
                         r=["iota", "posE"], w=["ohE%d" % ob])
                    for sc in range(4):
                        T.op("pe", lambda e: e.matmul(ps[pi][:, sc * 4:(sc + 1) * 4], lhsT=oh[ob][:, sc * 128:(sc + 1) * 128], rhs=vals[:, i, ex, :],
                                                      start=(i == 0 and sc == 0), stop=(i == NT - 1), skip_group_check=True),
                             r=["ohE%d" % ob, "valsE"], w=[PK[pi]])
                T.op("dve", lambda e: e.tensor_copy(out=pvs[:], in_=ps[pi][:, 0:16]), r=[PK[pi]], w=["pvsE"])
                pv = pvs[:, :].rearrange("p (s f) -> p s f", f=4)
                T.op("dve", lambda e: e.scalar_tensor_tensor(out=idf[:], in0=pv[:, :, 0], scalar=64.0, in1=pv[:, :, 1], op0=ALU.mult, op1=ALU.add),
                     r=["pvsE"], w=["idfE"])
                T.op("dve", lambda e: e.tensor_copy(out=idx_all[:, ex, :], in_=idf[:]), r=["idfE"], w=["idx_all"])
                T.op("dve", lambda e: e.tensor_tensor(out=g_all[:, ex, :], in0=pv[:, :, 2], in1=pv[:, :, 3], op=ALU.add), r=["pvsE"], w=["g_all"])
            T.barrier()
          if "idx" in dbg_d:
            T.dma("sp", lambda e: e.dma_start(out=dbg_d["idx"], in_=idx_all[:]), r=["idx_all"], w=["dbgidx"])
            T.dma("sp", lambda e: e.dma_start(out=dbg_d["g"], in_=g_all[:]), r=["g_all"], w=["dbgg"])

          if "F" in stages:
            wst = [sb(es, "wstF%d" % i, [128, 4, D]) for i in range(3)]
            wgb = sb(es, "wgb", [128, 8, D], BF16)
            wub = sb(es, "wub", [128, 8, D], BF16)
            wdb = sb(es, "wdb", [128, 8, D], BF16)
            xg = [sb(es, "xgF%d" % i, [128, D], BF16) for i in range(4)]
            xinT = sb(es, "xinT", [128, 8, 512], BF16)
            sg = sb(es, "sgF", [128, 512])
            hT = sb(es, "hTF", [128, 8, 512], BF16)
            eo = [sb(es, "eoF%d" % i, [128, D]) for i in range(2)]
            psb = [ps[6][:, :].bitcast(BF16), ps[7][:, :].bitcast(BF16)]
            wc = 0
            for ex in range(NE):
                for (wsrc, wdst, wk) in ((wg_d, wgb, "wgb"), (wu_d, wub, "wub"), (wd_d, wdb, "wdb")):
                    for hh in range(2):
                        sbuf_ = wst[wc % 3]
                        sk = "wstF%d" % (wc % 3)
                        eng = "pool" if wc % 2 == 0 else "act"
                        wc += 1
                        T.dma("sp", lambda e: e.dma_start(out=sbuf_[:], in_=wsrc[ex].rearrange("(j p) c -> p j c", p=128)[:, hh * 4:(hh + 1) * 4, :]), w=[sk])
                        T.op(eng, lambda e: (e.tensor_copy if eng == "pool" else e.copy)(out=wdst[:, hh * 4:(hh + 1) * 4, :], in_=sbuf_[:]), r=[sk], w=[wk])
                for sc in range(4):
                    T.dma("pool", lambda e: e.indirect_dma_start(out=xg[sc][:], out_offset=None, in_=u2_d[:, :],
                                                                 in_offset=bass.IndirectOffsetOnAxis(ap=idx_all[:, ex, sc:sc + 1], axis=0)),
                          r=["idx_all", "u2_d"], w=["xgF%d" % sc])
                    for j in range(8):
                        T.op("pe", lambda e: e.transpose(out=psb[sc % 2][:, j * 128:(j + 1) * 128], in_=xg[sc][:, j * 128:(j + 1) * 128], identity=ident_b[:]),
                             r=["xgF%d" % sc, "ident_b"], w=[PK[6 + sc % 2]])
                    T.op("act", lambda e: e.copy(out=xinT[:, :, sc * 128:(sc + 1) * 128], in_=psb[sc % 2].rearrange("p (j t) -> p j t", j=8)),
                         r=[PK[6 + sc % 2]], w=["xinT"])
                for ft in range(8):
                    pg, pu = (ft % 2) * 2, (ft % 2) * 2 + 1
                    for j in range(8):
                        T.op("pe", lambda e: e.matmul(ps[pg][:, :], lhsT=wgb[:, j, ft * 128:(ft + 1) * 128], rhs=xinT[:, j, :], start=(j == 0), stop=(j == 7)),
                             r=["wgb", "xinT"], w=[PK[pg]])
                    for j in range(8):
                        T.op("pe", lambda e: e.matmul(ps[pu][:, :], lhsT=wub[:, j, ft * 128:(ft + 1) * 128], rhs=xinT[:, j, :], start=(j == 0), stop=(j == 7)),
                             r=["wub", "xinT"], w=[PK[pu]])
                    T.op("act", lambda e: e.activation(out=sg[:], in_=ps[pg][:, :], func=AF.Silu), r=[PK[pg]], w=["sgF"])
                    T.op("dve", lambda e: e.tensor_tensor(out=hT[:, ft, :], in0=ps[pu][:, :], in1=sg[:], op=ALU.mult), r=[PK[pu], "sgF"], w=["hTF"])
                for sc in range(4):
                    eb = eo[sc % 2]
                    for half in range(2):
                        po = 4 + half
                        for ft in range(8):
                            T.op("pe", lambda e: e.matmul(ps[po][:, :], lhsT=hT[:, ft, sc * 128:(sc + 1) * 128], rhs=wdb[:, ft, half * 512:(half + 1) * 512],
                                                          start=(ft == 0), stop=(ft == 7)), r=["hTF", "wdb"], w=[PK[po]])
                        T.op("dve", lambda e: e.tensor_scalar(out=eb[:, half * 512:(half + 1) * 512], in0=ps[po][:, :], scalar1=g_all[:, ex, sc:sc + 1],
                                                              scalar2=None, op0=ALU.mult), r=[PK[po], "g_all"], w=["eoF%d" % (sc % 2)])
                    T.dma("pool", lambda e: e.indirect_dma_start(out=out_d[:, :], out_offset=bass.IndirectOffsetOnAxis(ap=idx_all[:, ex, sc:sc + 1], axis=0),
                                                                 in_=eb[:], in_offset=None, compute_op=ALU.add),
                          r=["eoF%d" % (sc % 2), "idx_all"], w=["out_d"])
            T.barrier()
    for k, v in dbg_d.items():
        pass
    T.barrier()
    top.close()
    return nc


_CONST = None


def make_inputs(inp, b):
    global _CONST
    if _CONST is None:
        _CONST = host_consts()
    f = lambda a: np.ascontiguousarray(np.asarray(a, dtype=np.float32))
    m = dict(_CONST)
    m["x"] = f(inp["x"][b])
    m["g1p"] = f(inp["norm1_g"][0].reshape(8, 128).T)
    m["w_in"] = f(inp["w_in"][0])
    scw = np.concatenate([inp["short_conv_w"][0], inp["short_conv_b"][0][None, :]], axis=0)
    m["scw"] = f(scw.reshape(4, 12, 128).transpose(2, 1, 0))
    qkg = np.stack([inp["q_norm_g"][0], inp["k_norm_g"][0]], axis=0)
    m["qkg"] = f(np.broadcast_to(qkg[None], (128, 2, 64)))
    m["lamv"] = f(np.stack([inp["lambda_q1"][0], inp["lambda_k1"][0], inp["lambda_q2"][0], inp["lambda_k2"][0]])[None])
    m["subg"] = f(np.broadcast_to(inp["subln_g"][0][None, :], (128, 128)))
    m["fw1"] = f(inp["filt_w1"][0])
    m["fw2"] = f(inp["filt_w2"][0])
    m["fw3"] = f(inp["filt_w3"][0])
    m["fbf"] = f(np.stack([inp["filt_b1"][0], inp["filt_b2"][0], inp["filt_b3"][0], inp["filt_freq"][0]], axis=1))
    m["fwo"] = f(inp["filt_w_out"][0])
    m["skipbc"] = f(np.broadcast_to(inp["hyena_skip"][0][None], (32, 2, 512)))
    m["wpa"] = f(inp["w_branch_attn"][0])
    m["wph"] = f(inp["w_branch_hyena"][0])
    m["wo"] = f(inp["w_out"][0])
    m["g2bc"] = f(np.broadcast_to(inp["norm2_g"][0][None, :], (128, D)))
    m["wr"] = f(inp["w_router"][0].reshape(8, 128, 16).transpose(1, 0, 2))
    m["wg"] = f(inp["w_gate"][0])
    m["wu"] = f(inp["w_up"][0])
    m["wd"] = f(inp["w_down"][0])
    return m


def kernel(**inputs):
    nc = build()
    maps = [make_inputs(inputs, c % 4) for c in range(4)]
    in_maps = [maps[c % 4] for c in range(NCORES)]
    res = run_bass_kernel_spmd(nc, in_maps, core_ids=list(range(NCORES)))
    out = np.stack([np.asarray(res.results[b]["out"], dtype=np.float32) for b in range(4)], axis=0)
    return out
```

```python
import math
from contextlib import ExitStack
import numpy as np
import ml_dtypes
import concourse.bass as bass
import concourse.mybir as mybir
from concourse.bass_utils import run_bass_kernel_spmd

F32 = mybir.dt.float32
BF16 = mybir.dt.bfloat16
I32 = mybir.dt.int32
AF = mybir.ActivationFunctionType
ALU = mybir.AluOpType
AX = mybir.AxisListType

L = 4096
D = 1024
NT = 32
NCORES = 8
CAP = 512
NE = 16
GC = 32
DEBUG = False


class Trk:
    def __init__(s, nc):
        s.nc = nc
        s.eng = dict(pe=nc.tensor, act=nc.scalar, dve=nc.vector, pool=nc.gpsimd, sp=nc.sync)
        s.sem = {}
        s.cnt = {}
        for k in ("pe", "act", "dve", "pool"):
            s.sem[k] = nc.alloc_semaphore(name="s_" + k)
            s.cnt[k] = 0
        s.ND = 16
        for i in range(s.ND):
            k = "d%d" % i
            s.sem[k] = nc.alloc_semaphore(name="s_" + k)
            s.cnt[k] = 0
        s.rr = 0
        s.seen = {e: {} for e in s.eng}
        s.lw = {}
        s.rd = {}

    def _wait(s, e, deps):
        for k, v in deps.items():
            if e == "pe" and k == "pe":
                continue
            if s.seen[e].get(k, 0) < v:
                s.eng[e].wait_ge(s.sem[k], v)
                s.seen[e][k] = v

    def _deps(s, r, w):
        d = {}

        def add(k, v):
            if d.get(k, 0) < v:
                d[k] = v
        for x in r:
            for k, v in s.lw.get(x, {}).items():
                add(k, v)
        for x in w:
            for k, v in s.lw.get(x, {}).items():
                add(k, v)
            for k, v in s.rd.get(x, {}).items():
                add(k, v)
        return d

    def _upd(s, toks, r, w):
        if isinstance(toks, tuple):
            toks = [toks]
        for x in r:
            m = s.rd.setdefault(x, {})
            for tok in toks:
                if m.get(tok[0], 0) < tok[1]:
                    m[tok[0]] = tok[1]
        for x in w:
            m = {}
            for tok in toks:
                if m.get(tok[0], 0) < tok[1]:
                    m[tok[0]] = tok[1]
            s.lw[x] = m
            s.rd[x] = {}

    def op(s, e, fn, r=(), w=()):
        s._wait(e, s._deps(r, w))
        ins = fn(s.eng[e])
        s.cnt[e] += 1
        ins.then_inc(s.sem[e], 1)
        s._upd((e, s.cnt[e]), r, w)

    def dma(s, q, fn, r=(), w=()):
        s._wait(q, s._deps(r, w))
        ins = fn(s.eng[q])
        k = "d%d" % s.rr
        s.rr = (s.rr + 1) % s.ND
        s.cnt[k] += 16
        ins.then_inc(s.sem[k], 16)
        s._upd((k, s.cnt[k]), r, w)

    def dmas(s, q, fns, r=(), w=()):
        s._wait(q, s._deps(r, w))
        toks = []
        for fn in fns:
            ins = fn(s.eng[q])
            k = "d%d" % s.rr
            s.rr = (s.rr + 1) % s.ND
            s.cnt[k] += 16
            ins.then_inc(s.sem[k], 16)
            toks.append((k, s.cnt[k]))
        s._upd(toks, r, w)

    def barrier(s, engines=None):
        tot = {k: v for k, v in s.cnt.items() if v > 0}
        for e in (engines or s.eng):
            s._wait(e, dict(tot))
        s.lw = {}
        s.rd = {}


def _bf(a):
    return np.ascontiguousarray(a.astype(np.float32)).astype(ml_dtypes.bfloat16)


def host_consts():
    c = {}
    t = np.arange(L, dtype=np.float32)
    inv_freq = (np.float32(500000.0) ** (-np.arange(0, 16, 2, dtype=np.float32) / np.float32(16))).astype(np.float32)
    ang = (t[:, None] * inv_freq[None, :]).astype(np.float32)
    cs = np.concatenate([np.cos(ang), np.sin(ang)], axis=-1).astype(np.float32)
    c["ropecs"] = np.ascontiguousarray(cs.reshape(NT, 128, 16).transpose(1, 0, 2))
    tt = t / np.float32(L - 1)
    bands = np.linspace(1e-4, 15, 16, dtype=np.float32)
    a2 = (np.float32(2.0 * math.pi / L) * t[:, None] * bands[None, :]).astype(np.float32)
    emb = np.concatenate([tt[:, None], np.cos(a2), -np.sin(a2)], axis=-1).astype(np.float32)
    c["embT"] = np.ascontiguousarray(emb.T)
    c["posrow"] = np.ascontiguousarray(np.broadcast_to(tt[None, :], (128, L))).astype(np.float32)
    min_decay = math.log(1e-2) / 1.5
    max_decay = math.log(1e-2) / 0.3
    deltas = np.abs(np.linspace(min_decay, max_decay, 512, dtype=np.float32))
    c["ndelta"] = np.ascontiguousarray((-deltas).reshape(4, 128).T).astype(np.float32)
    n1 = np.arange(32)[:, None]
    k1 = np.arange(64)[None, :]
    th = 2 * np.pi * n1 * k1 / 64.0
    c["FA"] = _bf(np.concatenate([np.cos(th), -np.sin(th)], axis=1))
    n2 = np.arange(128)[:, None, None]
    k1 = np.arange(64)[None, :, None]
    k2 = np.arange(128)[None, None, :]
    th = 2 * np.pi * ((n2 * (k1 + 64 * k2)) % 8192) / 8192.0
    c["TGr"] = _bf(np.cos(th))
    c["TGi"] = _bf(-np.sin(th))
    c["TGn"] = _bf(np.sin(th))
    kk2 = np.arange(128)[:, None]
    nn2 = np.arange(128)[None, :]
    th = 2 * np.pi * ((kk2 * nn2) % 128) / 128.0
    fr, fi = np.cos(th), np.sin(th)
    c["R1"] = _bf(np.concatenate([fr, fi], axis=1))
    c["R2"] = _bf(np.concatenate([-fi, fr], axis=1))
    k1 = np.arange(64)[:, None, None]
    n2 = np.arange(128)[None, :, None]
    n1 = np.arange(32)[None, None, :]
    th = 2 * np.pi * ((k1 * (n2 + 128 * n1)) % 8192) / 8192.0
    th_ = np.stack([np.cos(th), -np.sin(th)], axis=2) / 8192.0
    c["TH"] = _bf(th_)
    tid = np.arange(L).reshape(NT, 128).T
    c["tidhl"] = _bf(np.stack([tid // 64, tid % 64], axis=-1))
    c["iota512"] = np.ascontiguousarray(np.broadcast_to(np.arange(512, dtype=np.float32)[None, :], (128, 512)))
    tri = (np.arange(128)[:, None] <= np.arange(128)[None, :]).astype(np.float32)
    c["tri"] = tri
    return c


def build(dbg=None):
    nc = bass.Bass("TRN2", target_bir_lowering=False)
    dbg = dbg or {}

    def din(name, shape, dt=F32):
        return nc.dram_tensor(name, list(shape), dt, kind="ExternalInput").ap()

    def dscr(name, shape, dt=F32):
        kind = "ExternalOutput" if name in dbg.get("_ext", ()) else "Internal"
        return nc.dram_tensor(name, list(shape), dt, kind=kind).ap()

    x_d = din("x", [L, D])
    g1p_d = din("g1p", [128, 8])
    w_in_d = din("w_in", [D, 5120])
    scw_d = din("scw", [128, 12, 4])
    qkg_d = din("qkg", [128, 2, 64])
    lam_d = din("lamv", [1, 4, 64])
    subg_d = din("subg", [128, 128])
    fw1_d = din("fw1", [33, 64])
    fw2_d = din("fw2", [64, 64])
    fw3_d = din("fw3", [64, 64])
    fbf_d = din("fbf", [64, 4])
    fwo_d = din("fwo", [64, 2048])
    skip_d = din("skipbc", [32, 2, 512])
    wpa_d = din("wpa", [512, D])
    wph_d = din("wph", [512, D])
    wo_d = din("wo", [D, D])
    g2bc_d = din("g2bc", [128, D])
    wr_d = din("wr", [128, 8, 16])
    wg_d = din("wg", [NE, D, D])
    wu_d = din("wu", [NE, D, D])
    wd_d = din("wd", [NE, D, D])
    ropecs_d = din("ropecs", [128, NT, 16])
    embT_d = din("embT", [33, L])
    posrow_d = din("posrow", [128, L])
    ndelta_d = din("ndelta", [128, 4])
    FA_d = din("FA", [32, 128], BF16)
    TGr_d = din("TGr", [128, 64, 128], BF16)
    TGi_d = din("TGi", [128, 64, 128], BF16)
    TGn_d = din("TGn", [128, 64, 128], BF16)
    R1_d = din("R1", [128, 256], BF16)
    R2_d = din("R2", [128, 256], BF16)
    TH_d = din("TH", [64, 128, 2, 32], BF16)
    tidhl_d = din("tidhl", [128, NT, 2], BF16)
    iota_d = din("iota512", [128, 512])
    tri_d = din("tri", [128, 128])

    out_d = nc.dram_tensor("out", [L, D], F32, kind="ExternalOutput").ap()

    qT_d = dscr("qT_s", [4, 128, L], BF16)
    kT_d = dscr("kT_s", [4, 128, L], BF16)
    v_d = dscr("v_s", [L, 512], BF16)
    hyc_d = dscr("hyc_s", [1536, L], F32)
    gateT_d = dscr("gateT_s", [2048, L], BF16)
    kern_d = dscr("kern_s", [2, 2, 512, L], BF16)
    attnT_d = dscr("attnT_s", [512, L], BF16)
    hyoT_d = dscr("hyoT_s", [512, L], BF16)
    u2_d = dscr("u2_s", [L, D], BF16)

    dbg_d = {}
    for k, (shape, dt) in ((k, v) for k, v in dbg.items() if not k.startswith("_")):
        dbg_d[k] = nc.dram_tensor("dbg_" + k, list(shape), dt, kind="ExternalOutput").ap()

    T = Trk(nc)
    top = ExitStack()

    sbc = [0]

    def sb(es, name, shape, dt=F32):
        sbc[0] += 1
        return es.enter_context(nc.sbuf_tensor("sb%d_%s" % (sbc[0], name), list(shape), dt))

    ps = [top.enter_context(nc.psum_tensor("ps%d" % i, [128, 512], F32)) for i in range(8)]
    PK = ["ps%d" % i for i in range(8)]

    ident_f = sb(top, "ident_f", [128, 128])
    ident_b = sb(top, "ident_b", [128, 128], BF16)
    eps = sb(top, "eps", [128, 1])
    T.op("pool", lambda e: e.memset(ident_f[:], 0.0), w=["ident_f"])
    T.op("pool", lambda e: e.affine_select(out=ident_f[:], in_=ident_f[:], pattern=[[-1, 128]],
                                           compare_op=ALU.not_equal, fill=1.0, base=0, channel_multiplier=1),
         r=["ident_f"], w=["ident_f"])
    T.op("pool", lambda e: e.tensor_copy(out=ident_b[:], in_=ident_f[:]), r=["ident_f"], w=["ident_b"])
    T.op("pool", lambda e: e.memset(eps[:], 1e-6), w=["eps"])

    stages = dbg.get("_stages", "AKBCDEF") if isinstance(dbg.get("_stages", None), str) else "AKBCDEF"

    if "A" in stages:
      with ExitStack() as es:
        uT = sb(es, "uT", [128, 8, L + 2], BF16)
        g1p = sb(es, "g1p", [128, 8])
        scw = sb(es, "scw", [128, 12, 4])
        qkg = sb(es, "qkg", [128, 2, 64])
        ropecs = sb(es, "ropecs", [128, NT, 16])
        xt = [sb(es, "xt%d" % i, [128, D]) for i in range(2)]
        xb = [sb(es, "xb%d" % i, [128, D], BF16) for i in range(2)]
        junk = sb(es, "junkA", [128, D])
        ss = sb(es, "ssA", [128, NT])
        rs = sb(es, "rsA", [128, NT])
        wf = sb(es, "wf", [128, 8, 512])
        wb = [sb(es, "wb%d" % i, [128, 8, 512], BF16) for i in range(2)]
        T.dma("sp", lambda e: e.dma_start(out=g1p[:], in_=g1p_d[:, :]), w=["g1p"])
        T.dma("sp", lambda e: e.dma_start(out=scw[:], in_=scw_d[:, :, :]), w=["scw"])
        T.dma("sp", lambda e: e.dma_start(out=qkg[:], in_=qkg_d[:, :, :]), w=["qkg"])
        T.dma("sp", lambda e: e.dma_start(out=ropecs[:], in_=ropecs_d[:, :, :]), w=["ropecs"])
        T.op("dve", lambda e: e.memset(ss[:], 0.0), w=["ssA"])
        T.op("dve", lambda e: e.memset(uT[:, :, 0:1], 0.0), w=["uTpad0"])
        T.op("dve", lambda e: e.memset(uT[:, :, L + 1:L + 2], 0.0), w=["uTpad1"])
        T.op("dve", lambda e: e.tensor_scalar(out=qkg[:, 0, :], in0=qkg[:, 0, :], scalar1=0.125, scalar2=None,
                                              op0=ALU.mult), r=["qkg"], w=["qkg"])
        psb = [ps[6][:, :].bitcast(BF16), ps[7][:, :].bitcast(BF16)]
        for i in range(NT):
            b = i % 2
            T.dma("sp", lambda e: e.dma_start(out=xt[b][:], in_=x_d[i * 128:(i + 1) * 128, :]), w=["xt%d" % b])
            T.op("act", lambda e: e.activation(out=junk[:], in_=xt[b][:], func=AF.Square,
                                               accum_out=ss[:, i:i + 1]), r=["xt%d" % b, "ssA"], w=["junkA", "ss%d" % i])
            T.op("act", lambda e: e.activation(out=rs[:, i:i + 1], in_=ss[:, i:i + 1], func=AF.Sqrt,
                                               scale=1.0 / D, bias=eps[:]), r=["ss%d" % i, "eps"], w=["rs%d" % i])
            T.op("dve", lambda e: e.reciprocal(out=rs[:, i:i + 1], in_=rs[:, i:i + 1]), r=["rs%d" % i], w=["rs%d" % i])
            T.op("dve", lambda e: e.tensor_scalar(out=xb[b][:], in0=xt[b][:], scalar1=rs[:, i:i + 1], scalar2=None,
                                                  op0=ALU.mult), r=["xt%d" % b, "rs%d" % i], w=["xb%d" % b])
            for j in range(8):
                T.op("pe", lambda e: e.transpose(out=psb[b][:, j * 128:(j + 1) * 128],
                                                 in_=xb[b][:, j * 128:(j + 1) * 128], identity=ident_b[:]),
                     r=["xb%d" % b, "ident_b"], w=[PK[6 + b]])
            T.op("act", lambda e: e.copy(out=uT[:, :, 1 + i * 128:1 + (i + 1) * 128],
                                         in_=psb[b].rearrange("p (j t) -> p j t", j=8)),
                 r=[PK[6 + b]], w=["uT%d" % i])
        UTK = ["uT%d" % i for i in range(NT)] + ["uTpad0", "uTpad1"]

        sq = sb(es, "sqA", [128, 512])
        ssq = sb(es, "ssqA", [128, 8])
        qn = sb(es, "qnA", [128, 8, 64])
        qb = sb(es, "qbA", [128, 512], BF16)
        rt = [sb(es, "rtA%d" % i, [128, 8, 8]) for i in range(4)]
        qTs = [sb(es, "qTs%d" % i, [128, 4, 128], BF16) for i in range(2)]
        vst = [sb(es, "vst%d" % i, [128, 512], BF16) for i in range(2)]
        hraw = sb(es, "hraw", [128, L + 2])
        hc = sb(es, "hcA", [128, L])
        gst = sb(es, "gstA", [128, L], BF16)
        T.op("pool", lambda e: e.memset(hraw[:, 0:1], 0.0), w=["hrawp0"])
        T.op("pool", lambda e: e.memset(hraw[:, L + 1:L + 2], 0.0), w=["hrawp1"])
        pcnt = [0]

        def nextps():
            pcnt[0] += 1
            return pcnt[0] % 4

        for cc in dbg.get('_ccs', range(10)):
            wbk = "wb%d" % (cc % 2)
            wbt = wb[cc % 2]
            T.dma("sp", lambda e: e.dma_start(
                out=wf[:], in_=w_in_d.rearrange("(j p) c -> p j c", p=128)[:, :, cc * 512:(cc + 1) * 512]), w=["wf"])
            for j in range(8):
                T.op("pool", lambda e: e.tensor_scalar(out=wbt[:, j, :], in0=wf[:, j, :], scalar1=g1p[:, j:j + 1],
                                                       scalar2=None, op0=ALU.mult), r=["wf", "g1p"], w=[wbk])
            if cc < 3:
                for i in range(NT):
                    pi = nextps()
                    for j in range(8):
                        T.op("pe", lambda e: e.matmul(ps[pi][:, :], lhsT=uT[:, j, 1 + i * 128:1 + (i + 1) * 128],
                                                      rhs=wbt[:, j, :], start=(j == 0), stop=(j == 7)),
                             r=["uT%d" % i, wbk], w=[PK[pi]])
                    if cc == 2:
                        vs = vst[i % 2]
                        T.op("act", lambda e: e.copy(out=vs[:], in_=ps[pi][:, :]), r=[PK[pi]], w=["vst%d" % (i % 2)])
                        T.dma("sp", lambda e: e.dma_start(out=v_d[i * 128:(i + 1) * 128, :], in_=vs[:]),
                              r=["vst%d" % (i % 2)], w=["v_d"])
                        continue
                    T.op("act", lambda e: e.activation(out=sq[:], in_=ps[pi][:, :], func=AF.Square), r=[PK[pi]], w=["sqA"])
                    T.op("dve", lambda e: e.tensor_reduce(out=ssq[:], in_=sq[:].rearrange("p (g d) -> p g d", d=64),
                                                          axis=AX.X, op=ALU.add), r=["sqA"], w=["ssqA"])
                    T.op("act", lambda e: e.activation(out=ssq[:], in_=ssq[:], func=AF.Sqrt, scale=1.0 / 64,
                                                       bias=eps[:]), r=["ssqA", "eps"], w=["ssqA"])
                    T.op("dve", lambda e: e.reciprocal(out=ssq[:], in_=ssq[:]), r=["ssqA"], w=["ssqA"])
                    T.op("dve", lambda e: e.tensor_tensor(out=qn[:], in0=ps[pi][:, :].rearrange("p (g d) -> p g d", d=64),
                                                          in1=ssq[:, :].unsqueeze(2).broadcast_to([128, 8, 64]),
                                                          op=ALU.mult), r=[PK[pi], "ssqA"], w=["qnA"])
                    T.op("dve", lambda e: e.tensor_tensor(out=qn[:], in0=qn[:],
                                                          in1=qkg[:, cc, :].unsqueeze(1).broadcast_to([128, 8, 64]),
                                                          op=ALU.mult), r=["qnA", "qkg"], w=["qnA"])
                    T.op("act", lambda e: e.copy(out=qb[:].rearrange("p (g d) -> p g d", d=64), in_=qn[:]),
                         r=["qnA"], w=["qbA"])
                    cosb = ropecs[:, i, 0:8].unsqueeze(1).broadcast_to([128, 8, 8])
                    sinb = ropecs[:, i, 8:16].unsqueeze(1).broadcast_to([128, 8, 8])
                    r1 = qn[:, :, 0:8]
                    r2 = qn[:, :, 8:16]
                    T.op("dve", lambda e: e.tensor_tensor(out=rt[0][:], in0=r1, in1=cosb, op=ALU.mult), r=["qnA", "ropecs"], w=["rt0"])
                    T.op("dve", lambda e: e.tensor_tensor(out=rt[1][:], in0=r2, in1=sinb, op=ALU.mult), r=["qnA", "ropecs"], w=["rt1"])
                    T.op("dve", lambda e: e.tensor_tensor(out=rt[2][:], in0=r2, in1=cosb, op=ALU.mult), r=["qnA", "ropecs"], w=["rt2"])
                    T.op("dve", lambda e: e.tensor_tensor(out=rt[3][:], in0=r1, in1=sinb, op=ALU.mult), r=["qnA", "ropecs"], w=["rt3"])
                    qbv = qb[:].rearrange("p (g d) -> p g d", d=64)
                    T.op("dve", lambda e: e.tensor_tensor(out=qbv[:, :, 0:8], in0=rt[0][:], in1=rt[1][:], op=ALU.subtract),
                         r=["rt0", "rt1"], w=["qbA"])
                    T.op("dve", lambda e: e.tensor_tensor(out=qbv[:, :, 8:16], in0=rt[2][:], in1=rt[3][:], op=ALU.add),
                         r=["rt2", "rt3"], w=["qbA"])
                    pb = 6 + (i % 2)
                    for h in range(4):
                        T.op("pe", lambda e: e.transpose(out=psb[i % 2][:, h * 128:(h + 1) * 128],
                                                         in_=qb[:, h * 128:(h + 1) * 128], identity=ident_b[:]),
                             r=["qbA", "ident_b"], w=[PK[pb]])
                    st = qTs[i % 2]
                    T.op("act", lambda e: e.copy(out=st[:], in_=psb[i % 2][:, 0:512].rearrange("p (h t) -> p h t", h=4)),
                         r=[PK[pb]], w=["qTs%d" % (i % 2)])
                    dst = (qT_d if cc == 0 else kT_d).rearrange("h p t -> p h t")[:, :, i * 128:(i + 1) * 128]
                    T.dma("sp", lambda e: e.dma_start(out=dst, in_=st[:]), r=["qTs%d" % (i % 2)], w=["qkT_d"])
            else:
                for ct in range(4):
                    for tq in range(8):
                        pi = nextps()
                        for j in range(8):
                            T.op("pe", lambda e: e.matmul(ps[pi][:, :], lhsT=wbt[:, j, ct * 128:(ct + 1) * 128],
                                                          rhs=uT[:, j, 1 + tq * 512:1 + (tq + 1) * 512],
                                                          start=(j == 0), stop=(j == 7)),
                                 r=UTK[4 * tq:4 * tq + 4] + [wbk], w=[PK[pi]])
                        if cc < 6:
                            T.op("act", lambda e: e.copy(out=hraw[:, 1 + tq * 512:1 + (tq + 1) * 512], in_=ps[pi][:, :]),
                                 r=[PK[pi]], w=["hraw"])
                        else:
                            T.op("act", lambda e: e.activation(out=gst[:, tq * 512:(tq + 1) * 512], in_=ps[pi][:, :],
                                                               func=AF.Sigmoid), r=[PK[pi]], w=["gstA"])
                    if cc < 6:
                        cti = (cc - 3) * 4 + ct
                        lvl = dbg.get('_lvl', 9)
                        if lvl >= 1:
                          T.op("act", lambda e: e.activation(out=hc[:], in_=hraw[:, 1:L + 1], func=AF.Identity,
                                                           scale=scw[:, cti, 1:2], bias=scw[:, cti, 3:4]),
                             r=["hraw", "scw"], w=["hcA"])
                        if lvl >= 2:
                          T.op("dve", lambda e: e.scalar_tensor_tensor(out=hc[:], in0=hraw[:, 0:L], scalar=scw[:, cti, 0:1],
                                                                     in1=hc[:], op0=ALU.mult, op1=ALU.add),
                             r=["hraw", "hrawp0", "scw", "hcA"], w=["hcA"])
                          T.op("dve", lambda e: e.scalar_tensor_tensor(out=hc[:], in0=hraw[:, 2:L + 2], scalar=scw[:, cti, 2:3],
                                                                     in1=hc[:], op0=ALU.mult, op1=ALU.add),
                             r=["hraw", "hrawp1", "scw", "hcA"], w=["hcA"])
                        if lvl >= 3:
                          for q4 in range(4):
                              T.dma("sp", lambda e: e.dma_start(out=hyc_d[cti * 128:(cti + 1) * 128, q4 * 1024:(q4 + 1) * 1024],
                                                                in_=hc[:, q4 * 1024:(q4 + 1) * 1024]),
                                    r=["hcA"], w=["hyc_d%d" % q4])
                    else:
                        gti = (cc - 6) * 4 + ct
                        T.dmas("sp", [(lambda e, a=a: e.dma_start(out=gateT_d[gti * 128:(gti + 1) * 128, a:a + 2048], in_=gst[:, a:a + 2048]))
                                      for a in (0, 2048)], r=["gstA"], w=["gateT_d"])
        T.barrier()

    if "K" in stages:
      with ExitStack() as es:
        embT = sb(es, "embT", [33, L])
        fw1 = sb(es, "fw1", [33, 64])
        fw2 = sb(es, "fw2", [64, 64])
        fw3 = sb(es, "fw3", [64, 64])
        fbf = sb(es, "fbf", [64, 4])
        frb = sb(es, "frb", [64, 3])
        fwo = sb(es, "fwo", [64, 2048])
        posrow = sb(es, "posrow", [128, L])
        ndelta = sb(es, "ndelta", [128, 4])
        hid = [sb(es, "hid%d" % i, [64, L]) for i in range(2)]
        hidb = sb(es, "hidb", [64, L])
        dec = sb(es, "decK", [128, L])
        kf = sb(es, "kfK", [128, L])
        kb = sb(es, "kbK", [128, L])
        hp = sb(es, "hpK", [128, L], BF16)
        hm = sb(es, "hmK", [128, L], BF16)
        for tns, src, k in ((fw1, fw1_d, "fw1"), (fw2, fw2_d, "fw2"), (fw3, fw3_d, "fw3"),
                            (fbf, fbf_d, "fbf"), (ndelta, ndelta_d, "ndelta")):
            T.dma("sp", lambda e: e.dma_start(out=tns[:], in_=src), w=[k])
        for tns, src, k, n in ((embT, embT_d, "embT", L), (fwo, fwo_d, "fwo", 2048), (posrow, posrow_d, "posrow", L)):
            T.dmas("sp", [(lambda e, a=a: e.dma_start(out=tns[:, a:a + 1024], in_=src[:, a:a + 1024]))
                          for a in range(0, n, 1024)], w=[k])
        for l in range(3):
            T.op("dve", lambda e: e.tensor_tensor(out=frb[:, l:l + 1], in0=fbf[:, l:l + 1], in1=fbf[:, 3:4], op=ALU.mult),
                 r=["fbf"], w=["frb"])
        TWO_PI = 2.0 * math.pi
        srcs = [(embT, "embT", 33, fw1, "fw1"), (hid[0], "hid0", 64, fw2, "fw2"), (hid[1], "hid1", 64, fw3, "fw3")]
        outs = [(hid[0], "hid0"), (hid[1], "hid1"), (hid[0], "hid0")]
        negpi = sb(es, "negpi", [64, 1])
        T.op("dve", lambda e: e.memset(negpi[:], 4.0 * math.pi), w=["negpi"])
        kcnt = sb(es, "kcntK", [64, L])
        for l in range(3):
            src, sk, kk, w_, wk = srcs[l]
            dst, dk = outs[l]
            for tq in range(8):
                pi = tq % 4
                T.op("pe", lambda e: e.matmul(ps[pi][0:64, :], lhsT=w_[0:kk, :], rhs=src[0:kk, tq * 512:(tq + 1) * 512],
                                              start=True, stop=True), r=[sk, wk], w=[PK[pi]])
                T.op("dve", lambda e: e.tensor_scalar(out=hidb[:, tq * 512:(tq + 1) * 512], in0=ps[pi][0:64, :],
                                                      scalar1=fbf[:, 3:4], scalar2=frb[:, l:l + 1], op0=ALU.mult,
                                                      op1=ALU.add), r=[PK[pi], "fbf", "frb"], w=["hidb"])
                xs_ = hidb[:, tq * 512:(tq + 1) * 512]
                ks_ = kcnt[:, tq * 512:(tq + 1) * 512]
                T.op("dve", lambda e: e.tensor_scalar(out=ks_, in0=xs_, scalar1=-3.0 * math.pi, scalar2=None, op0=ALU.is_gt),
                     r=["hidb"], w=["kcnt"])
                for thr in (-math.pi, math.pi, 3.0 * math.pi):
                    T.op("dve", lambda e: e.scalar_tensor_tensor(out=ks_, in0=xs_, scalar=thr, in1=ks_, op0=ALU.is_gt, op1=ALU.add),
                         r=["hidb", "kcnt"], w=["kcnt"])
                T.op("dve", lambda e: e.scalar_tensor_tensor(out=xs_, in0=ks_, scalar=-TWO_PI, in1=xs_, op0=ALU.mult, op1=ALU.add),
                     r=["hidb", "kcnt"], w=["hidb"])
                T.op("act", lambda e: e.activation(out=dst[:, tq * 512:(tq + 1) * 512], in_=xs_,
                                                   func=AF.Sin, bias=negpi[:]), r=["hidb", "negpi"], w=[dk])
        h3 = hid[0]
        for ct in range(4):
            T.op("act", lambda e: e.activation(out=dec[:], in_=posrow[:], func=AF.Exp, scale=ndelta[:, ct:ct + 1]),
                 r=["posrow", "ndelta"], w=["decK"])
            for o in range(2):
                for d_, (dst, dk) in enumerate(((kf, "kfK"), (kb, "kbK"))):
                    col = (o * 2 + d_) * 512 + ct * 128
                    for tq in range(8):
                        pi = tq % 4
                        T.op("pe", lambda e: e.matmul(ps[pi][:, :], lhsT=fwo[:, col:col + 128],
                                                      rhs=h3[:, tq * 512:(tq + 1) * 512], start=True, stop=True),
                             r=["hid0", "fwo"], w=[PK[pi]])
                        T.op("dve", lambda e: e.tensor_tensor(out=dst[:, tq * 512:(tq + 1) * 512], in0=ps[pi][:, :],
                                                              in1=dec[:, tq * 512:(tq + 1) * 512], op=ALU.mult),
                             r=[PK[pi], "decK"], w=[dk])
                T.op("dve", lambda e: e.memset(kb[:, 0:1], 0.0), r=["kbK"], w=["kbK"])
                T.op("pool", lambda e: e.tensor_tensor(out=hp[:], in0=kf[:], in1=kb[:], op=ALU.add), r=["kfK", "kbK"], w=["hpK"])
                T.op("pool", lambda e: e.tensor_tensor(out=hm[:], in0=kf[:], in1=kb[:], op=ALU.subtract), r=["kfK", "kbK"], w=["hmK"])
                T.dmas("sp", [(lambda e, a=a: e.dma_start(out=kern_d[o, 0, ct * 128:(ct + 1) * 128, a:a + 2048], in_=hp[:, a:a + 2048]))
                              for a in range(0, L, 2048)], r=["hpK"], w=["kern_d0"])
                T.dmas("sp", [(lambda e, a=a: e.dma_start(out=kern_d[o, 1, ct * 128:(ct + 1) * 128, a:a + 2048], in_=hm[:, a:a + 2048]))
                              for a in range(0, L, 2048)], r=["hmK"], w=["kern_d1"])
        T.barrier()

    if "B" in stages:
      with ExitStack() as es:
        qT = sb(es, "qT", [128, 4, L], BF16)
        kT = sb(es, "kT", [128, 2, 4, L], BF16)
        V = sb(es, "Vsb", [128, NT, 4, 132], BF16)
        lamv = sb(es, "lamv", [1, 4, 64])
        lamt = sb(es, "lamt", [1, 8])
        ones1 = sb(es, "ones1", [1, 128])
        nlam = sb(es, "nlam", [128, 1])
        subg = sb(es, "subg", [128, 128])
        E = [sb(es, "E%d" % i, [128, 512], BF16) for i in range(3)]
        aacc = sb(es, "aacc", [128, 128])
        ab = sb(es, "abB", [128, 128], BF16)
        junkb = sb(es, "junkB", [128, 128])
        sm = sb(es, "smB", [128, 8])
        aTs = [sb(es, "aTs%d" % i, [128, 512], BF16) for i in range(2)]
        T.dmas("sp", [(lambda e, h=h, a=a: e.dma_start(out=qT[:, h, a:a + 2048], in_=qT_d[h, :, a:a + 2048]))
                      for h in range(4) for a in (0, 2048)], w=["qT"])
        T.op("pool", lambda e: e.memset(kT[64:128, 0, :, :], 0.0), w=["kTz0"])
        T.op("dve", lambda e: e.memset(kT[0:64, 1, :, :], 0.0), w=["kTz1"])
        T.dmas("sp", [(lambda e, h=h, a=a, m=m: e.dma_start(out=kT[m * 64:(m + 1) * 64, m, h, a:a + 2048],
                                                            in_=kT_d[h, m * 64:(m + 1) * 64, a:a + 2048]))
                      for m in range(2) for h in range(4) for a in (0, 2048)], w=["kT"])
        T.dmas("sp", [(lambda e, i=i: e.dma_start(out=V[:, i, :, 0:128],
                                                  in_=v_d[i * 128:(i + 1) * 128, :].rearrange("p (h d) -> p h d", d=128)))
                      for i in range(NT)], w=["V"])
        T.op("pool", lambda e: e.memset(V[:, :, :, 128:129], 1.0), w=["Vones"])
        T.dma("sp", lambda e: e.dma_start(out=lamv[:], in_=lam_d[:, :, :]), w=["lamv"])
        T.dma("sp", lambda e: e.dma_start(out=subg[:], in_=subg_d[:, :]), w=["subg"])
        T.op("dve", lambda e: e.tensor_scalar(out=subg[:], in0=subg[:], scalar1=0.8, scalar2=None, op0=ALU.mult),
             r=["subg"], w=["subg"])
        T.op("dve", lambda e: e.memset(ones1[:], 1.0), w=["ones1"])
        T.op("dve", lambda e: e.memset(lamt[:], 0.0), w=["lamt"])
        T.op("dve", lambda e: e.tensor_tensor(out=lamv[:, 0, :], in0=lamv[:, 0, :], in1=lamv[:, 1, :], op=ALU.mult), r=["lamv"], w=["lamv"])
        T.op("dve", lambda e: e.tensor_tensor(out=lamv[:, 2, :], in0=lamv[:, 2, :], in1=lamv[:, 3, :], op=ALU.mult), r=["lamv"], w=["lamv"])
        T.op("dve", lambda e: e.tensor_reduce(out=lamt[:, 0:1], in_=lamv[:, 0, :], axis=AX.X, op=ALU.add), r=["lamv", "lamt"], w=["lamt"])
        T.op("dve", lambda e: e.tensor_reduce(out=lamt[:, 1:2], in_=lamv[:, 2, :], axis=AX.X, op=ALU.add), r=["lamv", "lamt"], w=["lamt"])
        T.op("act", lambda e: e.activation(out=lamt[:, 2:4], in_=lamt[:, 0:2], func=AF.Exp), r=["lamt"], w=["lamt"])
        T.op("dve", lambda e: e.tensor_tensor(out=lamt[:, 4:5], in0=lamt[:, 3:4], in1=lamt[:, 2:3], op=ALU.subtract), r=["lamt"], w=["lamt"])
        T.op("dve", lambda e: e.tensor_scalar(out=lamt[:, 5:6], in0=lamt[:, 4:5], scalar1=-0.2, scalar2=None, op0=ALU.add), r=["lamt"], w=["lamt"])
        T.op("pe", lambda e: e.matmul(ps[7][:, 0:1], lhsT=ones1[:, :], rhs=lamt[:, 5:6], start=True, stop=True),
             r=["ones1", "lamt"], w=[PK[7]])
        T.op("dve", lambda e: e.tensor_copy(out=nlam[:], in_=ps[7][:, 0:1]), r=[PK[7]], w=["nlam"])
        psT = ps[6][:, :].bitcast(BF16)
        ec = 0
        for h in range(4):
            for qc in range(8):
                for m in range(2):
                    pr = slice(m * 64, (m + 1) * 64)
                    for kt in range(NT):
                        sp_ = kt % 2
                        T.op("pe", lambda e: e.matmul(ps[sp_][:, :], lhsT=kT[:, m, h, kt * 128:(kt + 1) * 128],
                                                      rhs=qT[:, h, qc * 512:(qc + 1) * 512], start=True, stop=True),
                             r=["kT", "kTz0", "kTz1", "qT"], w=[PK[sp_]])
                        eb = ec % 3
                        ec += 1
                        T.op("act", lambda e: e.activation(out=E[eb][:], in_=ps[sp_][:, :], func=AF.Exp),
                             r=[PK[sp_]], w=["E%d" % eb])
                        for qs in range(4):
                            bank = 2 + m * 2 + qs // 2
                            T.op("pe", lambda e: e.matmul(ps[bank][:, (qs % 2) * 256:(qs % 2) * 256 + 129],
                                                          lhsT=E[eb][:, qs * 128:(qs + 1) * 128], rhs=V[:, kt, h, 0:129],
                                                          start=(kt == 0 and qs % 2 == 0), stop=(kt == NT - 1),
                                                          skip_group_check=True),
                                 r=["E%d" % eb, "V", "Vones"], w=[PK[bank]])
                for qs in range(4):
                    o1 = ps[2 + qs // 2][:, (qs % 2) * 256:(qs % 2) * 256 + 129]
                    o2 = ps[4 + qs // 2][:, (qs % 2) * 256:(qs % 2) * 256 + 129]
                    k1_, k2_ = PK[2 + qs // 2], PK[4 + qs // 2]
                    T.op("dve", lambda e: e.reciprocal(out=sm[:, 0:1], in_=o1[:, 128:129]), r=[k1_], w=["smB"])
                    T.op("dve", lambda e: e.reciprocal(out=sm[:, 1:2], in_=o2[:, 128:129]), r=[k2_, "smB"], w=["smB"])
                    T.op("dve", lambda e: e.tensor_tensor(out=sm[:, 2:3], in0=sm[:, 1:2], in1=nlam[:], op=ALU.mult),
                         r=["smB", "nlam"], w=["smB"])
                    T.op("dve", lambda e: e.tensor_scalar(out=aacc[:], in0=o1[:, 0:128], scalar1=sm[:, 0:1], scalar2=None,
                                                          op0=ALU.mult), r=[k1_, "smB"], w=["aacc"])
                    T.op("dve", lambda e: e.scalar_tensor_tensor(out=aacc[:], in0=o2[:, 0:128], scalar=sm[:, 2:3], in1=aacc[:],
                                                                 op0=ALU.mult, op1=ALU.add), r=[k2_, "smB", "aacc"], w=["aacc"])
                    T.op("dve", lambda e: e.memset(sm[:, 3:4], 0.0), r=["smB"], w=["smB"])
                    T.op("act", lambda e: e.activation(out=junkb[:], in_=aacc[:], func=AF.Square, accum_out=sm[:, 3:4]),
                         r=["aacc", "smB"], w=["junkB", "smB"])
                    T.op("act", lambda e: e.activation(out=sm[:, 4:5], in_=sm[:, 3:4], func=AF.Sqrt, scale=1.0 / 128,
                                                       bias=eps[:]), r=["smB", "eps"], w=["smB"])
                    T.op("dve", lambda e: e.reciprocal(out=sm[:, 5:6], in_=sm[:, 4:5]), r=["smB"], w=["smB"])
                    T.op("dve", lambda e: e.scalar_tensor_tensor(out=ab[:], in0=aacc[:], scalar=sm[:, 5:6], in1=subg[:],
                                                                 op0=ALU.mult, op1=ALU.mult), r=["aacc", "smB", "subg"], w=["abB"])
                    T.op("pe", lambda e: e.transpose(out=psT[:, qs * 128:(qs + 1) * 128], in_=ab[:], identity=ident_b[:]),
                         r=["abB", "ident_b"], w=[PK[6]])
                st = aTs[qc % 2]
                T.op("act", lambda e: e.copy(out=st[:], in_=psT[:, 0:512]), r=[PK[6]], w=["aTs%d" % (qc % 2)])
                T.dma("sp", lambda e: e.dma_start(out=attnT_d[h * 128:(h + 1) * 128, qc * 512:(qc + 1) * 512], in_=st[:]),
                      r=["aTs%d" % (qc % 2)], w=["attnT_d"])
        T.barrier()

    if "C" in stages:
      with ExitStack() as es:
        FA = sb(es, "FA", [32, 128], BF16)
        TGr = sb(es, "TGr", [128, 64, 128], BF16)
        TGi = sb(es, "TGi", [128, 64, 128], BF16)
        TGn = sb(es, "TGn", [128, 64, 128], BF16)
        R1 = sb(es, "R1", [128, 256], BF16)
        R2 = sb(es, "R2", [128, 256], BF16)
        TH = sb(es, "TH", [64, 128, 2, 32], BF16)
        skipbc = sb(es, "skipbc", [32, 2, GC])
        for tns, src, k in ((FA, FA_d, "FA"), (R1, R1_d, "R1"), (R2, R2_d, "R2")):
            T.dma("sp", lambda e: e.dma_start(out=tns[:], in_=src), w=[k])
        for tns, src, k in ((TGr, TGr_d, "TGr"), (TGi, TGi_d, "TGi"), (TGn, TGn_d, "TGn")):
            T.dmas("sp", [(lambda e, a=a: e.dma_start(out=tns[:, a:a + 16, :], in_=src[:, a:a + 16, :])) for a in range(0, 64, 16)], w=[k])
        T.dmas("sp", [(lambda e, a=a: e.dma_start(out=TH[:, a:a + 32, :, :], in_=TH_d[:, a:a + 32, :, :])) for a in range(0, 128, 32)], w=["TH"])
        zf = sb(es, "zf", [32, GC, 128])
        gt = sb(es, "gtC", [32, GC, 128])
        zs = sb(es, "zsC", [32, GC, 128])
        srcb = [sb(es, "srcb%d" % i, [32, GC, 128], BF16) for i in range(3)]
        Bm_ = [sb(es, "Bm%d" % i, [128, 128, GC], BF16) for i in range(3)]
        srs = sb(es, "srsC", [128, 512])
        sis = sb(es, "sisC", [128, 512])
        tm = [sb(es, "tmC%d" % i, [128, 512]) for i in range(4)]
        Y = sb(es, "YC", [128, GC, 2, 64], BF16)
        Dm = sb(es, "DmC", [64, 256, GC], BF16)
        tmpy = sb(es, "tmpyC", [32, GC, 16])
        hob = srcb[0]

        def flay(ap2d):
            return ap2d.rearrange("c (a b) -> a c b", b=128)

        for g in range(512 // GC):
            c0 = g * GC
            T.dma("sp", lambda e: e.dma_start(out=zf[:], in_=flay(hyc_d[c0:c0 + GC, :])), w=["zf"])
            T.dma("sp", lambda e: e.dma_start(out=skipbc[:], in_=skip_d[:, :, c0:c0 + GC]), w=["skipbc"])
            for o in range(2):
                T.dma("sp", lambda e: e.dma_start(out=gt[:], in_=flay(hyc_d[512 * (o + 1) + c0:512 * (o + 1) + c0 + GC, :])),
                      w=["gtC"])
                T.op("act", lambda e: e.copy(out=srcb[0][:], in_=zf[:]), r=["zf"], w=["srcb0"])
                for pm in range(2):
                    T.dma("sp", lambda e: e.dma_start(out=srcb[1 + pm][:], in_=flay(kern_d[o, pm, c0:c0 + GC, :])),
                          w=["srcb%d" % (1 + pm)])
                T.op("pool", lambda e: e.tensor_tensor(out=zs[:], in0=zf[:],
                                                       in1=skipbc[:, o, :].unsqueeze(2).broadcast_to([32, GC, 128]),
                                                       op=ALU.mult), r=["zf", "skipbc"], w=["zsC"])
                for s_ in range(3):
                    for cq in range(GC // 4):
                        pi = cq % 2
                        for c4 in range(4):
                            c = cq * 4 + c4
                            T.op("pe", lambda e: e.matmul(ps[pi][:, c4 * 128:(c4 + 1) * 128], lhsT=srcb[s_][:, c, :],
                                                          rhs=FA[:, :], start=True, stop=True),
                                 r=["srcb%d" % s_, "FA"], w=[PK[pi]])
                        T.op("act" if cq % 2 == 0 else "dve",
                             lambda e: (e.copy if cq % 2 == 0 else e.tensor_copy)(
                                 out=Bm_[s_][:, :, cq * 4:(cq + 1) * 4].rearrange("p f c -> p c f"),
                                 in_=ps[pi][:, :].rearrange("p (c f) -> p c f", c=4)),
                             r=[PK[pi]], w=["Bm%d" % s_])
                for kc in range(4):
                    bZr, bZi, bSr, bSi = (2, 3, 4, 5) if kc % 2 == 0 else (0, 1, 6, 7)
                    for j in range(16):
                        k1 = kc * 16 + j
                        cs_ = slice(j * GC, (j + 1) * GC)
                        bz, bp, bm = Bm_[0], Bm_[1], Bm_[2]
                        T.op("pe", lambda e: e.matmul(ps[bZr][:, cs_], lhsT=TGr[:, k1, :], rhs=bz[:, k1, :], start=True, stop=False), r=["TGr", "Bm0"], w=[PK[bZr]])
                        T.op("pe", lambda e: e.matmul(ps[bZr][:, cs_], lhsT=TGn[:, k1, :], rhs=bz[:, 64 + k1, :], start=False, stop=True), r=["TGn", "Bm0"], w=[PK[bZr]])
                        T.op("pe", lambda e: e.matmul(ps[bZi][:, cs_], lhsT=TGi[:, k1, :], rhs=bz[:, k1, :], start=True, stop=False), r=["TGi", "Bm0"], w=[PK[bZi]])
                        T.op("pe", lambda e: e.matmul(ps[bZi][:, cs_], lhsT=TGr[:, k1, :], rhs=bz[:, 64 + k1, :], start=False, stop=True), r=["TGr", "Bm0"], w=[PK[bZi]])
                        T.op("pe", lambda e: e.matmul(ps[bSr][:, cs_], lhsT=TGr[:, k1, :], rhs=bp[:, k1, :], start=True, stop=False), r=["TGr", "Bm1"], w=[PK[bSr]])
                        T.op("pe", lambda e: e.matmul(ps[bSr][:, cs_], lhsT=TGn[:, k1, :], rhs=bp[:, 64 + k1, :], start=False, stop=True), r=["TGn", "Bm1"], w=[PK[bSr]])
                        T.op("pe", lambda e: e.matmul(ps[bSi][:, cs_], lhsT=TGi[:, k1, :], rhs=bm[:, k1, :], start=True, stop=False), r=["TGi", "Bm2"], w=[PK[bSi]])
                        T.op("pe", lambda e: e.matmul(ps[bSi][:, cs_], lhsT=TGr[:, k1, :], rhs=bm[:, 64 + k1, :], start=False, stop=True), r=["TGr", "Bm2"], w=[PK[bSi]])
                    T.op("act", lambda e: e.copy(out=srs[:], in_=ps[bSr][:, :]), r=[PK[bSr]], w=["srsC"])
                    T.op("act", lambda e: e.copy(out=sis[:], in_=ps[bSi][:, :]), r=[PK[bSi]], w=["sisC"])
                    T.op("dve", lambda e: e.tensor_tensor(out=tm[0][:], in0=ps[bZr][:, :], in1=srs[:], op=ALU.mult), r=[PK[bZr], "srsC"], w=["tm0"])
                    T.op("dve", lambda e: e.tensor_tensor(out=tm[1][:], in0=ps[bZi][:, :], in1=sis[:], op=ALU.mult), r=[PK[bZi], "sisC"], w=["tm1"])
                    T.op("dve", lambda e: e.tensor_tensor(out=tm[2][:], in0=ps[bZr][:, :], in1=sis[:], op=ALU.mult), r=[PK[bZr], "sisC"], w=["tm2"])
                    T.op("dve", lambda e: e.tensor_tensor(out=tm[3][:], in0=ps[bZi][:, :], in1=srs[:], op=ALU.mult), r=[PK[bZi], "srsC"], w=["tm3"])
                    yv_r = Y[:, :, 0, kc * 16:(kc + 1) * 16].rearrange("p c k -> p k c")
                    yv_i = Y[:, :, 1, kc * 16:(kc + 1) * 16].rearrange("p c k -> p k c")
                    T.op("pool", lambda e: e.tensor_tensor(out=yv_r, in0=tm[0][:].rearrange("p (k c) -> p k c", c=GC),
                                                           in1=tm[1][:].rearrange("p (k c) -> p k c", c=GC), op=ALU.subtract),
                         r=["tm0", "tm1"], w=["YC"])
                    T.op("pool", lambda e: e.tensor_tensor(out=yv_i, in0=tm[2][:].rearrange("p (k c) -> p k c", c=GC),
                                                           in1=tm[3][:].rearrange("p (k c) -> p k c", c=GC), op=ALU.add),
                         r=["tm2", "tm3"], w=["YC"])
                for cq in range(GC // 2):
                    pi = cq % 2
                    for c2 in range(2):
                        c = cq * 2 + c2
                        T.op("pe", lambda e: e.matmul(ps[pi][0:64, c2 * 256:(c2 + 1) * 256], lhsT=Y[:, c, 0, :], rhs=R1[:, :],
                                                      start=True, stop=False), r=["YC", "R1"], w=[PK[pi]])
                        T.op("pe", lambda e: e.matmul(ps[pi][0:64, c2 * 256:(c2 + 1) * 256], lhsT=Y[:, c, 1, :], rhs=R2[:, :],
                                                      start=False, stop=True), r=["YC", "R2"], w=[PK[pi]])
                    T.op("act" if cq % 2 == 0 else "dve",
                         lambda e: (e.copy if cq % 2 == 0 else e.tensor_copy)(
                             out=Dm[:, :, cq * 2:(cq + 1) * 2].rearrange("p f c -> p c f"),
                             in_=ps[pi][0:64, :].rearrange("p (c f) -> p c f", c=2)),
                         r=[PK[pi]], w=["DmC"])
                for nq in range(8):
                    pi = 6 + nq % 2
                    for j in range(16):
                        n2 = nq * 16 + j
                        T.op("pe", lambda e: e.matmul(ps[pi][0:32, j * GC:(j + 1) * GC], lhsT=TH[:, n2, 0, :], rhs=Dm[:, n2, :],
                                                      start=True, stop=False), r=["TH", "DmC"], w=[PK[pi]])
                        T.op("pe", lambda e: e.matmul(ps[pi][0:32, j * GC:(j + 1) * GC], lhsT=TH[:, n2, 1, :], rhs=Dm[:, 128 + n2, :],
                                                      start=False, stop=True), r=["TH", "DmC"], w=[PK[pi]])
                    nsl = slice(nq * 16, (nq + 1) * 16)
                    T.op("dve", lambda e: e.tensor_tensor(out=tmpy[:], in0=ps[pi][0:32, :].rearrange("p (n c) -> p c n", c=GC),
                                                          in1=zs[:, :, nsl], op=ALU.add), r=[PK[pi], "zsC"], w=["tmpyC"])
                    T.op("dve", lambda e: e.tensor_tensor(out=zf[:, :, nsl], in0=tmpy[:], in1=gt[:, :, nsl], op=ALU.mult),
                         r=["tmpyC", "gtC", "srcb0", "zsC"], w=["zf"])
            T.op("act", lambda e: e.copy(out=hob[:], in_=zf[:]), r=["zf"], w=["srcb0"])
            T.dma("sp", lambda e: e.dma_start(out=flay(hyoT_d[c0:c0 + GC, :]), in_=hob[:]), r=["srcb0"], w=["hyoT_d"])
        T.barrier()

    if "D" in stages:
      with ExitStack() as es:
        aff_all = sb(es, "aff_all", [128, NT, NE])
        with ExitStack() as es2:
            attnT = sb(es2, "attnT", [128, 4, L], BF16)
            hyoT = sb(es2, "hyoT", [128, 4, L], BF16)
            wpa = sb(es2, "wpa", [128, 4, D], BF16)
            wph = sb(es2, "wph", [128, 4, D], BF16)
            wo = sb(es2, "wo", [128, 8, D], BF16)
            wst = sb(es2, "wstD", [128, 8, D])
            g2bc = sb(es2, "g2bc", [128, D])
            wr = sb(es2, "wr", [128, 8, 16])
            gat = sb(es2, "gatD", [128, 16, 512], BF16)
            m1 = sb(es2, "m1D", [128, 512])
            m2 = sb(es2, "m2D", [128, 512])
            mT = sb(es2, "mTD", [128, 8, 512], BF16)
            xin = sb(es2, "xinD", [128, D])
            x1 = sb(es2, "x1D", [128, D])
            u2 = sb(es2, "u2D", [128, D])
            u2b = sb(es2, "u2bD", [128, D], BF16)
            u2T = sb(es2, "u2TD", [128, 8, 128])
            junkd = sb(es2, "junkD", [128, D])
            st = sb(es2, "stD", [128, 8])
            lg = sb(es2, "lgD", [128, NE])
            for (tt_, td_, tk_) in ((attnT, attnT_d, "attnT"), (hyoT, hyoT_d, "hyoT")):
                T.dmas("sp", [(lambda e, j=j, a=a: e.dma_start(out=tt_[:, j, a:a + 2048], in_=td_[j * 128:(j + 1) * 128, a:a + 2048]))
                              for j in range(4) for a in (0, 2048)], w=[tk_])
            T.dma("sp", lambda e: e.dma_start(out=g2bc[:], in_=g2bc_d[:, :]), w=["g2bc"])
            T.dma("sp", lambda e: e.dma_start(out=wr[:], in_=wr_d[:, :, :]), w=["wr"])
            for (wsrc, nj, wdst, wk) in ((wpa_d, 4, wpa, "wpa"), (wph_d, 4, wph, "wph"), (wo_d, 8, wo, "wo")):
                T.dma("sp", lambda e: e.dma_start(out=wst[:, 0:nj, :], in_=wsrc.rearrange("(j p) c -> p j c", p=128)), w=["wstD"])
                T.op("pool", lambda e: e.tensor_copy(out=wdst[:], in_=wst[:, 0:nj, :]), r=["wstD"], w=[wk])
            for tq in range(8):
                tsl = slice(tq * 512, (tq + 1) * 512)
                T.dmas("sp", [(lambda e, a=a: e.dma_start(out=gat[:, a:a + 8, :],
                                                          in_=gateT_d.rearrange("(j p) t -> p j t", p=128)[:, a:a + 8, tsl])) for a in (0, 8)],
                       w=["gatD"])
                for dt in range(8):
                    for j in range(4):
                        T.op("pe", lambda e: e.matmul(ps[0][:, :], lhsT=wpa[:, j, dt * 128:(dt + 1) * 128], rhs=attnT[:, j, tsl],
                                                      start=(j == 0), stop=(j == 3)), r=["wpa", "attnT"], w=[PK[0]])
                    for j in range(4):
                        T.op("pe", lambda e: e.matmul(ps[1][:, :], lhsT=wph[:, j, dt * 128:(dt + 1) * 128], rhs=hyoT[:, j, tsl],
                                                      start=(j == 0), stop=(j == 3)), r=["wph", "hyoT"], w=[PK[1]])
                    T.op("dve", lambda e: e.tensor_tensor(out=m1[:], in0=ps[0][:, :], in1=gat[:, dt, :], op=ALU.mult), r=[PK[0], "gatD"], w=["m1D"])
                    T.op("dve", lambda e: e.tensor_tensor(out=m2[:], in0=ps[1][:, :], in1=gat[:, 8 + dt, :], op=ALU.mult), r=[PK[1], "gatD"], w=["m2D"])
                    T.op("pool", lambda e: e.tensor_tensor(out=mT[:, dt, :], in0=m1[:], in1=m2[:], op=ALU.add), r=["m1D", "m2D"], w=["mTD"])
                for ts in range(4):
                    i = tq * 4 + ts
                    T.dma("sp", lambda e: e.dma_start(out=xin[:], in_=x_d[i * 128:(i + 1) * 128, :]), w=["xinD"])
                    for half in range(2):
                        for dt in range(8):
                            T.op("pe", lambda e: e.matmul(ps[2 + half][:, :], lhsT=mT[:, dt, ts * 128:(ts + 1) * 128],
                                                          rhs=wo[:, dt, half * 512:(half + 1) * 512], start=(dt == 0), stop=(dt == 7)),
                                 r=["mTD", "wo"], w=[PK[2 + half]])
                        T.op("dve", lambda e: e.tensor_tensor(out=x1[:, half * 512:(half + 1) * 512], in0=ps[2 + half][:, :],
                                                              in1=xin[:, half * 512:(half + 1) * 512], op=ALU.add),
                             r=[PK[2 + half], "xinD"], w=["x1D"])
                    T.dma("sp", lambda e: e.dma_start(out=out_d[i * 128:(i + 1) * 128, :], in_=x1[:]), r=["x1D"], w=["out_d"])
                    T.op("dve", lambda e: e.memset(st[:, 0:1], 0.0), w=["stD"])
                    T.op("act", lambda e: e.activation(out=junkd[:], in_=x1[:], func=AF.Square, accum_out=st[:, 0:1]),
                         r=["x1D", "stD"], w=["junkD", "stD"])
                    T.op("act", lambda e: e.activation(out=st[:, 1:2], in_=st[:, 0:1], func=AF.Sqrt, scale=1.0 / D, bias=eps[:]),
                         r=["stD", "eps"], w=["stD"])
                    T.op("dve", lambda e: e.reciprocal(out=st[:, 2:3], in_=st[:, 1:2]), r=["stD"], w=["stD"])
                    T.op("dve", lambda e: e.scalar_tensor_tensor(out=u2[:], in0=x1[:], scalar=st[:, 2:3], in1=g2bc[:],
                                                                 op0=ALU.mult, op1=ALU.mult), r=["x1D", "stD", "g2bc"], w=["u2D"])
                    T.op("act", lambda e: e.copy(out=u2b[:], in_=u2[:]), r=["u2D"], w=["u2bD"])
                    T.dma("sp", lambda e: e.dma_start(out=u2_d[i * 128:(i + 1) * 128, :], in_=u2b[:]), r=["u2bD"], w=["u2_d"])
                    for j in range(8):
                        T.op("pe", lambda e: e.transpose(out=ps[4 + j // 4][:, (j % 4) * 128:(j % 4 + 1) * 128],
                                                         in_=u2[:, j * 128:(j + 1) * 128], identity=ident_f[:]),
                             r=["u2D", "ident_f"], w=[PK[4 + j // 4]])
                    for hh in range(2):
                        T.op("act", lambda e: e.copy(out=u2T[:, hh * 4:(hh + 1) * 4, :],
                                                     in_=ps[4 + hh][:, :].rearrange("p (j t) -> p j t", j=4)),
                             r=[PK[4 + hh]], w=["u2TD"])
                    for j in range(8):
                        T.op("pe", lambda e: e.matmul(ps[6][:, 0:NE], lhsT=u2T[:, j, :], rhs=wr[:, j, :], start=(j == 0), stop=(j == 7)),
                             r=["u2TD", "wr"], w=[PK[6]])
                    T.op("dve", lambda e: e.tensor_reduce(out=st[:, 3:4], in_=ps[6][:, 0:NE], axis=AX.X, op=ALU.max), r=[PK[6], "stD"], w=["stD"])
                    T.op("dve", lambda e: e.tensor_scalar(out=st[:, 4:5], in0=st[:, 3:4], scalar1=-1.0, scalar2=None, op0=ALU.mult), r=["stD"], w=["stD"])
                    T.op("dve", lambda e: e.memset(st[:, 5:6], 0.0), r=["stD"], w=["stD"])
                    T.op("act", lambda e: e.activation(out=lg[:], in_=ps[6][:, 0:NE], func=AF.Exp, bias=st[:, 4:5], accum_out=st[:, 5:6]),
                         r=[PK[6], "stD"], w=["lgD", "stD"])
                    T.op("dve", lambda e: e.reciprocal(out=st[:, 6:7], in_=st[:, 5:6]), r=["stD"], w=["stD"])
                    T.op("dve", lambda e: e.tensor_scalar(out=aff_all[:, i, :], in0=lg[:], scalar1=st[:, 6:7], scalar2=None, op0=ALU.mult),
                         r=["lgD", "stD"], w=["aff_all"])
            T.barrier()
        if "aff" in dbg_d:
            T.dma("sp", lambda e: e.dma_start(out=dbg_d["aff"], in_=aff_all[:]), r=["aff_all"], w=["dbgaff"])

        if "E" in stages:
          idx_all = sb(es, "idx_all", [128, NE, 4], I32)
          g_all = sb(es, "g_all", [128, NE, 4])
          with ExitStack() as es2:
            affT = sb(es2, "affT", [NE, L])
            cmpj = sb(es2, "cmpj", [NE, L])
            bs = sb(es2, "bsE", [NE, 8])
            tri = sb(es2, "tri", [128, 128])
            onesf = sb(es2, "onesf", [128, 128])
            iota = sb(es2, "iota", [128, 512])
            tidhl = sb(es2, "tidhl", [128, NT, 2], BF16)
            thbc = sb(es2, "thbc", [128, NE])
            dg = sb(es2, "dgE", [NE, NE])
            M = sb(es2, "ME", [128, NT, NE])
            pos = sb(es2, "posE", [128, NT, NE])
            srun = sb(es2, "srunE", [128, NE])
            cum = sb(es2, "cumE", [128, NE])
            vals = sb(es2, "valsE", [128, NT, NE, 4], BF16)
            afh = sb(es2, "afhE", [128, NT, NE], BF16)
            afr = sb(es2, "afrE", [128, NT, NE])
            oh = [sb(es2, "ohE%d" % i, [128, 512], BF16) for i in range(4)]
            idf = sb(es2, "idfE", [128, 4])
            pvs = sb(es2, "pvsE", [128, 16])
            T.dma("sp", lambda e: e.dma_start(out=tri[:], in_=tri_d[:, :]), w=["tri"])
            T.dma("sp", lambda e: e.dma_start(out=iota[:], in_=iota_d[:, :]), w=["iota"])
            T.dma("sp", lambda e: e.dma_start(out=tidhl[:], in_=tidhl_d[:, :, :]), w=["tidhl"])
            T.op("pool", lambda e: e.memset(onesf[:], 1.0), w=["onesf"])
            for i in range(NT):
                pi = i // 4
                T.op("pe", lambda e: e.transpose(out=ps[pi][0:NE, (i % 4) * 128:(i % 4 + 1) * 128], in_=aff_all[:, i, :],
                                                 identity=ident_f[:]), r=["aff_all", "ident_f"], w=[PK[pi]])
                if i % 4 == 3:
                    T.op("act", lambda e: e.copy(out=affT[:, (i - 3) * 128:(i + 1) * 128], in_=ps[pi][0:NE, :]), r=[PK[pi]], w=["affT"])
            T.op("dve", lambda e: e.memset(bs[:, 0:1], 0.0), w=["bsE"])
            T.op("dve", lambda e: e.memset(bs[:, 1:2], 1.0), r=["bsE"], w=["bsE"])
            for it in range(32):
                T.op("dve", lambda e: e.tensor_scalar(out=bs[:, 2:3], in0=bs[:, 0:1], scalar1=bs[:, 1:2], scalar2=0.5, op0=ALU.add, op1=ALU.mult), r=["bsE"], w=["bsE"])
                T.op("dve", lambda e: e.tensor_scalar(out=cmpj[:], in0=affT[:], scalar1=bs[:, 2:3], scalar2=None, op0=ALU.is_ge), r=["affT", "bsE"], w=["cmpj"])
                T.op("dve", lambda e: e.tensor_reduce(out=bs[:, 3:4], in_=cmpj[:], axis=AX.X, op=ALU.add), r=["cmpj", "bsE"], w=["bsE"])
                T.op("dve", lambda e: e.tensor_scalar(out=bs[:, 4:5], in0=bs[:, 3:4], scalar1=CAP - 0.5, scalar2=None, op0=ALU.is_ge), r=["bsE"], w=["bsE"])
                T.op("dve", lambda e: e.tensor_tensor(out=bs[:, 5:6], in0=bs[:, 2:3], in1=bs[:, 0:1], op=ALU.subtract), r=["bsE"], w=["bsE"])
                T.op("dve", lambda e: e.tensor_tensor(out=bs[:, 6:7], in0=bs[:, 1:2], in1=bs[:, 2:3], op=ALU.subtract), r=["bsE"], w=["bsE"])
                T.op("dve", lambda e: e.scalar_tensor_tensor(out=bs[:, 0:1], in0=bs[:, 5:6], scalar=bs[:, 4:5], in1=bs[:, 0:1], op0=ALU.mult, op1=ALU.add), r=["bsE"], w=["bsE"])
                T.op("dve", lambda e: e.scalar_tensor_tensor(out=bs[:, 1:2], in0=bs[:, 6:7], scalar=bs[:, 4:5], in1=bs[:, 2:3], op0=ALU.mult, op1=ALU.add), r=["bsE"], w=["bsE"])
            T.op("dve", lambda e: e.tensor_scalar(out=dg[:], in0=ident_f[0:NE, 0:NE], scalar1=bs[:, 0:1], scalar2=None, op0=ALU.mult), r=["bsE", "ident_f"], w=["dgE"])
            T.op("pe", lambda e: e.matmul(ps[0][:, 0:NE], lhsT=onesf[0:NE, :], rhs=dg[:, :], start=True, stop=True), r=["onesf", "dgE"], w=[PK[0]])
            T.op("dve", lambda e: e.tensor_copy(out=thbc[:], in_=ps[0][:, 0:NE]), r=[PK[0]], w=["thbc"])
            T.op("dve", lambda e: e.tensor_tensor(out=M[:], in0=aff_all[:], in1=thbc[:, :].unsqueeze(1).broadcast_to([128, NT, NE]), op=ALU.is_ge),
                 r=["aff_all", "thbc"], w=["ME"])
            T.op("dve", lambda e: e.memset(srun[:], 0.0), w=["srunE"])
            for i in range(NT):
                pi = 1 + i % 2
                T.op("pe", lambda e: e.matmul(ps[pi][:, 0:NE], lhsT=tri[:, :], rhs=M[:, i, :], start=True, stop=True), r=["tri", "ME"], w=[PK[pi]])
                T.op("pe", lambda e: e.matmul(ps[pi][:, NE:2 * NE], lhsT=onesf[:, :], rhs=M[:, i, :], start=True, stop=True), r=["onesf", "ME"], w=[PK[pi]])
                T.op("dve", lambda e: e.tensor_tensor(out=cum[:], in0=ps[pi][:, 0:NE], in1=srun[:], op=ALU.add), r=[PK[pi], "srunE"], w=["cumE"])
                T.op("dve", lambda e: e.tensor_tensor(out=srun[:], in0=ps[pi][:, NE:2 * NE], in1=srun[:], op=ALU.add), r=[PK[pi], "srunE", "cumE"], w=["srunE"])
                T.op("dve", lambda e: e.tensor_tensor(out=cum[:], in0=cum[:], in1=M[:, i, :], op=ALU.mult), r=["cumE", "ME"], w=["cumE"])
                T.op("dve", lambda e: e.tensor_scalar(out=pos[:, i, :], in0=cum[:], scalar1=-1.0, scalar2=None, op0=ALU.add), r=["cumE"], w=["posE"])
            T.op("dve", lambda e: e.tensor_copy(out=afh[:], in_=aff_all[:]), r=["aff_all"], w=["afhE"])
            T.op("dve", lambda e: e.tensor_tensor(out=afr[:], in0=aff_all[:], in1=afh[:], op=ALU.subtract), r=["aff_all", "afhE"], w=["afrE"])
            T.op("dve", lambda e: e.tensor_copy(out=vals[:, :, :, 2], in_=afh[:]), r=["afhE"], w=["valsE"])
            T.op("dve", lambda e: e.tensor_copy(out=vals[:, :, :, 3], in_=afr[:]), r=["afrE", "valsE"], w=["valsE"])
            T.op("dve", lambda e: e.tensor_copy(out=vals[:, :, :, 0:2], in_=tidhl[:, :, :].unsqueeze(2).broadcast_to([128, NT, NE, 2])),
                 r=["tidhl", "valsE"], w=["valsE"])
            oc = 0
            for ex in range(NE):
                pi = 3 + ex % 2
                for i in range(NT):
                    ob = oc % 4
                    eng = "dve"
                    oc += 1
                    T.op(eng, lambda e: e.tensor_scalar(out=oh[ob][:], in0=iota[:], scalar1=pos[:, i, ex:ex + 1], scalar2=None, op0=ALU.is_equal),
                         r=["iota", "posE"], w=["ohE%d" % ob])
                    for sc in range(4):
                        T.op("pe", lambda e: e.matmul(ps[pi][:, sc * 4:(sc + 1) * 4], lhsT=oh[ob][:, sc * 128:(sc + 1) * 128], rhs=vals[:, i, ex, :],
                                                      start=(i == 0 and sc == 0), stop=(i == NT - 1), skip_group_check=True),
                             r=["ohE%d" % ob, "valsE"], w=[PK[pi]])
                T.op("dve", lambda e: e.tensor_copy(out=pvs[:], in_=ps[pi][:, 0:16]), r=[PK[pi]], w=["pvsE"])
                pv = pvs[:, :].rearrange("p (s f) -> p s f", f=4)
                T.op("dve", lambda e: e.scalar_tensor_tensor(out=idf[:], in0=pv[:, :, 0], scalar=64.0, in1=pv[:, :, 1], op0=ALU.mult, op1=ALU.add),
                     r=["pvsE"], w=["idfE"])
                T.op("dve", lambda e: e.tensor_copy(out=idx_all[:, ex, :], in_=idf[:]), r=["idfE"], w=["idx_all"])
                T.op("dve", lambda e: e.tensor_tensor(out=g_all[:, ex, :], in0=pv[:, :, 2], in1=pv[:, :, 3], op=ALU.add), r=["pvsE"], w=["g_all"])
            T.barrier()
          if "idx" in dbg_d:
            T.dma("sp", lambda e: e.dma_start(out=dbg_d["idx"], in_=idx_all[:]), r=["idx_all"], w=["dbgidx"])
            T.dma("sp", lambda e: e.dma_start(out=dbg_d["g"], in_=g_all[:]), r=["g_all"], w=["dbgg"])

          if "F" in stages:
            wgb = [sb(es, "wgb%d" % i, [128, 8, D], BF16) for i in range(2)]
            wub = [sb(es, "wub%d" % i, [128, 8, D], BF16) for i in range(2)]
            wdb = [sb(es, "wdb%d" % i, [128, 8, D], BF16) for i in range(2)]
            xg = [sb(es, "xgF%d" % i, [128, D], BF16) for i in range(4)]
            xinT = sb(es, "xinT", [128, 8, 512], BF16)
            sg = sb(es, "sgF", [128, 512])
            hT = sb(es, "hTF", [128, 8, 512], BF16)
            eo = [sb(es, "eoF%d" % i, [128, D]) for i in range(2)]
            psb = [ps[6][:, :].bitcast(BF16), ps[7][:, :].bitcast(BF16)]

            def load_w(ex):
                for (wsrc, wdst, wk) in ((wg_d, wgb, "wgb"), (wu_d, wub, "wub"), (wd_d, wdb, "wdb")):
                    dst = wdst[ex % 2]
                    T.dmas("pool", [(lambda e, hh=hh: e.dma_start(out=dst[:, hh * 4:(hh + 1) * 4, :],
                                                                  in_=wsrc[ex].rearrange("(j p) c -> p j c", p=128)[:, hh * 4:(hh + 1) * 4, :]))
                                    for hh in range(2)], w=["%s%d" % (wk, ex % 2)])

            def gather(ex):
                for sc in range(4):
                    T.dma("pool", lambda e: e.indirect_dma_start(out=xg[sc][:], out_offset=None, in_=u2_d[:, :],
                                                                 in_offset=bass.IndirectOffsetOnAxis(ap=idx_all[:, ex, sc:sc + 1], axis=0)),
                          r=["idx_all", "u2_d"], w=["xgF%d" % sc])

            load_w(0)
            gather(0)
            for ex in range(NE):
                wgk, wuk, wdk = "wgb%d" % (ex % 2), "wub%d" % (ex % 2), "wdb%d" % (ex % 2)
                wgt, wut, wdt = wgb[ex % 2], wub[ex % 2], wdb[ex % 2]
                if ex + 1 < NE:
                    load_w(ex + 1)
                for sc in range(4):
                    for j in range(8):
                        T.op("pe", lambda e: e.transpose(out=psb[sc % 2][:, j * 128:(j + 1) * 128], in_=xg[sc][:, j * 128:(j + 1) * 128], identity=ident_b[:]),
                             r=["xgF%d" % sc, "ident_b"], w=[PK[6 + sc % 2]])
                    T.op("act", lambda e: e.copy(out=xinT[:, :, sc * 128:(sc + 1) * 128], in_=psb[sc % 2].rearrange("p (j t) -> p j t", j=8)),
                         r=[PK[6 + sc % 2]], w=["xinT"])
                if ex + 1 < NE:
                    gather(ex + 1)
                for ft in range(8):
                    pg, pu = (ft % 2) * 2, (ft % 2) * 2 + 1
                    for j in range(8):
                        T.op("pe", lambda e: e.matmul(ps[pg][:, :], lhsT=wgt[:, j, ft * 128:(ft + 1) * 128], rhs=xinT[:, j, :], start=(j == 0), stop=(j == 7)),
                             r=[wgk, "xinT"], w=[PK[pg]])
                    for j in range(8):
                        T.op("pe", lambda e: e.matmul(ps[pu][:, :], lhsT=wut[:, j, ft * 128:(ft + 1) * 128], rhs=xinT[:, j, :], start=(j == 0), stop=(j == 7)),
                             r=[wuk, "xinT"], w=[PK[pu]])
                    T.op("act", lambda e: e.activation(out=sg[:], in_=ps[pg][:, :], func=AF.Silu), r=[PK[pg]], w=["sgF"])
                    T.op("dve", lambda e: e.tensor_tensor(out=hT[:, ft, :], in0=ps[pu][:, :], in1=sg[:], op=ALU.mult), r=[PK[pu], "sgF"], w=["hTF"])
                for sc in range(4):
                    eb = eo[sc % 2]
                    for half in range(2):
                        po = 4 + half
                        for ft in range(8):
                            T.op("pe", lambda e: e.matmul(ps[po][:, :], lhsT=hT[:, ft, sc * 128:(sc + 1) * 128], rhs=wdt[:, ft, half * 512:(half + 1) * 512],
                                                          start=(ft == 0), stop=(ft == 7)), r=["hTF", wdk], w=[PK[po]])
                        T.op("dve", lambda e: e.tensor_scalar(out=eb[:, half * 512:(half + 1) * 512], in0=ps[po][:, :], scalar1=g_all[:, ex, sc:sc + 1],
                                                              scalar2=None, op0=ALU.mult), r=[PK[po], "g_all"], w=["eoF%d" % (sc % 2)])
                    T.dma("pool", lambda e: e.indirect_dma_start(out=out_d[:, :], out_offset=bass.IndirectOffsetOnAxis(ap=idx_all[:, ex, sc:sc + 1], axis=0),
                                                                 in_=eb[:], in_offset=None, compute_op=ALU.add),
                          r=["eoF%d" % (sc % 2), "idx_all"], w=["out_d"])
            T.barrier()
    for k, v in dbg_d.items():
        pass
    T.barrier()
    top.close()
    return nc


_CONST = None


def make_inputs(inp, b):
    global _CONST
    if _CONST is None:
        _CONST = host_consts()
    f = lambda a: np.ascontiguousarray(np.asarray(a, dtype=np.float32))
    m = dict(_CONST)
    m["x"] = f(inp["x"][b])
    m["g1p"] = f(inp["norm1_g"][0].reshape(8, 128).T)
    m["w_in"] = f(inp["w_in"][0])
    scw = np.concatenate([inp["short_conv_w"][0], inp["short_conv_b"][0][None, :]], axis=0)
    m["scw"] = f(scw.reshape(4, 12, 128).transpose(2, 1, 0))
    qkg = np.stack([inp["q_norm_g"][0], inp["k_norm_g"][0]], axis=0)
    m["qkg"] = f(np.broadcast_to(qkg[None], (128, 2, 64)))
    m["lamv"] = f(np.stack([inp["lambda_q1"][0], inp["lambda_k1"][0], inp["lambda_q2"][0], inp["lambda_k2"][0]])[None])
    m["subg"] = f(np.broadcast_to(inp["subln_g"][0][None, :], (128, 128)))
    m["fw1"] = f(inp["filt_w1"][0])
    m["fw2"] = f(inp["filt_w2"][0])
    m["fw3"] = f(inp["filt_w3"][0])
    m["fbf"] = f(np.stack([inp["filt_b1"][0], inp["filt_b2"][0], inp["filt_b3"][0], inp["filt_freq"][0]], axis=1))
    m["fwo"] = f(inp["filt_w_out"][0])
    m["skipbc"] = f(np.broadcast_to(inp["hyena_skip"][0][None], (32, 2, 512)))
    m["wpa"] = f(inp["w_branch_attn"][0])
    m["wph"] = f(inp["w_branch_hyena"][0])
    m["wo"] = f(inp["w_out"][0])
    m["g2bc"] = f(np.broadcast_to(inp["norm2_g"][0][None, :], (128, D)))
    m["wr"] = f(inp["w_router"][0].reshape(8, 128, 16).transpose(1, 0, 2))
    m["wg"] = f(inp["w_gate"][0])
    m["wu"] = f(inp["w_up"][0])
    m["wd"] = f(inp["w_down"][0])
    return m


def kernel(**inputs):
    nc = build()
    maps = [make_inputs(inputs, c % 4) for c in range(4)]
    in_maps = [maps[c % 4] for c in range(NCORES)]
    res = run_bass_kernel_spmd(nc, in_maps, core_ids=list(range(NCORES)))
    out = np.stack([np.asarray(res.results[b]["out"], dtype=np.float32) for b in range(4)], axis=0)
    return out
```

```python
import math
from contextlib import ExitStack
import numpy as np
import ml_dtypes
import concourse.bass as bass
import concourse.mybir as mybir
from concourse.bass_utils import run_bass_kernel_spmd

F32 = mybir.dt.float32
BF16 = mybir.dt.bfloat16
I32 = mybir.dt.int32
AF = mybir.ActivationFunctionType
ALU = mybir.AluOpType
AX = mybir.AxisListType

L = 4096
D = 1024
NT = 32
NCORES = 8
CAP = 512
NE = 16
GC = 32
DEBUG = False


class Trk:
    def __init__(s, nc):
        s.nc = nc
        s.eng = dict(pe=nc.tensor, act=nc.scalar, dve=nc.vector, pool=nc.gpsimd, sp=nc.sync)
        s.sem = {}
        s.cnt = {}
        for k in ("pe", "act", "dve", "pool"):
            s.sem[k] = nc.alloc_semaphore(name="s_" + k)
            s.cnt[k] = 0
        s.ND = 16
        for i in range(s.ND):
            k = "d%d" % i
            s.sem[k] = nc.alloc_semaphore(name="s_" + k)
            s.cnt[k] = 0
        s.rr = 0
        s.seen = {e: {} for e in s.eng}
        s.lw = {}
        s.rd = {}

    def _wait(s, e, deps):
        for k, v in deps.items():
            if e == "pe" and k == "pe":
                continue
            if s.seen[e].get(k, 0) < v:
                s.eng[e].wait_ge(s.sem[k], v)
                s.seen[e][k] = v

    def _deps(s, r, w):
        d = {}

        def add(k, v):
            if d.get(k, 0) < v:
                d[k] = v
        for x in r:
            for k, v in s.lw.get(x, {}).items():
                add(k, v)
        for x in w:
            for k, v in s.lw.get(x, {}).items():
                add(k, v)
            for k, v in s.rd.get(x, {}).items():
                add(k, v)
        return d

    def _upd(s, toks, r, w):
        if isinstance(toks, tuple):
            toks = [toks]
        for x in r:
            m = s.rd.setdefault(x, {})
            for tok in toks:
                if m.get(tok[0], 0) < tok[1]:
                    m[tok[0]] = tok[1]
        for x in w:
            m = {}
            for tok in toks:
                if m.get(tok[0], 0) < tok[1]:
                    m[tok[0]] = tok[1]
            s.lw[x] = m
            s.rd[x] = {}

    def op(s, e, fn, r=(), w=()):
        s._wait(e, s._deps(r, w))
        ins = fn(s.eng[e])
        s.cnt[e] += 1
        ins.then_inc(s.sem[e], 1)
        s._upd((e, s.cnt[e]), r, w)

    def dma(s, q, fn, r=(), w=()):
        s._wait(q, s._deps(r, w))
        ins = fn(s.eng[q])
        k = "d%d" % s.rr
        s.rr = (s.rr + 1) % s.ND
        s.cnt[k] += 16
        ins.then_inc(s.sem[k], 16)
        s._upd((k, s.cnt[k]), r, w)

    def dmas(s, q, fns, r=(), w=()):
        s._wait(q, s._deps(r, w))
        toks = []
        for fn in fns:
            ins = fn(s.eng[q])
            k = "d%d" % s.rr
            s.rr = (s.rr + 1) % s.ND
            s.cnt[k] += 16
            ins.then_inc(s.sem[k], 16)
            toks.append((k, s.cnt[k]))
        s._upd(toks, r, w)

    def barrier(s, engines=None):
        tot = {k: v for k, v in s.cnt.items() if v > 0}
        for e in (engines or s.eng):
            s._wait(e, dict(tot))
        s.lw = {}
        s.rd = {}


def _bf(a):
    return np.ascontiguousarray(a.astype(np.float32)).astype(ml_dtypes.bfloat16)


def host_consts():
    c = {}
    t = np.arange(L, dtype=np.float32)
    inv_freq = (np.float32(500000.0) ** (-np.arange(0, 16, 2, dtype=np.float32) / np.float32(16))).astype(np.float32)
    ang = (t[:, None] * inv_freq[None, :]).astype(np.float32)
    cs = np.concatenate([np.cos(ang), np.sin(ang)], axis=-1).astype(np.float32)
    c["ropecs"] = np.ascontiguousarray(cs.reshape(NT, 128, 16).transpose(1, 0, 2))
    tt = t / np.float32(L - 1)
    bands = np.linspace(1e-4, 15, 16, dtype=np.float32)
    a2 = (np.float32(2.0 * math.pi / L) * t[:, None] * bands[None, :]).astype(np.float32)
    emb = np.concatenate([tt[:, None], np.cos(a2), -np.sin(a2)], axis=-1).astype(np.float32)
    c["embT"] = np.ascontiguousarray(emb.T)
    c["posrow"] = np.ascontiguousarray(np.broadcast_to(tt[None, :], (128, L))).astype(np.float32)
    min_decay = math.log(1e-2) / 1.5
    max_decay = math.log(1e-2) / 0.3
    deltas = np.abs(np.linspace(min_decay, max_decay, 512, dtype=np.float32))
    c["ndelta"] = np.ascontiguousarray((-deltas).reshape(4, 128).T).astype(np.float32)
    n1 = np.arange(32)[:, None]
    k1 = np.arange(64)[None, :]
    th = 2 * np.pi * n1 * k1 / 64.0
    c["FA"] = _bf(np.concatenate([np.cos(th), -np.sin(th)], axis=1))
    n2 = np.arange(128)[:, None, None]
    k1 = np.arange(64)[None, :, None]
    k2 = np.arange(128)[None, None, :]
    th = 2 * np.pi * ((n2 * (k1 + 64 * k2)) % 8192) / 8192.0
    c["TGr"] = _bf(np.cos(th))
    c["TGi"] = _bf(-np.sin(th))
    c["TGn"] = _bf(np.sin(th))
    kk2 = np.arange(128)[:, None]
    nn2 = np.arange(128)[None, :]
    th = 2 * np.pi * ((kk2 * nn2) % 128) / 128.0
    fr, fi = np.cos(th), np.sin(th)
    c["R1"] = _bf(np.concatenate([fr, fi], axis=1))
    c["R2"] = _bf(np.concatenate([-fi, fr], axis=1))
    k1 = np.arange(64)[:, None, None]
    n2 = np.arange(128)[None, :, None]
    n1 = np.arange(32)[None, None, :]
    th = 2 * np.pi * ((k1 * (n2 + 128 * n1)) % 8192) / 8192.0
    th_ = np.stack([np.cos(th), -np.sin(th)], axis=2) / 8192.0
    c["TH"] = _bf(th_)
    tid = np.arange(L).reshape(NT, 128).T
    c["tidhl"] = _bf(np.stack([tid // 64, tid % 64], axis=-1))
    c["iota512"] = np.ascontiguousarray(np.broadcast_to(np.arange(512, dtype=np.float32)[None, :], (128, 512)))
    tri = (np.arange(128)[:, None] <= np.arange(128)[None, :]).astype(np.float32)
    c["tri"] = tri
    return c


def build(dbg=None):
    nc = bass.Bass("TRN2", target_bir_lowering=False)
    dbg = dbg or {}

    def din(name, shape, dt=F32):
        return nc.dram_tensor(name, list(shape), dt, kind="ExternalInput").ap()

    def dscr(name, shape, dt=F32):
        kind = "ExternalOutput" if name in dbg.get("_ext", ()) else "Internal"
        return nc.dram_tensor(name, list(shape), dt, kind=kind).ap()

    x_d = din("x", [L, D])
    g1p_d = din("g1p", [128, 8])
    w_in_d = din("w_in", [D, 5120])
    scw_d = din("scw", [128, 12, 4])
    qkg_d = din("qkg", [128, 2, 64])
    lam_d = din("lamv", [1, 4, 64])
    subg_d = din("subg", [128, 128])
    fw1_d = din("fw1", [33, 64])
    fw2_d = din("fw2", [64, 64])
    fw3_d = din("fw3", [64, 64])
    fbf_d = din("fbf", [64, 4])
    fwo_d = din("fwo", [64, 2048])
    skip_d = din("skipbc", [32, 2, 512])
    wpa_d = din("wpa", [512, D])
    wph_d = din("wph", [512, D])
    wo_d = din("wo", [D, D])
    g2bc_d = din("g2bc", [128, D])
    wr_d = din("wr", [128, 8, 16])
    wg_d = din("wg", [NE, D, D])
    wu_d = din("wu", [NE, D, D])
    wd_d = din("wd", [NE, D, D])
    ropecs_d = din("ropecs", [128, NT, 16])
    embT_d = din("embT", [33, L])
    posrow_d = din("posrow", [128, L])
    ndelta_d = din("ndelta", [128, 4])
    FA_d = din("FA", [32, 128], BF16)
    TGr_d = din("TGr", [128, 64, 128], BF16)
    TGi_d = din("TGi", [128, 64, 128], BF16)
    TGn_d = din("TGn", [128, 64, 128], BF16)
    R1_d = din("R1", [128, 256], BF16)
    R2_d = din("R2", [128, 256], BF16)
    TH_d = din("TH", [64, 128, 2, 32], BF16)
    tidhl_d = din("tidhl", [128, NT, 2], BF16)
    iota_d = din("iota512", [128, 512])
    tri_d = din("tri", [128, 128])

    out_d = nc.dram_tensor("out", [L, D], F32, kind="ExternalOutput").ap()

    qT_d = dscr("qT_s", [4, 128, L], BF16)
    kT_d = dscr("kT_s", [4, 128, L], BF16)
    v_d = dscr("v_s", [L, 512], BF16)
    hyc_d = dscr("hyc_s", [1536, L], F32)
    gateT_d = dscr("gateT_s", [2048, L], BF16)
    kern_d = dscr("kern_s", [2, 2, 512, L], BF16)
    attnT_d = dscr("attnT_s", [512, L], BF16)
    hyoT_d = dscr("hyoT_s", [512, L], BF16)
    u2_d = dscr("u2_s", [L, D], BF16)

    dbg_d = {}
    for k, (shape, dt) in ((k, v) for k, v in dbg.items() if not k.startswith("_")):
        dbg_d[k] = nc.dram_tensor("dbg_" + k, list(shape), dt, kind="ExternalOutput").ap()

    T = Trk(nc)
    top = ExitStack()

    sbc = [0]

    def sb(es, name, shape, dt=F32):
        sbc[0] += 1
        return es.enter_context(nc.sbuf_tensor("sb%d_%s" % (sbc[0], name), list(shape), dt))

    ps = [top.enter_context(nc.psum_tensor("ps%d" % i, [128, 512], F32)) for i in range(8)]
    PK = ["ps%d" % i for i in range(8)]

    ident_f = sb(top, "ident_f", [128, 128])
    ident_b = sb(top, "ident_b", [128, 128], BF16)
    eps = sb(top, "eps", [128, 1])
    T.op("pool", lambda e: e.memset(ident_f[:], 0.0), w=["ident_f"])
    T.op("pool", lambda e: e.affine_select(out=ident_f[:], in_=ident_f[:], pattern=[[-1, 128]],
                                           compare_op=ALU.not_equal, fill=1.0, base=0, channel_multiplier=1),
         r=["ident_f"], w=["ident_f"])
    T.op("pool", lambda e: e.tensor_copy(out=ident_b[:], in_=ident_f[:]), r=["ident_f"], w=["ident_b"])
    T.op("pool", lambda e: e.memset(eps[:], 1e-6), w=["eps"])

    stages = dbg.get("_stages", "AKBCDEF") if isinstance(dbg.get("_stages", None), str) else "AKBCDEF"

    if "A" in stages:
      with ExitStack() as es:
        uT = sb(es, "uT", [128, 8, L + 2], BF16)
        g1p = sb(es, "g1p", [128, 8])
        scw = sb(es, "scw", [128, 12, 4])
        qkg = sb(es, "qkg", [128, 2, 64])
        ropecs = sb(es, "ropecs", [128, NT, 16])
        xt = [sb(es, "xt%d" % i, [128, D]) for i in range(2)]
        xb = [sb(es, "xb%d" % i, [128, D], BF16) for i in range(2)]
        junk = sb(es, "junkA", [128, D])
        ss = sb(es, "ssA", [128, NT])
        rs = sb(es, "rsA", [128, NT])
        wf = sb(es, "wf", [128, 8, 512])
        wb = [sb(es, "wb%d" % i, [128, 8, 512], BF16) for i in range(2)]
        T.dma("sp", lambda e: e.dma_start(out=g1p[:], in_=g1p_d[:, :]), w=["g1p"])
        T.dma("sp", lambda e: e.dma_start(out=scw[:], in_=scw_d[:, :, :]), w=["scw"])
        T.dma("sp", lambda e: e.dma_start(out=qkg[:], in_=qkg_d[:, :, :]), w=["qkg"])
        T.dma("sp", lambda e: e.dma_start(out=ropecs[:], in_=ropecs_d[:, :, :]), w=["ropecs"])
        T.op("dve", lambda e: e.memset(ss[:], 0.0), w=["ssA"])
        T.op("dve", lambda e: e.memset(uT[:, :, 0:1], 0.0), w=["uTpad0"])
        T.op("dve", lambda e: e.memset(uT[:, :, L + 1:L + 2], 0.0), w=["uTpad1"])
        T.op("dve", lambda e: e.tensor_scalar(out=qkg[:, 0, :], in0=qkg[:, 0, :], scalar1=0.125, scalar2=None,
                                              op0=ALU.mult), r=["qkg"], w=["qkg"])
        psb = [ps[6][:, :].bitcast(BF16), ps[7][:, :].bitcast(BF16)]
        for i in range(NT):
            b = i % 2
            T.dma("sp", lambda e: e.dma_start(out=xt[b][:], in_=x_d[i * 128:(i + 1) * 128, :]), w=["xt%d" % b])
            T.op("act", lambda e: e.activation(out=junk[:], in_=xt[b][:], func=AF.Square,
                                               accum_out=ss[:, i:i + 1]), r=["xt%d" % b, "ssA"], w=["junkA", "ss%d" % i])
            T.op("act", lambda e: e.activation(out=rs[:, i:i + 1], in_=ss[:, i:i + 1], func=AF.Sqrt,
                                               scale=1.0 / D, bias=eps[:]), r=["ss%d" % i, "eps"], w=["rs%d" % i])
            T.op("dve", lambda e: e.reciprocal(out=rs[:, i:i + 1], in_=rs[:, i:i + 1]), r=["rs%d" % i], w=["rs%d" % i])
            T.op("dve", lambda e: e.tensor_scalar(out=xb[b][:], in0=xt[b][:], scalar1=rs[:, i:i + 1], scalar2=None,
                                                  op0=ALU.mult), r=["xt%d" % b, "rs%d" % i], w=["xb%d" % b])
            for j in range(8):
                T.op("pe", lambda e: e.transpose(out=psb[b][:, j * 128:(j + 1) * 128],
                                                 in_=xb[b][:, j * 128:(j + 1) * 128], identity=ident_b[:]),
                     r=["xb%d" % b, "ident_b"], w=[PK[6 + b]])
            T.op("act", lambda e: e.copy(out=uT[:, :, 1 + i * 128:1 + (i + 1) * 128],
                                         in_=psb[b].rearrange("p (j t) -> p j t", j=8)),
                 r=[PK[6 + b]], w=["uT%d" % i])
        UTK = ["uT%d" % i for i in range(NT)] + ["uTpad0", "uTpad1"]

        sq = sb(es, "sqA", [128, 512])
        ssq = sb(es, "ssqA", [128, 8])
        qn = sb(es, "qnA", [128, 8, 64])
        qb = sb(es, "qbA", [128, 512], BF16)
        rt = [sb(es, "rtA%d" % i, [128, 8, 8]) for i in range(4)]
        qTs = [sb(es, "qTs%d" % i, [128, 4, 128], BF16) for i in range(2)]
        vst = [sb(es, "vst%d" % i, [128, 512], BF16) for i in range(2)]
        hraw = sb(es, "hraw", [128, L + 2])
        hc = sb(es, "hcA", [128, L])
        gst = sb(es, "gstA", [128, L], BF16)
        T.op("pool", lambda e: e.memset(hraw[:, 0:1], 0.0), w=["hrawp0"])
        T.op("pool", lambda e: e.memset(hraw[:, L + 1:L + 2], 0.0), w=["hrawp1"])
        pcnt = [0]

        def nextps():
            pcnt[0] += 1
            return pcnt[0] % 4

        for cc in dbg.get('_ccs', range(10)):
            wbk = "wb%d" % (cc % 2)
            wbt = wb[cc % 2]
            T.dma("sp", lambda e: e.dma_start(
                out=wf[:], in_=w_in_d.rearrange("(j p) c -> p j c", p=128)[:, :, cc * 512:(cc + 1) * 512]), w=["wf"])
            for j in range(8):
                T.op("pool", lambda e: e.tensor_scalar(out=wbt[:, j, :], in0=wf[:, j, :], scalar1=g1p[:, j:j + 1],
                                                       scalar2=None, op0=ALU.mult), r=["wf", "g1p"], w=[wbk])
            if cc < 3:
                for i in range(NT):
                    pi = nextps()
                    for j in range(8):
                        T.op("pe", lambda e: e.matmul(ps[pi][:, :], lhsT=uT[:, j, 1 + i * 128:1 + (i + 1) * 128],
                                                      rhs=wbt[:, j, :], start=(j == 0), stop=(j == 7)),
                             r=["uT%d" % i, wbk], w=[PK[pi]])
                    if cc == 2:
                        vs = vst[i % 2]
                        T.op("act", lambda e: e.copy(out=vs[:], in_=ps[pi][:, :]), r=[PK[pi]], w=["vst%d" % (i % 2)])
                        T.dma("sp", lambda e: e.dma_start(out=v_d[i * 128:(i + 1) * 128, :], in_=vs[:]),
                              r=["vst%d" % (i % 2)], w=["v_d"])
                        continue
                    T.op("act", lambda e: e.activation(out=sq[:], in_=ps[pi][:, :], func=AF.Square), r=[PK[pi]], w=["sqA"])
                    T.op("dve", lambda e: e.tensor_reduce(out=ssq[:], in_=sq[:].rearrange("p (g d) -> p g d", d=64),
                                                          axis=AX.X, op=ALU.add), r=["sqA"], w=["ssqA"])
                    T.op("act", lambda e: e.activation(out=ssq[:], in_=ssq[:], func=AF.Sqrt, scale=1.0 / 64,
                                                       bias=eps[:]), r=["ssqA", "eps"], w=["ssqA"])
                    T.op("dve", lambda e: e.reciprocal(out=ssq[:], in_=ssq[:]), r=["ssqA"], w=["ssqA"])
                    T.op("dve", lambda e: e.tensor_tensor(out=qn[:], in0=ps[pi][:, :].rearrange("p (g d) -> p g d", d=64),
                                                          in1=ssq[:, :].unsqueeze(2).broadcast_to([128, 8, 64]),
                                                          op=ALU.mult), r=[PK[pi], "ssqA"], w=["qnA"])
                    T.op("dve", lambda e: e.tensor_tensor(out=qn[:], in0=qn[:],
                                                          in1=qkg[:, cc, :].unsqueeze(1).broadcast_to([128, 8, 64]),
                                                          op=ALU.mult), r=["qnA", "qkg"], w=["qnA"])
                    T.op("act", lambda e: e.copy(out=qb[:].rearrange("p (g d) -> p g d", d=64), in_=qn[:]),
                         r=["qnA"], w=["qbA"])
                    cosb = ropecs[:, i, 0:8].unsqueeze(1).broadcast_to([128, 8, 8])
                    sinb = ropecs[:, i, 8:16].unsqueeze(1).broadcast_to([128, 8, 8])
                    r1 = qn[:, :, 0:8]
                    r2 = qn[:, :, 8:16]
                    T.op("dve", lambda e: e.tensor_tensor(out=rt[0][:], in0=r1, in1=cosb, op=ALU.mult), r=["qnA", "ropecs"], w=["rt0"])
                    T.op("dve", lambda e: e.tensor_tensor(out=rt[1][:], in0=r2, in1=sinb, op=ALU.mult), r=["qnA", "ropecs"], w=["rt1"])
                    T.op("dve", lambda e: e.tensor_tensor(out=rt[2][:], in0=r2, in1=cosb, op=ALU.mult), r=["qnA", "ropecs"], w=["rt2"])
                    T.op("dve", lambda e: e.tensor_tensor(out=rt[3][:], in0=r1, in1=sinb, op=ALU.mult), r=["qnA", "ropecs"], w=["rt3"])
                    qbv = qb[:].rearrange("p (g d) -> p g d", d=64)
                    T.op("dve", lambda e: e.tensor_tensor(out=qbv[:, :, 0:8], in0=rt[0][:], in1=rt[1][:], op=ALU.subtract),
                         r=["rt0", "rt1"], w=["qbA"])
                    T.op("dve", lambda e: e.tensor_tensor(out=qbv[:, :, 8:16], in0=rt[2][:], in1=rt[3][:], op=ALU.add),
                         r=["rt2", "rt3"], w=["qbA"])
                    pb = 6 + (i % 2)
                    for h in range(4):
                        T.op("pe", lambda e: e.transpose(out=psb[i % 2][:, h * 128:(h + 1) * 128],
                                                         in_=qb[:, h * 128:(h + 1) * 128], identity=ident_b[:]),
                             r=["qbA", "ident_b"], w=[PK[pb]])
                    st = qTs[i % 2]
                    T.op("act", lambda e: e.copy(out=st[:], in_=psb[i % 2][:, 0:512].rearrange("p (h t) -> p h t", h=4)),
                         r=[PK[pb]], w=["qTs%d" % (i % 2)])
                    dst = (qT_d if cc == 0 else kT_d).rearrange("h p t -> p h t")[:, :, i * 128:(i + 1) * 128]
                    T.dma("sp", lambda e: e.dma_start(out=dst, in_=st[:]), r=["qTs%d" % (i % 2)], w=["qkT_d"])
            else:
                for ct in range(4):
                    for tq in range(8):
                        pi = nextps()
                        for j in range(8):
                            T.op("pe", lambda e: e.matmul(ps[pi][:, :], lhsT=wbt[:, j, ct * 128:(ct + 1) * 128],
                                                          rhs=uT[:, j, 1 + tq * 512:1 + (tq + 1) * 512],
                                                          start=(j == 0), stop=(j == 7)),
                                 r=UTK[4 * tq:4 * tq + 4] + [wbk], w=[PK[pi]])
                        if cc < 6:
                            T.op("act", lambda e: e.copy(out=hraw[:, 1 + tq * 512:1 + (tq + 1) * 512], in_=ps[pi][:, :]),
                                 r=[PK[pi]], w=["hraw"])
                        else:
                            T.op("act", lambda e: e.activation(out=gst[:, tq * 512:(tq + 1) * 512], in_=ps[pi][:, :],
                                                               func=AF.Sigmoid), r=[PK[pi]], w=["gstA"])
                    if cc < 6:
                        cti = (cc - 3) * 4 + ct
                        lvl = dbg.get('_lvl', 9)
                        if lvl >= 1:
                          T.op("act", lambda e: e.activation(out=hc[:], in_=hraw[:, 1:L + 1], func=AF.Identity,
                                                           scale=scw[:, cti, 1:2], bias=scw[:, cti, 3:4]),
                             r=["hraw", "scw"], w=["hcA"])
                        if lvl >= 2:
                          T.op("dve", lambda e: e.scalar_tensor_tensor(out=hc[:], in0=hraw[:, 0:L], scalar=scw[:, cti, 0:1],
                                                                     in1=hc[:], op0=ALU.mult, op1=ALU.add),
                             r=["hraw", "hrawp0", "scw", "hcA"], w=["hcA"])
                          T.op("dve", lambda e: e.scalar_tensor_tensor(out=hc[:], in0=hraw[:, 2:L + 2], scalar=scw[:, cti, 2:3],
                                                                     in1=hc[:], op0=ALU.mult, op1=ALU.add),
                             r=["hraw", "hrawp1", "scw", "hcA"], w=["hcA"])
                        if lvl >= 3:
                          for q4 in range(4):
                              T.dma("sp", lambda e: e.dma_start(out=hyc_d[cti * 128:(cti + 1) * 128, q4 * 1024:(q4 + 1) * 1024],
                                                                in_=hc[:, q4 * 1024:(q4 + 1) * 1024]),
                                    r=["hcA"], w=["hyc_d%d" % q4])
                    else:
                        gti = (cc - 6) * 4 + ct
                        T.dmas("sp", [(lambda e, a=a: e.dma_start(out=gateT_d[gti * 128:(gti + 1) * 128, a:a + 2048], in_=gst[:, a:a + 2048]))
                                      for a in (0, 2048)], r=["gstA"], w=["gateT_d"])
        T.barrier()

    if "K" in stages:
      with ExitStack() as es:
        embT = sb(es, "embT", [33, L])
        fw1 = sb(es, "fw1", [33, 64])
        fw2 = sb(es, "fw2", [64, 64])
        fw3 = sb(es, "fw3", [64, 64])
        fbf = sb(es, "fbf", [64, 4])
        frb = sb(es, "frb", [64, 3])
        fwo = sb(es, "fwo", [64, 2048])
        posrow = sb(es, "posrow", [128, L])
        ndelta = sb(es, "ndelta", [128, 4])
        hid = [sb(es, "hid%d" % i, [64, L]) for i in range(2)]
        hidb = sb(es, "hidb", [64, L])
        dec = sb(es, "decK", [128, L])
        kf = sb(es, "kfK", [128, L])
        kb = sb(es, "kbK", [128, L])
        hp = sb(es, "hpK", [128, L], BF16)
        hm = sb(es, "hmK", [128, L], BF16)
        for tns, src, k in ((fw1, fw1_d, "fw1"), (fw2, fw2_d, "fw2"), (fw3, fw3_d, "fw3"),
                            (fbf, fbf_d, "fbf"), (ndelta, ndelta_d, "ndelta")):
            T.dma("sp", lambda e: e.dma_start(out=tns[:], in_=src), w=[k])
        for tns, src, k, n in ((embT, embT_d, "embT", L), (fwo, fwo_d, "fwo", 2048), (posrow, posrow_d, "posrow", L)):
            T.dmas("sp", [(lambda e, a=a: e.dma_start(out=tns[:, a:a + 1024], in_=src[:, a:a + 1024]))
                          for a in range(0, n, 1024)], w=[k])
        for l in range(3):
            T.op("dve", lambda e: e.tensor_tensor(out=frb[:, l:l + 1], in0=fbf[:, l:l + 1], in1=fbf[:, 3:4], op=ALU.mult),
                 r=["fbf"], w=["frb"])
        TWO_PI = 2.0 * math.pi
        srcs = [(embT, "embT", 33, fw1, "fw1"), (hid[0], "hid0", 64, fw2, "fw2"), (hid[1], "hid1", 64, fw3, "fw3")]
        outs = [(hid[0], "hid0"), (hid[1], "hid1"), (hid[0], "hid0")]
        negpi = sb(es, "negpi", [64, 1])
        T.op("dve", lambda e: e.memset(negpi[:], 4.0 * math.pi), w=["negpi"])
        kcnt = sb(es, "kcntK", [64, L])
        for l in range(3):
            src, sk, kk, w_, wk = srcs[l]
            dst, dk = outs[l]
            for tq in range(8):
                pi = tq % 4
                T.op("pe", lambda e: e.matmul(ps[pi][0:64, :], lhsT=w_[0:kk, :], rhs=src[0:kk, tq * 512:(tq + 1) * 512],
                                              start=True, stop=True), r=[sk, wk], w=[PK[pi]])
                T.op("dve", lambda e: e.tensor_scalar(out=hidb[:, tq * 512:(tq + 1) * 512], in0=ps[pi][0:64, :],
                                                      scalar1=fbf[:, 3:4], scalar2=frb[:, l:l + 1], op0=ALU.mult,
                                                      op1=ALU.add), r=[PK[pi], "fbf", "frb"], w=["hidb"])
                xs_ = hidb[:, tq * 512:(tq + 1) * 512]
                ks_ = kcnt[:, tq * 512:(tq + 1) * 512]
                T.op("dve", lambda e: e.tensor_scalar(out=ks_, in0=xs_, scalar1=-3.0 * math.pi, scalar2=None, op0=ALU.is_gt),
                     r=["hidb"], w=["kcnt"])
                for thr in (-math.pi, math.pi, 3.0 * math.pi):
                    T.op("dve", lambda e: e.scalar_tensor_tensor(out=ks_, in0=xs_, scalar=thr, in1=ks_, op0=ALU.is_gt, op1=ALU.add),
                         r=["hidb", "kcnt"], w=["kcnt"])
                T.op("dve", lambda e: e.scalar_tensor_tensor(out=xs_, in0=ks_, scalar=-TWO_PI, in1=xs_, op0=ALU.mult, op1=ALU.add),
                     r=["hidb", "kcnt"], w=["hidb"])
                T.op("act", lambda e: e.activation(out=dst[:, tq * 512:(tq + 1) * 512], in_=xs_,
                                                   func=AF.Sin, bias=negpi[:]), r=["hidb", "negpi"], w=[dk])
        h3 = hid[0]
        for ct in range(4):
            T.op("act", lambda e: e.activation(out=dec[:], in_=posrow[:], func=AF.Exp, scale=ndelta[:, ct:ct + 1]),
                 r=["posrow", "ndelta"], w=["decK"])
            for o in range(2):
                for d_, (dst, dk) in enumerate(((kf, "kfK"), (kb, "kbK"))):
                    col = (o * 2 + d_) * 512 + ct * 128
                    for tq in range(8):
                        pi = tq % 4
                        T.op("pe", lambda e: e.matmul(ps[pi][:, :], lhsT=fwo[:, col:col + 128],
                                                      rhs=h3[:, tq * 512:(tq + 1) * 512], start=True, stop=True),
                             r=["hid0", "fwo"], w=[PK[pi]])
                        T.op("dve", lambda e: e.tensor_tensor(out=dst[:, tq * 512:(tq + 1) * 512], in0=ps[pi][:, :],
                                                              in1=dec[:, tq * 512:(tq + 1) * 512], op=ALU.mult),
                             r=[PK[pi], "decK"], w=[dk])
                T.op("dve", lambda e: e.memset(kb[:, 0:1], 0.0), r=["kbK"], w=["kbK"])
                T.op("pool", lambda e: e.tensor_tensor(out=hp[:], in0=kf[:], in1=kb[:], op=ALU.add), r=["kfK", "kbK"], w=["hpK"])
                T.op("pool", lambda e: e.tensor_tensor(out=hm[:], in0=kf[:], in1=kb[:], op=ALU.subtract), r=["kfK", "kbK"], w=["hmK"])
                T.dmas("sp", [(lambda e, a=a: e.dma_start(out=kern_d[o, 0, ct * 128:(ct + 1) * 128, a:a + 2048], in_=hp[:, a:a + 2048]))
                              for a in range(0, L, 2048)], r=["hpK"], w=["kern_d0"])
                T.dmas("sp", [(lambda e, a=a: e.dma_start(out=kern_d[o, 1, ct * 128:(ct + 1) * 128, a:a + 2048], in_=hm[:, a:a + 2048]))
                              for a in range(0, L, 2048)], r=["hmK"], w=["kern_d1"])
        T.barrier()

    if "B" in stages:
      with ExitStack() as es:
        qT = sb(es, "qT", [128, 4, L], BF16)
        kT = sb(es, "kT", [128, 2, 4, L], BF16)
        V = sb(es, "Vsb", [128, NT, 4, 132], BF16)
        lamv = sb(es, "lamv", [1, 4, 64])
        lamt = sb(es, "lamt", [1, 8])
        ones1 = sb(es, "ones1", [1, 128])
        nlam = sb(es, "nlam", [128, 1])
        subg = sb(es, "subg", [128, 128])
        E = [sb(es, "E%d" % i, [128, 512], BF16) for i in range(3)]
        osb = sb(es, "osbB", [128, 2, 4, 129])
        aacc = sb(es, "aacc", [128, 4, 128])
        ab = sb(es, "abB", [128, 4, 128], BF16)
        junkb = sb(es, "junkB", [128, 4, 128])
        sm = sb(es, "smB", [128, 8, 4])
        aTs = [sb(es, "aTs%d" % i, [128, 512], BF16) for i in range(2)]
        T.dmas("sp", [(lambda e, h=h, a=a: e.dma_start(out=qT[:, h, a:a + 2048], in_=qT_d[h, :, a:a + 2048]))
                      for h in range(4) for a in (0, 2048)], w=["qT"])
        T.op("pool", lambda e: e.memset(kT[64:128, 0, :, :], 0.0), w=["kTz0"])
        T.op("dve", lambda e: e.memset(kT[0:64, 1, :, :], 0.0), w=["kTz1"])
        T.dmas("sp", [(lambda e, h=h, a=a, m=m: e.dma_start(out=kT[m * 64:(m + 1) * 64, m, h, a:a + 2048],
                                                            in_=kT_d[h, m * 64:(m + 1) * 64, a:a + 2048]))
                      for m in range(2) for h in range(4) for a in (0, 2048)], w=["kT"])
        T.dmas("sp", [(lambda e, i=i: e.dma_start(out=V[:, i, :, 0:128],
                                                  in_=v_d[i * 128:(i + 1) * 128, :].rearrange("p (h d) -> p h d", d=128)))
                      for i in range(NT)], w=["V"])
        T.op("pool", lambda e: e.memset(V[:, :, :, 128:129], 1.0), w=["Vones"])
        T.dma("sp", lambda e: e.dma_start(out=lamv[:], in_=lam_d[:, :, :]), w=["lamv"])
        T.dma("sp", lambda e: e.dma_start(out=subg[:], in_=subg_d[:, :]), w=["subg"])
        T.op("dve", lambda e: e.tensor_scalar(out=subg[:], in0=subg[:], scalar1=0.8, scalar2=None, op0=ALU.mult),
             r=["subg"], w=["subg"])
        T.op("dve", lambda e: e.memset(ones1[:], 1.0), w=["ones1"])
        T.op("dve", lambda e: e.memset(lamt[:], 0.0), w=["lamt"])
        T.op("dve", lambda e: e.tensor_tensor(out=lamv[:, 0, :], in0=lamv[:, 0, :], in1=lamv[:, 1, :], op=ALU.mult), r=["lamv"], w=["lamv"])
        T.op("dve", lambda e: e.tensor_tensor(out=lamv[:, 2, :], in0=lamv[:, 2, :], in1=lamv[:, 3, :], op=ALU.mult), r=["lamv"], w=["lamv"])
        T.op("dve", lambda e: e.tensor_reduce(out=lamt[:, 0:1], in_=lamv[:, 0, :], axis=AX.X, op=ALU.add), r=["lamv", "lamt"], w=["lamt"])
        T.op("dve", lambda e: e.tensor_reduce(out=lamt[:, 1:2], in_=lamv[:, 2, :], axis=AX.X, op=ALU.add), r=["lamv", "lamt"], w=["lamt"])
        T.op("act", lambda e: e.activation(out=lamt[:, 2:4], in_=lamt[:, 0:2], func=AF.Exp), r=["lamt"], w=["lamt"])
        T.op("dve", lambda e: e.tensor_tensor(out=lamt[:, 4:5], in0=lamt[:, 3:4], in1=lamt[:, 2:3], op=ALU.subtract), r=["lamt"], w=["lamt"])
        T.op("dve", lambda e: e.tensor_scalar(out=lamt[:, 5:6], in0=lamt[:, 4:5], scalar1=-0.2, scalar2=None, op0=ALU.add), r=["lamt"], w=["lamt"])
        T.op("pe", lambda e: e.matmul(ps[7][:, 0:1], lhsT=ones1[:, :], rhs=lamt[:, 5:6], start=True, stop=True),
             r=["ones1", "lamt"], w=[PK[7]])
        T.op("dve", lambda e: e.tensor_copy(out=nlam[:], in_=ps[7][:, 0:1]), r=[PK[7]], w=["nlam"])
        psT = ps[6][:, :].bitcast(BF16)
        steps = [(h, qc, m, kt) for h in range(4) for qc in range(8) for m in range(2) for kt in range(NT)]
        USE_POW = False

        def issue_S(idx):
            h, qc, m, kt = steps[idx]
            sp_ = idx % 2
            eb = idx % 3
            T.op("pe", lambda e: e.matmul(ps[sp_][:, :], lhsT=kT[:, m, h, kt * 128:(kt + 1) * 128],
                                          rhs=qT[:, h, qc * 512:(qc + 1) * 512], start=True, stop=True),
                 r=["kT", "kTz0", "kTz1", "qT"], w=[PK[sp_]])
            T.op("act", lambda e: e.activation(out=E[eb][:], in_=ps[sp_][:, :], func=AF.Exp),
                 r=[PK[sp_]], w=["E%d" % eb])

        def issue_PV(idx):
            h, qc, m, kt = steps[idx]
            eb = idx % 3
            for qs in range(4):
                bank = 2 + m * 2 + qs // 2
                T.op("pe", lambda e: e.matmul(ps[bank][:, (qs % 2) * 256:(qs % 2) * 256 + 129],
                                              lhsT=E[eb][:, qs * 128:(qs + 1) * 128], rhs=V[:, kt, h, 0:129],
                                              start=(kt == 0 and qs % 2 == 0), stop=(kt == NT - 1),
                                              skip_group_check=True),
                     r=["E%d" % eb, "V", "Vones"], w=[PK[bank]])

        def bc4(ap2):
            return ap2.unsqueeze(2).broadcast_to([128, 4, 128])

        def epi_head():
            for m in range(2):
                for half in range(2):
                    bank = 2 + m * 2 + half
                    src = ps[bank][:, :].rearrange("p (a c) -> p a c", a=2)[:, :, 0:129]
                    dst = osb[:, m, half * 2:(half + 1) * 2, :]
                    if half == 0:
                        T.op("dve", lambda e: e.tensor_copy(out=dst, in_=src), r=[PK[bank]], w=["osb%d%d" % (m, half)])
                    else:
                        T.op("pool" if False else "dve", lambda e: e.tensor_copy(out=dst, in_=src), r=[PK[bank]], w=["osb%d%d" % (m, half)])
            OS = ["osb00", "osb01", "osb10", "osb11"]
            T.op("dve", lambda e: e.reciprocal(out=sm[:, 0:2, :], in_=osb[:, :, :, 128]), r=OS, w=["smB"])
            T.op("dve", lambda e: e.tensor_scalar(out=sm[:, 2, :], in0=sm[:, 1, :], scalar1=nlam[:, 0:1], scalar2=None, op0=ALU.mult),
                 r=["smB", "nlam"], w=["smB"])
            T.op("dve", lambda e: e.tensor_tensor(out=aacc[:], in0=osb[:, 0, :, 0:128], in1=bc4(sm[:, 0, :]), op=ALU.mult),
                 r=OS + ["smB"], w=["aacc"])
            T.op("dve", lambda e: e.tensor_tensor(out=junkb[:], in0=osb[:, 1, :, 0:128], in1=bc4(sm[:, 2, :]), op=ALU.mult),
                 r=OS + ["smB"], w=["junkB"])
            T.op("dve", lambda e: e.tensor_tensor(out=aacc[:], in0=aacc[:], in1=junkb[:], op=ALU.add), r=["aacc", "junkB"], w=["aacc"])
            T.op("dve", lambda e: e.tensor_tensor(out=junkb[:], in0=aacc[:], in1=aacc[:], op=ALU.mult), r=["aacc"], w=["junkB"])
            T.op("dve", lambda e: e.tensor_reduce(out=sm[:, 3, :], in_=junkb[:], axis=AX.X, op=ALU.add), r=["junkB", "smB"], w=["smB"])
            T.op("dve", lambda e: e.tensor_scalar(out=sm[:, 4, :], in0=sm[:, 3, :], scalar1=1.0 / 128, scalar2=1e-6, op0=ALU.mult, op1=ALU.add),
                 r=["smB"], w=["smB"])
            if USE_POW:
                T.op("dve", lambda e: e.tensor_scalar(out=sm[:, 5, :], in0=sm[:, 4, :], scalar1=-0.5, scalar2=None, op0=ALU.pow),
                     r=["smB"], w=["smB"])
            else:
                T.op("act", lambda e: e.activation(out=sm[:, 6, :], in_=sm[:, 4, :], func=AF.Sqrt), r=["smB"], w=["smB"])
                T.op("dve", lambda e: e.reciprocal(out=sm[:, 5, :], in_=sm[:, 6, :]), r=["smB"], w=["smB"])
            T.op("dve", lambda e: e.tensor_tensor(out=aacc[:], in0=aacc[:], in1=bc4(sm[:, 5, :]), op=ALU.mult), r=["aacc", "smB"], w=["aacc"])
            T.op("dve", lambda e: e.tensor_tensor(out=ab[:], in0=aacc[:], in1=subg[:, :].unsqueeze(1).broadcast_to([128, 4, 128]), op=ALU.mult),
                 r=["aacc", "subg"], w=["abB"])

        def epi_tail(h, qc):
            for qs in range(4):
                T.op("pe", lambda e: e.transpose(out=psT[:, qs * 128:(qs + 1) * 128], in_=ab[:, qs, :], identity=ident_b[:]),
                     r=["abB", "ident_b"], w=[PK[6]])
            st = aTs[qc % 2]
            T.op("act", lambda e: e.copy(out=st[:], in_=psT[:, 0:512]), r=[PK[6]], w=["aTs%d" % (qc % 2)])
            T.dma("sp", lambda e: e.dma_start(out=attnT_d[h * 128:(h + 1) * 128, qc * 512:(qc + 1) * 512], in_=st[:]),
                  r=["aTs%d" % (qc % 2)], w=["attnT_d"])

        pending = None
        issue_S(0)
        for idx in range(len(steps)):
            h, qc, m, kt = steps[idx]
            if idx + 1 < len(steps):
                issue_S(idx + 1)
            issue_PV(idx)
            if pending is not None and m == 0 and kt == 12:
                epi_tail(*pending)
                pending = None
            if m == 1 and kt == NT - 1:
                epi_head()
                pending = (h, qc)
        epi_tail(*pending)
        T.barrier()

    if "C" in stages:
      with ExitStack() as es:
        FA = sb(es, "FA", [32, 128], BF16)
        TGr = sb(es, "TGr", [128, 64, 128], BF16)
        TGi = sb(es, "TGi", [128, 64, 128], BF16)
        TGn = sb(es, "TGn", [128, 64, 128], BF16)
        R1 = sb(es, "R1", [128, 256], BF16)
        R2 = sb(es, "R2", [128, 256], BF16)
        TH = sb(es, "TH", [64, 128, 2, 32], BF16)
        skipbc = sb(es, "skipbc", [32, 2, GC])
        for tns, src, k in ((FA, FA_d, "FA"), (R1, R1_d, "R1"), (R2, R2_d, "R2")):
            T.dma("sp", lambda e: e.dma_start(out=tns[:], in_=src), w=[k])
        for tns, src, k in ((TGr, TGr_d, "TGr"), (TGi, TGi_d, "TGi"), (TGn, TGn_d, "TGn")):
            T.dmas("sp", [(lambda e, a=a: e.dma_start(out=tns[:, a:a + 16, :], in_=src[:, a:a + 16, :])) for a in range(0, 64, 16)], w=[k])
        T.dmas("sp", [(lambda e, a=a: e.dma_start(out=TH[:, a:a + 32, :, :], in_=TH_d[:, a:a + 32, :, :])) for a in range(0, 128, 32)], w=["TH"])
        zf = sb(es, "zf", [32, GC, 128])
        gt = sb(es, "gtC", [32, GC, 128])
        zs = sb(es, "zsC", [32, GC, 128])
        srcb = [sb(es, "srcb%d" % i, [32, GC, 128], BF16) for i in range(3)]
        Bm_ = [sb(es, "Bm%d" % i, [128, 128, GC], BF16) for i in range(3)]
        srs = sb(es, "srsC", [128, 512])
        sis = sb(es, "sisC", [128, 512])
        tm = [sb(es, "tmC%d" % i, [128, 512]) for i in range(4)]
        Y = sb(es, "YC", [128, GC, 2, 64], BF16)
        Dm = sb(es, "DmC", [64, 256, GC], BF16)
        tmpy = sb(es, "tmpyC", [32, GC, 16])
        hob = srcb[0]

        def flay(ap2d):
            return ap2d.rearrange("c (a b) -> a c b", b=128)

        for g in range(512 // GC):
            c0 = g * GC
            T.dma("sp", lambda e: e.dma_start(out=zf[:], in_=flay(hyc_d[c0:c0 + GC, :])), w=["zf"])
            T.dma("sp", lambda e: e.dma_start(out=skipbc[:], in_=skip_d[:, :, c0:c0 + GC]), w=["skipbc"])
            for o in range(2):
                T.dma("sp", lambda e: e.dma_start(out=gt[:], in_=flay(hyc_d[512 * (o + 1) + c0:512 * (o + 1) + c0 + GC, :])),
                      w=["gtC"])
                T.op("act", lambda e: e.copy(out=srcb[0][:], in_=zf[:]), r=["zf"], w=["srcb0"])
                for pm in range(2):
                    T.dma("sp", lambda e: e.dma_start(out=srcb[1 + pm][:], in_=flay(kern_d[o, pm, c0:c0 + GC, :])),
                          w=["srcb%d" % (1 + pm)])
                T.op("pool", lambda e: e.tensor_tensor(out=zs[:], in0=zf[:],
                                                       in1=skipbc[:, o, :].unsqueeze(2).broadcast_to([32, GC, 128]),
                                                       op=ALU.mult), r=["zf", "skipbc"], w=["zsC"])
                for s_ in (1, 2, 0):
                    for cq in range(GC // 4):
                        pi = cq % 2
                        for c4 in range(4):
                            c = cq * 4 + c4
                            T.op("pe", lambda e: e.matmul(ps[pi][:, c4 * 128:(c4 + 1) * 128], lhsT=srcb[s_][:, c, :],
                                                          rhs=FA[:, :], start=True, stop=True),
                                 r=["srcb%d" % s_, "FA"], w=[PK[pi]])
                        T.op("act" if cq % 2 == 0 else "dve",
                             lambda e: (e.copy if cq % 2 == 0 else e.tensor_copy)(
                                 out=Bm_[s_][:, :, cq * 4:(cq + 1) * 4].rearrange("p f c -> p c f"),
                                 in_=ps[pi][:, :].rearrange("p (c f) -> p c f", c=4)),
                             r=[PK[pi]], w=["Bm%d" % s_])
                for kc in range(4):
                    bZr, bZi, bSr, bSi = (2, 3, 4, 5) if kc % 2 == 0 else (0, 1, 6, 7)
                    for j in range(16):
                        k1 = kc * 16 + j
                        cs_ = slice(j * GC, (j + 1) * GC)
                        bz, bp, bm = Bm_[0], Bm_[1], Bm_[2]
                        T.op("pe", lambda e: e.matmul(ps[bZr][:, cs_], lhsT=TGr[:, k1, :], rhs=bz[:, k1, :], start=True, stop=False), r=["TGr", "Bm0"], w=[PK[bZr]])
                        T.op("pe", lambda e: e.matmul(ps[bZr][:, cs_], lhsT=TGn[:, k1, :], rhs=bz[:, 64 + k1, :], start=False, stop=True), r=["TGn", "Bm0"], w=[PK[bZr]])
                        T.op("pe", lambda e: e.matmul(ps[bZi][:, cs_], lhsT=TGi[:, k1, :], rhs=bz[:, k1, :], start=True, stop=False), r=["TGi", "Bm0"], w=[PK[bZi]])
                        T.op("pe", lambda e: e.matmul(ps[bZi][:, cs_], lhsT=TGr[:, k1, :], rhs=bz[:, 64 + k1, :], start=False, stop=True), r=["TGr", "Bm0"], w=[PK[bZi]])
                        T.op("pe", lambda e: e.matmul(ps[bSr][:, cs_], lhsT=TGr[:, k1, :], rhs=bp[:, k1, :], start=True, stop=False), r=["TGr", "Bm1"], w=[PK[bSr]])
                        T.op("pe", lambda e: e.matmul(ps[bSr][:, cs_], lhsT=TGn[:, k1, :], rhs=bp[:, 64 + k1, :], start=False, stop=True), r=["TGn", "Bm1"], w=[PK[bSr]])
                        T.op("pe", lambda e: e.matmul(ps[bSi][:, cs_], lhsT=TGi[:, k1, :], rhs=bm[:, k1, :], start=True, stop=False), r=["TGi", "Bm2"], w=[PK[bSi]])
                        T.op("pe", lambda e: e.matmul(ps[bSi][:, cs_], lhsT=TGr[:, k1, :], rhs=bm[:, 64 + k1, :], start=False, stop=True), r=["TGr", "Bm2"], w=[PK[bSi]])
                    T.op("act", lambda e: e.copy(out=srs[:], in_=ps[bSr][:, :]), r=[PK[bSr]], w=["srsC"])
                    T.op("act", lambda e: e.copy(out=sis[:], in_=ps[bSi][:, :]), r=[PK[bSi]], w=["sisC"])
                    T.op("dve", lambda e: e.tensor_tensor(out=tm[0][:], in0=ps[bZr][:, :], in1=srs[:], op=ALU.mult), r=[PK[bZr], "srsC"], w=["tm0"])
                    T.op("dve", lambda e: e.tensor_tensor(out=tm[1][:], in0=ps[bZi][:, :], in1=sis[:], op=ALU.mult), r=[PK[bZi], "sisC"], w=["tm1"])
                    T.op("dve", lambda e: e.tensor_tensor(out=tm[2][:], in0=ps[bZr][:, :], in1=sis[:], op=ALU.mult), r=[PK[bZr], "sisC"], w=["tm2"])
                    T.op("dve", lambda e: e.tensor_tensor(out=tm[3][:], in0=ps[bZi][:, :], in1=srs[:], op=ALU.mult), r=[PK[bZi], "srsC"], w=["tm3"])
                    yv_r = Y[:, :, 0, kc * 16:(kc + 1) * 16].rearrange("p c k -> p k c")
                    yv_i = Y[:, :, 1, kc * 16:(kc + 1) * 16].rearrange("p c k -> p k c")
                    T.op("pool", lambda e: e.tensor_tensor(out=yv_r, in0=tm[0][:].rearrange("p (k c) -> p k c", c=GC),
                                                           in1=tm[1][:].rearrange("p (k c) -> p k c", c=GC), op=ALU.subtract),
                         r=["tm0", "tm1"], w=["YC"])
                    T.op("pool", lambda e: e.tensor_tensor(out=yv_i, in0=tm[2][:].rearrange("p (k c) -> p k c", c=GC),
                                                           in1=tm[3][:].rearrange("p (k c) -> p k c", c=GC), op=ALU.add),
                         r=["tm2", "tm3"], w=["YC"])
                for cq in range(GC // 2):
                    pi = cq % 2
                    for c2 in range(2):
                        c = cq * 2 + c2
                        T.op("pe", lambda e: e.matmul(ps[pi][0:64, c2 * 256:(c2 + 1) * 256], lhsT=Y[:, c, 0, :], rhs=R1[:, :],
                                                      start=True, stop=False), r=["YC", "R1"], w=[PK[pi]])
                        T.op("pe", lambda e: e.matmul(ps[pi][0:64, c2 * 256:(c2 + 1) * 256], lhsT=Y[:, c, 1, :], rhs=R2[:, :],
                                                      start=False, stop=True), r=["YC", "R2"], w=[PK[pi]])
                    T.op("act" if cq % 2 == 0 else "dve",
                         lambda e: (e.copy if cq % 2 == 0 else e.tensor_copy)(
                             out=Dm[:, :, cq * 2:(cq + 1) * 2].rearrange("p f c -> p c f"),
                             in_=ps[pi][0:64, :].rearrange("p (c f) -> p c f", c=2)),
                         r=[PK[pi]], w=["DmC"])
                for nq in range(8):
                    pi = 6 + nq % 2
                    for j in range(16):
                        n2 = nq * 16 + j
                        T.op("pe", lambda e: e.matmul(ps[pi][0:32, j * GC:(j + 1) * GC], lhsT=TH[:, n2, 0, :], rhs=Dm[:, n2, :],
                                                      start=True, stop=False), r=["TH", "DmC"], w=[PK[pi]])
                        T.op("pe", lambda e: e.matmul(ps[pi][0:32, j * GC:(j + 1) * GC], lhsT=TH[:, n2, 1, :], rhs=Dm[:, 128 + n2, :],
                                                      start=False, stop=True), r=["TH", "DmC"], w=[PK[pi]])
                    nsl = slice(nq * 16, (nq + 1) * 16)
                    T.op("dve", lambda e: e.tensor_tensor(out=tmpy[:], in0=ps[pi][0:32, :].rearrange("p (n c) -> p c n", c=GC),
                                                          in1=zs[:, :, nsl], op=ALU.add), r=[PK[pi], "zsC"], w=["tmpyC"])
                    T.op("dve", lambda e: e.tensor_tensor(out=zf[:, :, nsl], in0=tmpy[:], in1=gt[:, :, nsl], op=ALU.mult),
                         r=["tmpyC", "gtC", "srcb0", "zsC"], w=["zf"])
            T.op("act", lambda e: e.copy(out=hob[:], in_=zf[:]), r=["zf"], w=["srcb0"])
            T.dma("sp", lambda e: e.dma_start(out=flay(hyoT_d[c0:c0 + GC, :]), in_=hob[:]), r=["srcb0"], w=["hyoT_d"])
        T.barrier()

    if "D" in stages:
      with ExitStack() as es:
        aff_all = sb(es, "aff_all", [128, NT, NE])
        with ExitStack() as es2:
            attnT = sb(es2, "attnT", [128, 4, L], BF16)
            hyoT = sb(es2, "hyoT", [128, 4, L], BF16)
            wpa = sb(es2, "wpa", [128, 4, D], BF16)
            wph = sb(es2, "wph", [128, 4, D], BF16)
            wo = sb(es2, "wo", [128, 8, D], BF16)
            wst = sb(es2, "wstD", [128, 8, D])
            g2bc = sb(es2, "g2bc", [128, D])
            wr = sb(es2, "wr", [128, 8, 16])
            gat = sb(es2, "gatD", [128, 16, 512], BF16)
            m1 = sb(es2, "m1D", [128, 512])
            m2 = sb(es2, "m2D", [128, 512])
            mT = sb(es2, "mTD", [128, 8, 512], BF16)
            xin = sb(es2, "xinD", [128, D])
            x1 = sb(es2, "x1D", [128, D])
            u2 = sb(es2, "u2D", [128, D])
            u2b = sb(es2, "u2bD", [128, D], BF16)
            u2T = sb(es2, "u2TD", [128, 8, 128])
            junkd = sb(es2, "junkD", [128, D])
            st = sb(es2, "stD", [128, 8])
            lg = sb(es2, "lgD", [128, NE])
            for (tt_, td_, tk_) in ((attnT, attnT_d, "attnT"), (hyoT, hyoT_d, "hyoT")):
                T.dmas("sp", [(lambda e, j=j, a=a: e.dma_start(out=tt_[:, j, a:a + 2048], in_=td_[j * 128:(j + 1) * 128, a:a + 2048]))
                              for j in range(4) for a in (0, 2048)], w=[tk_])
            T.dma("sp", lambda e: e.dma_start(out=g2bc[:], in_=g2bc_d[:, :]), w=["g2bc"])
            T.dma("sp", lambda e: e.dma_start(out=wr[:], in_=wr_d[:, :, :]), w=["wr"])
            for (wsrc, nj, wdst, wk) in ((wpa_d, 4, wpa, "wpa"), (wph_d, 4, wph, "wph"), (wo_d, 8, wo, "wo")):
                T.dma("sp", lambda e: e.dma_start(out=wst[:, 0:nj, :], in_=wsrc.rearrange("(j p) c -> p j c", p=128)), w=["wstD"])
                T.op("pool", lambda e: e.tensor_copy(out=wdst[:], in_=wst[:, 0:nj, :]), r=["wstD"], w=[wk])
            for tq in range(8):
                tsl = slice(tq * 512, (tq + 1) * 512)
                T.dmas("sp", [(lambda e, a=a: e.dma_start(out=gat[:, a:a + 8, :],
                                                          in_=gateT_d.rearrange("(j p) t -> p j t", p=128)[:, a:a + 8, tsl])) for a in (0, 8)],
                       w=["gatD"])
                for dt in range(8):
                    for j in range(4):
                        T.op("pe", lambda e: e.matmul(ps[0][:, :], lhsT=wpa[:, j, dt * 128:(dt + 1) * 128], rhs=attnT[:, j, tsl],
                                                      start=(j == 0), stop=(j == 3)), r=["wpa", "attnT"], w=[PK[0]])
                    for j in range(4):
                        T.op("pe", lambda e: e.matmul(ps[1][:, :], lhsT=wph[:, j, dt * 128:(dt + 1) * 128], rhs=hyoT[:, j, tsl],
                                                      start=(j == 0), stop=(j == 3)), r=["wph", "hyoT"], w=[PK[1]])
                    T.op("dve", lambda e: e.tensor_tensor(out=m1[:], in0=ps[0][:, :], in1=gat[:, dt, :], op=ALU.mult), r=[PK[0], "gatD"], w=["m1D"])
                    T.op("dve", lambda e: e.tensor_tensor(out=m2[:], in0=ps[1][:, :], in1=gat[:, 8 + dt, :], op=ALU.mult), r=[PK[1], "gatD"], w=["m2D"])
                    T.op("pool", lambda e: e.tensor_tensor(out=mT[:, dt, :], in0=m1[:], in1=m2[:], op=ALU.add), r=["m1D", "m2D"], w=["mTD"])
                for ts in range(4):
                    i = tq * 4 + ts
                    T.dma("sp", lambda e: e.dma_start(out=xin[:], in_=x_d[i * 128:(i + 1) * 128, :]), w=["xinD"])
                    for half in range(2):
                        for dt in range(8):
                            T.op("pe", lambda e: e.matmul(ps[2 + half][:, :], lhsT=mT[:, dt, ts * 128:(ts + 1) * 128],
                                                          rhs=wo[:, dt, half * 512:(half + 1) * 512], start=(dt == 0), stop=(dt == 7)),
                                 r=["mTD", "wo"], w=[PK[2 + half]])
                        T.op("dve", lambda e: e.tensor_tensor(out=x1[:, half * 512:(half + 1) * 512], in0=ps[2 + half][:, :],
                                                              in1=xin[:, half * 512:(half + 1) * 512], op=ALU.add),
                             r=[PK[2 + half], "xinD"], w=["x1D"])
                    T.dma("sp", lambda e: e.dma_start(out=out_d[i * 128:(i + 1) * 128, :], in_=x1[:]), r=["x1D"], w=["out_d"])
                    T.op("dve", lambda e: e.memset(st[:, 0:1], 0.0), w=["stD"])
                    T.op("act", lambda e: e.activation(out=junkd[:], in_=x1[:], func=AF.Square, accum_out=st[:, 0:1]),
                         r=["x1D", "stD"], w=["junkD", "stD"])
                    T.op("act", lambda e: e.activation(out=st[:, 1:2], in_=st[:, 0:1], func=AF.Sqrt, scale=1.0 / D, bias=eps[:]),
                         r=["stD", "eps"], w=["stD"])
                    T.op("dve", lambda e: e.reciprocal(out=st[:, 2:3], in_=st[:, 1:2]), r=["stD"], w=["stD"])
                    T.op("dve", lambda e: e.scalar_tensor_tensor(out=u2[:], in0=x1[:], scalar=st[:, 2:3], in1=g2bc[:],
                                                                 op0=ALU.mult, op1=ALU.mult), r=["x1D", "stD", "g2bc"], w=["u2D"])
                    T.op("act", lambda e: e.copy(out=u2b[:], in_=u2[:]), r=["u2D"], w=["u2bD"])
                    T.dma("sp", lambda e: e.dma_start(out=u2_d[i * 128:(i + 1) * 128, :], in_=u2b[:]), r=["u2bD"], w=["u2_d"])
                    for j in range(8):
                        T.op("pe", lambda e: e.transpose(out=ps[4 + j // 4][:, (j % 4) * 128:(j % 4 + 1) * 128],
                                                         in_=u2[:, j * 128:(j + 1) * 128], identity=ident_f[:]),
                             r=["u2D", "ident_f"], w=[PK[4 + j // 4]])
                    for hh in range(2):
                        T.op("act", lambda e: e.copy(out=u2T[:, hh * 4:(hh + 1) * 4, :],
                                                     in_=ps[4 + hh][:, :].rearrange("p (j t) -> p j t", j=4)),
                             r=[PK[4 + hh]], w=["u2TD"])
                    for j in range(8):
                        T.op("pe", lambda e: e.matmul(ps[6][:, 0:NE], lhsT=u2T[:, j, :], rhs=wr[:, j, :], start=(j == 0), stop=(j == 7)),
                             r=["u2TD", "wr"], w=[PK[6]])
                    T.op("dve", lambda e: e.tensor_reduce(out=st[:, 3:4], in_=ps[6][:, 0:NE], axis=AX.X, op=ALU.max), r=[PK[6], "stD"], w=["stD"])
                    T.op("dve", lambda e: e.tensor_scalar(out=st[:, 4:5], in0=st[:, 3:4], scalar1=-1.0, scalar2=None, op0=ALU.mult), r=["stD"], w=["stD"])
                    T.op("dve", lambda e: e.memset(st[:, 5:6], 0.0), r=["stD"], w=["stD"])
                    T.op("act", lambda e: e.activation(out=lg[:], in_=ps[6][:, 0:NE], func=AF.Exp, bias=st[:, 4:5], accum_out=st[:, 5:6]),
                         r=[PK[6], "stD"], w=["lgD", "stD"])
                    T.op("dve", lambda e: e.reciprocal(out=st[:, 6:7], in_=st[:, 5:6]), r=["stD"], w=["stD"])
                    T.op("dve", lambda e: e.tensor_scalar(out=aff_all[:, i, :], in0=lg[:], scalar1=st[:, 6:7], scalar2=None, op0=ALU.mult),
                         r=["lgD", "stD"], w=["aff_all"])
            T.barrier()
        if "aff" in dbg_d:
            T.dma("sp", lambda e: e.dma_start(out=dbg_d["aff"], in_=aff_all[:]), r=["aff_all"], w=["dbgaff"])

        if "E" in stages:
          idx_all = sb(es, "idx_all", [128, NE, 4], I32)
          g_all = sb(es, "g_all", [128, NE, 4])
          with ExitStack() as es2:
            affT = sb(es2, "affT", [NE, L])
            cmpj = sb(es2, "cmpj", [NE, L])
            bs = sb(es2, "bsE", [NE, 8])
            tri = sb(es2, "tri", [128, 128])
            onesf = sb(es2, "onesf", [128, 128])
            iota = sb(es2, "iota", [128, 512])
            tidhl = sb(es2, "tidhl", [128, NT, 2], BF16)
            thbc = sb(es2, "thbc", [128, NE])
            dg = sb(es2, "dgE", [NE, NE])
            M = sb(es2, "ME", [128, NT, NE])
            pos = sb(es2, "posE", [128, NT, NE])
            srun = sb(es2, "srunE", [128, NE])
            cum = sb(es2, "cumE", [128, NE])
            vals = sb(es2, "valsE", [128, NT, NE, 4], BF16)
            afh = sb(es2, "afhE", [128, NT, NE], BF16)
            afr = sb(es2, "afrE", [128, NT, NE])
            oh = [sb(es2, "ohE%d" % i, [128, 512], BF16) for i in range(4)]
            idf = sb(es2, "idfE", [128, 4])
            pvs = sb(es2, "pvsE", [128, 16])
            T.dma("sp", lambda e: e.dma_start(out=tri[:], in_=tri_d[:, :]), w=["tri"])
            T.dma("sp", lambda e: e.dma_start(out=iota[:], in_=iota_d[:, :]), w=["iota"])
            T.dma("sp", lambda e: e.dma_start(out=tidhl[:], in_=tidhl_d[:, :, :]), w=["tidhl"])
            T.op("pool", lambda e: e.memset(onesf[:], 1.0), w=["onesf"])
            for i in range(NT):
                pi = i // 4
                T.op("pe", lambda e: e.transpose(out=ps[pi][0:NE, (i % 4) * 128:(i % 4 + 1) * 128], in_=aff_all[:, i, :],
                                                 identity=ident_f[:]), r=["aff_all", "ident_f"], w=[PK[pi]])
                if i % 4 == 3:
                    T.op("act", lambda e: e.copy(out=affT[:, (i - 3) * 128:(i + 1) * 128], in_=ps[pi][0:NE, :]), r=[PK[pi]], w=["affT"])
            T.op("dve", lambda e: e.memset(bs[:, 0:1], 0.0), w=["bsE"])
            T.op("dve", lambda e: e.memset(bs[:, 1:2], 1.0), r=["bsE"], w=["bsE"])
            for it in range(32):
                T.op("dve", lambda e: e.tensor_scalar(out=bs[:, 2:3], in0=bs[:, 0:1], scalar1=bs[:, 1:2], scalar2=0.5, op0=ALU.add, op1=ALU.mult), r=["bsE"], w=["bsE"])
                T.op("dve", lambda e: e.tensor_scalar(out=cmpj[:], in0=affT[:], scalar1=bs[:, 2:3], scalar2=None, op0=ALU.is_ge), r=["affT", "bsE"], w=["cmpj"])
                T.op("dve", lambda e: e.tensor_reduce(out=bs[:, 3:4], in_=cmpj[:], axis=AX.X, op=ALU.add), r=["cmpj", "bsE"], w=["bsE"])
                T.op("dve", lambda e: e.tensor_scalar(out=bs[:, 4:5], in0=bs[:, 3:4], scalar1=CAP - 0.5, scalar2=None, op0=ALU.is_ge), r=["bsE"], w=["bsE"])
                T.op("dve", lambda e: e.tensor_tensor(out=bs[:, 5:6], in0=bs[:, 2:3], in1=bs[:, 0:1], op=ALU.subtract), r=["bsE"], w=["bsE"])
                T.op("dve", lambda e: e.tensor_tensor(out=bs[:, 6:7], in0=bs[:, 1:2], in1=bs[:, 2:3], op=ALU.subtract), r=["bsE"], w=["bsE"])
                T.op("dve", lambda e: e.scalar_tensor_tensor(out=bs[:, 0:1], in0=bs[:, 5:6], scalar=bs[:, 4:5], in1=bs[:, 0:1], op0=ALU.mult, op1=ALU.add), r=["bsE"], w=["bsE"])
                T.op("dve", lambda e: e.scalar_tensor_tensor(out=bs[:, 1:2], in0=bs[:, 6:7], scalar=bs[:, 4:5], in1=bs[:, 2:3], op0=ALU.mult, op1=ALU.add), r=["bsE"], w=["bsE"])
            T.op("dve", lambda e: e.tensor_scalar(out=dg[:], in0=ident_f[0:NE, 0:NE], scalar1=bs[:, 0:1], scalar2=None, op0=ALU.mult), r=["bsE", "ident_f"], w=["dgE"])
            T.op("pe", lambda e: e.matmul(ps[0][:, 0:NE], lhsT=onesf[0:NE, :], rhs=dg[:, :], start=True, stop=True), r=["onesf", "dgE"], w=[PK[0]])
            T.op("dve", lambda e: e.tensor_copy(out=thbc[:], in_=ps[0][:, 0:NE]), r=[PK[0]], w=["thbc"])
            T.op("dve", lambda e: e.tensor_tensor(out=M[:], in0=aff_all[:], in1=thbc[:, :].unsqueeze(1).broadcast_to([128, NT, NE]), op=ALU.is_ge),
                 r=["aff_all", "thbc"], w=["ME"])
            T.op("dve", lambda e: e.memset(srun[:], 0.0), w=["srunE"])
            for i in range(NT):
                pi = 1 + i % 2
                T.op("pe", lambda e: e.matmul(ps[pi][:, 0:NE], lhsT=tri[:, :], rhs=M[:, i, :], start=True, stop=True), r=["tri", "ME"], w=[PK[pi]])
                T.op("pe", lambda e: e.matmul(ps[pi][:, NE:2 * NE], lhsT=onesf[:, :], rhs=M[:, i, :], start=True, stop=True), r=["onesf", "ME"], w=[PK[pi]])
                T.op("dve", lambda e: e.tensor_tensor(out=cum[:], in0=ps[pi][:, 0:NE], in1=srun[:], op=ALU.add), r=[PK[pi], "srunE"], w=["cumE"])
                T.op("dve", lambda e: e.tensor_tensor(out=srun[:], in0=ps[pi][:, NE:2 * NE], in1=srun[:], op=ALU.add), r=[PK[pi], "srunE", "cumE"], w=["srunE"])
                T.op("dve", lambda e: e.tensor_tensor(out=cum[:], in0=cum[:], in1=M[:, i, :], op=ALU.mult), r=["cumE", "ME"], w=["cumE"])
                T.op("dve", lambda e: e.tensor_scalar(out=pos[:, i, :], in0=cum[:], scalar1=-1.0, scalar2=None, op0=ALU.add), r=["cumE"], w=["posE"])
            T.op("dve", lambda e: e.tensor_copy(out=afh[:], in_=aff_all[:]), r=["aff_all"], w=["afhE"])
            T.op("dve", lambda e: e.tensor_tensor(out=afr[:], in0=aff_all[:], in1=afh[:], op=ALU.subtract), r=["aff_all", "afhE"], w=["afrE"])
            T.op("dve", lambda e: e.tensor_copy(out=vals[:, :, :, 2], in_=afh[:]), r=["afhE"], w=["valsE"])
            T.op("dve", lambda e: e.tensor_copy(out=vals[:, :, :, 3], in_=afr[:]), r=["afrE", "valsE"], w=["valsE"])
            T.op("dve", lambda e: e.tensor_copy(out=vals[:, :, :, 0:2], in_=tidhl[:, :, :].unsqueeze(2).broadcast_to([128, NT, NE, 2])),
                 r=["tidhl", "valsE"], w=["valsE"])
            oc = 0
            for ex in range(NE):
                pi = 3 + ex % 2
                for i in range(NT):
                    ob = oc % 4
                    eng = "dve"
                    oc += 1
                    T.op(eng, lambda e: e.tensor_scalar(out=oh[ob][:], in0=iota[:], scalar1=pos[:, i, ex:ex + 1], scalar2=None, op0=ALU.is_equal),
                         r=["iota", "posE"], w=["ohE%d" % ob])
                    for sc in range(4):
                        T.op("pe", lambda e: e.matmul(ps[pi][:, sc * 4:(sc + 1) * 4], lhsT=oh[ob][:, sc * 128:(sc + 1) * 128], rhs=vals[:, i, ex, :],
                                                      start=(i == 0 and sc == 0), stop=(i == NT - 1), skip_group_check=True),
                             r=["ohE%d" % ob, "valsE"], w=[PK[pi]])
                T.op("dve", lambda e: e.tensor_copy(out=pvs[:], in_=ps[pi][:, 0:16]), r=[PK[pi]], w=["pvsE"])
                pv = pvs[:, :].rearrange("p (s f) -> p s f", f=4)
                T.op("dve", lambda e: e.scalar_tensor_tensor(out=idf[:], in0=pv[:, :, 0], scalar=64.0, in1=pv[:, :, 1], op0=ALU.mult, op1=ALU.add),
                     r=["pvsE"], w=["idfE"])
                T.op("dve", lambda e: e.tensor_copy(out=idx_all[:, ex, :], in_=idf[:]), r=["idfE"], w=["idx_all"])
                T.op("dve", lambda e: e.tensor_tensor(out=g_all[:, ex, :], in0=pv[:, :, 2], in1=pv[:, :, 3], op=ALU.add), r=["pvsE"], w=["g_all"])
            T.barrier()
          if "idx" in dbg_d:
            T.dma("sp", lambda e: e.dma_start(out=dbg_d["idx"], in_=idx_all[:]), r=["idx_all"], w=["dbgidx"])
            T.dma("sp", lambda e: e.dma_start(out=dbg_d["g"], in_=g_all[:]), r=["g_all"], w=["dbgg"])

          if "F" in stages:
            wgb = [sb(es, "wgb%d" % i, [128, 8, D], BF16) for i in range(2)]
            wub = [sb(es, "wub%d" % i, [128, 8, D], BF16) for i in range(2)]
            wdb = [sb(es, "wdb%d" % i, [128, 8, D], BF16) for i in range(2)]
            xg = [sb(es, "xgF%d" % i, [128, D], BF16) for i in range(4)]
            xinT = sb(es, "xinT", [128, 8, 512], BF16)
            sg = sb(es, "sgF", [128, 512])
            hT = sb(es, "hTF", [128, 8, 512], BF16)
            eo = [sb(es, "eoF%d" % i, [128, D]) for i in range(2)]
            psb = [ps[6][:, :].bitcast(BF16), ps[7][:, :].bitcast(BF16)]

            def load_w(ex):
                for (wsrc, wdst, wk) in ((wg_d, wgb, "wgb"), (wu_d, wub, "wub"), (wd_d, wdb, "wdb")):
                    dst = wdst[ex % 2]
                    T.dmas("pool", [(lambda e, hh=hh: e.dma_start(out=dst[:, hh * 4:(hh + 1) * 4, :],
                                                                  in_=wsrc[ex].rearrange("(j p) c -> p j c", p=128)[:, hh * 4:(hh + 1) * 4, :]))
                                    for hh in range(2)], w=["%s%d" % (wk, ex % 2)])

            def gather(ex):
                for sc in range(4):
                    T.dma("pool", lambda e: e.indirect_dma_start(out=xg[sc][:], out_offset=None, in_=u2_d[:, :],
                                                                 in_offset=bass.IndirectOffsetOnAxis(ap=idx_all[:, ex, sc:sc + 1], axis=0)),
                          r=["idx_all", "u2_d"], w=["xgF%d" % sc])

            load_w(0)
            gather(0)
            for ex in range(NE):
                wgk, wuk, wdk = "wgb%d" % (ex % 2), "wub%d" % (ex % 2), "wdb%d" % (ex % 2)
                wgt, wut, wdt = wgb[ex % 2], wub[ex % 2], wdb[ex % 2]
                if ex + 1 < NE:
                    load_w(ex + 1)
                for sc in range(4):
                    for j in range(8):
                        T.op("pe", lambda e: e.transpose(out=psb[sc % 2][:, j * 128:(j + 1) * 128], in_=xg[sc][:, j * 128:(j + 1) * 128], identity=ident_b[:]),
                             r=["xgF%d" % sc, "ident_b"], w=[PK[6 + sc % 2]])
                    T.op("act", lambda e: e.copy(out=xinT[:, :, sc * 128:(sc + 1) * 128], in_=psb[sc % 2].rearrange("p (j t) -> p j t", j=8)),
                         r=[PK[6 + sc % 2]], w=["xinT"])
                if ex + 1 < NE:
                    gather(ex + 1)
                for ft in range(8):
                    pg, pu = (ft % 2) * 2, (ft % 2) * 2 + 1
                    for j in range(8):
                        T.op("pe", lambda e: e.matmul(ps[pg][:, :], lhsT=wgt[:, j, ft * 128:(ft + 1) * 128], rhs=xinT[:, j, :], start=(j == 0), stop=(j == 7)),
                             r=[wgk, "xinT"], w=[PK[pg]])
                    for j in range(8):
                        T.op("pe", lambda e: e.matmul(ps[pu][:, :], lhsT=wut[:, j, ft * 128:(ft + 1) * 128], rhs=xinT[:, j, :], start=(j == 0), stop=(j == 7)),
                             r=[wuk, "xinT"], w=[PK[pu]])
                    T.op("act", lambda e: e.activation(out=sg[:], in_=ps[pg][:, :], func=AF.Silu), r=[PK[pg]], w=["sgF"])
                    T.op("dve", lambda e: e.tensor_tensor(out=hT[:, ft, :], in0=ps[pu][:, :], in1=sg[:], op=ALU.mult), r=[PK[pu], "sgF"], w=["hTF"])
                for sc in range(4):
                    eb = eo[sc % 2]
                    for half in range(2):
                        po = 4 + half
                        for ft in range(8):
                            T.op("pe", lambda e: e.matmul(ps[po][:, :], lhsT=hT[:, ft, sc * 128:(sc + 1) * 128], rhs=wdt[:, ft, half * 512:(half + 1) * 512],
                                                          start=(ft == 0), stop=(ft == 7)), r=["hTF", wdk], w=[PK[po]])
                        T.op("dve", lambda e: e.tensor_scalar(out=eb[:, half * 512:(half + 1) * 512], in0=ps[po][:, :], scalar1=g_all[:, ex, sc:sc + 1],
                                                              scalar2=None, op0=ALU.mult), r=[PK[po], "g_all"], w=["eoF%d" % (sc % 2)])
                    T.dma("pool", lambda e: e.indirect_dma_start(out=out_d[:, :], out_offset=bass.IndirectOffsetOnAxis(ap=idx_all[:, ex, sc:sc + 1], axis=0),
                                                                 in_=eb[:], in_offset=None, compute_op=ALU.add),
                          r=["eoF%d" % (sc % 2), "idx_all"], w=["out_d"])
            T.barrier()
    for k, v in dbg_d.items():
        pass
    T.barrier()
    top.close()
    return nc


_CONST = None


def make_inputs(inp, b):
    global _CONST
    if _CONST is None:
        _CONST = host_consts()
    f = lambda a: np.ascontiguousarray(np.asarray(a, dtype=np.float32))
    m = dict(_CONST)
    m["x"] = f(inp["x"][b])
    m["g1p"] = f(inp["norm1_g"][0].reshape(8, 128).T)
    m["w_in"] = f(inp["w_in"][0])
    scw = np.concatenate([inp["short_conv_w"][0], inp["short_conv_b"][0][None, :]], axis=0)
    m["scw"] = f(scw.reshape(4, 12, 128).transpose(2, 1, 0))
    qkg = np.stack([inp["q_norm_g"][0], inp["k_norm_g"][0]], axis=0)
    m["qkg"] = f(np.broadcast_to(qkg[None], (128, 2, 64)))
    m["lamv"] = f(np.stack([inp["lambda_q1"][0], inp["lambda_k1"][0], inp["lambda_q2"][0], inp["lambda_k2"][0]])[None])
    m["subg"] = f(np.broadcast_to(inp["subln_g"][0][None, :], (128, 128)))
    m["fw1"] = f(inp["filt_w1"][0])
    m["fw2"] = f(inp["filt_w2"][0])
    m["fw3"] = f(inp["filt_w3"][0])
    m["fbf"] = f(np.stack([inp["filt_b1"][0], inp["filt_b2"][0], inp["filt_b3"][0], inp["filt_freq"][0]], axis=1))
    m["fwo"] = f(inp["filt_w_out"][0])
    m["skipbc"] = f(np.broadcast_to(inp["hyena_skip"][0][None], (32, 2, 512)))
    m["wpa"] = f(inp["w_branch_attn"][0])
    m["wph"] = f(inp["w_branch_hyena"][0])
    m["wo"] = f(inp["w_out"][0])
    m["g2bc"] = f(np.broadcast_to(inp["norm2_g"][0][None, :], (128, D)))
    m["wr"] = f(inp["w_router"][0].reshape(8, 128, 16).transpose(1, 0, 2))
    m["wg"] = f(inp["w_gate"][0])
    m["wu"] = f(inp["w_up"][0])
    m["wd"] = f(inp["w_down"][0])
    return m


def kernel(**inputs):
    nc = build()
    maps = [make_inputs(inputs, c % 4) for c in range(4)]
    in_maps = [maps[c % 4] for c in range(NCORES)]
    res = run_bass_kernel_spmd(nc, in_maps, core_ids=list(range(NCORES)))
    out = np.stack([np.asarray(res.results[b]["out"], dtype=np.float32) for b in range(4)], axis=0)
    return out
```

```python
import math
from contextlib import ExitStack
import numpy as np
import ml_dtypes
import concourse.bass as bass
import concourse.mybir as mybir
from concourse.bass_utils import run_bass_kernel_spmd

F32 = mybir.dt.float32
BF16 = mybir.dt.bfloat16
I32 = mybir.dt.int32
AF = mybir.ActivationFunctionType
ALU = mybir.AluOpType
AX = mybir.AxisListType

L = 4096
D = 1024
NT = 32
NCORES = 8
CAP = 512
NE = 16
GC = 32
DEBUG = False


class Trk:
    def __init__(s, nc):
        s.nc = nc
        s.eng = dict(pe=nc.tensor, act=nc.scalar, dve=nc.vector, pool=nc.gpsimd, sp=nc.sync)
        s.sem = {}
        s.cnt = {}
        for k in ("pe", "act", "dve", "pool"):
            s.sem[k] = nc.alloc_semaphore(name="s_" + k)
            s.cnt[k] = 0
        s.ND = 16
        for i in range(s.ND):
            k = "d%d" % i
            s.sem[k] = nc.alloc_semaphore(name="s_" + k)
            s.cnt[k] = 0
        s.rr = 0
        s.seen = {e: {} for e in s.eng}
        s.lw = {}
        s.rd = {}

    def _wait(s, e, deps):
        for k, v in deps.items():
            if e == "pe" and k == "pe":
                continue
            if s.seen[e].get(k, 0) < v:
                s.eng[e].wait_ge(s.sem[k], v)
                s.seen[e][k] = v

    def _deps(s, r, w):
        d = {}

        def add(k, v):
            if d.get(k, 0) < v:
                d[k] = v
        for x in r:
            for k, v in s.lw.get(x, {}).items():
                add(k, v)
        for x in w:
            for k, v in s.lw.get(x, {}).items():
                add(k, v)
            for k, v in s.rd.get(x, {}).items():
                add(k, v)
        return d

    def _upd(s, toks, r, w):
        if isinstance(toks, tuple):
            toks = [toks]
        for x in r:
            m = s.rd.setdefault(x, {})
            for tok in toks:
                if m.get(tok[0], 0) < tok[1]:
                    m[tok[0]] = tok[1]
        for x in w:
            m = {}
            for tok in toks:
                if m.get(tok[0], 0) < tok[1]:
                    m[tok[0]] = tok[1]
            s.lw[x] = m
            s.rd[x] = {}

    def op(s, e, fn, r=(), w=()):
        s._wait(e, s._deps(r, w))
        ins = fn(s.eng[e])
        s.cnt[e] += 1
        ins.then_inc(s.sem[e], 1)
        s._upd((e, s.cnt[e]), r, w)

    def dma(s, q, fn, r=(), w=()):
        s._wait(q, s._deps(r, w))
        ins = fn(s.eng[q])
        k = "d%d" % s.rr
        s.rr = (s.rr + 1) % s.ND
        s.cnt[k] += 16
        ins.then_inc(s.sem[k], 16)
        s._upd((k, s.cnt[k]), r, w)

    def dmas(s, q, fns, r=(), w=()):
        s._wait(q, s._deps(r, w))
        toks = []
        for fn in fns:
            ins = fn(s.eng[q])
            k = "d%d" % s.rr
            s.rr = (s.rr + 1) % s.ND
            s.cnt[k] += 16
            ins.then_inc(s.sem[k], 16)
            toks.append((k, s.cnt[k]))
        s._upd(toks, r, w)

    def barrier(s, engines=None):
        tot = {k: v for k, v in s.cnt.items() if v > 0}
        for e in (engines or s.eng):
            s._wait(e, dict(tot))
        s.lw = {}
        s.rd = {}


def _bf(a):
    return np.ascontiguousarray(a.astype(np.float32)).astype(ml_dtypes.bfloat16)


def host_consts():
    c = {}
    t = np.arange(L, dtype=np.float32)
    inv_freq = (np.float32(500000.0) ** (-np.arange(0, 16, 2, dtype=np.float32) / np.float32(16))).astype(np.float32)
    ang = (t[:, None] * inv_freq[None, :]).astype(np.float32)
    cs = np.concatenate([np.cos(ang), np.sin(ang)], axis=-1).astype(np.float32)
    c["ropecs"] = np.ascontiguousarray(cs.reshape(NT, 128, 16).transpose(1, 0, 2))
    tt = t / np.float32(L - 1)
    bands = np.linspace(1e-4, 15, 16, dtype=np.float32)
    a2 = (np.float32(2.0 * math.pi / L) * t[:, None] * bands[None, :]).astype(np.float32)
    emb = np.concatenate([tt[:, None], np.cos(a2), -np.sin(a2)], axis=-1).astype(np.float32)
    c["embT"] = np.ascontiguousarray(emb.T)
    c["posrow"] = np.ascontiguousarray(np.broadcast_to(tt[None, :], (128, L))).astype(np.float32)
    min_decay = math.log(1e-2) / 1.5
    max_decay = math.log(1e-2) / 0.3
    deltas = np.abs(np.linspace(min_decay, max_decay, 512, dtype=np.float32))
    c["ndelta"] = np.ascontiguousarray((-deltas).reshape(4, 128).T).astype(np.float32)
    n1 = np.arange(32)[:, None]
    k1 = np.arange(64)[None, :]
    th = 2 * np.pi * n1 * k1 / 64.0
    c["FA"] = _bf(np.concatenate([np.cos(th), -np.sin(th)], axis=1))
    n2 = np.arange(128)[:, None, None]
    k1 = np.arange(64)[None, :, None]
    k2 = np.arange(128)[None, None, :]
    th = 2 * np.pi * ((n2 * (k1 + 64 * k2)) % 8192) / 8192.0
    c["TGr"] = _bf(np.cos(th))
    c["TGi"] = _bf(-np.sin(th))
    c["TGn"] = _bf(np.sin(th))
    kk2 = np.arange(128)[:, None]
    nn2 = np.arange(128)[None, :]
    th = 2 * np.pi * ((kk2 * nn2) % 128) / 128.0
    fr, fi = np.cos(th), np.sin(th)
    c["R1"] = _bf(np.concatenate([fr, fi], axis=1))
    c["R2"] = _bf(np.concatenate([-fi, fr], axis=1))
    k1 = np.arange(64)[:, None, None]
    n2 = np.arange(128)[None, :, None]
    n1 = np.arange(32)[None, None, :]
    th = 2 * np.pi * ((k1 * (n2 + 128 * n1)) % 8192) / 8192.0
    th_ = np.stack([np.cos(th), -np.sin(th)], axis=2) / 8192.0
    c["TH"] = _bf(th_)
    tid = np.arange(L).reshape(NT, 128).T
    c["tidhl"] = _bf(np.stack([tid // 64, tid % 64], axis=-1))
    c["iota512"] = np.ascontiguousarray(np.broadcast_to(np.arange(512, dtype=np.float32)[None, :], (128, 512)))
    tri = (np.arange(128)[:, None] <= np.arange(128)[None, :]).astype(np.float32)
    c["tri"] = tri
    return c


def build(dbg=None):
    nc = bass.Bass("TRN2", target_bir_lowering=False)
    dbg = dbg or {}

    def din(name, shape, dt=F32):
        return nc.dram_tensor(name, list(shape), dt, kind="ExternalInput").ap()

    def dscr(name, shape, dt=F32):
        kind = "ExternalOutput" if name in dbg.get("_ext", ()) else "Internal"
        return nc.dram_tensor(name, list(shape), dt, kind=kind).ap()

    x_d = din("x", [L, D])
    g1p_d = din("g1p", [128, 8])
    w_in_d = din("w_in", [D, 5120])
    scw_d = din("scw", [128, 12, 4])
    qkg_d = din("qkg", [128, 2, 64])
    lam_d = din("lamv", [1, 4, 64])
    subg_d = din("subg", [128, 128])
    fw1_d = din("fw1", [33, 64])
    fw2_d = din("fw2", [64, 64])
    fw3_d = din("fw3", [64, 64])
    fbf_d = din("fbf", [64, 4])
    fwo_d = din("fwo", [64, 2048])
    skip_d = din("skipbc", [32, 2, 512])
    wpa_d = din("wpa", [512, D])
    wph_d = din("wph", [512, D])
    wo_d = din("wo", [D, D])
    g2bc_d = din("g2bc", [128, D])
    wr_d = din("wr", [128, 8, 16])
    wg_d = din("wg", [NE, D, D])
    wu_d = din("wu", [NE, D, D])
    wd_d = din("wd", [NE, D, D])
    ropecs_d = din("ropecs", [128, NT, 16])
    embT_d = din("embT", [33, L])
    posrow_d = din("posrow", [128, L])
    ndelta_d = din("ndelta", [128, 4])
    FA_d = din("FA", [32, 128], BF16)
    TGr_d = din("TGr", [128, 64, 128], BF16)
    TGi_d = din("TGi", [128, 64, 128], BF16)
    TGn_d = din("TGn", [128, 64, 128], BF16)
    R1_d = din("R1", [128, 256], BF16)
    R2_d = din("R2", [128, 256], BF16)
    TH_d = din("TH", [64, 128, 2, 32], BF16)
    tidhl_d = din("tidhl", [128, NT, 2], BF16)
    iota_d = din("iota512", [128, 512])
    tri_d = din("tri", [128, 128])

    out_d = nc.dram_tensor("out", [L, D], F32, kind="ExternalOutput").ap()

    qT_d = dscr("qT_s", [4, 128, L], BF16)
    kT_d = dscr("kT_s", [4, 128, L], BF16)
    v_d = dscr("v_s", [L, 512], BF16)
    hyc_d = dscr("hyc_s", [1536, L], F32)
    gateT_d = dscr("gateT_s", [2048, L], BF16)
    kern_d = dscr("kern_s", [2, 2, 512, L], BF16)
    attnT_d = dscr("attnT_s", [512, L], BF16)
    hyoT_d = dscr("hyoT_s", [512, L], BF16)
    u2_d = dscr("u2_s", [L, D], BF16)

    dbg_d = {}
    for k, (shape, dt) in ((k, v) for k, v in dbg.items() if not k.startswith("_")):
        dbg_d[k] = nc.dram_tensor("dbg_" + k, list(shape), dt, kind="ExternalOutput").ap()

    T = Trk(nc)
    top = ExitStack()

    sbc = [0]

    def sb(es, name, shape, dt=F32):
        sbc[0] += 1
        return es.enter_context(nc.sbuf_tensor("sb%d_%s" % (sbc[0], name), list(shape), dt))

    ps = [top.enter_context(nc.psum_tensor("ps%d" % i, [128, 512], F32)) for i in range(8)]
    PK = ["ps%d" % i for i in range(8)]

    ident_f = sb(top, "ident_f", [128, 128])
    ident_b = sb(top, "ident_b", [128, 128], BF16)
    eps = sb(top, "eps", [128, 1])
    T.op("pool", lambda e: e.memset(ident_f[:], 0.0), w=["ident_f"])
    T.op("pool", lambda e: e.affine_select(out=ident_f[:], in_=ident_f[:], pattern=[[-1, 128]],
                                           compare_op=ALU.not_equal, fill=1.0, base=0, channel_multiplier=1),
         r=["ident_f"], w=["ident_f"])
    T.op("pool", lambda e: e.tensor_copy(out=ident_b[:], in_=ident_f[:]), r=["ident_f"], w=["ident_b"])
    T.op("pool", lambda e: e.memset(eps[:], 1e-6), w=["eps"])

    stages = dbg.get("_stages", "AKBCDEF") if isinstance(dbg.get("_stages", None), str) else "AKBCDEF"

    if "A" in stages:
      with ExitStack() as es:
        uT = sb(es, "uT", [128, 8, L + 2], BF16)
        g1p = sb(es, "g1p", [128, 8])
        scw = sb(es, "scw", [128, 12, 4])
        qkg = sb(es, "qkg", [128, 2, 64])
        ropecs = sb(es, "ropecs", [128, NT, 16])
        xt = [sb(es, "xt%d" % i, [128, D]) for i in range(2)]
        xb = [sb(es, "xb%d" % i, [128, D], BF16) for i in range(2)]
        junk = sb(es, "junkA", [128, D])
        ss = sb(es, "ssA", [128, NT])
        rs = sb(es, "rsA", [128, NT])
        wf = sb(es, "wf", [128, 8, 512])
        wb = [sb(es, "wb%d" % i, [128, 8, 512], BF16) for i in range(2)]
        T.dma("sp", lambda e: e.dma_start(out=g1p[:], in_=g1p_d[:, :]), w=["g1p"])
        T.dma("sp", lambda e: e.dma_start(out=scw[:], in_=scw_d[:, :, :]), w=["scw"])
        T.dma("sp", lambda e: e.dma_start(out=qkg[:], in_=qkg_d[:, :, :]), w=["qkg"])
        T.dma("sp", lambda e: e.dma_start(out=ropecs[:], in_=ropecs_d[:, :, :]), w=["ropecs"])
        T.op("dve", lambda e: e.memset(ss[:], 0.0), w=["ssA"])
        T.op("dve", lambda e: e.memset(uT[:, :, 0:1], 0.0), w=["uTpad0"])
        T.op("dve", lambda e: e.memset(uT[:, :, L + 1:L + 2], 0.0), w=["uTpad1"])
        T.op("dve", lambda e: e.tensor_scalar(out=qkg[:, 0, :], in0=qkg[:, 0, :], scalar1=0.125, scalar2=None,
                                              op0=ALU.mult), r=["qkg"], w=["qkg"])
        psb = [ps[6][:, :].bitcast(BF16), ps[7][:, :].bitcast(BF16)]
        for i in range(NT):
            b = i % 2
            T.dma("sp", lambda e: e.dma_start(out=xt[b][:], in_=x_d[i * 128:(i + 1) * 128, :]), w=["xt%d" % b])
            T.op("act", lambda e: e.activation(out=junk[:], in_=xt[b][:], func=AF.Square,
                                               accum_out=ss[:, i:i + 1]), r=["xt%d" % b, "ssA"], w=["junkA", "ss%d" % i])
            T.op("act", lambda e: e.activation(out=rs[:, i:i + 1], in_=ss[:, i:i + 1], func=AF.Sqrt,
                                               scale=1.0 / D, bias=eps[:]), r=["ss%d" % i, "eps"], w=["rs%d" % i])
            T.op("dve", lambda e: e.reciprocal(out=rs[:, i:i + 1], in_=rs[:, i:i + 1]), r=["rs%d" % i], w=["rs%d" % i])
            T.op("dve", lambda e: e.tensor_scalar(out=xb[b][:], in0=xt[b][:], scalar1=rs[:, i:i + 1], scalar2=None,
                                                  op0=ALU.mult), r=["xt%d" % b, "rs%d" % i], w=["xb%d" % b])
            for j in range(8):
                T.op("pe", lambda e: e.transpose(out=psb[b][:, j * 128:(j + 1) * 128],
                                                 in_=xb[b][:, j * 128:(j + 1) * 128], identity=ident_b[:]),
                     r=["xb%d" % b, "ident_b"], w=[PK[6 + b]])
            T.op("act", lambda e: e.copy(out=uT[:, :, 1 + i * 128:1 + (i + 1) * 128],
                                         in_=psb[b].rearrange("p (j t) -> p j t", j=8)),
                 r=[PK[6 + b]], w=["uT%d" % i])
        UTK = ["uT%d" % i for i in range(NT)] + ["uTpad0", "uTpad1"]

        sq = sb(es, "sqA", [128, 512])
        ssq = sb(es, "ssqA", [128, 8])
        qn = sb(es, "qnA", [128, 8, 64])
        qb2 = [sb(es, "qbA%d" % i, [128, 512], BF16) for i in range(2)]
        rt = [sb(es, "rtA%d" % i, [128, 8, 8]) for i in range(4)]
        qTs = [sb(es, "qTs%d" % i, [128, 4, 128], BF16) for i in range(2)]
        vst = [sb(es, "vst%d" % i, [128, 512], BF16) for i in range(2)]
        hraw = sb(es, "hraw", [128, L + 2])
        hc = sb(es, "hcA", [128, L])
        gst = sb(es, "gstA", [128, L], BF16)
        T.op("pool", lambda e: e.memset(hraw[:, 0:1], 0.0), w=["hrawp0"])
        T.op("pool", lambda e: e.memset(hraw[:, L + 1:L + 2], 0.0), w=["hrawp1"])
        pcnt = [0]

        def nextps():
            pcnt[0] += 1
            return pcnt[0] % 4

        for cc in dbg.get('_ccs', range(10)):
            wbk = "wb%d" % (cc % 2)
            wbt = wb[cc % 2]
            T.dma("sp", lambda e: e.dma_start(
                out=wf[:], in_=w_in_d.rearrange("(j p) c -> p j c", p=128)[:, :, cc * 512:(cc + 1) * 512]), w=["wf"])
            for j in range(8):
                T.op("pool", lambda e: e.tensor_scalar(out=wbt[:, j, :], in0=wf[:, j, :], scalar1=g1p[:, j:j + 1],
                                                       scalar2=None, op0=ALU.mult), r=["wf", "g1p"], w=[wbk])
            if cc < 3:
                def qk_tail(i):
                    qbt = qb2[i % 2]
                    pb = 6 + (i % 2)
                    for h in range(4):
                        T.op("pe", lambda e: e.transpose(out=psb[i % 2][:, h * 128:(h + 1) * 128],
                                                         in_=qbt[:, h * 128:(h + 1) * 128], identity=ident_b[:]),
                             r=["qbA%d" % (i % 2), "ident_b"], w=[PK[pb]])
                    st = qTs[i % 2]
                    T.op("act", lambda e: e.copy(out=st[:], in_=psb[i % 2][:, 0:512].rearrange("p (h t) -> p h t", h=4)),
                         r=[PK[pb]], w=["qTs%d" % (i % 2)])
                    dst = (qT_d if cc == 0 else kT_d).rearrange("h p t -> p h t")[:, :, i * 128:(i + 1) * 128]
                    T.dma("sp", lambda e: e.dma_start(out=dst, in_=st[:]), r=["qTs%d" % (i % 2)], w=["qkT_d"])

                for i in range(NT):
                    pi = nextps()
                    for j in range(8):
                        T.op("pe", lambda e: e.matmul(ps[pi][:, :], lhsT=uT[:, j, 1 + i * 128:1 + (i + 1) * 128],
                                                      rhs=wbt[:, j, :], start=(j == 0), stop=(j == 7)),
                             r=["uT%d" % i, wbk], w=[PK[pi]])
                    if cc == 2:
                        vs = vst[i % 2]
                        T.op("act", lambda e: e.copy(out=vs[:], in_=ps[pi][:, :]), r=[PK[pi]], w=["vst%d" % (i % 2)])
                        T.dma("sp", lambda e: e.dma_start(out=v_d[i * 128:(i + 1) * 128, :], in_=vs[:]),
                              r=["vst%d" % (i % 2)], w=["v_d"])
                        continue
                    if i > 0:
                        qk_tail(i - 1)
                    qb = qb2[i % 2]
                    qbk = "qbA%d" % (i % 2)
                    T.op("act", lambda e: e.activation(out=sq[:], in_=ps[pi][:, :], func=AF.Square), r=[PK[pi]], w=["sqA"])
                    T.op("dve", lambda e: e.tensor_reduce(out=ssq[:], in_=sq[:].rearrange("p (g d) -> p g d", d=64),
                                                          axis=AX.X, op=ALU.add), r=["sqA"], w=["ssqA"])
                    T.op("act", lambda e: e.activation(out=ssq[:], in_=ssq[:], func=AF.Sqrt, scale=1.0 / 64,
                                                       bias=eps[:]), r=["ssqA", "eps"], w=["ssqA"])
                    T.op("dve", lambda e: e.reciprocal(out=ssq[:], in_=ssq[:]), r=["ssqA"], w=["ssqA"])
                    T.op("dve", lambda e: e.tensor_tensor(out=qn[:], in0=ps[pi][:, :].rearrange("p (g d) -> p g d", d=64),
                                                          in1=ssq[:, :].unsqueeze(2).broadcast_to([128, 8, 64]),
                                                          op=ALU.mult), r=[PK[pi], "ssqA"], w=["qnA"])
                    T.op("dve", lambda e: e.tensor_tensor(out=qn[:], in0=qn[:],
                                                          in1=qkg[:, cc, :].unsqueeze(1).broadcast_to([128, 8, 64]),
                                                          op=ALU.mult), r=["qnA", "qkg"], w=["qnA"])
                    T.op("act", lambda e: e.copy(out=qb[:].rearrange("p (g d) -> p g d", d=64), in_=qn[:]),
                         r=["qnA"], w=[qbk])
                    cosb = ropecs[:, i, 0:8].unsqueeze(1).broadcast_to([128, 8, 8])
                    sinb = ropecs[:, i, 8:16].unsqueeze(1).broadcast_to([128, 8, 8])
                    r1 = qn[:, :, 0:8]
                    r2 = qn[:, :, 8:16]
                    T.op("dve", lambda e: e.tensor_tensor(out=rt[0][:], in0=r1, in1=cosb, op=ALU.mult), r=["qnA", "ropecs"], w=["rt0"])
                    T.op("dve", lambda e: e.tensor_tensor(out=rt[1][:], in0=r2, in1=sinb, op=ALU.mult), r=["qnA", "ropecs"], w=["rt1"])
                    T.op("dve", lambda e: e.tensor_tensor(out=rt[2][:], in0=r2, in1=cosb, op=ALU.mult), r=["qnA", "ropecs"], w=["rt2"])
                    T.op("dve", lambda e: e.tensor_tensor(out=rt[3][:], in0=r1, in1=sinb, op=ALU.mult), r=["qnA", "ropecs"], w=["rt3"])
                    qbv = qb[:].rearrange("p (g d) -> p g d", d=64)
                    T.op("dve", lambda e: e.tensor_tensor(out=qbv[:, :, 0:8], in0=rt[0][:], in1=rt[1][:], op=ALU.subtract),
                         r=["rt0", "rt1"], w=[qbk])
                    T.op("dve", lambda e: e.tensor_tensor(out=qbv[:, :, 8:16], in0=rt[2][:], in1=rt[3][:], op=ALU.add),
                         r=["rt2", "rt3"], w=[qbk])
                if cc < 2:
                    qk_tail(NT - 1)
            else:
                for ct in range(4):
                    for tq in range(8):
                        pi = nextps()
                        for j in range(8):
                            T.op("pe", lambda e: e.matmul(ps[pi][:, :], lhsT=wbt[:, j, ct * 128:(ct + 1) * 128],
                                                          rhs=uT[:, j, 1 + tq * 512:1 + (tq + 1) * 512],
                                                          start=(j == 0), stop=(j == 7)),
                                 r=UTK[4 * tq:4 * tq + 4] + [wbk], w=[PK[pi]])
                        if cc < 6:
                            T.op("act", lambda e: e.copy(out=hraw[:, 1 + tq * 512:1 + (tq + 1) * 512], in_=ps[pi][:, :]),
                                 r=[PK[pi]], w=["hraw"])
                        else:
                            T.op("act", lambda e: e.activation(out=gst[:, tq * 512:(tq + 1) * 512], in_=ps[pi][:, :],
                                                               func=AF.Sigmoid), r=[PK[pi]], w=["gstA"])
                    if cc < 6:
                        cti = (cc - 3) * 4 + ct
                        lvl = dbg.get('_lvl', 9)
                        if lvl >= 1:
                          T.op("act", lambda e: e.activation(out=hc[:], in_=hraw[:, 1:L + 1], func=AF.Identity,
                                                           scale=scw[:, cti, 1:2], bias=scw[:, cti, 3:4]),
                             r=["hraw", "scw"], w=["hcA"])
                        if lvl >= 2:
                          T.op("dve", lambda e: e.scalar_tensor_tensor(out=hc[:], in0=hraw[:, 0:L], scalar=scw[:, cti, 0:1],
                                                                     in1=hc[:], op0=ALU.mult, op1=ALU.add),
                             r=["hraw", "hrawp0", "scw", "hcA"], w=["hcA"])
                          T.op("dve", lambda e: e.scalar_tensor_tensor(out=hc[:], in0=hraw[:, 2:L + 2], scalar=scw[:, cti, 2:3],
                                                                     in1=hc[:], op0=ALU.mult, op1=ALU.add),
                             r=["hraw", "hrawp1", "scw", "hcA"], w=["hcA"])
                        if lvl >= 3:
                          for q4 in range(4):
                              T.dma("sp", lambda e: e.dma_start(out=hyc_d[cti * 128:(cti + 1) * 128, q4 * 1024:(q4 + 1) * 1024],
                                                                in_=hc[:, q4 * 1024:(q4 + 1) * 1024]),
                                    r=["hcA"], w=["hyc_d%d" % q4])
                    else:
                        gti = (cc - 6) * 4 + ct
                        T.dmas("sp", [(lambda e, a=a: e.dma_start(out=gateT_d[gti * 128:(gti + 1) * 128, a:a + 2048], in_=gst[:, a:a + 2048]))
                                      for a in (0, 2048)], r=["gstA"], w=["gateT_d"])
        T.barrier()

    if "K" in stages:
      with ExitStack() as es:
        embT = sb(es, "embT", [33, L])
        fw1 = sb(es, "fw1", [33, 64])
        fw2 = sb(es, "fw2", [64, 64])
        fw3 = sb(es, "fw3", [64, 64])
        fbf = sb(es, "fbf", [64, 4])
        frb = sb(es, "frb", [64, 3])
        fwo = sb(es, "fwo", [64, 2048])
        posrow = sb(es, "posrow", [128, L])
        ndelta = sb(es, "ndelta", [128, 4])
        hid = [sb(es, "hid%d" % i, [64, L]) for i in range(2)]
        hidb = sb(es, "hidb", [64, L])
        dec = sb(es, "decK", [128, L])
        kf = sb(es, "kfK", [128, L])
        kb = sb(es, "kbK", [128, L])
        hp = sb(es, "hpK", [128, L], BF16)
        hm = sb(es, "hmK", [128, L], BF16)
        for tns, src, k in ((fw1, fw1_d, "fw1"), (fw2, fw2_d, "fw2"), (fw3, fw3_d, "fw3"),
                            (fbf, fbf_d, "fbf"), (ndelta, ndelta_d, "ndelta")):
            T.dma("sp", lambda e: e.dma_start(out=tns[:], in_=src), w=[k])
        for tns, src, k, n in ((embT, embT_d, "embT", L), (fwo, fwo_d, "fwo", 2048), (posrow, posrow_d, "posrow", L)):
            T.dmas("sp", [(lambda e, a=a: e.dma_start(out=tns[:, a:a + 1024], in_=src[:, a:a + 1024]))
                          for a in range(0, n, 1024)], w=[k])
        for l in range(3):
            T.op("dve", lambda e: e.tensor_tensor(out=frb[:, l:l + 1], in0=fbf[:, l:l + 1], in1=fbf[:, 3:4], op=ALU.mult),
                 r=["fbf"], w=["frb"])
        TWO_PI = 2.0 * math.pi
        srcs = [(embT, "embT", 33, fw1, "fw1"), (hid[0], "hid0", 64, fw2, "fw2"), (hid[1], "hid1", 64, fw3, "fw3")]
        outs = [(hid[0], "hid0"), (hid[1], "hid1"), (hid[0], "hid0")]
        negpi = sb(es, "negpi", [64, 1])
        T.op("dve", lambda e: e.memset(negpi[:], 4.0 * math.pi), w=["negpi"])
        kcnt = sb(es, "kcntK", [64, L])
        for l in range(3):
            src, sk, kk, w_, wk = srcs[l]
            dst, dk = outs[l]
            for tq in range(8):
                pi = tq % 4
                T.op("pe", lambda e: e.matmul(ps[pi][0:64, :], lhsT=w_[0:kk, :], rhs=src[0:kk, tq * 512:(tq + 1) * 512],
                                              start=True, stop=True), r=[sk, wk], w=[PK[pi]])
                T.op("dve", lambda e: e.tensor_scalar(out=hidb[:, tq * 512:(tq + 1) * 512], in0=ps[pi][0:64, :],
                                                      scalar1=fbf[:, 3:4], scalar2=frb[:, l:l + 1], op0=ALU.mult,
                                                      op1=ALU.add), r=[PK[pi], "fbf", "frb"], w=["hidb"])
                xs_ = hidb[:, tq * 512:(tq + 1) * 512]
                ks_ = kcnt[:, tq * 512:(tq + 1) * 512]
                T.op("dve", lambda e: e.tensor_scalar(out=ks_, in0=xs_, scalar1=-3.0 * math.pi, scalar2=None, op0=ALU.is_gt),
                     r=["hidb"], w=["kcnt"])
                for thr in (-math.pi, math.pi, 3.0 * math.pi):
                    T.op("dve", lambda e: e.scalar_tensor_tensor(out=ks_, in0=xs_, scalar=thr, in1=ks_, op0=ALU.is_gt, op1=ALU.add),
                         r=["hidb", "kcnt"], w=["kcnt"])
                T.op("dve", lambda e: e.scalar_tensor_tensor(out=xs_, in0=ks_, scalar=-TWO_PI, in1=xs_, op0=ALU.mult, op1=ALU.add),
                     r=["hidb", "kcnt"], w=["hidb"])
                T.op("act", lambda e: e.activation(out=dst[:, tq * 512:(tq + 1) * 512], in_=xs_,
                                                   func=AF.Sin, bias=negpi[:]), r=["hidb", "negpi"], w=[dk])
        h3 = hid[0]
        for ct in range(4):
            T.op("act", lambda e: e.activation(out=dec[:], in_=posrow[:], func=AF.Exp, scale=ndelta[:, ct:ct + 1]),
                 r=["posrow", "ndelta"], w=["decK"])
            for o in range(2):
                for d_, (dst, dk) in enumerate(((kf, "kfK"), (kb, "kbK"))):
                    col = (o * 2 + d_) * 512 + ct * 128
                    for tq in range(8):
                        pi = tq % 4
                        T.op("pe", lambda e: e.matmul(ps[pi][:, :], lhsT=fwo[:, col:col + 128],
                                                      rhs=h3[:, tq * 512:(tq + 1) * 512], start=True, stop=True),
                             r=["hid0", "fwo"], w=[PK[pi]])
                        T.op("dve", lambda e: e.tensor_tensor(out=dst[:, tq * 512:(tq + 1) * 512], in0=ps[pi][:, :],
                                                              in1=dec[:, tq * 512:(tq + 1) * 512], op=ALU.mult),
                             r=[PK[pi], "decK"], w=[dk])
                T.op("dve", lambda e: e.memset(kb[:, 0:1], 0.0), r=["kbK"], w=["kbK"])
                T.op("pool", lambda e: e.tensor_tensor(out=hp[:], in0=kf[:], in1=kb[:], op=ALU.add), r=["kfK", "kbK"], w=["hpK"])
                T.op("pool", lambda e: e.tensor_tensor(out=hm[:], in0=kf[:], in1=kb[:], op=ALU.subtract), r=["kfK", "kbK"], w=["hmK"])
                T.dmas("sp", [(lambda e, a=a: e.dma_start(out=kern_d[o, 0, ct * 128:(ct + 1) * 128, a:a + 2048], in_=hp[:, a:a + 2048]))
                              for a in range(0, L, 2048)], r=["hpK"], w=["kern_d0"])
                T.dmas("sp", [(lambda e, a=a: e.dma_start(out=kern_d[o, 1, ct * 128:(ct + 1) * 128, a:a + 2048], in_=hm[:, a:a + 2048]))
                              for a in range(0, L, 2048)], r=["hmK"], w=["kern_d1"])
        T.barrier()

    if "B" in stages:
      with ExitStack() as es:
        qT = sb(es, "qT", [128, 4, L], BF16)
        kT = sb(es, "kT", [128, 2, 4, L], BF16)
        V = sb(es, "Vsb", [128, NT, 4, 132], BF16)
        lamv = sb(es, "lamv", [1, 4, 64])
        lamt = sb(es, "lamt", [1, 8])
        ones1 = sb(es, "ones1", [1, 128])
        nlam = sb(es, "nlam", [128, 1])
        subg = sb(es, "subg", [128, 128])
        E = [sb(es, "E%d" % i, [128, 512], BF16) for i in range(3)]
        osb = sb(es, "osbB", [128, 2, 4, 129])
        aacc = sb(es, "aacc", [128, 4, 128])
        ab = sb(es, "abB", [128, 4, 128], BF16)
        junkb = sb(es, "junkB", [128, 4, 128])
        sm = sb(es, "smB", [128, 8, 4])
        aTs = [sb(es, "aTs%d" % i, [128, 512], BF16) for i in range(2)]
        T.dmas("sp", [(lambda e, h=h, a=a: e.dma_start(out=qT[:, h, a:a + 2048], in_=qT_d[h, :, a:a + 2048]))
                      for h in range(4) for a in (0, 2048)], w=["qT"])
        T.op("pool", lambda e: e.memset(kT[64:128, 0, :, :], 0.0), w=["kTz0"])
        T.op("dve", lambda e: e.memset(kT[0:64, 1, :, :], 0.0), w=["kTz1"])
        T.dmas("sp", [(lambda e, h=h, a=a, m=m: e.dma_start(out=kT[m * 64:(m + 1) * 64, m, h, a:a + 2048],
                                                            in_=kT_d[h, m * 64:(m + 1) * 64, a:a + 2048]))
                      for m in range(2) for h in range(4) for a in (0, 2048)], w=["kT"])
        T.dmas("sp", [(lambda e, i=i: e.dma_start(out=V[:, i, :, 0:128],
                                                  in_=v_d[i * 128:(i + 1) * 128, :].rearrange("p (h d) -> p h d", d=128)))
                      for i in range(NT)], w=["V"])
        T.op("pool", lambda e: e.memset(V[:, :, :, 128:129], 1.0), w=["Vones"])
        T.dma("sp", lambda e: e.dma_start(out=lamv[:], in_=lam_d[:, :, :]), w=["lamv"])
        T.dma("sp", lambda e: e.dma_start(out=subg[:], in_=subg_d[:, :]), w=["subg"])
        T.op("dve", lambda e: e.tensor_scalar(out=subg[:], in0=subg[:], scalar1=0.8, scalar2=None, op0=ALU.mult),
             r=["subg"], w=["subg"])
        T.op("dve", lambda e: e.memset(ones1[:], 1.0), w=["ones1"])
        T.op("dve", lambda e: e.memset(lamt[:], 0.0), w=["lamt"])
        T.op("dve", lambda e: e.tensor_tensor(out=lamv[:, 0, :], in0=lamv[:, 0, :], in1=lamv[:, 1, :], op=ALU.mult), r=["lamv"], w=["lamv"])
        T.op("dve", lambda e: e.tensor_tensor(out=lamv[:, 2, :], in0=lamv[:, 2, :], in1=lamv[:, 3, :], op=ALU.mult), r=["lamv"], w=["lamv"])
        T.op("dve", lambda e: e.tensor_reduce(out=lamt[:, 0:1], in_=lamv[:, 0, :], axis=AX.X, op=ALU.add), r=["lamv", "lamt"], w=["lamt"])
        T.op("dve", lambda e: e.tensor_reduce(out=lamt[:, 1:2], in_=lamv[:, 2, :], axis=AX.X, op=ALU.add), r=["lamv", "lamt"], w=["lamt"])
        T.op("act", lambda e: e.activation(out=lamt[:, 2:4], in_=lamt[:, 0:2], func=AF.Exp), r=["lamt"], w=["lamt"])
        T.op("dve", lambda e: e.tensor_tensor(out=lamt[:, 4:5], in0=lamt[:, 3:4], in1=lamt[:, 2:3], op=ALU.subtract), r=["lamt"], w=["lamt"])
        T.op("dve", lambda e: e.tensor_scalar(out=lamt[:, 5:6], in0=lamt[:, 4:5], scalar1=-0.2, scalar2=None, op0=ALU.add), r=["lamt"], w=["lamt"])
        T.op("pe", lambda e: e.matmul(ps[7][:, 0:1], lhsT=ones1[:, :], rhs=lamt[:, 5:6], start=True, stop=True),
             r=["ones1", "lamt"], w=[PK[7]])
        T.op("dve", lambda e: e.tensor_copy(out=nlam[:], in_=ps[7][:, 0:1]), r=[PK[7]], w=["nlam"])
        psT = ps[6][:, :].bitcast(BF16)
        steps = [(h, qc, m, kt) for h in range(4) for qc in range(8) for m in range(2) for kt in range(NT)]
        USE_POW = False

        def issue_S(idx):
            h, qc, m, kt = steps[idx]
            sp_ = idx % 2
            eb = idx % 3
            T.op("pe", lambda e: e.matmul(ps[sp_][:, :], lhsT=kT[:, m, h, kt * 128:(kt + 1) * 128],
                                          rhs=qT[:, h, qc * 512:(qc + 1) * 512], start=True, stop=True),
                 r=["kT", "kTz0", "kTz1", "qT"], w=[PK[sp_]])
            T.op("act", lambda e: e.activation(out=E[eb][:], in_=ps[sp_][:, :], func=AF.Exp),
                 r=[PK[sp_]], w=["E%d" % eb])

        def issue_PV(idx):
            h, qc, m, kt = steps[idx]
            eb = idx % 3
            for qs in range(4):
                bank = 2 + m * 2 + qs // 2
                T.op("pe", lambda e: e.matmul(ps[bank][:, (qs % 2) * 256:(qs % 2) * 256 + 129],
                                              lhsT=E[eb][:, qs * 128:(qs + 1) * 128], rhs=V[:, kt, h, 0:129],
                                              start=(kt == 0 and qs % 2 == 0), stop=(kt == NT - 1),
                                              skip_group_check=True),
                     r=["E%d" % eb, "V", "Vones"], w=[PK[bank]])

        def bc4(ap2):
            return ap2.unsqueeze(2).broadcast_to([128, 4, 128])

        def epi_head():
            for m in range(2):
                for half in range(2):
                    bank = 2 + m * 2 + half
                    src = ps[bank][:, :].rearrange("p (a c) -> p a c", a=2)[:, :, 0:129]
                    dst = osb[:, m, half * 2:(half + 1) * 2, :]
                    if half == 0:
                        T.op("dve", lambda e: e.tensor_copy(out=dst, in_=src), r=[PK[bank]], w=["osb%d%d" % (m, half)])
                    else:
                        T.op("pool" if False else "dve", lambda e: e.tensor_copy(out=dst, in_=src), r=[PK[bank]], w=["osb%d%d" % (m, half)])
            OS = ["osb00", "osb01", "osb10", "osb11"]
            T.op("dve", lambda e: e.reciprocal(out=sm[:, 0:2, :], in_=osb[:, :, :, 128]), r=OS, w=["smB"])
            T.op("dve", lambda e: e.tensor_scalar(out=sm[:, 2, :], in0=sm[:, 1, :], scalar1=nlam[:, 0:1], scalar2=None, op0=ALU.mult),
                 r=["smB", "nlam"], w=["smB"])
            T.op("dve", lambda e: e.tensor_tensor(out=aacc[:], in0=osb[:, 0, :, 0:128], in1=bc4(sm[:, 0, :]), op=ALU.mult),
                 r=OS + ["smB"], w=["aacc"])
            T.op("dve", lambda e: e.tensor_tensor(out=junkb[:], in0=osb[:, 1, :, 0:128], in1=bc4(sm[:, 2, :]), op=ALU.mult),
                 r=OS + ["smB"], w=["junkB"])
            T.op("dve", lambda e: e.tensor_tensor(out=aacc[:], in0=aacc[:], in1=junkb[:], op=ALU.add), r=["aacc", "junkB"], w=["aacc"])
            T.op("dve", lambda e: e.tensor_tensor(out=junkb[:], in0=aacc[:], in1=aacc[:], op=ALU.mult), r=["aacc"], w=["junkB"])
            T.op("dve", lambda e: e.tensor_reduce(out=sm[:, 3, :], in_=junkb[:], axis=AX.X, op=ALU.add), r=["junkB", "smB"], w=["smB"])
            T.op("dve", lambda e: e.tensor_scalar(out=sm[:, 4, :], in0=sm[:, 3, :], scalar1=1.0 / 128, scalar2=1e-6, op0=ALU.mult, op1=ALU.add),
                 r=["smB"], w=["smB"])
            if USE_POW:
                T.op("dve", lambda e: e.tensor_scalar(out=sm[:, 5, :], in0=sm[:, 4, :], scalar1=-0.5, scalar2=None, op0=ALU.pow),
                     r=["smB"], w=["smB"])
            else:
                T.op("act", lambda e: e.activation(out=sm[:, 6, :], in_=sm[:, 4, :], func=AF.Sqrt), r=["smB"], w=["smB"])
                T.op("dve", lambda e: e.reciprocal(out=sm[:, 5, :], in_=sm[:, 6, :]), r=["smB"], w=["smB"])
            T.op("dve", lambda e: e.tensor_tensor(out=aacc[:], in0=aacc[:], in1=bc4(sm[:, 5, :]), op=ALU.mult), r=["aacc", "smB"], w=["aacc"])
            T.op("dve", lambda e: e.tensor_tensor(out=ab[:], in0=aacc[:], in1=subg[:, :].unsqueeze(1).broadcast_to([128, 4, 128]), op=ALU.mult),
                 r=["aacc", "subg"], w=["abB"])

        def epi_tail(h, qc):
            for qs in range(4):
                T.op("pe", lambda e: e.transpose(out=psT[:, qs * 128:(qs + 1) * 128], in_=ab[:, qs, :], identity=ident_b[:]),
                     r=["abB", "ident_b"], w=[PK[6]])
            st = aTs[qc % 2]
            T.op("act", lambda e: e.copy(out=st[:], in_=psT[:, 0:512]), r=[PK[6]], w=["aTs%d" % (qc % 2)])
            T.dma("sp", lambda e: e.dma_start(out=attnT_d[h * 128:(h + 1) * 128, qc * 512:(qc + 1) * 512], in_=st[:]),
                  r=["aTs%d" % (qc % 2)], w=["attnT_d"])

        pending = None
        issue_S(0)
        for idx in range(len(steps)):
            h, qc, m, kt = steps[idx]
            if idx + 1 < len(steps):
                issue_S(idx + 1)
            issue_PV(idx)
            if pending is not None and m == 0 and kt == 12:
                epi_tail(*pending)
                pending = None
            if m == 1 and kt == NT - 1:
                epi_head()
                pending = (h, qc)
        epi_tail(*pending)
        T.barrier()

    if "C" in stages:
      with ExitStack() as es:
        FA = sb(es, "FA", [32, 128], BF16)
        TGr = sb(es, "TGr", [128, 64, 128], BF16)
        TGi = sb(es, "TGi", [128, 64, 128], BF16)
        TGn = sb(es, "TGn", [128, 64, 128], BF16)
        R1 = sb(es, "R1", [128, 256], BF16)
        R2 = sb(es, "R2", [128, 256], BF16)
        TH = sb(es, "TH", [64, 128, 2, 32], BF16)
        skipbc = sb(es, "skipbc", [32, 2, GC])
        for tns, src, k in ((FA, FA_d, "FA"), (R1, R1_d, "R1"), (R2, R2_d, "R2")):
            T.dma("sp", lambda e: e.dma_start(out=tns[:], in_=src), w=[k])
        for tns, src, k in ((TGr, TGr_d, "TGr"), (TGi, TGi_d, "TGi"), (TGn, TGn_d, "TGn")):
            T.dmas("sp", [(lambda e, a=a: e.dma_start(out=tns[:, a:a + 16, :], in_=src[:, a:a + 16, :])) for a in range(0, 64, 16)], w=[k])
        T.dmas("sp", [(lambda e, a=a: e.dma_start(out=TH[:, a:a + 32, :, :], in_=TH_d[:, a:a + 32, :, :])) for a in range(0, 128, 32)], w=["TH"])
        zf = sb(es, "zf", [32, GC, 128])
        gt = sb(es, "gtC", [32, GC, 128])
        zs = sb(es, "zsC", [32, GC, 128])
        srcb = [sb(es, "srcb%d" % i, [32, GC, 128], BF16) for i in range(3)]
        Bm_ = [sb(es, "Bm%d" % i, [128, 128, GC], BF16) for i in range(3)]
        srs = sb(es, "srsC", [128, 512])
        sis = sb(es, "sisC", [128, 512])
        tm = [sb(es, "tmC%d" % i, [128, 512]) for i in range(4)]
        Y = sb(es, "YC", [128, GC, 2, 64], BF16)
        Dm = sb(es, "DmC", [64, 256, GC], BF16)
        tmpy = sb(es, "tmpyC", [32, GC, 16])
        hob = srcb[0]

        def flay(ap2d):
            return ap2d.rearrange("c (a b) -> a c b", b=128)

        def load_filters(g_, o_):
            for pm in range(2):
                T.dma("sp", lambda e: e.dma_start(out=srcb[1 + pm][:], in_=flay(kern_d[o_, pm, g_ * GC:(g_ + 1) * GC, :])),
                      w=["srcb%d" % (1 + pm)])

        for g in range(512 // GC):
            c0 = g * GC
            T.dma("sp", lambda e: e.dma_start(out=zf[:], in_=flay(hyc_d[c0:c0 + GC, :])), w=["zf"])
            T.dma("sp", lambda e: e.dma_start(out=skipbc[:], in_=skip_d[:, :, c0:c0 + GC]), w=["skipbc"])
            for o in range(2):
                T.dma("sp", lambda e: e.dma_start(out=gt[:], in_=flay(hyc_d[512 * (o + 1) + c0:512 * (o + 1) + c0 + GC, :])),
                      w=["gtC"])
                T.op("act", lambda e: e.copy(out=srcb[0][:], in_=zf[:]), r=["zf"], w=["srcb0"])
                if g == 0 and o == 0:
                    load_filters(0, 0)
                T.op("pool", lambda e: e.tensor_tensor(out=zs[:], in0=zf[:],
                                                       in1=skipbc[:, o, :].unsqueeze(2).broadcast_to([32, GC, 128]),
                                                       op=ALU.mult), r=["zf", "skipbc"], w=["zsC"])
                for s_ in (1, 2, 0):
                    for cq in range(GC // 4):
                        pi = cq % 2
                        for c4 in range(4):
                            c = cq * 4 + c4
                            T.op("pe", lambda e: e.matmul(ps[pi][:, c4 * 128:(c4 + 1) * 128], lhsT=srcb[s_][:, c, :],
                                                          rhs=FA[:, :], start=True, stop=True),
                                 r=["srcb%d" % s_, "FA"], w=[PK[pi]])
                        T.op("act" if cq % 2 == 0 else "dve",
                             lambda e: (e.copy if cq % 2 == 0 else e.tensor_copy)(
                                 out=Bm_[s_][:, :, cq * 4:(cq + 1) * 4],
                                 in_=ps[pi][:, :].rearrange("p (c f) -> p f c", c=4)),
                             r=[PK[pi]], w=["Bm%d" % s_])
                nxt = (g, 1) if o == 0 else (g + 1, 0)
                if nxt[0] < 512 // GC:
                    load_filters(*nxt)
                for kc in range(4):
                    bZr, bZi, bSr, bSi = (2, 3, 4, 5) if kc % 2 == 0 else (0, 1, 6, 7)
                    for j in range(16):
                        k1 = kc * 16 + j
                        cs_ = slice(j * GC, (j + 1) * GC)
                        bz, bp, bm = Bm_[0], Bm_[1], Bm_[2]
                        T.op("pe", lambda e: e.matmul(ps[bZr][:, cs_], lhsT=TGr[:, k1, :], rhs=bz[:, k1, :], start=True, stop=False), r=["TGr", "Bm0"], w=[PK[bZr]])
                        T.op("pe", lambda e: e.matmul(ps[bZr][:, cs_], lhsT=TGn[:, k1, :], rhs=bz[:, 64 + k1, :], start=False, stop=True), r=["TGn", "Bm0"], w=[PK[bZr]])
                        T.op("pe", lambda e: e.matmul(ps[bZi][:, cs_], lhsT=TGi[:, k1, :], rhs=bz[:, k1, :], start=True, stop=False), r=["TGi", "Bm0"], w=[PK[bZi]])
                        T.op("pe", lambda e: e.matmul(ps[bZi][:, cs_], lhsT=TGr[:, k1, :], rhs=bz[:, 64 + k1, :], start=False, stop=True), r=["TGr", "Bm0"], w=[PK[bZi]])
                        T.op("pe", lambda e: e.matmul(ps[bSr][:, cs_], lhsT=TGr[:, k1, :], rhs=bp[:, k1, :], start=True, stop=False), r=["TGr", "Bm1"], w=[PK[bSr]])
                        T.op("pe", lambda e: e.matmul(ps[bSr][:, cs_], lhsT=TGn[:, k1, :], rhs=bp[:, 64 + k1, :], start=False, stop=True), r=["TGn", "Bm1"], w=[PK[bSr]])
                        T.op("pe", lambda e: e.matmul(ps[bSi][:, cs_], lhsT=TGi[:, k1, :], rhs=bm[:, k1, :], start=True, stop=False), r=["TGi", "Bm2"], w=[PK[bSi]])
                        T.op("pe", lambda e: e.matmul(ps[bSi][:, cs_], lhsT=TGr[:, k1, :], rhs=bm[:, 64 + k1, :], start=False, stop=True), r=["TGr", "Bm2"], w=[PK[bSi]])
                    T.op("act", lambda e: e.copy(out=srs[:], in_=ps[bSr][:, :]), r=[PK[bSr]], w=["srsC"])
                    T.op("act", lambda e: e.copy(out=sis[:], in_=ps[bSi][:, :]), r=[PK[bSi]], w=["sisC"])
                    T.op("dve", lambda e: e.tensor_tensor(out=tm[0][:], in0=ps[bZr][:, :], in1=srs[:], op=ALU.mult), r=[PK[bZr], "srsC"], w=["tm0"])
                    T.op("dve", lambda e: e.tensor_tensor(out=tm[1][:], in0=ps[bZi][:, :], in1=sis[:], op=ALU.mult), r=[PK[bZi], "sisC"], w=["tm1"])
                    T.op("dve", lambda e: e.tensor_tensor(out=tm[2][:], in0=ps[bZr][:, :], in1=sis[:], op=ALU.mult), r=[PK[bZr], "sisC"], w=["tm2"])
                    T.op("dve", lambda e: e.tensor_tensor(out=tm[3][:], in0=ps[bZi][:, :], in1=srs[:], op=ALU.mult), r=[PK[bZi], "srsC"], w=["tm3"])
                    yv_r = Y[:, :, 0, kc * 16:(kc + 1) * 16]
                    yv_i = Y[:, :, 1, kc * 16:(kc + 1) * 16]
                    T.op("pool", lambda e: e.tensor_tensor(out=yv_r, in0=tm[0][:].rearrange("p (k c) -> p c k", c=GC),
                                                           in1=tm[1][:].rearrange("p (k c) -> p c k", c=GC), op=ALU.subtract),
                         r=["tm0", "tm1"], w=["YC"])
                    T.op("pool", lambda e: e.tensor_tensor(out=yv_i, in0=tm[2][:].rearrange("p (k c) -> p c k", c=GC),
                                                           in1=tm[3][:].rearrange("p (k c) -> p c k", c=GC), op=ALU.add),
                         r=["tm2", "tm3"], w=["YC"])
                for cq in range(GC // 2):
                    pi = cq % 2
                    for c2 in range(2):
                        c = cq * 2 + c2
                        T.op("pe", lambda e: e.matmul(ps[pi][0:64, c2 * 256:(c2 + 1) * 256], lhsT=Y[:, c, 0, :], rhs=R1[:, :],
                                                      start=True, stop=False), r=["YC", "R1"], w=[PK[pi]])
                        T.op("pe", lambda e: e.matmul(ps[pi][0:64, c2 * 256:(c2 + 1) * 256], lhsT=Y[:, c, 1, :], rhs=R2[:, :],
                                                      start=False, stop=True), r=["YC", "R2"], w=[PK[pi]])
                    T.op("act" if cq % 2 == 0 else "dve",
                         lambda e: (e.copy if cq % 2 == 0 else e.tensor_copy)(
                             out=Dm[:, :, cq * 2:(cq + 1) * 2],
                             in_=ps[pi][0:64, :].rearrange("p (c f) -> p f c", c=2)),
                         r=[PK[pi]], w=["DmC"])
                for nq in range(8):
                    pi = 6 + nq % 2
                    for j in range(16):
                        n2 = nq * 16 + j
                        T.op("pe", lambda e: e.matmul(ps[pi][0:32, j * GC:(j + 1) * GC], lhsT=TH[:, n2, 0, :], rhs=Dm[:, n2, :],
                                                      start=True, stop=False), r=["TH", "DmC"], w=[PK[pi]])
                        T.op("pe", lambda e: e.matmul(ps[pi][0:32, j * GC:(j + 1) * GC], lhsT=TH[:, n2, 1, :], rhs=Dm[:, 128 + n2, :],
                                                      start=False, stop=True), r=["TH", "DmC"], w=[PK[pi]])
                    nsl = slice(nq * 16, (nq + 1) * 16)
                    T.op("dve", lambda e: e.tensor_tensor(out=tmpy[:], in0=ps[pi][0:32, :].rearrange("p (n c) -> p c n", c=GC),
                                                          in1=zs[:, :, nsl], op=ALU.add), r=[PK[pi], "zsC"], w=["tmpyC"])
                    T.op("dve", lambda e: e.tensor_tensor(out=zf[:, :, nsl], in0=tmpy[:], in1=gt[:, :, nsl], op=ALU.mult),
                         r=["tmpyC", "gtC", "srcb0", "zsC"], w=["zf"])
            T.op("act", lambda e: e.copy(out=hob[:], in_=zf[:]), r=["zf"], w=["srcb0"])
            T.dma("sp", lambda e: e.dma_start(out=flay(hyoT_d[c0:c0 + GC, :]), in_=hob[:]), r=["srcb0"], w=["hyoT_d"])
        T.barrier()

    if "D" in stages:
      with ExitStack() as es:
        aff_all = sb(es, "aff_all", [128, NT, NE])
        with ExitStack() as es2:
            attnT = sb(es2, "attnT", [128, 4, L], BF16)
            hyoT = sb(es2, "hyoT", [128, 4, L], BF16)
            wpa = sb(es2, "wpa", [128, 4, D], BF16)
            wph = sb(es2, "wph", [128, 4, D], BF16)
            wo = sb(es2, "wo", [128, 8, D], BF16)
            wst = sb(es2, "wstD", [128, 8, D])
            g2bc = sb(es2, "g2bc", [128, D])
            wr = sb(es2, "wr", [128, 8, 16])
            gat = sb(es2, "gatD", [128, 16, 512], BF16)
            m1 = sb(es2, "m1D", [128, 512])
            m2 = sb(es2, "m2D", [128, 512])
            mT = sb(es2, "mTD", [128, 8, 512], BF16)
            xin = sb(es2, "xinD", [128, D])
            x1 = sb(es2, "x1D", [128, D])
            u2 = sb(es2, "u2D", [128, D])
            u2b = sb(es2, "u2bD", [128, D], BF16)
            u2T = sb(es2, "u2TD", [128, 8, 128])
            junkd = sb(es2, "junkD", [128, D])
            st = sb(es2, "stD", [128, 8])
            lg = sb(es2, "lgD", [128, NE])
            for (tt_, td_, tk_) in ((attnT, attnT_d, "attnT"), (hyoT, hyoT_d, "hyoT")):
                T.dmas("sp", [(lambda e, j=j, a=a: e.dma_start(out=tt_[:, j, a:a + 2048], in_=td_[j * 128:(j + 1) * 128, a:a + 2048]))
                              for j in range(4) for a in (0, 2048)], w=[tk_])
            T.dma("sp", lambda e: e.dma_start(out=g2bc[:], in_=g2bc_d[:, :]), w=["g2bc"])
            T.dma("sp", lambda e: e.dma_start(out=wr[:], in_=wr_d[:, :, :]), w=["wr"])
            for (wsrc, nj, wdst, wk) in ((wpa_d, 4, wpa, "wpa"), (wph_d, 4, wph, "wph"), (wo_d, 8, wo, "wo")):
                T.dma("sp", lambda e: e.dma_start(out=wst[:, 0:nj, :], in_=wsrc.rearrange("(j p) c -> p j c", p=128)), w=["wstD"])
                T.op("pool", lambda e: e.tensor_copy(out=wdst[:], in_=wst[:, 0:nj, :]), r=["wstD"], w=[wk])
            for tq in range(8):
                tsl = slice(tq * 512, (tq + 1) * 512)
                T.dmas("sp", [(lambda e, a=a: e.dma_start(out=gat[:, a:a + 8, :],
                                                          in_=gateT_d.rearrange("(j p) t -> p j t", p=128)[:, a:a + 8, tsl])) for a in (0, 8)],
                       w=["gatD"])
                for dt in range(8):
                    for j in range(4):
                        T.op("pe", lambda e: e.matmul(ps[0][:, :], lhsT=wpa[:, j, dt * 128:(dt + 1) * 128], rhs=attnT[:, j, tsl],
                                                      start=(j == 0), stop=(j == 3)), r=["wpa", "attnT"], w=[PK[0]])
                    for j in range(4):
                        T.op("pe", lambda e: e.matmul(ps[1][:, :], lhsT=wph[:, j, dt * 128:(dt + 1) * 128], rhs=hyoT[:, j, tsl],
                                                      start=(j == 0), stop=(j == 3)), r=["wph", "hyoT"], w=[PK[1]])
                    T.op("dve", lambda e: e.tensor_tensor(out=m1[:], in0=ps[0][:, :], in1=gat[:, dt, :], op=ALU.mult), r=[PK[0], "gatD"], w=["m1D"])
                    T.op("dve", lambda e: e.tensor_tensor(out=m2[:], in0=ps[1][:, :], in1=gat[:, 8 + dt, :], op=ALU.mult), r=[PK[1], "gatD"], w=["m2D"])
                    T.op("pool", lambda e: e.tensor_tensor(out=mT[:, dt, :], in0=m1[:], in1=m2[:], op=ALU.add), r=["m1D", "m2D"], w=["mTD"])
                for ts in range(4):
                    i = tq * 4 + ts
                    T.dma("sp", lambda e: e.dma_start(out=xin[:], in_=x_d[i * 128:(i + 1) * 128, :]), w=["xinD"])
                    for half in range(2):
                        for dt in range(8):
                            T.op("pe", lambda e: e.matmul(ps[2 + half][:, :], lhsT=mT[:, dt, ts * 128:(ts + 1) * 128],
                                                          rhs=wo[:, dt, half * 512:(half + 1) * 512], start=(dt == 0), stop=(dt == 7)),
                                 r=["mTD", "wo"], w=[PK[2 + half]])
                        T.op("dve", lambda e: e.tensor_tensor(out=x1[:, half * 512:(half + 1) * 512], in0=ps[2 + half][:, :],
                                                              in1=xin[:, half * 512:(half + 1) * 512], op=ALU.add),
                             r=[PK[2 + half], "xinD"], w=["x1D"])
                    T.dma("sp", lambda e: e.dma_start(out=out_d[i * 128:(i + 1) * 128, :], in_=x1[:]), r=["x1D"], w=["out_d"])
                    T.op("dve", lambda e: e.memset(st[:, 0:1], 0.0), w=["stD"])
                    T.op("act", lambda e: e.activation(out=junkd[:], in_=x1[:], func=AF.Square, accum_out=st[:, 0:1]),
                         r=["x1D", "stD"], w=["junkD", "stD"])
                    T.op("act", lambda e: e.activation(out=st[:, 1:2], in_=st[:, 0:1], func=AF.Sqrt, scale=1.0 / D, bias=eps[:]),
                         r=["stD", "eps"], w=["stD"])
                    T.op("dve", lambda e: e.reciprocal(out=st[:, 2:3], in_=st[:, 1:2]), r=["stD"], w=["stD"])
                    T.op("dve", lambda e: e.scalar_tensor_tensor(out=u2[:], in0=x1[:], scalar=st[:, 2:3], in1=g2bc[:],
                                                                 op0=ALU.mult, op1=ALU.mult), r=["x1D", "stD", "g2bc"], w=["u2D"])
                    T.op("act", lambda e: e.copy(out=u2b[:], in_=u2[:]), r=["u2D"], w=["u2bD"])
                    T.dma("sp", lambda e: e.dma_start(out=u2_d[i * 128:(i + 1) * 128, :], in_=u2b[:]), r=["u2bD"], w=["u2_d"])
                    for j in range(8):
                        T.op("pe", lambda e: e.transpose(out=ps[4 + j // 4][:, (j % 4) * 128:(j % 4 + 1) * 128],
                                                         in_=u2[:, j * 128:(j + 1) * 128], identity=ident_f[:]),
                             r=["u2D", "ident_f"], w=[PK[4 + j // 4]])
                    for hh in range(2):
                        T.op("act", lambda e: e.copy(out=u2T[:, hh * 4:(hh + 1) * 4, :],
                                                     in_=ps[4 + hh][:, :].rearrange("p (j t) -> p j t", j=4)),
                             r=[PK[4 + hh]], w=["u2TD"])
                    for j in range(8):
                        T.op("pe", lambda e: e.matmul(ps[6][:, 0:NE], lhsT=u2T[:, j, :], rhs=wr[:, j, :], start=(j == 0), stop=(j == 7)),
                             r=["u2TD", "wr"], w=[PK[6]])
                    T.op("dve", lambda e: e.tensor_reduce(out=st[:, 3:4], in_=ps[6][:, 0:NE], axis=AX.X, op=ALU.max), r=[PK[6], "stD"], w=["stD"])
                    T.op("dve", lambda e: e.tensor_scalar(out=st[:, 4:5], in0=st[:, 3:4], scalar1=-1.0, scalar2=None, op0=ALU.mult), r=["stD"], w=["stD"])
                    T.op("dve", lambda e: e.memset(st[:, 5:6], 0.0), r=["stD"], w=["stD"])
                    T.op("act", lambda e: e.activation(out=lg[:], in_=ps[6][:, 0:NE], func=AF.Exp, bias=st[:, 4:5], accum_out=st[:, 5:6]),
                         r=[PK[6], "stD"], w=["lgD", "stD"])
                    T.op("dve", lambda e: e.reciprocal(out=st[:, 6:7], in_=st[:, 5:6]), r=["stD"], w=["stD"])
                    T.op("dve", lambda e: e.tensor_scalar(out=aff_all[:, i, :], in0=lg[:], scalar1=st[:, 6:7], scalar2=None, op0=ALU.mult),
                         r=["lgD", "stD"], w=["aff_all"])
            T.barrier()
        if "aff" in dbg_d:
            T.dma("sp", lambda e: e.dma_start(out=dbg_d["aff"], in_=aff_all[:]), r=["aff_all"], w=["dbgaff"])

        if "E" in stages:
          idx_all = sb(es, "idx_all", [128, NE, 4], I32)
          g_all = sb(es, "g_all", [128, NE, 4])
          with ExitStack() as es2:
            affT = sb(es2, "affT", [NE, L])
            cmpj = sb(es2, "cmpj", [NE, L])
            bs = sb(es2, "bsE", [NE, 8])
            tri = sb(es2, "tri", [128, 128])
            onesf = sb(es2, "onesf", [128, 128])
            iota = sb(es2, "iota", [128, 512])
            tidhl = sb(es2, "tidhl", [128, NT, 2], BF16)
            thbc = sb(es2, "thbc", [128, NE])
            dg = sb(es2, "dgE", [NE, NE])
            M = sb(es2, "ME", [128, NT, NE])
            pos = sb(es2, "posE", [128, NT, NE])
            srun = sb(es2, "srunE", [128, NE])
            cum = sb(es2, "cumE", [128, NE])
            vals = sb(es2, "valsE", [128, NT, NE, 4], BF16)
            afh = sb(es2, "afhE", [128, NT, NE], BF16)
            afr = sb(es2, "afrE", [128, NT, NE])
            oh = [sb(es2, "ohE%d" % i, [128, 512], BF16) for i in range(4)]
            idf = sb(es2, "idfE", [128, 4])
            pvs = sb(es2, "pvsE", [128, 16])
            T.dma("sp", lambda e: e.dma_start(out=tri[:], in_=tri_d[:, :]), w=["tri"])
            T.dma("sp", lambda e: e.dma_start(out=iota[:], in_=iota_d[:, :]), w=["iota"])
            T.dma("sp", lambda e: e.dma_start(out=tidhl[:], in_=tidhl_d[:, :, :]), w=["tidhl"])
            T.op("pool", lambda e: e.memset(onesf[:], 1.0), w=["onesf"])
            for i in range(NT):
                pi = i // 4
                T.op("pe", lambda e: e.transpose(out=ps[pi][0:NE, (i % 4) * 128:(i % 4 + 1) * 128], in_=aff_all[:, i, :],
                                                 identity=ident_f[:]), r=["aff_all", "ident_f"], w=[PK[pi]])
                if i % 4 == 3:
                    T.op("act", lambda e: e.copy(out=affT[:, (i - 3) * 128:(i + 1) * 128], in_=ps[pi][0:NE, :]), r=[PK[pi]], w=["affT"])
            T.op("dve", lambda e: e.memset(bs[:, 0:1], 0.0), w=["bsE"])
            T.op("dve", lambda e: e.memset(bs[:, 1:2], 1.0), r=["bsE"], w=["bsE"])
            for it in range(32):
                T.op("dve", lambda e: e.tensor_scalar(out=bs[:, 2:3], in0=bs[:, 0:1], scalar1=bs[:, 1:2], scalar2=0.5, op0=ALU.add, op1=ALU.mult), r=["bsE"], w=["bsE"])
                T.op("dve", lambda e: e.tensor_scalar(out=cmpj[:], in0=affT[:], scalar1=bs[:, 2:3], scalar2=None, op0=ALU.is_ge), r=["affT", "bsE"], w=["cmpj"])
                T.op("dve", lambda e: e.tensor_reduce(out=bs[:, 3:4], in_=cmpj[:], axis=AX.X, op=ALU.add), r=["cmpj", "bsE"], w=["bsE"])
                T.op("dve", lambda e: e.tensor_scalar(out=bs[:, 4:5], in0=bs[:, 3:4], scalar1=CAP - 0.5, scalar2=None, op0=ALU.is_ge), r=["bsE"], w=["bsE"])
                T.op("dve", lambda e: e.tensor_tensor(out=bs[:, 5:6], in0=bs[:, 2:3], in1=bs[:, 0:1], op=ALU.subtract), r=["bsE"], w=["bsE"])
                T.op("dve", lambda e: e.tensor_tensor(out=bs[:, 6:7], in0=bs[:, 1:2], in1=bs[:, 2:3], op=ALU.subtract), r=["bsE"], w=["bsE"])
                T.op("dve", lambda e: e.scalar_tensor_tensor(out=bs[:, 0:1], in0=bs[:, 5:6], scalar=bs[:, 4:5], in1=bs[:, 0:1], op0=ALU.mult, op1=ALU.add), r=["bsE"], w=["bsE"])
                T.op("dve", lambda e: e.scalar_tensor_tensor(out=bs[:, 1:2], in0=bs[:, 6:7], scalar=bs[:, 4:5], in1=bs[:, 2:3], op0=ALU.mult, op1=ALU.add), r=["bsE"], w=["bsE"])
            T.op("dve", lambda e: e.tensor_scalar(out=dg[:], in0=ident_f[0:NE, 0:NE], scalar1=bs[:, 0:1], scalar2=None, op0=ALU.mult), r=["bsE", "ident_f"], w=["dgE"])
            T.op("pe", lambda e: e.matmul(ps[0][:, 0:NE], lhsT=onesf[0:NE, :], rhs=dg[:, :], start=True, stop=True), r=["onesf", "dgE"], w=[PK[0]])
            T.op("dve", lambda e: e.tensor_copy(out=thbc[:], in_=ps[0][:, 0:NE]), r=[PK[0]], w=["thbc"])
            T.op("dve", lambda e: e.tensor_tensor(out=M[:], in0=aff_all[:], in1=thbc[:, :].unsqueeze(1).broadcast_to([128, NT, NE]), op=ALU.is_ge),
                 r=["aff_all", "thbc"], w=["ME"])
            T.op("dve", lambda e: e.memset(srun[:], 0.0), w=["srunE"])
            for i in range(NT):
                pi = 1 + i % 2
                T.op("pe", lambda e: e.matmul(ps[pi][:, 0:NE], lhsT=tri[:, :], rhs=M[:, i, :], start=True, stop=True), r=["tri", "ME"], w=[PK[pi]])
                T.op("pe", lambda e: e.matmul(ps[pi][:, NE:2 * NE], lhsT=onesf[:, :], rhs=M[:, i, :], start=True, stop=True), r=["onesf", "ME"], w=[PK[pi]])
                T.op("dve", lambda e: e.tensor_tensor(out=cum[:], in0=ps[pi][:, 0:NE], in1=srun[:], op=ALU.add), r=[PK[pi], "srunE"], w=["cumE"])
                T.op("dve", lambda e: e.tensor_tensor(out=srun[:], in0=ps[pi][:, NE:2 * NE], in1=srun[:], op=ALU.add), r=[PK[pi], "srunE", "cumE"], w=["srunE"])
                T.op("dve", lambda e: e.tensor_tensor(out=cum[:], in0=cum[:], in1=M[:, i, :], op=ALU.mult), r=["cumE", "ME"], w=["cumE"])
                T.op("dve", lambda e: e.tensor_scalar(out=pos[:, i, :], in0=cum[:], scalar1=-1.0, scalar2=None, op0=ALU.add), r=["cumE"], w=["posE"])
            T.op("dve", lambda e: e.tensor_copy(out=afh[:], in_=aff_all[:]), r=["aff_all"], w=["afhE"])
            T.op("dve", lambda e: e.tensor_tensor(out=afr[:], in0=aff_all[:], in1=afh[:], op=ALU.subtract), r=["aff_all", "afhE"], w=["afrE"])
            T.op("dve", lambda e: e.tensor_copy(out=vals[:, :, :, 2], in_=afh[:]), r=["afhE"], w=["valsE"])
            T.op("dve", lambda e: e.tensor_copy(out=vals[:, :, :, 3], in_=afr[:]), r=["afrE", "valsE"], w=["valsE"])
            T.op("dve", lambda e: e.tensor_copy(out=vals[:, :, :, 0:2], in_=tidhl[:, :, :].unsqueeze(2).broadcast_to([128, NT, NE, 2])),
                 r=["tidhl", "valsE"], w=["valsE"])
            oc = 0
            for ex in range(NE):
                pi = 3 + ex % 2
                for i in range(NT):
                    ob = oc % 4
                    eng = "dve"
                    oc += 1
                    T.op(eng, lambda e: e.tensor_scalar(out=oh[ob][:], in0=iota[:], scalar1=pos[:, i, ex:ex + 1], scalar2=None, op0=ALU.is_equal),
                         r=["iota", "posE"], w=["ohE%d" % ob])
                    for sc in range(4):
                        T.op("pe", lambda e: e.matmul(ps[pi][:, sc * 4:(sc + 1) * 4], lhsT=oh[ob][:, sc * 128:(sc + 1) * 128], rhs=vals[:, i, ex, :],
                                                      start=(i == 0 and sc == 0), stop=(i == NT - 1), skip_group_check=True),
                             r=["ohE%d" % ob, "valsE"], w=[PK[pi]])
                T.op("dve", lambda e: e.tensor_copy(out=pvs[:], in_=ps[pi][:, 0:16]), r=[PK[pi]], w=["pvsE"])
                pv = pvs[:, :].rearrange("p (s f) -> p s f", f=4)
                T.op("dve", lambda e: e.scalar_tensor_tensor(out=idf[:], in0=pv[:, :, 0], scalar=64.0, in1=pv[:, :, 1], op0=ALU.mult, op1=ALU.add),
                     r=["pvsE"], w=["idfE"])
                T.op("dve", lambda e: e.tensor_copy(out=idx_all[:, ex, :], in_=idf[:]), r=["idfE"], w=["idx_all"])
                T.op("dve", lambda e: e.tensor_tensor(out=g_all[:, ex, :], in0=pv[:, :, 2], in1=pv[:, :, 3], op=ALU.add), r=["pvsE"], w=["g_all"])
            T.barrier()
          if "idx" in dbg_d:
            T.dma("sp", lambda e: e.dma_start(out=dbg_d["idx"], in_=idx_all[:]), r=["idx_all"], w=["dbgidx"])
            T.dma("sp", lambda e: e.dma_start(out=dbg_d["g"], in_=g_all[:]), r=["g_all"], w=["dbgg"])

          if "F" in stages:
            wgb = [sb(es, "wgb%d" % i, [128, 8, D], BF16) for i in range(2)]
            wub = [sb(es, "wub%d" % i, [128, 8, D], BF16) for i in range(2)]
            wdb = [sb(es, "wdb%d" % i, [128, 8, D], BF16) for i in range(2)]
            xg = [sb(es, "xgF%d" % i, [128, D], BF16) for i in range(4)]
            xinT = sb(es, "xinT", [128, 8, 512], BF16)
            sg = sb(es, "sgF", [128, 512])
            hT = sb(es, "hTF", [128, 8, 512], BF16)
            eo = [sb(es, "eoF%d" % i, [128, D]) for i in range(2)]
            psb = [ps[6][:, :].bitcast(BF16), ps[7][:, :].bitcast(BF16)]

            def load_w(ex):
                for (wsrc, wdst, wk) in ((wg_d, wgb, "wgb"), (wu_d, wub, "wub"), (wd_d, wdb, "wdb")):
                    dst = wdst[ex % 2]
                    T.dmas("pool", [(lambda e, hh=hh: e.dma_start(out=dst[:, hh * 4:(hh + 1) * 4, :],
                                                                  in_=wsrc[ex].rearrange("(j p) c -> p j c", p=128)[:, hh * 4:(hh + 1) * 4, :]))
                                    for hh in range(2)], w=["%s%d" % (wk, ex % 2)])

            def gather(ex):
                for sc in range(4):
                    T.dma("pool", lambda e: e.indirect_dma_start(out=xg[sc][:], out_offset=None, in_=u2_d[:, :],
                                                                 in_offset=bass.IndirectOffsetOnAxis(ap=idx_all[:, ex, sc:sc + 1], axis=0)),
                          r=["idx_all", "u2_d"], w=["xgF%d" % sc])

            load_w(0)
            gather(0)
            for ex in range(NE):
                wgk, wuk, wdk = "wgb%d" % (ex % 2), "wub%d" % (ex % 2), "wdb%d" % (ex % 2)
                wgt, wut, wdt = wgb[ex % 2], wub[ex % 2], wdb[ex % 2]
                if ex + 1 < NE:
                    load_w(ex + 1)
                for sc in range(4):
                    for j in range(8):
                        T.op("pe", lambda e: e.transpose(out=psb[sc % 2][:, j * 128:(j + 1) * 128], in_=xg[sc][:, j * 128:(j + 1) * 128], identity=ident_b[:]),
                             r=["xgF%d" % sc, "ident_b"], w=[PK[6 + sc % 2]])
                    T.op("act", lambda e: e.copy(out=xinT[:, :, sc * 128:(sc + 1) * 128], in_=psb[sc % 2].rearrange("p (j t) -> p j t", j=8)),
                         r=[PK[6 + sc % 2]], w=["xinT"])
                if ex + 1 < NE:
                    gather(ex + 1)
                for ft in range(8):
                    pg, pu = (ft % 2) * 2, (ft % 2) * 2 + 1
                    for j in range(8):
                        T.op("pe", lambda e: e.matmul(ps[pg][:, :], lhsT=wgt[:, j, ft * 128:(ft + 1) * 128], rhs=xinT[:, j, :], start=(j == 0), stop=(j == 7)),
                             r=[wgk, "xinT"], w=[PK[pg]])
                    for j in range(8):
                        T.op("pe", lambda e: e.matmul(ps[pu][:, :], lhsT=wut[:, j, ft * 128:(ft + 1) * 128], rhs=xinT[:, j, :], start=(j == 0), stop=(j == 7)),
                             r=[wuk, "xinT"], w=[PK[pu]])
                    T.op("act", lambda e: e.activation(out=sg[:], in_=ps[pg][:, :], func=AF.Silu), r=[PK[pg]], w=["sgF"])
                    T.op("dve", lambda e: e.tensor_tensor(out=hT[:, ft, :], in0=ps[pu][:, :], in1=sg[:], op=ALU.mult), r=[PK[pu], "sgF"], w=["hTF"])
                for sc in range(4):
                    eb = eo[sc % 2]
                    for half in range(2):
                        po = 4 + half
                        for ft in range(8):
                            T.op("pe", lambda e: e.matmul(ps[po][:, :], lhsT=hT[:, ft, sc * 128:(sc + 1) * 128], rhs=wdt[:, ft, half * 512:(half + 1) * 512],
                                                          start=(ft == 0), stop=(ft == 7)), r=["hTF", wdk], w=[PK[po]])
                        T.op("dve", lambda e: e.tensor_scalar(out=eb[:, half * 512:(half + 1) * 512], in0=ps[po][:, :], scalar1=g_all[:, ex, sc:sc + 1],
                                                              scalar2=None, op0=ALU.mult), r=[PK[po], "g_all"], w=["eoF%d" % (sc % 2)])
                    T.dma("pool", lambda e: e.indirect_dma_start(out=out_d[:, :], out_offset=bass.IndirectOffsetOnAxis(ap=idx_all[:, ex, sc:sc + 1], axis=0),
                                                                 in_=eb[:], in_offset=None, compute_op=ALU.add),
                          r=["eoF%d" % (sc % 2), "idx_all"], w=["out_d"])
            T.barrier()
    for k, v in dbg_d.items():
        pass
    T.barrier()
    top.close()
    return nc


_CONST = None


def make_inputs(inp, b):
    global _CONST
    if _CONST is None:
        _CONST = host_consts()
    f = lambda a: np.ascontiguousarray(np.asarray(a, dtype=np.float32))
    m = dict(_CONST)
    m["x"] = f(inp["x"][b])
    m["g1p"] = f(inp["norm1_g"][0].reshape(8, 128).T)
    m["w_in"] = f(inp["w_in"][0])
    scw = np.concatenate([inp["short_conv_w"][0], inp["short_conv_b"][0][None, :]], axis=0)
    m["scw"] = f(scw.reshape(4, 12, 128).transpose(2, 1, 0))
    qkg = np.stack([inp["q_norm_g"][0], inp["k_norm_g"][0]], axis=0)
    m["qkg"] = f(np.broadcast_to(qkg[None], (128, 2, 64)))
    m["lamv"] = f(np.stack([inp["lambda_q1"][0], inp["lambda_k1"][0], inp["lambda_q2"][0], inp["lambda_k2"][0]])[None])
    m["subg"] = f(np.broadcast_to(inp["subln_g"][0][None, :], (128, 128)))
    m["fw1"] = f(inp["filt_w1"][0])
    m["fw2"] = f(inp["filt_w2"][0])
    m["fw3"] = f(inp["filt_w3"][0])
    m["fbf"] = f(np.stack([inp["filt_b1"][0], inp["filt_b2"][0], inp["filt_b3"][0], inp["filt_freq"][0]], axis=1))
    m["fwo"] = f(inp["filt_w_out"][0])
    m["skipbc"] = f(np.broadcast_to(inp["hyena_skip"][0][None], (32, 2, 512)))
    m["wpa"] = f(inp["w_branch_attn"][0])
    m["wph"] = f(inp["w_branch_hyena"][0])
    m["wo"] = f(inp["w_out"][0])
    m["g2bc"] = f(np.broadcast_to(inp["norm2_g"][0][None, :], (128, D)))
    m["wr"] = f(inp["w_router"][0].reshape(8, 128, 16).transpose(1, 0, 2))
    m["wg"] = f(inp["w_gate"][0])
    m["wu"] = f(inp["w_up"][0])
    m["wd"] = f(inp["w_down"][0])
    return m


def kernel(**inputs):
    nc = build()
    maps = [make_inputs(inputs, c % 4) for c in range(4)]
    in_maps = [maps[c % 4] for c in range(NCORES)]
    res = run_bass_kernel_spmd(nc, in_maps, core_ids=list(range(NCORES)))
    out = np.stack([np.asarray(res.results[b]["out"], dtype=np.float32) for b in range(4)], axis=0)
    return out
```

```python
import math
from contextlib import ExitStack
import numpy as np
import ml_dtypes
import concourse.bass as bass
import concourse.mybir as mybir
from concourse.bass_utils import run_bass_kernel_spmd

F32 = mybir.dt.float32
BF16 = mybir.dt.bfloat16
I32 = mybir.dt.int32
AF = mybir.ActivationFunctionType
ALU = mybir.AluOpType
AX = mybir.AxisListType

L = 4096
D = 1024
NT = 32
NCORES = 8
CAP = 512
NE = 16
GC = 32
DEBUG = False


class Trk:
    def __init__(s, nc):
        s.nc = nc
        s.eng = dict(pe=nc.tensor, act=nc.scalar, dve=nc.vector, pool=nc.gpsimd, sp=nc.sync)
        s.sem = {}
        s.cnt = {}
        for k in ("pe", "act", "dve", "pool"):
            s.sem[k] = nc.alloc_semaphore(name="s_" + k)
            s.cnt[k] = 0
        s.ND = 16
        for i in range(s.ND):
            k = "d%d" % i
            s.sem[k] = nc.alloc_semaphore(name="s_" + k)
            s.cnt[k] = 0
        s.pool_of = {"sp": list(range(0, 8)), "pool": list(range(8, 16))}
        s.rrq = {"sp": 0, "pool": 0}
        s.seen = {e: {} for e in s.eng}
        s.lw = {}
        s.rd = {}

    def _wait(s, e, deps):
        for k, v in deps.items():
            if e == "pe" and k == "pe":
                continue
            if s.seen[e].get(k, 0) < v:
                s.eng[e].wait_ge(s.sem[k], v)
                s.seen[e][k] = v

    def _deps(s, r, w):
        d = {}

        def add(k, v):
            if d.get(k, 0) < v:
                d[k] = v
        for x in r:
            for k, v in s.lw.get(x, {}).items():
                add(k, v)
        for x in w:
            for k, v in s.lw.get(x, {}).items():
                add(k, v)
            for k, v in s.rd.get(x, {}).items():
                add(k, v)
        return d

    def _upd(s, toks, r, w):
        if isinstance(toks, tuple):
            toks = [toks]
        for x in r:
            m = s.rd.setdefault(x, {})
            for tok in toks:
                if m.get(tok[0], 0) < tok[1]:
                    m[tok[0]] = tok[1]
        for x in w:
            m = {}
            for tok in toks:
                if m.get(tok[0], 0) < tok[1]:
                    m[tok[0]] = tok[1]
            s.lw[x] = m
            s.rd[x] = {}

    def op(s, e, fn, r=(), w=()):
        s._wait(e, s._deps(r, w))
        ins = fn(s.eng[e])
        s.cnt[e] += 1
        ins.then_inc(s.sem[e], 1)
        s._upd((e, s.cnt[e]), r, w)

    def _next_sem(s, q):
        pool = s.pool_of[q]
        k = "d%d" % pool[s.rrq[q] % len(pool)]
        s.rrq[q] += 1
        if s.cnt[k] > 0:
            s._wait(q, {k: s.cnt[k]})
        return k

    def dma(s, q, fn, r=(), w=()):
        s._wait(q, s._deps(r, w))
        k = s._next_sem(q)
        ins = fn(s.eng[q])
        s.cnt[k] += 16
        ins.then_inc(s.sem[k], 16)
        s._upd((k, s.cnt[k]), r, w)

    def dmas(s, q, fns, r=(), w=()):
        s._wait(q, s._deps(r, w))
        toks = []
        for fn in fns:
            k = s._next_sem(q)
            ins = fn(s.eng[q])
            s.cnt[k] += 16
            ins.then_inc(s.sem[k], 16)
            toks.append((k, s.cnt[k]))
        s._upd(toks, r, w)

    def barrier(s, engines=None):
        tot = {k: v for k, v in s.cnt.items() if v > 0}
        for e in (engines or s.eng):
            s._wait(e, dict(tot))
        s.lw = {}
        s.rd = {}


def _bf(a):
    return np.ascontiguousarray(a.astype(np.float32)).astype(ml_dtypes.bfloat16)


def host_consts():
    c = {}
    t = np.arange(L, dtype=np.float32)
    inv_freq = (np.float32(500000.0) ** (-np.arange(0, 16, 2, dtype=np.float32) / np.float32(16))).astype(np.float32)
    ang = (t[:, None] * inv_freq[None, :]).astype(np.float32)
    cs = np.concatenate([np.cos(ang), np.sin(ang)], axis=-1).astype(np.float32)
    c["ropecs"] = np.ascontiguousarray(cs.reshape(NT, 128, 16).transpose(1, 0, 2))
    tt = t / np.float32(L - 1)
    bands = np.linspace(1e-4, 15, 16, dtype=np.float32)
    a2 = (np.float32(2.0 * math.pi / L) * t[:, None] * bands[None, :]).astype(np.float32)
    emb = np.concatenate([tt[:, None], np.cos(a2), -np.sin(a2)], axis=-1).astype(np.float32)
    c["embT"] = np.ascontiguousarray(emb.T)
    c["posrow"] = np.ascontiguousarray(np.broadcast_to(tt[None, :], (128, L))).astype(np.float32)
    min_decay = math.log(1e-2) / 1.5
    max_decay = math.log(1e-2) / 0.3
    deltas = np.abs(np.linspace(min_decay, max_decay, 512, dtype=np.float32))
    c["ndelta"] = np.ascontiguousarray((-deltas).reshape(4, 128).T).astype(np.float32)
    n1 = np.arange(32)[:, None]
    k1 = np.arange(64)[None, :]
    th = 2 * np.pi * n1 * k1 / 64.0
    c["FA"] = _bf(np.concatenate([np.cos(th), -np.sin(th)], axis=1))
    n2 = np.arange(128)[:, None, None]
    k1 = np.arange(64)[None, :, None]
    k2 = np.arange(128)[None, None, :]
    th = 2 * np.pi * ((n2 * (k1 + 64 * k2)) % 8192) / 8192.0
    c["TGr"] = _bf(np.cos(th))
    c["TGi"] = _bf(-np.sin(th))
    c["TGn"] = _bf(np.sin(th))
    kk2 = np.arange(128)[:, None]
    nn2 = np.arange(128)[None, :]
    th = 2 * np.pi * ((kk2 * nn2) % 128) / 128.0
    fr, fi = np.cos(th), np.sin(th)
    c["R1"] = _bf(np.concatenate([fr, fi], axis=1))
    c["R2"] = _bf(np.concatenate([-fi, fr], axis=1))
    k1 = np.arange(64)[:, None, None]
    n2 = np.arange(128)[None, :, None]
    n1 = np.arange(32)[None, None, :]
    th = 2 * np.pi * ((k1 * (n2 + 128 * n1)) % 8192) / 8192.0
    th_ = np.stack([np.cos(th), -np.sin(th)], axis=2) / 8192.0
    c["TH"] = _bf(th_)
    tid = np.arange(L).reshape(NT, 128).T
    c["tidhl"] = _bf(np.stack([tid // 64, tid % 64], axis=-1))
    c["iota512"] = np.ascontiguousarray(np.broadcast_to(np.arange(512, dtype=np.float32)[None, :], (128, 512)))
    tri = (np.arange(128)[:, None] <= np.arange(128)[None, :]).astype(np.float32)
    c["tri"] = tri
    return c


def build(dbg=None):
    nc = bass.Bass("TRN2", target_bir_lowering=False)
    dbg = dbg or {}

    def din(name, shape, dt=F32):
        return nc.dram_tensor(name, list(shape), dt, kind="ExternalInput").ap()

    def dscr(name, shape, dt=F32):
        kind = "ExternalOutput" if name in dbg.get("_ext", ()) else "Internal"
        return nc.dram_tensor(name, list(shape), dt, kind=kind).ap()

    x_d = din("x", [L, D])
    g1bc_d = din("g1bc", [128, D])
    w_in_d = din("w_in", [D, 5120])
    scw_d = din("scw", [128, 12, 4])
    qkg_d = din("qkg", [128, 2, 64])
    lam_d = din("lamv", [1, 4, 64])
    subg_d = din("subg", [128, 128])
    fw1_d = din("fw1", [33, 64])
    fw2_d = din("fw2", [64, 64])
    fw3_d = din("fw3", [64, 64])
    fbf_d = din("fbf", [64, 4])
    fwo_d = din("fwo", [64, 2048])
    skip_d = din("skipbc", [32, 2, 512])
    wpa_d = din("wpa", [512, D])
    wph_d = din("wph", [512, D])
    wo_d = din("wo", [D, D])
    g2bc_d = din("g2bc", [128, D])
    wr_d = din("wr", [128, 8, 16])
    wg_d = din("wg", [NE, D, D])
    wu_d = din("wu", [NE, D, D])
    wd_d = din("wd", [NE, D, D])
    ropecs_d = din("ropecs", [128, NT, 16])
    embT_d = din("embT", [33, L])
    posrow_d = din("posrow", [128, L])
    ndelta_d = din("ndelta", [128, 4])
    FA_d = din("FA", [32, 128], BF16)
    TGr_d = din("TGr", [128, 64, 128], BF16)
    TGi_d = din("TGi", [128, 64, 128], BF16)
    TGn_d = din("TGn", [128, 64, 128], BF16)
    R1_d = din("R1", [128, 256], BF16)
    R2_d = din("R2", [128, 256], BF16)
    TH_d = din("TH", [64, 128, 2, 32], BF16)
    tidhl_d = din("tidhl", [128, NT, 2], BF16)
    iota_d = din("iota512", [128, 512])
    tri_d = din("tri", [128, 128])

    out_d = nc.dram_tensor("out", [L, D], F32, kind="ExternalOutput").ap()

    qT_d = dscr("qT_s", [4, 128, L], BF16)
    kT_d = dscr("kT_s", [4, 128, L], BF16)
    v_d = dscr("v_s", [L, 512], BF16)
    hyc_d = dscr("hyc_s", [1536, L], F32)
    gateT_d = dscr("gateT_s", [2048, L], BF16)
    kern_d = dscr("kern_s", [2, 2, 512, L], BF16)
    attnT_d = dscr("attnT_s", [512, L], BF16)
    hyoT_d = dscr("hyoT_s", [512, L], BF16)
    u2_d = dscr("u2_s", [L, D], BF16)

    dbg_d = {}
    for k, (shape, dt) in ((k, v) for k, v in dbg.items() if not k.startswith("_")):
        dbg_d[k] = nc.dram_tensor("dbg_" + k, list(shape), dt, kind="ExternalOutput").ap()

    T = Trk(nc)
    top = ExitStack()

    sbc = [0]

    def sb(es, name, shape, dt=F32):
        sbc[0] += 1
        return es.enter_context(nc.sbuf_tensor("sb%d_%s" % (sbc[0], name), list(shape), dt))

    ps = [top.enter_context(nc.psum_tensor("ps%d" % i, [128, 512], F32)) for i in range(8)]
    PK = ["ps%d" % i for i in range(8)]

    ident_f = sb(top, "ident_f", [128, 128])
    ident_b = sb(top, "ident_b", [128, 128], BF16)
    eps = sb(top, "eps", [128, 1])
    T.op("pool", lambda e: e.memset(ident_f[:], 0.0), w=["ident_f"])
    T.op("pool", lambda e: e.affine_select(out=ident_f[:], in_=ident_f[:], pattern=[[-1, 128]],
                                           compare_op=ALU.not_equal, fill=1.0, base=0, channel_multiplier=1),
         r=["ident_f"], w=["ident_f"])
    T.op("pool", lambda e: e.tensor_copy(out=ident_b[:], in_=ident_f[:]), r=["ident_f"], w=["ident_b"])
    T.op("pool", lambda e: e.memset(eps[:], 1e-6), w=["eps"])

    stages = dbg.get("_stages", "AKBCDEF") if isinstance(dbg.get("_stages", None), str) else "AKBCDEF"

    if "A" in stages:
      with ExitStack() as es:
        uT = sb(es, "uT", [128, 8, L + 2], BF16)
        g1bc = sb(es, "g1bc", [128, D])
        scw = sb(es, "scw", [128, 12, 4])
        qkg = sb(es, "qkg", [128, 2, 64])
        ropecs = sb(es, "ropecs", [128, NT, 16])
        xt = [sb(es, "xt%d" % i, [128, D]) for i in range(2)]
        xb = [sb(es, "xb%d" % i, [128, D], BF16) for i in range(2)]
        junk = sb(es, "junkA", [128, D])
        ss = sb(es, "ssA", [128, NT])
        rs = sb(es, "rsA", [128, NT])
        wb = [sb(es, "wb%d" % i, [128, 8, 512], BF16) for i in range(2)]
        T.dma("sp", lambda e: e.dma_start(out=g1bc[:], in_=g1bc_d[:, :]), w=["g1bc"])
        T.dma("sp", lambda e: e.dma_start(out=scw[:], in_=scw_d[:, :, :]), w=["scw"])
        T.dma("sp", lambda e: e.dma_start(out=qkg[:], in_=qkg_d[:, :, :]), w=["qkg"])
        T.dma("sp", lambda e: e.dma_start(out=ropecs[:], in_=ropecs_d[:, :, :]), w=["ropecs"])
        T.op("dve", lambda e: e.memset(ss[:], 0.0), w=["ssA"])
        T.op("dve", lambda e: e.memset(uT[:, :, 0:1], 0.0), w=["uTpad0"])
        T.op("dve", lambda e: e.memset(uT[:, :, L + 1:L + 2], 0.0), w=["uTpad1"])
        T.op("dve", lambda e: e.tensor_scalar(out=qkg[:, 0, :], in0=qkg[:, 0, :], scalar1=0.125, scalar2=None,
                                              op0=ALU.mult), r=["qkg"], w=["qkg"])
        psb = [ps[6][:, :].bitcast(BF16), ps[7][:, :].bitcast(BF16)]
        for i in range(NT):
            b = i % 2
            T.dma("sp", lambda e: e.dma_start(out=xt[b][:], in_=x_d[i * 128:(i + 1) * 128, :]), w=["xt%d" % b])
            T.op("act", lambda e: e.activation(out=junk[:], in_=xt[b][:], func=AF.Square,
                                               accum_out=ss[:, i:i + 1]), r=["xt%d" % b, "ssA"], w=["junkA", "ss%d" % i])
            T.op("act", lambda e: e.activation(out=rs[:, i:i + 1], in_=ss[:, i:i + 1], func=AF.Sqrt,
                                               scale=1.0 / D, bias=eps[:]), r=["ss%d" % i, "eps"], w=["rs%d" % i])
            T.op("dve", lambda e: e.reciprocal(out=rs[:, i:i + 1], in_=rs[:, i:i + 1]), r=["rs%d" % i], w=["rs%d" % i])
            T.op("dve", lambda e: e.scalar_tensor_tensor(out=xb[b][:], in0=xt[b][:], scalar=rs[:, i:i + 1], in1=g1bc[:],
                                                         op0=ALU.mult, op1=ALU.mult),
                 r=["xt%d" % b, "rs%d" % i, "g1bc"], w=["xb%d" % b])
            for j in range(8):
                T.op("pe", lambda e: e.transpose(out=psb[b][:, j * 128:(j + 1) * 128],
                                                 in_=xb[b][:, j * 128:(j + 1) * 128], identity=ident_b[:]),
                     r=["xb%d" % b, "ident_b"], w=[PK[6 + b]])
            T.op("act", lambda e: e.copy(out=uT[:, :, 1 + i * 128:1 + (i + 1) * 128],
                                         in_=psb[b].rearrange("p (j t) -> p j t", j=8)),
                 r=[PK[6 + b]], w=["uT%d" % i])
        UTK = ["uT%d" % i for i in range(NT)] + ["uTpad0", "uTpad1"]

        sq = sb(es, "sqA", [128, 512])
        ssq = sb(es, "ssqA", [128, 8])
        qn = sb(es, "qnA", [128, 8, 64])
        qb2 = [sb(es, "qbA%d" % i, [128, 512], BF16) for i in range(2)]
        rt = [sb(es, "rtA%d" % i, [128, 8, 8]) for i in range(4)]
        qTs = [sb(es, "qTs%d" % i, [128, 4, 128], BF16) for i in range(2)]
        vst = [sb(es, "vst%d" % i, [128, 512], BF16) for i in range(2)]
        hraw = sb(es, "hraw", [128, L + 2])
        hc = sb(es, "hcA", [128, L])
        gst = sb(es, "gstA", [128, L], BF16)
        T.op("pool", lambda e: e.memset(hraw[:, 0:1], 0.0), w=["hrawp0"])
        T.op("pool", lambda e: e.memset(hraw[:, L + 1:L + 2], 0.0), w=["hrawp1"])
        pcnt = [0]

        def nextps():
            pcnt[0] += 1
            return pcnt[0] % 4

        for cc in dbg.get('_ccs', range(10)):
            wbk = "wb%d" % (cc % 2)
            wbt = wb[cc % 2]
            T.dmas("pool", [(lambda e, a=a: e.dma_start(
                out=wbt[:, a:a + 4, :], in_=w_in_d.rearrange("(j p) c -> p j c", p=128)[:, a:a + 4, cc * 512:(cc + 1) * 512]))
                for a in (0, 4)], w=[wbk])
            if cc < 3:
                def qk_tail(i):
                    qbt = qb2[i % 2]
                    pb = 6 + (i % 2)
                    for h in range(4):
                        T.op("pe", lambda e: e.transpose(out=psb[i % 2][:, h * 128:(h + 1) * 128],
                                                         in_=qbt[:, h * 128:(h + 1) * 128], identity=ident_b[:]),
                             r=["qbA%d" % (i % 2), "ident_b"], w=[PK[pb]])
                    st = qTs[i % 2]
                    T.op("act", lambda e: e.copy(out=st[:], in_=psb[i % 2][:, 0:512].rearrange("p (h t) -> p h t", h=4)),
                         r=[PK[pb]], w=["qTs%d" % (i % 2)])
                    dst = (qT_d if cc == 0 else kT_d).rearrange("h p t -> p h t")[:, :, i * 128:(i + 1) * 128]
                    T.dma("sp", lambda e: e.dma_start(out=dst, in_=st[:]), r=["qTs%d" % (i % 2)], w=["qkT_d"])

                for i in range(NT):
                    pi = nextps()
                    for j in range(8):
                        T.op("pe", lambda e: e.matmul(ps[pi][:, :], lhsT=uT[:, j, 1 + i * 128:1 + (i + 1) * 128],
                                                      rhs=wbt[:, j, :], start=(j == 0), stop=(j == 7)),
                             r=["uT%d" % i, wbk], w=[PK[pi]])
                    if cc == 2:
                        vs = vst[i % 2]
                        T.op("act", lambda e: e.copy(out=vs[:], in_=ps[pi][:, :]), r=[PK[pi]], w=["vst%d" % (i % 2)])
                        T.dma("sp", lambda e: e.dma_start(out=v_d[i * 128:(i + 1) * 128, :], in_=vs[:]),
                              r=["vst%d" % (i % 2)], w=["v_d"])
                        continue
                    if i > 0:
                        qk_tail(i - 1)
                    qb = qb2[i % 2]
                    qbk = "qbA%d" % (i % 2)
                    T.op("act", lambda e: e.activation(out=sq[:], in_=ps[pi][:, :], func=AF.Square), r=[PK[pi]], w=["sqA"])
                    T.op("dve", lambda e: e.tensor_reduce(out=ssq[:], in_=sq[:].rearrange("p (g d) -> p g d", d=64),
                                                          axis=AX.X, op=ALU.add), r=["sqA"], w=["ssqA"])
                    T.op("act", lambda e: e.activation(out=ssq[:], in_=ssq[:], func=AF.Sqrt, scale=1.0 / 64,
                                                       bias=eps[:]), r=["ssqA", "eps"], w=["ssqA"])
                    T.op("dve", lambda e: e.reciprocal(out=ssq[:], in_=ssq[:]), r=["ssqA"], w=["ssqA"])
                    T.op("dve", lambda e: e.tensor_tensor(out=qn[:], in0=ps[pi][:, :].rearrange("p (g d) -> p g d", d=64),
                                                          in1=ssq[:, :].unsqueeze(2).broadcast_to([128, 8, 64]),
                                                          op=ALU.mult), r=[PK[pi], "ssqA"], w=["qnA"])
                    T.op("dve", lambda e: e.tensor_tensor(out=qn[:], in0=qn[:],
                                                          in1=qkg[:, cc, :].unsqueeze(1).broadcast_to([128, 8, 64]),
                                                          op=ALU.mult), r=["qnA", "qkg"], w=["qnA"])
                    T.op("act", lambda e: e.copy(out=qb[:].rearrange("p (g d) -> p g d", d=64), in_=qn[:]),
                         r=["qnA"], w=[qbk])
                    cosb = ropecs[:, i, 0:8].unsqueeze(1).broadcast_to([128, 8, 8])
                    sinb = ropecs[:, i, 8:16].unsqueeze(1).broadcast_to([128, 8, 8])
                    r1 = qn[:, :, 0:8]
                    r2 = qn[:, :, 8:16]
                    T.op("dve", lambda e: e.tensor_tensor(out=rt[0][:], in0=r1, in1=cosb, op=ALU.mult), r=["qnA", "ropecs"], w=["rt0"])
                    T.op("dve", lambda e: e.tensor_tensor(out=rt[1][:], in0=r2, in1=sinb, op=ALU.mult), r=["qnA", "ropecs"], w=["rt1"])
                    T.op("dve", lambda e: e.tensor_tensor(out=rt[2][:], in0=r2, in1=cosb, op=ALU.mult), r=["qnA", "ropecs"], w=["rt2"])
                    T.op("dve", lambda e: e.tensor_tensor(out=rt[3][:], in0=r1, in1=sinb, op=ALU.mult), r=["qnA", "ropecs"], w=["rt3"])
                    qbv = qb[:].rearrange("p (g d) -> p g d", d=64)
                    T.op("dve", lambda e: e.tensor_tensor(out=qbv[:, :, 0:8], in0=rt[0][:], in1=rt[1][:], op=ALU.subtract),
                         r=["rt0", "rt1"], w=[qbk])
                    T.op("dve", lambda e: e.tensor_tensor(out=qbv[:, :, 8:16], in0=rt[2][:], in1=rt[3][:], op=ALU.add),
                         r=["rt2", "rt3"], w=[qbk])
                if cc < 2:
                    qk_tail(NT - 1)
            else:
                for ct in range(4):
                    for tq in range(8):
                        pi = nextps()
                        for j in range(8):
                            T.op("pe", lambda e: e.matmul(ps[pi][:, :], lhsT=wbt[:, j, ct * 128:(ct + 1) * 128],
                                                          rhs=uT[:, j, 1 + tq * 512:1 + (tq + 1) * 512],
                                                          start=(j == 0), stop=(j == 7)),
                                 r=UTK[4 * tq:4 * tq + 4] + [wbk], w=[PK[pi]])
                        if cc < 6:
                            T.op("act", lambda e: e.copy(out=hraw[:, 1 + tq * 512:1 + (tq + 1) * 512], in_=ps[pi][:, :]),
                                 r=[PK[pi]], w=["hraw"])
                        else:
                            T.op("act", lambda e: e.activation(out=gst[:, tq * 512:(tq + 1) * 512], in_=ps[pi][:, :],
                                                               func=AF.Sigmoid), r=[PK[pi]], w=["gstA"])
                    if cc < 6:
                        cti = (cc - 3) * 4 + ct
                        lvl = dbg.get('_lvl', 9)
                        if lvl >= 1:
                          T.op("act", lambda e: e.activation(out=hc[:], in_=hraw[:, 1:L + 1], func=AF.Identity,
                                                           scale=scw[:, cti, 1:2], bias=scw[:, cti, 3:4]),
                             r=["hraw", "scw"], w=["hcA"])
                        if lvl >= 2:
                          T.op("dve", lambda e: e.scalar_tensor_tensor(out=hc[:], in0=hraw[:, 0:L], scalar=scw[:, cti, 0:1],
                                                                     in1=hc[:], op0=ALU.mult, op1=ALU.add),
                             r=["hraw", "hrawp0", "scw", "hcA"], w=["hcA"])
                          T.op("dve", lambda e: e.scalar_tensor_tensor(out=hc[:], in0=hraw[:, 2:L + 2], scalar=scw[:, cti, 2:3],
                                                                     in1=hc[:], op0=ALU.mult, op1=ALU.add),
                             r=["hraw", "hrawp1", "scw", "hcA"], w=["hcA"])
                        if lvl >= 3:
                          for q4 in range(4):
                              T.dma("sp", lambda e: e.dma_start(out=hyc_d[cti * 128:(cti + 1) * 128, q4 * 1024:(q4 + 1) * 1024],
                                                                in_=hc[:, q4 * 1024:(q4 + 1) * 1024]),
                                    r=["hcA"], w=["hyc_d%d" % q4])
                    else:
                        gti = (cc - 6) * 4 + ct
                        T.dmas("sp", [(lambda e, a=a: e.dma_start(out=gateT_d[gti * 128:(gti + 1) * 128, a:a + 2048], in_=gst[:, a:a + 2048]))
                                      for a in (0, 2048)], r=["gstA"], w=["gateT_d"])
        T.barrier()

    if "K" in stages:
      with ExitStack() as es:
        embT = sb(es, "embT", [33, L])
        fw1 = sb(es, "fw1", [33, 64])
        fw2 = sb(es, "fw2", [64, 64])
        fw3 = sb(es, "fw3", [64, 64])
        fbf = sb(es, "fbf", [64, 4])
        frb = sb(es, "frb", [64, 3])
        fwo = sb(es, "fwo", [64, 2048])
        posrow = sb(es, "posrow", [128, L])
        ndelta = sb(es, "ndelta", [128, 4])
        hid = [sb(es, "hid%d" % i, [64, L]) for i in range(2)]
        hidb = sb(es, "hidb", [64, L])
        dec = sb(es, "decK", [128, L])
        kf = sb(es, "kfK", [128, L])
        kb = sb(es, "kbK", [128, L])
        hp = sb(es, "hpK", [128, L], BF16)
        hm = sb(es, "hmK", [128, L], BF16)
        for tns, src, k in ((fw1, fw1_d, "fw1"), (fw2, fw2_d, "fw2"), (fw3, fw3_d, "fw3"),
                            (fbf, fbf_d, "fbf"), (ndelta, ndelta_d, "ndelta")):
            T.dma("sp", lambda e: e.dma_start(out=tns[:], in_=src), w=[k])
        for tns, src, k, n in ((embT, embT_d, "embT", L), (fwo, fwo_d, "fwo", 2048), (posrow, posrow_d, "posrow", L)):
            T.dmas("sp", [(lambda e, a=a: e.dma_start(out=tns[:, a:a + 1024], in_=src[:, a:a + 1024]))
                          for a in range(0, n, 1024)], w=[k])
        for l in range(3):
            T.op("dve", lambda e: e.tensor_tensor(out=frb[:, l:l + 1], in0=fbf[:, l:l + 1], in1=fbf[:, 3:4], op=ALU.mult),
                 r=["fbf"], w=["frb"])
        TWO_PI = 2.0 * math.pi
        srcs = [(embT, "embT", 33, fw1, "fw1"), (hid[0], "hid0", 64, fw2, "fw2"), (hid[1], "hid1", 64, fw3, "fw3")]
        outs = [(hid[0], "hid0"), (hid[1], "hid1"), (hid[0], "hid0")]
        negpi = sb(es, "negpi", [64, 1])
        T.op("dve", lambda e: e.memset(negpi[:], 4.0 * math.pi), w=["negpi"])
        kcnt = sb(es, "kcntK", [64, L])
        for l in range(3):
            src, sk, kk, w_, wk = srcs[l]
            dst, dk = outs[l]
            for tq in range(8):
                pi = tq % 4
                T.op("pe", lambda e: e.matmul(ps[pi][0:64, :], lhsT=w_[0:kk, :], rhs=src[0:kk, tq * 512:(tq + 1) * 512],
                                              start=True, stop=True), r=[sk, wk], w=[PK[pi]])
                T.op("dve", lambda e: e.tensor_scalar(out=hidb[:, tq * 512:(tq + 1) * 512], in0=ps[pi][0:64, :],
                                                      scalar1=fbf[:, 3:4], scalar2=frb[:, l:l + 1], op0=ALU.mult,
                                                      op1=ALU.add), r=[PK[pi], "fbf", "frb"], w=["hidb"])
                xs_ = hidb[:, tq * 512:(tq + 1) * 512]
                ks_ = kcnt[:, tq * 512:(tq + 1) * 512]
                T.op("dve", lambda e: e.tensor_scalar(out=ks_, in0=xs_, scalar1=-3.0 * math.pi, scalar2=None, op0=ALU.is_gt),
                     r=["hidb"], w=["kcnt"])
                for thr in (-math.pi, math.pi, 3.0 * math.pi):
                    T.op("dve", lambda e: e.scalar_tensor_tensor(out=ks_, in0=xs_, scalar=thr, in1=ks_, op0=ALU.is_gt, op1=ALU.add),
                         r=["hidb", "kcnt"], w=["kcnt"])
                T.op("dve", lambda e: e.scalar_tensor_tensor(out=xs_, in0=ks_, scalar=-TWO_PI, in1=xs_, op0=ALU.mult, op1=ALU.add),
                     r=["hidb", "kcnt"], w=["hidb"])
                T.op("act", lambda e: e.activation(out=dst[:, tq * 512:(tq + 1) * 512], in_=xs_,
                                                   func=AF.Sin, bias=negpi[:]), r=["hidb", "negpi"], w=[dk])
        h3 = hid[0]
        for ct in range(4):
            T.op("act", lambda e: e.activation(out=dec[:], in_=posrow[:], func=AF.Exp, scale=ndelta[:, ct:ct + 1]),
                 r=["posrow", "ndelta"], w=["decK"])
            for o in range(2):
                for d_, (dst, dk) in enumerate(((kf, "kfK"), (kb, "kbK"))):
                    col = (o * 2 + d_) * 512 + ct * 128
                    for tq in range(8):
                        pi = tq % 4
                        T.op("pe", lambda e: e.matmul(ps[pi][:, :], lhsT=fwo[:, col:col + 128],
                                                      rhs=h3[:, tq * 512:(tq + 1) * 512], start=True, stop=True),
                             r=["hid0", "fwo"], w=[PK[pi]])
                        T.op("dve", lambda e: e.tensor_tensor(out=dst[:, tq * 512:(tq + 1) * 512], in0=ps[pi][:, :],
                                                              in1=dec[:, tq * 512:(tq + 1) * 512], op=ALU.mult),
                             r=[PK[pi], "decK"], w=[dk])
                T.op("dve", lambda e: e.memset(kb[:, 0:1], 0.0), r=["kbK"], w=["kbK"])
                T.op("pool", lambda e: e.tensor_tensor(out=hp[:], in0=kf[:], in1=kb[:], op=ALU.add), r=["kfK", "kbK"], w=["hpK"])
                T.op("pool", lambda e: e.tensor_tensor(out=hm[:], in0=kf[:], in1=kb[:], op=ALU.subtract), r=["kfK", "kbK"], w=["hmK"])
                T.dmas("sp", [(lambda e, a=a: e.dma_start(out=kern_d[o, 0, ct * 128:(ct + 1) * 128, a:a + 2048], in_=hp[:, a:a + 2048]))
                              for a in range(0, L, 2048)], r=["hpK"], w=["kern_d0"])
                T.dmas("sp", [(lambda e, a=a: e.dma_start(out=kern_d[o, 1, ct * 128:(ct + 1) * 128, a:a + 2048], in_=hm[:, a:a + 2048]))
                              for a in range(0, L, 2048)], r=["hmK"], w=["kern_d1"])
        T.barrier()

    if "B" in stages:
      with ExitStack() as es:
        qT = sb(es, "qT", [128, 4, L], BF16)
        kT = sb(es, "kT", [128, 2, 4, L], BF16)
        V = sb(es, "Vsb", [128, NT, 4, 132], BF16)
        lamv = sb(es, "lamv", [1, 4, 64])
        lamt = sb(es, "lamt", [1, 8])
        ones1 = sb(es, "ones1", [1, 128])
        nlam = sb(es, "nlam", [128, 1])
        subg = sb(es, "subg", [128, 128])
        E = [sb(es, "E%d" % i, [128, 512], BF16) for i in range(3)]
        osb = sb(es, "osbB", [128, 2, 4, 129])
        aacc = sb(es, "aacc", [128, 4, 128])
        ab = sb(es, "abB", [128, 4, 128], BF16)
        junkb = sb(es, "junkB", [128, 4, 128])
        sm = sb(es, "smB", [128, 8, 4])
        aTs = [sb(es, "aTs%d" % i, [128, 512], BF16) for i in range(2)]
        T.dmas("sp", [(lambda e, h=h, a=a: e.dma_start(out=qT[:, h, a:a + 2048], in_=qT_d[h, :, a:a + 2048]))
                      for h in range(4) for a in (0, 2048)], w=["qT"])
        T.op("pool", lambda e: e.memset(kT[64:128, 0, :, :], 0.0), w=["kTz0"])
        T.op("dve", lambda e: e.memset(kT[0:64, 1, :, :], 0.0), w=["kTz1"])
        T.dmas("sp", [(lambda e, h=h, a=a, m=m: e.dma_start(out=kT[m * 64:(m + 1) * 64, m, h, a:a + 2048],
                                                            in_=kT_d[h, m * 64:(m + 1) * 64, a:a + 2048]))
                      for m in range(2) for h in range(4) for a in (0, 2048)], w=["kT"])
        T.dmas("sp", [(lambda e, i=i: e.dma_start(out=V[:, i, :, 0:128],
                                                  in_=v_d[i * 128:(i + 1) * 128, :].rearrange("p (h d) -> p h d", d=128)))
                      for i in range(NT)], w=["V"])
        T.op("pool", lambda e: e.memset(V[:, :, :, 128:129], 1.0), w=["Vones"])
        T.dma("sp", lambda e: e.dma_start(out=lamv[:], in_=lam_d[:, :, :]), w=["lamv"])
        T.dma("sp", lambda e: e.dma_start(out=subg[:], in_=subg_d[:, :]), w=["subg"])
        T.op("dve", lambda e: e.tensor_scalar(out=subg[:], in0=subg[:], scalar1=0.8, scalar2=None, op0=ALU.mult),
             r=["subg"], w=["subg"])
        T.op("dve", lambda e: e.memset(ones1[:], 1.0), w=["ones1"])
        T.op("dve", lambda e: e.memset(lamt[:], 0.0), w=["lamt"])
        T.op("dve", lambda e: e.tensor_tensor(out=lamv[:, 0, :], in0=lamv[:, 0, :], in1=lamv[:, 1, :], op=ALU.mult), r=["lamv"], w=["lamv"])
        T.op("dve", lambda e: e.tensor_tensor(out=lamv[:, 2, :], in0=lamv[:, 2, :], in1=lamv[:, 3, :], op=ALU.mult), r=["lamv"], w=["lamv"])
        T.op("dve", lambda e: e.tensor_reduce(out=lamt[:, 0:1], in_=lamv[:, 0, :], axis=AX.X, op=ALU.add), r=["lamv", "lamt"], w=["lamt"])
        T.op("dve", lambda e: e.tensor_reduce(out=lamt[:, 1:2], in_=lamv[:, 2, :], axis=AX.X, op=ALU.add), r=["lamv", "lamt"], w=["lamt"])
        T.op("act", lambda e: e.activation(out=lamt[:, 2:4], in_=lamt[:, 0:2], func=AF.Exp), r=["lamt"], w=["lamt"])
        T.op("dve", lambda e: e.tensor_tensor(out=lamt[:, 4:5], in0=lamt[:, 3:4], in1=lamt[:, 2:3], op=ALU.subtract), r=["lamt"], w=["lamt"])
        T.op("dve", lambda e: e.tensor_scalar(out=lamt[:, 5:6], in0=lamt[:, 4:5], scalar1=-0.2, scalar2=None, op0=ALU.add), r=["lamt"], w=["lamt"])
        T.op("pe", lambda e: e.matmul(ps[7][:, 0:1], lhsT=ones1[:, :], rhs=lamt[:, 5:6], start=True, stop=True),
             r=["ones1", "lamt"], w=[PK[7]])
        T.op("dve", lambda e: e.tensor_copy(out=nlam[:], in_=ps[7][:, 0:1]), r=[PK[7]], w=["nlam"])
        psT = ps[6][:, :].bitcast(BF16)
        steps = [(h, qc, m, kt) for h in range(4) for qc in range(8) for m in range(2) for kt in range(NT)]
        USE_POW = False

        def issue_S(idx):
            h, qc, m, kt = steps[idx]
            sp_ = idx % 2
            eb = idx % 3
            T.op("pe", lambda e: e.matmul(ps[sp_][:, :], lhsT=kT[:, m, h, kt * 128:(kt + 1) * 128],
                                          rhs=qT[:, h, qc * 512:(qc + 1) * 512], start=True, stop=True),
                 r=["kT", "kTz0", "kTz1", "qT"], w=[PK[sp_]])
            T.op("act", lambda e: e.activation(out=E[eb][:], in_=ps[sp_][:, :], func=AF.Exp),
                 r=[PK[sp_]], w=["E%d" % eb])

        def issue_PV(idx):
            h, qc, m, kt = steps[idx]
            eb = idx % 3
            for qs in range(4):
                bank = 2 + m * 2 + qs // 2
                T.op("pe", lambda e: e.matmul(ps[bank][:, (qs % 2) * 256:(qs % 2) * 256 + 129],
                                              lhsT=E[eb][:, qs * 128:(qs + 1) * 128], rhs=V[:, kt, h, 0:129],
                                              start=(kt == 0 and qs % 2 == 0), stop=(kt == NT - 1),
                                              skip_group_check=True),
                     r=["E%d" % eb, "V", "Vones"], w=[PK[bank]])

        def bc4(ap2):
            return ap2.unsqueeze(2).broadcast_to([128, 4, 128])

        def epi_head():
            for m in range(2):
                for half in range(2):
                    bank = 2 + m * 2 + half
                    src = ps[bank][:, :].rearrange("p (a c) -> p a c", a=2)[:, :, 0:129]
                    dst = osb[:, m, half * 2:(half + 1) * 2, :]
                    if half == 0:
                        T.op("dve", lambda e: e.tensor_copy(out=dst, in_=src), r=[PK[bank]], w=["osb%d%d" % (m, half)])
                    else:
                        T.op("pool" if False else "dve", lambda e: e.tensor_copy(out=dst, in_=src), r=[PK[bank]], w=["osb%d%d" % (m, half)])
            OS = ["osb00", "osb01", "osb10", "osb11"]
            T.op("dve", lambda e: e.reciprocal(out=sm[:, 0:2, :], in_=osb[:, :, :, 128]), r=OS, w=["smB"])
            T.op("dve", lambda e: e.tensor_scalar(out=sm[:, 2, :], in0=sm[:, 1, :], scalar1=nlam[:, 0:1], scalar2=None, op0=ALU.mult),
                 r=["smB", "nlam"], w=["smB"])
            T.op("dve", lambda e: e.tensor_tensor(out=aacc[:], in0=osb[:, 0, :, 0:128], in1=bc4(sm[:, 0, :]), op=ALU.mult),
                 r=OS + ["smB"], w=["aacc"])
            T.op("dve", lambda e: e.tensor_tensor(out=junkb[:], in0=osb[:, 1, :, 0:128], in1=bc4(sm[:, 2, :]), op=ALU.mult),
                 r=OS + ["smB"], w=["junkB"])
            T.op("dve", lambda e: e.tensor_tensor(out=aacc[:], in0=aacc[:], in1=junkb[:], op=ALU.add), r=["aacc", "junkB"], w=["aacc"])
            T.op("dve", lambda e: e.tensor_tensor(out=junkb[:], in0=aacc[:], in1=aacc[:], op=ALU.mult), r=["aacc"], w=["junkB"])
            T.op("dve", lambda e: e.tensor_reduce(out=sm[:, 3, :], in_=junkb[:], axis=AX.X, op=ALU.add), r=["junkB", "smB"], w=["smB"])
            T.op("dve", lambda e: e.tensor_scalar(out=sm[:, 4, :], in0=sm[:, 3, :], scalar1=1.0 / 128, scalar2=1e-6, op0=ALU.mult, op1=ALU.add),
                 r=["smB"], w=["smB"])
            if USE_POW:
                T.op("dve", lambda e: e.tensor_scalar(out=sm[:, 5, :], in0=sm[:, 4, :], scalar1=-0.5, scalar2=None, op0=ALU.pow),
                     r=["smB"], w=["smB"])
            else:
                T.op("act", lambda e: e.activation(out=sm[:, 6, :], in_=sm[:, 4, :], func=AF.Sqrt), r=["smB"], w=["smB"])
                T.op("dve", lambda e: e.reciprocal(out=sm[:, 5, :], in_=sm[:, 6, :]), r=["smB"], w=["smB"])
            T.op("dve", lambda e: e.tensor_tensor(out=aacc[:], in0=aacc[:], in1=bc4(sm[:, 5, :]), op=ALU.mult), r=["aacc", "smB"], w=["aacc"])
            T.op("dve", lambda e: e.tensor_tensor(out=ab[:], in0=aacc[:], in1=subg[:, :].unsqueeze(1).broadcast_to([128, 4, 128]), op=ALU.mult),
                 r=["aacc", "subg"], w=["abB"])

        def epi_tail(h, qc):
            for qs in range(4):
                T.op("pe", lambda e: e.transpose(out=psT[:, qs * 128:(qs + 1) * 128], in_=ab[:, qs, :], identity=ident_b[:]),
                     r=["abB", "ident_b"], w=[PK[6]])
            st = aTs[qc % 2]
            T.op("act", lambda e: e.copy(out=st[:], in_=psT[:, 0:512]), r=[PK[6]], w=["aTs%d" % (qc % 2)])
            T.dma("sp", lambda e: e.dma_start(out=attnT_d[h * 128:(h + 1) * 128, qc * 512:(qc + 1) * 512], in_=st[:]),
                  r=["aTs%d" % (qc % 2)], w=["attnT_d"])

        pending = None
        issue_S(0)
        for idx in range(len(steps)):
            h, qc, m, kt = steps[idx]
            if idx + 1 < len(steps):
                issue_S(idx + 1)
            issue_PV(idx)
            if pending is not None and m == 0 and kt == 12:
                epi_tail(*pending)
                pending = None
            if m == 1 and kt == NT - 1:
                epi_head()
                pending = (h, qc)
        epi_tail(*pending)
        T.barrier()

    if "C" in stages:
      with ExitStack() as es:
        FA = sb(es, "FA", [32, 128], BF16)
        TGr = sb(es, "TGr", [128, 64, 128], BF16)
        TGi = sb(es, "TGi", [128, 64, 128], BF16)
        TGn = sb(es, "TGn", [128, 64, 128], BF16)
        R1 = sb(es, "R1", [128, 256], BF16)
        R2 = sb(es, "R2", [128, 256], BF16)
        TH = sb(es, "TH", [64, 128, 2, 32], BF16)
        skipbc = sb(es, "skipbc", [32, 2, GC])
        for tns, src, k in ((FA, FA_d, "FA"), (R1, R1_d, "R1"), (R2, R2_d, "R2")):
            T.dma("sp", lambda e: e.dma_start(out=tns[:], in_=src), w=[k])
        for tns, src, k in ((TGr, TGr_d, "TGr"), (TGi, TGi_d, "TGi"), (TGn, TGn_d, "TGn")):
            T.dmas("sp", [(lambda e, a=a: e.dma_start(out=tns[:, a:a + 16, :], in_=src[:, a:a + 16, :])) for a in range(0, 64, 16)], w=[k])
        T.dmas("sp", [(lambda e, a=a: e.dma_start(out=TH[:, a:a + 32, :, :], in_=TH_d[:, a:a + 32, :, :])) for a in range(0, 128, 32)], w=["TH"])
        zf = sb(es, "zf", [32, GC, 128])
        gt = sb(es, "gtC", [32, GC, 128])
        zs = sb(es, "zsC", [32, GC, 128])
        srcb = [sb(es, "srcb%d" % i, [32, GC, 128], BF16) for i in range(3)]
        Bm_ = [sb(es, "Bm%d" % i, [128, 128, GC], BF16) for i in range(3)]
        srs = sb(es, "srsC", [128, 512])
        sis = sb(es, "sisC", [128, 512])
        tm = [sb(es, "tmC%d" % i, [128, 512]) for i in range(4)]
        Y = sb(es, "YC", [128, GC, 2, 64], BF16)
        Dm = sb(es, "DmC", [64, 256, GC], BF16)
        tmpy = sb(es, "tmpyC", [32, GC, 16])
        hob = srcb[0]

        def flay(ap2d):
            return ap2d.rearrange("c (a b) -> a c b", b=128)

        def load_filters(g_, o_):
            for pm in range(2):
                T.dma("sp", lambda e: e.dma_start(out=srcb[1 + pm][:], in_=flay(kern_d[o_, pm, g_ * GC:(g_ + 1) * GC, :])),
                      w=["srcb%d" % (1 + pm)])

        for g in range(512 // GC):
            c0 = g * GC
            T.dma("sp", lambda e: e.dma_start(out=zf[:], in_=flay(hyc_d[c0:c0 + GC, :])), w=["zf"])
            T.dma("sp", lambda e: e.dma_start(out=skipbc[:], in_=skip_d[:, :, c0:c0 + GC]), w=["skipbc"])
            for o in range(2):
                T.dma("sp", lambda e: e.dma_start(out=gt[:], in_=flay(hyc_d[512 * (o + 1) + c0:512 * (o + 1) + c0 + GC, :])),
                      w=["gtC"])
                T.op("act", lambda e: e.copy(out=srcb[0][:], in_=zf[:]), r=["zf"], w=["srcb0"])
                if g == 0 and o == 0:
                    load_filters(0, 0)
                T.op("pool", lambda e: e.tensor_tensor(out=zs[:], in0=zf[:],
                                                       in1=skipbc[:, o, :].unsqueeze(2).broadcast_to([32, GC, 128]),
                                                       op=ALU.mult), r=["zf", "skipbc"], w=["zsC"])
                for s_ in (1, 2, 0):
                    for cq in range(GC // 4):
                        pi = cq % 2
                        for c4 in range(4):
                            c = cq * 4 + c4
                            T.op("pe", lambda e: e.matmul(ps[pi][:, c4 * 128:(c4 + 1) * 128], lhsT=srcb[s_][:, c, :],
                                                          rhs=FA[:, :], start=True, stop=True),
                                 r=["srcb%d" % s_, "FA"], w=[PK[pi]])
                        T.op("act" if cq % 2 == 0 else "dve",
                             lambda e: (e.copy if cq % 2 == 0 else e.tensor_copy)(
                                 out=Bm_[s_][:, :, cq * 4:(cq + 1) * 4],
                                 in_=ps[pi][:, :].rearrange("p (c f) -> p f c", c=4)),
                             r=[PK[pi]], w=["Bm%d" % s_])
                nxt = (g, 1) if o == 0 else (g + 1, 0)
                if nxt[0] < 512 // GC:
                    load_filters(*nxt)
                for kc in range(4):
                    bZr, bZi, bSr, bSi = (2, 3, 4, 5) if kc % 2 == 0 else (0, 1, 6, 7)
                    for j in range(16):
                        k1 = kc * 16 + j
                        cs_ = slice(j * GC, (j + 1) * GC)
                        bz, bp, bm = Bm_[0], Bm_[1], Bm_[2]
                        T.op("pe", lambda e: e.matmul(ps[bZr][:, cs_], lhsT=TGr[:, k1, :], rhs=bz[:, k1, :], start=True, stop=False), r=["TGr", "Bm0"], w=[PK[bZr]])
                        T.op("pe", lambda e: e.matmul(ps[bZr][:, cs_], lhsT=TGn[:, k1, :], rhs=bz[:, 64 + k1, :], start=False, stop=True), r=["TGn", "Bm0"], w=[PK[bZr]])
                        T.op("pe", lambda e: e.matmul(ps[bZi][:, cs_], lhsT=TGi[:, k1, :], rhs=bz[:, k1, :], start=True, stop=False), r=["TGi", "Bm0"], w=[PK[bZi]])
                        T.op("pe", lambda e: e.matmul(ps[bZi][:, cs_], lhsT=TGr[:, k1, :], rhs=bz[:, 64 + k1, :], start=False, stop=True), r=["TGr", "Bm0"], w=[PK[bZi]])
                        T.op("pe", lambda e: e.matmul(ps[bSr][:, cs_], lhsT=TGr[:, k1, :], rhs=bp[:, k1, :], start=True, stop=False), r=["TGr", "Bm1"], w=[PK[bSr]])
                        T.op("pe", lambda e: e.matmul(ps[bSr][:, cs_], lhsT=TGn[:, k1, :], rhs=bp[:, 64 + k1, :], start=False, stop=True), r=["TGn", "Bm1"], w=[PK[bSr]])
                        T.op("pe", lambda e: e.matmul(ps[bSi][:, cs_], lhsT=TGi[:, k1, :], rhs=bm[:, k1, :], start=True, stop=False), r=["TGi", "Bm2"], w=[PK[bSi]])
                        T.op("pe", lambda e: e.matmul(ps[bSi][:, cs_], lhsT=TGr[:, k1, :], rhs=bm[:, 64 + k1, :], start=False, stop=True), r=["TGr", "Bm2"], w=[PK[bSi]])
                    T.op("act", lambda e: e.copy(out=srs[:], in_=ps[bSr][:, :]), r=[PK[bSr]], w=["srsC"])
                    T.op("act", lambda e: e.copy(out=sis[:], in_=ps[bSi][:, :]), r=[PK[bSi]], w=["sisC"])
                    T.op("dve", lambda e: e.tensor_tensor(out=tm[0][:], in0=ps[bZr][:, :], in1=srs[:], op=ALU.mult), r=[PK[bZr], "srsC"], w=["tm0"])
                    T.op("dve", lambda e: e.tensor_tensor(out=tm[1][:], in0=ps[bZi][:, :], in1=sis[:], op=ALU.mult), r=[PK[bZi], "sisC"], w=["tm1"])
                    T.op("dve", lambda e: e.tensor_tensor(out=tm[2][:], in0=ps[bZr][:, :], in1=sis[:], op=ALU.mult), r=[PK[bZr], "sisC"], w=["tm2"])
                    T.op("dve", lambda e: e.tensor_tensor(out=tm[3][:], in0=ps[bZi][:, :], in1=srs[:], op=ALU.mult), r=[PK[bZi], "srsC"], w=["tm3"])
                    yv_r = Y[:, :, 0, kc * 16:(kc + 1) * 16]
                    yv_i = Y[:, :, 1, kc * 16:(kc + 1) * 16]
                    T.op("pool", lambda e: e.tensor_tensor(out=yv_r, in0=tm[0][:].rearrange("p (k c) -> p c k", c=GC),
                                                           in1=tm[1][:].rearrange("p (k c) -> p c k", c=GC), op=ALU.subtract),
                         r=["tm0", "tm1"], w=["YC"])
                    T.op("pool", lambda e: e.tensor_tensor(out=yv_i, in0=tm[2][:].rearrange("p (k c) -> p c k", c=GC),
                                                           in1=tm[3][:].rearrange("p (k c) -> p c k", c=GC), op=ALU.add),
                         r=["tm2", "tm3"], w=["YC"])
                for cq in range(GC // 2):
                    pi = cq % 2
                    for c2 in range(2):
                        c = cq * 2 + c2
                        T.op("pe", lambda e: e.matmul(ps[pi][0:64, c2 * 256:(c2 + 1) * 256], lhsT=Y[:, c, 0, :], rhs=R1[:, :],
                                                      start=True, stop=False), r=["YC", "R1"], w=[PK[pi]])
                        T.op("pe", lambda e: e.matmul(ps[pi][0:64, c2 * 256:(c2 + 1) * 256], lhsT=Y[:, c, 1, :], rhs=R2[:, :],
                                                      start=False, stop=True), r=["YC", "R2"], w=[PK[pi]])
                    T.op("act" if cq % 2 == 0 else "dve",
                         lambda e: (e.copy if cq % 2 == 0 else e.tensor_copy)(
                             out=Dm[:, :, cq * 2:(cq + 1) * 2],
                             in_=ps[pi][0:64, :].rearrange("p (c f) -> p f c", c=2)),
                         r=[PK[pi]], w=["DmC"])
                for nq in range(8):
                    pi = 6 + nq % 2
                    for j in range(16):
                        n2 = nq * 16 + j
                        T.op("pe", lambda e: e.matmul(ps[pi][0:32, j * GC:(j + 1) * GC], lhsT=TH[:, n2, 0, :], rhs=Dm[:, n2, :],
                                                      start=True, stop=False), r=["TH", "DmC"], w=[PK[pi]])
                        T.op("pe", lambda e: e.matmul(ps[pi][0:32, j * GC:(j + 1) * GC], lhsT=TH[:, n2, 1, :], rhs=Dm[:, 128 + n2, :],
                                                      start=False, stop=True), r=["TH", "DmC"], w=[PK[pi]])
                    nsl = slice(nq * 16, (nq + 1) * 16)
                    T.op("dve", lambda e: e.tensor_tensor(out=tmpy[:], in0=ps[pi][0:32, :].rearrange("p (n c) -> p c n", c=GC),
                                                          in1=zs[:, :, nsl], op=ALU.add), r=[PK[pi], "zsC"], w=["tmpyC"])
                    T.op("dve", lambda e: e.tensor_tensor(out=zf[:, :, nsl], in0=tmpy[:], in1=gt[:, :, nsl], op=ALU.mult),
                         r=["tmpyC", "gtC", "srcb0", "zsC"], w=["zf"])
            T.op("act", lambda e: e.copy(out=hob[:], in_=zf[:]), r=["zf"], w=["srcb0"])
            T.dma("sp", lambda e: e.dma_start(out=flay(hyoT_d[c0:c0 + GC, :]), in_=hob[:]), r=["srcb0"], w=["hyoT_d"])
        T.barrier()

    if "D" in stages:
      with ExitStack() as es:
        aff_all = sb(es, "aff_all", [128, NT, NE])
        with ExitStack() as es2:
            attnT = sb(es2, "attnT", [128, 4, L], BF16)
            hyoT = sb(es2, "hyoT", [128, 4, L], BF16)
            wpa = sb(es2, "wpa", [128, 4, D], BF16)
            wph = sb(es2, "wph", [128, 4, D], BF16)
            wo = sb(es2, "wo", [128, 8, D], BF16)
            g2bc = sb(es2, "g2bc", [128, D])
            wr = sb(es2, "wr", [128, 8, 16])
            gat = sb(es2, "gatD", [128, 16, 512], BF16)
            m1 = sb(es2, "m1D", [128, 512])
            m2 = sb(es2, "m2D", [128, 512])
            mT = sb(es2, "mTD", [128, 8, 512], BF16)
            xin = sb(es2, "xinD", [128, D])
            x1 = sb(es2, "x1D", [128, D])
            u2 = sb(es2, "u2D0", [128, D])
            u2b = sb(es2, "u2bD", [128, D], BF16)
            u2T = sb(es2, "u2TD", [128, 8, 128])
            junkd = sb(es2, "junkD", [128, D])
            st = sb(es2, "stD", [128, 8])
            lg = sb(es2, "lgD", [128, NE])
            for (tt_, td_, tk_) in ((attnT, attnT_d, "attnT"), (hyoT, hyoT_d, "hyoT")):
                T.dmas("sp", [(lambda e, j=j, a=a: e.dma_start(out=tt_[:, j, a:a + 2048], in_=td_[j * 128:(j + 1) * 128, a:a + 2048]))
                              for j in range(4) for a in (0, 2048)], w=[tk_])
            T.dma("sp", lambda e: e.dma_start(out=g2bc[:], in_=g2bc_d[:, :]), w=["g2bc"])
            T.dma("sp", lambda e: e.dma_start(out=wr[:], in_=wr_d[:, :, :]), w=["wr"])
            for (wsrc, nj, wdst, wk) in ((wpa_d, 4, wpa, "wpa"), (wph_d, 4, wph, "wph"), (wo_d, 8, wo, "wo")):
                T.dmas("pool", [(lambda e, a=a: e.dma_start(out=wdst[:, a:a + 4, :],
                                                            in_=wsrc.rearrange("(j p) c -> p j c", p=128)[:, a:a + 4, :]))
                                for a in range(0, nj, 4)], w=[wk])
            u2s = [u2, sb(es2, "u2D1", [128, D])]

            def d_tail(i):
                u2t = u2s[i % 2]
                for j in range(8):
                    T.op("pe", lambda e: e.transpose(out=ps[4 + j // 4][:, (j % 4) * 128:(j % 4 + 1) * 128],
                                                     in_=u2t[:, j * 128:(j + 1) * 128], identity=ident_f[:]),
                         r=["u2D%d" % (i % 2), "ident_f"], w=[PK[4 + j // 4]])
                for hh in range(2):
                    T.op("act", lambda e: e.copy(out=u2T[:, hh * 4:(hh + 1) * 4, :],
                                                 in_=ps[4 + hh][:, :].rearrange("p (j t) -> p j t", j=4)),
                         r=[PK[4 + hh]], w=["u2TD"])
                for j in range(8):
                    T.op("pe", lambda e: e.matmul(ps[6][:, 0:NE], lhsT=u2T[:, j, :], rhs=wr[:, j, :], start=(j == 0), stop=(j == 7)),
                         r=["u2TD", "wr"], w=[PK[6]])
                T.op("dve", lambda e: e.tensor_reduce(out=st2[:, 3:4], in_=ps[6][:, 0:NE], axis=AX.X, op=ALU.max), r=[PK[6], "st2D"], w=["st2D"])
                T.op("dve", lambda e: e.tensor_scalar(out=st2[:, 4:5], in0=st2[:, 3:4], scalar1=-1.0, scalar2=None, op0=ALU.mult), r=["st2D"], w=["st2D"])
                T.op("dve", lambda e: e.memset(st2[:, 5:6], 0.0), r=["st2D"], w=["st2D"])
                T.op("act", lambda e: e.activation(out=lg[:], in_=ps[6][:, 0:NE], func=AF.Exp, bias=st2[:, 4:5], accum_out=st2[:, 5:6]),
                     r=[PK[6], "st2D"], w=["lgD", "st2D"])
                T.op("dve", lambda e: e.reciprocal(out=st2[:, 6:7], in_=st2[:, 5:6]), r=["st2D"], w=["st2D"])
                T.op("dve", lambda e: e.tensor_scalar(out=aff_all[:, i, :], in0=lg[:], scalar1=st2[:, 6:7], scalar2=None, op0=ALU.mult),
                     r=["lgD", "st2D"], w=["aff_all"])

            st2 = sb(es2, "st2D", [128, 8])
            prev_i = None
            for tq in range(8):
                tsl = slice(tq * 512, (tq + 1) * 512)
                T.dmas("sp", [(lambda e, a=a: e.dma_start(out=gat[:, a:a + 8, :],
                                                          in_=gateT_d.rearrange("(j p) t -> p j t", p=128)[:, a:a + 8, tsl])) for a in (0, 8)],
                       w=["gatD"])
                for dt in range(8):
                    pa = 0 if dt % 2 == 0 else 7
                    for j in range(4):
                        T.op("pe", lambda e: e.matmul(ps[pa][:, :], lhsT=wpa[:, j, dt * 128:(dt + 1) * 128], rhs=attnT[:, j, tsl],
                                                      start=(j == 0), stop=(j == 3)), r=["wpa", "attnT"], w=[PK[pa]])
                    for j in range(4):
                        T.op("pe", lambda e: e.matmul(ps[1][:, :], lhsT=wph[:, j, dt * 128:(dt + 1) * 128], rhs=hyoT[:, j, tsl],
                                                      start=(j == 0), stop=(j == 3)), r=["wph", "hyoT"], w=[PK[1]])
                    T.op("dve", lambda e: e.tensor_tensor(out=m1[:], in0=ps[pa][:, :], in1=gat[:, dt, :], op=ALU.mult), r=[PK[pa], "gatD"], w=["m1D"])
                    T.op("dve", lambda e: e.tensor_tensor(out=m2[:], in0=ps[1][:, :], in1=gat[:, 8 + dt, :], op=ALU.mult), r=[PK[1], "gatD"], w=["m2D"])
                    T.op("pool", lambda e: e.tensor_tensor(out=mT[:, dt, :], in0=m1[:], in1=m2[:], op=ALU.add), r=["m1D", "m2D"], w=["mTD"])
                for ts in range(4):
                    i = tq * 4 + ts
                    u2t = u2s[i % 2]
                    u2k = "u2D%d" % (i % 2)
                    T.dma("sp", lambda e: e.dma_start(out=xin[:], in_=x_d[i * 128:(i + 1) * 128, :]), w=["xinD"])
                    for half in range(2):
                        for dt in range(8):
                            T.op("pe", lambda e: e.matmul(ps[2 + half][:, :], lhsT=mT[:, dt, ts * 128:(ts + 1) * 128],
                                                          rhs=wo[:, dt, half * 512:(half + 1) * 512], start=(dt == 0), stop=(dt == 7)),
                                 r=["mTD", "wo"], w=[PK[2 + half]])
                        T.op("dve", lambda e: e.tensor_tensor(out=x1[:, half * 512:(half + 1) * 512], in0=ps[2 + half][:, :],
                                                              in1=xin[:, half * 512:(half + 1) * 512], op=ALU.add),
                             r=[PK[2 + half], "xinD"], w=["x1D"])
                    if prev_i is not None:
                        d_tail(prev_i)
                    T.dma("sp", lambda e: e.dma_start(out=out_d[i * 128:(i + 1) * 128, :], in_=x1[:]), r=["x1D"], w=["out_d"])
                    T.op("dve", lambda e: e.memset(st[:, 0:1], 0.0), w=["stD"])
                    T.op("act", lambda e: e.activation(out=junkd[:], in_=x1[:], func=AF.Square, accum_out=st[:, 0:1]),
                         r=["x1D", "stD"], w=["junkD", "stD"])
                    T.op("act", lambda e: e.activation(out=st[:, 1:2], in_=st[:, 0:1], func=AF.Sqrt, scale=1.0 / D, bias=eps[:]),
                         r=["stD", "eps"], w=["stD"])
                    T.op("dve", lambda e: e.reciprocal(out=st[:, 2:3], in_=st[:, 1:2]), r=["stD"], w=["stD"])
                    T.op("dve", lambda e: e.scalar_tensor_tensor(out=u2t[:], in0=x1[:], scalar=st[:, 2:3], in1=g2bc[:],
                                                                 op0=ALU.mult, op1=ALU.mult), r=["x1D", "stD", "g2bc"], w=[u2k])
                    T.op("act", lambda e: e.copy(out=u2b[:], in_=u2t[:]), r=[u2k], w=["u2bD"])
                    T.dma("sp", lambda e: e.dma_start(out=u2_d[i * 128:(i + 1) * 128, :], in_=u2b[:]), r=["u2bD"], w=["u2_d"])
                    prev_i = i
            d_tail(prev_i)
            T.barrier()
        if "aff" in dbg_d:
            T.dma("sp", lambda e: e.dma_start(out=dbg_d["aff"], in_=aff_all[:]), r=["aff_all"], w=["dbgaff"])

        if "E" in stages:
          idx_all = sb(es, "idx_all", [128, NE, 4], I32)
          g_all = sb(es, "g_all", [128, NE, 4])
          with ExitStack() as es2:
            affT = sb(es2, "affT", [NE, L])
            cmpj = sb(es2, "cmpj", [NE, L])
            bs = sb(es2, "bsE", [NE, 8])
            tri = sb(es2, "tri", [128, 128])
            onesf = sb(es2, "onesf", [128, 128])
            iota = sb(es2, "iota", [128, 512])
            tidhl = sb(es2, "tidhl", [128, NT, 2], BF16)
            thbc = sb(es2, "thbc", [128, NE])
            dg = sb(es2, "dgE", [NE, NE])
            M = sb(es2, "ME", [128, NT, NE])
            pos = sb(es2, "posE", [128, NT, NE])
            srun = sb(es2, "srunE", [128, NE])
            cum = sb(es2, "cumE", [128, NE])
            vals = sb(es2, "valsE", [128, NT, NE, 4], BF16)
            afh = sb(es2, "afhE", [128, NT, NE], BF16)
            afr = sb(es2, "afrE", [128, NT, NE])
            oh = [sb(es2, "ohE%d" % i, [128, 512], BF16) for i in range(4)]
            idf = sb(es2, "idfE", [128, 4])
            pvs = sb(es2, "pvsE", [128, 16])
            T.dma("sp", lambda e: e.dma_start(out=tri[:], in_=tri_d[:, :]), w=["tri"])
            T.dma("sp", lambda e: e.dma_start(out=iota[:], in_=iota_d[:, :]), w=["iota"])
            T.dma("sp", lambda e: e.dma_start(out=tidhl[:], in_=tidhl_d[:, :, :]), w=["tidhl"])
            T.op("pool", lambda e: e.memset(onesf[:], 1.0), w=["onesf"])
            for i in range(NT):
                pi = i // 4
                T.op("pe", lambda e: e.transpose(out=ps[pi][0:NE, (i % 4) * 128:(i % 4 + 1) * 128], in_=aff_all[:, i, :],
                                                 identity=ident_f[:]), r=["aff_all", "ident_f"], w=[PK[pi]])
                if i % 4 == 3:
                    T.op("act", lambda e: e.copy(out=affT[:, (i - 3) * 128:(i + 1) * 128], in_=ps[pi][0:NE, :]), r=[PK[pi]], w=["affT"])
            T.op("dve", lambda e: e.memset(bs[:, 0:1], 0.0), w=["bsE"])
            T.op("dve", lambda e: e.memset(bs[:, 1:2], 1.0), r=["bsE"], w=["bsE"])
            for it in range(32):
                T.op("dve", lambda e: e.tensor_scalar(out=bs[:, 2:3], in0=bs[:, 0:1], scalar1=bs[:, 1:2], scalar2=0.5, op0=ALU.add, op1=ALU.mult), r=["bsE"], w=["bsE"])
                T.op("dve", lambda e: e.tensor_scalar(out=cmpj[:], in0=affT[:], scalar1=bs[:, 2:3], scalar2=None, op0=ALU.is_ge), r=["affT", "bsE"], w=["cmpj"])
                T.op("dve", lambda e: e.tensor_reduce(out=bs[:, 3:4], in_=cmpj[:], axis=AX.X, op=ALU.add), r=["cmpj", "bsE"], w=["bsE"])
                T.op("dve", lambda e: e.tensor_scalar(out=bs[:, 4:5], in0=bs[:, 3:4], scalar1=CAP - 0.5, scalar2=None, op0=ALU.is_ge), r=["bsE"], w=["bsE"])
                T.op("dve", lambda e: e.tensor_tensor(out=bs[:, 5:6], in0=bs[:, 2:3], in1=bs[:, 0:1], op=ALU.subtract), r=["bsE"], w=["bsE"])
                T.op("dve", lambda e: e.tensor_tensor(out=bs[:, 6:7], in0=bs[:, 1:2], in1=bs[:, 2:3], op=ALU.subtract), r=["bsE"], w=["bsE"])
                T.op("dve", lambda e: e.scalar_tensor_tensor(out=bs[:, 0:1], in0=bs[:, 5:6], scalar=bs[:, 4:5], in1=bs[:, 0:1], op0=ALU.mult, op1=ALU.add), r=["bsE"], w=["bsE"])
                T.op("dve", lambda e: e.scalar_tensor_tensor(out=bs[:, 1:2], in0=bs[:, 6:7], scalar=bs[:, 4:5], in1=bs[:, 2:3], op0=ALU.mult, op1=ALU.add), r=["bsE"], w=["bsE"])
            T.op("dve", lambda e: e.tensor_scalar(out=dg[:], in0=ident_f[0:NE, 0:NE], scalar1=bs[:, 0:1], scalar2=None, op0=ALU.mult), r=["bsE", "ident_f"], w=["dgE"])
            T.op("pe", lambda e: e.matmul(ps[0][:, 0:NE], lhsT=onesf[0:NE, :], rhs=dg[:, :], start=True, stop=True), r=["onesf", "dgE"], w=[PK[0]])
            T.op("dve", lambda e: e.tensor_copy(out=thbc[:], in_=ps[0][:, 0:NE]), r=[PK[0]], w=["thbc"])
            T.op("dve", lambda e: e.tensor_tensor(out=M[:], in0=aff_all[:], in1=thbc[:, :].unsqueeze(1).broadcast_to([128, NT, NE]), op=ALU.is_ge),
                 r=["aff_all", "thbc"], w=["ME"])
            T.op("dve", lambda e: e.memset(srun[:], 0.0), w=["srunE"])
            for i in range(NT):
                pi = 1 + i % 2
                T.op("pe", lambda e: e.matmul(ps[pi][:, 0:NE], lhsT=tri[:, :], rhs=M[:, i, :], start=True, stop=True), r=["tri", "ME"], w=[PK[pi]])
                T.op("pe", lambda e: e.matmul(ps[pi][:, NE:2 * NE], lhsT=onesf[:, :], rhs=M[:, i, :], start=True, stop=True), r=["onesf", "ME"], w=[PK[pi]])
                T.op("dve", lambda e: e.tensor_tensor(out=cum[:], in0=ps[pi][:, 0:NE], in1=srun[:], op=ALU.add), r=[PK[pi], "srunE"], w=["cumE"])
                T.op("dve", lambda e: e.tensor_tensor(out=srun[:], in0=ps[pi][:, NE:2 * NE], in1=srun[:], op=ALU.add), r=[PK[pi], "srunE", "cumE"], w=["srunE"])
                T.op("dve", lambda e: e.tensor_tensor(out=cum[:], in0=cum[:], in1=M[:, i, :], op=ALU.mult), r=["cumE", "ME"], w=["cumE"])
                T.op("dve", lambda e: e.tensor_scalar(out=pos[:, i, :], in0=cum[:], scalar1=-1.0, scalar2=None, op0=ALU.add), r=["cumE"], w=["posE"])
            T.op("dve", lambda e: e.tensor_copy(out=afh[:], in_=aff_all[:]), r=["aff_all"], w=["afhE"])
            T.op("dve", lambda e: e.tensor_tensor(out=afr[:], in0=aff_all[:], in1=afh[:], op=ALU.subtract), r=["aff_all", "afhE"], w=["afrE"])
            T.op("dve", lambda e: e.tensor_copy(out=vals[:, :, :, 2], in_=afh[:]), r=["afhE"], w=["valsE"])
            T.op("dve", lambda e: e.tensor_copy(out=vals[:, :, :, 3], in_=afr[:]), r=["afrE", "valsE"], w=["valsE"])
            T.op("dve", lambda e: e.tensor_copy(out=vals[:, :, :, 0:2], in_=tidhl[:, :, :].unsqueeze(2).broadcast_to([128, NT, NE, 2])),
                 r=["tidhl", "valsE"], w=["valsE"])
            oc = 0
            for ex in range(NE):
                pi = 3 + ex % 2
                for i in range(NT):
                    ob = oc % 4
                    eng = "dve"
                    oc += 1
                    T.op(eng, lambda e: e.tensor_scalar(out=oh[ob][:], in0=iota[:], scalar1=pos[:, i, ex:ex + 1], scalar2=None, op0=ALU.is_equal),
                         r=["iota", "posE"], w=["ohE%d" % ob])
                    for sc in range(4):
                        T.op("pe", lambda e: e.matmul(ps[pi][:, sc * 4:(sc + 1) * 4], lhsT=oh[ob][:, sc * 128:(sc + 1) * 128], rhs=vals[:, i, ex, :],
                                                      start=(i == 0 and sc == 0), stop=(i == NT - 1), skip_group_check=True),
                             r=["ohE%d" % ob, "valsE"], w=[PK[pi]])
                T.op("dve", lambda e: e.tensor_copy(out=pvs[:], in_=ps[pi][:, 0:16]), r=[PK[pi]], w=["pvsE"])
                pv = pvs[:, :].rearrange("p (s f) -> p s f", f=4)
                T.op("dve", lambda e: e.scalar_tensor_tensor(out=idf[:], in0=pv[:, :, 0], scalar=64.0, in1=pv[:, :, 1], op0=ALU.mult, op1=ALU.add),
                     r=["pvsE"], w=["idfE"])
                T.op("dve", lambda e: e.tensor_copy(out=idx_all[:, ex, :], in_=idf[:]), r=["idfE"], w=["idx_all"])
                T.op("dve", lambda e: e.tensor_tensor(out=g_all[:, ex, :], in0=pv[:, :, 2], in1=pv[:, :, 3], op=ALU.add), r=["pvsE"], w=["g_all"])
            T.barrier()
          if "idx" in dbg_d:
            T.dma("sp", lambda e: e.dma_start(out=dbg_d["idx"], in_=idx_all[:]), r=["idx_all"], w=["dbgidx"])
            T.dma("sp", lambda e: e.dma_start(out=dbg_d["g"], in_=g_all[:]), r=["g_all"], w=["dbgg"])

          if "F" in stages:
            wgb = [sb(es, "wgb%d" % i, [128, 8, D], BF16) for i in range(2)]
            wub = [sb(es, "wub%d" % i, [128, 8, D], BF16) for i in range(2)]
            wdb = [sb(es, "wdb%d" % i, [128, 8, D], BF16) for i in range(2)]
            xg = [sb(es, "xgF%d" % i, [128, D], BF16) for i in range(4)]
            xinT = sb(es, "xinT", [128, 8, 512], BF16)
            sg = sb(es, "sgF", [128, 512])
            hT = sb(es, "hTF", [128, 8, 512], BF16)
            eo = [sb(es, "eoF%d" % i, [128, D]) for i in range(2)]
            psb = [ps[6][:, :].bitcast(BF16), ps[7][:, :].bitcast(BF16)]

            def load_w(ex):
                for (wsrc, wdst, wk) in ((wg_d, wgb, "wgb"), (wu_d, wub, "wub"), (wd_d, wdb, "wdb")):
                    dst = wdst[ex % 2]
                    T.dmas("pool", [(lambda e, hh=hh: e.dma_start(out=dst[:, hh * 4:(hh + 1) * 4, :],
                                                                  in_=wsrc[ex].rearrange("(j p) c -> p j c", p=128)[:, hh * 4:(hh + 1) * 4, :]))
                                    for hh in range(2)], w=["%s%d" % (wk, ex % 2)])

            def gather(ex):
                for sc in range(4):
                    T.dma("pool", lambda e: e.indirect_dma_start(out=xg[sc][:], out_offset=None, in_=u2_d[:, :],
                                                                 in_offset=bass.IndirectOffsetOnAxis(ap=idx_all[:, ex, sc:sc + 1], axis=0)),
                          r=["idx_all", "u2_d"], w=["xgF%d" % sc])

            load_w(0)
            gather(0)
            for ex in range(NE):
                wgk, wuk, wdk = "wgb%d" % (ex % 2), "wub%d" % (ex % 2), "wdb%d" % (ex % 2)
                wgt, wut, wdt = wgb[ex % 2], wub[ex % 2], wdb[ex % 2]
                if ex + 1 < NE:
                    load_w(ex + 1)
                for sc in range(4):
                    for j in range(8):
                        T.op("pe", lambda e: e.transpose(out=psb[sc % 2][:, j * 128:(j + 1) * 128], in_=xg[sc][:, j * 128:(j + 1) * 128], identity=ident_b[:]),
                             r=["xgF%d" % sc, "ident_b"], w=[PK[6 + sc % 2]])
                    T.op("act", lambda e: e.copy(out=xinT[:, :, sc * 128:(sc + 1) * 128], in_=psb[sc % 2].rearrange("p (j t) -> p j t", j=8)),
                         r=[PK[6 + sc % 2]], w=["xinT"])
                if ex + 1 < NE:
                    gather(ex + 1)
                for ft in range(8):
                    pg, pu = (ft % 2) * 2, (ft % 2) * 2 + 1
                    for j in range(8):
                        T.op("pe", lambda e: e.matmul(ps[pg][:, :], lhsT=wgt[:, j, ft * 128:(ft + 1) * 128], rhs=xinT[:, j, :], start=(j == 0), stop=(j == 7)),
                             r=[wgk, "xinT"], w=[PK[pg]])
                    for j in range(8):
                        T.op("pe", lambda e: e.matmul(ps[pu][:, :], lhsT=wut[:, j, ft * 128:(ft + 1) * 128], rhs=xinT[:, j, :], start=(j == 0), stop=(j == 7)),
                             r=[wuk, "xinT"], w=[PK[pu]])
                    T.op("act", lambda e: e.activation(out=sg[:], in_=ps[pg][:, :], func=AF.Silu), r=[PK[pg]], w=["sgF"])
                    T.op("dve", lambda e: e.tensor_tensor(out=hT[:, ft, :], in0=ps[pu][:, :], in1=sg[:], op=ALU.mult), r=[PK[pu], "sgF"], w=["hTF"])
                for sc in range(4):
                    eb = eo[sc % 2]
                    for half in range(2):
                        po = 4 + half
                        for ft in range(8):
                            T.op("pe", lambda e: e.matmul(ps[po][:, :], lhsT=hT[:, ft, sc * 128:(sc + 1) * 128], rhs=wdt[:, ft, half * 512:(half + 1) * 512],
                                                          start=(ft == 0), stop=(ft == 7)), r=["hTF", wdk], w=[PK[po]])
                        T.op("dve", lambda e: e.tensor_scalar(out=eb[:, half * 512:(half + 1) * 512], in0=ps[po][:, :], scalar1=g_all[:, ex, sc:sc + 1],
                                                              scalar2=None, op0=ALU.mult), r=[PK[po], "g_all"], w=["eoF%d" % (sc % 2)])
                    T.dma("pool", lambda e: e.indirect_dma_start(out=out_d[:, :], out_offset=bass.IndirectOffsetOnAxis(ap=idx_all[:, ex, sc:sc + 1], axis=0),
                                                                 in_=eb[:], in_offset=None, compute_op=ALU.add),
                          r=["eoF%d" % (sc % 2), "idx_all"], w=["out_d"])
            T.barrier()
    for k, v in dbg_d.items():
        pass
    T.barrier()
    top.close()
    return nc


_CONST = None


def make_inputs(inp, b):
    global _CONST
    if _CONST is None:
        _CONST = host_consts()
    f = lambda a: np.ascontiguousarray(np.asarray(a, dtype=np.float32))
    m = dict(_CONST)
    m["x"] = f(inp["x"][b])
    m["g1bc"] = f(np.broadcast_to(inp["norm1_g"][0][None, :], (128, D)))
    m["w_in"] = f(inp["w_in"][0])
    scw = np.concatenate([inp["short_conv_w"][0], inp["short_conv_b"][0][None, :]], axis=0)
    m["scw"] = f(scw.reshape(4, 12, 128).transpose(2, 1, 0))
    qkg = np.stack([inp["q_norm_g"][0], inp["k_norm_g"][0]], axis=0)
    m["qkg"] = f(np.broadcast_to(qkg[None], (128, 2, 64)))
    m["lamv"] = f(np.stack([inp["lambda_q1"][0], inp["lambda_k1"][0], inp["lambda_q2"][0], inp["lambda_k2"][0]])[None])
    m["subg"] = f(np.broadcast_to(inp["subln_g"][0][None, :], (128, 128)))
    m["fw1"] = f(inp["filt_w1"][0])
    m["fw2"] = f(inp["filt_w2"][0])
    m["fw3"] = f(inp["filt_w3"][0])
    m["fbf"] = f(np.stack([inp["filt_b1"][0], inp["filt_b2"][0], inp["filt_b3"][0], inp["filt_freq"][0]], axis=1))
    m["fwo"] = f(inp["filt_w_out"][0])
    m["skipbc"] = f(np.broadcast_to(inp["hyena_skip"][0][None], (32, 2, 512)))
    m["wpa"] = f(inp["w_branch_attn"][0])
    m["wph"] = f(inp["w_branch_hyena"][0])
    m["wo"] = f(inp["w_out"][0])
    m["g2bc"] = f(np.broadcast_to(inp["norm2_g"][0][None, :], (128, D)))
    m["wr"] = f(inp["w_router"][0].reshape(8, 128, 16).transpose(1, 0, 2))
    m["wg"] = f(inp["w_gate"][0])
    m["wu"] = f(inp["w_up"][0])
    m["wd"] = f(inp["w_down"][0])
    return m


def kernel(**inputs):
    nc = build()
    maps = [make_inputs(inputs, c % 4) for c in range(4)]
    in_maps = [maps[c % 4] for c in range(NCORES)]
    res = run_bass_kernel_spmd(nc, in_maps, core_ids=list(range(NCORES)))
    out = np.stack([np.asarray(res.results[b]["out"], dtype=np.float32) for b in range(4)], axis=0)
    return out
```

```python
import math
from contextlib import ExitStack
import numpy as np
import ml_dtypes
import concourse.bass as bass
import concourse.mybir as mybir
from concourse.bass_utils import run_bass_kernel_spmd

F32 = mybir.dt.float32
BF16 = mybir.dt.bfloat16
I32 = mybir.dt.int32
AF = mybir.ActivationFunctionType
ALU = mybir.AluOpType
AX = mybir.AxisListType

L = 4096
D = 1024
NT = 32
NCORES = 8
CAP = 512
NE = 16
GC = 32
NK = 33
DEBUG = False


class Trk:
    def __init__(s, nc):
        s.nc = nc
        s.eng = dict(pe=nc.tensor, act=nc.scalar, dve=nc.vector, pool=nc.gpsimd, sp=nc.sync)
        s.sem = {}
        s.cnt = {}
        for k in ("pe", "act", "dve", "pool"):
            s.sem[k] = nc.alloc_semaphore(name="s_" + k)
            s.cnt[k] = 0
        s.ND = 16
        for i in range(s.ND):
            k = "d%d" % i
            s.sem[k] = nc.alloc_semaphore(name="s_" + k)
            s.cnt[k] = 0
        s.pool_of = {"sp": list(range(0, 8)), "pool": list(range(8, 16))}
        s.rrq = {"sp": 0, "pool": 0}
        s.seen = {e: {} for e in s.eng}
        s.lw = {}
        s.rd = {}

    def _wait(s, e, deps):
        for k, v in deps.items():
            if e == "pe" and k == "pe":
                continue
            if s.seen[e].get(k, 0) < v:
                s.eng[e].wait_ge(s.sem[k], v)
                s.seen[e][k] = v

    def _deps(s, r, w):
        d = {}

        def add(k, v):
            if d.get(k, 0) < v:
                d[k] = v
        for x in r:
            for k, v in s.lw.get(x, {}).items():
                add(k, v)
        for x in w:
            for k, v in s.lw.get(x, {}).items():
                add(k, v)
            for k, v in s.rd.get(x, {}).items():
                add(k, v)
        return d

    def _upd(s, toks, r, w):
        if isinstance(toks, tuple):
            toks = [toks]
        for x in r:
            m = s.rd.setdefault(x, {})
            for tok in toks:
                if m.get(tok[0], 0) < tok[1]:
                    m[tok[0]] = tok[1]
        for x in w:
            m = {}
            for tok in toks:
                if m.get(tok[0], 0) < tok[1]:
                    m[tok[0]] = tok[1]
            s.lw[x] = m
            s.rd[x] = {}

    def op(s, e, fn, r=(), w=()):
        s._wait(e, s._deps(r, w))
        ins = fn(s.eng[e])
        s.cnt[e] += 1
        ins.then_inc(s.sem[e], 1)
        s._upd((e, s.cnt[e]), r, w)

    def _next_sem(s, q):
        pool = s.pool_of[q]
        k = "d%d" % pool[s.rrq[q] % len(pool)]
        s.rrq[q] += 1
        if s.cnt[k] > 0:
            s._wait(q, {k: s.cnt[k]})
        return k

    def dma(s, q, fn, r=(), w=()):
        s._wait(q, s._deps(r, w))
        k = s._next_sem(q)
        ins = fn(s.eng[q])
        s.cnt[k] += 16
        ins.then_inc(s.sem[k], 16)
        s._upd((k, s.cnt[k]), r, w)

    def dmas(s, q, fns, r=(), w=()):
        s._wait(q, s._deps(r, w))
        toks = []
        for fn in fns:
            k = s._next_sem(q)
            ins = fn(s.eng[q])
            s.cnt[k] += 16
            ins.then_inc(s.sem[k], 16)
            toks.append((k, s.cnt[k]))
        s._upd(toks, r, w)

    def barrier(s, engines=None):
        tot = {k: v for k, v in s.cnt.items() if v > 0}
        for e in (engines or s.eng):
            s._wait(e, dict(tot))
        s.lw = {}
        s.rd = {}


def _bf(a):
    return np.ascontiguousarray(a.astype(np.float32)).astype(ml_dtypes.bfloat16)


def host_consts():
    c = {}
    t = np.arange(L, dtype=np.float32)
    inv_freq = (np.float32(500000.0) ** (-np.arange(0, 16, 2, dtype=np.float32) / np.float32(16))).astype(np.float32)
    ang = (t[:, None] * inv_freq[None, :]).astype(np.float32)
    cs = np.concatenate([np.cos(ang), np.sin(ang)], axis=-1).astype(np.float32)
    c["ropecs"] = np.ascontiguousarray(cs.reshape(NT, 128, 16).transpose(1, 0, 2))
    tt = t / np.float32(L - 1)
    bands = np.linspace(1e-4, 15, 16, dtype=np.float32)
    a2 = (np.float32(2.0 * math.pi / L) * t[:, None] * bands[None, :]).astype(np.float32)
    emb = np.concatenate([tt[:, None], np.cos(a2), -np.sin(a2)], axis=-1).astype(np.float32)
    c["embT"] = np.ascontiguousarray(emb.T)
    c["posrow"] = np.ascontiguousarray(np.broadcast_to(tt[None, :], (128, L))).astype(np.float32)
    min_decay = math.log(1e-2) / 1.5
    max_decay = math.log(1e-2) / 0.3
    deltas = np.abs(np.linspace(min_decay, max_decay, 512, dtype=np.float32))
    c["ndelta"] = np.ascontiguousarray((-deltas).reshape(4, 128).T).astype(np.float32)
    n1 = np.arange(32)[:, None]
    k1 = np.arange(NK)[None, :]
    th = 2 * np.pi * n1 * k1 / 64.0
    c["FA"] = _bf(np.concatenate([np.cos(th), -np.sin(th)], axis=1))
    n2 = np.arange(128)[:, None, None]
    k1 = np.arange(NK)[None, :, None]
    k2 = np.arange(128)[None, None, :]
    th = 2 * np.pi * ((n2 * (k1 + 64 * k2)) % 8192) / 8192.0
    c["TGr"] = _bf(np.cos(th))
    c["TGi"] = _bf(-np.sin(th))
    c["TGn"] = _bf(np.sin(th))
    kk2 = np.arange(128)[:, None]
    nn2 = np.arange(128)[None, :]
    th = 2 * np.pi * ((kk2 * nn2) % 128) / 128.0
    fr, fi = np.cos(th), np.sin(th)
    c["R1"] = _bf(np.concatenate([fr, fi], axis=1))
    c["R2"] = _bf(np.concatenate([-fi, fr], axis=1))
    k1 = np.arange(NK)[:, None, None]
    n2 = np.arange(128)[None, :, None]
    n1 = np.arange(32)[None, None, :]
    th = 2 * np.pi * ((k1 * (n2 + 128 * n1)) % 8192) / 8192.0
    coef = np.where((k1 == 0) | (k1 == 32), 1.0, 2.0)[:, :, None, :]
    th_ = np.stack([np.cos(th), -np.sin(th)], axis=2) * coef / 8192.0
    c["TH"] = _bf(th_)
    tid = np.arange(L).reshape(NT, 128).T
    c["tidhl"] = _bf(np.stack([tid // 64, tid % 64], axis=-1))
    c["iota512"] = np.ascontiguousarray(np.broadcast_to(np.arange(512, dtype=np.float32)[None, :], (128, 512)))
    tri = (np.arange(128)[:, None] <= np.arange(128)[None, :]).astype(np.float32)
    c["tri"] = tri
    return c


def build(dbg=None):
    nc = bass.Bass("TRN2", target_bir_lowering=False)
    dbg = dbg or {}

    def din(name, shape, dt=F32):
        return nc.dram_tensor(name, list(shape), dt, kind="ExternalInput").ap()

    def dscr(name, shape, dt=F32):
        kind = "ExternalOutput" if name in dbg.get("_ext", ()) else "Internal"
        return nc.dram_tensor(name, list(shape), dt, kind=kind).ap()

    x_d = din("x", [L, D])
    g1bc_d = din("g1bc", [128, D])
    w_in_d = din("w_in", [D, 5120])
    scw_d = din("scw", [128, 12, 4])
    qkg_d = din("qkg", [128, 2, 64])
    lam_d = din("lamv", [1, 4, 64])
    subg_d = din("subg", [128, 128])
    fw1_d = din("fw1", [33, 64])
    fw2_d = din("fw2", [64, 64])
    fw3_d = din("fw3", [64, 64])
    fbf_d = din("fbf", [64, 4])
    fwo_d = din("fwo", [64, 2048])
    skip_d = din("skipbc", [32, 2, 512])
    wpa_d = din("wpa", [512, D])
    wph_d = din("wph", [512, D])
    wo_d = din("wo", [D, D])
    g2bc_d = din("g2bc", [128, D])
    wr_d = din("wr", [128, 8, 16])
    wg_d = din("wg", [NE, D, D])
    wu_d = din("wu", [NE, D, D])
    wd_d = din("wd", [NE, D, D])
    ropecs_d = din("ropecs", [128, NT, 16])
    embT_d = din("embT", [33, L])
    posrow_d = din("posrow", [128, L])
    ndelta_d = din("ndelta", [128, 4])
    FA_d = din("FA", [32, 2 * NK], BF16)
    TGr_d = din("TGr", [128, NK, 128], BF16)
    TGi_d = din("TGi", [128, NK, 128], BF16)
    TGn_d = din("TGn", [128, NK, 128], BF16)
    R1_d = din("R1", [128, 256], BF16)
    R2_d = din("R2", [128, 256], BF16)
    TH_d = din("TH", [NK, 128, 2, 32], BF16)
    tidhl_d = din("tidhl", [128, NT, 2], BF16)
    iota_d = din("iota512", [128, 512])
    tri_d = din("tri", [128, 128])

    out_d = nc.dram_tensor("out", [L, D], F32, kind="ExternalOutput").ap()

    qT_d = dscr("qT_s", [4, 128, L], BF16)
    kT_d = dscr("kT_s", [4, 128, L], BF16)
    v_d = dscr("v_s", [L, 512], BF16)
    hyc_d = dscr("hyc_s", [1536, L], F32)
    gateT_d = dscr("gateT_s", [2048, L], BF16)
    kern_d = dscr("kern_s", [2, 2, 512, L], BF16)
    attnT_d = dscr("attnT_s", [512, L], BF16)
    hyoT_d = dscr("hyoT_s", [512, L], BF16)
    u2_d = dscr("u2_s", [L, D], BF16)

    dbg_d = {}
    for k, (shape, dt) in ((k, v) for k, v in dbg.items() if not k.startswith("_")):
        dbg_d[k] = nc.dram_tensor("dbg_" + k, list(shape), dt, kind="ExternalOutput").ap()

    T = Trk(nc)
    top = ExitStack()

    sbc = [0]

    def sb(es, name, shape, dt=F32):
        sbc[0] += 1
        return es.enter_context(nc.sbuf_tensor("sb%d_%s" % (sbc[0], name), list(shape), dt))

    ps = [top.enter_context(nc.psum_tensor("ps%d" % i, [128, 512], F32)) for i in range(8)]
    PK = ["ps%d" % i for i in range(8)]

    ident_f = sb(top, "ident_f", [128, 128])
    ident_b = sb(top, "ident_b", [128, 128], BF16)
    eps = sb(top, "eps", [128, 1])
    T.op("pool", lambda e: e.memset(ident_f[:], 0.0), w=["ident_f"])
    T.op("pool", lambda e: e.affine_select(out=ident_f[:], in_=ident_f[:], pattern=[[-1, 128]],
                                           compare_op=ALU.not_equal, fill=1.0, base=0, channel_multiplier=1),
         r=["ident_f"], w=["ident_f"])
    T.op("pool", lambda e: e.tensor_copy(out=ident_b[:], in_=ident_f[:]), r=["ident_f"], w=["ident_b"])
    T.op("pool", lambda e: e.memset(eps[:], 1e-6), w=["eps"])

    stages = dbg.get("_stages", "AKBCDEF") if isinstance(dbg.get("_stages", None), str) else "AKBCDEF"

    if "A" in stages:
      with ExitStack() as es:
        uT = sb(es, "uT", [128, 8, L + 2], BF16)
        g1bc = sb(es, "g1bc", [128, D])
        scw = sb(es, "scw", [128, 12, 4])
        qkg = sb(es, "qkg", [128, 2, 64])
        ropecs = sb(es, "ropecs", [128, NT, 16])
        xt = [sb(es, "xt%d" % i, [128, D]) for i in range(2)]
        xb = [sb(es, "xb%d" % i, [128, D], BF16) for i in range(2)]
        junk = sb(es, "junkA", [128, D])
        ss = sb(es, "ssA", [128, NT])
        rs = sb(es, "rsA", [128, NT])
        wb = [sb(es, "wb%d" % i, [128, 8, 512], BF16) for i in range(2)]
        T.dma("sp", lambda e: e.dma_start(out=g1bc[:], in_=g1bc_d[:, :]), w=["g1bc"])
        T.dma("sp", lambda e: e.dma_start(out=scw[:], in_=scw_d[:, :, :]), w=["scw"])
        T.dma("sp", lambda e: e.dma_start(out=qkg[:], in_=qkg_d[:, :, :]), w=["qkg"])
        T.dma("sp", lambda e: e.dma_start(out=ropecs[:], in_=ropecs_d[:, :, :]), w=["ropecs"])
        T.op("dve", lambda e: e.memset(ss[:], 0.0), w=["ssA"])
        T.op("dve", lambda e: e.memset(uT[:, :, 0:1], 0.0), w=["uTpad0"])
        T.op("dve", lambda e: e.memset(uT[:, :, L + 1:L + 2], 0.0), w=["uTpad1"])
        T.op("dve", lambda e: e.tensor_scalar(out=qkg[:, 0, :], in0=qkg[:, 0, :], scalar1=0.125, scalar2=None,
                                              op0=ALU.mult), r=["qkg"], w=["qkg"])
        psb = [ps[6][:, :].bitcast(BF16), ps[7][:, :].bitcast(BF16)]
        for i in range(NT):
            b = i % 2
            T.dma("sp", lambda e: e.dma_start(out=xt[b][:], in_=x_d[i * 128:(i + 1) * 128, :]), w=["xt%d" % b])
            T.op("act", lambda e: e.activation(out=junk[:], in_=xt[b][:], func=AF.Square,
                                               accum_out=ss[:, i:i + 1]), r=["xt%d" % b, "ssA"], w=["junkA", "ss%d" % i])
            T.op("act", lambda e: e.activation(out=rs[:, i:i + 1], in_=ss[:, i:i + 1], func=AF.Sqrt,
                                               scale=1.0 / D, bias=eps[:]), r=["ss%d" % i, "eps"], w=["rs%d" % i])
            T.op("dve", lambda e: e.reciprocal(out=rs[:, i:i + 1], in_=rs[:, i:i + 1]), r=["rs%d" % i], w=["rs%d" % i])
            T.op("dve", lambda e: e.scalar_tensor_tensor(out=xb[b][:], in0=xt[b][:], scalar=rs[:, i:i + 1], in1=g1bc[:],
                                                         op0=ALU.mult, op1=ALU.mult),
                 r=["xt%d" % b, "rs%d" % i, "g1bc"], w=["xb%d" % b])
            for j in range(8):
                T.op("pe", lambda e: e.transpose(out=psb[b][:, j * 128:(j + 1) * 128],
                                                 in_=xb[b][:, j * 128:(j + 1) * 128], identity=ident_b[:]),
                     r=["xb%d" % b, "ident_b"], w=[PK[6 + b]])
            T.op("act", lambda e: e.copy(out=uT[:, :, 1 + i * 128:1 + (i + 1) * 128],
                                         in_=psb[b].rearrange("p (j t) -> p j t", j=8)),
                 r=[PK[6 + b]], w=["uT%d" % i])
        UTK = ["uT%d" % i for i in range(NT)] + ["uTpad0", "uTpad1"]

        sq = sb(es, "sqA", [128, 512])
        ssq = sb(es, "ssqA", [128, 8])
        qn = sb(es, "qnA", [128, 8, 64])
        qb2 = [sb(es, "qbA%d" % i, [128, 512], BF16) for i in range(2)]
        rt = [sb(es, "rtA%d" % i, [128, 8, 8]) for i in range(4)]
        qTs = [sb(es, "qTs%d" % i, [128, 4, 128], BF16) for i in range(2)]
        vst = [sb(es, "vst%d" % i, [128, 512], BF16) for i in range(2)]
        hraw = sb(es, "hraw", [128, L + 2])
        hc = sb(es, "hcA", [128, L])
        gst = sb(es, "gstA", [128, L], BF16)
        T.op("pool", lambda e: e.memset(hraw[:, 0:1], 0.0), w=["hrawp0"])
        T.op("pool", lambda e: e.memset(hraw[:, L + 1:L + 2], 0.0), w=["hrawp1"])
        pcnt = [0]

        def nextps():
            pcnt[0] += 1
            return pcnt[0] % 4

        for cc in dbg.get('_ccs', range(10)):
            wbk = "wb%d" % (cc % 2)
            wbt = wb[cc % 2]
            T.dmas("pool", [(lambda e, a=a: e.dma_start(
                out=wbt[:, a:a + 4, :], in_=w_in_d.rearrange("(j p) c -> p j c", p=128)[:, a:a + 4, cc * 512:(cc + 1) * 512]))
                for a in (0, 4)], w=[wbk])
            if cc < 3:
                def qk_tail(i):
                    qbt = qb2[i % 2]
                    pb = 6 + (i % 2)
                    for h in range(4):
                        T.op("pe", lambda e: e.transpose(out=psb[i % 2][:, h * 128:(h + 1) * 128],
                                                         in_=qbt[:, h * 128:(h + 1) * 128], identity=ident_b[:]),
                             r=["qbA%d" % (i % 2), "ident_b"], w=[PK[pb]])
                    st = qTs[i % 2]
                    T.op("act", lambda e: e.copy(out=st[:], in_=psb[i % 2][:, 0:512].rearrange("p (h t) -> p h t", h=4)),
                         r=[PK[pb]], w=["qTs%d" % (i % 2)])
                    dst = (qT_d if cc == 0 else kT_d).rearrange("h p t -> p h t")[:, :, i * 128:(i + 1) * 128]
                    T.dma("sp", lambda e: e.dma_start(out=dst, in_=st[:]), r=["qTs%d" % (i % 2)], w=["qkT_d"])

                for i in range(NT):
                    pi = nextps()
                    for j in range(8):
                        T.op("pe", lambda e: e.matmul(ps[pi][:, :], lhsT=uT[:, j, 1 + i * 128:1 + (i + 1) * 128],
                                                      rhs=wbt[:, j, :], start=(j == 0), stop=(j == 7)),
                             r=["uT%d" % i, wbk], w=[PK[pi]])
                    if cc == 2:
                        vs = vst[i % 2]
                        T.op("act", lambda e: e.copy(out=vs[:], in_=ps[pi][:, :]), r=[PK[pi]], w=["vst%d" % (i % 2)])
                        T.dma("sp", lambda e: e.dma_start(out=v_d[i * 128:(i + 1) * 128, :], in_=vs[:]),
                              r=["vst%d" % (i % 2)], w=["v_d"])
                        continue
                    if i > 0:
                        qk_tail(i - 1)
                    qb = qb2[i % 2]
                    qbk = "qbA%d" % (i % 2)
                    T.op("act", lambda e: e.activation(out=sq[:], in_=ps[pi][:, :], func=AF.Square), r=[PK[pi]], w=["sqA"])
                    T.op("dve", lambda e: e.tensor_reduce(out=ssq[:], in_=sq[:].rearrange("p (g d) -> p g d", d=64),
                                                          axis=AX.X, op=ALU.add), r=["sqA"], w=["ssqA"])
                    T.op("act", lambda e: e.activation(out=ssq[:], in_=ssq[:], func=AF.Sqrt, scale=1.0 / 64,
                                                       bias=eps[:]), r=["ssqA", "eps"], w=["ssqA"])
                    T.op("dve", lambda e: e.reciprocal(out=ssq[:], in_=ssq[:]), r=["ssqA"], w=["ssqA"])
                    T.op("dve", lambda e: e.tensor_tensor(out=qn[:], in0=ps[pi][:, :].rearrange("p (g d) -> p g d", d=64),
                                                          in1=ssq[:, :].unsqueeze(2).broadcast_to([128, 8, 64]),
                                                          op=ALU.mult), r=[PK[pi], "ssqA"], w=["qnA"])
                    T.op("dve", lambda e: e.tensor_tensor(out=qn[:], in0=qn[:],
                                                          in1=qkg[:, cc, :].unsqueeze(1).broadcast_to([128, 8, 64]),
                                                          op=ALU.mult), r=["qnA", "qkg"], w=["qnA"])
                    T.op("act", lambda e: e.copy(out=qb[:].rearrange("p (g d) -> p g d", d=64), in_=qn[:]),
                         r=["qnA"], w=[qbk])
                    cosb = ropecs[:, i, 0:8].unsqueeze(1).broadcast_to([128, 8, 8])
                    sinb = ropecs[:, i, 8:16].unsqueeze(1).broadcast_to([128, 8, 8])
                    r1 = qn[:, :, 0:8]
                    r2 = qn[:, :, 8:16]
                    T.op("dve", lambda e: e.tensor_tensor(out=rt[0][:], in0=r1, in1=cosb, op=ALU.mult), r=["qnA", "ropecs"], w=["rt0"])
                    T.op("dve", lambda e: e.tensor_tensor(out=rt[1][:], in0=r2, in1=sinb, op=ALU.mult), r=["qnA", "ropecs"], w=["rt1"])
                    T.op("dve", lambda e: e.tensor_tensor(out=rt[2][:], in0=r2, in1=cosb, op=ALU.mult), r=["qnA", "ropecs"], w=["rt2"])
                    T.op("dve", lambda e: e.tensor_tensor(out=rt[3][:], in0=r1, in1=sinb, op=ALU.mult), r=["qnA", "ropecs"], w=["rt3"])
                    qbv = qb[:].rearrange("p (g d) -> p g d", d=64)
                    T.op("dve", lambda e: e.tensor_tensor(out=qbv[:, :, 0:8], in0=rt[0][:], in1=rt[1][:], op=ALU.subtract),
                         r=["rt0", "rt1"], w=[qbk])
                    T.op("dve", lambda e: e.tensor_tensor(out=qbv[:, :, 8:16], in0=rt[2][:], in1=rt[3][:], op=ALU.add),
                         r=["rt2", "rt3"], w=[qbk])
                if cc < 2:
                    qk_tail(NT - 1)
            else:
                for ct in range(4):
                    for tq in range(8):
                        pi = nextps()
                        for j in range(8):
                            T.op("pe", lambda e: e.matmul(ps[pi][:, :], lhsT=wbt[:, j, ct * 128:(ct + 1) * 128],
                                                          rhs=uT[:, j, 1 + tq * 512:1 + (tq + 1) * 512],
                                                          start=(j == 0), stop=(j == 7)),
                                 r=UTK[4 * tq:4 * tq + 4] + [wbk], w=[PK[pi]])
                        if cc < 6:
                            T.op("act", lambda e: e.copy(out=hraw[:, 1 + tq * 512:1 + (tq + 1) * 512], in_=ps[pi][:, :]),
                                 r=[PK[pi]], w=["hraw"])
                        else:
                            T.op("act", lambda e: e.activation(out=gst[:, tq * 512:(tq + 1) * 512], in_=ps[pi][:, :],
                                                               func=AF.Sigmoid), r=[PK[pi]], w=["gstA"])
                    if cc < 6:
                        cti = (cc - 3) * 4 + ct
                        lvl = dbg.get('_lvl', 9)
                        if lvl >= 1:
                          T.op("act", lambda e: e.activation(out=hc[:], in_=hraw[:, 1:L + 1], func=AF.Identity,
                                                           scale=scw[:, cti, 1:2], bias=scw[:, cti, 3:4]),
                             r=["hraw", "scw"], w=["hcA"])
                        if lvl >= 2:
                          T.op("dve", lambda e: e.scalar_tensor_tensor(out=hc[:], in0=hraw[:, 0:L], scalar=scw[:, cti, 0:1],
                                                                     in1=hc[:], op0=ALU.mult, op1=ALU.add),
                             r=["hraw", "hrawp0", "scw", "hcA"], w=["hcA"])
                          T.op("dve", lambda e: e.scalar_tensor_tensor(out=hc[:], in0=hraw[:, 2:L + 2], scalar=scw[:, cti, 2:3],
                                                                     in1=hc[:], op0=ALU.mult, op1=ALU.add),
                             r=["hraw", "hrawp1", "scw", "hcA"], w=["hcA"])
                        if lvl >= 3:
                          for q4 in range(4):
                              T.dma("sp", lambda e: e.dma_start(out=hyc_d[cti * 128:(cti + 1) * 128, q4 * 1024:(q4 + 1) * 1024],
                                                                in_=hc[:, q4 * 1024:(q4 + 1) * 1024]),
                                    r=["hcA"], w=["hyc_d%d" % q4])
                    else:
                        gti = (cc - 6) * 4 + ct
                        T.dmas("sp", [(lambda e, a=a: e.dma_start(out=gateT_d[gti * 128:(gti + 1) * 128, a:a + 2048], in_=gst[:, a:a + 2048]))
                                      for a in (0, 2048)], r=["gstA"], w=["gateT_d"])
        T.barrier()

    if "K" in stages:
      with ExitStack() as es:
        embT = sb(es, "embT", [33, L])
        fw1 = sb(es, "fw1", [33, 64])
        fw2 = sb(es, "fw2", [64, 64])
        fw3 = sb(es, "fw3", [64, 64])
        fbf = sb(es, "fbf", [64, 4])
        frb = sb(es, "frb", [64, 3])
        fwo = sb(es, "fwo", [64, 2048])
        posrow = sb(es, "posrow", [128, L])
        ndelta = sb(es, "ndelta", [128, 4])
        hid = [sb(es, "hid%d" % i, [64, L]) for i in range(2)]
        hidb = sb(es, "hidb", [64, L])
        dec = sb(es, "decK", [128, L])
        kf = sb(es, "kfK", [128, L])
        kb = sb(es, "kbK", [128, L])
        hp = sb(es, "hpK", [128, L], BF16)
        hm = sb(es, "hmK", [128, L], BF16)
        for tns, src, k in ((fw1, fw1_d, "fw1"), (fw2, fw2_d, "fw2"), (fw3, fw3_d, "fw3"),
                            (fbf, fbf_d, "fbf"), (ndelta, ndelta_d, "ndelta")):
            T.dma("sp", lambda e: e.dma_start(out=tns[:], in_=src), w=[k])
        for tns, src, k, n in ((embT, embT_d, "embT", L), (fwo, fwo_d, "fwo", 2048), (posrow, posrow_d, "posrow", L)):
            T.dmas("sp", [(lambda e, a=a: e.dma_start(out=tns[:, a:a + 1024], in_=src[:, a:a + 1024]))
                          for a in range(0, n, 1024)], w=[k])
        for l in range(3):
            T.op("dve", lambda e: e.tensor_tensor(out=frb[:, l:l + 1], in0=fbf[:, l:l + 1], in1=fbf[:, 3:4], op=ALU.mult),
                 r=["fbf"], w=["frb"])
        TWO_PI = 2.0 * math.pi
        srcs = [(embT, "embT", 33, fw1, "fw1"), (hid[0], "hid0", 64, fw2, "fw2"), (hid[1], "hid1", 64, fw3, "fw3")]
        outs = [(hid[0], "hid0"), (hid[1], "hid1"), (hid[0], "hid0")]
        negpi = sb(es, "negpi", [64, 1])
        T.op("dve", lambda e: e.memset(negpi[:], 4.0 * math.pi), w=["negpi"])
        kcnt = sb(es, "kcntK", [64, L])
        for l in range(3):
            src, sk, kk, w_, wk = srcs[l]
            dst, dk = outs[l]
            for tq in range(8):
                pi = tq % 4
                T.op("pe", lambda e: e.matmul(ps[pi][0:64, :], lhsT=w_[0:kk, :], rhs=src[0:kk, tq * 512:(tq + 1) * 512],
                                              start=True, stop=True), r=[sk, wk], w=[PK[pi]])
                T.op("dve", lambda e: e.tensor_scalar(out=hidb[:, tq * 512:(tq + 1) * 512], in0=ps[pi][0:64, :],
                                                      scalar1=fbf[:, 3:4], scalar2=frb[:, l:l + 1], op0=ALU.mult,
                                                      op1=ALU.add), r=[PK[pi], "fbf", "frb"], w=["hidb"])
                xs_ = hidb[:, tq * 512:(tq + 1) * 512]
                ks_ = kcnt[:, tq * 512:(tq + 1) * 512]
                T.op("dve", lambda e: e.tensor_scalar(out=ks_, in0=xs_, scalar1=-3.0 * math.pi, scalar2=None, op0=ALU.is_gt),
                     r=["hidb"], w=["kcnt"])
                for thr in (-math.pi, math.pi, 3.0 * math.pi):
                    T.op("dve", lambda e: e.scalar_tensor_tensor(out=ks_, in0=xs_, scalar=thr, in1=ks_, op0=ALU.is_gt, op1=ALU.add),
                         r=["hidb", "kcnt"], w=["kcnt"])
                T.op("dve", lambda e: e.scalar_tensor_tensor(out=xs_, in0=ks_, scalar=-TWO_PI, in1=xs_, op0=ALU.mult, op1=ALU.add),
                     r=["hidb", "kcnt"], w=["hidb"])
                T.op("act", lambda e: e.activation(out=dst[:, tq * 512:(tq + 1) * 512], in_=xs_,
                                                   func=AF.Sin, bias=negpi[:]), r=["hidb", "negpi"], w=[dk])
        h3 = hid[0]
        for ct in range(4):
            T.op("act", lambda e: e.activation(out=dec[:], in_=posrow[:], func=AF.Exp, scale=ndelta[:, ct:ct + 1]),
                 r=["posrow", "ndelta"], w=["decK"])
            for o in range(2):
                for d_, (dst, dk) in enumerate(((kf, "kfK"), (kb, "kbK"))):
                    col = (o * 2 + d_) * 512 + ct * 128
                    for tq in range(8):
                        pi = tq % 4
                        T.op("pe", lambda e: e.matmul(ps[pi][:, :], lhsT=fwo[:, col:col + 128],
                                                      rhs=h3[:, tq * 512:(tq + 1) * 512], start=True, stop=True),
                             r=["hid0", "fwo"], w=[PK[pi]])
                        T.op("dve", lambda e: e.tensor_tensor(out=dst[:, tq * 512:(tq + 1) * 512], in0=ps[pi][:, :],
                                                              in1=dec[:, tq * 512:(tq + 1) * 512], op=ALU.mult),
                             r=[PK[pi], "decK"], w=[dk])
                T.op("dve", lambda e: e.memset(kb[:, 0:1], 0.0), r=["kbK"], w=["kbK"])
                T.op("pool", lambda e: e.tensor_tensor(out=hp[:], in0=kf[:], in1=kb[:], op=ALU.add), r=["kfK", "kbK"], w=["hpK"])
                T.op("pool", lambda e: e.tensor_tensor(out=hm[:], in0=kf[:], in1=kb[:], op=ALU.subtract), r=["kfK", "kbK"], w=["hmK"])
                T.dmas("sp", [(lambda e, a=a: e.dma_start(out=kern_d[o, 0, ct * 128:(ct + 1) * 128, a:a + 2048], in_=hp[:, a:a + 2048]))
                              for a in range(0, L, 2048)], r=["hpK"], w=["kern_d0"])
                T.dmas("sp", [(lambda e, a=a: e.dma_start(out=kern_d[o, 1, ct * 128:(ct + 1) * 128, a:a + 2048], in_=hm[:, a:a + 2048]))
                              for a in range(0, L, 2048)], r=["hmK"], w=["kern_d1"])
        T.barrier()

    if "B" in stages:
      with ExitStack() as es:
        qT = sb(es, "qT", [128, 4, L], BF16)
        kT = sb(es, "kT", [128, 2, 4, L], BF16)
        V = sb(es, "Vsb", [128, NT, 4, 132], BF16)
        lamv = sb(es, "lamv", [1, 4, 64])
        lamt = sb(es, "lamt", [1, 8])
        ones1 = sb(es, "ones1", [1, 128])
        nlam = sb(es, "nlam", [128, 1])
        subg = sb(es, "subg", [128, 128])
        E = [sb(es, "E%d" % i, [128, 512], BF16) for i in range(3)]
        osb = sb(es, "osbB", [128, 2, 4, 129])
        aacc = sb(es, "aacc", [128, 4, 128])
        ab = sb(es, "abB", [128, 4, 128], BF16)
        junkb = sb(es, "junkB", [128, 4, 128])
        sm = sb(es, "smB", [128, 8, 4])
        aTs = [sb(es, "aTs%d" % i, [128, 512], BF16) for i in range(2)]
        T.dmas("sp", [(lambda e, h=h, a=a: e.dma_start(out=qT[:, h, a:a + 2048], in_=qT_d[h, :, a:a + 2048]))
                      for h in range(4) for a in (0, 2048)], w=["qT"])
        T.op("pool", lambda e: e.memset(kT[64:128, 0, :, :], 0.0), w=["kTz0"])
        T.op("dve", lambda e: e.memset(kT[0:64, 1, :, :], 0.0), w=["kTz1"])
        T.dmas("sp", [(lambda e, h=h, a=a, m=m: e.dma_start(out=kT[m * 64:(m + 1) * 64, m, h, a:a + 2048],
                                                            in_=kT_d[h, m * 64:(m + 1) * 64, a:a + 2048]))
                      for m in range(2) for h in range(4) for a in (0, 2048)], w=["kT"])
        T.dmas("sp", [(lambda e, i=i: e.dma_start(out=V[:, i, :, 0:128],
                                                  in_=v_d[i * 128:(i + 1) * 128, :].rearrange("p (h d) -> p h d", d=128)))
                      for i in range(NT)], w=["V"])
        T.op("pool", lambda e: e.memset(V[:, :, :, 128:129], 1.0), w=["Vones"])
        T.dma("sp", lambda e: e.dma_start(out=lamv[:], in_=lam_d[:, :, :]), w=["lamv"])
        T.dma("sp", lambda e: e.dma_start(out=subg[:], in_=subg_d[:, :]), w=["subg"])
        T.op("dve", lambda e: e.tensor_scalar(out=subg[:], in0=subg[:], scalar1=0.8, scalar2=None, op0=ALU.mult),
             r=["subg"], w=["subg"])
        T.op("dve", lambda e: e.memset(ones1[:], 1.0), w=["ones1"])
        T.op("dve", lambda e: e.memset(lamt[:], 0.0), w=["lamt"])
        T.op("dve", lambda e: e.tensor_tensor(out=lamv[:, 0, :], in0=lamv[:, 0, :], in1=lamv[:, 1, :], op=ALU.mult), r=["lamv"], w=["lamv"])
        T.op("dve", lambda e: e.tensor_tensor(out=lamv[:, 2, :], in0=lamv[:, 2, :], in1=lamv[:, 3, :], op=ALU.mult), r=["lamv"], w=["lamv"])
        T.op("dve", lambda e: e.tensor_reduce(out=lamt[:, 0:1], in_=lamv[:, 0, :], axis=AX.X, op=ALU.add), r=["lamv", "lamt"], w=["lamt"])
        T.op("dve", lambda e: e.tensor_reduce(out=lamt[:, 1:2], in_=lamv[:, 2, :], axis=AX.X, op=ALU.add), r=["lamv", "lamt"], w=["lamt"])
        T.op("act", lambda e: e.activation(out=lamt[:, 2:4], in_=lamt[:, 0:2], func=AF.Exp), r=["lamt"], w=["lamt"])
        T.op("dve", lambda e: e.tensor_tensor(out=lamt[:, 4:5], in0=lamt[:, 3:4], in1=lamt[:, 2:3], op=ALU.subtract), r=["lamt"], w=["lamt"])
        T.op("dve", lambda e: e.tensor_scalar(out=lamt[:, 5:6], in0=lamt[:, 4:5], scalar1=-0.2, scalar2=None, op0=ALU.add), r=["lamt"], w=["lamt"])
        T.op("pe", lambda e: e.matmul(ps[7][:, 0:1], lhsT=ones1[:, :], rhs=lamt[:, 5:6], start=True, stop=True),
             r=["ones1", "lamt"], w=[PK[7]])
        T.op("dve", lambda e: e.tensor_copy(out=nlam[:], in_=ps[7][:, 0:1]), r=[PK[7]], w=["nlam"])
        psT = ps[6][:, :].bitcast(BF16)
        steps = [(h, qc, m, kt) for h in range(4) for qc in range(8) for m in range(2) for kt in range(NT)]
        USE_POW = False

        def issue_S(idx):
            h, qc, m, kt = steps[idx]
            sp_ = idx % 2
            eb = idx % 3
            T.op("pe", lambda e: e.matmul(ps[sp_][:, :], lhsT=kT[:, m, h, kt * 128:(kt + 1) * 128],
                                          rhs=qT[:, h, qc * 512:(qc + 1) * 512], start=True, stop=True),
                 r=["kT", "kTz0", "kTz1", "qT"], w=[PK[sp_]])
            T.op("act", lambda e: e.activation(out=E[eb][:], in_=ps[sp_][:, :], func=AF.Exp),
                 r=[PK[sp_]], w=["E%d" % eb])

        def issue_PV(idx):
            h, qc, m, kt = steps[idx]
            eb = idx % 3
            for qs in range(4):
                bank = 2 + m * 2 + qs // 2
                T.op("pe", lambda e: e.matmul(ps[bank][:, (qs % 2) * 256:(qs % 2) * 256 + 129],
                                              lhsT=E[eb][:, qs * 128:(qs + 1) * 128], rhs=V[:, kt, h, 0:129],
                                              start=(kt == 0 and qs % 2 == 0), stop=(kt == NT - 1),
                                              skip_group_check=True),
                     r=["E%d" % eb, "V", "Vones"], w=[PK[bank]])

        def bc4(ap2):
            return ap2.unsqueeze(2).broadcast_to([128, 4, 128])

        def epi_head():
            for m in range(2):
                for half in range(2):
                    bank = 2 + m * 2 + half
                    src = ps[bank][:, :].rearrange("p (a c) -> p a c", a=2)[:, :, 0:129]
                    dst = osb[:, m, half * 2:(half + 1) * 2, :]
                    if half == 0:
                        T.op("dve", lambda e: e.tensor_copy(out=dst, in_=src), r=[PK[bank]], w=["osb%d%d" % (m, half)])
                    else:
                        T.op("pool" if False else "dve", lambda e: e.tensor_copy(out=dst, in_=src), r=[PK[bank]], w=["osb%d%d" % (m, half)])
            OS = ["osb00", "osb01", "osb10", "osb11"]
            T.op("dve", lambda e: e.reciprocal(out=sm[:, 0:2, :], in_=osb[:, :, :, 128]), r=OS, w=["smB"])
            T.op("dve", lambda e: e.tensor_scalar(out=sm[:, 2, :], in0=sm[:, 1, :], scalar1=nlam[:, 0:1], scalar2=None, op0=ALU.mult),
                 r=["smB", "nlam"], w=["smB"])
            T.op("dve", lambda e: e.tensor_tensor(out=aacc[:], in0=osb[:, 0, :, 0:128], in1=bc4(sm[:, 0, :]), op=ALU.mult),
                 r=OS + ["smB"], w=["aacc"])
            T.op("dve", lambda e: e.tensor_tensor(out=junkb[:], in0=osb[:, 1, :, 0:128], in1=bc4(sm[:, 2, :]), op=ALU.mult),
                 r=OS + ["smB"], w=["junkB"])
            T.op("dve", lambda e: e.tensor_tensor(out=aacc[:], in0=aacc[:], in1=junkb[:], op=ALU.add), r=["aacc", "junkB"], w=["aacc"])
            T.op("dve", lambda e: e.tensor_tensor(out=junkb[:], in0=aacc[:], in1=aacc[:], op=ALU.mult), r=["aacc"], w=["junkB"])
            T.op("dve", lambda e: e.tensor_reduce(out=sm[:, 3, :], in_=junkb[:], axis=AX.X, op=ALU.add), r=["junkB", "smB"], w=["smB"])
            T.op("dve", lambda e: e.tensor_scalar(out=sm[:, 4, :], in0=sm[:, 3, :], scalar1=1.0 / 128, scalar2=1e-6, op0=ALU.mult, op1=ALU.add),
                 r=["smB"], w=["smB"])
            if USE_POW:
                T.op("dve", lambda e: e.tensor_scalar(out=sm[:, 5, :], in0=sm[:, 4, :], scalar1=-0.5, scalar2=None, op0=ALU.pow),
                     r=["smB"], w=["smB"])
            else:
                T.op("act", lambda e: e.activation(out=sm[:, 6, :], in_=sm[:, 4, :], func=AF.Sqrt), r=["smB"], w=["smB"])
                T.op("dve", lambda e: e.reciprocal(out=sm[:, 5, :], in_=sm[:, 6, :]), r=["smB"], w=["smB"])
            T.op("dve", lambda e: e.tensor_tensor(out=aacc[:], in0=aacc[:], in1=bc4(sm[:, 5, :]), op=ALU.mult), r=["aacc", "smB"], w=["aacc"])
            T.op("dve", lambda e: e.tensor_tensor(out=ab[:], in0=aacc[:], in1=subg[:, :].unsqueeze(1).broadcast_to([128, 4, 128]), op=ALU.mult),
                 r=["aacc", "subg"], w=["abB"])

        def epi_tail(h, qc):
            for qs in range(4):
                T.op("pe", lambda e: e.transpose(out=psT[:, qs * 128:(qs + 1) * 128], in_=ab[:, qs, :], identity=ident_b[:]),
                     r=["abB", "ident_b"], w=[PK[6]])
            st = aTs[qc % 2]
            T.op("act", lambda e: e.copy(out=st[:], in_=psT[:, 0:512]), r=[PK[6]], w=["aTs%d" % (qc % 2)])
            T.dma("sp", lambda e: e.dma_start(out=attnT_d[h * 128:(h + 1) * 128, qc * 512:(qc + 1) * 512], in_=st[:]),
                  r=["aTs%d" % (qc % 2)], w=["attnT_d"])

        pending = None
        issue_S(0)
        for idx in range(len(steps)):
            h, qc, m, kt = steps[idx]
            if idx + 1 < len(steps):
                issue_S(idx + 1)
            issue_PV(idx)
            if pending is not None and m == 0 and kt == 12:
                epi_tail(*pending)
                pending = None
            if m == 1 and kt == NT - 1:
                epi_head()
                pending = (h, qc)
        epi_tail(*pending)
        T.barrier()

    if "C" in stages:
      with ExitStack() as es:
        FA = sb(es, "FA", [32, 2 * NK], BF16)
        TGr = sb(es, "TGr", [128, NK, 128], BF16)
        TGi = sb(es, "TGi", [128, NK, 128], BF16)
        TGn = sb(es, "TGn", [128, NK, 128], BF16)
        KCS = [(0, 16), (16, 32), (32, NK)]
        R1 = sb(es, "R1", [128, 256], BF16)
        R2 = sb(es, "R2", [128, 256], BF16)
        TH = sb(es, "TH", [NK, 128, 2, 32], BF16)
        skipbc = sb(es, "skipbc", [32, 2, GC])
        for tns, src, k in ((FA, FA_d, "FA"), (R1, R1_d, "R1"), (R2, R2_d, "R2")):
            T.dma("sp", lambda e: e.dma_start(out=tns[:], in_=src), w=[k])
        for tns, src, k in ((TGr, TGr_d, "TGr"), (TGi, TGi_d, "TGi"), (TGn, TGn_d, "TGn")):
            T.dmas("sp", [(lambda e, a=a, b=b: e.dma_start(out=tns[:, a:b, :], in_=src[:, a:b, :])) for (a, b) in KCS], w=[k])
        T.dmas("sp", [(lambda e, a=a: e.dma_start(out=TH[:, a:a + 32, :, :], in_=TH_d[:, a:a + 32, :, :])) for a in range(0, 128, 32)], w=["TH"])
        zf = sb(es, "zf", [32, GC, 128])
        gt = sb(es, "gtC", [32, GC, 128])
        zs = sb(es, "zsC", [32, GC, 128])
        srcb = [sb(es, "srcb%d" % i, [32, GC, 128], BF16) for i in range(3)]
        Bm_ = [sb(es, "Bm%d" % i, [128, 2 * NK, GC], BF16) for i in range(3)]
        srs = sb(es, "srsC", [128, 512])
        sis = sb(es, "sisC", [128, 512])
        tm = [sb(es, "tmC%d" % i, [128, 512]) for i in range(4)]
        Y = sb(es, "YC", [128, GC, 2, NK], BF16)
        Dm = sb(es, "DmC", [NK, 256, GC], BF16)
        tmpy = sb(es, "tmpyC", [32, GC, 16])
        hob = srcb[0]

        def flay(ap2d):
            return ap2d.rearrange("c (a b) -> a c b", b=128)

        def load_filters(g_, o_):
            for pm in range(2):
                T.dma("sp", lambda e: e.dma_start(out=srcb[1 + pm][:], in_=flay(kern_d[o_, pm, g_ * GC:(g_ + 1) * GC, :])),
                      w=["srcb%d" % (1 + pm)])

        for g in range(512 // GC):
            c0 = g * GC
            T.dma("sp", lambda e: e.dma_start(out=zf[:], in_=flay(hyc_d[c0:c0 + GC, :])), w=["zf"])
            T.dma("sp", lambda e: e.dma_start(out=skipbc[:], in_=skip_d[:, :, c0:c0 + GC]), w=["skipbc"])
            for o in range(2):
                T.dma("sp", lambda e: e.dma_start(out=gt[:], in_=flay(hyc_d[512 * (o + 1) + c0:512 * (o + 1) + c0 + GC, :])),
                      w=["gtC"])
                T.op("act", lambda e: e.copy(out=srcb[0][:], in_=zf[:]), r=["zf"], w=["srcb0"])
                if g == 0 and o == 0:
                    load_filters(0, 0)
                T.op("pool", lambda e: e.tensor_tensor(out=zs[:], in0=zf[:],
                                                       in1=skipbc[:, o, :].unsqueeze(2).broadcast_to([32, GC, 128]),
                                                       op=ALU.mult), r=["zf", "skipbc"], w=["zsC"])
                for s_ in (1, 2, 0):
                    for cq, (ca, cb) in enumerate(((0, 7), (7, 14), (14, 21), (21, 28), (28, 32))):
                        pi = cq % 2
                        ncg = cb - ca
                        for c4 in range(ncg):
                            c = ca + c4
                            T.op("pe", lambda e: e.matmul(ps[pi][:, c4 * 2 * NK:(c4 + 1) * 2 * NK], lhsT=srcb[s_][:, c, :],
                                                          rhs=FA[:, :], start=True, stop=True),
                                 r=["srcb%d" % s_, "FA"], w=[PK[pi]])
                        T.op("act" if cq % 2 == 0 else "dve",
                             lambda e: (e.copy if cq % 2 == 0 else e.tensor_copy)(
                                 out=Bm_[s_][:, :, ca:cb],
                                 in_=ps[pi][:, 0:ncg * 2 * NK].rearrange("p (c f) -> p f c", c=ncg)),
                             r=[PK[pi]], w=["Bm%d" % s_])
                nxt = (g, 1) if o == 0 else (g + 1, 0)
                if nxt[0] < 512 // GC:
                    load_filters(*nxt)
                for kc, (ka, kb_) in enumerate(KCS):
                    bZr, bZi, bSr, bSi = (2, 3, 4, 5) if kc % 2 == 0 else (0, 1, 6, 7)
                    nk = kb_ - ka
                    wd = nk * GC
                    for j in range(nk):
                        k1 = ka + j
                        cs_ = slice(j * GC, (j + 1) * GC)
                        bz, bp, bm = Bm_[0], Bm_[1], Bm_[2]
                        T.op("pe", lambda e: e.matmul(ps[bZr][:, cs_], lhsT=TGr[:, k1, :], rhs=bz[:, k1, :], start=True, stop=False), r=["TGr", "Bm0"], w=[PK[bZr]])
                        T.op("pe", lambda e: e.matmul(ps[bZr][:, cs_], lhsT=TGn[:, k1, :], rhs=bz[:, NK + k1, :], start=False, stop=True), r=["TGn", "Bm0"], w=[PK[bZr]])
                        T.op("pe", lambda e: e.matmul(ps[bZi][:, cs_], lhsT=TGi[:, k1, :], rhs=bz[:, k1, :], start=True, stop=False), r=["TGi", "Bm0"], w=[PK[bZi]])
                        T.op("pe", lambda e: e.matmul(ps[bZi][:, cs_], lhsT=TGr[:, k1, :], rhs=bz[:, NK + k1, :], start=False, stop=True), r=["TGr", "Bm0"], w=[PK[bZi]])
                        T.op("pe", lambda e: e.matmul(ps[bSr][:, cs_], lhsT=TGr[:, k1, :], rhs=bp[:, k1, :], start=True, stop=False), r=["TGr", "Bm1"], w=[PK[bSr]])
                        T.op("pe", lambda e: e.matmul(ps[bSr][:, cs_], lhsT=TGn[:, k1, :], rhs=bp[:, NK + k1, :], start=False, stop=True), r=["TGn", "Bm1"], w=[PK[bSr]])
                        T.op("pe", lambda e: e.matmul(ps[bSi][:, cs_], lhsT=TGi[:, k1, :], rhs=bm[:, k1, :], start=True, stop=False), r=["TGi", "Bm2"], w=[PK[bSi]])
                        T.op("pe", lambda e: e.matmul(ps[bSi][:, cs_], lhsT=TGr[:, k1, :], rhs=bm[:, NK + k1, :], start=False, stop=True), r=["TGr", "Bm2"], w=[PK[bSi]])
                    T.op("act", lambda e: e.copy(out=srs[:, 0:wd], in_=ps[bSr][:, 0:wd]), r=[PK[bSr]], w=["srsC"])
                    T.op("act", lambda e: e.copy(out=sis[:, 0:wd], in_=ps[bSi][:, 0:wd]), r=[PK[bSi]], w=["sisC"])
                    T.op("dve", lambda e: e.tensor_tensor(out=tm[0][:, 0:wd], in0=ps[bZr][:, 0:wd], in1=srs[:, 0:wd], op=ALU.mult), r=[PK[bZr], "srsC"], w=["tm0"])
                    T.op("dve", lambda e: e.tensor_tensor(out=tm[1][:, 0:wd], in0=ps[bZi][:, 0:wd], in1=sis[:, 0:wd], op=ALU.mult), r=[PK[bZi], "sisC"], w=["tm1"])
                    T.op("dve", lambda e: e.tensor_tensor(out=tm[2][:, 0:wd], in0=ps[bZr][:, 0:wd], in1=sis[:, 0:wd], op=ALU.mult), r=[PK[bZr], "sisC"], w=["tm2"])
                    T.op("dve", lambda e: e.tensor_tensor(out=tm[3][:, 0:wd], in0=ps[bZi][:, 0:wd], in1=srs[:, 0:wd], op=ALU.mult), r=[PK[bZi], "srsC"], w=["tm3"])
                    yv_r = Y[:, :, 0, ka:kb_]
                    yv_i = Y[:, :, 1, ka:kb_]
                    T.op("pool", lambda e: e.tensor_tensor(out=yv_r, in0=tm[0][:, 0:wd].rearrange("p (k c) -> p c k", c=GC),
                                                           in1=tm[1][:, 0:wd].rearrange("p (k c) -> p c k", c=GC), op=ALU.subtract),
                         r=["tm0", "tm1"], w=["YC"])
                    T.op("pool", lambda e: e.tensor_tensor(out=yv_i, in0=tm[2][:, 0:wd].rearrange("p (k c) -> p c k", c=GC),
                                                           in1=tm[3][:, 0:wd].rearrange("p (k c) -> p c k", c=GC), op=ALU.add),
                         r=["tm2", "tm3"], w=["YC"])
                for cq in range(GC // 2):
                    pi = cq % 2
                    for c2 in range(2):
                        c = cq * 2 + c2
                        T.op("pe", lambda e: e.matmul(ps[pi][0:NK, c2 * 256:(c2 + 1) * 256], lhsT=Y[:, c, 0, :], rhs=R1[:, :],
                                                      start=True, stop=False), r=["YC", "R1"], w=[PK[pi]])
                        T.op("pe", lambda e: e.matmul(ps[pi][0:NK, c2 * 256:(c2 + 1) * 256], lhsT=Y[:, c, 1, :], rhs=R2[:, :],
                                                      start=False, stop=True), r=["YC", "R2"], w=[PK[pi]])
                    T.op("act" if cq % 2 == 0 else "dve",
                         lambda e: (e.copy if cq % 2 == 0 else e.tensor_copy)(
                             out=Dm[:, :, cq * 2:(cq + 1) * 2],
                             in_=ps[pi][0:NK, :].rearrange("p (c f) -> p f c", c=2)),
                         r=[PK[pi]], w=["DmC"])
                for nq in range(8):
                    pi = 6 + nq % 2
                    for j in range(16):
                        n2 = nq * 16 + j
                        T.op("pe", lambda e: e.matmul(ps[pi][0:32, j * GC:(j + 1) * GC], lhsT=TH[:, n2, 0, :], rhs=Dm[:, n2, :],
                                                      start=True, stop=False), r=["TH", "DmC"], w=[PK[pi]])
                        T.op("pe", lambda e: e.matmul(ps[pi][0:32, j * GC:(j + 1) * GC], lhsT=TH[:, n2, 1, :], rhs=Dm[:, 128 + n2, :],
                                                      start=False, stop=True), r=["TH", "DmC"], w=[PK[pi]])
                    nsl = slice(nq * 16, (nq + 1) * 16)
                    T.op("dve", lambda e: e.tensor_tensor(out=tmpy[:], in0=ps[pi][0:32, :].rearrange("p (n c) -> p c n", c=GC),
                                                          in1=zs[:, :, nsl], op=ALU.add), r=[PK[pi], "zsC"], w=["tmpyC"])
                    T.op("dve", lambda e: e.tensor_tensor(out=zf[:, :, nsl], in0=tmpy[:], in1=gt[:, :, nsl], op=ALU.mult),
                         r=["tmpyC", "gtC", "srcb0", "zsC"], w=["zf"])
            T.op("act", lambda e: e.copy(out=hob[:], in_=zf[:]), r=["zf"], w=["srcb0"])
            T.dma("sp", lambda e: e.dma_start(out=flay(hyoT_d[c0:c0 + GC, :]), in_=hob[:]), r=["srcb0"], w=["hyoT_d"])
        T.barrier()

    if "D" in stages:
      with ExitStack() as es:
        aff_all = sb(es, "aff_all", [128, NT, NE])
        with ExitStack() as es2:
            attnT = sb(es2, "attnT", [128, 4, L], BF16)
            hyoT = sb(es2, "hyoT", [128, 4, L], BF16)
            wpa = sb(es2, "wpa", [128, 4, D], BF16)
            wph = sb(es2, "wph", [128, 4, D], BF16)
            wo = sb(es2, "wo", [128, 8, D], BF16)
            g2bc = sb(es2, "g2bc", [128, D])
            wr = sb(es2, "wr", [128, 8, 16])
            gat = sb(es2, "gatD", [128, 16, 512], BF16)
            m1 = sb(es2, "m1D", [128, 512])
            m2 = sb(es2, "m2D", [128, 512])
            mT = sb(es2, "mTD", [128, 8, 512], BF16)
            xin = sb(es2, "xinD", [128, D])
            x1 = sb(es2, "x1D", [128, D])
            u2 = sb(es2, "u2D0", [128, D])
            u2b = sb(es2, "u2bD", [128, D], BF16)
            u2T = sb(es2, "u2TD", [128, 8, 128])
            junkd = sb(es2, "junkD", [128, D])
            st = sb(es2, "stD", [128, 8])
            lg = sb(es2, "lgD", [128, NE])
            for (tt_, td_, tk_) in ((attnT, attnT_d, "attnT"), (hyoT, hyoT_d, "hyoT")):
                T.dmas("sp", [(lambda e, j=j, a=a: e.dma_start(out=tt_[:, j, a:a + 2048], in_=td_[j * 128:(j + 1) * 128, a:a + 2048]))
                              for j in range(4) for a in (0, 2048)], w=[tk_])
            T.dma("sp", lambda e: e.dma_start(out=g2bc[:], in_=g2bc_d[:, :]), w=["g2bc"])
            T.dma("sp", lambda e: e.dma_start(out=wr[:], in_=wr_d[:, :, :]), w=["wr"])
            for (wsrc, nj, wdst, wk) in ((wpa_d, 4, wpa, "wpa"), (wph_d, 4, wph, "wph"), (wo_d, 8, wo, "wo")):
                T.dmas("pool", [(lambda e, a=a: e.dma_start(out=wdst[:, a:a + 4, :],
                                                            in_=wsrc.rearrange("(j p) c -> p j c", p=128)[:, a:a + 4, :]))
                                for a in range(0, nj, 4)], w=[wk])
            u2s = [u2, sb(es2, "u2D1", [128, D])]

            def d_tail(i):
                u2t = u2s[i % 2]
                for j in range(8):
                    T.op("pe", lambda e: e.transpose(out=ps[4 + j // 4][:, (j % 4) * 128:(j % 4 + 1) * 128],
                                                     in_=u2t[:, j * 128:(j + 1) * 128], identity=ident_f[:]),
                         r=["u2D%d" % (i % 2), "ident_f"], w=[PK[4 + j // 4]])
                for hh in range(2):
                    T.op("act", lambda e: e.copy(out=u2T[:, hh * 4:(hh + 1) * 4, :],
                                                 in_=ps[4 + hh][:, :].rearrange("p (j t) -> p j t", j=4)),
                         r=[PK[4 + hh]], w=["u2TD"])
                for j in range(8):
                    T.op("pe", lambda e: e.matmul(ps[6][:, 0:NE], lhsT=u2T[:, j, :], rhs=wr[:, j, :], start=(j == 0), stop=(j == 7)),
                         r=["u2TD", "wr"], w=[PK[6]])
                T.op("dve", lambda e: e.tensor_reduce(out=st2[:, 3:4], in_=ps[6][:, 0:NE], axis=AX.X, op=ALU.max), r=[PK[6], "st2D"], w=["st2D"])
                T.op("dve", lambda e: e.tensor_scalar(out=st2[:, 4:5], in0=st2[:, 3:4], scalar1=-1.0, scalar2=None, op0=ALU.mult), r=["st2D"], w=["st2D"])
                T.op("dve", lambda e: e.memset(st2[:, 5:6], 0.0), r=["st2D"], w=["st2D"])
                T.op("act", lambda e: e.activation(out=lg[:], in_=ps[6][:, 0:NE], func=AF.Exp, bias=st2[:, 4:5], accum_out=st2[:, 5:6]),
                     r=[PK[6], "st2D"], w=["lgD", "st2D"])
                T.op("dve", lambda e: e.reciprocal(out=st2[:, 6:7], in_=st2[:, 5:6]), r=["st2D"], w=["st2D"])
                T.op("dve", lambda e: e.tensor_scalar(out=aff_all[:, i, :], in0=lg[:], scalar1=st2[:, 6:7], scalar2=None, op0=ALU.mult),
                     r=["lgD", "st2D"], w=["aff_all"])

            st2 = sb(es2, "st2D", [128, 8])
            prev_i = None
            for tq in range(8):
                tsl = slice(tq * 512, (tq + 1) * 512)
                T.dmas("sp", [(lambda e, a=a: e.dma_start(out=gat[:, a:a + 8, :],
                                                          in_=gateT_d.rearrange("(j p) t -> p j t", p=128)[:, a:a + 8, tsl])) for a in (0, 8)],
                       w=["gatD"])
                for dt in range(8):
                    pa = 0 if dt % 2 == 0 else 7
                    for j in range(4):
                        T.op("pe", lambda e: e.matmul(ps[pa][:, :], lhsT=wpa[:, j, dt * 128:(dt + 1) * 128], rhs=attnT[:, j, tsl],
                                                      start=(j == 0), stop=(j == 3)), r=["wpa", "attnT"], w=[PK[pa]])
                    for j in range(4):
                        T.op("pe", lambda e: e.matmul(ps[1][:, :], lhsT=wph[:, j, dt * 128:(dt + 1) * 128], rhs=hyoT[:, j, tsl],
                                                      start=(j == 0), stop=(j == 3)), r=["wph", "hyoT"], w=[PK[1]])
                    T.op("dve", lambda e: e.tensor_tensor(out=m1[:], in0=ps[pa][:, :], in1=gat[:, dt, :], op=ALU.mult), r=[PK[pa], "gatD"], w=["m1D"])
                    T.op("dve", lambda e: e.tensor_tensor(out=m2[:], in0=ps[1][:, :], in1=gat[:, 8 + dt, :], op=ALU.mult), r=[PK[1], "gatD"], w=["m2D"])
                    T.op("pool", lambda e: e.tensor_tensor(out=mT[:, dt, :], in0=m1[:], in1=m2[:], op=ALU.add), r=["m1D", "m2D"], w=["mTD"])
                for ts in range(4):
                    i = tq * 4 + ts
                    u2t = u2s[i % 2]
                    u2k = "u2D%d" % (i % 2)
                    T.dma("sp", lambda e: e.dma_start(out=xin[:], in_=x_d[i * 128:(i + 1) * 128, :]), w=["xinD"])
                    for half in range(2):
                        for dt in range(8):
                            T.op("pe", lambda e: e.matmul(ps[2 + half][:, :], lhsT=mT[:, dt, ts * 128:(ts + 1) * 128],
                                                          rhs=wo[:, dt, half * 512:(half + 1) * 512], start=(dt == 0), stop=(dt == 7)),
                                 r=["mTD", "wo"], w=[PK[2 + half]])
                        T.op("dve", lambda e: e.tensor_tensor(out=x1[:, half * 512:(half + 1) * 512], in0=ps[2 + half][:, :],
                                                              in1=xin[:, half * 512:(half + 1) * 512], op=ALU.add),
                             r=[PK[2 + half], "xinD"], w=["x1D"])
                    if prev_i is not None:
                        d_tail(prev_i)
                    T.dma("sp", lambda e: e.dma_start(out=out_d[i * 128:(i + 1) * 128, :], in_=x1[:]), r=["x1D"], w=["out_d"])
                    T.op("dve", lambda e: e.memset(st[:, 0:1], 0.0), w=["stD"])
                    T.op("act", lambda e: e.activation(out=junkd[:], in_=x1[:], func=AF.Square, accum_out=st[:, 0:1]),
                         r=["x1D", "stD"], w=["junkD", "stD"])
                    T.op("act", lambda e: e.activation(out=st[:, 1:2], in_=st[:, 0:1], func=AF.Sqrt, scale=1.0 / D, bias=eps[:]),
                         r=["stD", "eps"], w=["stD"])
                    T.op("dve", lambda e: e.reciprocal(out=st[:, 2:3], in_=st[:, 1:2]), r=["stD"], w=["stD"])
                    T.op("dve", lambda e: e.scalar_tensor_tensor(out=u2t[:], in0=x1[:], scalar=st[:, 2:3], in1=g2bc[:],
                                                                 op0=ALU.mult, op1=ALU.mult), r=["x1D", "stD", "g2bc"], w=[u2k])
                    T.op("act", lambda e: e.copy(out=u2b[:], in_=u2t[:]), r=[u2k], w=["u2bD"])
                    T.dma("sp", lambda e: e.dma_start(out=u2_d[i * 128:(i + 1) * 128, :], in_=u2b[:]), r=["u2bD"], w=["u2_d"])
                    prev_i = i
            d_tail(prev_i)
            T.barrier()
        if "aff" in dbg_d:
            T.dma("sp", lambda e: e.dma_start(out=dbg_d["aff"], in_=aff_all[:]), r=["aff_all"], w=["dbgaff"])

        if "E" in stages:
          idx_all = sb(es, "idx_all", [128, NE, 4], I32)
          g_all = sb(es, "g_all", [128, NE, 4])
          with ExitStack() as es2:
            affT = sb(es2, "affT", [NE, L])
            cmpj = sb(es2, "cmpj", [NE, L])
            bs = sb(es2, "bsE", [NE, 8])
            tri = sb(es2, "tri", [128, 128])
            onesf = sb(es2, "onesf", [128, 128])
            iota = sb(es2, "iota", [128, 512])
            tidhl = sb(es2, "tidhl", [128, NT, 2], BF16)
            thbc = sb(es2, "thbc", [128, NE])
            dg = sb(es2, "dgE", [NE, NE])
            M = sb(es2, "ME", [128, NT, NE])
            pos = sb(es2, "posE", [128, NT, NE])
            srun = sb(es2, "srunE", [128, NE])
            cum = sb(es2, "cumE", [128, NE])
            vals = sb(es2, "valsE", [128, NT, NE, 4], BF16)
            afh = sb(es2, "afhE", [128, NT, NE], BF16)
            afr = sb(es2, "afrE", [128, NT, NE])
            oh = [sb(es2, "ohE%d" % i, [128, 512], BF16) for i in range(4)]
            idf = sb(es2, "idfE", [128, 4])
            pvs = sb(es2, "pvsE", [128, 16])
            T.dma("sp", lambda e: e.dma_start(out=tri[:], in_=tri_d[:, :]), w=["tri"])
            T.dma("sp", lambda e: e.dma_start(out=iota[:], in_=iota_d[:, :]), w=["iota"])
            T.dma("sp", lambda e: e.dma_start(out=tidhl[:], in_=tidhl_d[:, :, :]), w=["tidhl"])
            T.op("pool", lambda e: e.memset(onesf[:], 1.0), w=["onesf"])
            for i in range(NT):
                pi = i // 4
                T.op("pe", lambda e: e.transpose(out=ps[pi][0:NE, (i % 4) * 128:(i % 4 + 1) * 128], in_=aff_all[:, i, :],
                                                 identity=ident_f[:]), r=["aff_all", "ident_f"], w=[PK[pi]])
                if i % 4 == 3:
                    T.op("act", lambda e: e.copy(out=affT[:, (i - 3) * 128:(i + 1) * 128], in_=ps[pi][0:NE, :]), r=[PK[pi]], w=["affT"])
            T.op("dve", lambda e: e.memset(bs[:, 0:1], 0.0), w=["bsE"])
            T.op("dve", lambda e: e.memset(bs[:, 1:2], 1.0), r=["bsE"], w=["bsE"])
            for it in range(32):
                T.op("dve", lambda e: e.tensor_scalar(out=bs[:, 2:3], in0=bs[:, 0:1], scalar1=bs[:, 1:2], scalar2=0.5, op0=ALU.add, op1=ALU.mult), r=["bsE"], w=["bsE"])
                T.op("dve", lambda e: e.tensor_scalar(out=cmpj[:], in0=affT[:], scalar1=bs[:, 2:3], scalar2=None, op0=ALU.is_ge), r=["affT", "bsE"], w=["cmpj"])
                T.op("dve", lambda e: e.tensor_reduce(out=bs[:, 3:4], in_=cmpj[:], axis=AX.X, op=ALU.add), r=["cmpj", "bsE"], w=["bsE"])
                T.op("dve", lambda e: e.tensor_scalar(out=bs[:, 4:5], in0=bs[:, 3:4], scalar1=CAP - 0.5, scalar2=None, op0=ALU.is_ge), r=["bsE"], w=["bsE"])
                T.op("dve", lambda e: e.tensor_tensor(out=bs[:, 5:6], in0=bs[:, 2:3], in1=bs[:, 0:1], op=ALU.subtract), r=["bsE"], w=["bsE"])
                T.op("dve", lambda e: e.tensor_tensor(out=bs[:, 6:7], in0=bs[:, 1:2], in1=bs[:, 2:3], op=ALU.subtract), r=["bsE"], w=["bsE"])
                T.op("dve", lambda e: e.scalar_tensor_tensor(out=bs[:, 0:1], in0=bs[:, 5:6], scalar=bs[:, 4:5], in1=bs[:, 0:1], op0=ALU.mult, op1=ALU.add), r=["bsE"], w=["bsE"])
                T.op("dve", lambda e: e.scalar_tensor_tensor(out=bs[:, 1:2], in0=bs[:, 6:7], scalar=bs[:, 4:5], in1=bs[:, 2:3], op0=ALU.mult, op1=ALU.add), r=["bsE"], w=["bsE"])
            T.op("dve", lambda e: e.tensor_scalar(out=dg[:], in0=ident_f[0:NE, 0:NE], scalar1=bs[:, 0:1], scalar2=None, op0=ALU.mult), r=["bsE", "ident_f"], w=["dgE"])
            T.op("pe", lambda e: e.matmul(ps[0][:, 0:NE], lhsT=onesf[0:NE, :], rhs=dg[:, :], start=True, stop=True), r=["onesf", "dgE"], w=[PK[0]])
            T.op("dve", lambda e: e.tensor_copy(out=thbc[:], in_=ps[0][:, 0:NE]), r=[PK[0]], w=["thbc"])
            T.op("dve", lambda e: e.tensor_tensor(out=M[:], in0=aff_all[:], in1=thbc[:, :].unsqueeze(1).broadcast_to([128, NT, NE]), op=ALU.is_ge),
                 r=["aff_all", "thbc"], w=["ME"])
            T.op("dve", lambda e: e.memset(srun[:], 0.0), w=["srunE"])
            for i in range(NT):
                pi = 1 + i % 2
                T.op("pe", lambda e: e.matmul(ps[pi][:, 0:NE], lhsT=tri[:, :], rhs=M[:, i, :], start=True, stop=True), r=["tri", "ME"], w=[PK[pi]])
                T.op("pe", lambda e: e.matmul(ps[pi][:, NE:2 * NE], lhsT=onesf[:, :], rhs=M[:, i, :], start=True, stop=True), r=["onesf", "ME"], w=[PK[pi]])
                T.op("dve", lambda e: e.tensor_tensor(out=cum[:], in0=ps[pi][:, 0:NE], in1=srun[:], op=ALU.add), r=[PK[pi], "srunE"], w=["cumE"])
                T.op("dve", lambda e: e.tensor_tensor(out=srun[:], in0=ps[pi][:, NE:2 * NE], in1=srun[:], op=ALU.add), r=[PK[pi], "srunE", "cumE"], w=["srunE"])
                T.op("dve", lambda e: e.tensor_tensor(out=cum[:], in0=cum[:], in1=M[:, i, :], op=ALU.mult), r=["cumE", "ME"], w=["cumE"])
                T.op("dve", lambda e: e.tensor_scalar(out=pos[:, i, :], in0=cum[:], scalar1=-1.0, scalar2=None, op0=ALU.add), r=["cumE"], w=["posE"])
            T.op("dve", lambda e: e.tensor_copy(out=afh[:], in_=aff_all[:]), r=["aff_all"], w=["afhE"])
            T.op("dve", lambda e: e.tensor_tensor(out=afr[:], in0=aff_all[:], in1=afh[:], op=ALU.subtract), r=["aff_all", "afhE"], w=["afrE"])
            T.op("dve", lambda e: e.tensor_copy(out=vals[:, :, :, 2], in_=afh[:]), r=["afhE"], w=["valsE"])
            T.op("dve", lambda e: e.tensor_copy(out=vals[:, :, :, 3], in_=afr[:]), r=["afrE", "valsE"], w=["valsE"])
            T.op("dve", lambda e: e.tensor_copy(out=vals[:, :, :, 0:2], in_=tidhl[:, :, :].unsqueeze(2).broadcast_to([128, NT, NE, 2])),
                 r=["tidhl", "valsE"], w=["valsE"])
            oc = 0
            for ex in range(NE):
                pi = 3 + ex % 2
                for i in range(NT):
                    ob = oc % 4
                    eng = "dve"
                    oc += 1
                    T.op(eng, lambda e: e.tensor_scalar(out=oh[ob][:], in0=iota[:], scalar1=pos[:, i, ex:ex + 1], scalar2=None, op0=ALU.is_equal),
                         r=["iota", "posE"], w=["ohE%d" % ob])
                    for sc in range(4):
                        T.op("pe", lambda e: e.matmul(ps[pi][:, sc * 4:(sc + 1) * 4], lhsT=oh[ob][:, sc * 128:(sc + 1) * 128], rhs=vals[:, i, ex, :],
                                                      start=(i == 0 and sc == 0), stop=(i == NT - 1), skip_group_check=True),
                             r=["ohE%d" % ob, "valsE"], w=[PK[pi]])
                T.op("dve", lambda e: e.tensor_copy(out=pvs[:], in_=ps[pi][:, 0:16]), r=[PK[pi]], w=["pvsE"])
                pv = pvs[:, :].rearrange("p (s f) -> p s f", f=4)
                T.op("dve", lambda e: e.scalar_tensor_tensor(out=idf[:], in0=pv[:, :, 0], scalar=64.0, in1=pv[:, :, 1], op0=ALU.mult, op1=ALU.add),
                     r=["pvsE"], w=["idfE"])
                T.op("dve", lambda e: e.tensor_copy(out=idx_all[:, ex, :], in_=idf[:]), r=["idfE"], w=["idx_all"])
                T.op("dve", lambda e: e.tensor_tensor(out=g_all[:, ex, :], in0=pv[:, :, 2], in1=pv[:, :, 3], op=ALU.add), r=["pvsE"], w=["g_all"])
            T.barrier()
          if "idx" in dbg_d:
            T.dma("sp", lambda e: e.dma_start(out=dbg_d["idx"], in_=idx_all[:]), r=["idx_all"], w=["dbgidx"])
            T.dma("sp", lambda e: e.dma_start(out=dbg_d["g"], in_=g_all[:]), r=["g_all"], w=["dbgg"])

          if "F" in stages:
            wgb = [sb(es, "wgb%d" % i, [128, 8, D], BF16) for i in range(2)]
            wub = [sb(es, "wub%d" % i, [128, 8, D], BF16) for i in range(2)]
            wdb = [sb(es, "wdb%d" % i, [128, 8, D], BF16) for i in range(2)]
            xg = [sb(es, "xgF%d" % i, [128, D], BF16) for i in range(4)]
            xinT = sb(es, "xinT", [128, 8, 512], BF16)
            sg = sb(es, "sgF", [128, 512])
            hT = sb(es, "hTF", [128, 8, 512], BF16)
            eo = [sb(es, "eoF%d" % i, [128, D]) for i in range(2)]
            psb = [ps[6][:, :].bitcast(BF16), ps[7][:, :].bitcast(BF16)]

            def load_w(ex):
                for (wsrc, wdst, wk) in ((wg_d, wgb, "wgb"), (wu_d, wub, "wub"), (wd_d, wdb, "wdb")):
                    dst = wdst[ex % 2]
                    T.dmas("pool", [(lambda e, hh=hh: e.dma_start(out=dst[:, hh * 4:(hh + 1) * 4, :],
                                                                  in_=wsrc[ex].rearrange("(j p) c -> p j c", p=128)[:, hh * 4:(hh + 1) * 4, :]))
                                    for hh in range(2)], w=["%s%d" % (wk, ex % 2)])

            def gather(ex):
                for sc in range(4):
                    T.dma("pool", lambda e: e.indirect_dma_start(out=xg[sc][:], out_offset=None, in_=u2_d[:, :],
                                                                 in_offset=bass.IndirectOffsetOnAxis(ap=idx_all[:, ex, sc:sc + 1], axis=0)),
                          r=["idx_all", "u2_d"], w=["xgF%d" % sc])

            load_w(0)
            gather(0)
            for ex in range(NE):
                wgk, wuk, wdk = "wgb%d" % (ex % 2), "wub%d" % (ex % 2), "wdb%d" % (ex % 2)
                wgt, wut, wdt = wgb[ex % 2], wub[ex % 2], wdb[ex % 2]
                if ex + 1 < NE:
                    load_w(ex + 1)
                for sc in range(4):
                    for j in range(8):
                        T.op("pe", lambda e: e.transpose(out=psb[sc % 2][:, j * 128:(j + 1) * 128], in_=xg[sc][:, j * 128:(j + 1) * 128], identity=ident_b[:]),
                             r=["xgF%d" % sc, "ident_b"], w=[PK[6 + sc % 2]])
                    T.op("act", lambda e: e.copy(out=xinT[:, :, sc * 128:(sc + 1) * 128], in_=psb[sc % 2].rearrange("p (j t) -> p j t", j=8)),
                         r=[PK[6 + sc % 2]], w=["xinT"])
                if ex + 1 < NE:
                    gather(ex + 1)
                for ft in range(8):
                    pg, pu = (ft % 2) * 2, (ft % 2) * 2 + 1
                    for j in range(8):
                        T.op("pe", lambda e: e.matmul(ps[pg][:, :], lhsT=wgt[:, j, ft * 128:(ft + 1) * 128], rhs=xinT[:, j, :], start=(j == 0), stop=(j == 7)),
                             r=[wgk, "xinT"], w=[PK[pg]])
                    for j in range(8):
                        T.op("pe", lambda e: e.matmul(ps[pu][:, :], lhsT=wut[:, j, ft * 128:(ft + 1) * 128], rhs=xinT[:, j, :], start=(j == 0), stop=(j == 7)),
                             r=[wuk, "xinT"], w=[PK[pu]])
                    T.op("act", lambda e: e.activation(out=sg[:], in_=ps[pg][:, :], func=AF.Silu), r=[PK[pg]], w=["sgF"])
                    T.op("dve", lambda e: e.tensor_tensor(out=hT[:, ft, :], in0=ps[pu][:, :], in1=sg[:], op=ALU.mult), r=[PK[pu], "sgF"], w=["hTF"])
                for sc in range(4):
                    eb = eo[sc % 2]
                    for half in range(2):
                        po = 4 + half
                        for ft in range(8):
                            T.op("pe", lambda e: e.matmul(ps[po][:, :], lhsT=hT[:, ft, sc * 128:(sc + 1) * 128], rhs=wdt[:, ft, half * 512:(half + 1) * 512],
                                                          start=(ft == 0), stop=(ft == 7)), r=["hTF", wdk], w=[PK[po]])
                        T.op("dve", lambda e: e.tensor_scalar(out=eb[:, half * 512:(half + 1) * 512], in0=ps[po][:, :], scalar1=g_all[:, ex, sc:sc + 1],
                                                              scalar2=None, op0=ALU.mult), r=[PK[po], "g_all"], w=["eoF%d" % (sc % 2)])
                    T.dma("pool", lambda e: e.indirect_dma_start(out=out_d[:, :], out_offset=bass.IndirectOffsetOnAxis(ap=idx_all[:, ex, sc:sc + 1], axis=0),
                                                                 in_=eb[:], in_offset=None, compute_op=ALU.add),
                          r=["eoF%d" % (sc % 2), "idx_all"], w=["out_d"])
            T.barrier()
    for k, v in dbg_d.items():
        pass
    T.barrier()
    top.close()
    return nc


_CONST = None


def make_inputs(inp, b):
    global _CONST
    if _CONST is None:
        _CONST = host_consts()
    f = lambda a: np.ascontiguousarray(np.asarray(a, dtype=np.float32))
    m = dict(_CONST)
    m["x"] = f(inp["x"][b])
    m["g1bc"] = f(np.broadcast_to(inp["norm1_g"][0][None, :], (128, D)))
    m["w_in"] = f(inp["w_in"][0])
    scw = np.concatenate([inp["short_conv_w"][0], inp["short_conv_b"][0][None, :]], axis=0)
    m["scw"] = f(scw.reshape(4, 12, 128).transpose(2, 1, 0))
    qkg = np.stack([inp["q_norm_g"][0], inp["k_norm_g"][0]], axis=0)
    m["qkg"] = f(np.broadcast_to(qkg[None], (128, 2, 64)))
    m["lamv"] = f(np.stack([inp["lambda_q1"][0], inp["lambda_k1"][0], inp["lambda_q2"][0], inp["lambda_k2"][0]])[None])
    m["subg"] = f(np.broadcast_to(inp["subln_g"][0][None, :], (128, 128)))
    m["fw1"] = f(inp["filt_w1"][0])
    m["fw2"] = f(inp["filt_w2"][0])
    m["fw3"] = f(inp["filt_w3"][0])
    m["fbf"] = f(np.stack([inp["filt_b1"][0], inp["filt_b2"][0], inp["filt_b3"][0], inp["filt_freq"][0]], axis=1))
    m["fwo"] = f(inp["filt_w_out"][0])
    m["skipbc"] = f(np.broadcast_to(inp["hyena_skip"][0][None], (32, 2, 512)))
    m["wpa"] = f(inp["w_branch_attn"][0])
    m["wph"] = f(inp["w_branch_hyena"][0])
    m["wo"] = f(inp["w_out"][0])
    m["g2bc"] = f(np.broadcast_to(inp["norm2_g"][0][None, :], (128, D)))
    m["wr"] = f(inp["w_router"][0].reshape(8, 128, 16).transpose(1, 0, 2))
    m["wg"] = f(inp["w_gate"][0])
    m["wu"] = f(inp["w_up"][0])
    m["wd"] = f(inp["w_down"][0])
    return m


def kernel(**inputs):
    nc = build()
    maps = [make_inputs(inputs, c % 4) for c in range(4)]
    in_maps = [maps[c % 4] for c in range(NCORES)]
    res = run_bass_kernel_spmd(nc, in_maps, core_ids=list(range(NCORES)))
    out = np.stack([np.asarray(res.results[b]["out"], dtype=np.float32) for b in range(4)], axis=0)
    return out
```

```python
import math
from contextlib import ExitStack
import numpy as np
import ml_dtypes
import concourse.bass as bass
import concourse.mybir as mybir
from concourse.bass_utils import run_bass_kernel_spmd

F32 = mybir.dt.float32
BF16 = mybir.dt.bfloat16
I32 = mybir.dt.int32
AF = mybir.ActivationFunctionType
ALU = mybir.AluOpType
AX = mybir.AxisListType

L = 4096
D = 1024
NT = 32
NCORES = 8
CAP = 512
NE = 16
GC = 32
NK = 33
DEBUG = False


class Trk:
    def __init__(s, nc):
        s.nc = nc
        s.eng = dict(pe=nc.tensor, act=nc.scalar, dve=nc.vector, pool=nc.gpsimd, sp=nc.sync)
        s.sem = {}
        s.cnt = {}
        for k in ("pe", "act", "dve", "pool"):
            s.sem[k] = nc.alloc_semaphore(name="s_" + k)
            s.cnt[k] = 0
        s.ND = 16
        for i in range(s.ND):
            k = "d%d" % i
            s.sem[k] = nc.alloc_semaphore(name="s_" + k)
            s.cnt[k] = 0
        s.pool_of = {"sp": list(range(0, 8)), "pool": list(range(8, 16))}
        s.rrq = {"sp": 0, "pool": 0}
        s.seen = {e: {} for e in s.eng}
        s.lw = {}
        s.rd = {}

    def _wait(s, e, deps):
        for k, v in deps.items():
            if e == "pe" and k == "pe":
                continue
            if s.seen[e].get(k, 0) < v:
                s.eng[e].wait_ge(s.sem[k], v)
                s.seen[e][k] = v

    def _deps(s, r, w):
        d = {}

        def add(k, v):
            if d.get(k, 0) < v:
                d[k] = v
        for x in r:
            for k, v in s.lw.get(x, {}).items():
                add(k, v)
        for x in w:
            for k, v in s.lw.get(x, {}).items():
                add(k, v)
            for k, v in s.rd.get(x, {}).items():
                add(k, v)
        return d

    def _upd(s, toks, r, w):
        if isinstance(toks, tuple):
            toks = [toks]
        for x in r:
            m = s.rd.setdefault(x, {})
            for tok in toks:
                if m.get(tok[0], 0) < tok[1]:
                    m[tok[0]] = tok[1]
        for x in w:
            m = {}
            for tok in toks:
                if m.get(tok[0], 0) < tok[1]:
                    m[tok[0]] = tok[1]
            s.lw[x] = m
            s.rd[x] = {}

    def op(s, e, fn, r=(), w=()):
        s._wait(e, s._deps(r, w))
        ins = fn(s.eng[e])
        s.cnt[e] += 1
        ins.then_inc(s.sem[e], 1)
        s._upd((e, s.cnt[e]), r, w)

    def _next_sem(s, q):
        pool = s.pool_of[q]
        k = "d%d" % pool[s.rrq[q] % len(pool)]
        s.rrq[q] += 1
        if s.cnt[k] > 0:
            s._wait(q, {k: s.cnt[k]})
        return k

    def dma(s, q, fn, r=(), w=()):
        s._wait(q, s._deps(r, w))
        k = s._next_sem(q)
        ins = fn(s.eng[q])
        s.cnt[k] += 16
        ins.then_inc(s.sem[k], 16)
        s._upd((k, s.cnt[k]), r, w)

    def dmas(s, q, fns, r=(), w=()):
        s._wait(q, s._deps(r, w))
        toks = []
        for fn in fns:
            k = s._next_sem(q)
            ins = fn(s.eng[q])
            s.cnt[k] += 16
            ins.then_inc(s.sem[k], 16)
            toks.append((k, s.cnt[k]))
        s._upd(toks, r, w)

    def barrier(s, engines=None):
        tot = {k: v for k, v in s.cnt.items() if v > 0}
        for e in (engines or s.eng):
            s._wait(e, dict(tot))
        s.lw = {}
        s.rd = {}


def _bf(a):
    return np.ascontiguousarray(a.astype(np.float32)).astype(ml_dtypes.bfloat16)


def host_consts():
    c = {}
    t = np.arange(L, dtype=np.float32)
    inv_freq = (np.float32(500000.0) ** (-np.arange(0, 16, 2, dtype=np.float32) / np.float32(16))).astype(np.float32)
    ang = (t[:, None] * inv_freq[None, :]).astype(np.float32)
    cs = np.concatenate([np.cos(ang), np.sin(ang)], axis=-1).astype(np.float32)
    c["ropecs"] = np.ascontiguousarray(cs.reshape(NT, 128, 16).transpose(1, 0, 2))
    tt = t / np.float32(L - 1)
    bands = np.linspace(1e-4, 15, 16, dtype=np.float32)
    a2 = (np.float32(2.0 * math.pi / L) * t[:, None] * bands[None, :]).astype(np.float32)
    emb = np.concatenate([tt[:, None], np.cos(a2), -np.sin(a2)], axis=-1).astype(np.float32)
    c["embT"] = np.ascontiguousarray(emb.T)
    c["posrow"] = np.ascontiguousarray(np.broadcast_to(tt[None, :], (128, L))).astype(np.float32)
    min_decay = math.log(1e-2) / 1.5
    max_decay = math.log(1e-2) / 0.3
    deltas = np.abs(np.linspace(min_decay, max_decay, 512, dtype=np.float32))
    c["ndelta"] = np.ascontiguousarray((-deltas).reshape(4, 128).T).astype(np.float32)
    n1 = np.arange(32)[:, None]
    k1 = np.arange(NK)[None, :]
    th = 2 * np.pi * n1 * k1 / 64.0
    c["FA"] = _bf(np.concatenate([np.cos(th), -np.sin(th)], axis=1))
    n2 = np.arange(128)[:, None, None]
    k1 = np.arange(NK)[None, :, None]
    k2 = np.arange(128)[None, None, :]
    th = 2 * np.pi * ((n2 * (k1 + 64 * k2)) % 8192) / 8192.0
    c["TGr"] = _bf(np.cos(th))
    c["TGi"] = _bf(-np.sin(th))
    c["TGn"] = _bf(np.sin(th))
    kk2 = np.arange(128)[:, None]
    nn2 = np.arange(128)[None, :]
    th = 2 * np.pi * ((kk2 * nn2) % 128) / 128.0
    fr, fi = np.cos(th), np.sin(th)
    c["R1"] = _bf(np.concatenate([fr, fi], axis=1))
    c["R2"] = _bf(np.concatenate([-fi, fr], axis=1))
    k1 = np.arange(NK)[:, None, None]
    n2 = np.arange(128)[None, :, None]
    n1 = np.arange(32)[None, None, :]
    th = 2 * np.pi * ((k1 * (n2 + 128 * n1)) % 8192) / 8192.0
    coef = np.where((k1 == 0) | (k1 == 32), 1.0, 2.0)[:, :, None, :]
    th_ = np.stack([np.cos(th), -np.sin(th)], axis=2) * coef / 8192.0
    c["TH"] = _bf(th_)
    tid = np.arange(L).reshape(NT, 128).T
    c["tidhl"] = _bf(np.stack([tid // 64, tid % 64], axis=-1))
    c["iota512"] = np.ascontiguousarray(np.broadcast_to(np.arange(512, dtype=np.float32)[None, :], (128, 512)))
    tri = (np.arange(128)[:, None] <= np.arange(128)[None, :]).astype(np.float32)
    c["tri"] = tri
    return c


def build(dbg=None):
    nc = bass.Bass("TRN2", target_bir_lowering=False)
    dbg = dbg or {}

    def din(name, shape, dt=F32):
        return nc.dram_tensor(name, list(shape), dt, kind="ExternalInput").ap()

    def dscr(name, shape, dt=F32):
        kind = "ExternalOutput" if name in dbg.get("_ext", ()) else "Internal"
        return nc.dram_tensor(name, list(shape), dt, kind=kind).ap()

    x_d = din("x", [L, D])
    g1bc_d = din("g1bc", [128, D])
    w_in_d = din("w_in", [D, 5120])
    scw_d = din("scw", [128, 12, 4])
    qkg_d = din("qkg", [128, 2, 64])
    lam_d = din("lamv", [1, 4, 64])
    subg_d = din("subg", [128, 128])
    fw1_d = din("fw1", [33, 64])
    fw2_d = din("fw2", [64, 64])
    fw3_d = din("fw3", [64, 64])
    fbf_d = din("fbf", [64, 4])
    fwo_d = din("fwo", [64, 2048])
    skip_d = din("skipbc", [32, 2, 512])
    wpa_d = din("wpa", [512, D])
    wph_d = din("wph", [512, D])
    wo_d = din("wo", [D, D])
    g2bc_d = din("g2bc", [128, D])
    wr_d = din("wr", [128, 8, 16])
    wg_d = din("wg", [NE, D, D])
    wu_d = din("wu", [NE, D, D])
    wd_d = din("wd", [NE, D, D])
    ropecs_d = din("ropecs", [128, NT, 16])
    embT_d = din("embT", [33, L])
    posrow_d = din("posrow", [128, L])
    ndelta_d = din("ndelta", [128, 4])
    FA_d = din("FA", [32, 2 * NK], BF16)
    TGr_d = din("TGr", [128, NK, 128], BF16)
    TGi_d = din("TGi", [128, NK, 128], BF16)
    TGn_d = din("TGn", [128, NK, 128], BF16)
    R1_d = din("R1", [128, 256], BF16)
    R2_d = din("R2", [128, 256], BF16)
    TH_d = din("TH", [NK, 128, 2, 32], BF16)
    tidhl_d = din("tidhl", [128, NT, 2], BF16)
    iota_d = din("iota512", [128, 512])
    tri_d = din("tri", [128, 128])

    out_d = nc.dram_tensor("out", [L, D], F32, kind="ExternalOutput").ap()

    qT_d = dscr("qT_s", [4, 128, L], BF16)
    kT_d = dscr("kT_s", [4, 128, L], BF16)
    v_d = dscr("v_s", [L, 512], BF16)
    hyc_d = dscr("hyc_s", [1536, L], F32)
    gateT_d = dscr("gateT_s", [2048, L], BF16)
    kern_d = dscr("kern_s", [2, 2, 512, L], BF16)
    attnT_d = dscr("attnT_s", [512, L], BF16)
    hyoT_d = dscr("hyoT_s", [512, L], BF16)
    u2_d = dscr("u2_s", [L, D], BF16)

    dbg_d = {}
    for k, (shape, dt) in ((k, v) for k, v in dbg.items() if not k.startswith("_")):
        dbg_d[k] = nc.dram_tensor("dbg_" + k, list(shape), dt, kind="ExternalOutput").ap()

    T = Trk(nc)
    top = ExitStack()

    sbc = [0]

    def sb(es, name, shape, dt=F32):
        sbc[0] += 1
        return es.enter_context(nc.sbuf_tensor("sb%d_%s" % (sbc[0], name), list(shape), dt))

    ps = [top.enter_context(nc.psum_tensor("ps%d" % i, [128, 512], F32)) for i in range(8)]
    PK = ["ps%d" % i for i in range(8)]

    ident_f = sb(top, "ident_f", [128, 128])
    ident_b = sb(top, "ident_b", [128, 128], BF16)
    eps = sb(top, "eps", [128, 1])
    T.op("pool", lambda e: e.memset(ident_f[:], 0.0), w=["ident_f"])
    T.op("pool", lambda e: e.affine_select(out=ident_f[:], in_=ident_f[:], pattern=[[-1, 128]],
                                           compare_op=ALU.not_equal, fill=1.0, base=0, channel_multiplier=1),
         r=["ident_f"], w=["ident_f"])
    T.op("pool", lambda e: e.tensor_copy(out=ident_b[:], in_=ident_f[:]), r=["ident_f"], w=["ident_b"])
    T.op("pool", lambda e: e.memset(eps[:], 1e-6), w=["eps"])

    stages = dbg.get("_stages", "AKBCDEF") if isinstance(dbg.get("_stages", None), str) else "AKBCDEF"

    if "A" in stages:
      with ExitStack() as es:
        uT = sb(es, "uT", [128, 8, L + 2], BF16)
        g1bc = sb(es, "g1bc", [128, D])
        scw = sb(es, "scw", [128, 12, 4])
        qkg = sb(es, "qkg", [128, 2, 64])
        ropecs = sb(es, "ropecs", [128, NT, 16])
        xt = [sb(es, "xt%d" % i, [128, D]) for i in range(2)]
        xb = [sb(es, "xb%d" % i, [128, D], BF16) for i in range(2)]
        junk = sb(es, "junkA", [128, D])
        ss = sb(es, "ssA", [128, NT])
        rs = sb(es, "rsA", [128, NT])
        wb = [sb(es, "wb%d" % i, [128, 8, 512], BF16) for i in range(2)]
        T.dma("sp", lambda e: e.dma_start(out=g1bc[:], in_=g1bc_d[:, :]), w=["g1bc"])
        T.dma("sp", lambda e: e.dma_start(out=scw[:], in_=scw_d[:, :, :]), w=["scw"])
        T.dma("sp", lambda e: e.dma_start(out=qkg[:], in_=qkg_d[:, :, :]), w=["qkg"])
        T.dma("sp", lambda e: e.dma_start(out=ropecs[:], in_=ropecs_d[:, :, :]), w=["ropecs"])
        T.op("dve", lambda e: e.memset(ss[:], 0.0), w=["ssA"])
        T.op("dve", lambda e: e.memset(uT[:, :, 0:1], 0.0), w=["uTpad0"])
        T.op("dve", lambda e: e.memset(uT[:, :, L + 1:L + 2], 0.0), w=["uTpad1"])
        T.op("dve", lambda e: e.tensor_scalar(out=qkg[:, 0, :], in0=qkg[:, 0, :], scalar1=0.125, scalar2=None,
                                              op0=ALU.mult), r=["qkg"], w=["qkg"])
        psb = [ps[6][:, :].bitcast(BF16), ps[7][:, :].bitcast(BF16)]
        for i in range(NT):
            b = i % 2
            T.dma("sp", lambda e: e.dma_start(out=xt[b][:], in_=x_d[i * 128:(i + 1) * 128, :]), w=["xt%d" % b])
            T.op("act", lambda e: e.activation(out=junk[:], in_=xt[b][:], func=AF.Square,
                                               accum_out=ss[:, i:i + 1]), r=["xt%d" % b, "ssA"], w=["junkA", "ss%d" % i])
            T.op("act", lambda e: e.activation(out=rs[:, i:i + 1], in_=ss[:, i:i + 1], func=AF.Sqrt,
                                               scale=1.0 / D, bias=eps[:]), r=["ss%d" % i, "eps"], w=["rs%d" % i])
            T.op("dve", lambda e: e.reciprocal(out=rs[:, i:i + 1], in_=rs[:, i:i + 1]), r=["rs%d" % i], w=["rs%d" % i])
            T.op("dve", lambda e: e.scalar_tensor_tensor(out=xb[b][:], in0=xt[b][:], scalar=rs[:, i:i + 1], in1=g1bc[:],
                                                         op0=ALU.mult, op1=ALU.mult),
                 r=["xt%d" % b, "rs%d" % i, "g1bc"], w=["xb%d" % b])
            for j in range(8):
                T.op("pe", lambda e: e.transpose(out=psb[b][:, j * 128:(j + 1) * 128],
                                                 in_=xb[b][:, j * 128:(j + 1) * 128], identity=ident_b[:]),
                     r=["xb%d" % b, "ident_b"], w=[PK[6 + b]])
            T.op("act", lambda e: e.copy(out=uT[:, :, 1 + i * 128:1 + (i + 1) * 128],
                                         in_=psb[b].rearrange("p (j t) -> p j t", j=8)),
                 r=[PK[6 + b]], w=["uT%d" % i])
        UTK = ["uT%d" % i for i in range(NT)] + ["uTpad0", "uTpad1"]

        sq = sb(es, "sqA", [128, 512])
        ssq = sb(es, "ssqA", [128, 8])
        qn = sb(es, "qnA", [128, 8, 64])
        qb2 = [sb(es, "qbA%d" % i, [128, 512], BF16) for i in range(2)]
        rt = [sb(es, "rtA%d" % i, [128, 8, 8]) for i in range(4)]
        qTs = [sb(es, "qTs%d" % i, [128, 4, 128], BF16) for i in range(2)]
        vst = [sb(es, "vst%d" % i, [128, 512], BF16) for i in range(2)]
        hraw = sb(es, "hraw", [128, L + 2])
        hc = sb(es, "hcA", [128, L])
        gst = sb(es, "gstA", [128, L], BF16)
        T.op("pool", lambda e: e.memset(hraw[:, 0:1], 0.0), w=["hrawp0"])
        T.op("pool", lambda e: e.memset(hraw[:, L + 1:L + 2], 0.0), w=["hrawp1"])
        pcnt = [0]

        def nextps():
            pcnt[0] += 1
            return pcnt[0] % 4

        for cc in dbg.get('_ccs', range(10)):
            wbk = "wb%d" % (cc % 2)
            wbt = wb[cc % 2]
            T.dmas("pool", [(lambda e, a=a: e.dma_start(
                out=wbt[:, a:a + 4, :], in_=w_in_d.rearrange("(j p) c -> p j c", p=128)[:, a:a + 4, cc * 512:(cc + 1) * 512]))
                for a in (0, 4)], w=[wbk])
            if cc < 3:
                def qk_tail(i):
                    qbt = qb2[i % 2]
                    pb = 6 + (i % 2)
                    for h in range(4):
                        T.op("pe", lambda e: e.transpose(out=psb[i % 2][:, h * 128:(h + 1) * 128],
                                                         in_=qbt[:, h * 128:(h + 1) * 128], identity=ident_b[:]),
                             r=["qbA%d" % (i % 2), "ident_b"], w=[PK[pb]])
                    st = qTs[i % 2]
                    T.op("act", lambda e: e.copy(out=st[:], in_=psb[i % 2][:, 0:512].rearrange("p (h t) -> p h t", h=4)),
                         r=[PK[pb]], w=["qTs%d" % (i % 2)])
                    dst = (qT_d if cc == 0 else kT_d).rearrange("h p t -> p h t")[:, :, i * 128:(i + 1) * 128]
                    T.dma("sp", lambda e: e.dma_start(out=dst, in_=st[:]), r=["qTs%d" % (i % 2)], w=["qkT_d"])

                for i in range(NT):
                    pi = nextps()
                    for j in range(8):
                        T.op("pe", lambda e: e.matmul(ps[pi][:, :], lhsT=uT[:, j, 1 + i * 128:1 + (i + 1) * 128],
                                                      rhs=wbt[:, j, :], start=(j == 0), stop=(j == 7)),
                             r=["uT%d" % i, wbk], w=[PK[pi]])
                    if cc == 2:
                        vs = vst[i % 2]
                        T.op("act", lambda e: e.copy(out=vs[:], in_=ps[pi][:, :]), r=[PK[pi]], w=["vst%d" % (i % 2)])
                        T.dma("sp", lambda e: e.dma_start(out=v_d[i * 128:(i + 1) * 128, :], in_=vs[:]),
                              r=["vst%d" % (i % 2)], w=["v_d"])
                        continue
                    if i > 0:
                        qk_tail(i - 1)
                    qb = qb2[i % 2]
                    qbk = "qbA%d" % (i % 2)
                    T.op("act", lambda e: e.activation(out=sq[:], in_=ps[pi][:, :], func=AF.Square), r=[PK[pi]], w=["sqA"])
                    T.op("dve", lambda e: e.tensor_reduce(out=ssq[:], in_=sq[:].rearrange("p (g d) -> p g d", d=64),
                                                          axis=AX.X, op=ALU.add), r=["sqA"], w=["ssqA"])
                    T.op("act", lambda e: e.activation(out=ssq[:], in_=ssq[:], func=AF.Sqrt, scale=1.0 / 64,
                                                       bias=eps[:]), r=["ssqA", "eps"], w=["ssqA"])
                    T.op("dve", lambda e: e.reciprocal(out=ssq[:], in_=ssq[:]), r=["ssqA"], w=["ssqA"])
                    T.op("dve", lambda e: e.tensor_tensor(out=qn[:], in0=ps[pi][:, :].rearrange("p (g d) -> p g d", d=64),
                                                          in1=ssq[:, :].unsqueeze(2).broadcast_to([128, 8, 64]),
                                                          op=ALU.mult), r=[PK[pi], "ssqA"], w=["qnA"])
                    T.op("dve", lambda e: e.tensor_tensor(out=qn[:], in0=qn[:],
                                                          in1=qkg[:, cc, :].unsqueeze(1).broadcast_to([128, 8, 64]),
                                                          op=ALU.mult), r=["qnA", "qkg"], w=["qnA"])
                    T.op("act", lambda e: e.copy(out=qb[:].rearrange("p (g d) -> p g d", d=64), in_=qn[:]),
                         r=["qnA"], w=[qbk])
                    cosb = ropecs[:, i, 0:8].unsqueeze(1).broadcast_to([128, 8, 8])
                    sinb = ropecs[:, i, 8:16].unsqueeze(1).broadcast_to([128, 8, 8])
                    r1 = qn[:, :, 0:8]
                    r2 = qn[:, :, 8:16]
                    T.op("dve", lambda e: e.tensor_tensor(out=rt[0][:], in0=r1, in1=cosb, op=ALU.mult), r=["qnA", "ropecs"], w=["rt0"])
                    T.op("dve", lambda e: e.tensor_tensor(out=rt[1][:], in0=r2, in1=sinb, op=ALU.mult), r=["qnA", "ropecs"], w=["rt1"])
                    T.op("dve", lambda e: e.tensor_tensor(out=rt[2][:], in0=r2, in1=cosb, op=ALU.mult), r=["qnA", "ropecs"], w=["rt2"])
                    T.op("dve", lambda e: e.tensor_tensor(out=rt[3][:], in0=r1, in1=sinb, op=ALU.mult), r=["qnA", "ropecs"], w=["rt3"])
                    qbv = qb[:].rearrange("p (g d) -> p g d", d=64)
                    T.op("dve", lambda e: e.tensor_tensor(out=qbv[:, :, 0:8], in0=rt[0][:], in1=rt[1][:], op=ALU.subtract),
                         r=["rt0", "rt1"], w=[qbk])
                    T.op("dve", lambda e: e.tensor_tensor(out=qbv[:, :, 8:16], in0=rt[2][:], in1=rt[3][:], op=ALU.add),
                         r=["rt2", "rt3"], w=[qbk])
                if cc < 2:
                    qk_tail(NT - 1)
            else:
                for ct in range(4):
                    for tq in range(8):
                        pi = nextps()
                        for j in range(8):
                            T.op("pe", lambda e: e.matmul(ps[pi][:, :], lhsT=wbt[:, j, ct * 128:(ct + 1) * 128],
                                                          rhs=uT[:, j, 1 + tq * 512:1 + (tq + 1) * 512],
                                                          start=(j == 0), stop=(j == 7)),
                                 r=UTK[4 * tq:4 * tq + 4] + [wbk], w=[PK[pi]])
                        if cc < 6:
                            T.op("act", lambda e: e.copy(out=hraw[:, 1 + tq * 512:1 + (tq + 1) * 512], in_=ps[pi][:, :]),
                                 r=[PK[pi]], w=["hraw"])
                        else:
                            T.op("act", lambda e: e.activation(out=gst[:, tq * 512:(tq + 1) * 512], in_=ps[pi][:, :],
                                                               func=AF.Sigmoid), r=[PK[pi]], w=["gstA"])
                    if cc < 6:
                        cti = (cc - 3) * 4 + ct
                        lvl = dbg.get('_lvl', 9)
                        if lvl >= 1:
                          T.op("act", lambda e: e.activation(out=hc[:], in_=hraw[:, 1:L + 1], func=AF.Identity,
                                                           scale=scw[:, cti, 1:2], bias=scw[:, cti, 3:4]),
                             r=["hraw", "scw"], w=["hcA"])
                        if lvl >= 2:
                          T.op("dve", lambda e: e.scalar_tensor_tensor(out=hc[:], in0=hraw[:, 0:L], scalar=scw[:, cti, 0:1],
                                                                     in1=hc[:], op0=ALU.mult, op1=ALU.add),
                             r=["hraw", "hrawp0", "scw", "hcA"], w=["hcA"])
                          T.op("dve", lambda e: e.scalar_tensor_tensor(out=hc[:], in0=hraw[:, 2:L + 2], scalar=scw[:, cti, 2:3],
                                                                     in1=hc[:], op0=ALU.mult, op1=ALU.add),
                             r=["hraw", "hrawp1", "scw", "hcA"], w=["hcA"])
                        if lvl >= 3:
                          for q4 in range(4):
                              T.dma("sp", lambda e: e.dma_start(out=hyc_d[cti * 128:(cti + 1) * 128, q4 * 1024:(q4 + 1) * 1024],
                                                                in_=hc[:, q4 * 1024:(q4 + 1) * 1024]),
                                    r=["hcA"], w=["hyc_d%d" % q4])
                    else:
                        gti = (cc - 6) * 4 + ct
                        T.dmas("sp", [(lambda e, a=a: e.dma_start(out=gateT_d[gti * 128:(gti + 1) * 128, a:a + 2048], in_=gst[:, a:a + 2048]))
                                      for a in (0, 2048)], r=["gstA"], w=["gateT_d"])
        T.barrier()

    if "K" in stages:
      with ExitStack() as es:
        embT = sb(es, "embT", [33, L])
        fw1 = sb(es, "fw1", [33, 64])
        fw2 = sb(es, "fw2", [64, 64])
        fw3 = sb(es, "fw3", [64, 64])
        fbf = sb(es, "fbf", [64, 4])
        frb = sb(es, "frb", [64, 3])
        fwo = sb(es, "fwo", [64, 2048])
        posrow = sb(es, "posrow", [128, L])
        ndelta = sb(es, "ndelta", [128, 4])
        hid = [sb(es, "hid%d" % i, [64, L]) for i in range(2)]
        hidb = sb(es, "hidb", [64, L])
        dec = sb(es, "decK", [128, L])
        kf = sb(es, "kfK", [128, L])
        kb = sb(es, "kbK", [128, L])
        hp = sb(es, "hpK", [128, L], BF16)
        hm = sb(es, "hmK", [128, L], BF16)
        for tns, src, k in ((fw1, fw1_d, "fw1"), (fw2, fw2_d, "fw2"), (fw3, fw3_d, "fw3"),
                            (fbf, fbf_d, "fbf"), (ndelta, ndelta_d, "ndelta")):
            T.dma("sp", lambda e: e.dma_start(out=tns[:], in_=src), w=[k])
        for tns, src, k, n in ((embT, embT_d, "embT", L), (fwo, fwo_d, "fwo", 2048), (posrow, posrow_d, "posrow", L)):
            T.dmas("sp", [(lambda e, a=a: e.dma_start(out=tns[:, a:a + 1024], in_=src[:, a:a + 1024]))
                          for a in range(0, n, 1024)], w=[k])
        for l in range(3):
            T.op("dve", lambda e: e.tensor_tensor(out=frb[:, l:l + 1], in0=fbf[:, l:l + 1], in1=fbf[:, 3:4], op=ALU.mult),
                 r=["fbf"], w=["frb"])
        TWO_PI = 2.0 * math.pi
        srcs = [(embT, "embT", 33, fw1, "fw1"), (hid[0], "hid0", 64, fw2, "fw2"), (hid[1], "hid1", 64, fw3, "fw3")]
        outs = [(hid[0], "hid0"), (hid[1], "hid1"), (hid[0], "hid0")]
        negpi = sb(es, "negpi", [64, 1])
        T.op("dve", lambda e: e.memset(negpi[:], 4.0 * math.pi), w=["negpi"])
        kcnt = sb(es, "kcntK", [64, L])
        for l in range(3):
            src, sk, kk, w_, wk = srcs[l]
            dst, dk = outs[l]
            for tq in range(8):
                pi = tq % 4
                T.op("pe", lambda e: e.matmul(ps[pi][0:64, :], lhsT=w_[0:kk, :], rhs=src[0:kk, tq * 512:(tq + 1) * 512],
                                              start=True, stop=True), r=[sk, wk], w=[PK[pi]])
                T.op("dve", lambda e: e.tensor_scalar(out=hidb[:, tq * 512:(tq + 1) * 512], in0=ps[pi][0:64, :],
                                                      scalar1=fbf[:, 3:4], scalar2=frb[:, l:l + 1], op0=ALU.mult,
                                                      op1=ALU.add), r=[PK[pi], "fbf", "frb"], w=["hidb"])
                xs_ = hidb[:, tq * 512:(tq + 1) * 512]
                ks_ = kcnt[:, tq * 512:(tq + 1) * 512]
                T.op("dve", lambda e: e.tensor_scalar(out=ks_, in0=xs_, scalar1=-3.0 * math.pi, scalar2=None, op0=ALU.is_gt),
                     r=["hidb"], w=["kcnt"])
                for thr in (-math.pi, math.pi, 3.0 * math.pi):
                    T.op("dve", lambda e: e.scalar_tensor_tensor(out=ks_, in0=xs_, scalar=thr, in1=ks_, op0=ALU.is_gt, op1=ALU.add),
                         r=["hidb", "kcnt"], w=["kcnt"])
                T.op("dve", lambda e: e.scalar_tensor_tensor(out=xs_, in0=ks_, scalar=-TWO_PI, in1=xs_, op0=ALU.mult, op1=ALU.add),
                     r=["hidb", "kcnt"], w=["hidb"])
                T.op("act", lambda e: e.activation(out=dst[:, tq * 512:(tq + 1) * 512], in_=xs_,
                                                   func=AF.Sin, bias=negpi[:]), r=["hidb", "negpi"], w=[dk])
        h3 = hid[0]
        for ct in range(4):
            T.op("act", lambda e: e.activation(out=dec[:], in_=posrow[:], func=AF.Exp, scale=ndelta[:, ct:ct + 1]),
                 r=["posrow", "ndelta"], w=["decK"])
            for o in range(2):
                for d_, (dst, dk) in enumerate(((kf, "kfK"), (kb, "kbK"))):
                    col = (o * 2 + d_) * 512 + ct * 128
                    for tq in range(8):
                        pi = tq % 4
                        T.op("pe", lambda e: e.matmul(ps[pi][:, :], lhsT=fwo[:, col:col + 128],
                                                      rhs=h3[:, tq * 512:(tq + 1) * 512], start=True, stop=True),
                             r=["hid0", "fwo"], w=[PK[pi]])
                        T.op("dve", lambda e: e.tensor_tensor(out=dst[:, tq * 512:(tq + 1) * 512], in0=ps[pi][:, :],
                                                              in1=dec[:, tq * 512:(tq + 1) * 512], op=ALU.mult),
                             r=[PK[pi], "decK"], w=[dk])
                T.op("dve", lambda e: e.memset(kb[:, 0:1], 0.0), r=["kbK"], w=["kbK"])
                T.op("pool", lambda e: e.tensor_tensor(out=hp[:], in0=kf[:], in1=kb[:], op=ALU.add), r=["kfK", "kbK"], w=["hpK"])
                T.op("pool", lambda e: e.tensor_tensor(out=hm[:], in0=kf[:], in1=kb[:], op=ALU.subtract), r=["kfK", "kbK"], w=["hmK"])
                T.dmas("sp", [(lambda e, a=a: e.dma_start(out=kern_d[o, 0, ct * 128:(ct + 1) * 128, a:a + 2048], in_=hp[:, a:a + 2048]))
                              for a in range(0, L, 2048)], r=["hpK"], w=["kern_d0"])
                T.dmas("sp", [(lambda e, a=a: e.dma_start(out=kern_d[o, 1, ct * 128:(ct + 1) * 128, a:a + 2048], in_=hm[:, a:a + 2048]))
                              for a in range(0, L, 2048)], r=["hmK"], w=["kern_d1"])
        T.barrier()

    if "B" in stages:
      with ExitStack() as es:
        qT = sb(es, "qT", [128, 4, L], BF16)
        kT = sb(es, "kT", [128, 2, 4, L], BF16)
        V = sb(es, "Vsb", [128, NT, 4, 132], BF16)
        lamv = sb(es, "lamv", [1, 4, 64])
        lamt = sb(es, "lamt", [1, 8])
        ones1 = sb(es, "ones1", [1, 128])
        nlam = sb(es, "nlam", [128, 1])
        subg = sb(es, "subg", [128, 128])
        E = [sb(es, "E%d" % i, [128, 512], BF16) for i in range(3)]
        osb = sb(es, "osbB", [128, 2, 4, 129])
        aacc = sb(es, "aacc", [128, 4, 128])
        ab = sb(es, "abB", [128, 4, 128], BF16)
        junkb = sb(es, "junkB", [128, 4, 128])
        sm = sb(es, "smB", [128, 8, 4])
        aTs = [sb(es, "aTs%d" % i, [128, 512], BF16) for i in range(2)]
        T.dmas("sp", [(lambda e, h=h, a=a: e.dma_start(out=qT[:, h, a:a + 2048], in_=qT_d[h, :, a:a + 2048]))
                      for h in range(4) for a in (0, 2048)], w=["qT"])
        T.op("pool", lambda e: e.memset(kT[64:128, 0, :, :], 0.0), w=["kTz0"])
        T.op("dve", lambda e: e.memset(kT[0:64, 1, :, :], 0.0), w=["kTz1"])
        T.dmas("sp", [(lambda e, h=h, a=a, m=m: e.dma_start(out=kT[m * 64:(m + 1) * 64, m, h, a:a + 2048],
                                                            in_=kT_d[h, m * 64:(m + 1) * 64, a:a + 2048]))
                      for m in range(2) for h in range(4) for a in (0, 2048)], w=["kT"])
        T.dmas("sp", [(lambda e, i=i: e.dma_start(out=V[:, i, :, 0:128],
                                                  in_=v_d[i * 128:(i + 1) * 128, :].rearrange("p (h d) -> p h d", d=128)))
                      for i in range(NT)], w=["V"])
        T.op("pool", lambda e: e.memset(V[:, :, :, 128:129], 1.0), w=["Vones"])
        T.dma("sp", lambda e: e.dma_start(out=lamv[:], in_=lam_d[:, :, :]), w=["lamv"])
        T.dma("sp", lambda e: e.dma_start(out=subg[:], in_=subg_d[:, :]), w=["subg"])
        T.op("dve", lambda e: e.tensor_scalar(out=subg[:], in0=subg[:], scalar1=0.8, scalar2=None, op0=ALU.mult),
             r=["subg"], w=["subg"])
        T.op("dve", lambda e: e.memset(ones1[:], 1.0), w=["ones1"])
        T.op("dve", lambda e: e.memset(lamt[:], 0.0), w=["lamt"])
        T.op("dve", lambda e: e.tensor_tensor(out=lamv[:, 0, :], in0=lamv[:, 0, :], in1=lamv[:, 1, :], op=ALU.mult), r=["lamv"], w=["lamv"])
        T.op("dve", lambda e: e.tensor_tensor(out=lamv[:, 2, :], in0=lamv[:, 2, :], in1=lamv[:, 3, :], op=ALU.mult), r=["lamv"], w=["lamv"])
        T.op("dve", lambda e: e.tensor_reduce(out=lamt[:, 0:1], in_=lamv[:, 0, :], axis=AX.X, op=ALU.add), r=["lamv", "lamt"], w=["lamt"])
        T.op("dve", lambda e: e.tensor_reduce(out=lamt[:, 1:2], in_=lamv[:, 2, :], axis=AX.X, op=ALU.add), r=["lamv", "lamt"], w=["lamt"])
        T.op("act", lambda e: e.activation(out=lamt[:, 2:4], in_=lamt[:, 0:2], func=AF.Exp), r=["lamt"], w=["lamt"])
        T.op("dve", lambda e: e.tensor_tensor(out=lamt[:, 4:5], in0=lamt[:, 3:4], in1=lamt[:, 2:3], op=ALU.subtract), r=["lamt"], w=["lamt"])
        T.op("dve", lambda e: e.tensor_scalar(out=lamt[:, 5:6], in0=lamt[:, 4:5], scalar1=-0.2, scalar2=None, op0=ALU.add), r=["lamt"], w=["lamt"])
        T.op("pe", lambda e: e.matmul(ps[7][:, 0:1], lhsT=ones1[:, :], rhs=lamt[:, 5:6], start=True, stop=True),
             r=["ones1", "lamt"], w=[PK[7]])
        T.op("dve", lambda e: e.tensor_copy(out=nlam[:], in_=ps[7][:, 0:1]), r=[PK[7]], w=["nlam"])
        psT = ps[6][:, :].bitcast(BF16)
        steps = [(h, qc, m, kt) for h in range(4) for qc in range(8) for m in range(2) for kt in range(NT)]
        USE_POW = False

        def issue_S(idx):
            h, qc, m, kt = steps[idx]
            sp_ = idx % 2
            eb = idx % 3
            T.op("pe", lambda e: e.matmul(ps[sp_][:, :], lhsT=kT[:, m, h, kt * 128:(kt + 1) * 128],
                                          rhs=qT[:, h, qc * 512:(qc + 1) * 512], start=True, stop=True),
                 r=["kT", "kTz0", "kTz1", "qT"], w=[PK[sp_]])
            T.op("act", lambda e: e.activation(out=E[eb][:], in_=ps[sp_][:, :], func=AF.Exp),
                 r=[PK[sp_]], w=["E%d" % eb])

        def issue_PV(idx):
            h, qc, m, kt = steps[idx]
            eb = idx % 3
            for qs in range(4):
                bank = 2 + m * 2 + qs // 2
                T.op("pe", lambda e: e.matmul(ps[bank][:, (qs % 2) * 256:(qs % 2) * 256 + 129],
                                              lhsT=E[eb][:, qs * 128:(qs + 1) * 128], rhs=V[:, kt, h, 0:129],
                                              start=(kt == 0 and qs % 2 == 0), stop=(kt == NT - 1),
                                              skip_group_check=True),
                     r=["E%d" % eb, "V", "Vones"], w=[PK[bank]])

        def bc4(ap2):
            return ap2.unsqueeze(2).broadcast_to([128, 4, 128])

        def epi_head():
            for m in range(2):
                for half in range(2):
                    bank = 2 + m * 2 + half
                    src = ps[bank][:, :].rearrange("p (a c) -> p a c", a=2)[:, :, 0:129]
                    dst = osb[:, m, half * 2:(half + 1) * 2, :]
                    if half == 0:
                        T.op("dve", lambda e: e.tensor_copy(out=dst, in_=src), r=[PK[bank]], w=["osb%d%d" % (m, half)])
                    else:
                        T.op("pool" if False else "dve", lambda e: e.tensor_copy(out=dst, in_=src), r=[PK[bank]], w=["osb%d%d" % (m, half)])
            OS = ["osb00", "osb01", "osb10", "osb11"]
            T.op("dve", lambda e: e.reciprocal(out=sm[:, 0:2, :], in_=osb[:, :, :, 128]), r=OS, w=["smB"])
            T.op("dve", lambda e: e.tensor_scalar(out=sm[:, 2, :], in0=sm[:, 1, :], scalar1=nlam[:, 0:1], scalar2=None, op0=ALU.mult),
                 r=["smB", "nlam"], w=["smB"])
            T.op("dve", lambda e: e.tensor_tensor(out=aacc[:], in0=osb[:, 0, :, 0:128], in1=bc4(sm[:, 0, :]), op=ALU.mult),
                 r=OS + ["smB"], w=["aacc"])
            T.op("dve", lambda e: e.tensor_tensor(out=junkb[:], in0=osb[:, 1, :, 0:128], in1=bc4(sm[:, 2, :]), op=ALU.mult),
                 r=OS + ["smB"], w=["junkB"])
            T.op("dve", lambda e: e.tensor_tensor(out=aacc[:], in0=aacc[:], in1=junkb[:], op=ALU.add), r=["aacc", "junkB"], w=["aacc"])
            T.op("dve", lambda e: e.tensor_tensor(out=junkb[:], in0=aacc[:], in1=aacc[:], op=ALU.mult), r=["aacc"], w=["junkB"])
            T.op("dve", lambda e: e.tensor_reduce(out=sm[:, 3, :], in_=junkb[:], axis=AX.X, op=ALU.add), r=["junkB", "smB"], w=["smB"])
            T.op("dve", lambda e: e.tensor_scalar(out=sm[:, 4, :], in0=sm[:, 3, :], scalar1=1.0 / 128, scalar2=1e-6, op0=ALU.mult, op1=ALU.add),
                 r=["smB"], w=["smB"])
            if USE_POW:
                T.op("dve", lambda e: e.tensor_scalar(out=sm[:, 5, :], in0=sm[:, 4, :], scalar1=-0.5, scalar2=None, op0=ALU.pow),
                     r=["smB"], w=["smB"])
            else:
                T.op("act", lambda e: e.activation(out=sm[:, 6, :], in_=sm[:, 4, :], func=AF.Sqrt), r=["smB"], w=["smB"])
                T.op("dve", lambda e: e.reciprocal(out=sm[:, 5, :], in_=sm[:, 6, :]), r=["smB"], w=["smB"])
            T.op("dve", lambda e: e.tensor_tensor(out=aacc[:], in0=aacc[:], in1=bc4(sm[:, 5, :]), op=ALU.mult), r=["aacc", "smB"], w=["aacc"])
            T.op("dve", lambda e: e.tensor_tensor(out=ab[:], in0=aacc[:], in1=subg[:, :].unsqueeze(1).broadcast_to([128, 4, 128]), op=ALU.mult),
                 r=["aacc", "subg"], w=["abB"])

        def epi_tail(h, qc):
            for qs in range(4):
                T.op("pe", lambda e: e.transpose(out=psT[:, qs * 128:(qs + 1) * 128], in_=ab[:, qs, :], identity=ident_b[:]),
                     r=["abB", "ident_b"], w=[PK[6]])
            st = aTs[qc % 2]
            T.op("act", lambda e: e.copy(out=st[:], in_=psT[:, 0:512]), r=[PK[6]], w=["aTs%d" % (qc % 2)])
            T.dma("sp", lambda e: e.dma_start(out=attnT_d[h * 128:(h + 1) * 128, qc * 512:(qc + 1) * 512], in_=st[:]),
                  r=["aTs%d" % (qc % 2)], w=["attnT_d"])

        pending = None
        issue_S(0)
        for idx in range(len(steps)):
            h, qc, m, kt = steps[idx]
            if idx + 1 < len(steps):
                issue_S(idx + 1)
            issue_PV(idx)
            if pending is not None and m == 0 and kt == 12:
                epi_tail(*pending)
                pending = None
            if m == 1 and kt == NT - 1:
                epi_head()
                pending = (h, qc)
        epi_tail(*pending)
        T.barrier()

    if "C" in stages:
      with ExitStack() as es:
        FA = sb(es, "FA", [32, 2 * NK], BF16)
        TGr = sb(es, "TGr", [128, NK, 128], BF16)
        TGi = sb(es, "TGi", [128, NK, 128], BF16)
        TGn = sb(es, "TGn", [128, NK, 128], BF16)
        KCS = [(0, 16), (16, 32), (32, NK)]
        R1 = sb(es, "R1", [128, 256], BF16)
        R2 = sb(es, "R2", [128, 256], BF16)
        TH = sb(es, "TH", [NK, 128, 2, 32], BF16)
        skipbc = sb(es, "skipbc", [32, 2, GC])
        for tns, src, k in ((FA, FA_d, "FA"), (R1, R1_d, "R1"), (R2, R2_d, "R2")):
            T.dma("sp", lambda e: e.dma_start(out=tns[:], in_=src), w=[k])
        for tns, src, k in ((TGr, TGr_d, "TGr"), (TGi, TGi_d, "TGi"), (TGn, TGn_d, "TGn")):
            T.dmas("sp", [(lambda e, a=a, b=b: e.dma_start(out=tns[:, a:b, :], in_=src[:, a:b, :])) for (a, b) in KCS], w=[k])
        T.dmas("sp", [(lambda e, a=a: e.dma_start(out=TH[:, a:a + 32, :, :], in_=TH_d[:, a:a + 32, :, :])) for a in range(0, 128, 32)], w=["TH"])
        zf = sb(es, "zf", [32, GC, 128])
        gt = sb(es, "gtC", [32, GC, 128])
        zs = sb(es, "zsC", [32, GC, 128])
        srcb = [sb(es, "srcb%d" % i, [32, GC, 128], BF16) for i in range(3)]
        Bm_ = [sb(es, "Bm%d" % i, [128, 2 * NK, GC], BF16) for i in range(3)]
        srs = sb(es, "srsC", [128, 512])
        sis = sb(es, "sisC", [128, 512])
        tm = [sb(es, "tmC%d" % i, [128, 512]) for i in range(4)]
        Y = sb(es, "YC", [128, GC, 2, NK], BF16)
        Dm = sb(es, "DmC", [NK, 256, GC], BF16)
        tmpy = sb(es, "tmpyC", [32, GC, 16])
        hob = srcb[0]

        def flay(ap2d):
            return ap2d.rearrange("c (a b) -> a c b", b=128)

        def load_filters(g_, o_):
            for pm in range(2):
                T.dma("sp", lambda e: e.dma_start(out=srcb[1 + pm][:], in_=flay(kern_d[o_, pm, g_ * GC:(g_ + 1) * GC, :])),
                      w=["srcb%d" % (1 + pm)])

        for g in range(512 // GC):
            c0 = g * GC
            T.dma("sp", lambda e: e.dma_start(out=zf[:], in_=flay(hyc_d[c0:c0 + GC, :])), w=["zf"])
            T.dma("sp", lambda e: e.dma_start(out=skipbc[:], in_=skip_d[:, :, c0:c0 + GC]), w=["skipbc"])
            for o in range(2):
                T.dma("sp", lambda e: e.dma_start(out=gt[:], in_=flay(hyc_d[512 * (o + 1) + c0:512 * (o + 1) + c0 + GC, :])),
                      w=["gtC"])
                if g == 0 and o == 0:
                    load_filters(0, 0)
                for s_ in (1, 2, 0):
                    if s_ == 0:
                        T.op("act", lambda e: e.copy(out=srcb[0][:], in_=zf[:]), r=["zf"], w=["srcb0"])
                        T.op("pool", lambda e: e.tensor_tensor(out=zs[:], in0=zf[:],
                                                               in1=skipbc[:, o, :].unsqueeze(2).broadcast_to([32, GC, 128]),
                                                               op=ALU.mult), r=["zf", "skipbc"], w=["zsC"])
                    for cq, (ca, cb) in enumerate(((0, 7), (7, 14), (14, 21), (21, 28), (28, 32))):
                        pi = cq % 2
                        ncg = cb - ca
                        for c4 in range(ncg):
                            c = ca + c4
                            T.op("pe", lambda e: e.matmul(ps[pi][:, c4 * 2 * NK:(c4 + 1) * 2 * NK], lhsT=srcb[s_][:, c, :],
                                                          rhs=FA[:, :], start=True, stop=True),
                                 r=["srcb%d" % s_, "FA"], w=[PK[pi]])
                        T.op("act" if cq % 2 == 0 else "dve",
                             lambda e: (e.copy if cq % 2 == 0 else e.tensor_copy)(
                                 out=Bm_[s_][:, :, ca:cb],
                                 in_=ps[pi][:, 0:ncg * 2 * NK].rearrange("p (c f) -> p f c", c=ncg)),
                             r=[PK[pi]], w=["Bm%d" % s_])
                nxt = (g, 1) if o == 0 else (g + 1, 0)
                if nxt[0] < 512 // GC:
                    load_filters(*nxt)
                for kc, (ka, kb_) in enumerate(KCS):
                    bZr, bZi, bSr, bSi = (2, 3, 4, 5) if kc % 2 == 0 else (0, 1, 6, 7)
                    nk = kb_ - ka
                    wd = nk * GC
                    for j in range(nk):
                        k1 = ka + j
                        cs_ = slice(j * GC, (j + 1) * GC)
                        bz, bp, bm = Bm_[0], Bm_[1], Bm_[2]
                        T.op("pe", lambda e: e.matmul(ps[bZr][:, cs_], lhsT=TGr[:, k1, :], rhs=bz[:, k1, :], start=True, stop=False), r=["TGr", "Bm0"], w=[PK[bZr]])
                        T.op("pe", lambda e: e.matmul(ps[bZr][:, cs_], lhsT=TGn[:, k1, :], rhs=bz[:, NK + k1, :], start=False, stop=True), r=["TGn", "Bm0"], w=[PK[bZr]])
                        T.op("pe", lambda e: e.matmul(ps[bZi][:, cs_], lhsT=TGi[:, k1, :], rhs=bz[:, k1, :], start=True, stop=False), r=["TGi", "Bm0"], w=[PK[bZi]])
                        T.op("pe", lambda e: e.matmul(ps[bZi][:, cs_], lhsT=TGr[:, k1, :], rhs=bz[:, NK + k1, :], start=False, stop=True), r=["TGr", "Bm0"], w=[PK[bZi]])
                        T.op("pe", lambda e: e.matmul(ps[bSr][:, cs_], lhsT=TGr[:, k1, :], rhs=bp[:, k1, :], start=True, stop=False), r=["TGr", "Bm1"], w=[PK[bSr]])
                        T.op("pe", lambda e: e.matmul(ps[bSr][:, cs_], lhsT=TGn[:, k1, :], rhs=bp[:, NK + k1, :], start=False, stop=True), r=["TGn", "Bm1"], w=[PK[bSr]])
                        T.op("pe", lambda e: e.matmul(ps[bSi][:, cs_], lhsT=TGi[:, k1, :], rhs=bm[:, k1, :], start=True, stop=False), r=["TGi", "Bm2"], w=[PK[bSi]])
                        T.op("pe", lambda e: e.matmul(ps[bSi][:, cs_], lhsT=TGr[:, k1, :], rhs=bm[:, NK + k1, :], start=False, stop=True), r=["TGr", "Bm2"], w=[PK[bSi]])
                    T.op("act", lambda e: e.copy(out=srs[:, 0:wd], in_=ps[bSr][:, 0:wd]), r=[PK[bSr]], w=["srsC"])
                    T.op("act", lambda e: e.copy(out=sis[:, 0:wd], in_=ps[bSi][:, 0:wd]), r=[PK[bSi]], w=["sisC"])
                    T.op("dve", lambda e: e.tensor_tensor(out=tm[0][:, 0:wd], in0=ps[bZr][:, 0:wd], in1=srs[:, 0:wd], op=ALU.mult), r=[PK[bZr], "srsC"], w=["tm0"])
                    T.op("dve", lambda e: e.tensor_tensor(out=tm[1][:, 0:wd], in0=ps[bZi][:, 0:wd], in1=sis[:, 0:wd], op=ALU.mult), r=[PK[bZi], "sisC"], w=["tm1"])
                    T.op("dve", lambda e: e.tensor_tensor(out=tm[2][:, 0:wd], in0=ps[bZr][:, 0:wd], in1=sis[:, 0:wd], op=ALU.mult), r=[PK[bZr], "sisC"], w=["tm2"])
                    T.op("dve", lambda e: e.tensor_tensor(out=tm[3][:, 0:wd], in0=ps[bZi][:, 0:wd], in1=srs[:, 0:wd], op=ALU.mult), r=[PK[bZi], "srsC"], w=["tm3"])
                    yv_r = Y[:, :, 0, ka:kb_]
                    yv_i = Y[:, :, 1, ka:kb_]
                    T.op("dve", lambda e: e.tensor_tensor(out=yv_r, in0=tm[0][:, 0:wd].rearrange("p (k c) -> p c k", c=GC),
                                                           in1=tm[1][:, 0:wd].rearrange("p (k c) -> p c k", c=GC), op=ALU.subtract),
                         r=["tm0", "tm1"], w=["YC"])
                    T.op("dve", lambda e: e.tensor_tensor(out=yv_i, in0=tm[2][:, 0:wd].rearrange("p (k c) -> p c k", c=GC),
                                                           in1=tm[3][:, 0:wd].rearrange("p (k c) -> p c k", c=GC), op=ALU.add),
                         r=["tm2", "tm3"], w=["YC"])
                for cq in range(GC // 2):
                    pi = cq % 2
                    for c2 in range(2):
                        c = cq * 2 + c2
                        T.op("pe", lambda e: e.matmul(ps[pi][0:NK, c2 * 256:(c2 + 1) * 256], lhsT=Y[:, c, 0, :], rhs=R1[:, :],
                                                      start=True, stop=False), r=["YC", "R1"], w=[PK[pi]])
                        T.op("pe", lambda e: e.matmul(ps[pi][0:NK, c2 * 256:(c2 + 1) * 256], lhsT=Y[:, c, 1, :], rhs=R2[:, :],
                                                      start=False, stop=True), r=["YC", "R2"], w=[PK[pi]])
                    T.op("act" if cq % 2 == 0 else "dve",
                         lambda e: (e.copy if cq % 2 == 0 else e.tensor_copy)(
                             out=Dm[:, :, cq * 2:(cq + 1) * 2],
                             in_=ps[pi][0:NK, :].rearrange("p (c f) -> p f c", c=2)),
                         r=[PK[pi]], w=["DmC"])
                for nq in range(8):
                    pi = 6 + nq % 2
                    for j in range(16):
                        n2 = nq * 16 + j
                        T.op("pe", lambda e: e.matmul(ps[pi][0:32, j * GC:(j + 1) * GC], lhsT=TH[:, n2, 0, :], rhs=Dm[:, n2, :],
                                                      start=True, stop=False), r=["TH", "DmC"], w=[PK[pi]])
                        T.op("pe", lambda e: e.matmul(ps[pi][0:32, j * GC:(j + 1) * GC], lhsT=TH[:, n2, 1, :], rhs=Dm[:, 128 + n2, :],
                                                      start=False, stop=True), r=["TH", "DmC"], w=[PK[pi]])
                    nsl = slice(nq * 16, (nq + 1) * 16)
                    T.op("dve", lambda e: e.tensor_tensor(out=tmpy[:], in0=ps[pi][0:32, :].rearrange("p (n c) -> p c n", c=GC),
                                                          in1=zs[:, :, nsl], op=ALU.add), r=[PK[pi], "zsC"], w=["tmpyC"])
                    T.op("dve", lambda e: e.tensor_tensor(out=zf[:, :, nsl], in0=tmpy[:], in1=gt[:, :, nsl], op=ALU.mult),
                         r=["tmpyC", "gtC", "srcb0", "zsC"], w=["zf"])
            T.op("act", lambda e: e.copy(out=hob[:], in_=zf[:]), r=["zf"], w=["srcb0"])
            T.dma("sp", lambda e: e.dma_start(out=flay(hyoT_d[c0:c0 + GC, :]), in_=hob[:]), r=["srcb0"], w=["hyoT_d"])
        T.barrier()

    if "D" in stages:
      with ExitStack() as es:
        aff_all = sb(es, "aff_all", [128, NT, NE])
        with ExitStack() as es2:
            attnT = sb(es2, "attnT", [128, 4, L], BF16)
            hyoT = sb(es2, "hyoT", [128, 4, L], BF16)
            wpa = sb(es2, "wpa", [128, 4, D], BF16)
            wph = sb(es2, "wph", [128, 4, D], BF16)
            wo = sb(es2, "wo", [128, 8, D], BF16)
            g2bc = sb(es2, "g2bc", [128, D])
            wr = sb(es2, "wr", [128, 8, 16])
            gat2 = [sb(es2, "gatD%d" % i, [128, 16, 512], BF16) for i in range(2)]
            m1 = sb(es2, "m1D", [128, 512])
            m2 = sb(es2, "m2D", [128, 512])
            mT = sb(es2, "mTD", [128, 8, 512], BF16)
            xin = sb(es2, "xinD", [128, D])
            x1 = sb(es2, "x1D", [128, D])
            u2 = sb(es2, "u2D0", [128, D])
            u2b = sb(es2, "u2bD", [128, D], BF16)
            u2T = sb(es2, "u2TD", [128, 8, 128])
            junkd = sb(es2, "junkD", [128, D])
            st = sb(es2, "stD", [128, 8])
            lg = sb(es2, "lgD", [128, NE])
            for (tt_, td_, tk_) in ((attnT, attnT_d, "attnT"), (hyoT, hyoT_d, "hyoT")):
                T.dmas("sp", [(lambda e, j=j, a=a: e.dma_start(out=tt_[:, j, a:a + 2048], in_=td_[j * 128:(j + 1) * 128, a:a + 2048]))
                              for j in range(4) for a in (0, 2048)], w=[tk_])
            T.dma("sp", lambda e: e.dma_start(out=g2bc[:], in_=g2bc_d[:, :]), w=["g2bc"])
            T.dma("sp", lambda e: e.dma_start(out=wr[:], in_=wr_d[:, :, :]), w=["wr"])
            for (wsrc, nj, wdst, wk) in ((wpa_d, 4, wpa, "wpa"), (wph_d, 4, wph, "wph"), (wo_d, 8, wo, "wo")):
                T.dmas("pool", [(lambda e, a=a: e.dma_start(out=wdst[:, a:a + 4, :],
                                                            in_=wsrc.rearrange("(j p) c -> p j c", p=128)[:, a:a + 4, :]))
                                for a in range(0, nj, 4)], w=[wk])
            u2s = [u2, sb(es2, "u2D1", [128, D])]

            def d_tail(i):
                u2t = u2s[i % 2]
                for j in range(8):
                    T.op("pe", lambda e: e.transpose(out=ps[4 + j // 4][:, (j % 4) * 128:(j % 4 + 1) * 128],
                                                     in_=u2t[:, j * 128:(j + 1) * 128], identity=ident_f[:]),
                         r=["u2D%d" % (i % 2), "ident_f"], w=[PK[4 + j // 4]])
                for hh in range(2):
                    T.op("act", lambda e: e.copy(out=u2T[:, hh * 4:(hh + 1) * 4, :],
                                                 in_=ps[4 + hh][:, :].rearrange("p (j t) -> p j t", j=4)),
                         r=[PK[4 + hh]], w=["u2TD"])
                for j in range(8):
                    T.op("pe", lambda e: e.matmul(ps[6][:, 0:NE], lhsT=u2T[:, j, :], rhs=wr[:, j, :], start=(j == 0), stop=(j == 7)),
                         r=["u2TD", "wr"], w=[PK[6]])
                T.op("dve", lambda e: e.tensor_reduce(out=st2[:, 3:4], in_=ps[6][:, 0:NE], axis=AX.X, op=ALU.max), r=[PK[6], "st2D"], w=["st2D"])
                T.op("dve", lambda e: e.tensor_scalar(out=st2[:, 4:5], in0=st2[:, 3:4], scalar1=-1.0, scalar2=None, op0=ALU.mult), r=["st2D"], w=["st2D"])
                T.op("dve", lambda e: e.memset(st2[:, 5:6], 0.0), r=["st2D"], w=["st2D"])
                T.op("act", lambda e: e.activation(out=lg[:], in_=ps[6][:, 0:NE], func=AF.Exp, bias=st2[:, 4:5], accum_out=st2[:, 5:6]),
                     r=[PK[6], "st2D"], w=["lgD", "st2D"])
                T.op("dve", lambda e: e.reciprocal(out=st2[:, 6:7], in_=st2[:, 5:6]), r=["st2D"], w=["st2D"])
                T.op("dve", lambda e: e.tensor_scalar(out=aff_all[:, i, :], in0=lg[:], scalar1=st2[:, 6:7], scalar2=None, op0=ALU.mult),
                     r=["lgD", "st2D"], w=["aff_all"])

            st2 = sb(es2, "st2D", [128, 8])
            prev_i = None
            for tq in range(8):
                tsl = slice(tq * 512, (tq + 1) * 512)
                gat = gat2[tq % 2]
                gk = "gatD%d" % (tq % 2)
                T.dmas("sp", [(lambda e, a=a: e.dma_start(out=gat[:, a:a + 8, :],
                                                          in_=gateT_d.rearrange("(j p) t -> p j t", p=128)[:, a:a + 8, tsl])) for a in (0, 8)],
                       w=[gk])
                for dt in range(8):
                    pa = 0 if dt % 2 == 0 else 7
                    for j in range(4):
                        T.op("pe", lambda e: e.matmul(ps[pa][:, :], lhsT=wpa[:, j, dt * 128:(dt + 1) * 128], rhs=attnT[:, j, tsl],
                                                      start=(j == 0), stop=(j == 3)), r=["wpa", "attnT"], w=[PK[pa]])
                    for j in range(4):
                        T.op("pe", lambda e: e.matmul(ps[1][:, :], lhsT=wph[:, j, dt * 128:(dt + 1) * 128], rhs=hyoT[:, j, tsl],
                                                      start=(j == 0), stop=(j == 3)), r=["wph", "hyoT"], w=[PK[1]])
                    T.op("dve", lambda e: e.tensor_tensor(out=m1[:], in0=ps[pa][:, :], in1=gat[:, dt, :], op=ALU.mult), r=[PK[pa], gk], w=["m1D"])
                    T.op("dve", lambda e: e.tensor_tensor(out=m2[:], in0=ps[1][:, :], in1=gat[:, 8 + dt, :], op=ALU.mult), r=[PK[1], gk], w=["m2D"])
                    T.op("pool", lambda e: e.tensor_tensor(out=mT[:, dt, :], in0=m1[:], in1=m2[:], op=ALU.add), r=["m1D", "m2D"], w=["mTD"])
                for ts in range(4):
                    i = tq * 4 + ts
                    u2t = u2s[i % 2]
                    u2k = "u2D%d" % (i % 2)
                    T.dma("sp", lambda e: e.dma_start(out=xin[:], in_=x_d[i * 128:(i + 1) * 128, :]), w=["xinD"])
                    for half in range(2):
                        for dt in range(8):
                            T.op("pe", lambda e: e.matmul(ps[2 + half][:, :], lhsT=mT[:, dt, ts * 128:(ts + 1) * 128],
                                                          rhs=wo[:, dt, half * 512:(half + 1) * 512], start=(dt == 0), stop=(dt == 7)),
                                 r=["mTD", "wo"], w=[PK[2 + half]])
                        T.op("dve", lambda e: e.tensor_tensor(out=x1[:, half * 512:(half + 1) * 512], in0=ps[2 + half][:, :],
                                                              in1=xin[:, half * 512:(half + 1) * 512], op=ALU.add),
                             r=[PK[2 + half], "xinD"], w=["x1D"])
                    if prev_i is not None:
                        d_tail(prev_i)
                    T.dma("sp", lambda e: e.dma_start(out=out_d[i * 128:(i + 1) * 128, :], in_=x1[:]), r=["x1D"], w=["out_d"])
                    T.op("dve", lambda e: e.memset(st[:, 0:1], 0.0), w=["stD"])
                    T.op("act", lambda e: e.activation(out=junkd[:], in_=x1[:], func=AF.Square, accum_out=st[:, 0:1]),
                         r=["x1D", "stD"], w=["junkD", "stD"])
                    T.op("act", lambda e: e.activation(out=st[:, 1:2], in_=st[:, 0:1], func=AF.Sqrt, scale=1.0 / D, bias=eps[:]),
                         r=["stD", "eps"], w=["stD"])
                    T.op("dve", lambda e: e.reciprocal(out=st[:, 2:3], in_=st[:, 1:2]), r=["stD"], w=["stD"])
                    T.op("dve", lambda e: e.scalar_tensor_tensor(out=u2t[:], in0=x1[:], scalar=st[:, 2:3], in1=g2bc[:],
                                                                 op0=ALU.mult, op1=ALU.mult), r=["x1D", "stD", "g2bc"], w=[u2k])
                    T.op("act", lambda e: e.copy(out=u2b[:], in_=u2t[:]), r=[u2k], w=["u2bD"])
                    T.dma("sp", lambda e: e.dma_start(out=u2_d[i * 128:(i + 1) * 128, :], in_=u2b[:]), r=["u2bD"], w=["u2_d"])
                    prev_i = i
            d_tail(prev_i)
            T.barrier()
        if "aff" in dbg_d:
            T.dma("sp", lambda e: e.dma_start(out=dbg_d["aff"], in_=aff_all[:]), r=["aff_all"], w=["dbgaff"])

        if "E" in stages:
          idx_all = sb(es, "idx_all", [128, NE, 4], I32)
          g_all = sb(es, "g_all", [128, NE, 4])
          with ExitStack() as es2:
            affT = sb(es2, "affT", [NE, L])
            cmpj = sb(es2, "cmpj", [NE, L])
            bs = sb(es2, "bsE", [NE, 8])
            tri = sb(es2, "tri", [128, 128])
            onesf = sb(es2, "onesf", [128, 128])
            iota = sb(es2, "iota", [128, 512])
            tidhl = sb(es2, "tidhl", [128, NT, 2], BF16)
            thbc = sb(es2, "thbc", [128, NE])
            dg = sb(es2, "dgE", [NE, NE])
            M = sb(es2, "ME", [128, NT, NE])
            pos = sb(es2, "posE", [128, NT, NE])
            srun = sb(es2, "srunE", [128, NE])
            cum = sb(es2, "cumE", [128, NE])
            vals = sb(es2, "valsE", [128, NT, NE, 4], BF16)
            afh = sb(es2, "afhE", [128, NT, NE], BF16)
            afr = sb(es2, "afrE", [128, NT, NE])
            oh = [sb(es2, "ohE%d" % i, [128, 512], BF16) for i in range(4)]
            idf = sb(es2, "idfE", [128, 4])
            pvs = sb(es2, "pvsE", [128, 16])
            T.dma("sp", lambda e: e.dma_start(out=tri[:], in_=tri_d[:, :]), w=["tri"])
            T.dma("sp", lambda e: e.dma_start(out=iota[:], in_=iota_d[:, :]), w=["iota"])
            T.dma("sp", lambda e: e.dma_start(out=tidhl[:], in_=tidhl_d[:, :, :]), w=["tidhl"])
            T.op("pool", lambda e: e.memset(onesf[:], 1.0), w=["onesf"])
            for i in range(NT):
                pi = i // 4
                T.op("pe", lambda e: e.transpose(out=ps[pi][0:NE, (i % 4) * 128:(i % 4 + 1) * 128], in_=aff_all[:, i, :],
                                                 identity=ident_f[:]), r=["aff_all", "ident_f"], w=[PK[pi]])
                if i % 4 == 3:
                    T.op("act", lambda e: e.copy(out=affT[:, (i - 3) * 128:(i + 1) * 128], in_=ps[pi][0:NE, :]), r=[PK[pi]], w=["affT"])
            T.op("dve", lambda e: e.memset(bs[:, 0:1], 0.0), w=["bsE"])
            T.op("dve", lambda e: e.memset(bs[:, 1:2], 1.0), r=["bsE"], w=["bsE"])
            for it in range(32):
                T.op("dve", lambda e: e.tensor_scalar(out=bs[:, 2:3], in0=bs[:, 0:1], scalar1=bs[:, 1:2], scalar2=0.5, op0=ALU.add, op1=ALU.mult), r=["bsE"], w=["bsE"])
                T.op("dve", lambda e: e.tensor_scalar(out=cmpj[:], in0=affT[:], scalar1=bs[:, 2:3], scalar2=None, op0=ALU.is_ge), r=["affT", "bsE"], w=["cmpj"])
                T.op("dve", lambda e: e.tensor_reduce(out=bs[:, 3:4], in_=cmpj[:], axis=AX.X, op=ALU.add), r=["cmpj", "bsE"], w=["bsE"])
                T.op("dve", lambda e: e.tensor_scalar(out=bs[:, 4:5], in0=bs[:, 3:4], scalar1=CAP - 0.5, scalar2=None, op0=ALU.is_ge), r=["bsE"], w=["bsE"])
                T.op("dve", lambda e: e.tensor_tensor(out=bs[:, 5:6], in0=bs[:, 2:3], in1=bs[:, 0:1], op=ALU.subtract), r=["bsE"], w=["bsE"])
                T.op("dve", lambda e: e.tensor_tensor(out=bs[:, 6:7], in0=bs[:, 1:2], in1=bs[:, 2:3], op=ALU.subtract), r=["bsE"], w=["bsE"])
                T.op("dve", lambda e: e.scalar_tensor_tensor(out=bs[:, 0:1], in0=bs[:, 5:6], scalar=bs[:, 4:5], in1=bs[:, 0:1], op0=ALU.mult, op1=ALU.add), r=["bsE"], w=["bsE"])
                T.op("dve", lambda e: e.scalar_tensor_tensor(out=bs[:, 1:2], in0=bs[:, 6:7], scalar=bs[:, 4:5], in1=bs[:, 2:3], op0=ALU.mult, op1=ALU.add), r=["bsE"], w=["bsE"])
            T.op("dve", lambda e: e.tensor_scalar(out=dg[:], in0=ident_f[0:NE, 0:NE], scalar1=bs[:, 0:1], scalar2=None, op0=ALU.mult), r=["bsE", "ident_f"], w=["dgE"])
            T.op("pe", lambda e: e.matmul(ps[0][:, 0:NE], lhsT=onesf[0:NE, :], rhs=dg[:, :], start=True, stop=True), r=["onesf", "dgE"], w=[PK[0]])
            T.op("dve", lambda e: e.tensor_copy(out=thbc[:], in_=ps[0][:, 0:NE]), r=[PK[0]], w=["thbc"])
            T.op("dve", lambda e: e.tensor_tensor(out=M[:], in0=aff_all[:], in1=thbc[:, :].unsqueeze(1).broadcast_to([128, NT, NE]), op=ALU.is_ge),
                 r=["aff_all", "thbc"], w=["ME"])
            T.op("dve", lambda e: e.memset(srun[:], 0.0), w=["srunE"])
            for i in range(NT):
                pi = 1 + i % 2
                T.op("pe", lambda e: e.matmul(ps[pi][:, 0:NE], lhsT=tri[:, :], rhs=M[:, i, :], start=True, stop=True), r=["tri", "ME"], w=[PK[pi]])
                T.op("pe", lambda e: e.matmul(ps[pi][:, NE:2 * NE], lhsT=onesf[:, :], rhs=M[:, i, :], start=True, stop=True), r=["onesf", "ME"], w=[PK[pi]])
                T.op("dve", lambda e: e.tensor_tensor(out=cum[:], in0=ps[pi][:, 0:NE], in1=srun[:], op=ALU.add), r=[PK[pi], "srunE"], w=["cumE"])
                T.op("dve", lambda e: e.tensor_tensor(out=srun[:], in0=ps[pi][:, NE:2 * NE], in1=srun[:], op=ALU.add), r=[PK[pi], "srunE", "cumE"], w=["srunE"])
                T.op("dve", lambda e: e.tensor_tensor(out=cum[:], in0=cum[:], in1=M[:, i, :], op=ALU.mult), r=["cumE", "ME"], w=["cumE"])
                T.op("dve", lambda e: e.tensor_scalar(out=pos[:, i, :], in0=cum[:], scalar1=-1.0, scalar2=None, op0=ALU.add), r=["cumE"], w=["posE"])
            T.op("dve", lambda e: e.tensor_copy(out=afh[:], in_=aff_all[:]), r=["aff_all"], w=["afhE"])
            T.op("dve", lambda e: e.tensor_tensor(out=afr[:], in0=aff_all[:], in1=afh[:], op=ALU.subtract), r=["aff_all", "afhE"], w=["afrE"])
            T.op("dve", lambda e: e.tensor_copy(out=vals[:, :, :, 2], in_=afh[:]), r=["afhE"], w=["valsE"])
            T.op("dve", lambda e: e.tensor_copy(out=vals[:, :, :, 3], in_=afr[:]), r=["afrE", "valsE"], w=["valsE"])
            T.op("dve", lambda e: e.tensor_copy(out=vals[:, :, :, 0:2], in_=tidhl[:, :, :].unsqueeze(2).broadcast_to([128, NT, NE, 2])),
                 r=["tidhl", "valsE"], w=["valsE"])
            oc = 0
            for ex in range(NE):
                pi = 3 + ex % 2
                for i in range(NT):
                    ob = oc % 4
                    eng = "dve"
                    oc += 1
                    T.op(eng, lambda e: e.tensor_scalar(out=oh[ob][:], in0=iota[:], scalar1=pos[:, i, ex:ex + 1], scalar2=None, op0=ALU.is_equal),
                         r=["iota", "posE"], w=["ohE%d" % ob])
                    for sc in range(4):
                        T.op("pe", lambda e: e.matmul(ps[pi][:, sc * 4:(sc + 1) * 4], lhsT=oh[ob][:, sc * 128:(sc + 1) * 128], rhs=vals[:, i, ex, :],
                                                      start=(i == 0 and sc == 0), stop=(i == NT - 1), skip_group_check=True),
                             r=["ohE%d" % ob, "valsE"], w=[PK[pi]])
                T.op("dve", lambda e: e.tensor_copy(out=pvs[:], in_=ps[pi][:, 0:16]), r=[PK[pi]], w=["pvsE"])
                pv = pvs[:, :].rearrange("p (s f) -> p s f", f=4)
                T.op("dve", lambda e: e.scalar_tensor_tensor(out=idf[:], in0=pv[:, :, 0], scalar=64.0, in1=pv[:, :, 1], op0=ALU.mult, op1=ALU.add),
                     r=["pvsE"], w=["idfE"])
                T.op("dve", lambda e: e.tensor_copy(out=idx_all[:, ex, :], in_=idf[:]), r=["idfE"], w=["idx_all"])
                T.op("dve", lambda e: e.tensor_tensor(out=g_all[:, ex, :], in0=pv[:, :, 2], in1=pv[:, :, 3], op=ALU.add), r=["pvsE"], w=["g_all"])
            T.barrier()
          if "idx" in dbg_d:
            T.dma("sp", lambda e: e.dma_start(out=dbg_d["idx"], in_=idx_all[:]), r=["idx_all"], w=["dbgidx"])
            T.dma("sp", lambda e: e.dma_start(out=dbg_d["g"], in_=g_all[:]), r=["g_all"], w=["dbgg"])

          if "F" in stages:
            wgb = [sb(es, "wgb%d" % i, [128, 8, D], BF16) for i in range(2)]
            wub = [sb(es, "wub%d" % i, [128, 8, D], BF16) for i in range(2)]
            wdb = [sb(es, "wdb%d" % i, [128, 8, D], BF16) for i in range(2)]
            xg = [sb(es, "xgF%d" % i, [128, D], BF16) for i in range(4)]
            xinT = sb(es, "xinT", [128, 8, 512], BF16)
            sg = sb(es, "sgF", [128, 512])
            hT = sb(es, "hTF", [128, 8, 512], BF16)
            eo = [sb(es, "eoF%d" % i, [128, D]) for i in range(2)]
            psb = [ps[6][:, :].bitcast(BF16), ps[7][:, :].bitcast(BF16)]

            def load_w(ex):
                for (wsrc, wdst, wk) in ((wg_d, wgb, "wgb"), (wu_d, wub, "wub"), (wd_d, wdb, "wdb")):
                    dst = wdst[ex % 2]
                    T.dmas("pool", [(lambda e, hh=hh: e.dma_start(out=dst[:, hh * 4:(hh + 1) * 4, :],
                                                                  in_=wsrc[ex].rearrange("(j p) c -> p j c", p=128)[:, hh * 4:(hh + 1) * 4, :]))
                                    for hh in range(2)], w=["%s%d" % (wk, ex % 2)])

            def gather(ex):
                for sc in range(4):
                    T.dma("pool", lambda e: e.indirect_dma_start(out=xg[sc][:], out_offset=None, in_=u2_d[:, :],
                                                                 in_offset=bass.IndirectOffsetOnAxis(ap=idx_all[:, ex, sc:sc + 1], axis=0)),
                          r=["idx_all", "u2_d"], w=["xgF%d" % sc])

            load_w(0)
            gather(0)
            for ex in range(NE):
                wgk, wuk, wdk = "wgb%d" % (ex % 2), "wub%d" % (ex % 2), "wdb%d" % (ex % 2)
                wgt, wut, wdt = wgb[ex % 2], wub[ex % 2], wdb[ex % 2]
                if ex + 1 < NE:
                    load_w(ex + 1)
                for sc in range(4):
                    for j in range(8):
                        T.op("pe", lambda e: e.transpose(out=psb[sc % 2][:, j * 128:(j + 1) * 128], in_=xg[sc][:, j * 128:(j + 1) * 128], identity=ident_b[:]),
                             r=["xgF%d" % sc, "ident_b"], w=[PK[6 + sc % 2]])
                    T.op("act", lambda e: e.copy(out=xinT[:, :, sc * 128:(sc + 1) * 128], in_=psb[sc % 2].rearrange("p (j t) -> p j t", j=8)),
                         r=[PK[6 + sc % 2]], w=["xinT"])
                if ex + 1 < NE:
                    gather(ex + 1)
                for ft in range(8):
                    pg, pu = (ft % 2) * 2, (ft % 2) * 2 + 1
                    for j in range(8):
                        T.op("pe", lambda e: e.matmul(ps[pg][:, :], lhsT=wgt[:, j, ft * 128:(ft + 1) * 128], rhs=xinT[:, j, :], start=(j == 0), stop=(j == 7)),
                             r=[wgk, "xinT"], w=[PK[pg]])
                    for j in range(8):
                        T.op("pe", lambda e: e.matmul(ps[pu][:, :], lhsT=wut[:, j, ft * 128:(ft + 1) * 128], rhs=xinT[:, j, :], start=(j == 0), stop=(j == 7)),
                             r=[wuk, "xinT"], w=[PK[pu]])
                    T.op("act", lambda e: e.activation(out=sg[:], in_=ps[pg][:, :], func=AF.Silu), r=[PK[pg]], w=["sgF"])
                    T.op("dve", lambda e: e.tensor_tensor(out=hT[:, ft, :], in0=ps[pu][:, :], in1=sg[:], op=ALU.mult), r=[PK[pu], "sgF"], w=["hTF"])
                for sc in range(4):
                    eb = eo[sc % 2]
                    for half in range(2):
                        po = 4 + half
                        for ft in range(8):
                            T.op("pe", lambda e: e.matmul(ps[po][:, :], lhsT=hT[:, ft, sc * 128:(sc + 1) * 128], rhs=wdt[:, ft, half * 512:(half + 1) * 512],
                                                          start=(ft == 0), stop=(ft == 7)), r=["hTF", wdk], w=[PK[po]])
                        T.op("dve", lambda e: e.tensor_scalar(out=eb[:, half * 512:(half + 1) * 512], in0=ps[po][:, :], scalar1=g_all[:, ex, sc:sc + 1],
                                                              scalar2=None, op0=ALU.mult), r=[PK[po], "g_all"], w=["eoF%d" % (sc % 2)])
                    T.dma("pool", lambda e: e.indirect_dma_start(out=out_d[:, :], out_offset=bass.IndirectOffsetOnAxis(ap=idx_all[:, ex, sc:sc + 1], axis=0),
                                                                 in_=eb[:], in_offset=None, compute_op=ALU.add),
                          r=["eoF%d" % (sc % 2), "idx_all"], w=["out_d"])
            T.barrier()
    for k, v in dbg_d.items():
        pass
    T.barrier()
    top.close()
    return nc


_CONST = None


def make_inputs(inp, b):
    global _CONST
    if _CONST is None:
        _CONST = host_consts()
    f = lambda a: np.ascontiguousarray(np.asarray(a, dtype=np.float32))
    m = dict(_CONST)
    m["x"] = f(inp["x"][b])
    m["g1bc"] = f(np.broadcast_to(inp["norm1_g"][0][None, :], (128, D)))
    m["w_in"] = f(inp["w_in"][0])
    scw = np.concatenate([inp["short_conv_w"][0], inp["short_conv_b"][0][None, :]], axis=0)
    m["scw"] = f(scw.reshape(4, 12, 128).transpose(2, 1, 0))
    qkg = np.stack([inp["q_norm_g"][0], inp["k_norm_g"][0]], axis=0)
    m["qkg"] = f(np.broadcast_to(qkg[None], (128, 2, 64)))
    m["lamv"] = f(np.stack([inp["lambda_q1"][0], inp["lambda_k1"][0], inp["lambda_q2"][0], inp["lambda_k2"][0]])[None])
    m["subg"] = f(np.broadcast_to(inp["subln_g"][0][None, :], (128, 128)))
    m["fw1"] = f(inp["filt_w1"][0])
    m["fw2"] = f(inp["filt_w2"][0])
    m["fw3"] = f(inp["filt_w3"][0])
    m["fbf"] = f(np.stack([inp["filt_b1"][0], inp["filt_b2"][0], inp["filt_b3"][0], inp["filt_freq"][0]], axis=1))
    m["fwo"] = f(inp["filt_w_out"][0])
    m["skipbc"] = f(np.broadcast_to(inp["hyena_skip"][0][None], (32, 2, 512)))
    m["wpa"] = f(inp["w_branch_attn"][0])
    m["wph"] = f(inp["w_branch_hyena"][0])
    m["wo"] = f(inp["w_out"][0])
    m["g2bc"] = f(np.broadcast_to(inp["norm2_g"][0][None, :], (128, D)))
    m["wr"] = f(inp["w_router"][0].reshape(8, 128, 16).transpose(1, 0, 2))
    m["wg"] = f(inp["w_gate"][0])
    m["wu"] = f(inp["w_up"][0])
    m["wd"] = f(inp["w_down"][0])
    return m


def kernel(**inputs):
    nc = build()
    maps = [make_inputs(inputs, c % 4) for c in range(4)]
    in_maps = [maps[c % 4] for c in range(NCORES)]
    res = run_bass_kernel_spmd(nc, in_maps, core_ids=list(range(NCORES)))
    out = np.stack([np.asarray(res.results[b]["out"], dtype=np.float32) for b in range(4)], axis=0)
    return out
```

```python
import math
from contextlib import ExitStack
import numpy as np
import ml_dtypes
import concourse.bass as bass
import concourse.mybir as mybir
from concourse.bass_utils import run_bass_kernel_spmd

F32 = mybir.dt.float32
BF16 = mybir.dt.bfloat16
I32 = mybir.dt.int32
AF = mybir.ActivationFunctionType
ALU = mybir.AluOpType
AX = mybir.AxisListType

L = 4096
D = 1024
NT = 32
NCORES = 8
CAP = 512
NE = 16
GC = 32
NK = 33
DEBUG = False


class Trk:
    def __init__(s, nc):
        s.nc = nc
        s.eng = dict(pe=nc.tensor, act=nc.scalar, dve=nc.vector, pool=nc.gpsimd, sp=nc.sync)
        s.sem = {}
        s.cnt = {}
        for k in ("pe", "act", "dve", "pool"):
            s.sem[k] = nc.alloc_semaphore(name="s_" + k)
            s.cnt[k] = 0
        s.ND = 16
        for i in range(s.ND):
            k = "d%d" % i
            s.sem[k] = nc.alloc_semaphore(name="s_" + k)
            s.cnt[k] = 0
        s.pool_of = {"sp": list(range(0, 8)), "pool": list(range(8, 16))}
        s.rrq = {"sp": 0, "pool": 0}
        s.seen = {e: {} for e in s.eng}
        s.lw = {}
        s.rd = {}

    def _wait(s, e, deps):
        for k, v in deps.items():
            if e == "pe" and k == "pe":
                continue
            if s.seen[e].get(k, 0) < v:
                s.eng[e].wait_ge(s.sem[k], v)
                s.seen[e][k] = v

    def _deps(s, r, w):
        d = {}

        def add(k, v):
            if d.get(k, 0) < v:
                d[k] = v
        for x in r:
            for k, v in s.lw.get(x, {}).items():
                add(k, v)
        for x in w:
            for k, v in s.lw.get(x, {}).items():
                add(k, v)
            for k, v in s.rd.get(x, {}).items():
                add(k, v)
        return d

    def _upd(s, toks, r, w):
        if isinstance(toks, tuple):
            toks = [toks]
        for x in r:
            m = s.rd.setdefault(x, {})
            for tok in toks:
                if m.get(tok[0], 0) < tok[1]:
                    m[tok[0]] = tok[1]
        for x in w:
            m = {}
            for tok in toks:
                if m.get(tok[0], 0) < tok[1]:
                    m[tok[0]] = tok[1]
            s.lw[x] = m
            s.rd[x] = {}

    def op(s, e, fn, r=(), w=()):
        s._wait(e, s._deps(r, w))
        ins = fn(s.eng[e])
        s.cnt[e] += 1
        ins.then_inc(s.sem[e], 1)
        s._upd((e, s.cnt[e]), r, w)

    def _next_sem(s, q):
        pool = s.pool_of[q]
        k = "d%d" % pool[s.rrq[q] % len(pool)]
        s.rrq[q] += 1
        if s.cnt[k] > 0:
            s._wait(q, {k: s.cnt[k]})
        return k

    def dma(s, q, fn, r=(), w=()):
        s._wait(q, s._deps(r, w))
        k = s._next_sem(q)
        ins = fn(s.eng[q])
        s.cnt[k] += 16
        ins.then_inc(s.sem[k], 16)
        s._upd((k, s.cnt[k]), r, w)

    def dmas(s, q, fns, r=(), w=()):
        s._wait(q, s._deps(r, w))
        toks = []
        for fn in fns:
            k = s._next_sem(q)
            ins = fn(s.eng[q])
            s.cnt[k] += 16
            ins.then_inc(s.sem[k], 16)
            toks.append((k, s.cnt[k]))
        s._upd(toks, r, w)

    def barrier(s, engines=None):
        tot = {k: v for k, v in s.cnt.items() if v > 0}
        for e in (engines or s.eng):
            s._wait(e, dict(tot))
        s.lw = {}
        s.rd = {}


def _bf(a):
    return np.ascontiguousarray(a.astype(np.float32)).astype(ml_dtypes.bfloat16)


def host_consts():
    c = {}
    t = np.arange(L, dtype=np.float32)
    inv_freq = (np.float32(500000.0) ** (-np.arange(0, 16, 2, dtype=np.float32) / np.float32(16))).astype(np.float32)
    ang = (t[:, None] * inv_freq[None, :]).astype(np.float32)
    cs = np.concatenate([np.cos(ang), np.sin(ang)], axis=-1).astype(np.float32)
    c["ropecs"] = np.ascontiguousarray(cs.reshape(NT, 128, 16).transpose(1, 0, 2))
    tt = t / np.float32(L - 1)
    bands = np.linspace(1e-4, 15, 16, dtype=np.float32)
    a2 = (np.float32(2.0 * math.pi / L) * t[:, None] * bands[None, :]).astype(np.float32)
    emb = np.concatenate([tt[:, None], np.cos(a2), -np.sin(a2)], axis=-1).astype(np.float32)
    c["embT"] = np.ascontiguousarray(emb.T)
    c["posrow"] = np.ascontiguousarray(np.broadcast_to(tt[None, :], (128, L))).astype(np.float32)
    min_decay = math.log(1e-2) / 1.5
    max_decay = math.log(1e-2) / 0.3
    deltas = np.abs(np.linspace(min_decay, max_decay, 512, dtype=np.float32))
    c["ndelta"] = np.ascontiguousarray((-deltas).reshape(4, 128).T).astype(np.float32)
    n1 = np.arange(32)[:, None]
    k1 = np.arange(NK)[None, :]
    th = 2 * np.pi * n1 * k1 / 64.0
    c["FA"] = _bf(np.concatenate([np.cos(th), -np.sin(th)], axis=1))
    n2 = np.arange(128)[:, None, None]
    k1 = np.arange(NK)[None, :, None]
    k2 = np.arange(128)[None, None, :]
    th = 2 * np.pi * ((n2 * (k1 + 64 * k2)) % 8192) / 8192.0
    c["TGr"] = _bf(np.cos(th))
    c["TGi"] = _bf(-np.sin(th))
    c["TGn"] = _bf(np.sin(th))
    kk2 = np.arange(128)[:, None]
    nn2 = np.arange(128)[None, :]
    th = 2 * np.pi * ((kk2 * nn2) % 128) / 128.0
    fr, fi = np.cos(th), np.sin(th)
    c["R1"] = _bf(np.concatenate([fr, fi], axis=1))
    c["R2"] = _bf(np.concatenate([-fi, fr], axis=1))
    k1 = np.arange(NK)[:, None, None]
    n2 = np.arange(128)[None, :, None]
    n1 = np.arange(32)[None, None, :]
    th = 2 * np.pi * ((k1 * (n2 + 128 * n1)) % 8192) / 8192.0
    coef = np.where((k1 == 0) | (k1 == 32), 1.0, 2.0)[:, :, None, :]
    th_ = np.stack([np.cos(th), -np.sin(th)], axis=2) * coef / 8192.0
    c["TH"] = _bf(th_)
    tid = np.arange(L).reshape(NT, 128).T
    c["tidhl"] = _bf(np.stack([tid // 64, tid % 64], axis=-1))
    c["iota512"] = np.ascontiguousarray(np.broadcast_to(np.arange(512, dtype=np.float32)[None, :], (128, 512)))
    tri = (np.arange(128)[:, None] <= np.arange(128)[None, :]).astype(np.float32)
    c["tri"] = tri
    return c


def build(dbg=None):
    nc = bass.Bass("TRN2", target_bir_lowering=False)
    dbg = dbg or {}

    def din(name, shape, dt=F32):
        return nc.dram_tensor(name, list(shape), dt, kind="ExternalInput").ap()

    def dscr(name, shape, dt=F32):
        kind = "ExternalOutput" if name in dbg.get("_ext", ()) else "Internal"
        return nc.dram_tensor(name, list(shape), dt, kind=kind).ap()

    x_d = din("x", [L, D])
    g1bc_d = din("g1bc", [128, D])
    w_in_d = din("w_in", [D, 5120])
    scw_d = din("scw", [128, 12, 4])
    qkg_d = din("qkg", [128, 2, 64])
    lam_d = din("lamv", [1, 4, 64])
    subg_d = din("subg", [128, 128])
    fw1_d = din("fw1", [33, 64])
    fw2_d = din("fw2", [64, 64])
    fw3_d = din("fw3", [64, 64])
    fbf_d = din("fbf", [64, 4])
    fwo_d = din("fwo", [64, 2048])
    skip_d = din("skipbc", [32, 2, 512])
    wpa_d = din("wpa", [512, D])
    wph_d = din("wph", [512, D])
    wo_d = din("wo", [D, D])
    g2bc_d = din("g2bc", [128, D])
    wr_d = din("wr", [128, 8, 16])
    wg_d = din("wg", [NE, D, D])
    wu_d = din("wu", [NE, D, D])
    wd_d = din("wd", [NE, D, D])
    ropecs_d = din("ropecs", [128, NT, 16])
    embT_d = din("embT", [33, L])
    posrow_d = din("posrow", [128, L])
    ndelta_d = din("ndelta", [128, 4])
    FA_d = din("FA", [32, 2 * NK], BF16)
    TGr_d = din("TGr", [128, NK, 128], BF16)
    TGi_d = din("TGi", [128, NK, 128], BF16)
    TGn_d = din("TGn", [128, NK, 128], BF16)
    R1_d = din("R1", [128, 256], BF16)
    R2_d = din("R2", [128, 256], BF16)
    TH_d = din("TH", [NK, 128, 2, 32], BF16)
    tidhl_d = din("tidhl", [128, NT, 2], BF16)
    iota_d = din("iota512", [128, 512])
    tri_d = din("tri", [128, 128])

    out_d = nc.dram_tensor("out", [L, D], F32, kind="ExternalOutput").ap()

    qT_d = dscr("qT_s", [4, 128, L], BF16)
    kT_d = dscr("kT_s", [4, 128, L], BF16)
    v_d = dscr("v_s", [L, 512], BF16)
    hyc_d = dscr("hyc_s", [1536, L], F32)
    gateT_d = dscr("gateT_s", [2048, L], BF16)
    kern_d = dscr("kern_s", [2, 2, 512, L], BF16)
    attnT_d = dscr("attnT_s", [512, L], BF16)
    hyoT_d = dscr("hyoT_s", [512, L], BF16)
    u2_d = dscr("u2_s", [L, D], BF16)

    dbg_d = {}
    for k, (shape, dt) in ((k, v) for k, v in dbg.items() if not k.startswith("_")):
        dbg_d[k] = nc.dram_tensor("dbg_" + k, list(shape), dt, kind="ExternalOutput").ap()

    T = Trk(nc)
    top = ExitStack()

    sbc = [0]

    def sb(es, name, shape, dt=F32):
        sbc[0] += 1
        return es.enter_context(nc.sbuf_tensor("sb%d_%s" % (sbc[0], name), list(shape), dt))

    ps = [top.enter_context(nc.psum_tensor("ps%d" % i, [128, 512], F32)) for i in range(8)]
    PK = ["ps%d" % i for i in range(8)]

    ident_f = sb(top, "ident_f", [128, 128])
    ident_b = sb(top, "ident_b", [128, 128], BF16)
    eps = sb(top, "eps", [128, 1])
    T.op("pool", lambda e: e.memset(ident_f[:], 0.0), w=["ident_f"])
    T.op("pool", lambda e: e.affine_select(out=ident_f[:], in_=ident_f[:], pattern=[[-1, 128]],
                                           compare_op=ALU.not_equal, fill=1.0, base=0, channel_multiplier=1),
         r=["ident_f"], w=["ident_f"])
    T.op("pool", lambda e: e.tensor_copy(out=ident_b[:], in_=ident_f[:]), r=["ident_f"], w=["ident_b"])
    T.op("pool", lambda e: e.memset(eps[:], 1e-6), w=["eps"])

    stages = dbg.get("_stages", "AKBCDEF") if isinstance(dbg.get("_stages", None), str) else "AKBCDEF"

    if "A" in stages:
      with ExitStack() as es:
        uT = sb(es, "uT", [128, 8, L + 2], BF16)
        g1bc = sb(es, "g1bc", [128, D])
        scw = sb(es, "scw", [128, 12, 4])
        qkg = sb(es, "qkg", [128, 2, 64])
        ropecs = sb(es, "ropecs", [128, NT, 16])
        xt = [sb(es, "xt%d" % i, [128, D]) for i in range(4)]
        xb = [sb(es, "xb%d" % i, [128, D], BF16) for i in range(4)]
        junk = sb(es, "junkA", [128, D])
        ss = sb(es, "ssA", [128, NT])
        rs = sb(es, "rsA", [128, NT])
        wb = [sb(es, "wb%d" % i, [128, 8, 512], BF16) for i in range(2)]
        T.dma("sp", lambda e: e.dma_start(out=g1bc[:], in_=g1bc_d[:, :]), w=["g1bc"])
        T.dma("sp", lambda e: e.dma_start(out=scw[:], in_=scw_d[:, :, :]), w=["scw"])
        T.dma("sp", lambda e: e.dma_start(out=qkg[:], in_=qkg_d[:, :, :]), w=["qkg"])
        T.dma("sp", lambda e: e.dma_start(out=ropecs[:], in_=ropecs_d[:, :, :]), w=["ropecs"])
        T.op("dve", lambda e: e.memset(ss[:], 0.0), w=["ssA"])
        T.op("dve", lambda e: e.memset(uT[:, :, 0:1], 0.0), w=["uTpad0"])
        T.op("dve", lambda e: e.memset(uT[:, :, L + 1:L + 2], 0.0), w=["uTpad1"])
        T.op("dve", lambda e: e.tensor_scalar(out=qkg[:, 0, :], in0=qkg[:, 0, :], scalar1=0.125, scalar2=None,
                                              op0=ALU.mult), r=["qkg"], w=["qkg"])
        psb = [ps[6][:, :].bitcast(BF16), ps[7][:, :].bitcast(BF16)]
        for i in range(NT):
            b = i % 4
            pb2 = i % 2
            T.dma("sp", lambda e: e.dma_start(out=xt[b][:], in_=x_d[i * 128:(i + 1) * 128, :]), w=["xt%d" % b])
            T.op("act", lambda e: e.activation(out=junk[:], in_=xt[b][:], func=AF.Square,
                                               accum_out=ss[:, i:i + 1]), r=["xt%d" % b, "ssA"], w=["junkA", "ss%d" % i])
            T.op("act", lambda e: e.activation(out=rs[:, i:i + 1], in_=ss[:, i:i + 1], func=AF.Sqrt,
                                               scale=1.0 / D, bias=eps[:]), r=["ss%d" % i, "eps"], w=["rs%d" % i])
            T.op("dve", lambda e: e.reciprocal(out=rs[:, i:i + 1], in_=rs[:, i:i + 1]), r=["rs%d" % i], w=["rs%d" % i])
            T.op("dve", lambda e: e.scalar_tensor_tensor(out=xb[b][:], in0=xt[b][:], scalar=rs[:, i:i + 1], in1=g1bc[:],
                                                         op0=ALU.mult, op1=ALU.mult),
                 r=["xt%d" % b, "rs%d" % i, "g1bc"], w=["xb%d" % b])
            for j in range(8):
                T.op("pe", lambda e: e.transpose(out=psb[pb2][:, j * 128:(j + 1) * 128],
                                                 in_=xb[b][:, j * 128:(j + 1) * 128], identity=ident_b[:]),
                     r=["xb%d" % b, "ident_b"], w=[PK[6 + pb2]])
            T.op("act", lambda e: e.copy(out=uT[:, :, 1 + i * 128:1 + (i + 1) * 128],
                                         in_=psb[pb2].rearrange("p (j t) -> p j t", j=8)),
                 r=[PK[6 + pb2]], w=["uT%d" % i])
        UTK = ["uT%d" % i for i in range(NT)] + ["uTpad0", "uTpad1"]

        sq = sb(es, "sqA", [128, 512])
        ssq = sb(es, "ssqA", [128, 8])
        qn = sb(es, "qnA", [128, 8, 64])
        qb2 = [sb(es, "qbA%d" % i, [128, 512], BF16) for i in range(2)]
        rt = [sb(es, "rtA%d" % i, [128, 8, 8]) for i in range(4)]
        qTs = [sb(es, "qTs%d" % i, [128, 4, 128], BF16) for i in range(2)]
        vst = [sb(es, "vst%d" % i, [128, 512], BF16) for i in range(2)]
        hraw = sb(es, "hraw", [128, L + 2])
        hc = sb(es, "hcA", [128, L])
        gst = sb(es, "gstA", [128, L], BF16)
        T.op("pool", lambda e: e.memset(hraw[:, 0:1], 0.0), w=["hrawp0"])
        T.op("pool", lambda e: e.memset(hraw[:, L + 1:L + 2], 0.0), w=["hrawp1"])
        pcnt = [0]

        def nextps():
            pcnt[0] += 1
            return pcnt[0] % 4

        for cc in dbg.get('_ccs', range(10)):
            wbk = "wb%d" % (cc % 2)
            wbt = wb[cc % 2]
            T.dmas("pool", [(lambda e, a=a: e.dma_start(
                out=wbt[:, a:a + 4, :], in_=w_in_d.rearrange("(j p) c -> p j c", p=128)[:, a:a + 4, cc * 512:(cc + 1) * 512]))
                for a in (0, 4)], w=[wbk])
            if cc < 3:
                def qk_tail(i):
                    qbt = qb2[i % 2]
                    pb = 6 + (i % 2)
                    for h in range(4):
                        T.op("pe", lambda e: e.transpose(out=psb[i % 2][:, h * 128:(h + 1) * 128],
                                                         in_=qbt[:, h * 128:(h + 1) * 128], identity=ident_b[:]),
                             r=["qbA%d" % (i % 2), "ident_b"], w=[PK[pb]])
                    st = qTs[i % 2]
                    T.op("act", lambda e: e.copy(out=st[:], in_=psb[i % 2][:, 0:512].rearrange("p (h t) -> p h t", h=4)),
                         r=[PK[pb]], w=["qTs%d" % (i % 2)])
                    dst = (qT_d if cc == 0 else kT_d).rearrange("h p t -> p h t")[:, :, i * 128:(i + 1) * 128]
                    T.dma("sp", lambda e: e.dma_start(out=dst, in_=st[:]), r=["qTs%d" % (i % 2)], w=["qkT_d"])

                for i in range(NT):
                    pi = nextps()
                    for j in range(8):
                        T.op("pe", lambda e: e.matmul(ps[pi][:, :], lhsT=uT[:, j, 1 + i * 128:1 + (i + 1) * 128],
                                                      rhs=wbt[:, j, :], start=(j == 0), stop=(j == 7)),
                             r=["uT%d" % i, wbk], w=[PK[pi]])
                    if cc == 2:
                        vs = vst[i % 2]
                        T.op("act", lambda e: e.copy(out=vs[:], in_=ps[pi][:, :]), r=[PK[pi]], w=["vst%d" % (i % 2)])
                        T.dma("sp", lambda e: e.dma_start(out=v_d[i * 128:(i + 1) * 128, :], in_=vs[:]),
                              r=["vst%d" % (i % 2)], w=["v_d"])
                        continue
                    if i > 0:
                        qk_tail(i - 1)
                    qb = qb2[i % 2]
                    qbk = "qbA%d" % (i % 2)
                    T.op("act", lambda e: e.activation(out=sq[:], in_=ps[pi][:, :], func=AF.Square), r=[PK[pi]], w=["sqA"])
                    T.op("dve", lambda e: e.tensor_reduce(out=ssq[:], in_=sq[:].rearrange("p (g d) -> p g d", d=64),
                                                          axis=AX.X, op=ALU.add), r=["sqA"], w=["ssqA"])
                    T.op("act", lambda e: e.activation(out=ssq[:], in_=ssq[:], func=AF.Sqrt, scale=1.0 / 64,
                                                       bias=eps[:]), r=["ssqA", "eps"], w=["ssqA"])
                    T.op("dve", lambda e: e.reciprocal(out=ssq[:], in_=ssq[:]), r=["ssqA"], w=["ssqA"])
                    T.op("dve", lambda e: e.tensor_tensor(out=qn[:], in0=ps[pi][:, :].rearrange("p (g d) -> p g d", d=64),
                                                          in1=ssq[:, :].unsqueeze(2).broadcast_to([128, 8, 64]),
                                                          op=ALU.mult), r=[PK[pi], "ssqA"], w=["qnA"])
                    T.op("dve", lambda e: e.tensor_tensor(out=qn[:], in0=qn[:],
                                                          in1=qkg[:, cc, :].unsqueeze(1).broadcast_to([128, 8, 64]),
                                                          op=ALU.mult), r=["qnA", "qkg"], w=["qnA"])
                    T.op("act", lambda e: e.copy(out=qb[:].rearrange("p (g d) -> p g d", d=64), in_=qn[:]),
                         r=["qnA"], w=[qbk])
                    cosb = ropecs[:, i, 0:8].unsqueeze(1).broadcast_to([128, 8, 8])
                    sinb = ropecs[:, i, 8:16].unsqueeze(1).broadcast_to([128, 8, 8])
                    r1 = qn[:, :, 0:8]
                    r2 = qn[:, :, 8:16]
                    T.op("dve", lambda e: e.tensor_tensor(out=rt[0][:], in0=r1, in1=cosb, op=ALU.mult), r=["qnA", "ropecs"], w=["rt0"])
                    T.op("dve", lambda e: e.tensor_tensor(out=rt[1][:], in0=r2, in1=sinb, op=ALU.mult), r=["qnA", "ropecs"], w=["rt1"])
                    T.op("dve", lambda e: e.tensor_tensor(out=rt[2][:], in0=r2, in1=cosb, op=ALU.mult), r=["qnA", "ropecs"], w=["rt2"])
                    T.op("dve", lambda e: e.tensor_tensor(out=rt[3][:], in0=r1, in1=sinb, op=ALU.mult), r=["qnA", "ropecs"], w=["rt3"])
                    qbv = qb[:].rearrange("p (g d) -> p g d", d=64)
                    T.op("dve", lambda e: e.tensor_tensor(out=qbv[:, :, 0:8], in0=rt[0][:], in1=rt[1][:], op=ALU.subtract),
                         r=["rt0", "rt1"], w=[qbk])
                    T.op("dve", lambda e: e.tensor_tensor(out=qbv[:, :, 8:16], in0=rt[2][:], in1=rt[3][:], op=ALU.add),
                         r=["rt2", "rt3"], w=[qbk])
                if cc < 2:
                    qk_tail(NT - 1)
            else:
                for ct in range(4):
                    for tq in range(8):
                        pi = nextps()
                        for j in range(8):
                            T.op("pe", lambda e: e.matmul(ps[pi][:, :], lhsT=wbt[:, j, ct * 128:(ct + 1) * 128],
                                                          rhs=uT[:, j, 1 + tq * 512:1 + (tq + 1) * 512],
                                                          start=(j == 0), stop=(j == 7)),
                                 r=UTK[4 * tq:4 * tq + 4] + [wbk], w=[PK[pi]])
                        if cc < 6:
                            T.op("act", lambda e: e.copy(out=hraw[:, 1 + tq * 512:1 + (tq + 1) * 512], in_=ps[pi][:, :]),
                                 r=[PK[pi]], w=["hraw"])
                        else:
                            T.op("act", lambda e: e.activation(out=gst[:, tq * 512:(tq + 1) * 512], in_=ps[pi][:, :],
                                                               func=AF.Sigmoid), r=[PK[pi]], w=["gstA"])
                    if cc < 6:
                        cti = (cc - 3) * 4 + ct
                        lvl = dbg.get('_lvl', 9)
                        if lvl >= 1:
                          T.op("act", lambda e: e.activation(out=hc[:], in_=hraw[:, 1:L + 1], func=AF.Identity,
                                                           scale=scw[:, cti, 1:2], bias=scw[:, cti, 3:4]),
                             r=["hraw", "scw"], w=["hcA"])
                        if lvl >= 2:
                          T.op("dve", lambda e: e.scalar_tensor_tensor(out=hc[:], in0=hraw[:, 0:L], scalar=scw[:, cti, 0:1],
                                                                     in1=hc[:], op0=ALU.mult, op1=ALU.add),
                             r=["hraw", "hrawp0", "scw", "hcA"], w=["hcA"])
                          T.op("dve", lambda e: e.scalar_tensor_tensor(out=hc[:], in0=hraw[:, 2:L + 2], scalar=scw[:, cti, 2:3],
                                                                     in1=hc[:], op0=ALU.mult, op1=ALU.add),
                             r=["hraw", "hrawp1", "scw", "hcA"], w=["hcA"])
                        if lvl >= 3:
                          for q4 in range(4):
                              T.dma("sp", lambda e: e.dma_start(out=hyc_d[cti * 128:(cti + 1) * 128, q4 * 1024:(q4 + 1) * 1024],
                                                                in_=hc[:, q4 * 1024:(q4 + 1) * 1024]),
                                    r=["hcA"], w=["hyc_d%d" % q4])
                    else:
                        gti = (cc - 6) * 4 + ct
                        T.dmas("sp", [(lambda e, a=a: e.dma_start(out=gateT_d[gti * 128:(gti + 1) * 128, a:a + 2048], in_=gst[:, a:a + 2048]))
                                      for a in (0, 2048)], r=["gstA"], w=["gateT_d"])
        T.barrier()

    if "K" in stages:
      with ExitStack() as es:
        embT = sb(es, "embT", [33, L])
        fw1 = sb(es, "fw1", [33, 64])
        fw2 = sb(es, "fw2", [64, 64])
        fw3 = sb(es, "fw3", [64, 64])
        fbf = sb(es, "fbf", [64, 4])
        frb = sb(es, "frb", [64, 3])
        fwo = sb(es, "fwo", [64, 2048])
        posrow = sb(es, "posrow", [128, L])
        ndelta = sb(es, "ndelta", [128, 4])
        hid = [sb(es, "hid%d" % i, [64, L]) for i in range(2)]
        hidb = sb(es, "hidb", [64, L])
        dec = sb(es, "decK", [128, L])
        hpm = [[sb(es, "hpmK%d%d" % (o, pm), [128, L], BF16) for pm in range(2)] for o in range(2)]
        wpm = sb(es, "wpmK", [64, 2, 2, 512])
        for tns, src, k in ((fw1, fw1_d, "fw1"), (fw2, fw2_d, "fw2"), (fw3, fw3_d, "fw3"),
                            (fbf, fbf_d, "fbf"), (ndelta, ndelta_d, "ndelta")):
            T.dma("sp", lambda e: e.dma_start(out=tns[:], in_=src), w=[k])
        for tns, src, k, n in ((embT, embT_d, "embT", L), (fwo, fwo_d, "fwo", 2048), (posrow, posrow_d, "posrow", L)):
            T.dmas("sp", [(lambda e, a=a: e.dma_start(out=tns[:, a:a + 1024], in_=src[:, a:a + 1024]))
                          for a in range(0, n, 1024)], w=[k])
        for l in range(3):
            T.op("dve", lambda e: e.tensor_tensor(out=frb[:, l:l + 1], in0=fbf[:, l:l + 1], in1=fbf[:, 3:4], op=ALU.mult),
                 r=["fbf"], w=["frb"])
        TWO_PI = 2.0 * math.pi
        srcs = [(embT, "embT", 33, fw1, "fw1"), (hid[0], "hid0", 64, fw2, "fw2"), (hid[1], "hid1", 64, fw3, "fw3")]
        outs = [(hid[0], "hid0"), (hid[1], "hid1"), (hid[0], "hid0")]
        negpi = sb(es, "negpi", [64, 1])
        T.op("dve", lambda e: e.memset(negpi[:], 4.0 * math.pi), w=["negpi"])
        kcnt = sb(es, "kcntK", [64, L])
        for l in range(3):
            src, sk, kk, w_, wk = srcs[l]
            dst, dk = outs[l]
            for tq in range(8):
                pi = tq % 4
                T.op("pe", lambda e: e.matmul(ps[pi][0:64, :], lhsT=w_[0:kk, :], rhs=src[0:kk, tq * 512:(tq + 1) * 512],
                                              start=True, stop=True), r=[sk, wk], w=[PK[pi]])
                T.op("dve", lambda e: e.tensor_scalar(out=hidb[:, tq * 512:(tq + 1) * 512], in0=ps[pi][0:64, :],
                                                      scalar1=fbf[:, 3:4], scalar2=frb[:, l:l + 1], op0=ALU.mult,
                                                      op1=ALU.add), r=[PK[pi], "fbf", "frb"], w=["hidb"])
                xs_ = hidb[:, tq * 512:(tq + 1) * 512]
                ks_ = kcnt[:, tq * 512:(tq + 1) * 512]
                T.op("dve", lambda e: e.tensor_scalar(out=ks_, in0=xs_, scalar1=-3.0 * math.pi, scalar2=None, op0=ALU.is_gt),
                     r=["hidb"], w=["kcnt"])
                for thr in (-math.pi, math.pi, 3.0 * math.pi):
                    T.op("dve", lambda e: e.scalar_tensor_tensor(out=ks_, in0=xs_, scalar=thr, in1=ks_, op0=ALU.is_gt, op1=ALU.add),
                         r=["hidb", "kcnt"], w=["kcnt"])
                T.op("dve", lambda e: e.scalar_tensor_tensor(out=xs_, in0=ks_, scalar=-TWO_PI, in1=xs_, op0=ALU.mult, op1=ALU.add),
                     r=["hidb", "kcnt"], w=["hidb"])
                T.op("act", lambda e: e.activation(out=dst[:, tq * 512:(tq + 1) * 512], in_=xs_,
                                                   func=AF.Sin, bias=negpi[:]), r=["hidb", "negpi"], w=[dk])
        h3 = hid[0]
        for o in range(2):
            wf_ = fwo[:, (o * 2) * 512:(o * 2 + 1) * 512]
            wb_ = fwo[:, (o * 2 + 1) * 512:(o * 2 + 2) * 512]
            T.op("dve", lambda e: e.tensor_tensor(out=wpm[:, o, 0, :], in0=wf_, in1=wb_, op=ALU.add), r=["fwo"], w=["wpmK"])
            T.op("dve", lambda e: e.tensor_tensor(out=wpm[:, o, 1, :], in0=wf_, in1=wb_, op=ALU.subtract), r=["fwo"], w=["wpmK"])
        for ct in range(4):
            T.op("act", lambda e: e.activation(out=dec[:], in_=posrow[:], func=AF.Exp, scale=ndelta[:, ct:ct + 1]),
                 r=["posrow", "ndelta"], w=["decK"])
            for o in range(2):
                for pm in range(2):
                    dst = hpm[o][pm]
                    dk = "hpmK%d%d" % (o, pm)
                    for tq in range(8):
                        pi = tq % 4
                        T.op("pe", lambda e: e.matmul(ps[pi][:, :], lhsT=wpm[:, o, pm, ct * 128:(ct + 1) * 128],
                                                      rhs=h3[:, tq * 512:(tq + 1) * 512], start=True, stop=True),
                             r=["hid0", "wpmK"], w=[PK[pi]])
                        T.op("dve", lambda e: e.tensor_tensor(out=dst[:, tq * 512:(tq + 1) * 512], in0=ps[pi][:, :],
                                                              in1=dec[:, tq * 512:(tq + 1) * 512], op=ALU.mult),
                             r=[PK[pi], "decK"], w=[dk])
                colf = (o * 2) * 512 + ct * 128
                T.op("pe", lambda e: e.matmul(ps[4][:, 0:1], lhsT=fwo[:, colf:colf + 128], rhs=h3[:, 0:1], start=True, stop=True),
                     r=["hid0", "fwo"], w=[PK[4]])
                for pm in range(2):
                    T.op("dve", lambda e: e.tensor_tensor(out=hpm[o][pm][:, 0:1], in0=ps[4][:, 0:1], in1=dec[:, 0:1], op=ALU.mult),
                         r=[PK[4], "decK"], w=["hpmK%d%d" % (o, pm)])
                for pm in range(2):
                    T.dmas("sp", [(lambda e, a=a: e.dma_start(out=kern_d[o, pm, ct * 128:(ct + 1) * 128, a:a + 2048],
                                                              in_=hpm[o][pm][:, a:a + 2048]))
                                  for a in range(0, L, 2048)], r=["hpmK%d%d" % (o, pm)], w=["kern_d%d%d" % (o, pm)])
        T.barrier()

    if "B" in stages:
      with ExitStack() as es:
        qT = sb(es, "qT", [128, 4, L], BF16)
        kT = sb(es, "kT", [128, 2, 4, L], BF16)
        V = sb(es, "Vsb", [128, NT, 4, 132], BF16)
        lamv = sb(es, "lamv", [1, 4, 64])
        lamt = sb(es, "lamt", [1, 8])
        ones1 = sb(es, "ones1", [1, 128])
        nlam = sb(es, "nlam", [128, 1])
        subg = sb(es, "subg", [128, 128])
        E = [sb(es, "E%d" % i, [128, 512], BF16) for i in range(3)]
        osb = sb(es, "osbB", [128, 2, 4, 129])
        aacc = sb(es, "aacc", [128, 4, 128])
        ab = sb(es, "abB", [128, 4, 128], BF16)
        junkb = sb(es, "junkB", [128, 4, 128])
        sm = sb(es, "smB", [128, 8, 4])
        aTs = [sb(es, "aTs%d" % i, [128, 512], BF16) for i in range(2)]
        T.dmas("sp", [(lambda e, h=h, a=a: e.dma_start(out=qT[:, h, a:a + 2048], in_=qT_d[h, :, a:a + 2048]))
                      for h in range(4) for a in (0, 2048)], w=["qT"])
        T.op("pool", lambda e: e.memset(kT[64:128, 0, :, :], 0.0), w=["kTz0"])
        T.op("dve", lambda e: e.memset(kT[0:64, 1, :, :], 0.0), w=["kTz1"])
        T.dmas("sp", [(lambda e, h=h, a=a, m=m: e.dma_start(out=kT[m * 64:(m + 1) * 64, m, h, a:a + 2048],
                                                            in_=kT_d[h, m * 64:(m + 1) * 64, a:a + 2048]))
                      for m in range(2) for h in range(4) for a in (0, 2048)], w=["kT"])
        T.dmas("sp", [(lambda e, i=i: e.dma_start(out=V[:, i, :, 0:128],
                                                  in_=v_d[i * 128:(i + 1) * 128, :].rearrange("p (h d) -> p h d", d=128)))
                      for i in range(NT)], w=["V"])
        T.op("pool", lambda e: e.memset(V[:, :, :, 128:129], 1.0), w=["Vones"])
        T.dma("sp", lambda e: e.dma_start(out=lamv[:], in_=lam_d[:, :, :]), w=["lamv"])
        T.dma("sp", lambda e: e.dma_start(out=subg[:], in_=subg_d[:, :]), w=["subg"])
        T.op("dve", lambda e: e.tensor_scalar(out=subg[:], in0=subg[:], scalar1=0.8, scalar2=None, op0=ALU.mult),
             r=["subg"], w=["subg"])
        T.op("dve", lambda e: e.memset(ones1[:], 1.0), w=["ones1"])
        T.op("dve", lambda e: e.memset(lamt[:], 0.0), w=["lamt"])
        T.op("dve", lambda e: e.tensor_tensor(out=lamv[:, 0, :], in0=lamv[:, 0, :], in1=lamv[:, 1, :], op=ALU.mult), r=["lamv"], w=["lamv"])
        T.op("dve", lambda e: e.tensor_tensor(out=lamv[:, 2, :], in0=lamv[:, 2, :], in1=lamv[:, 3, :], op=ALU.mult), r=["lamv"], w=["lamv"])
        T.op("dve", lambda e: e.tensor_reduce(out=lamt[:, 0:1], in_=lamv[:, 0, :], axis=AX.X, op=ALU.add), r=["lamv", "lamt"], w=["lamt"])
        T.op("dve", lambda e: e.tensor_reduce(out=lamt[:, 1:2], in_=lamv[:, 2, :], axis=AX.X, op=ALU.add), r=["lamv", "lamt"], w=["lamt"])
        T.op("act", lambda e: e.activation(out=lamt[:, 2:4], in_=lamt[:, 0:2], func=AF.Exp), r=["lamt"], w=["lamt"])
        T.op("dve", lambda e: e.tensor_tensor(out=lamt[:, 4:5], in0=lamt[:, 3:4], in1=lamt[:, 2:3], op=ALU.subtract), r=["lamt"], w=["lamt"])
        T.op("dve", lambda e: e.tensor_scalar(out=lamt[:, 5:6], in0=lamt[:, 4:5], scalar1=-0.2, scalar2=None, op0=ALU.add), r=["lamt"], w=["lamt"])
        T.op("pe", lambda e: e.matmul(ps[7][:, 0:1], lhsT=ones1[:, :], rhs=lamt[:, 5:6], start=True, stop=True),
             r=["ones1", "lamt"], w=[PK[7]])
        T.op("dve", lambda e: e.tensor_copy(out=nlam[:], in_=ps[7][:, 0:1]), r=[PK[7]], w=["nlam"])
        psT = ps[6][:, :].bitcast(BF16)
        steps = [(h, qc, m, kt) for h in range(4) for qc in range(8) for m in range(2) for kt in range(NT)]
        USE_POW = False

        def issue_S(idx):
            h, qc, m, kt = steps[idx]
            sp_ = idx % 2
            eb = idx % 3
            T.op("pe", lambda e: e.matmul(ps[sp_][:, :], lhsT=kT[:, m, h, kt * 128:(kt + 1) * 128],
                                          rhs=qT[:, h, qc * 512:(qc + 1) * 512], start=True, stop=True),
                 r=["kT", "kTz0", "kTz1", "qT"], w=[PK[sp_]])
            T.op("act", lambda e: e.activation(out=E[eb][:], in_=ps[sp_][:, :], func=AF.Exp),
                 r=[PK[sp_]], w=["E%d" % eb])

        def issue_PV(idx):
            h, qc, m, kt = steps[idx]
            eb = idx % 3
            for qs in range(4):
                bank = 2 + m * 2 + qs // 2
                T.op("pe", lambda e: e.matmul(ps[bank][:, (qs % 2) * 256:(qs % 2) * 256 + 129],
                                              lhsT=E[eb][:, qs * 128:(qs + 1) * 128], rhs=V[:, kt, h, 0:129],
                                              start=(kt == 0 and qs % 2 == 0), stop=(kt == NT - 1),
                                              skip_group_check=True),
                     r=["E%d" % eb, "V", "Vones"], w=[PK[bank]])

        def bc4(ap2):
            return ap2.unsqueeze(2).broadcast_to([128, 4, 128])

        def epi_head():
            for m in range(2):
                for half in range(2):
                    bank = 2 + m * 2 + half
                    src = ps[bank][:, :].rearrange("p (a c) -> p a c", a=2)[:, :, 0:129]
                    dst = osb[:, m, half * 2:(half + 1) * 2, :]
                    if half == 0:
                        T.op("dve", lambda e: e.tensor_copy(out=dst, in_=src), r=[PK[bank]], w=["osb%d%d" % (m, half)])
                    else:
                        T.op("pool" if False else "dve", lambda e: e.tensor_copy(out=dst, in_=src), r=[PK[bank]], w=["osb%d%d" % (m, half)])
            OS = ["osb00", "osb01", "osb10", "osb11"]
            T.op("dve", lambda e: e.reciprocal(out=sm[:, 0:2, :], in_=osb[:, :, :, 128]), r=OS, w=["smB"])
            T.op("dve", lambda e: e.tensor_scalar(out=sm[:, 2, :], in0=sm[:, 1, :], scalar1=nlam[:, 0:1], scalar2=None, op0=ALU.mult),
                 r=["smB", "nlam"], w=["smB"])
            T.op("dve", lambda e: e.tensor_tensor(out=aacc[:], in0=osb[:, 0, :, 0:128], in1=bc4(sm[:, 0, :]), op=ALU.mult),
                 r=OS + ["smB"], w=["aacc"])
            T.op("dve", lambda e: e.tensor_tensor(out=junkb[:], in0=osb[:, 1, :, 0:128], in1=bc4(sm[:, 2, :]), op=ALU.mult),
                 r=OS + ["smB"], w=["junkB"])
            T.op("dve", lambda e: e.tensor_tensor(out=aacc[:], in0=aacc[:], in1=junkb[:], op=ALU.add), r=["aacc", "junkB"], w=["aacc"])
            T.op("dve", lambda e: e.tensor_tensor(out=junkb[:], in0=aacc[:], in1=aacc[:], op=ALU.mult), r=["aacc"], w=["junkB"])
            T.op("dve", lambda e: e.tensor_reduce(out=sm[:, 3, :], in_=junkb[:], axis=AX.X, op=ALU.add), r=["junkB", "smB"], w=["smB"])
            T.op("dve", lambda e: e.tensor_scalar(out=sm[:, 4, :], in0=sm[:, 3, :], scalar1=1.0 / 128, scalar2=1e-6, op0=ALU.mult, op1=ALU.add),
                 r=["smB"], w=["smB"])
            if USE_POW:
                T.op("dve", lambda e: e.tensor_scalar(out=sm[:, 5, :], in0=sm[:, 4, :], scalar1=-0.5, scalar2=None, op0=ALU.pow),
                     r=["smB"], w=["smB"])
            else:
                T.op("act", lambda e: e.activation(out=sm[:, 6, :], in_=sm[:, 4, :], func=AF.Sqrt), r=["smB"], w=["smB"])
                T.op("dve", lambda e: e.reciprocal(out=sm[:, 5, :], in_=sm[:, 6, :]), r=["smB"], w=["smB"])
            T.op("dve", lambda e: e.tensor_tensor(out=aacc[:], in0=aacc[:], in1=bc4(sm[:, 5, :]), op=ALU.mult), r=["aacc", "smB"], w=["aacc"])
            T.op("dve", lambda e: e.tensor_tensor(out=ab[:], in0=aacc[:], in1=subg[:, :].unsqueeze(1).broadcast_to([128, 4, 128]), op=ALU.mult),
                 r=["aacc", "subg"], w=["abB"])

        def epi_tail(h, qc):
            for qs in range(4):
                T.op("pe", lambda e: e.transpose(out=psT[:, qs * 128:(qs + 1) * 128], in_=ab[:, qs, :], identity=ident_b[:]),
                     r=["abB", "ident_b"], w=[PK[6]])
            st = aTs[qc % 2]
            T.op("act", lambda e: e.copy(out=st[:], in_=psT[:, 0:512]), r=[PK[6]], w=["aTs%d" % (qc % 2)])
            T.dma("sp", lambda e: e.dma_start(out=attnT_d[h * 128:(h + 1) * 128, qc * 512:(qc + 1) * 512], in_=st[:]),
                  r=["aTs%d" % (qc % 2)], w=["attnT_d"])

        pending = None
        issue_S(0)
        for idx in range(len(steps)):
            h, qc, m, kt = steps[idx]
            if idx + 1 < len(steps):
                issue_S(idx + 1)
            issue_PV(idx)
            if pending is not None and m == 0 and kt == 12:
                epi_tail(*pending)
                pending = None
            if m == 1 and kt == NT - 1:
                epi_head()
                pending = (h, qc)
        epi_tail(*pending)
        T.barrier()

    if "C" in stages:
      with ExitStack() as es:
        FA = sb(es, "FA", [32, 2 * NK], BF16)
        TGr = sb(es, "TGr", [128, NK, 128], BF16)
        TGi = sb(es, "TGi", [128, NK, 128], BF16)
        TGn = sb(es, "TGn", [128, NK, 128], BF16)
        KCS = [(0, 16), (16, 32), (32, NK)]
        R1 = sb(es, "R1", [128, 256], BF16)
        R2 = sb(es, "R2", [128, 256], BF16)
        TH = sb(es, "TH", [NK, 128, 2, 32], BF16)
        skipbc = sb(es, "skipbc", [32, 2, GC])
        for tns, src, k in ((FA, FA_d, "FA"), (R1, R1_d, "R1"), (R2, R2_d, "R2")):
            T.dma("sp", lambda e: e.dma_start(out=tns[:], in_=src), w=[k])
        for tns, src, k in ((TGr, TGr_d, "TGr"), (TGi, TGi_d, "TGi"), (TGn, TGn_d, "TGn")):
            T.dmas("sp", [(lambda e, a=a, b=b: e.dma_start(out=tns[:, a:b, :], in_=src[:, a:b, :])) for (a, b) in KCS], w=[k])
        T.dmas("sp", [(lambda e, a=a: e.dma_start(out=TH[:, a:a + 32, :, :], in_=TH_d[:, a:a + 32, :, :])) for a in range(0, 128, 32)], w=["TH"])
        zf = sb(es, "zf", [32, GC, 128])
        gt = sb(es, "gtC", [32, GC, 128])
        zs = sb(es, "zsC", [32, GC, 128])
        srcb = [sb(es, "srcb%d" % i, [32, GC, 128], BF16) for i in range(3)]
        Bm_ = [sb(es, "Bm%d" % i, [128, 2 * NK, GC], BF16) for i in range(3)]
        srs = sb(es, "srsC", [128, 512])
        sis = sb(es, "sisC", [128, 512])
        tm = [sb(es, "tmC%d" % i, [128, 512]) for i in range(4)]
        Y = sb(es, "YC", [128, GC, 2, NK], BF16)
        Dm = sb(es, "DmC", [NK, 256, GC], BF16)
        tmpy = sb(es, "tmpyC", [32, GC, 16])
        hob = srcb[0]

        def flay(ap2d):
            return ap2d.rearrange("c (a b) -> a c b", b=128)

        def load_filters(g_, o_):
            for pm in range(2):
                T.dma("sp", lambda e: e.dma_start(out=srcb[1 + pm][:], in_=flay(kern_d[o_, pm, g_ * GC:(g_ + 1) * GC, :])),
                      w=["srcb%d" % (1 + pm)])

        for g in range(512 // GC):
            c0 = g * GC
            T.dma("sp", lambda e: e.dma_start(out=zf[:], in_=flay(hyc_d[c0:c0 + GC, :])), w=["zf"])
            T.dma("sp", lambda e: e.dma_start(out=skipbc[:], in_=skip_d[:, :, c0:c0 + GC]), w=["skipbc"])
            for o in range(2):
                T.dma("sp", lambda e: e.dma_start(out=gt[:], in_=flay(hyc_d[512 * (o + 1) + c0:512 * (o + 1) + c0 + GC, :])),
                      w=["gtC"])
                if g == 0 and o == 0:
                    load_filters(0, 0)
                for s_ in (1, 2, 0):
                    if s_ == 0:
                        T.op("act", lambda e: e.copy(out=srcb[0][:], in_=zf[:]), r=["zf"], w=["srcb0"])
                        T.op("pool", lambda e: e.tensor_tensor(out=zs[:], in0=zf[:],
                                                               in1=skipbc[:, o, :].unsqueeze(2).broadcast_to([32, GC, 128]),
                                                               op=ALU.mult), r=["zf", "skipbc"], w=["zsC"])
                    for cq, (ca, cb) in enumerate(((0, 7), (7, 14), (14, 21), (21, 28), (28, 32))):
                        pi = cq % 2
                        ncg = cb - ca
                        for c4 in range(ncg):
                            c = ca + c4
                            T.op("pe", lambda e: e.matmul(ps[pi][:, c4 * 2 * NK:(c4 + 1) * 2 * NK], lhsT=srcb[s_][:, c, :],
                                                          rhs=FA[:, :], start=True, stop=True),
                                 r=["srcb%d" % s_, "FA"], w=[PK[pi]])
                        T.op("act" if cq % 2 == 0 else "dve",
                             lambda e: (e.copy if cq % 2 == 0 else e.tensor_copy)(
                                 out=Bm_[s_][:, :, ca:cb],
                                 in_=ps[pi][:, 0:ncg * 2 * NK].rearrange("p (c f) -> p f c", c=ncg)),
                             r=[PK[pi]], w=["Bm%d" % s_])
                nxt = (g, 1) if o == 0 else (g + 1, 0)
                if nxt[0] < 512 // GC:
                    load_filters(*nxt)
                for kc, (ka, kb_) in enumerate(KCS):
                    bZr, bZi, bSr, bSi = (2, 3, 4, 5) if kc % 2 == 0 else (0, 1, 6, 7)
                    nk = kb_ - ka
                    wd = nk * GC
                    for j in range(nk):
                        k1 = ka + j
                        cs_ = slice(j * GC, (j + 1) * GC)
                        bz, bp, bm = Bm_[0], Bm_[1], Bm_[2]
                        T.op("pe", lambda e: e.matmul(ps[bZr][:, cs_], lhsT=TGr[:, k1, :], rhs=bz[:, k1, :], start=True, stop=False), r=["TGr", "Bm0"], w=[PK[bZr]])
                        T.op("pe", lambda e: e.matmul(ps[bZr][:, cs_], lhsT=TGn[:, k1, :], rhs=bz[:, NK + k1, :], start=False, stop=True), r=["TGn", "Bm0"], w=[PK[bZr]])
                        T.op("pe", lambda e: e.matmul(ps[bZi][:, cs_], lhsT=TGi[:, k1, :], rhs=bz[:, k1, :], start=True, stop=False), r=["TGi", "Bm0"], w=[PK[bZi]])
                        T.op("pe", lambda e: e.matmul(ps[bZi][:, cs_], lhsT=TGr[:, k1, :], rhs=bz[:, NK + k1, :], start=False, stop=True), r=["TGr", "Bm0"], w=[PK[bZi]])
                        T.op("pe", lambda e: e.matmul(ps[bSr][:, cs_], lhsT=TGr[:, k1, :], rhs=bp[:, k1, :], start=True, stop=False), r=["TGr", "Bm1"], w=[PK[bSr]])
                        T.op("pe", lambda e: e.matmul(ps[bSr][:, cs_], lhsT=TGn[:, k1, :], rhs=bp[:, NK + k1, :], start=False, stop=True), r=["TGn", "Bm1"], w=[PK[bSr]])
                        T.op("pe", lambda e: e.matmul(ps[bSi][:, cs_], lhsT=TGi[:, k1, :], rhs=bm[:, k1, :], start=True, stop=False), r=["TGi", "Bm2"], w=[PK[bSi]])
                        T.op("pe", lambda e: e.matmul(ps[bSi][:, cs_], lhsT=TGr[:, k1, :], rhs=bm[:, NK + k1, :], start=False, stop=True), r=["TGr", "Bm2"], w=[PK[bSi]])
                    T.op("act", lambda e: e.copy(out=srs[:, 0:wd], in_=ps[bSr][:, 0:wd]), r=[PK[bSr]], w=["srsC"])
                    T.op("act", lambda e: e.copy(out=sis[:, 0:wd], in_=ps[bSi][:, 0:wd]), r=[PK[bSi]], w=["sisC"])
                    T.op("dve", lambda e: e.tensor_tensor(out=tm[0][:, 0:wd], in0=ps[bZr][:, 0:wd], in1=srs[:, 0:wd], op=ALU.mult), r=[PK[bZr], "srsC"], w=["tm0"])
                    T.op("dve", lambda e: e.tensor_tensor(out=tm[1][:, 0:wd], in0=ps[bZi][:, 0:wd], in1=sis[:, 0:wd], op=ALU.mult), r=[PK[bZi], "sisC"], w=["tm1"])
                    T.op("dve", lambda e: e.tensor_tensor(out=tm[2][:, 0:wd], in0=ps[bZr][:, 0:wd], in1=sis[:, 0:wd], op=ALU.mult), r=[PK[bZr], "sisC"], w=["tm2"])
                    T.op("dve", lambda e: e.tensor_tensor(out=tm[3][:, 0:wd], in0=ps[bZi][:, 0:wd], in1=srs[:, 0:wd], op=ALU.mult), r=[PK[bZi], "srsC"], w=["tm3"])
                    yv_r = Y[:, :, 0, ka:kb_]
                    yv_i = Y[:, :, 1, ka:kb_]
                    T.op("dve", lambda e: e.tensor_tensor(out=yv_r, in0=tm[0][:, 0:wd].rearrange("p (k c) -> p c k", c=GC),
                                                           in1=tm[1][:, 0:wd].rearrange("p (k c) -> p c k", c=GC), op=ALU.subtract),
                         r=["tm0", "tm1"], w=["YC"])
                    T.op("dve", lambda e: e.tensor_tensor(out=yv_i, in0=tm[2][:, 0:wd].rearrange("p (k c) -> p c k", c=GC),
                                                           in1=tm[3][:, 0:wd].rearrange("p (k c) -> p c k", c=GC), op=ALU.add),
                         r=["tm2", "tm3"], w=["YC"])
                for cq in range(GC // 2):
                    pi = cq % 2
                    for c2 in range(2):
                        c = cq * 2 + c2
                        T.op("pe", lambda e: e.matmul(ps[pi][0:NK, c2 * 256:(c2 + 1) * 256], lhsT=Y[:, c, 0, :], rhs=R1[:, :],
                                                      start=True, stop=False), r=["YC", "R1"], w=[PK[pi]])
                        T.op("pe", lambda e: e.matmul(ps[pi][0:NK, c2 * 256:(c2 + 1) * 256], lhsT=Y[:, c, 1, :], rhs=R2[:, :],
                                                      start=False, stop=True), r=["YC", "R2"], w=[PK[pi]])
                    T.op("act" if cq % 2 == 0 else "dve",
                         lambda e: (e.copy if cq % 2 == 0 else e.tensor_copy)(
                             out=Dm[:, :, cq * 2:(cq + 1) * 2],
                             in_=ps[pi][0:NK, :].rearrange("p (c f) -> p f c", c=2)),
                         r=[PK[pi]], w=["DmC"])
                for nq in range(8):
                    pi = 6 + nq % 2
                    for j in range(16):
                        n2 = nq * 16 + j
                        T.op("pe", lambda e: e.matmul(ps[pi][0:32, j * GC:(j + 1) * GC], lhsT=TH[:, n2, 0, :], rhs=Dm[:, n2, :],
                                                      start=True, stop=False), r=["TH", "DmC"], w=[PK[pi]])
                        T.op("pe", lambda e: e.matmul(ps[pi][0:32, j * GC:(j + 1) * GC], lhsT=TH[:, n2, 1, :], rhs=Dm[:, 128 + n2, :],
                                                      start=False, stop=True), r=["TH", "DmC"], w=[PK[pi]])
                    nsl = slice(nq * 16, (nq + 1) * 16)
                    T.op("dve", lambda e: e.tensor_tensor(out=tmpy[:], in0=ps[pi][0:32, :].rearrange("p (n c) -> p c n", c=GC),
                                                          in1=zs[:, :, nsl], op=ALU.add), r=[PK[pi], "zsC"], w=["tmpyC"])
                    T.op("dve", lambda e: e.tensor_tensor(out=zf[:, :, nsl], in0=tmpy[:], in1=gt[:, :, nsl], op=ALU.mult),
                         r=["tmpyC", "gtC", "srcb0", "zsC"], w=["zf"])
            T.op("act", lambda e: e.copy(out=hob[:], in_=zf[:]), r=["zf"], w=["srcb0"])
            T.dma("sp", lambda e: e.dma_start(out=flay(hyoT_d[c0:c0 + GC, :]), in_=hob[:]), r=["srcb0"], w=["hyoT_d"])
        T.barrier()

    if "D" in stages:
      with ExitStack() as es:
        aff_all = sb(es, "aff_all", [128, NT, NE])
        with ExitStack() as es2:
            attnT = sb(es2, "attnT", [128, 4, L], BF16)
            hyoT = sb(es2, "hyoT", [128, 4, L], BF16)
            wpa = sb(es2, "wpa", [128, 4, D], BF16)
            wph = sb(es2, "wph", [128, 4, D], BF16)
            wo = sb(es2, "wo", [128, 8, D], BF16)
            g2bc = sb(es2, "g2bc", [128, D])
            wr = sb(es2, "wr", [128, 8, 16])
            gat2 = [sb(es2, "gatD%d" % i, [128, 16, 512], BF16) for i in range(2)]
            m1 = sb(es2, "m1D", [128, 512])
            m2 = sb(es2, "m2D", [128, 512])
            mT = sb(es2, "mTD", [128, 8, 512], BF16)
            xin = sb(es2, "xinD", [128, D])
            x1 = sb(es2, "x1D", [128, D])
            u2 = sb(es2, "u2D0", [128, D])
            u2b = sb(es2, "u2bD", [128, D], BF16)
            u2T = sb(es2, "u2TD", [128, 8, 128])
            junkd = sb(es2, "junkD", [128, D])
            st = sb(es2, "stD", [128, 8])
            lg = sb(es2, "lgD", [128, NE])
            for (tt_, td_, tk_) in ((attnT, attnT_d, "attnT"), (hyoT, hyoT_d, "hyoT")):
                T.dmas("sp", [(lambda e, j=j, a=a: e.dma_start(out=tt_[:, j, a:a + 2048], in_=td_[j * 128:(j + 1) * 128, a:a + 2048]))
                              for j in range(4) for a in (0, 2048)], w=[tk_])
            T.dma("sp", lambda e: e.dma_start(out=g2bc[:], in_=g2bc_d[:, :]), w=["g2bc"])
            T.dma("sp", lambda e: e.dma_start(out=wr[:], in_=wr_d[:, :, :]), w=["wr"])
            for (wsrc, nj, wdst, wk) in ((wpa_d, 4, wpa, "wpa"), (wph_d, 4, wph, "wph"), (wo_d, 8, wo, "wo")):
                T.dmas("pool", [(lambda e, a=a: e.dma_start(out=wdst[:, a:a + 4, :],
                                                            in_=wsrc.rearrange("(j p) c -> p j c", p=128)[:, a:a + 4, :]))
                                for a in range(0, nj, 4)], w=[wk])
            u2s = [u2, sb(es2, "u2D1", [128, D])]

            def d_tail(i):
                u2t = u2s[i % 2]
                for j in range(8):
                    T.op("pe", lambda e: e.transpose(out=ps[4 + j // 4][:, (j % 4) * 128:(j % 4 + 1) * 128],
                                                     in_=u2t[:, j * 128:(j + 1) * 128], identity=ident_f[:]),
                         r=["u2D%d" % (i % 2), "ident_f"], w=[PK[4 + j // 4]])
                for hh in range(2):
                    T.op("act", lambda e: e.copy(out=u2T[:, hh * 4:(hh + 1) * 4, :],
                                                 in_=ps[4 + hh][:, :].rearrange("p (j t) -> p j t", j=4)),
                         r=[PK[4 + hh]], w=["u2TD"])
                for j in range(8):
                    T.op("pe", lambda e: e.matmul(ps[6][:, 0:NE], lhsT=u2T[:, j, :], rhs=wr[:, j, :], start=(j == 0), stop=(j == 7)),
                         r=["u2TD", "wr"], w=[PK[6]])
                T.op("dve", lambda e: e.tensor_reduce(out=st2[:, 3:4], in_=ps[6][:, 0:NE], axis=AX.X, op=ALU.max), r=[PK[6], "st2D"], w=["st2D"])
                T.op("dve", lambda e: e.tensor_scalar(out=st2[:, 4:5], in0=st2[:, 3:4], scalar1=-1.0, scalar2=None, op0=ALU.mult), r=["st2D"], w=["st2D"])
                T.op("dve", lambda e: e.memset(st2[:, 5:6], 0.0), r=["st2D"], w=["st2D"])
                T.op("act", lambda e: e.activation(out=lg[:], in_=ps[6][:, 0:NE], func=AF.Exp, bias=st2[:, 4:5], accum_out=st2[:, 5:6]),
                     r=[PK[6], "st2D"], w=["lgD", "st2D"])
                T.op("dve", lambda e: e.reciprocal(out=st2[:, 6:7], in_=st2[:, 5:6]), r=["st2D"], w=["st2D"])
                T.op("dve", lambda e: e.tensor_scalar(out=aff_all[:, i, :], in0=lg[:], scalar1=st2[:, 6:7], scalar2=None, op0=ALU.mult),
                     r=["lgD", "st2D"], w=["aff_all"])

            st2 = sb(es2, "st2D", [128, 8])
            prev_i = None
            for tq in range(8):
                tsl = slice(tq * 512, (tq + 1) * 512)
                gat = gat2[tq % 2]
                gk = "gatD%d" % (tq % 2)
                T.dmas("sp", [(lambda e, a=a: e.dma_start(out=gat[:, a:a + 8, :],
                                                          in_=gateT_d.rearrange("(j p) t -> p j t", p=128)[:, a:a + 8, tsl])) for a in (0, 8)],
                       w=[gk])
                for dt in range(8):
                    pa = 0 if dt % 2 == 0 else 7
                    for j in range(4):
                        T.op("pe", lambda e: e.matmul(ps[pa][:, :], lhsT=wpa[:, j, dt * 128:(dt + 1) * 128], rhs=attnT[:, j, tsl],
                                                      start=(j == 0), stop=(j == 3)), r=["wpa", "attnT"], w=[PK[pa]])
                    for j in range(4):
                        T.op("pe", lambda e: e.matmul(ps[1][:, :], lhsT=wph[:, j, dt * 128:(dt + 1) * 128], rhs=hyoT[:, j, tsl],
                                                      start=(j == 0), stop=(j == 3)), r=["wph", "hyoT"], w=[PK[1]])
                    T.op("dve", lambda e: e.tensor_tensor(out=m1[:], in0=ps[pa][:, :], in1=gat[:, dt, :], op=ALU.mult), r=[PK[pa], gk], w=["m1D"])
                    T.op("dve", lambda e: e.tensor_tensor(out=m2[:], in0=ps[1][:, :], in1=gat[:, 8 + dt, :], op=ALU.mult), r=[PK[1], gk], w=["m2D"])
                    T.op("pool", lambda e: e.tensor_tensor(out=mT[:, dt, :], in0=m1[:], in1=m2[:], op=ALU.add), r=["m1D", "m2D"], w=["mTD"])
                for ts in range(4):
                    i = tq * 4 + ts
                    u2t = u2s[i % 2]
                    u2k = "u2D%d" % (i % 2)
                    T.dma("sp", lambda e: e.dma_start(out=xin[:], in_=x_d[i * 128:(i + 1) * 128, :]), w=["xinD"])
                    for half in range(2):
                        for dt in range(8):
                            T.op("pe", lambda e: e.matmul(ps[2 + half][:, :], lhsT=mT[:, dt, ts * 128:(ts + 1) * 128],
                                                          rhs=wo[:, dt, half * 512:(half + 1) * 512], start=(dt == 0), stop=(dt == 7)),
                                 r=["mTD", "wo"], w=[PK[2 + half]])
                        T.op("dve", lambda e: e.tensor_tensor(out=x1[:, half * 512:(half + 1) * 512], in0=ps[2 + half][:, :],
                                                              in1=xin[:, half * 512:(half + 1) * 512], op=ALU.add),
                             r=[PK[2 + half], "xinD"], w=["x1D"])
                    if prev_i is not None:
                        d_tail(prev_i)
                    T.dma("sp", lambda e: e.dma_start(out=out_d[i * 128:(i + 1) * 128, :], in_=x1[:]), r=["x1D"], w=["out_d"])
                    T.op("dve", lambda e: e.memset(st[:, 0:1], 0.0), w=["stD"])
                    T.op("act", lambda e: e.activation(out=junkd[:], in_=x1[:], func=AF.Square, accum_out=st[:, 0:1]),
                         r=["x1D", "stD"], w=["junkD", "stD"])
                    T.op("act", lambda e: e.activation(out=st[:, 1:2], in_=st[:, 0:1], func=AF.Sqrt, scale=1.0 / D, bias=eps[:]),
                         r=["stD", "eps"], w=["stD"])
                    T.op("dve", lambda e: e.reciprocal(out=st[:, 2:3], in_=st[:, 1:2]), r=["stD"], w=["stD"])
                    T.op("dve", lambda e: e.scalar_tensor_tensor(out=u2t[:], in0=x1[:], scalar=st[:, 2:3], in1=g2bc[:],
                                                                 op0=ALU.mult, op1=ALU.mult), r=["x1D", "stD", "g2bc"], w=[u2k])
                    T.op("act", lambda e: e.copy(out=u2b[:], in_=u2t[:]), r=[u2k], w=["u2bD"])
                    T.dma("sp", lambda e: e.dma_start(out=u2_d[i * 128:(i + 1) * 128, :], in_=u2b[:]), r=["u2bD"], w=["u2_d"])
                    prev_i = i
            d_tail(prev_i)
            T.barrier()
        if "aff" in dbg_d:
            T.dma("sp", lambda e: e.dma_start(out=dbg_d["aff"], in_=aff_all[:]), r=["aff_all"], w=["dbgaff"])

        if "E" in stages:
          idx_all = sb(es, "idx_all", [128, NE, 4], I32)
          g_all = sb(es, "g_all", [128, NE, 4])
          with ExitStack() as es2:
            affT = sb(es2, "affT", [NE, L])
            cmpj = sb(es2, "cmpj", [NE, L])
            bs = sb(es2, "bsE", [NE, 8])
            tri = sb(es2, "tri", [128, 128])
            onesf = sb(es2, "onesf", [128, 128])
            iota = sb(es2, "iota", [128, 512])
            tidhl = sb(es2, "tidhl", [128, NT, 2], BF16)
            thbc = sb(es2, "thbc", [128, NE])
            dg = sb(es2, "dgE", [NE, NE])
            M = sb(es2, "ME", [128, NT, NE])
            pos = sb(es2, "posE", [128, NT, NE])
            srun = sb(es2, "srunE", [128, NE])
            cum = sb(es2, "cumE", [128, NE])
            vals = sb(es2, "valsE", [128, NT, NE, 4], BF16)
            afh = sb(es2, "afhE", [128, NT, NE], BF16)
            afr = sb(es2, "afrE", [128, NT, NE])
            oh = [sb(es2, "ohE%d" % i, [128, 512], BF16) for i in range(4)]
            idf = sb(es2, "idfE", [128, 4])
            pvs = sb(es2, "pvsE", [128, 16])
            T.dma("sp", lambda e: e.dma_start(out=tri[:], in_=tri_d[:, :]), w=["tri"])
            T.dma("sp", lambda e: e.dma_start(out=iota[:], in_=iota_d[:, :]), w=["iota"])
            T.dma("sp", lambda e: e.dma_start(out=tidhl[:], in_=tidhl_d[:, :, :]), w=["tidhl"])
            T.op("pool", lambda e: e.memset(onesf[:], 1.0), w=["onesf"])
            for i in range(NT):
                pi = i // 4
                T.op("pe", lambda e: e.transpose(out=ps[pi][0:NE, (i % 4) * 128:(i % 4 + 1) * 128], in_=aff_all[:, i, :],
                                                 identity=ident_f[:]), r=["aff_all", "ident_f"], w=[PK[pi]])
                if i % 4 == 3:
                    T.op("act", lambda e: e.copy(out=affT[:, (i - 3) * 128:(i + 1) * 128], in_=ps[pi][0:NE, :]), r=[PK[pi]], w=["affT"])
            T.op("dve", lambda e: e.memset(bs[:, 0:1], 0.0), w=["bsE"])
            T.op("dve", lambda e: e.memset(bs[:, 1:2], 1.0), r=["bsE"], w=["bsE"])
            for it in range(32):
                T.op("dve", lambda e: e.tensor_scalar(out=bs[:, 2:3], in0=bs[:, 0:1], scalar1=bs[:, 1:2], scalar2=0.5, op0=ALU.add, op1=ALU.mult), r=["bsE"], w=["bsE"])
                T.op("dve", lambda e: e.tensor_scalar(out=cmpj[:], in0=affT[:], scalar1=bs[:, 2:3], scalar2=None, op0=ALU.is_ge), r=["affT", "bsE"], w=["cmpj"])
                T.op("dve", lambda e: e.tensor_reduce(out=bs[:, 3:4], in_=cmpj[:], axis=AX.X, op=ALU.add), r=["cmpj", "bsE"], w=["bsE"])
                T.op("dve", lambda e: e.tensor_scalar(out=bs[:, 4:5], in0=bs[:, 3:4], scalar1=CAP - 0.5, scalar2=None, op0=ALU.is_ge), r=["bsE"], w=["bsE"])
                T.op("dve", lambda e: e.tensor_tensor(out=bs[:, 5:6], in0=bs[:, 2:3], in1=bs[:, 0:1], op=ALU.subtract), r=["bsE"], w=["bsE"])
                T.op("dve", lambda e: e.tensor_tensor(out=bs[:, 6:7], in0=bs[:, 1:2], in1=bs[:, 2:3], op=ALU.subtract), r=["bsE"], w=["bsE"])
                T.op("dve", lambda e: e.scalar_tensor_tensor(out=bs[:, 0:1], in0=bs[:, 5:6], scalar=bs[:, 4:5], in1=bs[:, 0:1], op0=ALU.mult, op1=ALU.add), r=["bsE"], w=["bsE"])
                T.op("dve", lambda e: e.scalar_tensor_tensor(out=bs[:, 1:2], in0=bs[:, 6:7], scalar=bs[:, 4:5], in1=bs[:, 2:3], op0=ALU.mult, op1=ALU.add), r=["bsE"], w=["bsE"])
            T.op("dve", lambda e: e.tensor_scalar(out=dg[:], in0=ident_f[0:NE, 0:NE], scalar1=bs[:, 0:1], scalar2=None, op0=ALU.mult), r=["bsE", "ident_f"], w=["dgE"])
            T.op("pe", lambda e: e.matmul(ps[0][:, 0:NE], lhsT=onesf[0:NE, :], rhs=dg[:, :], start=True, stop=True), r=["onesf", "dgE"], w=[PK[0]])
            T.op("dve", lambda e: e.tensor_copy(out=thbc[:], in_=ps[0][:, 0:NE]), r=[PK[0]], w=["thbc"])
            T.op("dve", lambda e: e.tensor_tensor(out=M[:], in0=aff_all[:], in1=thbc[:, :].unsqueeze(1).broadcast_to([128, NT, NE]), op=ALU.is_ge),
                 r=["aff_all", "thbc"], w=["ME"])
            T.op("dve", lambda e: e.memset(srun[:], 0.0), w=["srunE"])
            for i in range(NT):
                pi = 1 + i % 2
                T.op("pe", lambda e: e.matmul(ps[pi][:, 0:NE], lhsT=tri[:, :], rhs=M[:, i, :], start=True, stop=True), r=["tri", "ME"], w=[PK[pi]])
                T.op("pe", lambda e: e.matmul(ps[pi][:, NE:2 * NE], lhsT=onesf[:, :], rhs=M[:, i, :], start=True, stop=True), r=["onesf", "ME"], w=[PK[pi]])
                T.op("dve", lambda e: e.tensor_tensor(out=cum[:], in0=ps[pi][:, 0:NE], in1=srun[:], op=ALU.add), r=[PK[pi], "srunE"], w=["cumE"])
                T.op("dve", lambda e: e.tensor_tensor(out=srun[:], in0=ps[pi][:, NE:2 * NE], in1=srun[:], op=ALU.add), r=[PK[pi], "srunE", "cumE"], w=["srunE"])
                T.op("dve", lambda e: e.tensor_tensor(out=cum[:], in0=cum[:], in1=M[:, i, :], op=ALU.mult), r=["cumE", "ME"], w=["cumE"])
                T.op("dve", lambda e: e.tensor_scalar(out=pos[:, i, :], in0=cum[:], scalar1=-1.0, scalar2=None, op0=ALU.add), r=["cumE"], w=["posE"])
            T.op("dve", lambda e: e.tensor_copy(out=afh[:], in_=aff_all[:]), r=["aff_all"], w=["afhE"])
            T.op("dve", lambda e: e.tensor_tensor(out=afr[:], in0=aff_all[:], in1=afh[:], op=ALU.subtract), r=["aff_all", "afhE"], w=["afrE"])
            T.op("dve", lambda e: e.tensor_copy(out=vals[:, :, :, 2], in_=afh[:]), r=["afhE"], w=["valsE"])
            T.op("dve", lambda e: e.tensor_copy(out=vals[:, :, :, 3], in_=afr[:]), r=["afrE", "valsE"], w=["valsE"])
            T.op("dve", lambda e: e.tensor_copy(out=vals[:, :, :, 0:2], in_=tidhl[:, :, :].unsqueeze(2).broadcast_to([128, NT, NE, 2])),
                 r=["tidhl", "valsE"], w=["valsE"])
            oc = 0
            for ex in range(NE):
                pi = 3 + ex % 2
                for i in range(NT):
                    ob = oc % 4
                    eng = "dve"
                    oc += 1
                    T.op(eng, lambda e: e.tensor_scalar(out=oh[ob][:], in0=iota[:], scalar1=pos[:, i, ex:ex + 1], scalar2=None, op0=ALU.is_equal),
                         r=["iota", "posE"], w=["ohE%d" % ob])
                    for sc in range(4):
                        T.op("pe", lambda e: e.matmul(ps[pi][:, sc * 4:(sc + 1) * 4], lhsT=oh[ob][:, sc * 128:(sc + 1) * 128], rhs=vals[:, i, ex, :],
                                                      start=(i == 0 and sc == 0), stop=(i == NT - 1), skip_group_check=True),
                             r=["ohE%d" % ob, "valsE"], w=[PK[pi]])
                T.op("dve", lambda e: e.tensor_copy(out=pvs[:], in_=ps[pi][:, 0:16]), r=[PK[pi]], w=["pvsE"])
                pv = pvs[:, :].rearrange("p (s f) -> p s f", f=4)
                T.op("dve", lambda e: e.scalar_tensor_tensor(out=idf[:], in0=pv[:, :, 0], scalar=64.0, in1=pv[:, :, 1], op0=ALU.mult, op1=ALU.add),
                     r=["pvsE"], w=["idfE"])
                T.op("dve", lambda e: e.tensor_copy(out=idx_all[:, ex, :], in_=idf[:]), r=["idfE"], w=["idx_all"])
                T.op("dve", lambda e: e.tensor_tensor(out=g_all[:, ex, :], in0=pv[:, :, 2], in1=pv[:, :, 3], op=ALU.add), r=["pvsE"], w=["g_all"])
            T.barrier()
          if "idx" in dbg_d:
            T.dma("sp", lambda e: e.dma_start(out=dbg_d["idx"], in_=idx_all[:]), r=["idx_all"], w=["dbgidx"])
            T.dma("sp", lambda e: e.dma_start(out=dbg_d["g"], in_=g_all[:]), r=["g_all"], w=["dbgg"])

          if "F" in stages:
            wgb = [sb(es, "wgb%d" % i, [128, 8, D], BF16) for i in range(2)]
            wub = [sb(es, "wub%d" % i, [128, 8, D], BF16) for i in range(2)]
            wdb = [sb(es, "wdb%d" % i, [128, 8, D], BF16) for i in range(2)]
            xg = [sb(es, "xgF%d" % i, [128, D], BF16) for i in range(4)]
            xinT = sb(es, "xinT", [128, 8, 512], BF16)
            sg = sb(es, "sgF", [128, 512])
            hT = sb(es, "hTF", [128, 8, 512], BF16)
            eo = [sb(es, "eoF%d" % i, [128, D]) for i in range(2)]
            psb = [ps[6][:, :].bitcast(BF16), ps[7][:, :].bitcast(BF16)]

            def load_w(ex):
                for (wsrc, wdst, wk) in ((wg_d, wgb, "wgb"), (wu_d, wub, "wub"), (wd_d, wdb, "wdb")):
                    dst = wdst[ex % 2]
                    T.dmas("pool", [(lambda e, hh=hh: e.dma_start(out=dst[:, hh * 4:(hh + 1) * 4, :],
                                                                  in_=wsrc[ex].rearrange("(j p) c -> p j c", p=128)[:, hh * 4:(hh + 1) * 4, :]))
                                    for hh in range(2)], w=["%s%d" % (wk, ex % 2)])

            def gather(ex):
                for sc in range(4):
                    T.dma("pool", lambda e: e.indirect_dma_start(out=xg[sc][:], out_offset=None, in_=u2_d[:, :],
                                                                 in_offset=bass.IndirectOffsetOnAxis(ap=idx_all[:, ex, sc:sc + 1], axis=0)),
                          r=["idx_all", "u2_d"], w=["xgF%d" % sc])

            load_w(0)
            gather(0)
            for ex in range(NE):
                wgk, wuk, wdk = "wgb%d" % (ex % 2), "wub%d" % (ex % 2), "wdb%d" % (ex % 2)
                wgt, wut, wdt = wgb[ex % 2], wub[ex % 2], wdb[ex % 2]
                if ex + 1 < NE:
                    load_w(ex + 1)
                for sc in range(4):
                    for j in range(8):
                        T.op("pe", lambda e: e.transpose(out=psb[sc % 2][:, j * 128:(j + 1) * 128], in_=xg[sc][:, j * 128:(j + 1) * 128], identity=ident_b[:]),
                             r=["xgF%d" % sc, "ident_b"], w=[PK[6 + sc % 2]])
                    T.op("act", lambda e: e.copy(out=xinT[:, :, sc * 128:(sc + 1) * 128], in_=psb[sc % 2].rearrange("p (j t) -> p j t", j=8)),
                         r=[PK[6 + sc % 2]], w=["xinT"])
                if ex + 1 < NE:
                    gather(ex + 1)
                for ft in range(8):
                    pg, pu = (ft % 2) * 2, (ft % 2) * 2 + 1
                    for j in range(8):
                        T.op("pe", lambda e: e.matmul(ps[pg][:, :], lhsT=wgt[:, j, ft * 128:(ft + 1) * 128], rhs=xinT[:, j, :], start=(j == 0), stop=(j == 7)),
                             r=[wgk, "xinT"], w=[PK[pg]])
                    for j in range(8):
                        T.op("pe", lambda e: e.matmul(ps[pu][:, :], lhsT=wut[:, j, ft * 128:(ft + 1) * 128], rhs=xinT[:, j, :], start=(j == 0), stop=(j == 7)),
                             r=[wuk, "xinT"], w=[PK[pu]])
                    T.op("act", lambda e: e.activation(out=sg[:], in_=ps[pg][:, :], func=AF.Silu), r=[PK[pg]], w=["sgF"])
                    T.op("dve", lambda e: e.tensor_tensor(out=hT[:, ft, :], in0=ps[pu][:, :], in1=sg[:], op=ALU.mult), r=[PK[pu], "sgF"], w=["hTF"])
                for sc in range(4):
                    eb = eo[sc % 2]
                    for half in range(2):
                        po = 4 + half
                        for ft in range(8):
                            T.op("pe", lambda e: e.matmul(ps[po][:, :], lhsT=hT[:, ft, sc * 128:(sc + 1) * 128], rhs=wdt[:, ft, half * 512:(half + 1) * 512],
                                                          start=(ft == 0), stop=(ft == 7)), r=["hTF", wdk], w=[PK[po]])
                        T.op("dve", lambda e: e.tensor_scalar(out=eb[:, half * 512:(half + 1) * 512], in0=ps[po][:, :], scalar1=g_all[:, ex, sc:sc + 1],
                                                              scalar2=None, op0=ALU.mult), r=[PK[po], "g_all"], w=["eoF%d" % (sc % 2)])
                    T.dma("pool", lambda e: e.indirect_dma_start(out=out_d[:, :], out_offset=bass.IndirectOffsetOnAxis(ap=idx_all[:, ex, sc:sc + 1], axis=0),
                                                                 in_=eb[:], in_offset=None, compute_op=ALU.add),
                          r=["eoF%d" % (sc % 2), "idx_all"], w=["out_d"])
            T.barrier()
    for k, v in dbg_d.items():
        pass
    T.barrier()
    top.close()
    return nc


_CONST = None


def make_inputs(inp, b):
    global _CONST
    if _CONST is None:
        _CONST = host_consts()
    f = lambda a: np.ascontiguousarray(np.asarray(a, dtype=np.float32))
    m = dict(_CONST)
    m["x"] = f(inp["x"][b])
    m["g1bc"] = f(np.broadcast_to(inp["norm1_g"][0][None, :], (128, D)))
    m["w_in"] = f(inp["w_in"][0])
    scw = np.concatenate([inp["short_conv_w"][0], inp["short_conv_b"][0][None, :]], axis=0)
    m["scw"] = f(scw.reshape(4, 12, 128).transpose(2, 1, 0))
    qkg = np.stack([inp["q_norm_g"][0], inp["k_norm_g"][0]], axis=0)
    m["qkg"] = f(np.broadcast_to(qkg[None], (128, 2, 64)))
    m["lamv"] = f(np.stack([inp["lambda_q1"][0], inp["lambda_k1"][0], inp["lambda_q2"][0], inp["lambda_k2"][0]])[None])
    m["subg"] = f(np.broadcast_to(inp["subln_g"][0][None, :], (128, 128)))
    m["fw1"] = f(inp["filt_w1"][0])
    m["fw2"] = f(inp["filt_w2"][0])
    m["fw3"] = f(inp["filt_w3"][0])
    m["fbf"] = f(np.stack([inp["filt_b1"][0], inp["filt_b2"][0], inp["filt_b3"][0], inp["filt_freq"][0]], axis=1))
    m["fwo"] = f(inp["filt_w_out"][0])
    m["skipbc"] = f(np.broadcast_to(inp["hyena_skip"][0][None], (32, 2, 512)))
    m["wpa"] = f(inp["w_branch_attn"][0])
    m["wph"] = f(inp["w_branch_hyena"][0])
    m["wo"] = f(inp["w_out"][0])
    m["g2bc"] = f(np.broadcast_to(inp["norm2_g"][0][None, :], (128, D)))
    m["wr"] = f(inp["w_router"][0].reshape(8, 128, 16).transpose(1, 0, 2))
    m["wg"] = f(inp["w_gate"][0])
    m["wu"] = f(inp["w_up"][0])
    m["wd"] = f(inp["w_down"][0])
    return m


def kernel(**inputs):
    nc = build()
    maps = [make_inputs(inputs, c % 4) for c in range(4)]
    in_maps = [maps[c % 4] for c in range(NCORES)]
    res = run_bass_kernel_spmd(nc, in_maps, core_ids=list(range(NCORES)))
    out = np.stack([np.asarray(res.results[b]["out"], dtype=np.float32) for b in range(4)], axis=0)
    return out
```

```python
import math
from contextlib import ExitStack
import numpy as np
import ml_dtypes
import concourse.bass as bass
import concourse.mybir as mybir
from concourse.bass_utils import run_bass_kernel_spmd

F32 = mybir.dt.float32
BF16 = mybir.dt.bfloat16
I32 = mybir.dt.int32
AF = mybir.ActivationFunctionType
ALU = mybir.AluOpType
AX = mybir.AxisListType

L = 4096
D = 1024
NT = 32
NCORES = 8
CAP = 512
NE = 16
GC = 32
NK = 33
DEBUG = False


class Trk:
    def __init__(s, nc):
        s.nc = nc
        s.eng = dict(pe=nc.tensor, act=nc.scalar, dve=nc.vector, pool=nc.gpsimd, sp=nc.sync)
        s.sem = {}
        s.cnt = {}
        for k in ("pe", "act", "dve", "pool"):
            s.sem[k] = nc.alloc_semaphore(name="s_" + k)
            s.cnt[k] = 0
        s.ND = 16
        for i in range(s.ND):
            k = "d%d" % i
            s.sem[k] = nc.alloc_semaphore(name="s_" + k)
            s.cnt[k] = 0
        s.pool_of = {"sp": list(range(0, 8)), "pool": list(range(8, 16))}
        s.rrq = {"sp": 0, "pool": 0}
        s.seen = {e: {} for e in s.eng}
        s.lw = {}
        s.rd = {}

    def _wait(s, e, deps):
        for k, v in deps.items():
            if e == "pe" and k == "pe":
                continue
            if s.seen[e].get(k, 0) < v:
                s.eng[e].wait_ge(s.sem[k], v)
                s.seen[e][k] = v

    def _deps(s, r, w):
        d = {}

        def add(k, v):
            if d.get(k, 0) < v:
                d[k] = v
        for x in r:
            for k, v in s.lw.get(x, {}).items():
                add(k, v)
        for x in w:
            for k, v in s.lw.get(x, {}).items():
                add(k, v)
            for k, v in s.rd.get(x, {}).items():
                add(k, v)
        return d

    def _upd(s, toks, r, w):
        if isinstance(toks, tuple):
            toks = [toks]
        for x in r:
            m = s.rd.setdefault(x, {})
            for tok in toks:
                if m.get(tok[0], 0) < tok[1]:
                    m[tok[0]] = tok[1]
        for x in w:
            m = {}
            for tok in toks:
                if m.get(tok[0], 0) < tok[1]:
                    m[tok[0]] = tok[1]
            s.lw[x] = m
            s.rd[x] = {}

    def op(s, e, fn, r=(), w=()):
        s._wait(e, s._deps(r, w))
        ins = fn(s.eng[e])
        s.cnt[e] += 1
        ins.then_inc(s.sem[e], 1)
        s._upd((e, s.cnt[e]), r, w)

    def _next_sem(s, q):
        pool = s.pool_of[q]
        k = "d%d" % pool[s.rrq[q] % len(pool)]
        s.rrq[q] += 1
        if s.cnt[k] > 0:
            s._wait(q, {k: s.cnt[k]})
        return k

    def dma(s, q, fn, r=(), w=()):
        s._wait(q, s._deps(r, w))
        k = s._next_sem(q)
        ins = fn(s.eng[q])
        s.cnt[k] += 16
        ins.then_inc(s.sem[k], 16)
        s._upd((k, s.cnt[k]), r, w)

    def dmas(s, q, fns, r=(), w=()):
        s._wait(q, s._deps(r, w))
        toks = []
        for fn in fns:
            k = s._next_sem(q)
            ins = fn(s.eng[q])
            s.cnt[k] += 16
            ins.then_inc(s.sem[k], 16)
            toks.append((k, s.cnt[k]))
        s._upd(toks, r, w)

    def barrier(s, engines=None):
        tot = {k: v for k, v in s.cnt.items() if v > 0}
        for e in (engines or s.eng):
            s._wait(e, dict(tot))
        s.lw = {}
        s.rd = {}


def _bf(a):
    return np.ascontiguousarray(a.astype(np.float32)).astype(ml_dtypes.bfloat16)


def host_consts():
    c = {}
    t = np.arange(L, dtype=np.float32)
    inv_freq = (np.float32(500000.0) ** (-np.arange(0, 16, 2, dtype=np.float32) / np.float32(16))).astype(np.float32)
    ang = (t[:, None] * inv_freq[None, :]).astype(np.float32)
    cs = np.concatenate([np.cos(ang), np.sin(ang)], axis=-1).astype(np.float32)
    c["ropecs"] = np.ascontiguousarray(cs.reshape(NT, 128, 16).transpose(1, 0, 2))
    tt = t / np.float32(L - 1)
    bands = np.linspace(1e-4, 15, 16, dtype=np.float32)
    a2 = (np.float32(2.0 * math.pi / L) * t[:, None] * bands[None, :]).astype(np.float32)
    emb = np.concatenate([tt[:, None], np.cos(a2), -np.sin(a2)], axis=-1).astype(np.float32)
    c["embT"] = np.ascontiguousarray(emb.T)
    c["posrow"] = np.ascontiguousarray(np.broadcast_to(tt[None, :], (128, L))).astype(np.float32)
    min_decay = math.log(1e-2) / 1.5
    max_decay = math.log(1e-2) / 0.3
    deltas = np.abs(np.linspace(min_decay, max_decay, 512, dtype=np.float32))
    c["ndelta"] = np.ascontiguousarray((-deltas).reshape(4, 128).T).astype(np.float32)
    n1 = np.arange(32)[:, None]
    k1 = np.arange(NK)[None, :]
    th = 2 * np.pi * n1 * k1 / 64.0
    c["FA"] = _bf(np.concatenate([np.cos(th), -np.sin(th)], axis=1))
    n2 = np.arange(128)[:, None, None]
    k1 = np.arange(NK)[None, :, None]
    k2 = np.arange(128)[None, None, :]
    th = 2 * np.pi * ((n2 * (k1 + 64 * k2)) % 8192) / 8192.0
    c["TGr"] = _bf(np.cos(th))
    c["TGi"] = _bf(-np.sin(th))
    c["TGn"] = _bf(np.sin(th))
    kk2 = np.arange(128)[:, None]
    nn2 = np.arange(128)[None, :]
    th = 2 * np.pi * ((kk2 * nn2) % 128) / 128.0
    fr, fi = np.cos(th), np.sin(th)
    c["R1"] = _bf(np.concatenate([fr, fi], axis=1))
    c["R2"] = _bf(np.concatenate([-fi, fr], axis=1))
    k1 = np.arange(NK)[:, None, None]
    n2 = np.arange(128)[None, :, None]
    n1 = np.arange(32)[None, None, :]
    th = 2 * np.pi * ((k1 * (n2 + 128 * n1)) % 8192) / 8192.0
    coef = np.where((k1 == 0) | (k1 == 32), 1.0, 2.0)[:, :, None, :]
    th_ = np.stack([np.cos(th), -np.sin(th)], axis=2) * coef / 8192.0
    c["TH"] = _bf(th_)
    tid = np.arange(L).reshape(NT, 128).T
    c["tidhl"] = _bf(np.stack([tid // 64, tid % 64], axis=-1))
    c["iota512"] = np.ascontiguousarray(np.broadcast_to(np.arange(512, dtype=np.float32)[None, :], (128, 512)))
    tri = (np.arange(128)[:, None] <= np.arange(128)[None, :]).astype(np.float32)
    c["tri"] = tri
    return c


def build(dbg=None):
    nc = bass.Bass("TRN2", target_bir_lowering=False)
    dbg = dbg or {}

    def din(name, shape, dt=F32):
        return nc.dram_tensor(name, list(shape), dt, kind="ExternalInput").ap()

    def dscr(name, shape, dt=F32):
        kind = "ExternalOutput" if name in dbg.get("_ext", ()) else "Internal"
        return nc.dram_tensor(name, list(shape), dt, kind=kind).ap()

    x_d = din("x", [L, D])
    g1bc_d = din("g1bc", [128, D])
    w_in_d = din("w_in", [D, 5120])
    scw_d = din("scw", [128, 12, 4])
    qkg_d = din("qkg", [128, 2, 64])
    lam_d = din("lamv", [1, 4, 64])
    subg_d = din("subg", [128, 128])
    fw1_d = din("fw1", [33, 64])
    fw2_d = din("fw2", [64, 64])
    fw3_d = din("fw3", [64, 64])
    fbf_d = din("fbf", [64, 4])
    fwo_d = din("fwo", [64, 2048])
    skip_d = din("skipbc", [32, 2, 512])
    wpa_d = din("wpa", [512, D])
    wph_d = din("wph", [512, D])
    wo_d = din("wo", [D, D])
    g2bc_d = din("g2bc", [128, D])
    wr_d = din("wr", [128, 8, 16])
    wg_d = din("wg", [NE, D, D])
    wu_d = din("wu", [NE, D, D])
    wd_d = din("wd", [NE, D, D])
    ropecs_d = din("ropecs", [128, NT, 16])
    embT_d = din("embT", [33, L])
    posrow_d = din("posrow", [128, L])
    ndelta_d = din("ndelta", [128, 4])
    FA_d = din("FA", [32, 2 * NK], BF16)
    TGr_d = din("TGr", [128, NK, 128], BF16)
    TGi_d = din("TGi", [128, NK, 128], BF16)
    TGn_d = din("TGn", [128, NK, 128], BF16)
    R1_d = din("R1", [128, 256], BF16)
    R2_d = din("R2", [128, 256], BF16)
    TH_d = din("TH", [NK, 128, 2, 32], BF16)
    tidhl_d = din("tidhl", [128, NT, 2], BF16)
    iota_d = din("iota512", [128, 512])
    tri_d = din("tri", [128, 128])

    out_d = nc.dram_tensor("out", [L, D], F32, kind="ExternalOutput").ap()

    qT_d = dscr("qT_s", [4, 128, L], BF16)
    kT_d = dscr("kT_s", [4, 128, L], BF16)
    v_d = dscr("v_s", [L, 512], BF16)
    hyc_d = dscr("hyc_s", [1536, L], F32)
    gateT_d = dscr("gateT_s", [2048, L], BF16)
    kern_d = dscr("kern_s", [2, 2, 512, L], BF16)
    attnT_d = dscr("attnT_s", [512, L], BF16)
    hyoT_d = dscr("hyoT_s", [512, L], BF16)
    u2_d = dscr("u2_s", [L, D], BF16)

    dbg_d = {}
    for k, (shape, dt) in ((k, v) for k, v in dbg.items() if not k.startswith("_")):
        dbg_d[k] = nc.dram_tensor("dbg_" + k, list(shape), dt, kind="ExternalOutput").ap()

    T = Trk(nc)
    top = ExitStack()

    sbc = [0]

    def sb(es, name, shape, dt=F32):
        sbc[0] += 1
        return es.enter_context(nc.sbuf_tensor("sb%d_%s" % (sbc[0], name), list(shape), dt))

    ps = [top.enter_context(nc.psum_tensor("ps%d" % i, [128, 512], F32)) for i in range(8)]
    PK = ["ps%d" % i for i in range(8)]

    ident_f = sb(top, "ident_f", [128, 128])
    ident_b = sb(top, "ident_b", [128, 128], BF16)
    eps = sb(top, "eps", [128, 1])
    T.op("pool", lambda e: e.memset(ident_f[:], 0.0), w=["ident_f"])
    T.op("pool", lambda e: e.affine_select(out=ident_f[:], in_=ident_f[:], pattern=[[-1, 128]],
                                           compare_op=ALU.not_equal, fill=1.0, base=0, channel_multiplier=1),
         r=["ident_f"], w=["ident_f"])
    T.op("pool", lambda e: e.tensor_copy(out=ident_b[:], in_=ident_f[:]), r=["ident_f"], w=["ident_b"])
    T.op("pool", lambda e: e.memset(eps[:], 1e-6), w=["eps"])

    stages = dbg.get("_stages", "AKBCDEF") if isinstance(dbg.get("_stages", None), str) else "AKBCDEF"

    if "A" in stages:
      with ExitStack() as es:
        uT = sb(es, "uT", [128, 8, L + 2], BF16)
        g1bc = sb(es, "g1bc", [128, D])
        scw = sb(es, "scw", [128, 12, 4])
        qkg = sb(es, "qkg", [128, 2, 64])
        ropecs = sb(es, "ropecs", [128, NT, 16])
        xt = [sb(es, "xt%d" % i, [128, D]) for i in range(4)]
        xb = [sb(es, "xb%d" % i, [128, D], BF16) for i in range(4)]
        junk = sb(es, "junkA", [128, D])
        ss = sb(es, "ssA", [128, NT])
        rs = sb(es, "rsA", [128, NT])
        wb = [sb(es, "wb%d" % i, [128, 8, 512], BF16) for i in range(2)]
        T.dma("sp", lambda e: e.dma_start(out=g1bc[:], in_=g1bc_d[:, :]), w=["g1bc"])
        T.dma("sp", lambda e: e.dma_start(out=scw[:], in_=scw_d[:, :, :]), w=["scw"])
        T.dma("sp", lambda e: e.dma_start(out=qkg[:], in_=qkg_d[:, :, :]), w=["qkg"])
        T.dma("sp", lambda e: e.dma_start(out=ropecs[:], in_=ropecs_d[:, :, :]), w=["ropecs"])
        T.op("dve", lambda e: e.memset(ss[:], 0.0), w=["ssA"])
        T.op("dve", lambda e: e.memset(uT[:, :, 0:1], 0.0), w=["uTpad0"])
        T.op("dve", lambda e: e.memset(uT[:, :, L + 1:L + 2], 0.0), w=["uTpad1"])
        T.op("dve", lambda e: e.tensor_scalar(out=qkg[:, 0, :], in0=qkg[:, 0, :], scalar1=0.125, scalar2=None,
                                              op0=ALU.mult), r=["qkg"], w=["qkg"])
        psb = [ps[6][:, :].bitcast(BF16), ps[7][:, :].bitcast(BF16)]
        for i in range(NT):
            b = i % 4
            pb2 = i % 2
            T.dma("sp", lambda e: e.dma_start(out=xt[b][:], in_=x_d[i * 128:(i + 1) * 128, :]), w=["xt%d" % b])
            T.op("act", lambda e: e.activation(out=junk[:], in_=xt[b][:], func=AF.Square,
                                               accum_out=ss[:, i:i + 1]), r=["xt%d" % b, "ssA"], w=["junkA", "ss%d" % i])
            T.op("act", lambda e: e.activation(out=rs[:, i:i + 1], in_=ss[:, i:i + 1], func=AF.Sqrt,
                                               scale=1.0 / D, bias=eps[:]), r=["ss%d" % i, "eps"], w=["rs%d" % i])
            T.op("dve", lambda e: e.reciprocal(out=rs[:, i:i + 1], in_=rs[:, i:i + 1]), r=["rs%d" % i], w=["rs%d" % i])
            T.op("dve", lambda e: e.scalar_tensor_tensor(out=xb[b][:], in0=xt[b][:], scalar=rs[:, i:i + 1], in1=g1bc[:],
                                                         op0=ALU.mult, op1=ALU.mult),
                 r=["xt%d" % b, "rs%d" % i, "g1bc"], w=["xb%d" % b])
            for j in range(8):
                T.op("pe", lambda e: e.transpose(out=psb[pb2][:, j * 128:(j + 1) * 128],
                                                 in_=xb[b][:, j * 128:(j + 1) * 128], identity=ident_b[:]),
                     r=["xb%d" % b, "ident_b"], w=[PK[6 + pb2]])
            T.op("act", lambda e: e.copy(out=uT[:, :, 1 + i * 128:1 + (i + 1) * 128],
                                         in_=psb[pb2].rearrange("p (j t) -> p j t", j=8)),
                 r=[PK[6 + pb2]], w=["uT%d" % i])
        UTK = ["uT%d" % i for i in range(NT)] + ["uTpad0", "uTpad1"]

        sq = sb(es, "sqA", [128, 512])
        ssq = sb(es, "ssqA", [128, 8])
        qn = sb(es, "qnA", [128, 8, 64])
        qb2 = [sb(es, "qbA%d" % i, [128, 512], BF16) for i in range(2)]
        rt = [sb(es, "rtA%d" % i, [128, 8, 8]) for i in range(4)]
        qTs = [sb(es, "qTs%d" % i, [128, 4, 128], BF16) for i in range(2)]
        vst = [sb(es, "vst%d" % i, [128, 512], BF16) for i in range(2)]
        hraw = sb(es, "hraw", [128, L + 2])
        hc = sb(es, "hcA", [128, L])
        gst = sb(es, "gstA", [128, L], BF16)
        T.op("pool", lambda e: e.memset(hraw[:, 0:1], 0.0), w=["hrawp0"])
        T.op("pool", lambda e: e.memset(hraw[:, L + 1:L + 2], 0.0), w=["hrawp1"])
        pcnt = [0]

        def nextps():
            pcnt[0] += 1
            return pcnt[0] % 4

        for cc in dbg.get('_ccs', range(10)):
            wbk = "wb%d" % (cc % 2)
            wbt = wb[cc % 2]
            T.dmas("pool", [(lambda e, a=a: e.dma_start(
                out=wbt[:, a:a + 4, :], in_=w_in_d.rearrange("(j p) c -> p j c", p=128)[:, a:a + 4, cc * 512:(cc + 1) * 512]))
                for a in (0, 4)], w=[wbk])
            if cc < 3:
                def qk_tail(i):
                    qbt = qb2[i % 2]
                    pb = 6 + (i % 2)
                    for h in range(4):
                        T.op("pe", lambda e: e.transpose(out=psb[i % 2][:, h * 128:(h + 1) * 128],
                                                         in_=qbt[:, h * 128:(h + 1) * 128], identity=ident_b[:]),
                             r=["qbA%d" % (i % 2), "ident_b"], w=[PK[pb]])
                    st = qTs[i % 2]
                    T.op("act", lambda e: e.copy(out=st[:], in_=psb[i % 2][:, 0:512].rearrange("p (h t) -> p h t", h=4)),
                         r=[PK[pb]], w=["qTs%d" % (i % 2)])
                    dst = (qT_d if cc == 0 else kT_d).rearrange("h p t -> p h t")[:, :, i * 128:(i + 1) * 128]
                    T.dma("sp", lambda e: e.dma_start(out=dst, in_=st[:]), r=["qTs%d" % (i % 2)], w=["qkT_d"])

                for i in range(NT):
                    pi = nextps()
                    for j in range(8):
                        T.op("pe", lambda e: e.matmul(ps[pi][:, :], lhsT=uT[:, j, 1 + i * 128:1 + (i + 1) * 128],
                                                      rhs=wbt[:, j, :], start=(j == 0), stop=(j == 7)),
                             r=["uT%d" % i, wbk], w=[PK[pi]])
                    if cc == 2:
                        vs = vst[i % 2]
                        T.op("act", lambda e: e.copy(out=vs[:], in_=ps[pi][:, :]), r=[PK[pi]], w=["vst%d" % (i % 2)])
                        T.dma("sp", lambda e: e.dma_start(out=v_d[i * 128:(i + 1) * 128, :], in_=vs[:]),
                              r=["vst%d" % (i % 2)], w=["v_d"])
                        continue
                    if i > 0:
                        qk_tail(i - 1)
                    qb = qb2[i % 2]
                    qbk = "qbA%d" % (i % 2)
                    T.op("act", lambda e: e.activation(out=sq[:], in_=ps[pi][:, :], func=AF.Square), r=[PK[pi]], w=["sqA"])
                    T.op("dve", lambda e: e.tensor_reduce(out=ssq[:], in_=sq[:].rearrange("p (g d) -> p g d", d=64),
                                                          axis=AX.X, op=ALU.add), r=["sqA"], w=["ssqA"])
                    T.op("act", lambda e: e.activation(out=ssq[:], in_=ssq[:], func=AF.Sqrt, scale=1.0 / 64,
                                                       bias=eps[:]), r=["ssqA", "eps"], w=["ssqA"])
                    T.op("dve", lambda e: e.reciprocal(out=ssq[:], in_=ssq[:]), r=["ssqA"], w=["ssqA"])
                    T.op("dve", lambda e: e.tensor_tensor(out=qn[:], in0=ps[pi][:, :].rearrange("p (g d) -> p g d", d=64),
                                                          in1=ssq[:, :].unsqueeze(2).broadcast_to([128, 8, 64]),
                                                          op=ALU.mult), r=[PK[pi], "ssqA"], w=["qnA"])
                    T.op("dve", lambda e: e.tensor_tensor(out=qn[:], in0=qn[:],
                                                          in1=qkg[:, cc, :].unsqueeze(1).broadcast_to([128, 8, 64]),
                                                          op=ALU.mult), r=["qnA", "qkg"], w=["qnA"])
                    T.op("act", lambda e: e.copy(out=qb[:].rearrange("p (g d) -> p g d", d=64), in_=qn[:]),
                         r=["qnA"], w=[qbk])
                    cosb = ropecs[:, i, 0:8].unsqueeze(1).broadcast_to([128, 8, 8])
                    sinb = ropecs[:, i, 8:16].unsqueeze(1).broadcast_to([128, 8, 8])
                    r1 = qn[:, :, 0:8]
                    r2 = qn[:, :, 8:16]
                    T.op("dve", lambda e: e.tensor_tensor(out=rt[0][:], in0=r1, in1=cosb, op=ALU.mult), r=["qnA", "ropecs"], w=["rt0"])
                    T.op("dve", lambda e: e.tensor_tensor(out=rt[1][:], in0=r2, in1=sinb, op=ALU.mult), r=["qnA", "ropecs"], w=["rt1"])
                    T.op("dve", lambda e: e.tensor_tensor(out=rt[2][:], in0=r2, in1=cosb, op=ALU.mult), r=["qnA", "ropecs"], w=["rt2"])
                    T.op("dve", lambda e: e.tensor_tensor(out=rt[3][:], in0=r1, in1=sinb, op=ALU.mult), r=["qnA", "ropecs"], w=["rt3"])
                    qbv = qb[:].rearrange("p (g d) -> p g d", d=64)
                    T.op("dve", lambda e: e.tensor_tensor(out=qbv[:, :, 0:8], in0=rt[0][:], in1=rt[1][:], op=ALU.subtract),
                         r=["rt0", "rt1"], w=[qbk])
                    T.op("dve", lambda e: e.tensor_tensor(out=qbv[:, :, 8:16], in0=rt[2][:], in1=rt[3][:], op=ALU.add),
                         r=["rt2", "rt3"], w=[qbk])
                if cc < 2:
                    qk_tail(NT - 1)
            else:
                for ct in range(4):
                    for tq in range(8):
                        pi = nextps()
                        for j in range(8):
                            T.op("pe", lambda e: e.matmul(ps[pi][:, :], lhsT=wbt[:, j, ct * 128:(ct + 1) * 128],
                                                          rhs=uT[:, j, 1 + tq * 512:1 + (tq + 1) * 512],
                                                          start=(j == 0), stop=(j == 7)),
                                 r=UTK[4 * tq:4 * tq + 4] + [wbk], w=[PK[pi]])
                        if cc < 6:
                            T.op("act", lambda e: e.copy(out=hraw[:, 1 + tq * 512:1 + (tq + 1) * 512], in_=ps[pi][:, :]),
                                 r=[PK[pi]], w=["hraw"])
                        else:
                            T.op("act", lambda e: e.activation(out=gst[:, tq * 512:(tq + 1) * 512], in_=ps[pi][:, :],
                                                               func=AF.Sigmoid), r=[PK[pi]], w=["gstA"])
                    if cc < 6:
                        cti = (cc - 3) * 4 + ct
                        lvl = dbg.get('_lvl', 9)
                        if lvl >= 1:
                          T.op("act", lambda e: e.activation(out=hc[:], in_=hraw[:, 1:L + 1], func=AF.Identity,
                                                           scale=scw[:, cti, 1:2], bias=scw[:, cti, 3:4]),
                             r=["hraw", "scw"], w=["hcA"])
                        if lvl >= 2:
                          T.op("dve", lambda e: e.scalar_tensor_tensor(out=hc[:], in0=hraw[:, 0:L], scalar=scw[:, cti, 0:1],
                                                                     in1=hc[:], op0=ALU.mult, op1=ALU.add),
                             r=["hraw", "hrawp0", "scw", "hcA"], w=["hcA"])
                          T.op("dve", lambda e: e.scalar_tensor_tensor(out=hc[:], in0=hraw[:, 2:L + 2], scalar=scw[:, cti, 2:3],
                                                                     in1=hc[:], op0=ALU.mult, op1=ALU.add),
                             r=["hraw", "hrawp1", "scw", "hcA"], w=["hcA"])
                        if lvl >= 3:
                          for q4 in range(4):
                              T.dma("sp", lambda e: e.dma_start(out=hyc_d[cti * 128:(cti + 1) * 128, q4 * 1024:(q4 + 1) * 1024],
                                                                in_=hc[:, q4 * 1024:(q4 + 1) * 1024]),
                                    r=["hcA"], w=["hyc_d%d" % q4])
                    else:
                        gti = (cc - 6) * 4 + ct
                        T.dmas("sp", [(lambda e, a=a: e.dma_start(out=gateT_d[gti * 128:(gti + 1) * 128, a:a + 2048], in_=gst[:, a:a + 2048]))
                                      for a in (0, 2048)], r=["gstA"], w=["gateT_d"])
        T.barrier()

    if "K" in stages:
      with ExitStack() as es:
        embT = sb(es, "embT", [33, L])
        fw1 = sb(es, "fw1", [33, 64])
        fw2 = sb(es, "fw2", [64, 64])
        fw3 = sb(es, "fw3", [64, 64])
        fbf = sb(es, "fbf", [64, 4])
        frb = sb(es, "frb", [64, 3])
        fwo = sb(es, "fwo", [64, 2048])
        posrow = sb(es, "posrow", [128, L])
        ndelta = sb(es, "ndelta", [128, 4])
        hid = [sb(es, "hid%d" % i, [64, L]) for i in range(2)]
        hidb = sb(es, "hidb", [64, L])
        dec = sb(es, "decK", [128, L])
        hpm = [[sb(es, "hpmK%d%d" % (o, pm), [128, L], BF16) for pm in range(2)] for o in range(2)]
        wpm = sb(es, "wpmK", [64, 2, 2, 512])
        for tns, src, k in ((fw1, fw1_d, "fw1"), (fw2, fw2_d, "fw2"), (fw3, fw3_d, "fw3"),
                            (fbf, fbf_d, "fbf"), (ndelta, ndelta_d, "ndelta")):
            T.dma("sp", lambda e: e.dma_start(out=tns[:], in_=src), w=[k])
        for tns, src, k, n in ((embT, embT_d, "embT", L), (fwo, fwo_d, "fwo", 2048), (posrow, posrow_d, "posrow", L)):
            T.dmas("sp", [(lambda e, a=a: e.dma_start(out=tns[:, a:a + 1024], in_=src[:, a:a + 1024]))
                          for a in range(0, n, 1024)], w=[k])
        for l in range(3):
            T.op("dve", lambda e: e.tensor_tensor(out=frb[:, l:l + 1], in0=fbf[:, l:l + 1], in1=fbf[:, 3:4], op=ALU.mult),
                 r=["fbf"], w=["frb"])
        TWO_PI = 2.0 * math.pi
        srcs = [(embT, "embT", 33, fw1, "fw1"), (hid[0], "hid0", 64, fw2, "fw2"), (hid[1], "hid1", 64, fw3, "fw3")]
        outs = [(hid[0], "hid0"), (hid[1], "hid1"), (hid[0], "hid0")]
        negpi = sb(es, "negpi", [64, 1])
        T.op("dve", lambda e: e.memset(negpi[:], 4.0 * math.pi), w=["negpi"])
        kcnt = sb(es, "kcntK", [64, L])
        for l in range(3):
            src, sk, kk, w_, wk = srcs[l]
            dst, dk = outs[l]
            for tq in range(8):
                pi = tq % 4
                T.op("pe", lambda e: e.matmul(ps[pi][0:64, :], lhsT=w_[0:kk, :], rhs=src[0:kk, tq * 512:(tq + 1) * 512],
                                              start=True, stop=True), r=[sk, wk], w=[PK[pi]])
                T.op("dve", lambda e: e.tensor_scalar(out=hidb[:, tq * 512:(tq + 1) * 512], in0=ps[pi][0:64, :],
                                                      scalar1=fbf[:, 3:4], scalar2=frb[:, l:l + 1], op0=ALU.mult,
                                                      op1=ALU.add), r=[PK[pi], "fbf", "frb"], w=["hidb"])
                xs_ = hidb[:, tq * 512:(tq + 1) * 512]
                ks_ = kcnt[:, tq * 512:(tq + 1) * 512]
                T.op("dve", lambda e: e.tensor_scalar(out=ks_, in0=xs_, scalar1=-3.0 * math.pi, scalar2=None, op0=ALU.is_gt),
                     r=["hidb"], w=["kcnt"])
                for thr in (-math.pi, math.pi, 3.0 * math.pi):
                    T.op("dve", lambda e: e.scalar_tensor_tensor(out=ks_, in0=xs_, scalar=thr, in1=ks_, op0=ALU.is_gt, op1=ALU.add),
                         r=["hidb", "kcnt"], w=["kcnt"])
                T.op("dve", lambda e: e.scalar_tensor_tensor(out=xs_, in0=ks_, scalar=-TWO_PI, in1=xs_, op0=ALU.mult, op1=ALU.add),
                     r=["hidb", "kcnt"], w=["hidb"])
                T.op("act", lambda e: e.activation(out=dst[:, tq * 512:(tq + 1) * 512], in_=xs_,
                                                   func=AF.Sin, bias=negpi[:]), r=["hidb", "negpi"], w=[dk])
        h3 = hid[0]
        for o in range(2):
            wf_ = fwo[:, (o * 2) * 512:(o * 2 + 1) * 512]
            wb_ = fwo[:, (o * 2 + 1) * 512:(o * 2 + 2) * 512]
            T.op("dve", lambda e: e.tensor_tensor(out=wpm[:, o, 0, :], in0=wf_, in1=wb_, op=ALU.add), r=["fwo"], w=["wpmK"])
            T.op("dve", lambda e: e.tensor_tensor(out=wpm[:, o, 1, :], in0=wf_, in1=wb_, op=ALU.subtract), r=["fwo"], w=["wpmK"])
        for ct in range(4):
            T.op("act", lambda e: e.activation(out=dec[:], in_=posrow[:], func=AF.Exp, scale=ndelta[:, ct:ct + 1]),
                 r=["posrow", "ndelta"], w=["decK"])
            for o in range(2):
                for pm in range(2):
                    dst = hpm[o][pm]
                    dk = "hpmK%d%d" % (o, pm)
                    for tq in range(8):
                        pi = tq % 4
                        T.op("pe", lambda e: e.matmul(ps[pi][:, :], lhsT=wpm[:, o, pm, ct * 128:(ct + 1) * 128],
                                                      rhs=h3[:, tq * 512:(tq + 1) * 512], start=True, stop=True),
                             r=["hid0", "wpmK"], w=[PK[pi]])
                        T.op("dve", lambda e: e.tensor_tensor(out=dst[:, tq * 512:(tq + 1) * 512], in0=ps[pi][:, :],
                                                              in1=dec[:, tq * 512:(tq + 1) * 512], op=ALU.mult),
                             r=[PK[pi], "decK"], w=[dk])
                colf = (o * 2) * 512 + ct * 128
                T.op("pe", lambda e: e.matmul(ps[4][:, 0:1], lhsT=fwo[:, colf:colf + 128], rhs=h3[:, 0:1], start=True, stop=True),
                     r=["hid0", "fwo"], w=[PK[4]])
                for pm in range(2):
                    T.op("dve", lambda e: e.tensor_tensor(out=hpm[o][pm][:, 0:1], in0=ps[4][:, 0:1], in1=dec[:, 0:1], op=ALU.mult),
                         r=[PK[4], "decK"], w=["hpmK%d%d" % (o, pm)])
                for pm in range(2):
                    T.dmas("sp", [(lambda e, a=a: e.dma_start(out=kern_d[o, pm, ct * 128:(ct + 1) * 128, a:a + 2048],
                                                              in_=hpm[o][pm][:, a:a + 2048]))
                                  for a in range(0, L, 2048)], r=["hpmK%d%d" % (o, pm)], w=["kern_d%d%d" % (o, pm)])
        T.barrier()

    if "B" in stages:
      with ExitStack() as es:
        qT = sb(es, "qT", [128, 4, L], BF16)
        kT = sb(es, "kT", [128, 2, 4, L], BF16)
        V = sb(es, "Vsb", [128, NT, 4, 132], BF16)
        lamv = sb(es, "lamv", [1, 4, 64])
        lamt = sb(es, "lamt", [1, 8])
        ones1 = sb(es, "ones1", [1, 128])
        nlam = sb(es, "nlam", [128, 1])
        subg = sb(es, "subg", [128, 128])
        E = [sb(es, "E%d" % i, [128, 512], BF16) for i in range(3)]
        osb = sb(es, "osbB", [128, 2, 4, 129])
        aacc = sb(es, "aacc", [128, 4, 128])
        ab = sb(es, "abB", [128, 4, 128], BF16)
        junkb = sb(es, "junkB", [128, 4, 128])
        sm = sb(es, "smB", [128, 8, 4])
        aTs = [sb(es, "aTs%d" % i, [128, 512], BF16) for i in range(2)]
        T.dmas("sp", [(lambda e, h=h, a=a: e.dma_start(out=qT[:, h, a:a + 2048], in_=qT_d[h, :, a:a + 2048]))
                      for h in range(4) for a in (0, 2048)], w=["qT"])
        T.op("pool", lambda e: e.memset(kT[64:128, 0, :, :], 0.0), w=["kTz0"])
        T.op("dve", lambda e: e.memset(kT[0:64, 1, :, :], 0.0), w=["kTz1"])
        T.dmas("sp", [(lambda e, h=h, a=a, m=m: e.dma_start(out=kT[m * 64:(m + 1) * 64, m, h, a:a + 2048],
                                                            in_=kT_d[h, m * 64:(m + 1) * 64, a:a + 2048]))
                      for m in range(2) for h in range(4) for a in (0, 2048)], w=["kT"])
        T.dmas("sp", [(lambda e, i=i: e.dma_start(out=V[:, i, :, 0:128],
                                                  in_=v_d[i * 128:(i + 1) * 128, :].rearrange("p (h d) -> p h d", d=128)))
                      for i in range(NT)], w=["V"])
        T.op("pool", lambda e: e.memset(V[:, :, :, 128:129], 1.0), w=["Vones"])
        T.dma("sp", lambda e: e.dma_start(out=lamv[:], in_=lam_d[:, :, :]), w=["lamv"])
        T.dma("sp", lambda e: e.dma_start(out=subg[:], in_=subg_d[:, :]), w=["subg"])
        T.op("dve", lambda e: e.tensor_scalar(out=subg[:], in0=subg[:], scalar1=0.8, scalar2=None, op0=ALU.mult),
             r=["subg"], w=["subg"])
        T.op("dve", lambda e: e.memset(ones1[:], 1.0), w=["ones1"])
        T.op("dve", lambda e: e.memset(lamt[:], 0.0), w=["lamt"])
        T.op("dve", lambda e: e.tensor_tensor(out=lamv[:, 0, :], in0=lamv[:, 0, :], in1=lamv[:, 1, :], op=ALU.mult), r=["lamv"], w=["lamv"])
        T.op("dve", lambda e: e.tensor_tensor(out=lamv[:, 2, :], in0=lamv[:, 2, :], in1=lamv[:, 3, :], op=ALU.mult), r=["lamv"], w=["lamv"])
        T.op("dve", lambda e: e.tensor_reduce(out=lamt[:, 0:1], in_=lamv[:, 0, :], axis=AX.X, op=ALU.add), r=["lamv", "lamt"], w=["lamt"])
        T.op("dve", lambda e: e.tensor_reduce(out=lamt[:, 1:2], in_=lamv[:, 2, :], axis=AX.X, op=ALU.add), r=["lamv", "lamt"], w=["lamt"])
        T.op("act", lambda e: e.activation(out=lamt[:, 2:4], in_=lamt[:, 0:2], func=AF.Exp), r=["lamt"], w=["lamt"])
        T.op("dve", lambda e: e.tensor_tensor(out=lamt[:, 4:5], in0=lamt[:, 3:4], in1=lamt[:, 2:3], op=ALU.subtract), r=["lamt"], w=["lamt"])
        T.op("dve", lambda e: e.tensor_scalar(out=lamt[:, 5:6], in0=lamt[:, 4:5], scalar1=-0.2, scalar2=None, op0=ALU.add), r=["lamt"], w=["lamt"])
        T.op("pe", lambda e: e.matmul(ps[7][:, 0:1], lhsT=ones1[:, :], rhs=lamt[:, 5:6], start=True, stop=True),
             r=["ones1", "lamt"], w=[PK[7]])
        T.op("dve", lambda e: e.tensor_copy(out=nlam[:], in_=ps[7][:, 0:1]), r=[PK[7]], w=["nlam"])
        psT = ps[6][:, :].bitcast(BF16)
        steps = [(h, qc, m, kt) for h in range(4) for qc in range(8) for m in range(2) for kt in range(NT)]
        USE_POW = False

        def issue_S(idx):
            h, qc, m, kt = steps[idx]
            sp_ = idx % 2
            eb = idx % 3
            T.op("pe", lambda e: e.matmul(ps[sp_][:, :], lhsT=kT[:, m, h, kt * 128:(kt + 1) * 128],
                                          rhs=qT[:, h, qc * 512:(qc + 1) * 512], start=True, stop=True),
                 r=["kT", "kTz0", "kTz1", "qT"], w=[PK[sp_]])
            T.op("act", lambda e: e.activation(out=E[eb][:], in_=ps[sp_][:, :], func=AF.Exp),
                 r=[PK[sp_]], w=["E%d" % eb])

        def issue_PV(idx):
            h, qc, m, kt = steps[idx]
            eb = idx % 3
            for qs in range(4):
                bank = 2 + m * 2 + qs // 2
                T.op("pe", lambda e: e.matmul(ps[bank][:, (qs % 2) * 256:(qs % 2) * 256 + 129],
                                              lhsT=E[eb][:, qs * 128:(qs + 1) * 128], rhs=V[:, kt, h, 0:129],
                                              start=(kt == 0 and qs % 2 == 0), stop=(kt == NT - 1),
                                              skip_group_check=True),
                     r=["E%d" % eb, "V", "Vones"], w=[PK[bank]])

        def bc4(ap2):
            return ap2.unsqueeze(2).broadcast_to([128, 4, 128])

        def epi_head():
            for m in range(2):
                for half in range(2):
                    bank = 2 + m * 2 + half
                    src = ps[bank][:, :].rearrange("p (a c) -> p a c", a=2)[:, :, 0:129]
                    dst = osb[:, m, half * 2:(half + 1) * 2, :]
                    if half == 0:
                        T.op("dve", lambda e: e.tensor_copy(out=dst, in_=src), r=[PK[bank]], w=["osb%d%d" % (m, half)])
                    else:
                        T.op("pool" if False else "dve", lambda e: e.tensor_copy(out=dst, in_=src), r=[PK[bank]], w=["osb%d%d" % (m, half)])
            OS = ["osb00", "osb01", "osb10", "osb11"]
            T.op("dve", lambda e: e.reciprocal(out=sm[:, 0:2, :], in_=osb[:, :, :, 128]), r=OS, w=["smB"])
            T.op("dve", lambda e: e.tensor_scalar(out=sm[:, 2, :], in0=sm[:, 1, :], scalar1=nlam[:, 0:1], scalar2=None, op0=ALU.mult),
                 r=["smB", "nlam"], w=["smB"])
            T.op("dve", lambda e: e.tensor_tensor(out=aacc[:], in0=osb[:, 0, :, 0:128], in1=bc4(sm[:, 0, :]), op=ALU.mult),
                 r=OS + ["smB"], w=["aacc"])
            T.op("dve", lambda e: e.tensor_tensor(out=junkb[:], in0=osb[:, 1, :, 0:128], in1=bc4(sm[:, 2, :]), op=ALU.mult),
                 r=OS + ["smB"], w=["junkB"])
            T.op("dve", lambda e: e.tensor_tensor(out=aacc[:], in0=aacc[:], in1=junkb[:], op=ALU.add), r=["aacc", "junkB"], w=["aacc"])
            T.op("dve", lambda e: e.tensor_tensor(out=junkb[:], in0=aacc[:], in1=aacc[:], op=ALU.mult), r=["aacc"], w=["junkB"])
            T.op("dve", lambda e: e.tensor_reduce(out=sm[:, 3, :], in_=junkb[:], axis=AX.X, op=ALU.add), r=["junkB", "smB"], w=["smB"])
            T.op("dve", lambda e: e.tensor_scalar(out=sm[:, 4, :], in0=sm[:, 3, :], scalar1=1.0 / 128, scalar2=1e-6, op0=ALU.mult, op1=ALU.add),
                 r=["smB"], w=["smB"])
            if USE_POW:
                T.op("dve", lambda e: e.tensor_scalar(out=sm[:, 5, :], in0=sm[:, 4, :], scalar1=-0.5, scalar2=None, op0=ALU.pow),
                     r=["smB"], w=["smB"])
            else:
                T.op("act", lambda e: e.activation(out=sm[:, 6, :], in_=sm[:, 4, :], func=AF.Sqrt), r=["smB"], w=["smB"])
                T.op("dve", lambda e: e.reciprocal(out=sm[:, 5, :], in_=sm[:, 6, :]), r=["smB"], w=["smB"])
            T.op("dve", lambda e: e.tensor_tensor(out=aacc[:], in0=aacc[:], in1=bc4(sm[:, 5, :]), op=ALU.mult), r=["aacc", "smB"], w=["aacc"])
            T.op("dve", lambda e: e.tensor_tensor(out=ab[:], in0=aacc[:], in1=subg[:, :].unsqueeze(1).broadcast_to([128, 4, 128]), op=ALU.mult),
                 r=["aacc", "subg"], w=["abB"])

        def epi_tail(h, qc):
            for qs in range(4):
                T.op("pe", lambda e: e.transpose(out=psT[:, qs * 128:(qs + 1) * 128], in_=ab[:, qs, :], identity=ident_b[:]),
                     r=["abB", "ident_b"], w=[PK[6]])
            st = aTs[qc % 2]
            T.op("act", lambda e: e.copy(out=st[:], in_=psT[:, 0:512]), r=[PK[6]], w=["aTs%d" % (qc % 2)])
            T.dma("sp", lambda e: e.dma_start(out=attnT_d[h * 128:(h + 1) * 128, qc * 512:(qc + 1) * 512], in_=st[:]),
                  r=["aTs%d" % (qc % 2)], w=["attnT_d"])

        pending = None
        issue_S(0)
        for idx in range(len(steps)):
            h, qc, m, kt = steps[idx]
            if idx + 1 < len(steps):
                issue_S(idx + 1)
            issue_PV(idx)
            if pending is not None and m == 0 and kt == 12:
                epi_tail(*pending)
                pending = None
            if m == 1 and kt == NT - 1:
                epi_head()
                pending = (h, qc)
        epi_tail(*pending)
        T.barrier()

    if "C" in stages:
      with ExitStack() as es:
        FA = sb(es, "FA", [32, 2 * NK], BF16)
        TGr = sb(es, "TGr", [128, NK, 128], BF16)
        TGi = sb(es, "TGi", [128, NK, 128], BF16)
        TGn = sb(es, "TGn", [128, NK, 128], BF16)
        KCS = [(0, 16), (16, 32), (32, NK)]
        R1 = sb(es, "R1", [128, 256], BF16)
        R2 = sb(es, "R2", [128, 256], BF16)
        TH = sb(es, "TH", [NK, 128, 2, 32], BF16)
        skipbc = sb(es, "skipbc", [32, 2, GC])
        for tns, src, k in ((FA, FA_d, "FA"), (R1, R1_d, "R1"), (R2, R2_d, "R2")):
            T.dma("sp", lambda e: e.dma_start(out=tns[:], in_=src), w=[k])
        for tns, src, k in ((TGr, TGr_d, "TGr"), (TGi, TGi_d, "TGi"), (TGn, TGn_d, "TGn")):
            T.dmas("sp", [(lambda e, a=a, b=b: e.dma_start(out=tns[:, a:b, :], in_=src[:, a:b, :])) for (a, b) in KCS], w=[k])
        T.dmas("sp", [(lambda e, a=a: e.dma_start(out=TH[:, a:a + 32, :, :], in_=TH_d[:, a:a + 32, :, :])) for a in range(0, 128, 32)], w=["TH"])
        zf = sb(es, "zf", [32, GC, 128])
        gt = sb(es, "gtC", [32, GC, 128])
        zs = sb(es, "zsC", [32, GC, 128])
        srcb = [sb(es, "srcb%d" % i, [32, GC, 128], BF16) for i in range(3)]
        Bm_ = [sb(es, "Bm%d" % i, [128, 2 * NK, GC], BF16) for i in range(3)]
        srs = sb(es, "srsC", [128, 512])
        sis = sb(es, "sisC", [128, 512])
        tm = [sb(es, "tmC%d" % i, [128, 512]) for i in range(4)]
        Y = sb(es, "YC", [128, GC, 2, NK], BF16)
        Dm = sb(es, "DmC", [NK, 256, GC], BF16)
        tmpy = sb(es, "tmpyC", [32, GC, 16])
        hob = srcb[0]

        def flay(ap2d):
            return ap2d.rearrange("c (a b) -> a c b", b=128)

        def load_filters(g_, o_):
            for pm in range(2):
                T.dma("sp", lambda e: e.dma_start(out=srcb[1 + pm][:], in_=flay(kern_d[o_, pm, g_ * GC:(g_ + 1) * GC, :])),
                      w=["srcb%d" % (1 + pm)])

        for g in range(512 // GC):
            c0 = g * GC
            T.dma("sp", lambda e: e.dma_start(out=zf[:], in_=flay(hyc_d[c0:c0 + GC, :])), w=["zf"])
            T.dma("sp", lambda e: e.dma_start(out=skipbc[:], in_=skip_d[:, :, c0:c0 + GC]), w=["skipbc"])
            for o in range(2):
                T.dma("sp", lambda e: e.dma_start(out=gt[:], in_=flay(hyc_d[512 * (o + 1) + c0:512 * (o + 1) + c0 + GC, :])),
                      w=["gtC"])
                if g == 0 and o == 0:
                    load_filters(0, 0)
                for s_ in (1, 2, 0):
                    if s_ == 0:
                        T.op("act", lambda e: e.copy(out=srcb[0][:], in_=zf[:]), r=["zf"], w=["srcb0"])
                        T.op("pool", lambda e: e.tensor_tensor(out=zs[:], in0=zf[:],
                                                               in1=skipbc[:, o, :].unsqueeze(2).broadcast_to([32, GC, 128]),
                                                               op=ALU.mult), r=["zf", "skipbc"], w=["zsC"])
                    for cq, (ca, cb) in enumerate(((0, 7), (7, 14), (14, 21), (21, 28), (28, 32))):
                        pi = cq % 2
                        ncg = cb - ca
                        for c4 in range(ncg):
                            c = ca + c4
                            T.op("pe", lambda e: e.matmul(ps[pi][:, c4 * 2 * NK:(c4 + 1) * 2 * NK], lhsT=srcb[s_][:, c, :],
                                                          rhs=FA[:, :], start=True, stop=True),
                                 r=["srcb%d" % s_, "FA"], w=[PK[pi]])
                        T.op("act" if cq % 2 == 0 else "dve",
                             lambda e: (e.copy if cq % 2 == 0 else e.tensor_copy)(
                                 out=Bm_[s_][:, :, ca:cb],
                                 in_=ps[pi][:, 0:ncg * 2 * NK].rearrange("p (c f) -> p f c", c=ncg)),
                             r=[PK[pi]], w=["Bm%d" % s_])
                nxt = (g, 1) if o == 0 else (g + 1, 0)
                if nxt[0] < 512 // GC:
                    load_filters(*nxt)
                for kc, (ka, kb_) in enumerate(KCS):
                    bZr, bZi, bSr, bSi = (2, 3, 4, 5) if kc % 2 == 0 else (0, 1, 6, 7)
                    nk = kb_ - ka
                    wd = nk * GC
                    for j in range(nk):
                        k1 = ka + j
                        cs_ = slice(j * GC, (j + 1) * GC)
                        bz, bp, bm = Bm_[0], Bm_[1], Bm_[2]
                        T.op("pe", lambda e: e.matmul(ps[bZr][:, cs_], lhsT=TGr[:, k1, :], rhs=bz[:, k1, :], start=True, stop=False), r=["TGr", "Bm0"], w=[PK[bZr]])
                        T.op("pe", lambda e: e.matmul(ps[bZr][:, cs_], lhsT=TGn[:, k1, :], rhs=bz[:, NK + k1, :], start=False, stop=True), r=["TGn", "Bm0"], w=[PK[bZr]])
                        T.op("pe", lambda e: e.matmul(ps[bZi][:, cs_], lhsT=TGi[:, k1, :], rhs=bz[:, k1, :], start=True, stop=False), r=["TGi", "Bm0"], w=[PK[bZi]])
                        T.op("pe", lambda e: e.matmul(ps[bZi][:, cs_], lhsT=TGr[:, k1, :], rhs=bz[:, NK + k1, :], start=False, stop=True), r=["TGr", "Bm0"], w=[PK[bZi]])
                        T.op("pe", lambda e: e.matmul(ps[bSr][:, cs_], lhsT=TGr[:, k1, :], rhs=bp[:, k1, :], start=True, stop=False), r=["TGr", "Bm1"], w=[PK[bSr]])
                        T.op("pe", lambda e: e.matmul(ps[bSr][:, cs_], lhsT=TGn[:, k1, :], rhs=bp[:, NK + k1, :], start=False, stop=True), r=["TGn", "Bm1"], w=[PK[bSr]])
                        T.op("pe", lambda e: e.matmul(ps[bSi][:, cs_], lhsT=TGi[:, k1, :], rhs=bm[:, k1, :], start=True, stop=False), r=["TGi", "Bm2"], w=[PK[bSi]])
                        T.op("pe", lambda e: e.matmul(ps[bSi][:, cs_], lhsT=TGr[:, k1, :], rhs=bm[:, NK + k1, :], start=False, stop=True), r=["TGr", "Bm2"], w=[PK[bSi]])
                    T.op("act", lambda e: e.copy(out=srs[:, 0:wd], in_=ps[bSr][:, 0:wd]), r=[PK[bSr]], w=["srsC"])
                    T.op("act", lambda e: e.copy(out=sis[:, 0:wd], in_=ps[bSi][:, 0:wd]), r=[PK[bSi]], w=["sisC"])
                    T.op("dve", lambda e: e.tensor_tensor(out=tm[0][:, 0:wd], in0=ps[bZr][:, 0:wd], in1=srs[:, 0:wd], op=ALU.mult), r=[PK[bZr], "srsC"], w=["tm0"])
                    T.op("dve", lambda e: e.tensor_tensor(out=tm[1][:, 0:wd], in0=ps[bZi][:, 0:wd], in1=sis[:, 0:wd], op=ALU.mult), r=[PK[bZi], "sisC"], w=["tm1"])
                    T.op("dve", lambda e: e.tensor_tensor(out=tm[2][:, 0:wd], in0=ps[bZr][:, 0:wd], in1=sis[:, 0:wd], op=ALU.mult), r=[PK[bZr], "sisC"], w=["tm2"])
                    T.op("dve", lambda e: e.tensor_tensor(out=tm[3][:, 0:wd], in0=ps[bZi][:, 0:wd], in1=srs[:, 0:wd], op=ALU.mult), r=[PK[bZi], "srsC"], w=["tm3"])
                    yv_r = Y[:, :, 0, ka:kb_]
                    yv_i = Y[:, :, 1, ka:kb_]
                    T.op("dve", lambda e: e.tensor_tensor(out=yv_r, in0=tm[0][:, 0:wd].rearrange("p (k c) -> p c k", c=GC),
                                                           in1=tm[1][:, 0:wd].rearrange("p (k c) -> p c k", c=GC), op=ALU.subtract),
                         r=["tm0", "tm1"], w=["YC"])
                    T.op("dve", lambda e: e.tensor_tensor(out=yv_i, in0=tm[2][:, 0:wd].rearrange("p (k c) -> p c k", c=GC),
                                                           in1=tm[3][:, 0:wd].rearrange("p (k c) -> p c k", c=GC), op=ALU.add),
                         r=["tm2", "tm3"], w=["YC"])
                for cq in range(GC // 2):
                    pi = cq % 2
                    for c2 in range(2):
                        c = cq * 2 + c2
                        T.op("pe", lambda e: e.matmul(ps[pi][0:NK, c2 * 256:(c2 + 1) * 256], lhsT=Y[:, c, 0, :], rhs=R1[:, :],
                                                      start=True, stop=False), r=["YC", "R1"], w=[PK[pi]])
                        T.op("pe", lambda e: e.matmul(ps[pi][0:NK, c2 * 256:(c2 + 1) * 256], lhsT=Y[:, c, 1, :], rhs=R2[:, :],
                                                      start=False, stop=True), r=["YC", "R2"], w=[PK[pi]])
                    T.op("act" if cq % 2 == 0 else "dve",
                         lambda e: (e.copy if cq % 2 == 0 else e.tensor_copy)(
                             out=Dm[:, :, cq * 2:(cq + 1) * 2],
                             in_=ps[pi][0:NK, :].rearrange("p (c f) -> p f c", c=2)),
                         r=[PK[pi]], w=["DmC"])
                for nq in range(8):
                    pi = 6 + nq % 2
                    for j in range(16):
                        n2 = nq * 16 + j
                        T.op("pe", lambda e: e.matmul(ps[pi][0:32, j * GC:(j + 1) * GC], lhsT=TH[:, n2, 0, :], rhs=Dm[:, n2, :],
                                                      start=True, stop=False), r=["TH", "DmC"], w=[PK[pi]])
                        T.op("pe", lambda e: e.matmul(ps[pi][0:32, j * GC:(j + 1) * GC], lhsT=TH[:, n2, 1, :], rhs=Dm[:, 128 + n2, :],
                                                      start=False, stop=True), r=["TH", "DmC"], w=[PK[pi]])
                    nsl = slice(nq * 16, (nq + 1) * 16)
                    T.op("dve", lambda e: e.tensor_tensor(out=tmpy[:], in0=ps[pi][0:32, :].rearrange("p (n c) -> p c n", c=GC),
                                                          in1=zs[:, :, nsl], op=ALU.add), r=[PK[pi], "zsC"], w=["tmpyC"])
                    T.op("dve", lambda e: e.tensor_tensor(out=zf[:, :, nsl], in0=tmpy[:], in1=gt[:, :, nsl], op=ALU.mult),
                         r=["tmpyC", "gtC", "srcb0", "zsC"], w=["zf"])
            T.op("act", lambda e: e.copy(out=hob[:], in_=zf[:]), r=["zf"], w=["srcb0"])
            T.dma("sp", lambda e: e.dma_start(out=flay(hyoT_d[c0:c0 + GC, :]), in_=hob[:]), r=["srcb0"], w=["hyoT_d"])
        T.barrier()

    if "D" in stages:
      with ExitStack() as es:
        aff_all = sb(es, "aff_all", [128, NT, NE])
        with ExitStack() as es2:
            attnT = sb(es2, "attnT", [128, 4, L], BF16)
            hyoT = sb(es2, "hyoT", [128, 4, L], BF16)
            wpa = sb(es2, "wpa", [128, 4, D], BF16)
            wph = sb(es2, "wph", [128, 4, D], BF16)
            wo = sb(es2, "wo", [128, 8, D], BF16)
            g2bc = sb(es2, "g2bc", [128, D])
            wr = sb(es2, "wr", [128, 8, 16])
            gat2 = [sb(es2, "gatD%d" % i, [128, 16, 512], BF16) for i in range(2)]
            m1 = sb(es2, "m1D", [128, 512])
            m2 = sb(es2, "m2D", [128, 512])
            mT = sb(es2, "mTD", [128, 8, 512], BF16)
            xin = sb(es2, "xinD", [128, D])
            x1 = sb(es2, "x1D", [128, D])
            u2 = sb(es2, "u2D0", [128, D])
            u2b = sb(es2, "u2bD", [128, D], BF16)
            u2T = sb(es2, "u2TD", [128, 8, 128])
            junkd = sb(es2, "junkD", [128, D])
            st = sb(es2, "stD", [128, 8])
            lg = sb(es2, "lgD", [128, NE])
            for (tt_, td_, tk_) in ((attnT, attnT_d, "attnT"), (hyoT, hyoT_d, "hyoT")):
                T.dmas("sp", [(lambda e, j=j, a=a: e.dma_start(out=tt_[:, j, a:a + 2048], in_=td_[j * 128:(j + 1) * 128, a:a + 2048]))
                              for j in range(4) for a in (0, 2048)], w=[tk_])
            T.dma("sp", lambda e: e.dma_start(out=g2bc[:], in_=g2bc_d[:, :]), w=["g2bc"])
            T.dma("sp", lambda e: e.dma_start(out=wr[:], in_=wr_d[:, :, :]), w=["wr"])
            for (wsrc, nj, wdst, wk) in ((wpa_d, 4, wpa, "wpa"), (wph_d, 4, wph, "wph"), (wo_d, 8, wo, "wo")):
                T.dmas("pool", [(lambda e, a=a: e.dma_start(out=wdst[:, a:a + 4, :],
                                                            in_=wsrc.rearrange("(j p) c -> p j c", p=128)[:, a:a + 4, :]))
                                for a in range(0, nj, 4)], w=[wk])
            u2s = [u2, sb(es2, "u2D1", [128, D])]

            def d_tail(i):
                u2t = u2s[i % 2]
                for j in range(8):
                    T.op("pe", lambda e: e.transpose(out=ps[4 + j // 4][:, (j % 4) * 128:(j % 4 + 1) * 128],
                                                     in_=u2t[:, j * 128:(j + 1) * 128], identity=ident_f[:]),
                         r=["u2D%d" % (i % 2), "ident_f"], w=[PK[4 + j // 4]])
                for hh in range(2):
                    T.op("act", lambda e: e.copy(out=u2T[:, hh * 4:(hh + 1) * 4, :],
                                                 in_=ps[4 + hh][:, :].rearrange("p (j t) -> p j t", j=4)),
                         r=[PK[4 + hh]], w=["u2TD"])
                for j in range(8):
                    T.op("pe", lambda e: e.matmul(ps[6][:, 0:NE], lhsT=u2T[:, j, :], rhs=wr[:, j, :], start=(j == 0), stop=(j == 7)),
                         r=["u2TD", "wr"], w=[PK[6]])
                T.op("dve", lambda e: e.tensor_reduce(out=st2[:, 3:4], in_=ps[6][:, 0:NE], axis=AX.X, op=ALU.max), r=[PK[6], "st2D"], w=["st2D"])
                T.op("dve", lambda e: e.tensor_scalar(out=st2[:, 4:5], in0=st2[:, 3:4], scalar1=-1.0, scalar2=None, op0=ALU.mult), r=["st2D"], w=["st2D"])
                T.op("dve", lambda e: e.memset(st2[:, 5:6], 0.0), r=["st2D"], w=["st2D"])
                T.op("act", lambda e: e.activation(out=lg[:], in_=ps[6][:, 0:NE], func=AF.Exp, bias=st2[:, 4:5], accum_out=st2[:, 5:6]),
                     r=[PK[6], "st2D"], w=["lgD", "st2D"])
                T.op("dve", lambda e: e.reciprocal(out=st2[:, 6:7], in_=st2[:, 5:6]), r=["st2D"], w=["st2D"])
                T.op("dve", lambda e: e.tensor_scalar(out=aff_all[:, i, :], in0=lg[:], scalar1=st2[:, 6:7], scalar2=None, op0=ALU.mult),
                     r=["lgD", "st2D"], w=["aff_all"])

            st2 = sb(es2, "st2D", [128, 8])
            prev_i = None
            for tq in range(8):
                tsl = slice(tq * 512, (tq + 1) * 512)
                gat = gat2[tq % 2]
                gk = "gatD%d" % (tq % 2)
                T.dmas("sp", [(lambda e, a=a: e.dma_start(out=gat[:, a:a + 8, :],
                                                          in_=gateT_d.rearrange("(j p) t -> p j t", p=128)[:, a:a + 8, tsl])) for a in (0, 8)],
                       w=[gk])
                for dt in range(8):
                    pa = 0 if dt % 2 == 0 else 7
                    for j in range(4):
                        T.op("pe", lambda e: e.matmul(ps[pa][:, :], lhsT=wpa[:, j, dt * 128:(dt + 1) * 128], rhs=attnT[:, j, tsl],
                                                      start=(j == 0), stop=(j == 3)), r=["wpa", "attnT"], w=[PK[pa]])
                    for j in range(4):
                        T.op("pe", lambda e: e.matmul(ps[1][:, :], lhsT=wph[:, j, dt * 128:(dt + 1) * 128], rhs=hyoT[:, j, tsl],
                                                      start=(j == 0), stop=(j == 3)), r=["wph", "hyoT"], w=[PK[1]])
                    T.op("dve", lambda e: e.tensor_tensor(out=m1[:], in0=ps[pa][:, :], in1=gat[:, dt, :], op=ALU.mult), r=[PK[pa], gk], w=["m1D"])
                    T.op("dve", lambda e: e.tensor_tensor(out=m2[:], in0=ps[1][:, :], in1=gat[:, 8 + dt, :], op=ALU.mult), r=[PK[1], gk], w=["m2D"])
                    T.op("pool", lambda e: e.tensor_tensor(out=mT[:, dt, :], in0=m1[:], in1=m2[:], op=ALU.add), r=["m1D", "m2D"], w=["mTD"])
                for ts in range(4):
                    i = tq * 4 + ts
                    u2t = u2s[i % 2]
                    u2k = "u2D%d" % (i % 2)
                    T.dma("sp", lambda e: e.dma_start(out=xin[:], in_=x_d[i * 128:(i + 1) * 128, :]), w=["xinD"])
                    for half in range(2):
                        for dt in range(8):
                            T.op("pe", lambda e: e.matmul(ps[2 + half][:, :], lhsT=mT[:, dt, ts * 128:(ts + 1) * 128],
                                                          rhs=wo[:, dt, half * 512:(half + 1) * 512], start=(dt == 0), stop=(dt == 7)),
                                 r=["mTD", "wo"], w=[PK[2 + half]])
                        T.op("dve", lambda e: e.tensor_tensor(out=x1[:, half * 512:(half + 1) * 512], in0=ps[2 + half][:, :],
                                                              in1=xin[:, half * 512:(half + 1) * 512], op=ALU.add),
                             r=[PK[2 + half], "xinD"], w=["x1D"])
                    if prev_i is not None:
                        d_tail(prev_i)
                    T.dma("sp", lambda e: e.dma_start(out=out_d[i * 128:(i + 1) * 128, :], in_=x1[:]), r=["x1D"], w=["out_d"])
                    T.op("dve", lambda e: e.memset(st[:, 0:1], 0.0), w=["stD"])
                    T.op("act", lambda e: e.activation(out=junkd[:], in_=x1[:], func=AF.Square, accum_out=st[:, 0:1]),
                         r=["x1D", "stD"], w=["junkD", "stD"])
                    T.op("act", lambda e: e.activation(out=st[:, 1:2], in_=st[:, 0:1], func=AF.Sqrt, scale=1.0 / D, bias=eps[:]),
                         r=["stD", "eps"], w=["stD"])
                    T.op("dve", lambda e: e.reciprocal(out=st[:, 2:3], in_=st[:, 1:2]), r=["stD"], w=["stD"])
                    T.op("dve", lambda e: e.scalar_tensor_tensor(out=u2t[:], in0=x1[:], scalar=st[:, 2:3], in1=g2bc[:],
                                                                 op0=ALU.mult, op1=ALU.mult), r=["x1D", "stD", "g2bc"], w=[u2k])
                    T.op("act", lambda e: e.copy(out=u2b[:], in_=u2t[:]), r=[u2k], w=["u2bD"])
                    T.dma("sp", lambda e: e.dma_start(out=u2_d[i * 128:(i + 1) * 128, :], in_=u2b[:]), r=["u2bD"], w=["u2_d"])
                    prev_i = i
            d_tail(prev_i)
            T.barrier()
        if "aff" in dbg_d:
            T.dma("sp", lambda e: e.dma_start(out=dbg_d["aff"], in_=aff_all[:]), r=["aff_all"], w=["dbgaff"])

        if "E" in stages:
          idx_all = sb(es, "idx_all", [128, NE, 4], I32)
          g_all = sb(es, "g_all", [128, NE, 4])
          with ExitStack() as es2:
            affT = sb(es2, "affT", [NE, L])
            cmpj = sb(es2, "cmpj", [NE, L])
            bs = sb(es2, "bsE", [NE, 8])
            tri = sb(es2, "tri", [128, 128])
            onesf = sb(es2, "onesf", [128, 128])
            iota = sb(es2, "iota", [128, 512])
            tidhl = sb(es2, "tidhl", [128, NT, 2], BF16)
            thbc = sb(es2, "thbc", [128, NE])
            dg = sb(es2, "dgE", [NE, NE])
            M = sb(es2, "ME", [128, NT, NE])
            pos = sb(es2, "posE", [128, NT, NE])
            srun = sb(es2, "srunE", [128, NE])
            cum = sb(es2, "cumE", [128, NE])
            vals = sb(es2, "valsE", [128, NT, NE, 4], BF16)
            afh = sb(es2, "afhE", [128, NT, NE], BF16)
            afr = sb(es2, "afrE", [128, NT, NE])
            oh = [sb(es2, "ohE%d" % i, [128, 512], BF16) for i in range(4)]
            idf = sb(es2, "idfE", [128, 4])
            pvs = sb(es2, "pvsE", [128, 16])
            T.dma("sp", lambda e: e.dma_start(out=tri[:], in_=tri_d[:, :]), w=["tri"])
            T.dma("sp", lambda e: e.dma_start(out=iota[:], in_=iota_d[:, :]), w=["iota"])
            T.dma("sp", lambda e: e.dma_start(out=tidhl[:], in_=tidhl_d[:, :, :]), w=["tidhl"])
            T.op("pool", lambda e: e.memset(onesf[:], 1.0), w=["onesf"])
            for i in range(NT):
                pi = i // 4
                T.op("pe", lambda e: e.transpose(out=ps[pi][0:NE, (i % 4) * 128:(i % 4 + 1) * 128], in_=aff_all[:, i, :],
                                                 identity=ident_f[:]), r=["aff_all", "ident_f"], w=[PK[pi]])
                if i % 4 == 3:
                    T.op("act", lambda e: e.copy(out=affT[:, (i - 3) * 128:(i + 1) * 128], in_=ps[pi][0:NE, :]), r=[PK[pi]], w=["affT"])
            T.op("dve", lambda e: e.memset(bs[:, 0:1], 0.0), w=["bsE"])
            T.op("dve", lambda e: e.memset(bs[:, 1:2], 1.0), r=["bsE"], w=["bsE"])
            for it in range(32):
                T.op("dve", lambda e: e.tensor_scalar(out=bs[:, 2:3], in0=bs[:, 0:1], scalar1=bs[:, 1:2], scalar2=0.5, op0=ALU.add, op1=ALU.mult), r=["bsE"], w=["bsE"])
                T.op("dve", lambda e: e.tensor_scalar(out=bs[:, 7:8], in0=bs[:, 2:3], scalar1=-1.0, scalar2=None, op0=ALU.mult), r=["bsE"], w=["bsE"])
                T.op("dve", lambda e: e.memset(bs[:, 3:4], 0.0), r=["bsE"], w=["bsE"])
                T.op("act", lambda e: e.activation(out=cmpj[:], in_=affT[:], func=AF.Sign, bias=bs[:, 7:8], accum_out=bs[:, 3:4]),
                     r=["affT", "bsE"], w=["cmpj", "bsE"])
                T.op("dve", lambda e: e.tensor_scalar(out=bs[:, 4:5], in0=bs[:, 3:4], scalar1=float(2 * CAP - L - 1), scalar2=None, op0=ALU.is_ge), r=["bsE"], w=["bsE"])
                T.op("dve", lambda e: e.tensor_tensor(out=bs[:, 5:6], in0=bs[:, 2:3], in1=bs[:, 0:1], op=ALU.subtract), r=["bsE"], w=["bsE"])
                T.op("dve", lambda e: e.tensor_tensor(out=bs[:, 6:7], in0=bs[:, 1:2], in1=bs[:, 2:3], op=ALU.subtract), r=["bsE"], w=["bsE"])
                T.op("dve", lambda e: e.scalar_tensor_tensor(out=bs[:, 0:1], in0=bs[:, 5:6], scalar=bs[:, 4:5], in1=bs[:, 0:1], op0=ALU.mult, op1=ALU.add), r=["bsE"], w=["bsE"])
                T.op("dve", lambda e: e.scalar_tensor_tensor(out=bs[:, 1:2], in0=bs[:, 6:7], scalar=bs[:, 4:5], in1=bs[:, 2:3], op0=ALU.mult, op1=ALU.add), r=["bsE"], w=["bsE"])
            T.op("dve", lambda e: e.tensor_scalar(out=dg[:], in0=ident_f[0:NE, 0:NE], scalar1=bs[:, 0:1], scalar2=None, op0=ALU.mult), r=["bsE", "ident_f"], w=["dgE"])
            T.op("pe", lambda e: e.matmul(ps[0][:, 0:NE], lhsT=onesf[0:NE, :], rhs=dg[:, :], start=True, stop=True), r=["onesf", "dgE"], w=[PK[0]])
            T.op("dve", lambda e: e.tensor_copy(out=thbc[:], in_=ps[0][:, 0:NE]), r=[PK[0]], w=["thbc"])
            T.op("dve", lambda e: e.tensor_tensor(out=M[:], in0=aff_all[:], in1=thbc[:, :].unsqueeze(1).broadcast_to([128, NT, NE]), op=ALU.is_ge),
                 r=["aff_all", "thbc"], w=["ME"])
            T.op("dve", lambda e: e.memset(srun[:], 0.0), w=["srunE"])
            for i in range(NT):
                pi = 1 + i % 2
                T.op("pe", lambda e: e.matmul(ps[pi][:, 0:NE], lhsT=tri[:, :], rhs=M[:, i, :], start=True, stop=True), r=["tri", "ME"], w=[PK[pi]])
                T.op("pe", lambda e: e.matmul(ps[pi][:, NE:2 * NE], lhsT=onesf[:, :], rhs=M[:, i, :], start=True, stop=True), r=["onesf", "ME"], w=[PK[pi]])
                T.op("dve", lambda e: e.tensor_tensor(out=cum[:], in0=ps[pi][:, 0:NE], in1=srun[:], op=ALU.add), r=[PK[pi], "srunE"], w=["cumE"])
                T.op("dve", lambda e: e.tensor_tensor(out=srun[:], in0=ps[pi][:, NE:2 * NE], in1=srun[:], op=ALU.add), r=[PK[pi], "srunE", "cumE"], w=["srunE"])
                T.op("dve", lambda e: e.tensor_tensor(out=cum[:], in0=cum[:], in1=M[:, i, :], op=ALU.mult), r=["cumE", "ME"], w=["cumE"])
                T.op("dve", lambda e: e.tensor_scalar(out=pos[:, i, :], in0=cum[:], scalar1=-1.0, scalar2=None, op0=ALU.add), r=["cumE"], w=["posE"])
            T.op("dve", lambda e: e.tensor_copy(out=afh[:], in_=aff_all[:]), r=["aff_all"], w=["afhE"])
            T.op("dve", lambda e: e.tensor_tensor(out=afr[:], in0=aff_all[:], in1=afh[:], op=ALU.subtract), r=["aff_all", "afhE"], w=["afrE"])
            T.op("dve", lambda e: e.tensor_copy(out=vals[:, :, :, 2], in_=afh[:]), r=["afhE"], w=["valsE"])
            T.op("dve", lambda e: e.tensor_copy(out=vals[:, :, :, 3], in_=afr[:]), r=["afrE", "valsE"], w=["valsE"])
            T.op("dve", lambda e: e.tensor_copy(out=vals[:, :, :, 0:2], in_=tidhl[:, :, :].unsqueeze(2).broadcast_to([128, NT, NE, 2])),
                 r=["tidhl", "valsE"], w=["valsE"])
            oc = 0
            for ex in range(NE):
                pi = 3 + ex % 2
                for i in range(NT):
                    ob = oc % 4
                    eng = "dve"
                    oc += 1
                    T.op(eng, lambda e: e.tensor_scalar(out=oh[ob][:], in0=iota[:], scalar1=pos[:, i, ex:ex + 1], scalar2=None, op0=ALU.is_equal),
                         r=["iota", "posE"], w=["ohE%d" % ob])
                    for sc in range(4):
                        T.op("pe", lambda e: e.matmul(ps[pi][:, sc * 4:(sc + 1) * 4], lhsT=oh[ob][:, sc * 128:(sc + 1) * 128], rhs=vals[:, i, ex, :],
                                                      start=(i == 0 and sc == 0), stop=(i == NT - 1), skip_group_check=True),
                             r=["ohE%d" % ob, "valsE"], w=[PK[pi]])
                T.op("dve", lambda e: e.tensor_copy(out=pvs[:], in_=ps[pi][:, 0:16]), r=[PK[pi]], w=["pvsE"])
                pv = pvs[:, :].rearrange("p (s f) -> p s f", f=4)
                T.op("dve", lambda e: e.scalar_tensor_tensor(out=idf[:], in0=pv[:, :, 0], scalar=64.0, in1=pv[:, :, 1], op0=ALU.mult, op1=ALU.add),
                     r=["pvsE"], w=["idfE"])
                T.op("dve", lambda e: e.tensor_copy(out=idx_all[:, ex, :], in_=idf[:]), r=["idfE"], w=["idx_all"])
                T.op("dve", lambda e: e.tensor_tensor(out=g_all[:, ex, :], in0=pv[:, :, 2], in1=pv[:, :, 3], op=ALU.add), r=["pvsE"], w=["g_all"])
            T.barrier()
          if "idx" in dbg_d:
            T.dma("sp", lambda e: e.dma_start(out=dbg_d["idx"], in_=idx_all[:]), r=["idx_all"], w=["dbgidx"])
            T.dma("sp", lambda e: e.dma_start(out=dbg_d["g"], in_=g_all[:]), r=["g_all"], w=["dbgg"])

          if "F" in stages:
            wgb = [sb(es, "wgb%d" % i, [128, 8, D], BF16) for i in range(2)]
            wub = [sb(es, "wub%d" % i, [128, 8, D], BF16) for i in range(2)]
            wdb = [sb(es, "wdb%d" % i, [128, 8, D], BF16) for i in range(2)]
            xg = [sb(es, "xgF%d" % i, [128, D], BF16) for i in range(4)]
            xinT = sb(es, "xinT", [128, 8, 512], BF16)
            sg = sb(es, "sgF", [128, 512])
            hT = sb(es, "hTF", [128, 8, 512], BF16)
            eo = [sb(es, "eoF%d" % i, [128, D]) for i in range(2)]
            psb = [ps[6][:, :].bitcast(BF16), ps[7][:, :].bitcast(BF16)]

            def load_w(ex):
                for (wsrc, wdst, wk) in ((wg_d, wgb, "wgb"), (wu_d, wub, "wub"), (wd_d, wdb, "wdb")):
                    dst = wdst[ex % 2]
                    T.dmas("pool", [(lambda e, hh=hh: e.dma_start(out=dst[:, hh * 4:(hh + 1) * 4, :],
                                                                  in_=wsrc[ex].rearrange("(j p) c -> p j c", p=128)[:, hh * 4:(hh + 1) * 4, :]))
                                    for hh in range(2)], w=["%s%d" % (wk, ex % 2)])

            def gather(ex):
                for sc in range(4):
                    T.dma("pool", lambda e: e.indirect_dma_start(out=xg[sc][:], out_offset=None, in_=u2_d[:, :],
                                                                 in_offset=bass.IndirectOffsetOnAxis(ap=idx_all[:, ex, sc:sc + 1], axis=0)),
                          r=["idx_all", "u2_d"], w=["xgF%d" % sc])

            load_w(0)
            gather(0)
            for ex in range(NE):
                wgk, wuk, wdk = "wgb%d" % (ex % 2), "wub%d" % (ex % 2), "wdb%d" % (ex % 2)
                wgt, wut, wdt = wgb[ex % 2], wub[ex % 2], wdb[ex % 2]
                if ex + 1 < NE:
                    load_w(ex + 1)
                for sc in range(4):
                    for j in range(8):
                        T.op("pe", lambda e: e.transpose(out=psb[sc % 2][:, j * 128:(j + 1) * 128], in_=xg[sc][:, j * 128:(j + 1) * 128], identity=ident_b[:]),
                             r=["xgF%d" % sc, "ident_b"], w=[PK[6 + sc % 2]])
                    T.op("act", lambda e: e.copy(out=xinT[:, :, sc * 128:(sc + 1) * 128], in_=psb[sc % 2].rearrange("p (j t) -> p j t", j=8)),
                         r=[PK[6 + sc % 2]], w=["xinT"])
                if ex + 1 < NE:
                    gather(ex + 1)
                for ft in range(8):
                    pg, pu = (ft % 2) * 2, (ft % 2) * 2 + 1
                    for j in range(8):
                        T.op("pe", lambda e: e.matmul(ps[pg][:, :], lhsT=wgt[:, j, ft * 128:(ft + 1) * 128], rhs=xinT[:, j, :], start=(j == 0), stop=(j == 7)),
                             r=[wgk, "xinT"], w=[PK[pg]])
                    for j in range(8):
                        T.op("pe", lambda e: e.matmul(ps[pu][:, :], lhsT=wut[:, j, ft * 128:(ft + 1) * 128], rhs=xinT[:, j, :], start=(j == 0), stop=(j == 7)),
                             r=[wuk, "xinT"], w=[PK[pu]])
                    T.op("act", lambda e: e.activation(out=sg[:], in_=ps[pg][:, :], func=AF.Silu), r=[PK[pg]], w=["sgF"])
                    T.op("dve", lambda e: e.tensor_tensor(out=hT[:, ft, :], in0=ps[pu][:, :], in1=sg[:], op=ALU.mult), r=[PK[pu], "sgF"], w=["hTF"])
                for sc in range(4):
                    eb = eo[sc % 2]
                    for half in range(2):
                        po = 4 + half
                        for ft in range(8):
                            T.op("pe", lambda e: e.matmul(ps[po][:, :], lhsT=hT[:, ft, sc * 128:(sc + 1) * 128], rhs=wdt[:, ft, half * 512:(half + 1) * 512],
                                                          start=(ft == 0), stop=(ft == 7)), r=["hTF", wdk], w=[PK[po]])
                        T.op("dve", lambda e: e.tensor_scalar(out=eb[:, half * 512:(half + 1) * 512], in0=ps[po][:, :], scalar1=g_all[:, ex, sc:sc + 1],
                                                              scalar2=None, op0=ALU.mult), r=[PK[po], "g_all"], w=["eoF%d" % (sc % 2)])
                    T.dma("pool", lambda e: e.indirect_dma_start(out=out_d[:, :], out_offset=bass.IndirectOffsetOnAxis(ap=idx_all[:, ex, sc:sc + 1], axis=0),
                                                                 in_=eb[:], in_offset=None, compute_op=ALU.add),
                          r=["eoF%d" % (sc % 2), "idx_all"], w=["out_d"])
            T.barrier()
    for k, v in dbg_d.items():
        pass
    T.barrier()
    top.close()
    return nc


_CONST = None


def make_inputs(inp, b):
    global _CONST
    if _CONST is None:
        _CONST = host_consts()
    f = lambda a: np.ascontiguousarray(np.asarray(a, dtype=np.float32))
    m = dict(_CONST)
    m["x"] = f(inp["x"][b])
    m["g1bc"] = f(np.broadcast_to(inp["norm1_g"][0][None, :], (128, D)))
    m["w_in"] = f(inp["w_in"][0])
    scw = np.concatenate([inp["short_conv_w"][0], inp["short_conv_b"][0][None, :]], axis=0)
    m["scw"] = f(scw.reshape(4, 12, 128).transpose(2, 1, 0))
    qkg = np.stack([inp["q_norm_g"][0], inp["k_norm_g"][0]], axis=0)
    m["qkg"] = f(np.broadcast_to(qkg[None], (128, 2, 64)))
    m["lamv"] = f(np.stack([inp["lambda_q1"][0], inp["lambda_k1"][0], inp["lambda_q2"][0], inp["lambda_k2"][0]])[None])
    m["subg"] = f(np.broadcast_to(inp["subln_g"][0][None, :], (128, 128)))
    m["fw1"] = f(inp["filt_w1"][0])
    m["fw2"] = f(inp["filt_w2"][0])
    m["fw3"] = f(inp["filt_w3"][0])
    m["fbf"] = f(np.stack([inp["filt_b1"][0], inp["filt_b2"][0], inp["filt_b3"][0], inp["filt_freq"][0]], axis=1))
    m["fwo"] = f(inp["filt_w_out"][0])
    m["skipbc"] = f(np.broadcast_to(inp["hyena_skip"][0][None], (32, 2, 512)))
    m["wpa"] = f(inp["w_branch_attn"][0])
    m["wph"] = f(inp["w_branch_hyena"][0])
    m["wo"] = f(inp["w_out"][0])
    m["g2bc"] = f(np.broadcast_to(inp["norm2_g"][0][None, :], (128, D)))
    m["wr"] = f(inp["w_router"][0].reshape(8, 128, 16).transpose(1, 0, 2))
    m["wg"] = f(inp["w_gate"][0])
    m["wu"] = f(inp["w_up"][0])
    m["wd"] = f(inp["w_down"][0])
    return m


def kernel(**inputs):
    nc = build()
    maps = [make_inputs(inputs, c % 4) for c in range(4)]
    in_maps = [maps[c % 4] for c in range(NCORES)]
    res = run_bass_kernel_spmd(nc, in_maps, core_ids=list(range(NCORES)))
    out = np.stack([np.asarray(res.results[b]["out"], dtype=np.float32) for b in range(4)], axis=0)
    return out
```

```python
import math
from contextlib import ExitStack
import numpy as np
import ml_dtypes
import concourse.bass as bass
import concourse.mybir as mybir
from concourse.bass_utils import run_bass_kernel_spmd

F32 = mybir.dt.float32
BF16 = mybir.dt.bfloat16
I32 = mybir.dt.int32
AF = mybir.ActivationFunctionType
ALU = mybir.AluOpType
AX = mybir.AxisListType

L = 4096
D = 1024
NT = 32
NCORES = 8
CAP = 512
NE = 16
GC = 32
NK = 33
DEBUG = False


class Trk:
    def __init__(s, nc):
        s.nc = nc
        s.eng = dict(pe=nc.tensor, act=nc.scalar, dve=nc.vector, pool=nc.gpsimd, sp=nc.sync)
        s.sem = {}
        s.cnt = {}
        for k in ("pe", "act", "dve", "pool"):
            s.sem[k] = nc.alloc_semaphore(name="s_" + k)
            s.cnt[k] = 0
        s.ND = 16
        for i in range(s.ND):
            k = "d%d" % i
            s.sem[k] = nc.alloc_semaphore(name="s_" + k)
            s.cnt[k] = 0
        s.pool_of = {"sp": list(range(0, 8)), "pool": list(range(8, 16))}
        s.rrq = {"sp": 0, "pool": 0}
        s.seen = {e: {} for e in s.eng}
        s.lw = {}
        s.rd = {}

    def _wait(s, e, deps):
        for k, v in deps.items():
            if e == "pe" and k == "pe":
                continue
            if s.seen[e].get(k, 0) < v:
                s.eng[e].wait_ge(s.sem[k], v)
                s.seen[e][k] = v

    def _deps(s, r, w):
        d = {}

        def add(k, v):
            if d.get(k, 0) < v:
                d[k] = v
        for x in r:
            for k, v in s.lw.get(x, {}).items():
                add(k, v)
        for x in w:
            for k, v in s.lw.get(x, {}).items():
                add(k, v)
            for k, v in s.rd.get(x, {}).items():
                add(k, v)
        return d

    def _upd(s, toks, r, w):
        if isinstance(toks, tuple):
            toks = [toks]
        for x in r:
            m = s.rd.setdefault(x, {})
            for tok in toks:
                if m.get(tok[0], 0) < tok[1]:
                    m[tok[0]] = tok[1]
        for x in w:
            m = {}
            for tok in toks:
                if m.get(tok[0], 0) < tok[1]:
                    m[tok[0]] = tok[1]
            s.lw[x] = m
            s.rd[x] = {}

    def op(s, e, fn, r=(), w=()):
        s._wait(e, s._deps(r, w))
        ins = fn(s.eng[e])
        s.cnt[e] += 1
        ins.then_inc(s.sem[e], 1)
        s._upd((e, s.cnt[e]), r, w)

    def _next_sem(s, q):
        pool = s.pool_of[q]
        k = "d%d" % pool[s.rrq[q] % len(pool)]
        s.rrq[q] += 1
        if s.cnt[k] > 0:
            s._wait(q, {k: s.cnt[k]})
        return k

    def dma(s, q, fn, r=(), w=()):
        s._wait(q, s._deps(r, w))
        k = s._next_sem(q)
        ins = fn(s.eng[q])
        s.cnt[k] += 16
        ins.then_inc(s.sem[k], 16)
        s._upd((k, s.cnt[k]), r, w)

    def dmas(s, q, fns, r=(), w=()):
        s._wait(q, s._deps(r, w))
        toks = []
        for fn in fns:
            k = s._next_sem(q)
            ins = fn(s.eng[q])
            s.cnt[k] += 16
            ins.then_inc(s.sem[k], 16)
            toks.append((k, s.cnt[k]))
        s._upd(toks, r, w)

    def barrier(s, engines=None):
        tot = {k: v for k, v in s.cnt.items() if v > 0}
        for e in (engines or s.eng):
            s._wait(e, dict(tot))
        s.lw = {}
        s.rd = {}


def _bf(a):
    return np.ascontiguousarray(a.astype(np.float32)).astype(ml_dtypes.bfloat16)


def host_consts():
    c = {}
    t = np.arange(L, dtype=np.float32)
    inv_freq = (np.float32(500000.0) ** (-np.arange(0, 16, 2, dtype=np.float32) / np.float32(16))).astype(np.float32)
    ang = (t[:, None] * inv_freq[None, :]).astype(np.float32)
    cs = np.concatenate([np.cos(ang), np.sin(ang)], axis=-1).astype(np.float32)
    c["ropecs"] = np.ascontiguousarray(cs.reshape(NT, 128, 16).transpose(1, 0, 2))
    tt = t / np.float32(L - 1)
    bands = np.linspace(1e-4, 15, 16, dtype=np.float32)
    a2 = (np.float32(2.0 * math.pi / L) * t[:, None] * bands[None, :]).astype(np.float32)
    emb = np.concatenate([tt[:, None], np.cos(a2), -np.sin(a2)], axis=-1).astype(np.float32)
    c["embT"] = np.ascontiguousarray(emb.T)
    c["posrow"] = np.ascontiguousarray(np.broadcast_to(tt[None, :], (128, L))).astype(np.float32)
    min_decay = math.log(1e-2) / 1.5
    max_decay = math.log(1e-2) / 0.3
    deltas = np.abs(np.linspace(min_decay, max_decay, 512, dtype=np.float32))
    c["ndelta"] = np.ascontiguousarray((-deltas).reshape(4, 128).T).astype(np.float32)
    n1 = np.arange(32)[:, None]
    k1 = np.arange(NK)[None, :]
    th = 2 * np.pi * n1 * k1 / 64.0
    c["FA"] = _bf(np.concatenate([np.cos(th), -np.sin(th)], axis=1))
    n2 = np.arange(128)[:, None, None]
    k1 = np.arange(NK)[None, :, None]
    k2 = np.arange(128)[None, None, :]
    th = 2 * np.pi * ((n2 * (k1 + 64 * k2)) % 8192) / 8192.0
    c["TGr"] = _bf(np.cos(th))
    c["TGi"] = _bf(-np.sin(th))
    c["TGn"] = _bf(np.sin(th))
    kk2 = np.arange(128)[:, None]
    nn2 = np.arange(128)[None, :]
    th = 2 * np.pi * ((kk2 * nn2) % 128) / 128.0
    fr, fi = np.cos(th), np.sin(th)
    c["R1"] = _bf(np.concatenate([fr, fi], axis=1))
    c["R2"] = _bf(np.concatenate([-fi, fr], axis=1))
    k1 = np.arange(NK)[:, None, None]
    n2 = np.arange(128)[None, :, None]
    n1 = np.arange(32)[None, None, :]
    th = 2 * np.pi * ((k1 * (n2 + 128 * n1)) % 8192) / 8192.0
    coef = np.where((k1 == 0) | (k1 == 32), 1.0, 2.0)[:, :, None, :]
    th_ = np.stack([np.cos(th), -np.sin(th)], axis=2) * coef / 8192.0
    c["TH"] = _bf(th_)
    tid = np.arange(L).reshape(NT, 128).T
    c["tidhl"] = _bf(np.stack([tid // 64, tid % 64], axis=-1))
    c["iota512"] = np.ascontiguousarray(np.broadcast_to(np.arange(512, dtype=np.float32)[None, :], (128, 512)))
    tri = (np.arange(128)[:, None] <= np.arange(128)[None, :]).astype(np.float32)
    c["tri"] = tri
    return c


def build(dbg=None):
    nc = bass.Bass("TRN2", target_bir_lowering=False)
    dbg = dbg or {}

    def din(name, shape, dt=F32):
        return nc.dram_tensor(name, list(shape), dt, kind="ExternalInput").ap()

    def dscr(name, shape, dt=F32):
        kind = "ExternalOutput" if name in dbg.get("_ext", ()) else "Internal"
        return nc.dram_tensor(name, list(shape), dt, kind=kind).ap()

    x_d = din("x", [L, D])
    g1bc_d = din("g1bc", [128, D])
    w_in_d = din("w_in", [D, 5120])
    scw_d = din("scw", [128, 12, 4])
    qkg_d = din("qkg", [128, 2, 64])
    lam_d = din("lamv", [1, 4, 64])
    subg_d = din("subg", [128, 128])
    fw1_d = din("fw1", [33, 64])
    fw2_d = din("fw2", [64, 64])
    fw3_d = din("fw3", [64, 64])
    fbf_d = din("fbf", [64, 4])
    fwo_d = din("fwo", [64, 2048])
    skip_d = din("skipbc", [32, 2, 512])
    wpa_d = din("wpa", [512, D])
    wph_d = din("wph", [512, D])
    wo_d = din("wo", [D, D])
    g2bc_d = din("g2bc", [128, D])
    wr_d = din("wr", [128, 8, 16])
    wg_d = din("wg", [NE, D, D])
    wu_d = din("wu", [NE, D, D])
    wd_d = din("wd", [NE, D, D])
    ropecs_d = din("ropecs", [128, NT, 16])
    embT_d = din("embT", [33, L])
    posrow_d = din("posrow", [128, L])
    ndelta_d = din("ndelta", [128, 4])
    FA_d = din("FA", [32, 2 * NK], BF16)
    TGr_d = din("TGr", [128, NK, 128], BF16)
    TGi_d = din("TGi", [128, NK, 128], BF16)
    TGn_d = din("TGn", [128, NK, 128], BF16)
    R1_d = din("R1", [128, 256], BF16)
    R2_d = din("R2", [128, 256], BF16)
    TH_d = din("TH", [NK, 128, 2, 32], BF16)
    tidhl_d = din("tidhl", [128, NT, 2], BF16)
    iota_d = din("iota512", [128, 512])
    tri_d = din("tri", [128, 128])

    out_d = nc.dram_tensor("out", [L, D], F32, kind="ExternalOutput").ap()

    qT_d = dscr("qT_s", [4, 128, L], BF16)
    kT_d = dscr("kT_s", [4, 128, L], BF16)
    v_d = dscr("v_s", [L, 512], BF16)
    hyc_d = dscr("hyc_s", [1536, L], F32)
    gateT_d = dscr("gateT_s", [2048, L], BF16)
    kern_d = dscr("kern_s", [2, 2, 512, L], BF16)
    attnT_d = dscr("attnT_s", [512, L], BF16)
    hyoT_d = dscr("hyoT_s", [512, L], BF16)
    u2_d = dscr("u2_s", [L, D], BF16)

    dbg_d = {}
    for k, (shape, dt) in ((k, v) for k, v in dbg.items() if not k.startswith("_")):
        dbg_d[k] = nc.dram_tensor("dbg_" + k, list(shape), dt, kind="ExternalOutput").ap()

    T = Trk(nc)
    top = ExitStack()

    sbc = [0]

    def sb(es, name, shape, dt=F32):
        sbc[0] += 1
        return es.enter_context(nc.sbuf_tensor("sb%d_%s" % (sbc[0], name), list(shape), dt))

    ps = [top.enter_context(nc.psum_tensor("ps%d" % i, [128, 512], F32)) for i in range(8)]
    PK = ["ps%d" % i for i in range(8)]

    ident_f = sb(top, "ident_f", [128, 128])
    ident_b = sb(top, "ident_b", [128, 128], BF16)
    eps = sb(top, "eps", [128, 1])
    T.op("pool", lambda e: e.memset(ident_f[:], 0.0), w=["ident_f"])
    T.op("pool", lambda e: e.affine_select(out=ident_f[:], in_=ident_f[:], pattern=[[-1, 128]],
                                           compare_op=ALU.not_equal, fill=1.0, base=0, channel_multiplier=1),
         r=["ident_f"], w=["ident_f"])
    T.op("pool", lambda e: e.tensor_copy(out=ident_b[:], in_=ident_f[:]), r=["ident_f"], w=["ident_b"])
    T.op("pool", lambda e: e.memset(eps[:], 1e-6), w=["eps"])

    stages = dbg.get("_stages", "AKBCDEF") if isinstance(dbg.get("_stages", None), str) else "AKBCDEF"

    if "A" in stages:
      with ExitStack() as es:
        uT = sb(es, "uT", [128, 8, L + 2], BF16)
        g1bc = sb(es, "g1bc", [128, D])
        scw = sb(es, "scw", [128, 12, 4])
        qkg = sb(es, "qkg", [128, 2, 64])
        ropecs = sb(es, "ropecs", [128, NT, 16])
        xt = [sb(es, "xt%d" % i, [128, D]) for i in range(4)]
        xb = [sb(es, "xb%d" % i, [128, D], BF16) for i in range(4)]
        junk = sb(es, "junkA", [128, D])
        ss = sb(es, "ssA", [128, NT])
        rs = sb(es, "rsA", [128, NT])
        wb = [sb(es, "wb%d" % i, [128, 8, 512], BF16) for i in range(2)]
        T.dma("sp", lambda e: e.dma_start(out=g1bc[:], in_=g1bc_d[:, :]), w=["g1bc"])
        T.dma("sp", lambda e: e.dma_start(out=scw[:], in_=scw_d[:, :, :]), w=["scw"])
        T.dma("sp", lambda e: e.dma_start(out=qkg[:], in_=qkg_d[:, :, :]), w=["qkg"])
        T.dma("sp", lambda e: e.dma_start(out=ropecs[:], in_=ropecs_d[:, :, :]), w=["ropecs"])
        T.op("dve", lambda e: e.memset(ss[:], 0.0), w=["ssA"])
        T.op("dve", lambda e: e.memset(uT[:, :, 0:1], 0.0), w=["uTpad0"])
        T.op("dve", lambda e: e.memset(uT[:, :, L + 1:L + 2], 0.0), w=["uTpad1"])
        T.op("dve", lambda e: e.tensor_scalar(out=qkg[:, 0, :], in0=qkg[:, 0, :], scalar1=0.125, scalar2=None,
                                              op0=ALU.mult), r=["qkg"], w=["qkg"])
        psb = [ps[6][:, :].bitcast(BF16), ps[7][:, :].bitcast(BF16)]
        for i in range(NT):
            b = i % 4
            pb2 = i % 2
            T.dma("sp", lambda e: e.dma_start(out=xt[b][:], in_=x_d[i * 128:(i + 1) * 128, :]), w=["xt%d" % b])
            T.op("act", lambda e: e.activation(out=junk[:], in_=xt[b][:], func=AF.Square,
                                               accum_out=ss[:, i:i + 1]), r=["xt%d" % b, "ssA"], w=["junkA", "ss%d" % i])
            T.op("act", lambda e: e.activation(out=rs[:, i:i + 1], in_=ss[:, i:i + 1], func=AF.Sqrt,
                                               scale=1.0 / D, bias=eps[:]), r=["ss%d" % i, "eps"], w=["rs%d" % i])
            T.op("dve", lambda e: e.reciprocal(out=rs[:, i:i + 1], in_=rs[:, i:i + 1]), r=["rs%d" % i], w=["rs%d" % i])
            T.op("dve", lambda e: e.scalar_tensor_tensor(out=xb[b][:], in0=xt[b][:], scalar=rs[:, i:i + 1], in1=g1bc[:],
                                                         op0=ALU.mult, op1=ALU.mult),
                 r=["xt%d" % b, "rs%d" % i, "g1bc"], w=["xb%d" % b])
            for j in range(8):
                T.op("pe", lambda e: e.transpose(out=psb[pb2][:, j * 128:(j + 1) * 128],
                                                 in_=xb[b][:, j * 128:(j + 1) * 128], identity=ident_b[:]),
                     r=["xb%d" % b, "ident_b"], w=[PK[6 + pb2]])
            T.op("act", lambda e: e.copy(out=uT[:, :, 1 + i * 128:1 + (i + 1) * 128],
                                         in_=psb[pb2].rearrange("p (j t) -> p j t", j=8)),
                 r=[PK[6 + pb2]], w=["uT%d" % i])
        UTK = ["uT%d" % i for i in range(NT)] + ["uTpad0", "uTpad1"]

        sq = sb(es, "sqA", [128, 512])
        ssq = sb(es, "ssqA", [128, 8])
        qn = sb(es, "qnA", [128, 8, 64])
        qb2 = [sb(es, "qbA%d" % i, [128, 512], BF16) for i in range(2)]
        rt = [sb(es, "rtA%d" % i, [128, 8, 8]) for i in range(4)]
        qTs = [sb(es, "qTs%d" % i, [128, 4, 128], BF16) for i in range(2)]
        vst = [sb(es, "vst%d" % i, [128, 512], BF16) for i in range(2)]
        hraw = sb(es, "hraw", [128, L + 2])
        hc = sb(es, "hcA", [128, L])
        gst = sb(es, "gstA", [128, L], BF16)
        T.op("pool", lambda e: e.memset(hraw[:, 0:1], 0.0), w=["hrawp0"])
        T.op("pool", lambda e: e.memset(hraw[:, L + 1:L + 2], 0.0), w=["hrawp1"])
        pcnt = [0]

        def nextps():
            pcnt[0] += 1
            return pcnt[0] % 4

        for cc in dbg.get('_ccs', range(10)):
            wbk = "wb%d" % (cc % 2)
            wbt = wb[cc % 2]
            T.dmas("pool", [(lambda e, a=a: e.dma_start(
                out=wbt[:, a:a + 4, :], in_=w_in_d.rearrange("(j p) c -> p j c", p=128)[:, a:a + 4, cc * 512:(cc + 1) * 512]))
                for a in (0, 4)], w=[wbk])
            if cc < 3:
                def qk_tail(i):
                    qbt = qb2[i % 2]
                    pb = 6 + (i % 2)
                    for h in range(4):
                        T.op("pe", lambda e: e.transpose(out=psb[i % 2][:, h * 128:(h + 1) * 128],
                                                         in_=qbt[:, h * 128:(h + 1) * 128], identity=ident_b[:]),
                             r=["qbA%d" % (i % 2), "ident_b"], w=[PK[pb]])
                    st = qTs[i % 2]
                    T.op("act", lambda e: e.copy(out=st[:], in_=psb[i % 2][:, 0:512].rearrange("p (h t) -> p h t", h=4)),
                         r=[PK[pb]], w=["qTs%d" % (i % 2)])
                    dst = (qT_d if cc == 0 else kT_d).rearrange("h p t -> p h t")[:, :, i * 128:(i + 1) * 128]
                    T.dma("sp", lambda e: e.dma_start(out=dst, in_=st[:]), r=["qTs%d" % (i % 2)], w=["qkT_d"])

                for i in range(NT):
                    pi = nextps()
                    for j in range(8):
                        T.op("pe", lambda e: e.matmul(ps[pi][:, :], lhsT=uT[:, j, 1 + i * 128:1 + (i + 1) * 128],
                                                      rhs=wbt[:, j, :], start=(j == 0), stop=(j == 7)),
                             r=["uT%d" % i, wbk], w=[PK[pi]])
                    if cc == 2:
                        vs = vst[i % 2]
                        T.op("act", lambda e: e.copy(out=vs[:], in_=ps[pi][:, :]), r=[PK[pi]], w=["vst%d" % (i % 2)])
                        T.dma("sp", lambda e: e.dma_start(out=v_d[i * 128:(i + 1) * 128, :], in_=vs[:]),
                              r=["vst%d" % (i % 2)], w=["v_d"])
                        continue
                    if i > 0:
                        qk_tail(i - 1)
                    qb = qb2[i % 2]
                    qbk = "qbA%d" % (i % 2)
                    T.op("act", lambda e: e.activation(out=sq[:], in_=ps[pi][:, :], func=AF.Square), r=[PK[pi]], w=["sqA"])
                    T.op("dve", lambda e: e.tensor_reduce(out=ssq[:], in_=sq[:].rearrange("p (g d) -> p g d", d=64),
                                                          axis=AX.X, op=ALU.add), r=["sqA"], w=["ssqA"])
                    T.op("act", lambda e: e.activation(out=ssq[:], in_=ssq[:], func=AF.Sqrt, scale=1.0 / 64,
                                                       bias=eps[:]), r=["ssqA", "eps"], w=["ssqA"])
                    T.op("dve", lambda e: e.reciprocal(out=ssq[:], in_=ssq[:]), r=["ssqA"], w=["ssqA"])
                    T.op("dve", lambda e: e.tensor_tensor(out=qn[:], in0=ps[pi][:, :].rearrange("p (g d) -> p g d", d=64),
                                                          in1=ssq[:, :].unsqueeze(2).broadcast_to([128, 8, 64]),
                                                          op=ALU.mult), r=[PK[pi], "ssqA"], w=["qnA"])
                    T.op("dve", lambda e: e.tensor_tensor(out=qn[:], in0=qn[:],
                                                          in1=qkg[:, cc, :].unsqueeze(1).broadcast_to([128, 8, 64]),
                                                          op=ALU.mult), r=["qnA", "qkg"], w=["qnA"])
                    T.op("act", lambda e: e.copy(out=qb[:].rearrange("p (g d) -> p g d", d=64), in_=qn[:]),
                         r=["qnA"], w=[qbk])
                    cosb = ropecs[:, i, 0:8].unsqueeze(1).broadcast_to([128, 8, 8])
                    sinb = ropecs[:, i, 8:16].unsqueeze(1).broadcast_to([128, 8, 8])
                    r1 = qn[:, :, 0:8]
                    r2 = qn[:, :, 8:16]
                    T.op("dve", lambda e: e.tensor_tensor(out=rt[0][:], in0=r1, in1=cosb, op=ALU.mult), r=["qnA", "ropecs"], w=["rt0"])
                    T.op("dve", lambda e: e.tensor_tensor(out=rt[1][:], in0=r2, in1=sinb, op=ALU.mult), r=["qnA", "ropecs"], w=["rt1"])
                    T.op("dve", lambda e: e.tensor_tensor(out=rt[2][:], in0=r2, in1=cosb, op=ALU.mult), r=["qnA", "ropecs"], w=["rt2"])
                    T.op("dve", lambda e: e.tensor_tensor(out=rt[3][:], in0=r1, in1=sinb, op=ALU.mult), r=["qnA", "ropecs"], w=["rt3"])
                    qbv = qb[:].rearrange("p (g d) -> p g d", d=64)
                    T.op("dve", lambda e: e.tensor_tensor(out=qbv[:, :, 0:8], in0=rt[0][:], in1=rt[1][:], op=ALU.subtract),
                         r=["rt0", "rt1"], w=[qbk])
                    T.op("dve", lambda e: e.tensor_tensor(out=qbv[:, :, 8:16], in0=rt[2][:], in1=rt[3][:], op=ALU.add),
                         r=["rt2", "rt3"], w=[qbk])
                if cc < 2:
                    qk_tail(NT - 1)
            else:
                for ct in range(4):
                    for tq in range(8):
                        pi = nextps()
                        for j in range(8):
                            T.op("pe", lambda e: e.matmul(ps[pi][:, :], lhsT=wbt[:, j, ct * 128:(ct + 1) * 128],
                                                          rhs=uT[:, j, 1 + tq * 512:1 + (tq + 1) * 512],
                                                          start=(j == 0), stop=(j == 7)),
                                 r=UTK[4 * tq:4 * tq + 4] + [wbk], w=[PK[pi]])
                        if cc < 6:
                            T.op("act", lambda e: e.copy(out=hraw[:, 1 + tq * 512:1 + (tq + 1) * 512], in_=ps[pi][:, :]),
                                 r=[PK[pi]], w=["hraw"])
                        else:
                            T.op("act", lambda e: e.activation(out=gst[:, tq * 512:(tq + 1) * 512], in_=ps[pi][:, :],
                                                               func=AF.Sigmoid), r=[PK[pi]], w=["gstA"])
                    if cc < 6:
                        cti = (cc - 3) * 4 + ct
                        lvl = dbg.get('_lvl', 9)
                        if lvl >= 1:
                          T.op("act", lambda e: e.activation(out=hc[:], in_=hraw[:, 1:L + 1], func=AF.Identity,
                                                           scale=scw[:, cti, 1:2], bias=scw[:, cti, 3:4]),
                             r=["hraw", "scw"], w=["hcA"])
                        if lvl >= 2:
                          T.op("dve", lambda e: e.scalar_tensor_tensor(out=hc[:], in0=hraw[:, 0:L], scalar=scw[:, cti, 0:1],
                                                                     in1=hc[:], op0=ALU.mult, op1=ALU.add),
                             r=["hraw", "hrawp0", "scw", "hcA"], w=["hcA"])
                          T.op("dve", lambda e: e.scalar_tensor_tensor(out=hc[:], in0=hraw[:, 2:L + 2], scalar=scw[:, cti, 2:3],
                                                                     in1=hc[:], op0=ALU.mult, op1=ALU.add),
                             r=["hraw", "hrawp1", "scw", "hcA"], w=["hcA"])
                        if lvl >= 3:
                          for q4 in range(4):
                              T.dma("sp", lambda e: e.dma_start(out=hyc_d[cti * 128:(cti + 1) * 128, q4 * 1024:(q4 + 1) * 1024],
                                                                in_=hc[:, q4 * 1024:(q4 + 1) * 1024]),
                                    r=["hcA"], w=["hyc_d%d" % q4])
                    else:
                        gti = (cc - 6) * 4 + ct
                        T.dmas("sp", [(lambda e, a=a: e.dma_start(out=gateT_d[gti * 128:(gti + 1) * 128, a:a + 2048], in_=gst[:, a:a + 2048]))
                                      for a in (0, 2048)], r=["gstA"], w=["gateT_d"])
        T.barrier()

    if "K" in stages:
      with ExitStack() as es:
        embT = sb(es, "embT", [33, L])
        fw1 = sb(es, "fw1", [33, 64])
        fw2 = sb(es, "fw2", [64, 64])
        fw3 = sb(es, "fw3", [64, 64])
        fbf = sb(es, "fbf", [64, 4])
        frb = sb(es, "frb", [64, 3])
        fwo = sb(es, "fwo", [64, 2048])
        posrow = sb(es, "posrow", [128, L])
        ndelta = sb(es, "ndelta", [128, 4])
        hid = [sb(es, "hid%d" % i, [64, L]) for i in range(2)]
        hidb = sb(es, "hidb", [64, L])
        dec = sb(es, "decK", [128, L])
        hpm = [[sb(es, "hpmK%d%d" % (o, pm), [128, L], BF16) for pm in range(2)] for o in range(2)]
        wpm = sb(es, "wpmK", [64, 2, 2, 512])
        for tns, src, k in ((fw1, fw1_d, "fw1"), (fw2, fw2_d, "fw2"), (fw3, fw3_d, "fw3"),
                            (fbf, fbf_d, "fbf"), (ndelta, ndelta_d, "ndelta")):
            T.dma("sp", lambda e: e.dma_start(out=tns[:], in_=src), w=[k])
        for tns, src, k, n in ((embT, embT_d, "embT", L), (fwo, fwo_d, "fwo", 2048), (posrow, posrow_d, "posrow", L)):
            T.dmas("sp", [(lambda e, a=a: e.dma_start(out=tns[:, a:a + 1024], in_=src[:, a:a + 1024]))
                          for a in range(0, n, 1024)], w=[k])
        for l in range(3):
            T.op("dve", lambda e: e.tensor_tensor(out=frb[:, l:l + 1], in0=fbf[:, l:l + 1], in1=fbf[:, 3:4], op=ALU.mult),
                 r=["fbf"], w=["frb"])
        TWO_PI = 2.0 * math.pi
        srcs = [(embT, "embT", 33, fw1, "fw1"), (hid[0], "hid0", 64, fw2, "fw2"), (hid[1], "hid1", 64, fw3, "fw3")]
        outs = [(hid[0], "hid0"), (hid[1], "hid1"), (hid[0], "hid0")]
        negpi = sb(es, "negpi", [64, 1])
        T.op("dve", lambda e: e.memset(negpi[:], 4.0 * math.pi), w=["negpi"])
        kcnt = sb(es, "kcntK", [64, L])
        for l in range(3):
            src, sk, kk, w_, wk = srcs[l]
            dst, dk = outs[l]
            for tq in range(8):
                pi = tq % 4
                T.op("pe", lambda e: e.matmul(ps[pi][0:64, :], lhsT=w_[0:kk, :], rhs=src[0:kk, tq * 512:(tq + 1) * 512],
                                              start=True, stop=True), r=[sk, wk], w=[PK[pi]])
                T.op("dve", lambda e: e.tensor_scalar(out=hidb[:, tq * 512:(tq + 1) * 512], in0=ps[pi][0:64, :],
                                                      scalar1=fbf[:, 3:4], scalar2=frb[:, l:l + 1], op0=ALU.mult,
                                                      op1=ALU.add), r=[PK[pi], "fbf", "frb"], w=["hidb"])
                xs_ = hidb[:, tq * 512:(tq + 1) * 512]
                ks_ = kcnt[:, tq * 512:(tq + 1) * 512]
                T.op("dve", lambda e: e.tensor_scalar(out=ks_, in0=xs_, scalar1=-3.0 * math.pi, scalar2=None, op0=ALU.is_gt),
                     r=["hidb"], w=["kcnt"])
                for thr in (-math.pi, math.pi, 3.0 * math.pi):
                    T.op("dve", lambda e: e.scalar_tensor_tensor(out=ks_, in0=xs_, scalar=thr, in1=ks_, op0=ALU.is_gt, op1=ALU.add),
                         r=["hidb", "kcnt"], w=["kcnt"])
                T.op("dve", lambda e: e.scalar_tensor_tensor(out=xs_, in0=ks_, scalar=-TWO_PI, in1=xs_, op0=ALU.mult, op1=ALU.add),
                     r=["hidb", "kcnt"], w=["hidb"])
                T.op("act", lambda e: e.activation(out=dst[:, tq * 512:(tq + 1) * 512], in_=xs_,
                                                   func=AF.Sin, bias=negpi[:]), r=["hidb", "negpi"], w=[dk])
        h3 = hid[0]
        for o in range(2):
            wf_ = fwo[:, (o * 2) * 512:(o * 2 + 1) * 512]
            wb_ = fwo[:, (o * 2 + 1) * 512:(o * 2 + 2) * 512]
            T.op("dve", lambda e: e.tensor_tensor(out=wpm[:, o, 0, :], in0=wf_, in1=wb_, op=ALU.add), r=["fwo"], w=["wpmK"])
            T.op("dve", lambda e: e.tensor_tensor(out=wpm[:, o, 1, :], in0=wf_, in1=wb_, op=ALU.subtract), r=["fwo"], w=["wpmK"])
        for ct in range(4):
            T.op("act", lambda e: e.activation(out=dec[:], in_=posrow[:], func=AF.Exp, scale=ndelta[:, ct:ct + 1]),
                 r=["posrow", "ndelta"], w=["decK"])
            for o in range(2):
                for pm in range(2):
                    dst = hpm[o][pm]
                    dk = "hpmK%d%d" % (o, pm)
                    for tq in range(8):
                        pi = tq % 4
                        T.op("pe", lambda e: e.matmul(ps[pi][:, :], lhsT=wpm[:, o, pm, ct * 128:(ct + 1) * 128],
                                                      rhs=h3[:, tq * 512:(tq + 1) * 512], start=True, stop=True),
                             r=["hid0", "wpmK"], w=[PK[pi]])
                        T.op("dve", lambda e: e.tensor_tensor(out=dst[:, tq * 512:(tq + 1) * 512], in0=ps[pi][:, :],
                                                              in1=dec[:, tq * 512:(tq + 1) * 512], op=ALU.mult),
                             r=[PK[pi], "decK"], w=[dk])
                colf = (o * 2) * 512 + ct * 128
                T.op("pe", lambda e: e.matmul(ps[4][:, 0:1], lhsT=fwo[:, colf:colf + 128], rhs=h3[:, 0:1], start=True, stop=True),
                     r=["hid0", "fwo"], w=[PK[4]])
                for pm in range(2):
                    T.op("dve", lambda e: e.tensor_tensor(out=hpm[o][pm][:, 0:1], in0=ps[4][:, 0:1], in1=dec[:, 0:1], op=ALU.mult),
                         r=[PK[4], "decK"], w=["hpmK%d%d" % (o, pm)])
                for pm in range(2):
                    T.dmas("sp", [(lambda e, a=a: e.dma_start(out=kern_d[o, pm, ct * 128:(ct + 1) * 128, a:a + 2048],
                                                              in_=hpm[o][pm][:, a:a + 2048]))
                                  for a in range(0, L, 2048)], r=["hpmK%d%d" % (o, pm)], w=["kern_d%d%d" % (o, pm)])
        T.barrier()

    if "B" in stages:
      with ExitStack() as es:
        qT = sb(es, "qT", [128, 4, L], BF16)
        kT = sb(es, "kT", [128, 2, 4, L], BF16)
        V = sb(es, "Vsb", [128, NT, 4, 132], BF16)
        lamv = sb(es, "lamv", [1, 4, 64])
        lamt = sb(es, "lamt", [1, 8])
        ones1 = sb(es, "ones1", [1, 128])
        nlam = sb(es, "nlam", [128, 1])
        subg = sb(es, "subg", [128, 128])
        E = [sb(es, "E%d" % i, [128, 512], BF16) for i in range(3)]
        osb = sb(es, "osbB", [128, 2, 4, 129])
        aacc = sb(es, "aacc", [128, 4, 128])
        ab = sb(es, "abB", [128, 4, 128], BF16)
        junkb = sb(es, "junkB", [128, 4, 128])
        sm = sb(es, "smB", [128, 8, 4])
        aTs = [sb(es, "aTs%d" % i, [128, 512], BF16) for i in range(2)]
        T.dmas("sp", [(lambda e, h=h, a=a: e.dma_start(out=qT[:, h, a:a + 2048], in_=qT_d[h, :, a:a + 2048]))
                      for h in range(4) for a in (0, 2048)], w=["qT"])
        T.op("pool", lambda e: e.memset(kT[64:128, 0, :, :], 0.0), w=["kTz0"])
        T.op("dve", lambda e: e.memset(kT[0:64, 1, :, :], 0.0), w=["kTz1"])
        T.dmas("sp", [(lambda e, h=h, a=a, m=m: e.dma_start(out=kT[m * 64:(m + 1) * 64, m, h, a:a + 2048],
                                                            in_=kT_d[h, m * 64:(m + 1) * 64, a:a + 2048]))
                      for m in range(2) for h in range(4) for a in (0, 2048)], w=["kT"])
        T.dmas("sp", [(lambda e, i=i: e.dma_start(out=V[:, i, :, 0:128],
                                                  in_=v_d[i * 128:(i + 1) * 128, :].rearrange("p (h d) -> p h d", d=128)))
                      for i in range(NT)], w=["V"])
        T.op("pool", lambda e: e.memset(V[:, :, :, 128:129], 1.0), w=["Vones"])
        T.dma("sp", lambda e: e.dma_start(out=lamv[:], in_=lam_d[:, :, :]), w=["lamv"])
        T.dma("sp", lambda e: e.dma_start(out=subg[:], in_=subg_d[:, :]), w=["subg"])
        T.op("dve", lambda e: e.tensor_scalar(out=subg[:], in0=subg[:], scalar1=0.8, scalar2=None, op0=ALU.mult),
             r=["subg"], w=["subg"])
        T.op("dve", lambda e: e.memset(ones1[:], 1.0), w=["ones1"])
        T.op("dve", lambda e: e.memset(lamt[:], 0.0), w=["lamt"])
        T.op("dve", lambda e: e.tensor_tensor(out=lamv[:, 0, :], in0=lamv[:, 0, :], in1=lamv[:, 1, :], op=ALU.mult), r=["lamv"], w=["lamv"])
        T.op("dve", lambda e: e.tensor_tensor(out=lamv[:, 2, :], in0=lamv[:, 2, :], in1=lamv[:, 3, :], op=ALU.mult), r=["lamv"], w=["lamv"])
        T.op("dve", lambda e: e.tensor_reduce(out=lamt[:, 0:1], in_=lamv[:, 0, :], axis=AX.X, op=ALU.add), r=["lamv", "lamt"], w=["lamt"])
        T.op("dve", lambda e: e.tensor_reduce(out=lamt[:, 1:2], in_=lamv[:, 2, :], axis=AX.X, op=ALU.add), r=["lamv", "lamt"], w=["lamt"])
        T.op("act", lambda e: e.activation(out=lamt[:, 2:4], in_=lamt[:, 0:2], func=AF.Exp), r=["lamt"], w=["lamt"])
        T.op("dve", lambda e: e.tensor_tensor(out=lamt[:, 4:5], in0=lamt[:, 3:4], in1=lamt[:, 2:3], op=ALU.subtract), r=["lamt"], w=["lamt"])
        T.op("dve", lambda e: e.tensor_scalar(out=lamt[:, 5:6], in0=lamt[:, 4:5], scalar1=-0.2, scalar2=None, op0=ALU.add), r=["lamt"], w=["lamt"])
        T.op("pe", lambda e: e.matmul(ps[7][:, 0:1], lhsT=ones1[:, :], rhs=lamt[:, 5:6], start=True, stop=True),
             r=["ones1", "lamt"], w=[PK[7]])
        T.op("dve", lambda e: e.tensor_copy(out=nlam[:], in_=ps[7][:, 0:1]), r=[PK[7]], w=["nlam"])
        psT = ps[6][:, :].bitcast(BF16)
        steps = [(h, qc, m, kt) for h in range(4) for qc in range(8) for m in range(2) for kt in range(NT)]
        USE_POW = False

        def issue_S(idx):
            h, qc, m, kt = steps[idx]
            sp_ = idx % 2
            eb = idx % 3
            T.op("pe", lambda e: e.matmul(ps[sp_][:, :], lhsT=kT[:, m, h, kt * 128:(kt + 1) * 128],
                                          rhs=qT[:, h, qc * 512:(qc + 1) * 512], start=True, stop=True),
                 r=["kT", "kTz0", "kTz1", "qT"], w=[PK[sp_]])
            T.op("act", lambda e: e.activation(out=E[eb][:], in_=ps[sp_][:, :], func=AF.Exp),
                 r=[PK[sp_]], w=["E%d" % eb])

        def issue_PV(idx):
            h, qc, m, kt = steps[idx]
            eb = idx % 3
            for qs in range(4):
                bank = 2 + m * 2 + qs // 2
                T.op("pe", lambda e: e.matmul(ps[bank][:, (qs % 2) * 256:(qs % 2) * 256 + 129],
                                              lhsT=E[eb][:, qs * 128:(qs + 1) * 128], rhs=V[:, kt, h, 0:129],
                                              start=(kt == 0 and qs % 2 == 0), stop=(kt == NT - 1),
                                              skip_group_check=True),
                     r=["E%d" % eb, "V", "Vones"], w=[PK[bank]])

        def bc4(ap2):
            return ap2.unsqueeze(2).broadcast_to([128, 4, 128])

        def epi_head():
            for m in range(2):
                for half in range(2):
                    bank = 2 + m * 2 + half
                    src = ps[bank][:, :].rearrange("p (a c) -> p a c", a=2)[:, :, 0:129]
                    dst = osb[:, m, half * 2:(half + 1) * 2, :]
                    if half == 0:
                        T.op("dve", lambda e: e.tensor_copy(out=dst, in_=src), r=[PK[bank]], w=["osb%d%d" % (m, half)])
                    else:
                        T.op("pool" if False else "dve", lambda e: e.tensor_copy(out=dst, in_=src), r=[PK[bank]], w=["osb%d%d" % (m, half)])
            OS = ["osb00", "osb01", "osb10", "osb11"]
            T.op("dve", lambda e: e.reciprocal(out=sm[:, 0:2, :], in_=osb[:, :, :, 128]), r=OS, w=["smB"])
            T.op("dve", lambda e: e.tensor_scalar(out=sm[:, 2, :], in0=sm[:, 1, :], scalar1=nlam[:, 0:1], scalar2=None, op0=ALU.mult),
                 r=["smB", "nlam"], w=["smB"])
            T.op("dve", lambda e: e.tensor_tensor(out=aacc[:], in0=osb[:, 0, :, 0:128], in1=bc4(sm[:, 0, :]), op=ALU.mult),
                 r=OS + ["smB"], w=["aacc"])
            T.op("dve", lambda e: e.tensor_tensor(out=junkb[:], in0=osb[:, 1, :, 0:128], in1=bc4(sm[:, 2, :]), op=ALU.mult),
                 r=OS + ["smB"], w=["junkB"])
            T.op("dve", lambda e: e.tensor_tensor(out=aacc[:], in0=aacc[:], in1=junkb[:], op=ALU.add), r=["aacc", "junkB"], w=["aacc"])
            T.op("dve", lambda e: e.tensor_tensor(out=junkb[:], in0=aacc[:], in1=aacc[:], op=ALU.mult), r=["aacc"], w=["junkB"])
            T.op("dve", lambda e: e.tensor_reduce(out=sm[:, 3, :], in_=junkb[:], axis=AX.X, op=ALU.add), r=["junkB", "smB"], w=["smB"])
            T.op("dve", lambda e: e.tensor_scalar(out=sm[:, 4, :], in0=sm[:, 3, :], scalar1=1.0 / 128, scalar2=1e-6, op0=ALU.mult, op1=ALU.add),
                 r=["smB"], w=["smB"])
            if USE_POW:
                T.op("dve", lambda e: e.tensor_scalar(out=sm[:, 5, :], in0=sm[:, 4, :], scalar1=-0.5, scalar2=None, op0=ALU.pow),
                     r=["smB"], w=["smB"])
            else:
                T.op("act", lambda e: e.activation(out=sm[:, 6, :], in_=sm[:, 4, :], func=AF.Sqrt), r=["smB"], w=["smB"])
                T.op("dve", lambda e: e.reciprocal(out=sm[:, 5, :], in_=sm[:, 6, :]), r=["smB"], w=["smB"])
            T.op("dve", lambda e: e.tensor_tensor(out=aacc[:], in0=aacc[:], in1=bc4(sm[:, 5, :]), op=ALU.mult), r=["aacc", "smB"], w=["aacc"])
            T.op("dve", lambda e: e.tensor_tensor(out=ab[:], in0=aacc[:], in1=subg[:, :].unsqueeze(1).broadcast_to([128, 4, 128]), op=ALU.mult),
                 r=["aacc", "subg"], w=["abB"])

        def epi_tail(h, qc):
            for qs in range(4):
                T.op("pe", lambda e: e.transpose(out=psT[:, qs * 128:(qs + 1) * 128], in_=ab[:, qs, :], identity=ident_b[:]),
                     r=["abB", "ident_b"], w=[PK[6]])
            st = aTs[qc % 2]
            T.op("act", lambda e: e.copy(out=st[:], in_=psT[:, 0:512]), r=[PK[6]], w=["aTs%d" % (qc % 2)])
            T.dma("sp", lambda e: e.dma_start(out=attnT_d[h * 128:(h + 1) * 128, qc * 512:(qc + 1) * 512], in_=st[:]),
                  r=["aTs%d" % (qc % 2)], w=["attnT_d"])

        pending = None
        issue_S(0)
        for idx in range(len(steps)):
            h, qc, m, kt = steps[idx]
            if idx + 1 < len(steps):
                issue_S(idx + 1)
            issue_PV(idx)
            if pending is not None and m == 0 and kt == 12:
                epi_tail(*pending)
                pending = None
            if m == 1 and kt == NT - 1:
                epi_head()
                pending = (h, qc)
        epi_tail(*pending)
        T.barrier()

    if "C" in stages:
      with ExitStack() as es:
        FA = sb(es, "FA", [32, 2 * NK], BF16)
        TGr = sb(es, "TGr", [128, NK, 128], BF16)
        TGi = sb(es, "TGi", [128, NK, 128], BF16)
        TGn = sb(es, "TGn", [128, NK, 128], BF16)
        KCS = [(0, 16), (16, 32), (32, NK)]
        R1 = sb(es, "R1", [128, 256], BF16)
        R2 = sb(es, "R2", [128, 256], BF16)
        TH = sb(es, "TH", [NK, 128, 2, 32], BF16)
        skipbc = sb(es, "skipbc", [32, 2, GC])
        for tns, src, k in ((FA, FA_d, "FA"), (R1, R1_d, "R1"), (R2, R2_d, "R2")):
            T.dma("sp", lambda e: e.dma_start(out=tns[:], in_=src), w=[k])
        for tns, src, k in ((TGr, TGr_d, "TGr"), (TGi, TGi_d, "TGi"), (TGn, TGn_d, "TGn")):
            T.dmas("sp", [(lambda e, a=a, b=b: e.dma_start(out=tns[:, a:b, :], in_=src[:, a:b, :])) for (a, b) in KCS], w=[k])
        T.dmas("sp", [(lambda e, a=a: e.dma_start(out=TH[:, a:a + 32, :, :], in_=TH_d[:, a:a + 32, :, :])) for a in range(0, 128, 32)], w=["TH"])
        zf = sb(es, "zf", [32, GC, 128])
        gt = sb(es, "gtC", [32, GC, 128])
        zs = sb(es, "zsC", [32, GC, 128])
        srcb = [sb(es, "srcb%d" % i, [32, GC, 128], BF16) for i in range(3)]
        Bm_ = [sb(es, "Bm%d" % i, [128, 2 * NK, GC], BF16) for i in range(3)]
        srs = sb(es, "srsC", [128, 512])
        sis = sb(es, "sisC", [128, 512])
        tm = [sb(es, "tmC%d" % i, [128, 512]) for i in range(4)]
        Y = sb(es, "YC", [128, GC, 2, NK], BF16)
        Dm = sb(es, "DmC", [NK, 256, GC], BF16)
        tmpy = sb(es, "tmpyC", [32, GC, 16])
        hob = srcb[0]

        def flay(ap2d):
            return ap2d.rearrange("c (a b) -> a c b", b=128)

        def load_filters(g_, o_):
            for pm in range(2):
                T.dma("sp", lambda e: e.dma_start(out=srcb[1 + pm][:], in_=flay(kern_d[o_, pm, g_ * GC:(g_ + 1) * GC, :])),
                      w=["srcb%d" % (1 + pm)])

        for g in range(512 // GC):
            c0 = g * GC
            T.dma("sp", lambda e: e.dma_start(out=zf[:], in_=flay(hyc_d[c0:c0 + GC, :])), w=["zf"])
            T.dma("sp", lambda e: e.dma_start(out=skipbc[:], in_=skip_d[:, :, c0:c0 + GC]), w=["skipbc"])
            for o in range(2):
                T.dma("sp", lambda e: e.dma_start(out=gt[:], in_=flay(hyc_d[512 * (o + 1) + c0:512 * (o + 1) + c0 + GC, :])),
                      w=["gtC"])
                if g == 0 and o == 0:
                    load_filters(0, 0)
                for s_ in (1, 2, 0):
                    if s_ == 0:
                        T.op("act", lambda e: e.copy(out=srcb[0][:], in_=zf[:]), r=["zf"], w=["srcb0"])
                        T.op("pool", lambda e: e.tensor_tensor(out=zs[:], in0=zf[:],
                                                               in1=skipbc[:, o, :].unsqueeze(2).broadcast_to([32, GC, 128]),
                                                               op=ALU.mult), r=["zf", "skipbc"], w=["zsC"])
                    for cq, (ca, cb) in enumerate(((0, 7), (7, 14), (14, 21), (21, 28), (28, 32))):
                        pi = cq % 2
                        ncg = cb - ca
                        for c4 in range(ncg):
                            c = ca + c4
                            T.op("pe", lambda e: e.matmul(ps[pi][:, c4 * 2 * NK:(c4 + 1) * 2 * NK], lhsT=srcb[s_][:, c, :],
                                                          rhs=FA[:, :], start=True, stop=True),
                                 r=["srcb%d" % s_, "FA"], w=[PK[pi]])
                        T.op("act" if cq % 2 == 0 else "dve",
                             lambda e: (e.copy if cq % 2 == 0 else e.tensor_copy)(
                                 out=Bm_[s_][:, :, ca:cb],
                                 in_=ps[pi][:, 0:ncg * 2 * NK].rearrange("p (c f) -> p f c", c=ncg)),
                             r=[PK[pi]], w=["Bm%d" % s_])
                nxt = (g, 1) if o == 0 else (g + 1, 0)
                if nxt[0] < 512 // GC:
                    load_filters(*nxt)
                for kc, (ka, kb_) in enumerate(KCS):
                    bZr, bZi, bSr, bSi = (2, 3, 4, 5) if kc % 2 == 0 else (0, 1, 6, 7)
                    nk = kb_ - ka
                    wd = nk * GC
                    for j in range(nk):
                        k1 = ka + j
                        cs_ = slice(j * GC, (j + 1) * GC)
                        bz, bp, bm = Bm_[0], Bm_[1], Bm_[2]
                        T.op("pe", lambda e: e.matmul(ps[bZr][:, cs_], lhsT=TGr[:, k1, :], rhs=bz[:, k1, :], start=True, stop=False), r=["TGr", "Bm0"], w=[PK[bZr]])
                        T.op("pe", lambda e: e.matmul(ps[bZr][:, cs_], lhsT=TGn[:, k1, :], rhs=bz[:, NK + k1, :], start=False, stop=True), r=["TGn", "Bm0"], w=[PK[bZr]])
                        T.op("pe", lambda e: e.matmul(ps[bZi][:, cs_], lhsT=TGi[:, k1, :], rhs=bz[:, k1, :], start=True, stop=False), r=["TGi", "Bm0"], w=[PK[bZi]])
                        T.op("pe", lambda e: e.matmul(ps[bZi][:, cs_], lhsT=TGr[:, k1, :], rhs=bz[:, NK + k1, :], start=False, stop=True), r=["TGr", "Bm0"], w=[PK[bZi]])
                        T.op("pe", lambda e: e.matmul(ps[bSr][:, cs_], lhsT=TGr[:, k1, :], rhs=bp[:, k1, :], start=True, stop=False), r=["TGr", "Bm1"], w=[PK[bSr]])
                        T.op("pe", lambda e: e.matmul(ps[bSr][:, cs_], lhsT=TGn[:, k1, :], rhs=bp[:, NK + k1, :], start=False, stop=True), r=["TGn", "Bm1"], w=[PK[bSr]])
                        T.op("pe", lambda e: e.matmul(ps[bSi][:, cs_], lhsT=TGi[:, k1, :], rhs=bm[:, k1, :], start=True, stop=False), r=["TGi", "Bm2"], w=[PK[bSi]])
                        T.op("pe", lambda e: e.matmul(ps[bSi][:, cs_], lhsT=TGr[:, k1, :], rhs=bm[:, NK + k1, :], start=False, stop=True), r=["TGr", "Bm2"], w=[PK[bSi]])
                    T.op("act", lambda e: e.copy(out=srs[:, 0:wd], in_=ps[bSr][:, 0:wd]), r=[PK[bSr]], w=["srsC"])
                    T.op("act", lambda e: e.copy(out=sis[:, 0:wd], in_=ps[bSi][:, 0:wd]), r=[PK[bSi]], w=["sisC"])
                    T.op("dve", lambda e: e.tensor_tensor(out=tm[0][:, 0:wd], in0=ps[bZr][:, 0:wd], in1=srs[:, 0:wd], op=ALU.mult), r=[PK[bZr], "srsC"], w=["tm0"])
                    T.op("dve", lambda e: e.tensor_tensor(out=tm[1][:, 0:wd], in0=ps[bZi][:, 0:wd], in1=sis[:, 0:wd], op=ALU.mult), r=[PK[bZi], "sisC"], w=["tm1"])
                    T.op("dve", lambda e: e.tensor_tensor(out=tm[2][:, 0:wd], in0=ps[bZr][:, 0:wd], in1=sis[:, 0:wd], op=ALU.mult), r=[PK[bZr], "sisC"], w=["tm2"])
                    T.op("dve", lambda e: e.tensor_tensor(out=tm[3][:, 0:wd], in0=ps[bZi][:, 0:wd], in1=srs[:, 0:wd], op=ALU.mult), r=[PK[bZi], "srsC"], w=["tm3"])
                    yv_r = Y[:, :, 0, ka:kb_]
                    yv_i = Y[:, :, 1, ka:kb_]
                    T.op("dve", lambda e: e.tensor_tensor(out=yv_r, in0=tm[0][:, 0:wd].rearrange("p (k c) -> p c k", c=GC),
                                                           in1=tm[1][:, 0:wd].rearrange("p (k c) -> p c k", c=GC), op=ALU.subtract),
                         r=["tm0", "tm1"], w=["YC"])
                    T.op("dve", lambda e: e.tensor_tensor(out=yv_i, in0=tm[2][:, 0:wd].rearrange("p (k c) -> p c k", c=GC),
                                                           in1=tm[3][:, 0:wd].rearrange("p (k c) -> p c k", c=GC), op=ALU.add),
                         r=["tm2", "tm3"], w=["YC"])
                for cq in range(GC // 2):
                    pi = cq % 2
                    for c2 in range(2):
                        c = cq * 2 + c2
                        T.op("pe", lambda e: e.matmul(ps[pi][0:NK, c2 * 256:(c2 + 1) * 256], lhsT=Y[:, c, 0, :], rhs=R1[:, :],
                                                      start=True, stop=False), r=["YC", "R1"], w=[PK[pi]])
                        T.op("pe", lambda e: e.matmul(ps[pi][0:NK, c2 * 256:(c2 + 1) * 256], lhsT=Y[:, c, 1, :], rhs=R2[:, :],
                                                      start=False, stop=True), r=["YC", "R2"], w=[PK[pi]])
                    T.op("act" if cq % 2 == 0 else "dve",
                         lambda e: (e.copy if cq % 2 == 0 else e.tensor_copy)(
                             out=Dm[:, :, cq * 2:(cq + 1) * 2],
                             in_=ps[pi][0:NK, :].rearrange("p (c f) -> p f c", c=2)),
                         r=[PK[pi]], w=["DmC"])
                for nq in range(8):
                    pi = 6 + nq % 2
                    for j in range(16):
                        n2 = nq * 16 + j
                        T.op("pe", lambda e: e.matmul(ps[pi][0:32, j * GC:(j + 1) * GC], lhsT=TH[:, n2, 0, :], rhs=Dm[:, n2, :],
                                                      start=True, stop=False), r=["TH", "DmC"], w=[PK[pi]])
                        T.op("pe", lambda e: e.matmul(ps[pi][0:32, j * GC:(j + 1) * GC], lhsT=TH[:, n2, 1, :], rhs=Dm[:, 128 + n2, :],
                                                      start=False, stop=True), r=["TH", "DmC"], w=[PK[pi]])
                    nsl = slice(nq * 16, (nq + 1) * 16)
                    T.op("dve", lambda e: e.tensor_tensor(out=tmpy[:], in0=ps[pi][0:32, :].rearrange("p (n c) -> p c n", c=GC),
                                                          in1=zs[:, :, nsl], op=ALU.add), r=[PK[pi], "zsC"], w=["tmpyC"])
                    T.op("dve", lambda e: e.tensor_tensor(out=zf[:, :, nsl], in0=tmpy[:], in1=gt[:, :, nsl], op=ALU.mult),
                         r=["tmpyC", "gtC", "srcb0", "zsC"], w=["zf"])
            T.op("act", lambda e: e.copy(out=hob[:], in_=zf[:]), r=["zf"], w=["srcb0"])
            T.dma("sp", lambda e: e.dma_start(out=flay(hyoT_d[c0:c0 + GC, :]), in_=hob[:]), r=["srcb0"], w=["hyoT_d"])
        T.barrier()

    if "D" in stages:
      with ExitStack() as es:
        aff_all = sb(es, "aff_all", [128, NT, NE])
        with ExitStack() as es2:
            attnT = sb(es2, "attnT", [128, 4, L], BF16)
            hyoT = sb(es2, "hyoT", [128, 4, L], BF16)
            wpa = sb(es2, "wpa", [128, 4, D], BF16)
            wph = sb(es2, "wph", [128, 4, D], BF16)
            wo = sb(es2, "wo", [128, 8, D], BF16)
            g2bc = sb(es2, "g2bc", [128, D])
            wr = sb(es2, "wr", [128, 8, 16])
            gat2 = [sb(es2, "gatD%d" % i, [128, 16, 512], BF16) for i in range(2)]
            m1 = sb(es2, "m1D", [128, 512])
            m2 = sb(es2, "m2D", [128, 512])
            mT = sb(es2, "mTD", [128, 8, 512], BF16)
            xin = sb(es2, "xinD", [128, D])
            x1 = sb(es2, "x1D", [128, D])
            u2 = sb(es2, "u2D0", [128, D])
            u2b = sb(es2, "u2bD", [128, D], BF16)
            u2T = sb(es2, "u2TD", [128, 8, 128])
            junkd = sb(es2, "junkD", [128, D])
            st = sb(es2, "stD", [128, 8])
            lg = sb(es2, "lgD", [128, NE])
            for (tt_, td_, tk_) in ((attnT, attnT_d, "attnT"), (hyoT, hyoT_d, "hyoT")):
                T.dmas("sp", [(lambda e, j=j, a=a: e.dma_start(out=tt_[:, j, a:a + 2048], in_=td_[j * 128:(j + 1) * 128, a:a + 2048]))
                              for j in range(4) for a in (0, 2048)], w=[tk_])
            T.dma("sp", lambda e: e.dma_start(out=g2bc[:], in_=g2bc_d[:, :]), w=["g2bc"])
            T.dma("sp", lambda e: e.dma_start(out=wr[:], in_=wr_d[:, :, :]), w=["wr"])
            for (wsrc, nj, wdst, wk) in ((wpa_d, 4, wpa, "wpa"), (wph_d, 4, wph, "wph"), (wo_d, 8, wo, "wo")):
                T.dmas("pool", [(lambda e, a=a: e.dma_start(out=wdst[:, a:a + 4, :],
                                                            in_=wsrc.rearrange("(j p) c -> p j c", p=128)[:, a:a + 4, :]))
                                for a in range(0, nj, 4)], w=[wk])
            u2s = [u2, sb(es2, "u2D1", [128, D])]

            def d_tail(i):
                u2t = u2s[i % 2]
                for j in range(8):
                    T.op("pe", lambda e: e.transpose(out=ps[4 + j // 4][:, (j % 4) * 128:(j % 4 + 1) * 128],
                                                     in_=u2t[:, j * 128:(j + 1) * 128], identity=ident_f[:]),
                         r=["u2D%d" % (i % 2), "ident_f"], w=[PK[4 + j // 4]])
                for hh in range(2):
                    T.op("act", lambda e: e.copy(out=u2T[:, hh * 4:(hh + 1) * 4, :],
                                                 in_=ps[4 + hh][:, :].rearrange("p (j t) -> p j t", j=4)),
                         r=[PK[4 + hh]], w=["u2TD"])
                for j in range(8):
                    T.op("pe", lambda e: e.matmul(ps[6][:, 0:NE], lhsT=u2T[:, j, :], rhs=wr[:, j, :], start=(j == 0), stop=(j == 7)),
                         r=["u2TD", "wr"], w=[PK[6]])
                T.op("dve", lambda e: e.tensor_reduce(out=st2[:, 3:4], in_=ps[6][:, 0:NE], axis=AX.X, op=ALU.max), r=[PK[6], "st2D"], w=["st2D"])
                T.op("dve", lambda e: e.tensor_scalar(out=st2[:, 4:5], in0=st2[:, 3:4], scalar1=-1.0, scalar2=None, op0=ALU.mult), r=["st2D"], w=["st2D"])
                T.op("dve", lambda e: e.memset(st2[:, 5:6], 0.0), r=["st2D"], w=["st2D"])
                T.op("act", lambda e: e.activation(out=lg[:], in_=ps[6][:, 0:NE], func=AF.Exp, bias=st2[:, 4:5], accum_out=st2[:, 5:6]),
                     r=[PK[6], "st2D"], w=["lgD", "st2D"])
                T.op("dve", lambda e: e.reciprocal(out=st2[:, 6:7], in_=st2[:, 5:6]), r=["st2D"], w=["st2D"])
                T.op("dve", lambda e: e.tensor_scalar(out=aff_all[:, i, :], in0=lg[:], scalar1=st2[:, 6:7], scalar2=None, op0=ALU.mult),
                     r=["lgD", "st2D"], w=["aff_all"])

            st2 = sb(es2, "st2D", [128, 8])
            prev_i = None
            for tq in range(8):
                tsl = slice(tq * 512, (tq + 1) * 512)
                gat = gat2[tq % 2]
                gk = "gatD%d" % (tq % 2)
                T.dmas("sp", [(lambda e, a=a: e.dma_start(out=gat[:, a:a + 8, :],
                                                          in_=gateT_d.rearrange("(j p) t -> p j t", p=128)[:, a:a + 8, tsl])) for a in (0, 8)],
                       w=[gk])
                for dt in range(8):
                    pa = 0 if dt % 2 == 0 else 7
                    for j in range(4):
                        T.op("pe", lambda e: e.matmul(ps[pa][:, :], lhsT=wpa[:, j, dt * 128:(dt + 1) * 128], rhs=attnT[:, j, tsl],
                                                      start=(j == 0), stop=(j == 3)), r=["wpa", "attnT"], w=[PK[pa]])
                    for j in range(4):
                        T.op("pe", lambda e: e.matmul(ps[1][:, :], lhsT=wph[:, j, dt * 128:(dt + 1) * 128], rhs=hyoT[:, j, tsl],
                                                      start=(j == 0), stop=(j == 3)), r=["wph", "hyoT"], w=[PK[1]])
                    T.op("dve", lambda e: e.tensor_tensor(out=m1[:], in0=ps[pa][:, :], in1=gat[:, dt, :], op=ALU.mult), r=[PK[pa], gk], w=["m1D"])
                    T.op("dve", lambda e: e.tensor_tensor(out=m2[:], in0=ps[1][:, :], in1=gat[:, 8 + dt, :], op=ALU.mult), r=[PK[1], gk], w=["m2D"])
                    T.op("pool", lambda e: e.tensor_tensor(out=mT[:, dt, :], in0=m1[:], in1=m2[:], op=ALU.add), r=["m1D", "m2D"], w=["mTD"])
                for ts in range(4):
                    i = tq * 4 + ts
                    u2t = u2s[i % 2]
                    u2k = "u2D%d" % (i % 2)
                    T.dma("sp", lambda e: e.dma_start(out=xin[:], in_=x_d[i * 128:(i + 1) * 128, :]), w=["xinD"])
                    for half in range(2):
                        for dt in range(8):
                            T.op("pe", lambda e: e.matmul(ps[2 + half][:, :], lhsT=mT[:, dt, ts * 128:(ts + 1) * 128],
                                                          rhs=wo[:, dt, half * 512:(half + 1) * 512], start=(dt == 0), stop=(dt == 7)),
                                 r=["mTD", "wo"], w=[PK[2 + half]])
                        T.op("dve", lambda e: e.tensor_tensor(out=x1[:, half * 512:(half + 1) * 512], in0=ps[2 + half][:, :],
                                                              in1=xin[:, half * 512:(half + 1) * 512], op=ALU.add),
                             r=[PK[2 + half], "xinD"], w=["x1D"])
                    if prev_i is not None:
                        d_tail(prev_i)
                    T.dma("sp", lambda e: e.dma_start(out=out_d[i * 128:(i + 1) * 128, :], in_=x1[:]), r=["x1D"], w=["out_d"])
                    T.op("dve", lambda e: e.memset(st[:, 0:1], 0.0), w=["stD"])
                    T.op("act", lambda e: e.activation(out=junkd[:], in_=x1[:], func=AF.Square, accum_out=st[:, 0:1]),
                         r=["x1D", "stD"], w=["junkD", "stD"])
                    T.op("act", lambda e: e.activation(out=st[:, 1:2], in_=st[:, 0:1], func=AF.Sqrt, scale=1.0 / D, bias=eps[:]),
                         r=["stD", "eps"], w=["stD"])
                    T.op("dve", lambda e: e.reciprocal(out=st[:, 2:3], in_=st[:, 1:2]), r=["stD"], w=["stD"])
                    T.op("dve", lambda e: e.scalar_tensor_tensor(out=u2t[:], in0=x1[:], scalar=st[:, 2:3], in1=g2bc[:],
                                                                 op0=ALU.mult, op1=ALU.mult), r=["x1D", "stD", "g2bc"], w=[u2k])
                    T.op("act", lambda e: e.copy(out=u2b[:], in_=u2t[:]), r=[u2k], w=["u2bD"])
                    T.dma("sp", lambda e: e.dma_start(out=u2_d[i * 128:(i + 1) * 128, :], in_=u2b[:]), r=["u2bD"], w=["u2_d"])
                    prev_i = i
            d_tail(prev_i)
            T.barrier()
        if "aff" in dbg_d:
            T.dma("sp", lambda e: e.dma_start(out=dbg_d["aff"], in_=aff_all[:]), r=["aff_all"], w=["dbgaff"])

        if "E" in stages:
          idx_all = sb(es, "idx_all", [128, NE, 4], I32)
          g_all = sb(es, "g_all", [128, NE, 4])
          with ExitStack() as es2:
            affT = sb(es2, "affT", [NE, L])
            cmpj = sb(es2, "cmpj", [NE, L])
            bs = sb(es2, "bsE", [NE, 8])
            tri = sb(es2, "tri", [128, 128])
            onesf = sb(es2, "onesf", [128, 128])
            iota = sb(es2, "iota", [128, 512])
            tidhl = sb(es2, "tidhl", [128, NT, 2], BF16)
            thbc = sb(es2, "thbc", [128, NE])
            dg = sb(es2, "dgE", [NE, NE])
            M = sb(es2, "ME", [128, NT, NE])
            pos = sb(es2, "posE", [128, NT, NE])
            srun = sb(es2, "srunE", [128, NE])
            cum = sb(es2, "cumE", [128, NE])
            vals = sb(es2, "valsE", [128, NT, NE, 4], BF16)
            afh = sb(es2, "afhE", [128, NT, NE], BF16)
            afr = sb(es2, "afrE", [128, NT, NE])
            oh = [sb(es2, "ohE%d" % i, [128, 512], BF16) for i in range(4)]
            idf = sb(es2, "idfE", [128, 4])
            pvs = sb(es2, "pvsE", [128, 16])
            T.dma("sp", lambda e: e.dma_start(out=tri[:], in_=tri_d[:, :]), w=["tri"])
            T.dma("sp", lambda e: e.dma_start(out=iota[:], in_=iota_d[:, :]), w=["iota"])
            T.dma("sp", lambda e: e.dma_start(out=tidhl[:], in_=tidhl_d[:, :, :]), w=["tidhl"])
            T.op("pool", lambda e: e.memset(onesf[:], 1.0), w=["onesf"])
            for i in range(NT):
                pi = i // 4
                T.op("pe", lambda e: e.transpose(out=ps[pi][0:NE, (i % 4) * 128:(i % 4 + 1) * 128], in_=aff_all[:, i, :],
                                                 identity=ident_f[:]), r=["aff_all", "ident_f"], w=[PK[pi]])
                if i % 4 == 3:
                    T.op("act", lambda e: e.copy(out=affT[:, (i - 3) * 128:(i + 1) * 128], in_=ps[pi][0:NE, :]), r=[PK[pi]], w=["affT"])
            T.op("dve", lambda e: e.memset(bs[:, 0:1], 0.0), w=["bsE"])
            T.op("dve", lambda e: e.memset(bs[:, 1:2], 1.0), r=["bsE"], w=["bsE"])
            accE = sb(es2, "accE", [NE, 32])
            T.op("dve", lambda e: e.memset(accE[:], 0.0), w=["accE"])
            thr_cnt = float(2 * CAP - L - 1)
            for it in range(32):
                w_ = 0.5 ** (it + 1)
                T.op("dve", lambda e: e.tensor_scalar(out=bs[:, 7:8], in0=bs[:, 0:1], scalar1=w_, scalar2=-1.0, op0=ALU.add, op1=ALU.mult),
                     r=["bsE"], w=["bsE"])
                T.op("act", lambda e: e.activation(out=cmpj[:], in_=affT[:], func=AF.Sign, bias=bs[:, 7:8], accum_out=accE[:, it:it + 1]),
                     r=["affT", "bsE", "accE"], w=["cmpj", "accE"])
                T.op("dve", lambda e: e.tensor_scalar(out=bs[:, 4:5], in0=accE[:, it:it + 1], scalar1=thr_cnt, scalar2=w_, op0=ALU.is_ge, op1=ALU.mult),
                     r=["accE", "bsE"], w=["bsE"])
                T.op("dve", lambda e: e.tensor_tensor(out=bs[:, 0:1], in0=bs[:, 0:1], in1=bs[:, 4:5], op=ALU.add), r=["bsE"], w=["bsE"])
            T.op("dve", lambda e: e.tensor_scalar(out=dg[:], in0=ident_f[0:NE, 0:NE], scalar1=bs[:, 0:1], scalar2=None, op0=ALU.mult), r=["bsE", "ident_f"], w=["dgE"])
            T.op("pe", lambda e: e.matmul(ps[0][:, 0:NE], lhsT=onesf[0:NE, :], rhs=dg[:, :], start=True, stop=True), r=["onesf", "dgE"], w=[PK[0]])
            T.op("dve", lambda e: e.tensor_copy(out=thbc[:], in_=ps[0][:, 0:NE]), r=[PK[0]], w=["thbc"])
            T.op("dve", lambda e: e.tensor_tensor(out=M[:], in0=aff_all[:], in1=thbc[:, :].unsqueeze(1).broadcast_to([128, NT, NE]), op=ALU.is_ge),
                 r=["aff_all", "thbc"], w=["ME"])
            T.op("dve", lambda e: e.memset(srun[:], 0.0), w=["srunE"])
            for i in range(NT):
                pi = 1 + i % 2
                T.op("pe", lambda e: e.matmul(ps[pi][:, 0:NE], lhsT=tri[:, :], rhs=M[:, i, :], start=True, stop=True), r=["tri", "ME"], w=[PK[pi]])
                T.op("pe", lambda e: e.matmul(ps[pi][:, NE:2 * NE], lhsT=onesf[:, :], rhs=M[:, i, :], start=True, stop=True), r=["onesf", "ME"], w=[PK[pi]])
                T.op("dve", lambda e: e.tensor_tensor(out=cum[:], in0=ps[pi][:, 0:NE], in1=srun[:], op=ALU.add), r=[PK[pi], "srunE"], w=["cumE"])
                T.op("dve", lambda e: e.tensor_tensor(out=srun[:], in0=ps[pi][:, NE:2 * NE], in1=srun[:], op=ALU.add), r=[PK[pi], "srunE", "cumE"], w=["srunE"])
                T.op("dve", lambda e: e.tensor_tensor(out=cum[:], in0=cum[:], in1=M[:, i, :], op=ALU.mult), r=["cumE", "ME"], w=["cumE"])
                T.op("dve", lambda e: e.tensor_scalar(out=pos[:, i, :], in0=cum[:], scalar1=-1.0, scalar2=None, op0=ALU.add), r=["cumE"], w=["posE"])
            T.op("dve", lambda e: e.tensor_copy(out=afh[:], in_=aff_all[:]), r=["aff_all"], w=["afhE"])
            T.op("dve", lambda e: e.tensor_tensor(out=afr[:], in0=aff_all[:], in1=afh[:], op=ALU.subtract), r=["aff_all", "afhE"], w=["afrE"])
            T.op("dve", lambda e: e.tensor_copy(out=vals[:, :, :, 2], in_=afh[:]), r=["afhE"], w=["valsE"])
            T.op("dve", lambda e: e.tensor_copy(out=vals[:, :, :, 3], in_=afr[:]), r=["afrE", "valsE"], w=["valsE"])
            T.op("dve", lambda e: e.tensor_copy(out=vals[:, :, :, 0:2], in_=tidhl[:, :, :].unsqueeze(2).broadcast_to([128, NT, NE, 2])),
                 r=["tidhl", "valsE"], w=["valsE"])
            oc = 0
            for ex in range(NE):
                pi = 3 + ex % 2
                for i in range(NT):
                    ob = oc % 4
                    eng = "dve"
                    oc += 1
                    T.op(eng, lambda e: e.tensor_scalar(out=oh[ob][:], in0=iota[:], scalar1=pos[:, i, ex:ex + 1], scalar2=None, op0=ALU.is_equal),
                         r=["iota", "posE"], w=["ohE%d" % ob])
                    for sc in range(4):
                        T.op("pe", lambda e: e.matmul(ps[pi][:, sc * 4:(sc + 1) * 4], lhsT=oh[ob][:, sc * 128:(sc + 1) * 128], rhs=vals[:, i, ex, :],
                                                      start=(i == 0 and sc == 0), stop=(i == NT - 1), skip_group_check=True),
                             r=["ohE%d" % ob, "valsE"], w=[PK[pi]])
                T.op("dve", lambda e: e.tensor_copy(out=pvs[:], in_=ps[pi][:, 0:16]), r=[PK[pi]], w=["pvsE"])
                pv = pvs[:, :].rearrange("p (s f) -> p s f", f=4)
                T.op("dve", lambda e: e.scalar_tensor_tensor(out=idf[:], in0=pv[:, :, 0], scalar=64.0, in1=pv[:, :, 1], op0=ALU.mult, op1=ALU.add),
                     r=["pvsE"], w=["idfE"])
                T.op("dve", lambda e: e.tensor_copy(out=idx_all[:, ex, :], in_=idf[:]), r=["idfE"], w=["idx_all"])
                T.op("dve", lambda e: e.tensor_tensor(out=g_all[:, ex, :], in0=pv[:, :, 2], in1=pv[:, :, 3], op=ALU.add), r=["pvsE"], w=["g_all"])
            T.barrier()
          if "idx" in dbg_d:
            T.dma("sp", lambda e: e.dma_start(out=dbg_d["idx"], in_=idx_all[:]), r=["idx_all"], w=["dbgidx"])
            T.dma("sp", lambda e: e.dma_start(out=dbg_d["g"], in_=g_all[:]), r=["g_all"], w=["dbgg"])

          if "F" in stages:
            wgb = [sb(es, "wgb%d" % i, [128, 8, D], BF16) for i in range(2)]
            wub = [sb(es, "wub%d" % i, [128, 8, D], BF16) for i in range(2)]
            wdb = [sb(es, "wdb%d" % i, [128, 8, D], BF16) for i in range(2)]
            xg = [sb(es, "xgF%d" % i, [128, D], BF16) for i in range(4)]
            xinT = sb(es, "xinT", [128, 8, 512], BF16)
            sg = sb(es, "sgF", [128, 512])
            hT = sb(es, "hTF", [128, 8, 512], BF16)
            eo = [sb(es, "eoF%d" % i, [128, D]) for i in range(2)]
            psb = [ps[6][:, :].bitcast(BF16), ps[7][:, :].bitcast(BF16)]

            def load_w(ex):
                for (wsrc, wdst, wk) in ((wg_d, wgb, "wgb"), (wu_d, wub, "wub"), (wd_d, wdb, "wdb")):
                    dst = wdst[ex % 2]
                    T.dmas("pool", [(lambda e, hh=hh: e.dma_start(out=dst[:, hh * 4:(hh + 1) * 4, :],
                                                                  in_=wsrc[ex].rearrange("(j p) c -> p j c", p=128)[:, hh * 4:(hh + 1) * 4, :]))
                                    for hh in range(2)], w=["%s%d" % (wk, ex % 2)])

            def gather(ex):
                for sc in range(4):
                    T.dma("pool", lambda e: e.indirect_dma_start(out=xg[sc][:], out_offset=None, in_=u2_d[:, :],
                                                                 in_offset=bass.IndirectOffsetOnAxis(ap=idx_all[:, ex, sc:sc + 1], axis=0)),
                          r=["idx_all", "u2_d"], w=["xgF%d" % sc])

            load_w(0)
            gather(0)
            for ex in range(NE):
                wgk, wuk, wdk = "wgb%d" % (ex % 2), "wub%d" % (ex % 2), "wdb%d" % (ex % 2)
                wgt, wut, wdt = wgb[ex % 2], wub[ex % 2], wdb[ex % 2]
                if ex + 1 < NE:
                    load_w(ex + 1)
                for sc in range(4):
                    for j in range(8):
                        T.op("pe", lambda e: e.transpose(out=psb[sc % 2][:, j * 128:(j + 1) * 128], in_=xg[sc][:, j * 128:(j + 1) * 128], identity=ident_b[:]),
                             r=["xgF%d" % sc, "ident_b"], w=[PK[6 + sc % 2]])
                    T.op("act", lambda e: e.copy(out=xinT[:, :, sc * 128:(sc + 1) * 128], in_=psb[sc % 2].rearrange("p (j t) -> p j t", j=8)),
                         r=[PK[6 + sc % 2]], w=["xinT"])
                if ex + 1 < NE:
                    gather(ex + 1)
                for ft in range(8):
                    pg, pu = (ft % 2) * 2, (ft % 2) * 2 + 1
                    for j in range(8):
                        T.op("pe", lambda e: e.matmul(ps[pg][:, :], lhsT=wgt[:, j, ft * 128:(ft + 1) * 128], rhs=xinT[:, j, :], start=(j == 0), stop=(j == 7)),
                             r=[wgk, "xinT"], w=[PK[pg]])
                    for j in range(8):
                        T.op("pe", lambda e: e.matmul(ps[pu][:, :], lhsT=wut[:, j, ft * 128:(ft + 1) * 128], rhs=xinT[:, j, :], start=(j == 0), stop=(j == 7)),
                             r=[wuk, "xinT"], w=[PK[pu]])
                    T.op("act", lambda e: e.activation(out=sg[:], in_=ps[pg][:, :], func=AF.Silu), r=[PK[pg]], w=["sgF"])
                    T.op("dve", lambda e: e.tensor_tensor(out=hT[:, ft, :], in0=ps[pu][:, :], in1=sg[:], op=ALU.mult), r=[PK[pu], "sgF"], w=["hTF"])
                for sc in range(4):
                    eb = eo[sc % 2]
                    for half in range(2):
                        po = 4 + half
                        for ft in range(8):
                            T.op("pe", lambda e: e.matmul(ps[po][:, :], lhsT=hT[:, ft, sc * 128:(sc + 1) * 128], rhs=wdt[:, ft, half * 512:(half + 1) * 512],
                                                          start=(ft == 0), stop=(ft == 7)), r=["hTF", wdk], w=[PK[po]])
                        T.op("dve", lambda e: e.tensor_scalar(out=eb[:, half * 512:(half + 1) * 512], in0=ps[po][:, :], scalar1=g_all[:, ex, sc:sc + 1],
                                                              scalar2=None, op0=ALU.mult), r=[PK[po], "g_all"], w=["eoF%d" % (sc % 2)])
                    T.dma("pool", lambda e: e.indirect_dma_start(out=out_d[:, :], out_offset=bass.IndirectOffsetOnAxis(ap=idx_all[:, ex, sc:sc + 1], axis=0),
                                                                 in_=eb[:], in_offset=None, compute_op=ALU.add),
                          r=["eoF%d" % (sc % 2), "idx_all"], w=["out_d"])
            T.barrier()
    for k, v in dbg_d.items():
        pass
    T.barrier()
    top.close()
    return nc


_CONST = None


def make_inputs(inp, b):
    global _CONST
    if _CONST is None:
        _CONST = host_consts()
    f = lambda a: np.ascontiguousarray(np.asarray(a, dtype=np.float32))
    m = dict(_CONST)
    m["x"] = f(inp["x"][b])
    m["g1bc"] = f(np.broadcast_to(inp["norm1_g"][0][None, :], (128, D)))
    m["w_in"] = f(inp["w_in"][0])
    scw = np.concatenate([inp["short_conv_w"][0], inp["short_conv_b"][0][None, :]], axis=0)
    m["scw"] = f(scw.reshape(4, 12, 128).transpose(2, 1, 0))
    qkg = np.stack([inp["q_norm_g"][0], inp["k_norm_g"][0]], axis=0)
    m["qkg"] = f(np.broadcast_to(qkg[None], (128, 2, 64)))
    m["lamv"] = f(np.stack([inp["lambda_q1"][0], inp["lambda_k1"][0], inp["lambda_q2"][0], inp["lambda_k2"][0]])[None])
    m["subg"] = f(np.broadcast_to(inp["subln_g"][0][None, :], (128, 128)))
    m["fw1"] = f(inp["filt_w1"][0])
    m["fw2"] = f(inp["filt_w2"][0])
    m["fw3"] = f(inp["filt_w3"][0])
    m["fbf"] = f(np.stack([inp["filt_b1"][0], inp["filt_b2"][0], inp["filt_b3"][0], inp["filt_freq"][0]], axis=1))
    m["fwo"] = f(inp["filt_w_out"][0])
    m["skipbc"] = f(np.broadcast_to(inp["hyena_skip"][0][None], (32, 2, 512)))
    m["wpa"] = f(inp["w_branch_attn"][0])
    m["wph"] = f(inp["w_branch_hyena"][0])
    m["wo"] = f(inp["w_out"][0])
    m["g2bc"] = f(np.broadcast_to(inp["norm2_g"][0][None, :], (128, D)))
    m["wr"] = f(inp["w_router"][0].reshape(8, 128, 16).transpose(1, 0, 2))
    m["wg"] = f(inp["w_gate"][0])
    m["wu"] = f(inp["w_up"][0])
    m["wd"] = f(inp["w_down"][0])
    return m


def kernel(**inputs):
    nc = build()
    maps = [make_inputs(inputs, c % 4) for c in range(4)]
    in_maps = [maps[c % 4] for c in range(NCORES)]
    res = run_bass_kernel_spmd(nc, in_maps, core_ids=list(range(NCORES)))
    out = np.stack([np.asarray(res.results[b]["out"], dtype=np.float32) for b in range(4)], axis=0)
    return out
```
